# Optimizing a Trainium2 kernel written in Bass

```python
import math
import jax
import jax.numpy as jnp
from jax import lax
import numpy as np


D_MODEL = 1024
BATCH = 2
SEQ = 8192
DEPTH = 1

GRID_W = 64
CTX_LEN = 256
EPS = 1e-6
NEG_INF = -1e30

D_A = 512
SSM_CPG = 16
SSM_GROUPS = D_A // SSM_CPG
SSM_STATE = 64
DT_MIN = 1e-3
DT_MAX = 1e-1

NA_HEADS = 8
NA_HEAD_DIM = 64
D_B = NA_HEADS * NA_HEAD_DIM
WIN_R = 8
WIN_C = 16
Q_BLOCK = 16
K_BLOCK_W = Q_BLOCK + WIN_C
N_COL_BLOCKS = GRID_W // Q_BLOCK

IN_COLS = D_A + 3 * D_B + 2 * D_MODEL
SPLIT_POINTS = (D_A, D_A + D_B, D_A + 2 * D_B, D_A + 3 * D_B, D_A + 3 * D_B + D_MODEL)

N_EXPERTS = 16
CAPACITY_FACTOR = 2
D_EXPERT = 1024

kernel_name = "hybrid_s5_natten_ec_moe_dit_block"


def rmsnorm(x, g):
    xf = x.astype(jnp.float32)
    y = xf * lax.rsqrt(jnp.mean(xf * xf, axis=-1, keepdims=True) + EPS)
    return (y * g.astype(jnp.float32)).astype(x.dtype)


def modulate(h, shift, scale):
    return h * (1 + scale[:, None, :]) + shift[:, None, :]


def s5_discretize(lam_re, lam_im, log_dt, b_re, b_im):
    lam = lax.complex(lam_re.astype(jnp.float32), lam_im.astype(jnp.float32))
    dt = jnp.exp(log_dt.astype(jnp.float32))[:, None]
    lam_bar = jnp.exp(lam * dt)
    b = lax.complex(b_re.astype(jnp.float32), b_im.astype(jnp.float32))
    b_bar = ((lam_bar - 1.0) / lam)[..., None] * b
    return lam_bar, b_bar


def s5_drive(u, b_bar):
    re = jnp.einsum('bngc,gpc->bngp', u, jnp.real(b_bar))
    im = jnp.einsum('bngc,gpc->bngp', u, jnp.imag(b_bar))
    return lax.complex(re, im)


def s5_readout(xs, c_re, c_im):
    return (jnp.einsum('bngp,gcp->bngc', jnp.real(xs), c_re)
            - jnp.einsum('bngp,gcp->bngc', jnp.imag(xs), c_im))


def _linear_recurrence(e1, e2):
    a1, b1 = e1
    a2, b2 = e2
    return a1 * a2, a2 * b1 + b2


def s5_scan(lam_bar, bu, h0, reverse):
    if h0 is not None:
        first = -1 if reverse else 0
        bu = bu.at[:, first].add(lam_bar * h0)
    a = jnp.broadcast_to(lam_bar, bu.shape)
    _, xs = lax.associative_scan(_linear_recurrence, (a, bu), axis=1, reverse=reverse)
    return xs


def s5_glu(y, w_glu, b_glu):
    a = jax.nn.gelu(y)
    return a * jax.nn.sigmoid(a @ w_glu.astype(jnp.float32) + b_glu.astype(jnp.float32))


def s5_branch(u, u_ctx, lam_re, lam_im, log_dt, b_re, b_im, c_re, c_im, d_skip, w_glu, b_glu, update_ctx):
    bsz, n_lat, _ = u.shape
    n_ctx = u_ctx.shape[1]
    u32 = u.astype(jnp.float32).reshape(bsz, n_lat, SSM_GROUPS, SSM_CPG)
    uc32 = u_ctx.astype(jnp.float32).reshape(bsz, n_ctx, SSM_GROUPS, SSM_CPG)
    d32 = d_skip.astype(jnp.float32).reshape(SSM_GROUPS, SSM_CPG)
    y = u32 * d32
    yc = uc32 * d32 if update_ctx else None
    for direction, reverse in ((0, False), (1, True)):
        lam_bar, b_bar = s5_discretize(lam_re[direction], lam_im[direction], log_dt[direction],
                                       b_re[direction], b_im[direction])
        c_r = c_re[direction].astype(jnp.float32)
        c_i = c_im[direction].astype(jnp.float32)
        xs_ctx = s5_scan(lam_bar, s5_drive(uc32, b_bar), None, reverse)
        h_end = xs_ctx[:, 0] if reverse else xs_ctx[:, -1]
        xs_lat = s5_scan(lam_bar, s5_drive(u32, b_bar), h_end, reverse)
        y = y + s5_readout(xs_lat, c_r, c_i)
        if update_ctx:
            yc = yc + s5_readout(xs_ctx, c_r, c_i)
    out = s5_glu(y.reshape(bsz, n_lat, D_A), w_glu, b_glu).astype(u.dtype)
    out_ctx = s5_glu(yc.reshape(bsz, n_ctx, D_A), w_glu, b_glu).astype(u.dtype) if update_ctx else None
    return out, out_ctx


def na_branch(q, k, v, q_ctx, k_ctx, v_ctx, rpb, update_ctx):
    bsz, n_lat, n_heads, head_dim = q.shape
    rows = n_lat // GRID_W
    win_r = min(WIN_R, rows)
    n_nb = win_r * K_BLOCK_W
    r = jnp.arange(rows)
    row_idx = jnp.clip(r - WIN_R // 2, 0, rows - win_r)[:, None] + jnp.arange(win_r)[None, :]
    blk = jnp.arange(N_COL_BLOCKS)
    col_idx = (jnp.clip(blk * Q_BLOCK - WIN_C // 2, 0, GRID_W - K_BLOCK_W)[:, None]
               + jnp.arange(K_BLOCK_W)[None, :])
    key_row = jnp.repeat(row_idx, K_BLOCK_W, axis=1)
    key_col = jnp.tile(col_idx, (1, win_r))
    tok_idx = key_row[:, None, :] * GRID_W + key_col[None, :, :]
    k_nb = k[:, tok_idx]
    v_nb = v[:, tok_idx]
    q_blk = q.reshape(bsz, rows, N_COL_BLOCKS, Q_BLOCK, n_heads, head_dim)
    s_nb = jnp.einsum('brjqhd,brjkhd->bhrjqk', q_blk, k_nb).astype(jnp.float32)
    q_col = blk[:, None] * Q_BLOCK + jnp.arange(Q_BLOCK)[None, :]
    q_cs = jnp.clip(q_col - WIN_C // 2, 0, GRID_W - WIN_C)
    in_win = ((key_col[:, None, :] >= q_cs[:, :, None])
              & (key_col[:, None, :] < q_cs[:, :, None] + WIN_C))
    d_col = jnp.clip(key_col[:, None, :] - q_col[:, :, None] + WIN_C - 1, 0, 2 * WIN_C - 2)
    d_row = key_row - r[:, None] + WIN_R - 1
    bias = rpb[:, d_row[:, None, None, :], d_col[None]].astype(jnp.float32)
    s_nb = jnp.where(in_win, s_nb + bias, NEG_INF)
    s_ctx = jnp.einsum('bqhd,bkhd->bhqk', q, k_ctx).astype(jnp.float32)
    s_ctx = s_ctx.reshape(bsz, n_heads, rows, N_COL_BLOCKS, Q_BLOCK, -1)
    p = jax.nn.softmax(jnp.concatenate([s_nb, s_ctx], axis=-1), axis=-1).astype(v.dtype)
    o = (jnp.einsum('bhrjqk,brjkhd->brjqhd', p[..., :n_nb], v_nb)
         + jnp.einsum('bhrjqk,bkhd->brjqhd', p[..., n_nb:], v_ctx))
    out = o.reshape(bsz, n_lat, n_heads * head_dim)
    out_ctx = None
    if update_ctx:
        sc = jnp.einsum('bqhd,bkhd->bhqk', q_ctx, k_ctx).astype(jnp.float32)
        pc = jax.nn.softmax(sc, axis=-1).astype(v_ctx.dtype)
        out_ctx = jnp.einsum('bhqk,bkhd->bqhd', pc, v_ctx).reshape(bsz, q_ctx.shape[1], n_heads * head_dim)
    return out, out_ctx


def token_mixer(h, hc, w_in, ssm_lam_re, ssm_lam_im, ssm_log_dt, ssm_b_re, ssm_b_im, ssm_c_re, ssm_c_im,
                ssm_d, w_glu, b_glu, q_norm, k_norm, na_rpb, w_proj_a, w_proj_b, w_out, update_ctx):
    def project(t):
        z = t @ w_in
        u, q, k, v, ga, gb = jnp.split(z, list(SPLIT_POINTS), axis=-1)
        shape = t.shape[:2] + (NA_HEADS, NA_HEAD_DIM)
        q = rmsnorm(q.reshape(shape), q_norm) * (NA_HEAD_DIM ** -0.5)
        k = rmsnorm(k.reshape(shape), k_norm)
        return u, q, k, v.reshape(shape), ga, gb

    def merge(ya, yb, ga, gb):
        return (jax.nn.sigmoid(ga) * (ya @ w_proj_a) + jax.nn.sigmoid(gb) * (yb @ w_proj_b)) @ w_out

    u, q, k, v, ga, gb = project(h)
    uc, qc, kc, vc, gac, gbc = project(hc)
    ya, yac = s5_branch(u, uc, ssm_lam_re, ssm_lam_im, ssm_log_dt, ssm_b_re, ssm_b_im, ssm_c_re, ssm_c_im,
                        ssm_d, w_glu, b_glu, update_ctx)
    yb, ybc = na_branch(q, k, v, qc, kc, vc, na_rpb, update_ctx)
    y = merge(ya, yb, ga, gb)
    yc = merge(yac, ybc, gac, gbc) if update_ctx else None
    return y, yc


def ec_moe_set(h, w_router, w_e_gate, w_e_up, w_e_down):
    n_tok = h.shape[0]
    cap = CAPACITY_FACTOR * n_tok // N_EXPERTS
    aff = jax.nn.softmax((h @ w_router).astype(jnp.float32), axis=-1)
    g, idx = lax.top_k(aff.T, cap)
    xe = h[idx]
    hid = jax.nn.silu(jnp.einsum('ecd,edf->ecf', xe, w_e_gate)) * jnp.einsum('ecd,edf->ecf', xe, w_e_up)
    ye = jnp.einsum('ecf,efd->ecd', hid, w_e_down) * g[..., None].astype(h.dtype)
    return jnp.zeros_like(h).at[idx.reshape(-1)].add(ye.reshape(-1, h.shape[-1]))


def ec_moe(h, w_router, w_e_gate, w_e_up, w_e_down):
    return jax.vmap(ec_moe_set, in_axes=(0, None, None, None, None))(h, w_router, w_e_gate, w_e_up, w_e_down)


def block(x, ctx_s, mod, mod_ctx, norm_mix, norm_ffn, w_in, ssm_lam_re, ssm_lam_im, ssm_log_dt, ssm_b_re,
          ssm_b_im, ssm_c_re, ssm_c_im, ssm_d, w_glu, b_glu, q_norm, k_norm, na_rpb, w_proj_a, w_proj_b,
          w_out, w_router, w_e_gate, w_e_up, w_e_down, update_ctx):
    sh1, sc1, g1, sh2, sc2, g2 = jnp.split(mod, 6, axis=-1)
    csh1, csc1, cg1, csh2, csc2, cg2 = jnp.split(mod_ctx, 6, axis=-1)
    h = modulate(rmsnorm(x, norm_mix), sh1, sc1)
    hc = modulate(rmsnorm(ctx_s, norm_mix), csh1, csc1)
    y, yc = token_mixer(h, hc, w_in, ssm_lam_re, ssm_lam_im, ssm_log_dt, ssm_b_re, ssm_b_im, ssm_c_re,
                        ssm_c_im, ssm_d, w_glu, b_glu, q_norm, k_norm, na_rpb, w_proj_a, w_proj_b, w_out,
                        update_ctx)
    x = x + g1[:, None, :] * y
    h2 = modulate(rmsnorm(x, norm_ffn), sh2, sc2)
    x = x + g2[:, None, :] * ec_moe(h2, w_router, w_e_gate, w_e_up, w_e_down)
    if update_ctx:
        ctx_s = ctx_s + cg1[:, None, :] * yc
        hc2 = modulate(rmsnorm(ctx_s, norm_ffn), csh2, csc2)
        ctx_s = ctx_s + cg2[:, None, :] * ec_moe(hc2, w_router, w_e_gate, w_e_up, w_e_down)
    return x, ctx_s


def setup_inputs(seed: int = 0) -> dict:
    key = jax.random.key(seed)
    ks = jax.random.split(key, 32)
    f32 = jnp.float32

    def nrm(k, shape, s):
        return jax.random.normal(k, shape, f32) * s

    two = 2
    lam_im_base = jnp.pi * jnp.arange(SSM_STATE, dtype=f32)
    return {
        'x': nrm(ks[0], (BATCH, SEQ, D_MODEL), 1.0),
        'c': nrm(ks[1], (BATCH, D_MODEL), 1.0),
        'ctx': nrm(ks[2], (BATCH, CTX_LEN, D_MODEL), 1.0),
        'c_ctx': nrm(ks[3], (D_MODEL,), 1.0),
        'w_ada': nrm(ks[4], (DEPTH, D_MODEL, 6 * D_MODEL), 0.5 * D_MODEL ** -0.5),
        'b_ada': nrm(ks[5], (DEPTH, 6 * D_MODEL), 0.01),
        'norm_mix': 1.0 + nrm(ks[6], (DEPTH, D_MODEL), 0.01),
        'norm_ffn': 1.0 + nrm(ks[7], (DEPTH, D_MODEL), 0.01),
        'w_in': nrm(ks[8], (DEPTH, D_MODEL, IN_COLS), D_MODEL ** -0.5),
        'ssm_lam_re': -0.5 + nrm(ks[9], (DEPTH, two, SSM_GROUPS, SSM_STATE), 0.01),
        'ssm_lam_im': lam_im_base + nrm(ks[10], (DEPTH, two, SSM_GROUPS, SSM_STATE), 0.01),
        'ssm_log_dt': jax.random.uniform(ks[11], (DEPTH, two, SSM_GROUPS), f32,
                                         minval=math.log(DT_MIN), maxval=math.log(DT_MAX)),
        'ssm_b_re': nrm(ks[12], (DEPTH, two, SSM_GROUPS, SSM_STATE, SSM_CPG), (2 * SSM_CPG) ** -0.5),
        'ssm_b_im': nrm(ks[13], (DEPTH, two, SSM_GROUPS, SSM_STATE, SSM_CPG), (2 * SSM_CPG) ** -0.5),
        'ssm_c_re': nrm(ks[14], (DEPTH, two, SSM_GROUPS, SSM_CPG, SSM_STATE), (2 * SSM_STATE) ** -0.5),
        'ssm_c_im': nrm(ks[15], (DEPTH, two, SSM_GROUPS, SSM_CPG, SSM_STATE), (2 * SSM_STATE) ** -0.5),
        'ssm_d': nrm(ks[16], (DEPTH, D_A), 1.0),
        'w_glu': nrm(ks[17], (DEPTH, D_A, D_A), D_A ** -0.5),
        'b_glu': nrm(ks[18], (DEPTH, D_A), 0.01),
        'q_norm': 1.0 + nrm(ks[19], (DEPTH, NA_HEAD_DIM), 0.01),
        'k_norm': 1.0 + nrm(ks[20], (DEPTH, NA_HEAD_DIM), 0.01),
        'na_rpb': nrm(ks[21], (DEPTH, NA_HEADS, 2 * WIN_R - 1, 2 * WIN_C - 1), 0.02),
        'w_proj_a': nrm(ks[22], (DEPTH, D_A, D_MODEL), D_A ** -0.5),
        'w_proj_b': nrm(ks[23], (DEPTH, D_B, D_MODEL), D_B ** -0.5),
        'w_out': nrm(ks[24], (DEPTH, D_MODEL, D_MODEL), D_MODEL ** -0.5),
        'w_router': nrm(ks[25], (DEPTH, D_MODEL, N_EXPERTS), D_MODEL ** -0.5),
        'w_e_gate': nrm(ks[26], (DEPTH, N_EXPERTS, D_MODEL, D_EXPERT), D_MODEL ** -0.5),
        'w_e_up': nrm(ks[27], (DEPTH, N_EXPERTS, D_MODEL, D_EXPERT), D_MODEL ** -0.5),
        'w_e_down': nrm(ks[28], (DEPTH, N_EXPERTS, D_EXPERT, D_MODEL), D_EXPERT ** -0.5),
    }


def reference(x, c, ctx, c_ctx, w_ada, b_ada, norm_mix, norm_ffn, w_in, ssm_lam_re, ssm_lam_im, ssm_log_dt,
              ssm_b_re, ssm_b_im, ssm_c_re, ssm_c_im, ssm_d, w_glu, b_glu, q_norm, k_norm, na_rpb, w_proj_a,
              w_proj_b, w_out, w_router, w_e_gate, w_e_up, w_e_down):
    ctx_s = ctx
    for layer in range(DEPTH):
        update_ctx = layer < DEPTH - 1
        mod = jax.nn.silu(c) @ w_ada[layer] + b_ada[layer]
        mod_ctx = jnp.broadcast_to(jax.nn.silu(c_ctx) @ w_ada[layer] + b_ada[layer], mod.shape)
        x, ctx_s = block(x, ctx_s, mod, mod_ctx, norm_mix[layer], norm_ffn[layer], w_in[layer],
                         ssm_lam_re[layer], ssm_lam_im[layer], ssm_log_dt[layer], ssm_b_re[layer],
                         ssm_b_im[layer], ssm_c_re[layer], ssm_c_im[layer], ssm_d[layer], w_glu[layer],
                         b_glu[layer], q_norm[layer], k_norm[layer], na_rpb[layer], w_proj_a[layer],
                         w_proj_b[layer], w_out[layer], w_router[layer], w_e_gate[layer], w_e_up[layer],
                         w_e_down[layer], update_ctx)
    return x
```

```python
import numpy as np
from contextlib import ExitStack
import concourse.bass as bass
import concourse.mybir as mybir
from concourse.bass_utils import run_bass_kernel_spmd

F32 = mybir.dt.float32
BF16 = mybir.dt.bfloat16
I32 = mybir.dt.int32
U32 = mybir.dt.uint32
ALU = mybir.AluOpType
AF = mybir.ActivationFunctionType
AX = mybir.AxisListType
STRICT = True


class Sched:
    def __init__(self, nc, es, n_dma_sems=24):
        self.nc = nc
        self.e = {'pe': nc.tensor, 'act': nc.scalar, 'dve': nc.vector, 'pool': nc.gpsimd, 'sp': nc.sync}
        self.sems = {}
        self.cnt = {}
        for k in self.e:
            self.sems[k] = es.enter_context(nc.semaphore("sem_" + k))
            self.cnt[k] = 0
        self.nd = n_dma_sems
        self.qsems = {}
        for q in ('sp', 'pool', 'act'):
            n = n_dma_sems if q != 'act' else 16
            self.qsems[q] = []
            for i in range(n):
                sk = 'd_%s%d' % (q, i)
                self.sems[sk] = es.enter_context(nc.semaphore("dsem_%s%d" % (q, i)))
                self.cnt[sk] = 0
                self.qsems[q].append(sk)
        self.qrr = {'sp': 0, 'pool': 0, 'act': 0}
        self.drr = 0
        self.waited = {}
        self.lastw = {}
        self.readers = {}
        self.nwaits = 0
        self.nins = 0

    def _wait(self, eng, tok):
        sk, val, src = tok
        if self.waited.get((eng, sk), 0) >= val:
            return
        self.e[eng].wait_ge(self.sems[sk], val)
        self.waited[(eng, sk)] = val
        self.nwaits += 1

    def _deps(self, eng, reads, writes):
        for k in reads:
            t = self.lastw.get(k)
            if t is not None:
                self._wait(eng, t)
        for k in writes:
            t = self.lastw.get(k)
            if t is not None and (t[2] != eng or (STRICT and eng != 'pe')):
                self._wait(eng, t)
            for sk, (val, src) in self.readers.get(k, {}).items():
                if src != eng or (STRICT and eng != 'pe'):
                    self._wait(eng, (sk, val, src))

    def _record(self, tok, reads, writes):
        sk, val, src = tok
        for k in reads:
            self.readers.setdefault(k, {})[sk] = (val, src)
        for k in writes:
            self.lastw[k] = tok
            self.readers[k] = {}

    def op(self, eng, fn, reads=(), writes=(), sig=True):
        self._deps(eng, reads, writes)
        ins = fn(self.e[eng])
        if sig:
            self.cnt[eng] += 1
            ins.then_inc(self.sems[eng], 1)
            tok = (eng, self.cnt[eng], eng)
        else:
            tok = (eng, self.cnt[eng] + 1, eng)
        self._record(tok, reads, writes)
        self.nins += 1
        return tok

    def _dma_common(self, q, fn, reads, writes):
        self._deps(q, reads, writes)
        i = self.qrr[q]
        self.qrr[q] = (i + 1) % len(self.qsems[q])
        sk = self.qsems[q][i]
        if self.cnt[sk] > 0:
            self._wait(q, (sk, self.cnt[sk], None))
        ins = fn(self.e[q])
        self.cnt[sk] += 16
        ins.then_inc(self.sems[sk], 16)
        tok = (sk, self.cnt[sk], None)
        self._record(tok, reads, writes)
        self.nins += 1
        return tok

    def dma(self, q, out, in_, reads=(), writes=(), **kw):
        return self._dma_common(q, lambda e: e.dma_start(out=out, in_=in_, **kw), reads, writes)

    def custom_dma(self, q, fn, reads=(), writes=()):
        return self._dma_common(q, fn, reads, writes)

    def barrier(self, engines=None):
        for e in (engines or self.e):
            for sk, c in self.cnt.items():
                if c > 0:
                    self._wait(e, (sk, c, None))
        self.lastw = {}
        self.readers = {}

PI = float(np.pi)


def kn(t):
    n = t.name
    return n[2:] if n.startswith('s_') else n


def s5_phase(nc, S, es, sb, ps, din, dscr, dout, dbg, dbg_out, identf, identb, uT_d, yaT_d, nbanks=16):
    NEXT, NE2 = 8448, 8704
    lre_in = din("lamre_l", [128, 32]); lim_in = din("lamim_l", [128, 32]); ldt_in = din("logdt_l", [128, 32])
    Bre_in = din("Bre_c", [128, 32, 16]); Bim_in = din("Bim_c", [128, 32, 16])
    Cre_in = din("Cre_c", [128, 32, 16]); Cim_in = din("Cim_c", [128, 32, 16])
    dsk_in = din("dskipT", [128, 4]); wglu_in = din("w_glu", [512, 512]); bglu_in = din("bgluT", [128, 4])
    E_d = dscr("E_d", [128, 2, 4, 16, 2, 2, 128], BF16)
    G_d = dscr("G_d", [128, 64, 640], BF16)
    H_d = dscr("H_d", [2, 128, 16, 2, 544], BF16)

    with ExitStack() as s5:
        Kt_sb = sb("s_Kt_sb", [128, 128, 128], BF16, s5)
        aL = sb("s_aL", [128, 32], F32, s5); bL = sb("s_bL", [128, 32], F32, s5)
        diagD = sb("s_diagD", [128, 4, 128], BF16, s5)
        with ExitStack() as pa:
            def t32(name):
                return sb(name, [128, 32], F32, pa)
            lre, lim, ldt, dt, a, b = (t32(n) for n in ("lre", "lim", "ldt", "dt", "a_", "b_"))
            u1, u2, u3, u4, c16, s16, wre, wim = (t32(n) for n in ("u1", "u2", "u3", "u4", "c16", "s16", "wre", "wim"))
            Bcr = sb("s_Bcr", [128, 32, 16], F32, pa); Bci = sb("s_Bci", [128, 32, 16], F32, pa)
            Dr = sb("s_Dr", [128, 32, 32], F32, pa); Di = sb("s_Di", [128, 32, 32], F32, pa)
            Dr2 = sb("s_Dr2", [128, 32, 32], F32, pa); Di2 = sb("s_Di2", [128, 32, 32], F32, pa)
            Gr = sb("s_Gr", [128, 32, 32], F32, pa); Gi = sb("s_Gi", [128, 32, 32], F32, pa)
            T1 = sb("s_T1", [128, 32, 32], F32, pa); T2 = sb("s_T2", [128, 32, 32], F32, pa)
            T3 = sb("s_T3", [128, 32, 32], F32, pa); T4 = sb("s_T4", [128, 32, 32], F32, pa)
            P1 = sb("s_P1", [128, 32, 32], F32, pa); P2 = sb("s_P2", [128, 32, 32], F32, pa)
            P3 = sb("s_P3", [128, 32, 32], F32, pa); P4 = sb("s_P4", [128, 32, 32], F32, pa)
            Cbr = sb("s_Cbr", [128, 32, 32], BF16, pa); Cbni = sb("s_Cbni", [128, 32, 32], BF16, pa)
            Dbr = sb("s_Dbr", [128, 8, 160], BF16, pa); Dbi = sb("s_Dbi", [128, 8, 160], BF16, pa)
            Gbr = sb("s_Gbr", [128, 8, 160], BF16, pa); Gbni = sb("s_Gbni", [128, 8, 160], BF16, pa)
            Es = [sb("s_Es%d" % i, [128, 2, 4, 2, 2, 128], BF16, pa) for i in range(2)]
            m3 = sb("s_m3", [128, 1], F32, pa)
            dsk = sb("s_dsk", [128, 4], F32, pa)
            pk = [ps("s_pk%d" % i, [128, 128], F32, pa) for i in range(2)]
            pE = [ps("s_pE%d" % i, [128, 128], F32, pa) for i in range(4)]

            S.dma('sp', lre[:], lre_in, writes=['lre']); S.dma('sp', lim[:], lim_in, writes=['lim'])
            S.dma('sp', ldt[:], ldt_in, writes=['ldt']); S.dma('sp', dsk[:], dsk_in, writes=['dsk'])
            for X in (Dr, Di, Gr, Gi, Dr2, Di2):
                S.op('dve', lambda e: e.memset(X[:], 0.0), writes=[kn(X)])
            for X in (Dbr, Dbi, Gbr, Gbni):
                S.op('pool', lambda e: e.memset(X[:], 0.0), writes=[kn(X)])
            S.op('pool', lambda e: e.memset(Kt_sb[:], 0.0), writes=['Kt_sb'])
            for i in range(2):
                S.op('pool', lambda e: e.memset(Es[i][:], 0.0), writes=[('Es', i)])
            S.op('dve', lambda e: e.memset(m3[:], 1.0), writes=['m3'])
            S.op('dve', lambda e: e.memset(m3[64:96, :], 0.0), writes=['m3'])
            S.dma('sp', Bcr[:], Bre_in, writes=['Bcr']); S.dma('sp', Bci[:], Bim_in, writes=['Bci'])
            for h in range(2):
                S.dma('sp', Gr[64 * h:64 * h + 64, :, 16 * h:16 * h + 16], Cre_in[64 * h:64 * h + 64], reads=[], writes=['Gr'])
                S.dma('sp', Gi[64 * h:64 * h + 64, :, 16 * h:16 * h + 16], Cim_in[64 * h:64 * h + 64], reads=[], writes=['Gi'])
            for q in range(4):
                S.op('dve', lambda e: e.tensor_scalar(out=diagD[:, q, :], in0=identf[:], scalar1=dsk[:, q:q + 1], scalar2=None,
                                                      op0=ALU.mult), reads=['identf', 'dsk'], writes=['diagD'])
            V = lambda e: e
            def tt(out, in0, in1, op, r, w, eng='dve'):
                S.op(eng, lambda e: e.tensor_tensor(out=out, in0=in0, in1=in1, op=op), reads=r, writes=w)
            S.op('act', lambda e: e.activation(out=dt[:], in_=ldt[:], func=AF.Exp), reads=['ldt'], writes=['dt'])
            tt(u1[:], lre[:], dt[:], ALU.mult, ['lre', 'dt'], ['u1'])
            S.op('act', lambda e: e.activation(out=u1[:], in_=u1[:], func=AF.Exp), reads=['u1'], writes=['u1'])
            tt(u2[:], lim[:], dt[:], ALU.mult, ['lim', 'dt'], ['u2'])
            S.op('act', lambda e: e.activation(out=s16[:], in_=u2[:], func=AF.Sin, scale=1.0 / 16), reads=['u2'], writes=['s16'])
            S.op('act', lambda e: e.activation(out=u3[:], in_=u2[:], func=AF.Sin, scale=1.0 / 32), reads=['u2'], writes=['u3'])
            tt(u3[:], u3[:], u3[:], ALU.mult, ['u3'], ['u3'])
            S.op('dve', lambda e: e.tensor_scalar(out=c16[:], in0=u3[:], scalar1=-2.0, scalar2=1.0, op0=ALU.mult, op1=ALU.add),
                 reads=['u3'], writes=['c16'])

            def csquare(cr, ci, n):
                for _ in range(n):
                    tt(u3[:], cr[:], cr[:], ALU.mult, [kn(cr)], ['u3'])
                    tt(u4[:], ci[:], ci[:], ALU.mult, [kn(ci)], ['u4'])
                    tt(ci[:], cr[:], ci[:], ALU.mult, [kn(cr), kn(ci)], [kn(ci)])
                    S.op('dve', lambda e: e.tensor_scalar(out=ci[:], in0=ci[:], scalar1=2.0, scalar2=None, op0=ALU.mult),
                         reads=[kn(ci)], writes=[kn(ci)])
                    tt(cr[:], u3[:], u4[:], ALU.subtract, ['u3', 'u4'], [kn(cr)])
            csquare(c16, s16, 4)
            tt(a[:], u1[:], c16[:], ALU.mult, ['u1', 'c16'], ['a_'])
            tt(b[:], u1[:], s16[:], ALU.mult, ['u1', 's16'], ['b_'])
            S.op('dve', lambda e: e.tensor_copy(out=aL[:], in_=c16[:]), reads=['c16'], writes=['aL'])
            S.op('dve', lambda e: e.tensor_copy(out=bL[:], in_=s16[:]), reads=['s16'], writes=['bL'])
            csquare(aL, bL, 4)
            tt(u3[:], lre[:], dt[:], ALU.mult, ['lre', 'dt'], ['u3'])
            S.op('act', lambda e: e.activation(out=u3[:], in_=u3[:], func=AF.Exp, scale=16.0), reads=['u3'], writes=['u3'])
            tt(aL[:], aL[:], u3[:], ALU.mult, ['aL', 'u3'], ['aL'])
            tt(bL[:], bL[:], u3[:], ALU.mult, ['bL', 'u3'], ['bL'])
            S.op('dve', lambda e: e.tensor_scalar(out=u1[:], in0=a[:], scalar1=-1.0, scalar2=None, op0=ALU.add), reads=['a_'], writes=['u1'])
            tt(u2[:], lre[:], lre[:], ALU.mult, ['lre'], ['u2'])
            tt(u3[:], lim[:], lim[:], ALU.mult, ['lim'], ['u3'])
            tt(u2[:], u2[:], u3[:], ALU.add, ['u2', 'u3'], ['u2'])
            S.op('dve', lambda e: e.reciprocal(out=u2[:], in_=u2[:]), reads=['u2'], writes=['u2'])
            tt(u3[:], u1[:], lre[:], ALU.mult, ['u1', 'lre'], ['u3'])
            tt(u4[:], b[:], lim[:], ALU.mult, ['b_', 'lim'], ['u4'])
            tt(u3[:], u3[:], u4[:], ALU.add, ['u3', 'u4'], ['u3'])
            tt(wre[:], u3[:], u2[:], ALU.mult, ['u3', 'u2'], ['wre'])
            tt(u3[:], b[:], lre[:], ALU.mult, ['b_', 'lre'], ['u3'])
            tt(u4[:], u1[:], lim[:], ALU.mult, ['u1', 'lim'], ['u4'])
            tt(u3[:], u3[:], u4[:], ALU.subtract, ['u3', 'u4'], ['u3'])
            tt(wim[:], u3[:], u2[:], ALU.mult, ['u3', 'u2'], ['wim'])
            bc16 = lambda t: t[:].unsqueeze(2).to_broadcast([128, 32, 16])
            Tc = [T1[:, :, 0:16], T2[:, :, 0:16], T3[:, :, 0:16], T4[:, :, 0:16]]
            tt(Tc[0], Bcr[:], bc16(wre), ALU.mult, ['Bcr', 'wre'], ['T1'])
            tt(Tc[1], Bci[:], bc16(wim), ALU.mult, ['Bci', 'wim'], ['T2'])
            tt(Tc[2], Bci[:], bc16(wre), ALU.mult, ['Bci', 'wre'], ['T3'])
            tt(Tc[3], Bcr[:], bc16(wim), ALU.mult, ['Bcr', 'wim'], ['T4'])
            for h in range(2):
                sl = slice(64 * h, 64 * h + 64)
                tt(Dr[sl, :, 16 * h:16 * h + 16], T1[sl, :, 0:16], T2[sl, :, 0:16], ALU.subtract, ['T1', 'T2'], ['Dr'])
                tt(Di[sl, :, 16 * h:16 * h + 16], T3[sl, :, 0:16], T4[sl, :, 0:16], ALU.add, ['T3', 'T4'], ['Di'])
            S.op('dve', lambda e: e.tensor_copy(out=Cbr[:], in_=Gr[:]), reads=['Gr'], writes=['Cbr'])
            S.op('dve', lambda e: e.tensor_scalar(out=Cbni[:], in0=Gi[:], scalar1=-1.0, scalar2=None, op0=ALU.mult),
                 reads=['Gi'], writes=['Cbni'])
            bc32 = lambda t: t[:].unsqueeze(2).to_broadcast([128, 32, 32])

            def cmul(Xr, Xi, eng, Yr=None, Yi=None):
                Yr = Yr or Xr; Yi = Yi or Xi
                A1, A2, A3, A4 = (T1, T2, T3, T4) if eng == 'dve' else (P1, P2, P3, P4)
                tt(A1[:], Xr[:], bc32(a), ALU.mult, [kn(Xr), 'a_'], [kn(A1)], eng)
                tt(A2[:], Xi[:], bc32(b), ALU.mult, [kn(Xi), 'b_'], [kn(A2)], eng)
                tt(A3[:], Xi[:], bc32(a), ALU.mult, [kn(Xi), 'a_'], [kn(A3)], eng)
                tt(A4[:], Xr[:], bc32(b), ALU.mult, [kn(Xr), 'b_'], [kn(A4)], eng)
                tt(Yr[:], A1[:], A2[:], ALU.subtract, [kn(A1), kn(A2)], [kn(Yr)], eng)
                tt(Yi[:], A3[:], A4[:], ALU.add, [kn(A3), kn(A4)], [kn(Yi)], eng)

            def to5(dst, src, scale, key):
                d4 = dst[:].rearrange("p g (s c) -> p g s c", c=32)
                s4 = src[:].rearrange("p (g s) c -> p g s c", s=4)
                S.op('dve', lambda e: e.tensor_scalar(out=d4[:, :, 0:3, :], in0=s4[:, :, 0:3, :], scalar1=scale, scalar2=None, op0=ALU.mult),
                     reads=[kn(src)], writes=[key])
                S.op('dve', lambda e: e.tensor_scalar(out=d4[:, :, 4, :], in0=s4[:, :, 3, :], scalar1=scale, scalar2=None, op0=ALU.mult),
                     reads=[kn(src)], writes=[key])

            Dpairs = [(Dr, Di), (Dr2, Di2)]
            for j in range(16):
                Dr, Di = Dpairs[j % 2]
                to5(Dbr, Dr, 1.0, 'Dbr'); to5(Dbi, Di, 1.0, 'Dbi')
                if j < 15:
                    cmul(Dr, Di, 'dve', Dpairs[(j + 1) % 2][0], Dpairs[(j + 1) % 2][1])
                for d in range(2):
                    for q in range(4):
                        idx = (d * 4 + q) * 16 + j
                        pkt = pk[idx % 2]; pkk = ('pk', idx % 2)
                        for pl in range(4):
                            pair = d * 16 + q * 4 + pl
                            if pl < 3:
                                osl = pkt[32 * pl:32 * pl + 32, 32 * pl:32 * pl + 32]; csl = slice(32 * pl, 32 * pl + 32)
                            else:
                                osl = pkt[64:128, 96:128]; csl = slice(96, 160)
                            S.op('pe', lambda e: e.matmul(osl, lhsT=Dbr[:, d * 4 + q, csl], rhs=Cbr[:, pair, :], start=True, stop=False),
                                 reads=['Dbr', 'Cbr'], writes=[pkk])
                            S.op('pe', lambda e: e.matmul(osl, lhsT=Dbi[:, d * 4 + q, csl], rhs=Cbni[:, pair, :], start=False, stop=True),
                                 reads=['Dbi', 'Cbni'], writes=[pkk])
                        for pl in range(4):
                            if pl < 3:
                                rs_, cs_ = slice(32 * pl, 32 * pl + 32), slice(32 * pl, 32 * pl + 32)
                            else:
                                rs_, cs_ = slice(64, 128), slice(96, 128)
                            S.op('act', lambda e: e.activation(out=Kt_sb[rs_, idx, cs_], in_=pkt[rs_, cs_], func=AF.Copy),
                                 reads=[pkk], writes=['Kt_sb'])
                Et = Es[j % 2]; ek = ('Es', j % 2)
                n_e = 0
                for reim, X in ((0, Dr), (1, Di)):
                    for d in range(2):
                        for q in range(4):
                            pet = pE[n_e % 4]; pek = ('pE', n_e % 4); n_e += 1
                            src = X[:, d * 16 + q * 4:d * 16 + q * 4 + 4, :].rearrange("p a b -> p (a b)")
                            S.op('pe', lambda e: e.transpose(out=pet[:], in_=src, identity=identf[:]), reads=[kn(X), 'identf'], writes=[pek])
                            S.op('act', lambda e: e.activation(out=Et[:, d, q, reim, 0, :], in_=pet[:], func=AF.Copy), reads=[pek], writes=[ek])
                            S.op('dve', lambda e: e.tensor_scalar(out=Et[64:128, d, q, reim, 1, :], in0=pet[64:128, :], scalar1=m3[64:128, 0:1],
                                                                  scalar2=None, op0=ALU.mult), reads=[pek, 'm3'], writes=[ek])
                for d in range(2):
                    S.dma('sp', E_d[:, d, :, j, :, :, :], Et[:, d], reads=[ek], writes=['E_d'])
                cmul(Gr, Gi, 'pool')
                to5(Gbr, Gr, 1.0, 'Gbr'); to5(Gbni, Gi, -1.0, 'Gbni')
                for d in range(2):
                    S.dma('sp', G_d[:, (d * 16 + j) * 2 + 0, :], Gbr[:, d * 4:d * 4 + 4, :].rearrange("p a b -> p (a b)"), reads=['Gbr'], writes=['G_d'])
                    S.dma('sp', G_d[:, (d * 16 + j) * 2 + 1, :], Gbni[:, d * 4:d * 4 + 4, :].rearrange("p a b -> p (a b)"), reads=['Gbni'], writes=['G_d'])
            S.barrier()
        if 's5prep' in dbg:
            dbg_out['Kt'] = dout("dbg_Kt", [128, 128, 128], BF16)
            S.dma('sp', dbg_out['Kt'], Kt_sb[:], reads=['Kt_sb'])
            dbg_out['aL'] = dout("dbg_aL", [128, 2, 32])
            S.dma('sp', dbg_out['aL'][:, 0, :], aL[:], reads=['aL']); S.dma('sp', dbg_out['aL'][:, 1, :], bL[:], reads=['bL'])
            dbg_out['G'] = dout("dbg_G", [128, 64, 640], BF16)
            S.dma('sp', dbg_out['G'], G_d, reads=['G_d'])
            dbg_out['E'] = dout("dbg_E", [128, 2, 4, 16, 2, 2, 128], BF16)
            for d in range(2):
                S.dma('sp', dbg_out['E'][:, d], E_d[:, d], reads=['E_d'])
            return 'stop'
        return s5_main(nc, S, es, sb, ps, din, dscr, dout, dbg, dbg_out, identf, identb, uT_d, yaT_d, nbanks,
                       s5, Kt_sb, aL, bL, diagD, E_d, G_d, H_d, wglu_in, bglu_in)


def s5_main(nc, S, es, sb, ps, din, dscr, dout, dbg, dbg_out, identf, identb, uT_d, yaT_d, nbanks,
            s5, Kt_sb, aL, bL, diagD, E_d, G_d, H_d, wglu_in, bglu_in):
    NCH = 528
    with ExitStack() as pb:
        uTb = [sb("s_uTb%d" % i, [128, 2048], BF16, pb) for i in range(3)]
        Esl = [sb("s_Esl%d" % i, [128, 16, 2, 2, 128], BF16, pb) for i in range(2)]
        Ssb = [[sb("s_Ssb%d_%d" % (d, i), [128, 16, 2, 128], F32, pb) for i in range(2)] for d in range(2)]
        hist = [sb("s_hist%d" % d, [128, 16, 2, 129], F32, pb) for d in range(2)]
        histb = [sb("s_histb%d" % d, [128, 16, 2, 128], BF16, pb) for d in range(2)]
        tP = [sb("s_tP%d" % d, [128, 16, 2], F32, pb) for d in range(2)]
        tQ = [sb("s_tQ%d" % d, [128, 16, 2], F32, pb) for d in range(2)]
        pS = [ps("s_pS%d" % i, [128, 4, 128], F32, pb) for i in range(4)]
        for d in range(2):
            S.op('dve' if d == 0 else 'pool', lambda e: e.memset(hist[d][:], 0.0), writes=[('hist', d)])
        fblocks = [(0, 128), (128, 128), (256, 128), (384, 128), (512, 16)]
        bblocks = [(512, 16), (384, 128), (256, 128), (128, 128), (0, 128)]
        esl_ctr = 0
        ps_ctr = 0
        for step in range(5):
            for d in range(2):
                k0, nk = (fblocks if d == 0 else bblocks)[step]
                Sb = Ssb[d][step % 2]; skey = ('Ssb', d, step % 2)
                base = 0 if d == 0 else 256
                for q in range(4):
                    et = Esl[esl_ctr % 2]; ekey = ('Esl', esl_ctr % 2); esl_ctr += 1
                    S.dma('sp', et[:], E_d[:, d, q], reads=['E_d'], writes=[ekey])
                    ut = uTb[(esl_ctr - 1) % 3]; ukey = ('uTb', (esl_ctr - 1) % 3)
                    S.dma('sp', ut[:, 0:16 * nk], uT_d[q * 128:(q + 1) * 128, base + 16 * k0: base + 16 * k0 + 16 * nk], reads=['uT_d'], writes=[ukey])
                    for pl in range(4):
                        pst = pS[ps_ctr % 4]; pskey = ('pS', ps_ctr % 4); ps_ctr += 1
                        if pl < 3:
                            rsl, alt = slice(32 * pl, 32 * pl + 32), 0
                        else:
                            rsl, alt = slice(64, 128), 1
                        for reim in range(2):
                            for i in range(16):
                                j = 15 - i if d == 0 else i
                                rhs = ut[rsl, i:i + 16 * (nk - 1) + 1:16]
                                S.op('pe', lambda e: e.matmul(pst[:, reim, 0:nk], lhsT=et[rsl, j, reim, alt, :], rhs=rhs,
                                                              start=(i == 0), stop=(i == 15)),
                                     reads=[ekey, ukey], writes=[pskey])
                        S.op('act', lambda e: e.activation(out=Sb[:, q * 4 + pl, :, 0:nk], in_=pst[:, 0:2, 0:nk], func=AF.Copy),
                             reads=[pskey], writes=[skey])
            def chain_ops(d, kk):
                k0, nk = (fblocks if d == 0 else bblocks)[step]
                Sb = Ssb[d][step % 2]; skey = ('Ssb', d, step % 2)
                hk = ('hist', d)
                A2 = aL[:, d * 16:(d + 1) * 16].unsqueeze(2).to_broadcast([128, 16, 2])
                B2 = bL[:, d * 16:(d + 1) * 16].unsqueeze(2).to_broadcast([128, 16, 2])
                H = hist[d]
                if d == 0:
                    cprev, ccur, scol = kk, kk + 1, kk
                else:
                    cprev, ccur, scol = nk - kk, nk - 1 - kk, nk - 1 - kk
                prev = H[:, :, :, cprev]
                P_, Q_ = tP[d], tQ[d]
                eng = 'dve'
                return [
                    lambda: S.op(eng, lambda e: e.tensor_tensor(out=P_[:], in0=prev, in1=A2, op=ALU.mult), reads=[hk, 'aL'], writes=[('tP', d)]),
                    lambda: S.op(eng, lambda e: e.tensor_tensor(out=Q_[:], in0=prev, in1=B2, op=ALU.mult), reads=[hk, 'bL'], writes=[('tQ', d)]),
                    lambda: S.op(eng, lambda e: e.tensor_tensor(out=P_[:], in0=P_[:], in1=Sb[:, :, :, scol], op=ALU.add),
                                 reads=[('tP', d), skey], writes=[('tP', d)]),
                    lambda: S.op(eng, lambda e: e.tensor_tensor(out=H[:, :, 0, ccur], in0=P_[:, :, 0], in1=Q_[:, :, 1], op=ALU.subtract),
                                 reads=[('tP', d), ('tQ', d)], writes=[hk]),
                    lambda: S.op(eng, lambda e: e.tensor_tensor(out=H[:, :, 1, ccur], in0=P_[:, :, 1], in1=Q_[:, :, 0], op=ALU.add),
                                 reads=[('tP', d), ('tQ', d)], writes=[hk]),
                ]
            nks = [(fblocks if d == 0 else bblocks)[step][1] for d in range(2)]
            for kk in range(max(nks)):
                lists = [chain_ops(d, kk) for d in range(2) if kk < nks[d]]
                for oi in range(5):
                    for l in lists:
                        l[oi]()
            for d in range(2):
                eng = 'dve' if d == 0 else 'pool'
                k0, nk = (fblocks if d == 0 else bblocks)[step]
                hk = ('hist', d)
                H = hist[d]
                if d == 0:
                    S.op(eng, lambda e: e.tensor_copy(out=histb[d][:, :, :, 0:nk], in_=H[:, :, :, 1:nk + 1]), reads=[hk], writes=[('histb', d)])
                    S.op(eng, lambda e: e.tensor_copy(out=H[:, :, :, 0], in_=H[:, :, :, nk]), reads=[hk], writes=[hk])
                else:
                    S.op(eng, lambda e: e.tensor_copy(out=histb[d][:, :, :, 0:nk], in_=H[:, :, :, 0:nk]), reads=[hk], writes=[('histb', d)])
                    S.op(eng, lambda e: e.tensor_copy(out=H[:, :, :, 128], in_=H[:, :, :, 0]), reads=[hk], writes=[hk])
                S.dma('pool', H_d[d, :, :, :, k0:k0 + nk], histb[d][:, :, :, 0:nk], reads=[('histb', d)], writes=['H_d'])
        S.barrier()
    if 's5lvl3' in dbg:
        dbg_out['H'] = dout("dbg_H", [2, 128, 16, 2, 544], BF16)
        for d in range(2):
            S.dma('sp', dbg_out['H'][d], H_d[d], reads=['H_d'])
        return 'stop'

    with ExitStack() as pc:
        Gsb = sb("s_Gsb", [128, 64, 640], BF16, pc)
        wglu = sb("s_wglu", [128, 4, 512], BF16, pc)
        bglu = sb("s_bglu", [128, 4], F32, pc)
        ub = [sb("s_ub%d" % i, [128, 4, 544], BF16, pc) for i in range(2)]
        Hf = [sb("s_Hf%d" % i, [128, 16, 2, 32], BF16, pc) for i in range(2)]
        Hb = [sb("s_Hb%d" % i, [128, 16, 2, 32], BF16, pc) for i in range(2)]
        aT = [sb("s_aT%d" % i, [128, 4, 512], BF16, pc) for i in range(2)]
        sig = [sb("s_sig%d" % i, [128, 512], BF16, pc) for i in range(2)]
        yo = [sb("s_yo%d" % i, [128, 512], BF16, pc) for i in range(4)]
        py = [ps("s_py%d" % i, [128, 512], F32, pc) for i in range(4)]
        pg = [ps("s_pg%d" % i, [128, 512], F32, pc) for i in range(2)]
        for c in range(4):
            S.dma('sp', Gsb[:, c * 16:(c + 1) * 16, :], G_d[:, c * 16:(c + 1) * 16, :], reads=['G_d'], writes=[('Gsb', c)])
        gkeys = [('Gsb', c) for c in range(4)]
        for q in range(4):
            S.dma('pool', wglu[:, q, :], wglu_in[q * 128:(q + 1) * 128, :], writes=['wglu'])
        S.dma('sp', bglu[:], bglu_in, writes=['bglu'])
        yo_ctr = 0
        for n in range(nbanks):
            i2 = n % 2
            e0 = 256 + 512 * n
            S.dma('sp', ub[i2][:], uT_d[:, e0 - 16:e0 + 528].rearrange("(q p) t -> p q t", p=128), reads=['uT_d'], writes=[('ub', i2)])
            kf = 16 + 32 * n
            kb = 32 * n
            S.dma('sp', Hf[i2][:], H_d[0, :, :, :, kf - 1:kf + 31], reads=['H_d'], writes=[('Hf', i2)])
            S.dma('sp', Hb[i2][:], H_d[1, :, :, :, kb + 1:kb + 33], reads=['H_d'], writes=[('Hb', i2)])
            for q in range(4):
                pyt = py[q]; pk_ = ('py', q)
                pyv = pyt[:].rearrange("p (k j) -> p k j", j=16)
                uc = ub[i2][:, q, 16:528]
                ucv = uc.rearrange("p (k j) -> p k j", j=16)
                rd = [('ub', i2), 'Kt_sb', 'diagD']
                S.op('pe', lambda e: e.matmul(pyt[:], lhsT=diagD[:, q, :], rhs=uc, start=True, stop=False), reads=rd, writes=[pk_])
                S.op('pe', lambda e: e.matmul(pyt[:], lhsT=Kt_sb[:, (0 * 4 + q) * 16 + 0, :], rhs=uc, start=False, stop=False), reads=rd, writes=[pk_])
                for tau in range(1, 16):
                    S.op('pe', lambda e: e.matmul(pyv[:, :, tau:16], lhsT=Kt_sb[:, (0 * 4 + q) * 16 + tau, :], rhs=ucv[:, :, 0:16 - tau],
                                                  start=False, stop=False), reads=rd, writes=[pk_])
                S.op('pe', lambda e: e.matmul(pyt[:], lhsT=Kt_sb[:, (1 * 4 + q) * 16 + 0, :], rhs=uc, start=False, stop=False), reads=rd, writes=[pk_])
                for tau in range(1, 16):
                    S.op('pe', lambda e: e.matmul(pyv[:, :, 0:16 - tau], lhsT=Kt_sb[:, (1 * 4 + q) * 16 + tau, :], rhs=ucv[:, :, tau:16],
                                                  start=False, stop=False), reads=rd, writes=[pk_])
                for d in range(2):
                    Hx = Hf[i2] if d == 0 else Hb[i2]
                    hkey = ('Hf', i2) if d == 0 else ('Hb', i2)
                    for j in range(16):
                        m = j + 1 if d == 0 else 16 - j
                        for pl in range(4):
                            pair = q * 4 + pl
                            if pl < 3:
                                osl = pyv[32 * pl:32 * pl + 32, :, j]; csl = slice(q * 160 + 32 * pl, q * 160 + 32 * pl + 32)
                            else:
                                osl = pyv[64:128, :, j]; csl = slice(q * 160 + 96, q * 160 + 160)
                            for reim in range(2):
                                last = (d == 1 and j == 15 and pl == 3 and reim == 1)
                                S.op('pe', lambda e: e.matmul(osl, lhsT=Gsb[:, (d * 16 + m - 1) * 2 + reim, csl], rhs=Hx[:, pair, reim, :],
                                                              start=False, stop=last), reads=gkeys + [hkey], writes=[pk_])
                S.op('act', lambda e: e.activation(out=aT[i2][:, q, :], in_=pyt[:], func=AF.Gelu), reads=[pk_], writes=[('aT', i2, q)])
            for oc in range(4):
                pgt = pg[oc % 2]; pgk = ('pg', oc % 2)
                for q in range(4):
                    S.op('pe', lambda e: e.matmul(pgt[:], lhsT=wglu[:, q, oc * 128:(oc + 1) * 128], rhs=aT[i2][:, q, :],
                                                  start=(q == 0), stop=(q == 3)), reads=['wglu', ('aT', i2, q)], writes=[pgk])
                sg = sig[oc % 2]; sgk = ('sig', oc % 2)
                S.op('act', lambda e: e.activation(out=sg[:], in_=pgt[:], func=AF.Sigmoid, bias=bglu[:, oc:oc + 1]),
                     reads=[pgk, 'bglu'], writes=[sgk])
                yt = yo[yo_ctr % 4]; yk = ('yo', yo_ctr % 4); yo_ctr += 1
                S.op('dve', lambda e: e.tensor_tensor(out=yt[:], in0=aT[i2][:, oc, :], in1=sg[:], op=ALU.mult),
                     reads=[('aT', i2, oc), sgk], writes=[yk])
                S.dma('pool', yaT_d[oc * 128:(oc + 1) * 128, 512 * n:512 * n + 512], yt[:], reads=[yk], writes=['yaT_d'])
        S.barrier()
    if 's5' in dbg:
        dbg_out['yaT'] = dout("dbg_yaT", [512, 8192], BF16)
        for r0 in range(0, 512, 128):
            S.dma('sp', dbg_out['yaT'][r0:r0 + 128, :], yaT_d[r0:r0 + 128, :], reads=['yaT_d'])
    return 'ok'

NEG = -30000.0


def na_phase(nc, S, es, sb, ps, din, dscr, dout, dbg, dbg_out, identb, qT_d, kT_d, v_d, ybT_d, nblocks=16):
    rpbT_in = din("rpbT", [128, 8 * 14 * 64])
    maskT_in = din("maskT", [128, 64])
    with ExitStack() as pa:
        Tb = sb("n_Tb", [128, 8, 14, 64], F32, pa)
        mk = sb("n_mk", [128, 64], F32, pa)
        kctx = sb("n_kctx", [128, 4, 256], BF16, pa)
        vctx = sb("n_vctx", [128, 2, 8, 65], BF16, pa)
        qb = [sb("n_qb%d" % i, [128, 4, 512], BF16, pa) for i in range(2)]
        kw = [sb("n_kw%d" % i, [128, 4, 960], BF16, pa) for i in range(2)]
        vw = [sb("n_vw%d" % i, [128, 15, 8, 65], BF16, pa) for i in range(2)]
        sS = [sb("n_sS%d" % i, [128, 4, 64], F32, pa) for i in range(3)]
        P = [sb("n_Pn%d" % i, [128, 6, 64], BF16, pa) for i in range(3)]
        rc = [sb("n_rc%d" % i, [128, 8], F32, pa) for i in range(2)]
        ybt = [sb("n_ybt%d" % i, [128, 8, 64], BF16, pa) for i in range(2)]
        ybT = [sb("n_ybT%d" % i, [128, 4, 512], BF16, pa) for i in range(2)]
        pS = [ps("n_pS%d" % i, [128, 6, 64], F32, pa) for i in range(3)]
        pO = [ps("n_pO%d" % i, [128, 4, 65], F32, pa) for i in range(4)]
        pT = ps("n_pTn", [128, 4, 128], BF16, pa)

        S.dma('sp', Tb[:].rearrange("p h a q -> p (h a q)"), rpbT_in, writes=['Tb'])
        S.dma('sp', mk[:], maskT_in, writes=['mk'])
        S.dma('sp', kctx[:], kT_d[:, 0:256].rearrange("(c p) t -> p c t", p=128), reads=['kT_d'], writes=['kctx'])
        S.dma('sp', vctx[:], v_d[0:256].rearrange("(m p) h d -> p m h d", p=128), reads=['v_d'], writes=['vctx'])
        S.op('dve', lambda e: e.tensor_tensor(out=Tb[:].rearrange("p h a q -> p (h a) q"), in0=Tb[:].rearrange("p h a q -> p (h a) q"),
                                              in1=mk[:].unsqueeze(1).to_broadcast([128, 112, 64]), op=ALU.add),
             reads=['Tb', 'mk'], writes=['Tb'])
        item = 0
        pend = None
        for b in range(nblocks):
            i2 = b % 2
            r0 = 8 * b
            lo = min(max(r0 - 4, 0), 120)
            hi = min(max(r0 + 7 - 4, 0), 120) + 8
            nrows = hi - lo
            S.dma('sp', qb[i2][:], qT_d[:, 512 * b:512 * b + 512].rearrange("(c p) t -> p c t", p=128), reads=['qT_d'], writes=[('qb', i2)])
            S.dma('sp', kw[i2][:, :, 0:64 * nrows], kT_d[:, 256 + 64 * lo:256 + 64 * hi].rearrange("(c p) t -> p c t", p=128),
                  reads=['kT_d'], writes=[('kw', i2)])
            for s in range(nrows - 1):
                S.dma('sp', vw[i2][:, s], v_d[256 + 64 * (lo + s):256 + 64 * (lo + s) + 128], reads=['v_d'], writes=[('vw', i2)])
            for rr in range(8):
                r = r0 + rr
                krs = min(max(r - 4, 0), 120)
                a0 = krs - r + 7
                half = 64 * (r % 2)
                pp = (r // 2) % 2
                for h in range(8):
                    hc, hb = h // 2, 64 * (h % 2)
                    it = item % 3
                    item += 1
                    pst = pS[it]; psk = ('pS', it)
                    for c in range(4):
                        kc0 = 64 * (krs + 2 * c - lo)
                        S.op('pe', lambda e: e.matmul(pst[:, c, :], lhsT=kw[i2][hb:hb + 64, hc, kc0:kc0 + 128],
                                                      rhs=qb[i2][hb:hb + 64, hc, 64 * rr:64 * rr + 64], start=True, stop=True),
                             reads=[('kw', i2), ('qb', i2)], writes=[psk])
                    for c in range(2):
                        S.op('pe', lambda e: e.matmul(pst[:, 4 + c, :], lhsT=kctx[hb:hb + 64, hc, 128 * c:128 * c + 128],
                                                      rhs=qb[i2][hb:hb + 64, hc, 64 * rr:64 * rr + 64], start=True, stop=True),
                             reads=['kctx', ('qb', i2)], writes=[psk])
                    S.op('dve', lambda e: e.tensor_tensor(out=sS[it][:], in0=pst[:, 0:4, :], in1=Tb[:, h, a0:a0 + 7:2, :], op=ALU.add),
                         reads=[psk, 'Tb'], writes=[('sS', it)])
                    S.op('act', lambda e: e.activation(out=P[it][:, 0:4, :], in_=sS[it][:], func=AF.Exp), reads=[('sS', it)], writes=[('P', it)])
                    S.op('act', lambda e: e.activation(out=P[it][:, 4:6, :], in_=pst[:, 4:6, :], func=AF.Exp), reads=[psk], writes=[('P', it)])
                    if pend is not None:
                        pend()
                    def mk_pv(it=it, i2=i2, krs=krs, lo=lo, h=h, half=half, pp=pp, r=r, rr=rr, b=b):
                        def pv():
                            pot = pO[pp * 2 + h // 4]; pok = ('pO', pp * 2 + h // 4)
                            for c in range(6):
                                if c < 4:
                                    rhs = vw[i2][:, krs + 2 * c - lo, h, :]
                                    rk = ('vw', i2)
                                else:
                                    rhs = vctx[:, c - 4, h, :]
                                    rk = 'vctx'
                                S.op('pe', lambda e: e.matmul(pot[half:half + 64, h % 4, :], lhsT=P[it][:, c, :], rhs=rhs,
                                                              start=(c == 0), stop=(c == 5)), reads=[('P', it), rk], writes=[pok])
                            if h % 4 == 3:
                                hh = h // 4
                                yk = ('ybt', pp)
                                S.op('dve', lambda e: e.reciprocal(out=rc[pp][half:half + 64, 4 * hh:4 * hh + 4], in_=pot[half:half + 64, :, 64]),
                                     reads=[pok], writes=[('rc', pp)])
                                S.op('dve', lambda e: e.tensor_tensor(out=ybt[pp][half:half + 64, 4 * hh:4 * hh + 4, :], in0=pot[half:half + 64, :, 0:64],
                                                                      in1=rc[pp][half:half + 64, 4 * hh:4 * hh + 4].unsqueeze(2).to_broadcast([64, 4, 64]),
                                                                      op=ALU.mult), reads=[pok, ('rc', pp)], writes=[yk])
                            if h == 7 and r % 2 == 1:
                                tk = ('ybT', b % 2)
                                for c4 in range(4):
                                    S.op('pe', lambda e: e.transpose(out=pT[:, c4, :], in_=ybt[pp][:, 2 * c4:2 * c4 + 2, :].rearrange("p a d -> p (a d)"),
                                                                     identity=identb[:]), reads=[('ybt', pp), 'identb'], writes=['pTn'])
                                S.op('act', lambda e: e.activation(out=ybT[b % 2][:, :, 64 * (rr - 1):64 * (rr - 1) + 128], in_=pT[:], func=AF.Copy),
                                     reads=['pTn'], writes=[tk])
                                if rr == 7:
                                    S.dma('act', ybT_d[:, 512 * b:512 * b + 512].rearrange("(c p) t -> p c t", p=128), ybT[b % 2][:],
                                          reads=[tk], writes=['ybT_d'])
                        return pv
                    pend = mk_pv()
        pend()
        S.barrier()
    if 'na' in dbg:
        dbg_out['ybT'] = dout("dbg_ybT", [512, 8192], BF16)
        for r0 in range(0, 512, 128):
            S.dma('sp', dbg_out['ybT'][r0:r0 + 128, :], ybT_d[r0:r0 + 128, :], reads=['ybT_d'])


def na_host(inp):
    rpb = inp['na_rpb'][0].astype(np.float32)
    i = np.arange(2)[:, None, None, None]
    kc = np.arange(64)[None, :, None, None]
    a = np.arange(14)[None, None, :, None]
    qc = np.arange(64)[None, None, None, :]
    dcol = np.clip(kc - qc + 15, 0, 30)
    drow = a + i
    T = rpb[:, drow, dcol]
    T = np.broadcast_to(T, (8, 2, 64, 14, 64)).transpose(1, 2, 0, 3, 4).reshape(128, 8 * 14 * 64)
    q = np.arange(64)
    qcs = np.clip(q - 8, 0, 48)
    k = np.arange(64)[:, None]
    inw = (k >= qcs[None, :]) & (k < qcs[None, :] + 16)
    m = np.where(inw, 0.0, NEG).astype(np.float32)
    m = np.concatenate([m, m], axis=0)
    return {'rpbT': np.ascontiguousarray(T.astype(np.float32)), 'maskT': np.ascontiguousarray(m)}

D = 1024


def merge_phase(nc, S, es, sb, ps, din, dscr, dout, dbg, dbg_out, identf, identb, epsc, bc_g1, bc_mul2, bc_add2,
                xcat, out, yaT_d, ybT_d, sgaT_d, sgbT_d, h2_d, aff_d, affT_d, nblocks=16):
    wpa_in = din("w_proj_a", [512, D]); wpb_in = din("w_proj_b", [512, D]); wout_in = din("w_out", [D, D])
    wr_in = din("w_routerT", [128, 8, 16])
    with ExitStack() as pa:
        wpa = sb("g_wpa", [128, 4, D], BF16, pa); wpb = sb("g_wpb", [128, 4, D], BF16, pa)
        wout = sb("g_wout", [128, 8, D], BF16, pa)
        wr = sb("g_wr", [128, 8, 16], F32, pa)
        ya = [sb("g_mya%d" % i, [128, 4, 512], BF16, pa) for i in range(2)]
        yb = [sb("g_myb%d" % i, [128, 4, 512], BF16, pa) for i in range(2)]
        sga = [sb("g_msga%d" % i, [128, 8, 512], BF16, pa) for i in range(2)]
        sgb = [sb("g_msgb%d" % i, [128, 8, 512], BF16, pa) for i in range(2)]
        mT = [sb("g_mT%d" % i, [128, 8, 512], BF16, pa) for i in range(2)]
        t1 = [sb("g_mt1_%d" % i, [128, 512], F32, pa) for i in range(2)]
        t2 = [sb("g_mt2_%d" % i, [128, 512], F32, pa) for i in range(2)]
        xt = [sb("g_mxt%d" % i, [128, D], F32, pa) for i in range(2)]
        x1 = [sb("g_mx1_%d" % i, [128, D], F32, pa) for i in range(3)]
        h2 = [sb("g_mh2_%d" % i, [128, D], F32, pa) for i in range(3)]
        h2b = [sb("g_mh2b_%d" % i, [128, D], BF16, pa) for i in range(3)]
        h2T = [sb("g_mh2T_%d" % i, [128, 8, 128], F32, pa) for i in range(3)]
        junk = sb("g_mjunk", [128, D], BF16, pa)
        st = [sb("g_mst%d" % i, [128, 8], F32, pa) for i in range(3)]
        lg = [sb("g_mlg%d" % i, [128, 16], F32, pa) for i in range(3)]
        af = [sb("g_maf%d" % i, [128, 16], F32, pa) for i in range(3)]
        A16 = sb("g_A16", [16, 8192], F32, pa)
        pab = [ps("g_mpab%d" % i, [128, 512], F32, pa) for i in range(4)]
        pyo = ps("g_mpyo", [128, D], F32, pa)
        phT = ps("g_mphT", [128, 4, 128], F32, pa)
        pmisc = ps("g_mpmisc", [128, 256], F32, pa)
        plg = pmisc[:, 0:16]
        paT = pmisc[0:16, 128:256]

        for q in range(4):
            S.dma('pool', wpa[:, q, :], wpa_in[q * 128:(q + 1) * 128, :], writes=['wpa'])
            S.dma('pool', wpb[:, q, :], wpb_in[q * 128:(q + 1) * 128, :], writes=['wpb'])
        for dc in range(8):
            S.dma('pool', wout[:, dc, :], wout_in[dc * 128:(dc + 1) * 128, :], writes=['wout'])
        S.dma('sp', wr[:], wr_in, writes=['wr'])
        tile_ctr = 0
        pend = []
        for b in range(nblocks):
            i2 = b % 2
            cs = slice(512 * b, 512 * b + 512)
            S.dma('sp', ya[i2][:], yaT_d[:, cs].rearrange("(c p) t -> p c t", p=128), reads=['yaT_d'], writes=[('ya', i2)])
            S.dma('sp', yb[i2][:], ybT_d[:, cs].rearrange("(c p) t -> p c t", p=128), reads=['ybT_d'], writes=[('yb', i2)])
            S.dma('sp', sga[i2][:], sgaT_d[:, cs].rearrange("(c p) t -> p c t", p=128), reads=['sgaT_d'], writes=[('sga', i2)])
            S.dma('sp', sgb[i2][:], sgbT_d[:, cs].rearrange("(c p) t -> p c t", p=128), reads=['sgbT_d'], writes=[('sgb', i2)])
            for dc in range(8):
                j2 = dc % 2
                pA_, pB_ = pab[2 * j2], pab[2 * j2 + 1]
                for q in range(4):
                    S.op('pe', lambda e: e.matmul(pA_[:], lhsT=wpa[:, q, dc * 128:(dc + 1) * 128], rhs=ya[i2][:, q, :],
                                                  start=(q == 0), stop=(q == 3)), reads=['wpa', ('ya', i2)], writes=[('pA', j2)])
                for q in range(4):
                    S.op('pe', lambda e: e.matmul(pB_[:], lhsT=wpb[:, q, dc * 128:(dc + 1) * 128], rhs=yb[i2][:, q, :],
                                                  start=(q == 0), stop=(q == 3)), reads=['wpb', ('yb', i2)], writes=[('pB', j2)])
                S.op('dve', lambda e: e.tensor_tensor(out=t1[j2][:], in0=pA_[:], in1=sga[i2][:, dc, :], op=ALU.mult),
                     reads=[('pA', j2), ('sga', i2)], writes=[('t1', j2)])
                S.op('dve', lambda e: e.tensor_tensor(out=t2[j2][:], in0=pB_[:], in1=sgb[i2][:, dc, :], op=ALU.mult),
                     reads=[('pB', j2), ('sgb', i2)], writes=[('t2', j2)])
                S.op('pool', lambda e: e.tensor_tensor(out=mT[i2][:, dc, :], in0=t1[j2][:], in1=t2[j2][:], op=ALU.add),
                     reads=[('t1', j2), ('t2', j2)], writes=[('mT', i2)])
            for ti in range(4):
                k2 = tile_ctr % 3
                kx = tile_ctr % 2
                tile_ctr += 1
                tok0 = 512 * b + 128 * ti
                S.dma('sp', xt[kx][:], xcat[256 + tok0:256 + tok0 + 128, :], writes=[('xt', kx)])
                for hf in range(2):
                    for dc in range(8):
                        S.op('pe', lambda e: e.matmul(pyo[:, hf * 512:(hf + 1) * 512], lhsT=mT[i2][:, dc, ti * 128:(ti + 1) * 128],
                                                      rhs=wout[:, dc, hf * 512:(hf + 1) * 512], start=(dc == 0), stop=(dc == 7)),
                             reads=[('mT', i2), 'wout'], writes=['pyo'])
                S.op('dve', lambda e: e.tensor_tensor(out=x1[k2][:], in0=pyo[:], in1=bc_g1[:], op=ALU.mult),
                     reads=['pyo', 'bc_g1'], writes=[('x1', k2)])
                S.op('dve', lambda e: e.tensor_tensor(out=x1[k2][:], in0=x1[k2][:], in1=xt[kx][:], op=ALU.add),
                     reads=[('x1', k2), ('xt', kx)], writes=[('x1', k2)])
                S.dma('pool', out[tok0:tok0 + 128, :], x1[k2][:], reads=[('x1', k2)], writes=['out'])
                s_ = st[k2]; sk = ('st', k2)
                S.op('act', lambda e: e.activation(out=junk[:], in_=x1[k2][:], func=AF.Square, accum_out=s_[:, 0:1]),
                     reads=[('x1', k2)], writes=['mjunk', sk])
                S.op('act', lambda e: e.activation(out=s_[:, 6:7], in_=s_[:, 0:1], func=AF.Ln, scale=1.0 / D, bias=epsc[:, 0:1]),
                     reads=[sk, 'epsc'], writes=[sk])
                S.op('act', lambda e: e.activation(out=s_[:, 1:2], in_=s_[:, 6:7], func=AF.Exp, scale=-0.5), reads=[sk], writes=[sk])
                S.op('dve', lambda e: e.scalar_tensor_tensor(out=h2[k2][:], in0=x1[k2][:], scalar=s_[:, 1:2], in1=bc_mul2[:],
                                                             op0=ALU.mult, op1=ALU.mult), reads=[('x1', k2), sk, 'bc_mul2'], writes=[('h2', k2)])
                S.op('dve', lambda e: e.tensor_tensor(out=h2[k2][:], in0=h2[k2][:], in1=bc_add2[:], op=ALU.add),
                     reads=[('h2', k2), 'bc_add2'], writes=[('h2', k2)])
                S.op('act', lambda e: e.activation(out=h2b[k2][:], in_=h2[k2][:], func=AF.Copy), reads=[('h2', k2)], writes=[('h2b', k2)])
                S.dma('act', h2_d[tok0:tok0 + 128, :], h2b[k2][:], reads=[('h2b', k2)], writes=['h2_d'])
                def mk_tail(k2=k2, tok0=tok0, s_=s_, sk=sk):
                    def tail():
                        for hh in range(2):
                            for dc in range(4):
                                S.op('pe', lambda e: e.transpose(out=phT[:, dc, :], in_=h2[k2][:, (4 * hh + dc) * 128:(4 * hh + dc + 1) * 128],
                                                                 identity=identf[:]), reads=[('h2', k2), 'identf'], writes=['phT'])
                            S.op('act', lambda e: e.activation(out=h2T[k2][:, 4 * hh:4 * hh + 4, :], in_=phT[:], func=AF.Copy),
                                 reads=['phT'], writes=[('h2T', k2)])
                        for dc in range(8):
                            S.op('pe', lambda e: e.matmul(plg, lhsT=h2T[k2][:, dc, :], rhs=wr[:, dc, :], start=(dc == 0), stop=(dc == 7)),
                                 reads=[('h2T', k2), 'wr'], writes=['plg'])
                        S.op('dve', lambda e: e.tensor_copy(out=lg[k2][:], in_=plg), reads=['plg'], writes=[('lg', k2)])
                        S.op('dve', lambda e: e.reduce_max(out=s_[:, 2:3], in_=lg[k2][:], axis=AX.X), reads=[('lg', k2)], writes=[sk])
                        S.op('dve', lambda e: e.tensor_scalar(out=s_[:, 3:4], in0=s_[:, 2:3], scalar1=-1.0, scalar2=None, op0=ALU.mult),
                             reads=[sk], writes=[sk])
                        S.op('act', lambda e: e.activation(out=af[k2][:], in_=lg[k2][:], func=AF.Exp, bias=s_[:, 3:4], accum_out=s_[:, 4:5]),
                             reads=[('lg', k2), sk], writes=[('af', k2), sk])
                        S.op('dve', lambda e: e.reciprocal(out=s_[:, 5:6], in_=s_[:, 4:5]), reads=[sk], writes=[sk])
                        S.op('dve', lambda e: e.tensor_scalar(out=af[k2][:], in0=af[k2][:], scalar1=s_[:, 5:6], scalar2=None, op0=ALU.mult),
                             reads=[('af', k2), sk], writes=[('af', k2)])
                        S.dma('pool', aff_d[tok0:tok0 + 128, :], af[k2][:], reads=[('af', k2)], writes=['aff_d'])
                        S.op('pe', lambda e: e.transpose(out=paT, in_=af[k2][:], identity=identf[:]), reads=[('af', k2), 'identf'], writes=['paT'])
                        S.op('act', lambda e: e.activation(out=A16[:, tok0:tok0 + 128], in_=paT, func=AF.Copy), reads=['paT'], writes=['A16'])
                    return tail
                if len(pend) == 2:
                    pend.pop(0)()
                pend.append(mk_tail())
        for t_ in pend:
            t_()
        S.dma('sp', affT_d, A16[:], reads=['A16'], writes=['affT_d'])
        S.barrier()
        if 'mg2' in dbg:
            for nm, t, shp, dt in (('bcg1', bc_g1, [128, D], F32), ('mT0', mT[(nblocks - 1) % 2], [128, 8, 512], BF16), ('t1', t1[1], [128, 512], F32),
                                   ('sga0', sga[(nblocks - 1) % 2], [128, 8, 512], BF16), ('ya0', ya[(nblocks - 1) % 2], [128, 4, 512], BF16),
                                   ('yb0', yb[(nblocks - 1) % 2], [128, 4, 512], BF16), ('wpa', wpa, [128, 4, D], BF16), ('x1t', x1[1], [128, D], F32),
                                   ('xt', xt[1], [128, D], F32)):
                dbg_out[nm] = dout("dbg_" + nm, shp, dt)
                S.dma('sp', dbg_out[nm], t[:], reads=[])
    if 'mg' in dbg:
        dbg_out['aff'] = dout("dbg_aff", [8192, 16])
        S.dma('sp', dbg_out['aff'], aff_d, reads=['aff_d'])
        dbg_out['affT'] = dout("dbg_affT", [16, 8192])
        S.dma('sp', dbg_out['affT'], affT_d, reads=['affT_d'])
        dbg_out['h2'] = dout("dbg_h2", [8192, D], BF16)
        for r0 in range(0, 8192, 1024):
            S.dma('sp', dbg_out['h2'][r0:r0 + 1024, :], h2_d[r0:r0 + 1024, :], reads=['h2_d'])

D = 1024
CAP = 1024
NT = 8192


def moe_phase(nc, S, es, sb, ps, din, dscr, dout, dbg, dbg_out, identf, identb, bc_g2, out, h2_d, aff_d, affT_d, nexp=16):
    wg_in = din("w_e_gate", [16, D, D]); wu_in = din("w_e_up", [16, D, D]); wd_in = din("w_e_down", [16, D, D])
    m16_in = din("m16", [128, 128])
    with ExitStack() as pm:
        cum_d = dscr("cum_d", [16, NT], F32)
        slotv = sb("e_slotv", [128, 8], F32, pm)
        thr_dbg = sb("e_thr", [128, 1], F32, pm)
        S.op('pool', lambda e: e.iota(slotv[:], pattern=[[128, 8]], base=0, channel_multiplier=1, allow_small_or_imprecise_dtypes=True),
             writes=['slotv'])
        with ExitStack() as pa:
            A128 = sb("e_A128", [128, 1024], F32, pa)
            A16 = sb("e_A16", [16, NT], F32, pa)
            C16 = sb("e_C16", [16, NT], F32, pa)
            M16 = sb("e_M16", [128, 128], F32, pa)
            junk = sb("e_junk", [128, 1024], BF16, pa)
            lo = sb("e_lo", [128, 1], F32, pa); mid = sb("e_mid", [128, 1], F32, pa)
            cnt = sb("e_cnt", [128, 1], F32, pa); g = sb("e_g", [128, 1], F32, pa)
            one16 = sb("e_one16", [16, 1], F32, pa)
            ptot = ps("e_ptot", [128, 1], F32, pa)
            for s in range(8):
                S.dma('sp', A128[16 * s:16 * s + 16, :], affT_d[:, 1024 * s:1024 * s + 1024], reads=['affT_d'], writes=['A128'])
            S.dma('sp', A16[:], affT_d, reads=['affT_d'], writes=['A16'])
            S.dma('sp', M16[:], m16_in, writes=['M16'])
            S.op('dve', lambda e: e.memset(lo[:], 0.0), writes=['lo'])
            S.op('dve', lambda e: e.memset(one16[:], 1.0), writes=['one16'])
            for k in range(30):
                hk = 2.0 ** -(k + 1)
                S.op('dve', lambda e: e.tensor_scalar(out=mid[:], in0=lo[:], scalar1=hk, scalar2=None, op0=ALU.add), reads=['lo'], writes=['mid'])
                S.op('dve', lambda e: e.tensor_scalar(out=junk[:], in0=A128[:], scalar1=mid[:, 0:1], scalar2=0.0, op0=ALU.is_gt, op1=ALU.add,
                                                      accum_out=cnt[:]), reads=['A128', 'mid'], writes=['junk', 'cnt'])
                S.op('pe', lambda e: e.matmul(ptot[:], lhsT=M16[:], rhs=cnt[:], start=True, stop=True), reads=['M16', 'cnt'], writes=['ptot'])
                S.op('dve', lambda e: e.tensor_scalar(out=g[:], in0=ptot[:], scalar1=float(CAP), scalar2=hk, op0=ALU.is_ge, op1=ALU.mult),
                     reads=['ptot'], writes=['g'])
                S.op('dve', lambda e: e.tensor_tensor(out=lo[:], in0=lo[:], in1=g[:], op=ALU.add), reads=['lo', 'g'], writes=['lo'])
            S.op('dve', lambda e: e.tensor_copy(out=thr_dbg[:], in_=lo[:]), reads=['lo'], writes=['thr'])
            S.op('dve', lambda e: e.tensor_scalar(out=A16[:], in0=A16[:], scalar1=lo[0:16, 0:1], scalar2=None, op0=ALU.is_gt),
                 reads=['A16', 'lo'], writes=['A16'])
            S.op('dve', lambda e: e.tensor_tensor_scan(out=C16[:], data0=one16[:, 0:1].to_broadcast([16, NT]), data1=A16[:], initial=0.0,
                                                       op0=ALU.mult, op1=ALU.add), reads=['A16', 'one16'], writes=['C16'])
            S.dma('sp', cum_d, C16[:], reads=['C16'], writes=['cum_d'])
            S.barrier()
        if 'moe_thr' in dbg:
            dbg_out['thr'] = dout("dbg_thr", [128, 1])
            S.dma('sp', dbg_out['thr'], thr_dbg[:], reads=['thr'])
        with ExitStack() as pe_:
            Wg = [sb("e_Wg%d" % i, [128, 8, D], BF16, pe_) for i in range(2)]
            Wu = [sb("e_Wu%d" % i, [128, 8, D], BF16, pe_) for i in range(2)]
            Wd = [sb("e_Wd%d" % i, [128, 8, D], BF16, pe_) for i in range(2)]
            xe = [sb("e_xe%d" % i, [128, D], BF16, pe_) for i in range(8)]
            ag = [[sb("e_ag%d_%d" % (p_, i), [128, 16], F32, pe_) for i in range(8)] for p_ in range(2)]
            cq = [sb("e_cq%d" % i, [128, 2048], F32, pe_) for i in range(2)]
            junkq = sb("e_junkq", [128, 2048], BF16, pe_)
            acc = [sb("e_acc%d" % i, [128, 4, 8], F32, pe_) for i in range(3)]
            idxs = sb("e_idxs", [128, 8], F32, pe_)
            idxi = [sb("e_idxi%d" % i, [128, 8], I32, pe_) for i in range(3)]
            xeT = sb("e_xeT", [128, 8, CAP], BF16, pe_)
            hidT = sb("e_hidT", [128, 8, CAP], BF16, pe_)
            sg = [sb("e_sg%d" % i, [128, 512], F32, pe_) for i in range(2)]
            ye = [sb("e_ye%d" % i, [128, D], F32, pe_) for i in range(2)]
            pGU = [ps("e_pGU%d" % i, [128, 512], F32, pe_) for i in range(3)]
            pY = ps("e_pY", [128, D], F32, pe_)
            pT = ps("e_pT", [128, 8, 128], BF16, pe_)
            pTi = pT[:].bitcast(F32) if False else None

            def load_w(e):
                i2 = e % 2
                for dc in range(8):
                    S.dma('pool', Wg[i2][:, dc, :], wg_in[e, dc * 128:(dc + 1) * 128, :], writes=[('Wg', i2)])
                    S.dma('pool', Wu[i2][:, dc, :], wu_in[e, dc * 128:(dc + 1) * 128, :], writes=[('Wu', i2)])
                for dc in range(8):
                    S.dma('pool', Wd[i2][:, dc, :], wd_in[e, dc * 128:(dc + 1) * 128, :], writes=[('Wd', i2)])

            def load_cq(n):
                e_, qd = n // 4, n % 4
                if e_ >= nexp:
                    return
                S.dma('sp', cq[n % 2][:], cum_d[e_:e_ + 1, qd * 2048:(qd + 1) * 2048].to_broadcast([128, 2048]),
                      reads=['cum_d'], writes=[('cq', n % 2)])

            def idx_piece(e, piece):
                j3 = e % 3
                n = e * 4 + piece // 2
                if piece % 2 == 0:
                    if n == 0:
                        load_cq(0)
                    load_cq(n + 1)
                for sc in range(4 * (piece % 2), 4 * (piece % 2) + 4):
                    S.op('dve', lambda e_: e_.tensor_scalar(out=junkq[:], in0=cq[n % 2][:], scalar1=slotv[:, sc:sc + 1], scalar2=0.0,
                                                            op0=ALU.is_le, op1=ALU.add, accum_out=acc[j3][:, piece // 2, sc:sc + 1]),
                         reads=[('cq', n % 2), 'slotv'], writes=['junkq', ('acc', j3)])
                if piece == 7:
                    S.op('dve', lambda e_: e_.tensor_tensor(out=idxs[:], in0=acc[j3][:, 0, :], in1=acc[j3][:, 1, :], op=ALU.add),
                         reads=[('acc', j3)], writes=['idxs'])
                    S.op('dve', lambda e_: e_.tensor_tensor(out=idxs[:], in0=idxs[:], in1=acc[j3][:, 2, :], op=ALU.add),
                         reads=[('acc', j3), 'idxs'], writes=['idxs'])
                    S.op('dve', lambda e_: e_.tensor_tensor(out=idxs[:], in0=idxs[:], in1=acc[j3][:, 3, :], op=ALU.add),
                         reads=[('acc', j3), 'idxs'], writes=['idxs'])
                    S.op('dve', lambda e_: e_.tensor_copy(out=idxi[j3][:], in_=idxs[:]), reads=['idxs'], writes=[('idxi', j3)])

            def gather(e):
                j2 = e % 3
                for sc in range(8):
                    S.custom_dma('pool', lambda e_: e_.indirect_dma_start(out=xe[sc][:, :], out_offset=None, in_=h2_d[:, :],
                                 in_offset=bass.IndirectOffsetOnAxis(ap=idxi[j2][:, sc:sc + 1], axis=0)),
                                 reads=[('idxi', j2), 'h2_d'], writes=[('xe', sc)])
                    S.custom_dma('pool', lambda e_: e_.indirect_dma_start(out=ag[e % 2][sc][:, :], out_offset=None, in_=aff_d[:, :],
                                 in_offset=bass.IndirectOffsetOnAxis(ap=idxi[j2][:, sc:sc + 1], axis=0)),
                                 reads=[('idxi', j2), 'aff_d'], writes=[('ag', e % 2, sc)])

            gu_ctr = [0]

            def xpose(e):
                for sc in range(8):
                    for dc in range(8):
                        S.op('pe', lambda e_: e_.transpose(out=pT[:, dc, :], in_=xe[sc][:, dc * 128:(dc + 1) * 128], identity=identb[:]),
                             reads=[('xe', sc), 'identb'], writes=['pT'])
                    S.op('act', lambda e_: e_.activation(out=xeT[:, :, sc * 128:(sc + 1) * 128], in_=pT[:], func=AF.Copy),
                         reads=['pT'], writes=['xeT'])
            def ffn(e, nxt):
                i2 = e % 2; j2 = e % 3
                for fc in range(8):
                    for hf in range(2):
                        a = gu_ctr[0] % 3; gu_ctr[0] += 1
                        b = gu_ctr[0] % 3; gu_ctr[0] += 1
                        for dc in range(8):
                            S.op('pe', lambda e_: e_.matmul(pGU[a][:], lhsT=Wg[i2][:, dc, fc * 128:(fc + 1) * 128], rhs=xeT[:, dc, hf * 512:(hf + 1) * 512],
                                                            start=(dc == 0), stop=(dc == 7)), reads=[('Wg', i2), 'xeT'], writes=[('pGU', a)])
                        for dc in range(8):
                            S.op('pe', lambda e_: e_.matmul(pGU[b][:], lhsT=Wu[i2][:, dc, fc * 128:(fc + 1) * 128], rhs=xeT[:, dc, hf * 512:(hf + 1) * 512],
                                                            start=(dc == 0), stop=(dc == 7)), reads=[('Wu', i2), 'xeT'], writes=[('pGU', b)])
                        s2 = (fc * 2 + hf) % 2
                        S.op('act', lambda e_: e_.activation(out=sg[s2][:], in_=pGU[a][:], func=AF.Silu), reads=[('pGU', a)], writes=[('sg', s2)])
                        S.op('dve', lambda e_: e_.tensor_tensor(out=hidT[:, fc, hf * 512:(hf + 1) * 512], in0=pGU[b][:], in1=sg[s2][:], op=ALU.mult),
                             reads=[('pGU', b), ('sg', s2)], writes=['hidT'])
                    if nxt is not None:
                        idx_piece(nxt, fc)
                for sc in range(8):
                    for hf in range(2):
                        for fc in range(8):
                            S.op('pe', lambda e_: e_.matmul(pY[:, hf * 512:(hf + 1) * 512], lhsT=hidT[:, fc, sc * 128:(sc + 1) * 128],
                                                            rhs=Wd[i2][:, fc, hf * 512:(hf + 1) * 512], start=(fc == 0), stop=(fc == 7)),
                                 reads=['hidT', ('Wd', i2)], writes=['pY'])
                    y2 = sc % 2
                    S.op('dve', lambda e_: e_.scalar_tensor_tensor(out=ye[y2][:], in0=pY[:], scalar=ag[e % 2][sc][:, e:e + 1], in1=bc_g2[:],
                                                                   op0=ALU.mult, op1=ALU.mult), reads=['pY', ('ag', e % 2, sc), 'bc_g2'], writes=[('ye', y2)])
                    S.custom_dma('pool', lambda e_: e_.indirect_dma_start(out=out[:, :], out_offset=bass.IndirectOffsetOnAxis(ap=idxi[j2][:, sc:sc + 1], axis=0),
                                 in_=ye[y2][:, :], in_offset=None, compute_op=ALU.add), reads=[('ye', y2), ('idxi', j2)], writes=['out'])

            load_w(0)
            for p in range(8):
                idx_piece(0, p)
            gather(0)
            if nexp > 1:
                load_w(1)
                for p in range(8):
                    idx_piece(1, p)
            for e in range(nexp):
                xpose(e)
                if e + 1 < nexp:
                    gather(e + 1)
                    if e >= 1:
                        load_w(e + 1)
                ffn(e, e + 2 if e + 2 < nexp else None)
            if 'moe_idx' in dbg:
                dbg_out['idxi'] = dout("dbg_idxi", [128, 8], I32)
                S.dma('sp', dbg_out['idxi'], idxi[(nexp - 1) % 3][:], reads=[('idxi', (nexp - 1) % 3)])
            S.barrier()


def moe_host(inp):
    f = np.float32
    m16 = np.zeros((128, 128), f)
    for s in range(8):
        for s2 in range(8):
            m16[16 * s:16 * s + 16, 16 * s2:16 * s2 + 16] = np.eye(16, dtype=f)
    return {'w_e_gate': np.ascontiguousarray(inp['w_e_gate'][0]), 'w_e_up': np.ascontiguousarray(inp['w_e_up'][0]),
            'w_e_down': np.ascontiguousarray(inp['w_e_down'][0]), 'm16': m16}


D = 1024
NLAT = 8192
NCTX = 256
NEXT = NLAT + NCTX
NE2 = NEXT + NCTX
EPS = 1e-6
INCOLS = 4096


def build(stop_after=99, dbg=(), nblk_lim=17, skip_s5=False, mg_blocks=16, nexp=16):
    nc = bass.Bass("TRN2", target_bir_lowering=False)
    es = ExitStack()
    S = Sched(nc, es)
    dbg_out = {}

    def din(name, shape, dt=F32):
        return nc.dram_tensor(name, list(shape), dt, kind="ExternalInput").ap()

    def dscr(name, shape, dt):
        return nc.dram_tensor(name, list(shape), dt, kind="Internal").ap()

    def dout(name, shape, dt=F32):
        return nc.dram_tensor(name, list(shape), dt, kind="ExternalOutput").ap()

    def sb(name, shape, dt, stack=None):
        return (stack or es).enter_context(nc.sbuf_tensor(name, list(shape), dt))

    def ps(name, shape, dt, stack=None):
        return (stack or es).enter_context(nc.psum_tensor(name, list(shape), dt))

    xcat = din("xcat", [NEXT, D])
    cT = din("cT", [128, 8, 2])
    w_ada = din("w_ada", [D, 6 * D])
    b_ada = din("b_ada", [1, 6 * D])
    nmixT = din("nmixT", [128, 8])
    nffn = din("nffn", [1, D])
    w_in = din("w_in", [D, INCOLS])
    ident_in = din("ident", [128, 128])
    qkg = din("qkg", [128, 2])

    out = dout("out", [NLAT, D])

    uT_d = dscr("uT_d", [512, NE2], BF16)
    qT_d = dscr("qT_d", [512, NLAT], BF16)
    kT_d = dscr("kT_d", [512, NEXT], BF16)
    v_d = dscr("v_d", [NEXT, 8, 65], BF16)
    sgaT_d = dscr("sgaT_d", [D, NLAT], BF16)
    sgbT_d = dscr("sgbT_d", [D, NLAT], BF16)

    identf = sb("identf", [128, 128], F32)
    identb = sb("identb", [128, 128], BF16)
    mul1 = sb("mul1", [128, 8, 2], F32)
    add1 = sb("add1", [128, 8, 2], F32)
    bc_g1 = sb("bc_g1", [128, D], F32)
    bc_mul2 = sb("bc_mul2", [128, D], F32)
    bc_add2 = sb("bc_add2", [128, D], F32)
    bc_g2 = sb("bc_g2", [128, D], F32)

    epsc = sb("epsc", [128, 1], F32)
    S.op('dve', lambda e: e.memset(epsc[:], EPS), writes=['epsc'])
    S.dma('sp', identf[:], ident_in, writes=['identf'])
    S.dma('pool', identb[:], ident_in, writes=['identb'])

    with ExitStack() as p0:
        wada = sb("wada", [128, 8, 6 * D], BF16, p0)
        cs_f = sb("cs_f", [128, 8, 2], F32, p0)
        cs = sb("cs", [128, 8, 2], BF16, p0)
        brow = sb("brow", [2, 6 * D], F32, p0)
        modrow = sb("modrow", [2, 6 * D], F32, p0)
        sel = sb("sel", [2, 128], F32, p0)
        nmix = sb("nmix", [128, 8], F32, p0)
        colsb = sb("colsb", [128, 16, 2], F32, p0)
        pm = [ps("pm%d" % i, [2, 512], F32, p0) for i in range(2)]
        pcol = ps("pcol", [128, 16, 2], F32, p0)
        pbc = [ps("pbc%d" % i, [128, 512], F32, p0) for i in range(2)]

        S.dma('sp', cs_f[:], cT, writes=['cs_f'])
        S.dma('sp', brow[0:1, :], b_ada, writes=['brow0'])
        S.dma('sp', brow[1:2, :], b_ada, writes=['brow1'])
        S.dma('sp', nmix[:], nmixT, writes=['nmix'])
        S.dma('sp', bc_mul2[:], nffn.to_broadcast([128, D]), writes=['bc_mul2'])
        for dc in range(8):
            S.dma('pool', wada[:, dc, :].rearrange("p (a b) -> p a b", b=2048),
                  w_ada[dc * 128:(dc + 1) * 128, :].rearrange("p (a b) -> p a b", b=2048),
                  writes=[('wada', dc)])
        S.op('act', lambda e: e.activation(out=cs[:], in_=cs_f[:], func=AF.Silu), reads=['cs_f'], writes=['cs'])
        S.op('dve', lambda e: e.memset(sel[:], 0.0), writes=['sel'])
        S.op('dve', lambda e: e.memset(sel[0:1, :], 1.0), writes=['sel'])
        for cc in range(12):
            pt = pm[cc % 2]
            for dc in range(8):
                S.op('pe', lambda e: e.matmul(pt[:], lhsT=cs[:, dc, :], rhs=wada[:, dc, cc * 512:(cc + 1) * 512],
                                              start=(dc == 0), stop=(dc == 7)),
                     reads=['cs', ('wada', dc)], writes=[('pm', cc % 2)])
            S.op('dve', lambda e: e.tensor_tensor(out=modrow[:, cc * 512:(cc + 1) * 512], in0=pt[:],
                                                  in1=brow[:, cc * 512:(cc + 1) * 512], op=ALU.add),
                 reads=[('pm', cc % 2), 'brow0', 'brow1'], writes=[('modrow', cc)])
        for i in range(16):
            S.op('pe', lambda e: e.transpose(out=pcol[:, i, :], in_=modrow[0:2, i * 128:(i + 1) * 128],
                                             identity=identf[0:2, 0:2]),
                 reads=[('modrow', i // 4), 'identf'], writes=['pcol'])
        S.op('dve', lambda e: e.tensor_copy(out=colsb[:], in_=pcol[:]), reads=['pcol'], writes=['colsb'])
        S.op('dve', lambda e: e.tensor_copy(out=add1[:], in_=colsb[:, 0:8, :]), reads=['colsb'], writes=['add1'])
        S.op('dve', lambda e: e.tensor_scalar(out=colsb[:, 8:16, :], in0=colsb[:, 8:16, :], scalar1=1.0, scalar2=None,
                                              op0=ALU.add), reads=['colsb'], writes=['colsb'])
        S.op('dve', lambda e: e.tensor_tensor(out=mul1[:], in0=colsb[:, 8:16, :],
                                              in1=nmix[:].unsqueeze(2).to_broadcast([128, 8, 2]), op=ALU.mult),
             reads=['colsb', 'nmix'], writes=['mul1'])

        def bcast(dst, col0, mode, dkey):
            for h in range(2):
                pt = pbc[h]
                S.op('pe', lambda e: e.matmul(pt[:], lhsT=sel[:], rhs=modrow[:, col0 + h * 512: col0 + (h + 1) * 512],
                                              start=True, stop=True),
                     reads=['sel'] + [('modrow', c) for c in range(12)], writes=[('pbc', h)])
                dsl = dst[:, h * 512:(h + 1) * 512]
                if mode == 'copy':
                    S.op('dve', lambda e: e.tensor_copy(out=dsl, in_=pt[:]), reads=[('pbc', h)], writes=[dkey])
                else:
                    S.op('dve', lambda e: e.scalar_tensor_tensor(out=dsl, in0=pt[:], scalar=1.0, in1=dsl,
                                                                 op0=ALU.add, op1=ALU.mult),
                         reads=[('pbc', h), dkey], writes=[dkey])
        bcast(bc_g1, 2048, 'copy', 'bc_g1')
        bcast(bc_add2, 3072, 'copy', 'bc_add2')
        bcast(bc_mul2, 4096, 'mul1p', 'bc_mul2')
        bcast(bc_g2, 5120, 'copy', 'bc_g2')
        if 'mod' in dbg:
            dbg_out['mod'] = dout("dbg_mod", [2, 6 * D])
            S.dma('sp', dbg_out['mod'], modrow[:], reads=[('modrow', c) for c in range(12)])
            dbg_out['mul1'] = dout("dbg_mul1", [128, 8, 2])
            S.dma('sp', dbg_out['mul1'], mul1[:], reads=['mul1'])
            dbg_out['bcm2'] = dout("dbg_bcm2", [128, D])
            S.dma('sp', dbg_out['bcm2'], bc_mul2[:], reads=['bc_mul2'])
        S.barrier()

    if stop_after <= 0:
        return finish(nc, S, es, out, dbg_out)

    with ExitStack() as p1:
        win = sb("win", [128, 8, INCOLS], BF16, p1)
        xt = [sb("xt%d" % i, [128, D], F32, p1) for i in range(2)]
        junk = sb("junk", [128, D], BF16, p1)
        xn = [sb("xn%d" % i, [128, D], BF16, p1) for i in range(2)]
        ss = [sb("ss%d" % i, [128, 1], F32, p1) for i in range(2)]
        rstd = [sb("rstd%d" % i, [128, 1], F32, p1) for i in range(2)]
        hT = [sb("hT%d" % i, [128, 8, 512], BF16, p1) for i in range(2)]
        stg = [sb("stg%d" % i, [128, 512], BF16, p1) for i in range(4)]
        sq = [sb("sq%d" % i, [128, 512], BF16, p1) for i in range(2)]
        qk32 = [sb("qk32_%d" % i, [128, 512], F32, p1) for i in range(2)]
        rs = [sb("rs%d" % i, [128, 512], F32, p1) for i in range(2)]
        vst = [sb("vst%d" % i, [128, 8, 65], BF16, p1) for i in range(2)]
        bones = sb("bones", [128, 128], BF16, p1)
        gq = sb("gq", [128, 2], F32, p1)
        pT = [ps("pT%d" % i, [128, 8, 128], BF16, p1) for i in range(2)]
        pz = [ps("pz%d" % i, [128, 512], F32, p1) for i in range(4)]
        pms = [ps("pms%d" % i, [128, 512], F32, p1) for i in range(2)]

        for dc in range(8):
            S.dma('pool', win[:, dc, :].rearrange("p (a b) -> p a b", b=2048),
                  w_in[dc * 128:(dc + 1) * 128, :].rearrange("p (a b) -> p a b", b=2048),
                  writes=[('win', dc)])
        S.dma('sp', gq[:], qkg, writes=['gq'])
        for i in range(2):
            S.op('dve', lambda e: e.memset(vst[i][:], 1.0), writes=[('vst', i)])
        v_ctr = 0
        S.op('dve', lambda e: e.memset(bones[:], 0.0), writes=['bones'])
        S.op('dve', lambda e: e.memset(bones[0:64, 0:64], 1.0 / 64), writes=['bones'])
        S.op('dve', lambda e: e.memset(bones[64:128, 64:128], 1.0 / 64), writes=['bones'])

        nblk = (NEXT + 511) // 512
        tile_ctr = 0
        stg_ctr = 0
        z_ctr = 0
        for b in range(nblk_lim):
            if b == 0:
                t0, nt = 0, 256
            else:
                t0, nt = 256 + (b - 1) * 512, 512
            is_ctx = (b == 0)
            j = 1 if is_ctx else 0
            hb = hT[b % 2]
            hkey = ('hT', b % 2)
            for ti in range(nt // 128):
                i2 = tile_ctr % 2
                tile_ctr += 1
                r0 = t0 + ti * 128
                S.dma('sp', xt[i2][:], xcat[r0:r0 + 128, :], writes=[('xt', i2)])
                S.op('act', lambda e: e.activation(out=junk[:], in_=xt[i2][:], func=AF.Square, accum_out=ss[i2][:]),
                     reads=[('xt', i2)], writes=['junk', ('ss', i2)])
                S.op('act', lambda e: e.activation(out=rstd[i2][:], in_=ss[i2][:], func=AF.Sqrt, scale=1.0 / D, bias=epsc[:, 0:1]),
                     reads=[('ss', i2), 'epsc'], writes=[('rstd', i2)])
                S.op('dve', lambda e: e.reciprocal(out=rstd[i2][:], in_=rstd[i2][:]),
                     reads=[('rstd', i2)], writes=[('rstd', i2)])
                S.op('act', lambda e: e.activation(out=xn[i2][:], in_=xt[i2][:], func=AF.Copy, scale=rstd[i2][:, 0:1]),
                     reads=[('xt', i2), ('rstd', i2)], writes=[('xn', i2)])
                for dc in range(8):
                    S.op('pe', lambda e: e.transpose(out=pT[i2][:, dc, :], in_=xn[i2][:, dc * 128:(dc + 1) * 128],
                                                     identity=identb[:]),
                         reads=[('xn', i2), 'identb'], writes=[('pT', i2)])
                for dc in range(8):
                    S.op('dve', lambda e: e.tensor_scalar(out=hb[:, dc, ti * 128:(ti + 1) * 128], in0=pT[i2][:, dc, :],
                                                          scalar1=mul1[:, dc, j:j + 1], scalar2=add1[:, dc, j:j + 1],
                                                          op0=ALU.mult, op1=ALU.add),
                         reads=[('pT', i2), 'mul1', 'add1'], writes=[hkey])
            if is_ctx:
                ccs = list(range(0, 4)) + list(range(8, 12))
            else:
                ccs = list(range(0, 12)) + list(range(16, 32))
            for cc in ccs:
                zi = z_ctr % 4
                z_ctr += 1
                pzt = pz[zi]
                for dc in range(8):
                    S.op('pe', lambda e: e.matmul(pzt[:, 0:nt], lhsT=win[:, dc, cc * 128:(cc + 1) * 128], rhs=hb[:, dc, 0:nt],
                                                  start=(dc == 0), stop=(dc == 7)),
                         reads=[('win', dc), hkey], writes=[('pz', zi)], sig=(dc == 7))
                si = stg_ctr % 4
                stg_ctr += 1
                st = stg[si]
                skey = ('stg', si)
                if cc < 4:
                    S.op('act', lambda e: e.activation(out=st[:, 0:nt], in_=pzt[:, 0:nt], func=AF.Copy),
                         reads=[('pz', zi)], writes=[skey])
                    S.dma('sp', uT_d[cc * 128:(cc + 1) * 128, t0:t0 + nt], st[:, 0:nt], reads=[skey], writes=['uT_d'])
                    if is_ctx:
                        S.dma('sp', uT_d[cc * 128:(cc + 1) * 128, NEXT:NEXT + nt], st[:, 0:nt], reads=[skey], writes=['uT_d'])
                elif cc < 12:
                    isq = cc < 8
                    qi = z_ctr % 2
                    S.op('act', lambda e: e.activation(out=sq[qi][:, 0:nt], in_=pzt[:, 0:nt], func=AF.Square),
                         reads=[('pz', zi)], writes=[('sq', qi)])
                    S.op('pe', lambda e: e.matmul(pms[qi][:, 0:nt], lhsT=bones[:], rhs=sq[qi][:, 0:nt], start=True, stop=True),
                         reads=['bones', ('sq', qi)], writes=[('pms', qi)])
                    S.op('act', lambda e: e.activation(out=rs[qi][:, 0:nt], in_=pms[qi][:, 0:nt], func=AF.Sqrt, bias=epsc[:, 0:1]),
                         reads=[('pms', qi), 'epsc'], writes=[('rs', qi)])
                    S.op('dve', lambda e: e.reciprocal(out=rs[qi][:, 0:nt], in_=rs[qi][:, 0:nt]),
                         reads=[('rs', qi)], writes=[('rs', qi)])
                    S.op('dve', lambda e: e.scalar_tensor_tensor(out=qk32[qi][:, 0:nt], in0=pzt[:, 0:nt], scalar=(0.125 if isq else 1.0),
                                                                 in1=rs[qi][:, 0:nt], op0=ALU.mult, op1=ALU.mult),
                         reads=[('pz', zi), ('rs', qi)], writes=[('qk32', qi)])
                    gcol = gq[:, 0:1] if isq else gq[:, 1:2]
                    S.op('act', lambda e: e.activation(out=st[:, 0:nt], in_=qk32[qi][:, 0:nt], func=AF.Copy, scale=gcol),
                         reads=[('qk32', qi), 'gq'], writes=[skey])
                    if isq:
                        S.dma('sp', qT_d[(cc - 4) * 128:(cc - 3) * 128, t0 - NCTX:t0 - NCTX + nt], st[:, 0:nt],
                              reads=[skey], writes=['qT_d'])
                    else:
                        S.dma('sp', kT_d[(cc - 8) * 128:(cc - 7) * 128, t0:t0 + nt], st[:, 0:nt], reads=[skey], writes=['kT_d'])
                else:
                    S.op('act', lambda e: e.activation(out=st[:, 0:nt], in_=pzt[:, 0:nt], func=AF.Sigmoid),
                         reads=[('pz', zi)], writes=[skey])
                    if cc < 24:
                        dst = sgaT_d[(cc - 16) * 128:(cc - 15) * 128, t0 - NCTX:t0 - NCTX + nt]
                        dk = 'sgaT_d'
                    else:
                        dst = sgbT_d[(cc - 24) * 128:(cc - 23) * 128, t0 - NCTX:t0 - NCTX + nt]
                        dk = 'sgbT_d'
                    S.dma('sp', dst, st[:, 0:nt], reads=[skey], writes=[dk])
            for ti in range(nt // 128):
                zi = z_ctr % 4
                z_ctr += 1
                pzt = pz[zi]
                for dc in range(8):
                    S.op('pe', lambda e: e.matmul(pzt[:], lhsT=hb[:, dc, ti * 128:(ti + 1) * 128], rhs=win[:, dc, 1536:2048],
                                                  start=(dc == 0), stop=(dc == 7)),
                         reads=[('win', dc), hkey], writes=[('pz', zi)], sig=(dc == 7))
                vi = v_ctr % 2
                v_ctr += 1
                S.op('act', lambda e: e.activation(out=vst[vi][:, :, 0:64], in_=pzt[:].rearrange("p (h d) -> p h d", d=64), func=AF.Copy),
                     reads=[('pz', zi)], writes=[('vst', vi)])
                S.dma('sp', v_d[t0 + ti * 128:t0 + (ti + 1) * 128], vst[vi][:], reads=[('vst', vi)], writes=['v_d'])
        S.barrier()
    if 'p1' in dbg:
        for nm, t in (('uT', uT_d), ('qT', qT_d), ('kT', kT_d), ('sgaT', sgaT_d), ('sgbT', sgbT_d)):
            dbg_out[nm] = dout("dbg_" + nm, list(t.shape), BF16)
            for r0 in range(0, t.shape[0], 128):
                S.dma('sp', dbg_out[nm][r0:r0 + 128, :], t[r0:r0 + 128, :], reads=[])
    if stop_after <= 1:
        return finish(nc, S, es, out, dbg_out)
    yaT_d = dscr("yaT_d", [512, NLAT], BF16)
    r = 'ok' if skip_s5 else s5_phase(nc, S, es, sb, ps, din, dscr, dout, dbg, dbg_out, identf, identb, uT_d, yaT_d)
    if r == 'stop' or stop_after <= 2:
        return finish(nc, S, es, out, dbg_out)
    ybT_d = dscr("ybT_d", [512, NLAT], BF16)
    na_phase(nc, S, es, sb, ps, din, dscr, dout, dbg, dbg_out, identb, qT_d, kT_d, v_d, ybT_d)
    if stop_after <= 3:
        return finish(nc, S, es, out, dbg_out)
    h2_d = dscr("h2_d", [NLAT, D], BF16)
    aff_d = dscr("aff_d", [NLAT, 16], F32)
    affT_d = dscr("affT_d", [16, NLAT], F32)
    merge_phase(nc, S, es, sb, ps, din, dscr, dout, dbg, dbg_out, identf, identb, epsc, bc_g1, bc_mul2, bc_add2,
                xcat, out, yaT_d, ybT_d, sgaT_d, sgbT_d, h2_d, aff_d, affT_d, nblocks=mg_blocks)
    if stop_after <= 4:
        return finish(nc, S, es, out, dbg_out)
    moe_phase(nc, S, es, sb, ps, din, dscr, dout, dbg, dbg_out, identf, identb, bc_g2, out, h2_d, aff_d, affT_d, nexp=nexp)
    return finish(nc, S, es, out, dbg_out)


def finish(nc, S, es, out, dbg_out):
    S.barrier(['sp'])
    print("instructions", S.nins, "waits", S.nwaits)
    es.close()
    return nc, dbg_out


def host_inputs(inp, b):
    f = np.float32
    xcat = np.concatenate([inp['ctx'][b], inp['x'][b]], axis=0).astype(f)
    cT = np.stack([inp['c'][b].reshape(8, 128).T, inp['c_ctx'].reshape(8, 128).T], axis=-1).astype(f)
    qkg = np.stack([np.tile(inp['q_norm'][0], 2), np.tile(inp['k_norm'][0], 2)], axis=-1).astype(f)
    m = {
        'xcat': np.ascontiguousarray(xcat),
        'cT': np.ascontiguousarray(cT),
        'w_ada': np.ascontiguousarray(inp['w_ada'][0]),
        'b_ada': np.ascontiguousarray(inp['b_ada'][0][None, :]),
        'nmixT': np.ascontiguousarray(inp['norm_mix'][0].reshape(8, 128).T),
        'nffn': np.ascontiguousarray(inp['norm_ffn'][0][None, :]),
        'w_in': np.ascontiguousarray(inp['w_in'][0]),
        'ident': np.eye(128, dtype=f),
        'qkg': np.ascontiguousarray(qkg),
    }
    f32c = lambda a: np.ascontiguousarray(a.astype(f))
    lr = inp['ssm_lam_re'][0].reshape(2, 16, 2, 64)
    m['lamre_l'] = f32c(lr.transpose(2, 3, 0, 1).reshape(128, 32))
    li = inp['ssm_lam_im'][0].reshape(2, 16, 2, 64)
    m['lamim_l'] = f32c(li.transpose(2, 3, 0, 1).reshape(128, 32))
    ld = inp['ssm_log_dt'][0].reshape(2, 16, 2)
    m['logdt_l'] = f32c(np.broadcast_to(ld.transpose(2, 0, 1)[:, None, :, :], (2, 64, 2, 16)).reshape(128, 32))
    for nm, key in (('Bre_c', 'ssm_b_re'), ('Bim_c', 'ssm_b_im')):
        bb = inp[key][0].reshape(2, 16, 2, 64, 16)
        m[nm] = f32c(bb.transpose(2, 3, 0, 1, 4).reshape(128, 32, 16))
    for nm, key in (('Cre_c', 'ssm_c_re'), ('Cim_c', 'ssm_c_im')):
        cc = inp[key][0].reshape(2, 16, 2, 16, 64)
        m[nm] = f32c(cc.transpose(2, 4, 0, 1, 3).reshape(128, 32, 16))
    m['dskipT'] = f32c(inp['ssm_d'][0].reshape(4, 128).T)
    m['w_glu'] = f32c(inp['w_glu'][0])
    m['bgluT'] = f32c(inp['b_glu'][0].reshape(4, 128).T)
    m.update(na_host(inp))
    m['w_proj_a'] = f32c(inp['w_proj_a'][0]); m['w_proj_b'] = f32c(inp['w_proj_b'][0]); m['w_out'] = f32c(inp['w_out'][0])
    m.update(moe_host(inp))
    m['w_routerT'] = f32c(inp['w_router'][0].reshape(8, 128, 16).transpose(1, 0, 2))
    return m


_NC_CACHE = {}


def kernel(**inputs):
    inp = {k_: np.asarray(v_) for k_, v_ in inputs.items()}
    if 'nc' not in _NC_CACHE:
        nc, _ = build()
        _NC_CACHE['nc'] = nc
    nc = _NC_CACHE['nc']
    nb = inp['x'].shape[0]
    in_maps = [host_inputs(inp, b) for b in range(nb)]
    res = run_bass_kernel_spmd(nc, in_maps, core_ids=list(range(nb)))
    outs = [np.asarray(res.results[b]["out"], dtype=np.float32) for b in range(nb)]
    return np.stack(outs, axis=0)
```

```python
import numpy as np
from contextlib import ExitStack
import concourse.bass as bass
import concourse.mybir as mybir
from concourse.bass_utils import run_bass_kernel_spmd

F32 = mybir.dt.float32
BF16 = mybir.dt.bfloat16
I32 = mybir.dt.int32
U32 = mybir.dt.uint32
ALU = mybir.AluOpType
AF = mybir.ActivationFunctionType
AX = mybir.AxisListType
STRICT = False


class Sched:
    def __init__(self, nc, es, n_dma_sems=24):
        self.nc = nc
        self.e = {'pe': nc.tensor, 'act': nc.scalar, 'dve': nc.vector, 'pool': nc.gpsimd, 'sp': nc.sync}
        self.sems = {}
        self.cnt = {}
        for k in self.e:
            self.sems[k] = es.enter_context(nc.semaphore("sem_" + k))
            self.cnt[k] = 0
        self.nd = n_dma_sems
        self.qsems = {}
        for q in ('sp', 'pool', 'act'):
            n = n_dma_sems if q != 'act' else 16
            self.qsems[q] = []
            for i in range(n):
                sk = 'd_%s%d' % (q, i)
                self.sems[sk] = es.enter_context(nc.semaphore("dsem_%s%d" % (q, i)))
                self.cnt[sk] = 0
                self.qsems[q].append(sk)
        self.qrr = {'sp': 0, 'pool': 0, 'act': 0}
        self.drr = 0
        self.waited = {}
        self.lastw = {}
        self.readers = {}
        self.nwaits = 0
        self.nins = 0

    def _wait(self, eng, tok):
        sk, val, src = tok
        if self.waited.get((eng, sk), 0) >= val:
            return
        self.e[eng].wait_ge(self.sems[sk], val)
        self.waited[(eng, sk)] = val
        self.nwaits += 1

    def _deps(self, eng, reads, writes):
        for k in reads:
            t = self.lastw.get(k)
            if t is not None:
                self._wait(eng, t)
        for k in writes:
            t = self.lastw.get(k)
            if t is not None and (t[2] != eng or (STRICT and eng != 'pe')):
                self._wait(eng, t)
            for sk, (val, src) in self.readers.get(k, {}).items():
                if src != eng or (STRICT and eng != 'pe'):
                    self._wait(eng, (sk, val, src))

    def _record(self, tok, reads, writes):
        sk, val, src = tok
        for k in reads:
            self.readers.setdefault(k, {})[sk] = (val, src)
        for k in writes:
            self.lastw[k] = tok
            self.readers[k] = {}

    def op(self, eng, fn, reads=(), writes=(), sig=True):
        self._deps(eng, reads, writes)
        ins = fn(self.e[eng])
        if sig:
            self.cnt[eng] += 1
            ins.then_inc(self.sems[eng], 1)
            tok = (eng, self.cnt[eng], eng)
        else:
            tok = (eng, self.cnt[eng] + 1, eng)
        self._record(tok, reads, writes)
        self.nins += 1
        return tok

    def _dma_common(self, q, fn, reads, writes):
        self._deps(q, reads, writes)
        i = self.qrr[q]
        self.qrr[q] = (i + 1) % len(self.qsems[q])
        sk = self.qsems[q][i]
        if self.cnt[sk] > 0:
            self._wait(q, (sk, self.cnt[sk], None))
        ins = fn(self.e[q])
        self.cnt[sk] += 16
        ins.then_inc(self.sems[sk], 16)
        tok = (sk, self.cnt[sk], None)
        self._record(tok, reads, writes)
        self.nins += 1
        return tok

    def dma(self, q, out, in_, reads=(), writes=(), **kw):
        return self._dma_common(q, lambda e: e.dma_start(out=out, in_=in_, **kw), reads, writes)

    def custom_dma(self, q, fn, reads=(), writes=()):
        return self._dma_common(q, fn, reads, writes)

    def barrier(self, engines=None):
        for e in (engines or self.e):
            for sk, c in self.cnt.items():
                if c > 0:
                    self._wait(e, (sk, c, None))
        self.lastw = {}
        self.readers = {}

PI = float(np.pi)


def kn(t):
    n = t.name
    return n[2:] if n.startswith('s_') else n


def s5_phase(nc, S, es, sb, ps, din, dscr, dout, dbg, dbg_out, identf, identb, uT_d, yaT_d, nbanks=16):
    NEXT, NE2 = 8448, 8704
    lre_in = din("lamre_l", [128, 32]); lim_in = din("lamim_l", [128, 32]); ldt_in = din("logdt_l", [128, 32])
    Bre_in = din("Bre_c", [128, 32, 16]); Bim_in = din("Bim_c", [128, 32, 16])
    Cre_in = din("Cre_c", [128, 32, 16]); Cim_in = din("Cim_c", [128, 32, 16])
    dsk_in = din("dskipT", [128, 4]); wglu_in = din("w_glu", [512, 512]); bglu_in = din("bgluT", [128, 4])
    E_d = dscr("E_d", [128, 2, 4, 16, 2, 2, 128], BF16)
    G_d = dscr("G_d", [128, 64, 640], BF16)
    H_d = dscr("H_d", [2, 128, 16, 2, 544], BF16)

    with ExitStack() as s5:
        Kt_sb = sb("s_Kt_sb", [128, 128, 128], BF16, s5)
        aL = sb("s_aL", [128, 32], F32, s5); bL = sb("s_bL", [128, 32], F32, s5)
        diagD = sb("s_diagD", [128, 4, 128], BF16, s5)
        with ExitStack() as pa:
            def t32(name):
                return sb(name, [128, 32], F32, pa)
            lre, lim, ldt, dt, a, b = (t32(n) for n in ("lre", "lim", "ldt", "dt", "a_", "b_"))
            u1, u2, u3, u4, c16, s16, wre, wim = (t32(n) for n in ("u1", "u2", "u3", "u4", "c16", "s16", "wre", "wim"))
            Bcr = sb("s_Bcr", [128, 32, 16], F32, pa); Bci = sb("s_Bci", [128, 32, 16], F32, pa)
            Dr = sb("s_Dr", [128, 32, 32], F32, pa); Di = sb("s_Di", [128, 32, 32], F32, pa)
            Dr2 = sb("s_Dr2", [128, 32, 32], F32, pa); Di2 = sb("s_Di2", [128, 32, 32], F32, pa)
            Gr = sb("s_Gr", [128, 32, 32], F32, pa); Gi = sb("s_Gi", [128, 32, 32], F32, pa)
            T1 = sb("s_T1", [128, 32, 32], F32, pa); T2 = sb("s_T2", [128, 32, 32], F32, pa)
            T3 = sb("s_T3", [128, 32, 32], F32, pa); T4 = sb("s_T4", [128, 32, 32], F32, pa)
            P1 = sb("s_P1", [128, 32, 32], F32, pa); P2 = sb("s_P2", [128, 32, 32], F32, pa)
            P3 = sb("s_P3", [128, 32, 32], F32, pa); P4 = sb("s_P4", [128, 32, 32], F32, pa)
            Cbr = sb("s_Cbr", [128, 32, 32], BF16, pa); Cbni = sb("s_Cbni", [128, 32, 32], BF16, pa)
            Dbr = sb("s_Dbr", [128, 8, 160], BF16, pa); Dbi = sb("s_Dbi", [128, 8, 160], BF16, pa)
            Gbr = sb("s_Gbr", [128, 8, 160], BF16, pa); Gbni = sb("s_Gbni", [128, 8, 160], BF16, pa)
            Es = [sb("s_Es%d" % i, [128, 2, 4, 2, 2, 128], BF16, pa) for i in range(2)]
            m3 = sb("s_m3", [128, 1], F32, pa)
            dsk = sb("s_dsk", [128, 4], F32, pa)
            pk = [ps("s_pk%d" % i, [128, 128], F32, pa) for i in range(2)]
            pE = [ps("s_pE%d" % i, [128, 128], F32, pa) for i in range(4)]

            S.dma('sp', lre[:], lre_in, writes=['lre']); S.dma('sp', lim[:], lim_in, writes=['lim'])
            S.dma('sp', ldt[:], ldt_in, writes=['ldt']); S.dma('sp', dsk[:], dsk_in, writes=['dsk'])
            for X in (Dr, Di, Gr, Gi, Dr2, Di2):
                S.op('dve', lambda e: e.memset(X[:], 0.0), writes=[kn(X)])
            for X in (Dbr, Dbi, Gbr, Gbni):
                S.op('pool', lambda e: e.memset(X[:], 0.0), writes=[kn(X)])
            S.op('pool', lambda e: e.memset(Kt_sb[:], 0.0), writes=['Kt_sb'])
            for i in range(2):
                S.op('pool', lambda e: e.memset(Es[i][:], 0.0), writes=[('Es', i)])
            S.op('dve', lambda e: e.memset(m3[:], 1.0), writes=['m3'])
            S.op('dve', lambda e: e.memset(m3[64:96, :], 0.0), writes=['m3'])
            S.dma('sp', Bcr[:], Bre_in, writes=['Bcr']); S.dma('sp', Bci[:], Bim_in, writes=['Bci'])
            for h in range(2):
                S.dma('sp', Gr[64 * h:64 * h + 64, :, 16 * h:16 * h + 16], Cre_in[64 * h:64 * h + 64], reads=[], writes=['Gr'])
                S.dma('sp', Gi[64 * h:64 * h + 64, :, 16 * h:16 * h + 16], Cim_in[64 * h:64 * h + 64], reads=[], writes=['Gi'])
            for q in range(4):
                S.op('dve', lambda e: e.tensor_scalar(out=diagD[:, q, :], in0=identf[:], scalar1=dsk[:, q:q + 1], scalar2=None,
                                                      op0=ALU.mult), reads=['identf', 'dsk'], writes=['diagD'])
            V = lambda e: e
            def tt(out, in0, in1, op, r, w, eng='dve'):
                S.op(eng, lambda e: e.tensor_tensor(out=out, in0=in0, in1=in1, op=op), reads=r, writes=w)
            S.op('act', lambda e: e.activation(out=dt[:], in_=ldt[:], func=AF.Exp), reads=['ldt'], writes=['dt'])
            tt(u1[:], lre[:], dt[:], ALU.mult, ['lre', 'dt'], ['u1'])
            S.op('act', lambda e: e.activation(out=u1[:], in_=u1[:], func=AF.Exp), reads=['u1'], writes=['u1'])
            tt(u2[:], lim[:], dt[:], ALU.mult, ['lim', 'dt'], ['u2'])
            S.op('act', lambda e: e.activation(out=s16[:], in_=u2[:], func=AF.Sin, scale=1.0 / 16), reads=['u2'], writes=['s16'])
            S.op('act', lambda e: e.activation(out=u3[:], in_=u2[:], func=AF.Sin, scale=1.0 / 32), reads=['u2'], writes=['u3'])
            tt(u3[:], u3[:], u3[:], ALU.mult, ['u3'], ['u3'])
            S.op('dve', lambda e: e.tensor_scalar(out=c16[:], in0=u3[:], scalar1=-2.0, scalar2=1.0, op0=ALU.mult, op1=ALU.add),
                 reads=['u3'], writes=['c16'])

            def csquare(cr, ci, n):
                for _ in range(n):
                    tt(u3[:], cr[:], cr[:], ALU.mult, [kn(cr)], ['u3'])
                    tt(u4[:], ci[:], ci[:], ALU.mult, [kn(ci)], ['u4'])
                    tt(ci[:], cr[:], ci[:], ALU.mult, [kn(cr), kn(ci)], [kn(ci)])
                    S.op('dve', lambda e: e.tensor_scalar(out=ci[:], in0=ci[:], scalar1=2.0, scalar2=None, op0=ALU.mult),
                         reads=[kn(ci)], writes=[kn(ci)])
                    tt(cr[:], u3[:], u4[:], ALU.subtract, ['u3', 'u4'], [kn(cr)])
            csquare(c16, s16, 4)
            tt(a[:], u1[:], c16[:], ALU.mult, ['u1', 'c16'], ['a_'])
            tt(b[:], u1[:], s16[:], ALU.mult, ['u1', 's16'], ['b_'])
            S.op('dve', lambda e: e.tensor_copy(out=aL[:], in_=c16[:]), reads=['c16'], writes=['aL'])
            S.op('dve', lambda e: e.tensor_copy(out=bL[:], in_=s16[:]), reads=['s16'], writes=['bL'])
            csquare(aL, bL, 4)
            tt(u3[:], lre[:], dt[:], ALU.mult, ['lre', 'dt'], ['u3'])
            S.op('act', lambda e: e.activation(out=u3[:], in_=u3[:], func=AF.Exp, scale=16.0), reads=['u3'], writes=['u3'])
            tt(aL[:], aL[:], u3[:], ALU.mult, ['aL', 'u3'], ['aL'])
            tt(bL[:], bL[:], u3[:], ALU.mult, ['bL', 'u3'], ['bL'])
            S.op('dve', lambda e: e.tensor_scalar(out=u1[:], in0=a[:], scalar1=-1.0, scalar2=None, op0=ALU.add), reads=['a_'], writes=['u1'])
            tt(u2[:], lre[:], lre[:], ALU.mult, ['lre'], ['u2'])
            tt(u3[:], lim[:], lim[:], ALU.mult, ['lim'], ['u3'])
            tt(u2[:], u2[:], u3[:], ALU.add, ['u2', 'u3'], ['u2'])
            S.op('dve', lambda e: e.reciprocal(out=u2[:], in_=u2[:]), reads=['u2'], writes=['u2'])
            tt(u3[:], u1[:], lre[:], ALU.mult, ['u1', 'lre'], ['u3'])
            tt(u4[:], b[:], lim[:], ALU.mult, ['b_', 'lim'], ['u4'])
            tt(u3[:], u3[:], u4[:], ALU.add, ['u3', 'u4'], ['u3'])
            tt(wre[:], u3[:], u2[:], ALU.mult, ['u3', 'u2'], ['wre'])
            tt(u3[:], b[:], lre[:], ALU.mult, ['b_', 'lre'], ['u3'])
            tt(u4[:], u1[:], lim[:], ALU.mult, ['u1', 'lim'], ['u4'])
            tt(u3[:], u3[:], u4[:], ALU.subtract, ['u3', 'u4'], ['u3'])
            tt(wim[:], u3[:], u2[:], ALU.mult, ['u3', 'u2'], ['wim'])
            bc16 = lambda t: t[:].unsqueeze(2).to_broadcast([128, 32, 16])
            Tc = [T1[:, :, 0:16], T2[:, :, 0:16], T3[:, :, 0:16], T4[:, :, 0:16]]
            tt(Tc[0], Bcr[:], bc16(wre), ALU.mult, ['Bcr', 'wre'], ['T1'])
            tt(Tc[1], Bci[:], bc16(wim), ALU.mult, ['Bci', 'wim'], ['T2'])
            tt(Tc[2], Bci[:], bc16(wre), ALU.mult, ['Bci', 'wre'], ['T3'])
            tt(Tc[3], Bcr[:], bc16(wim), ALU.mult, ['Bcr', 'wim'], ['T4'])
            for h in range(2):
                sl = slice(64 * h, 64 * h + 64)
                tt(Dr[sl, :, 16 * h:16 * h + 16], T1[sl, :, 0:16], T2[sl, :, 0:16], ALU.subtract, ['T1', 'T2'], ['Dr'])
                tt(Di[sl, :, 16 * h:16 * h + 16], T3[sl, :, 0:16], T4[sl, :, 0:16], ALU.add, ['T3', 'T4'], ['Di'])
            S.op('dve', lambda e: e.tensor_copy(out=Cbr[:], in_=Gr[:]), reads=['Gr'], writes=['Cbr'])
            S.op('dve', lambda e: e.tensor_scalar(out=Cbni[:], in0=Gi[:], scalar1=-1.0, scalar2=None, op0=ALU.mult),
                 reads=['Gi'], writes=['Cbni'])
            bc32 = lambda t: t[:].unsqueeze(2).to_broadcast([128, 32, 32])

            def cmul(Xr, Xi, eng, Yr=None, Yi=None):
                Yr = Yr or Xr; Yi = Yi or Xi
                A1, A2, A3, A4 = (T1, T2, T3, T4) if eng == 'dve' else (P1, P2, P3, P4)
                tt(A1[:], Xr[:], bc32(a), ALU.mult, [kn(Xr), 'a_'], [kn(A1)], eng)
                tt(A2[:], Xi[:], bc32(b), ALU.mult, [kn(Xi), 'b_'], [kn(A2)], eng)
                tt(A3[:], Xi[:], bc32(a), ALU.mult, [kn(Xi), 'a_'], [kn(A3)], eng)
                tt(A4[:], Xr[:], bc32(b), ALU.mult, [kn(Xr), 'b_'], [kn(A4)], eng)
                tt(Yr[:], A1[:], A2[:], ALU.subtract, [kn(A1), kn(A2)], [kn(Yr)], eng)
                tt(Yi[:], A3[:], A4[:], ALU.add, [kn(A3), kn(A4)], [kn(Yi)], eng)

            def to5(dst, src, scale, key):
                d4 = dst[:].rearrange("p g (s c) -> p g s c", c=32)
                s4 = src[:].rearrange("p (g s) c -> p g s c", s=4)
                S.op('dve', lambda e: e.tensor_scalar(out=d4[:, :, 0:3, :], in0=s4[:, :, 0:3, :], scalar1=scale, scalar2=None, op0=ALU.mult),
                     reads=[kn(src)], writes=[key])
                S.op('dve', lambda e: e.tensor_scalar(out=d4[:, :, 4, :], in0=s4[:, :, 3, :], scalar1=scale, scalar2=None, op0=ALU.mult),
                     reads=[kn(src)], writes=[key])

            Dpairs = [(Dr, Di), (Dr2, Di2)]
            for j in range(16):
                Dr, Di = Dpairs[j % 2]
                to5(Dbr, Dr, 1.0, 'Dbr'); to5(Dbi, Di, 1.0, 'Dbi')
                if j < 15:
                    cmul(Dr, Di, 'dve', Dpairs[(j + 1) % 2][0], Dpairs[(j + 1) % 2][1])
                for d in range(2):
                    for q in range(4):
                        idx = (d * 4 + q) * 16 + j
                        pkt = pk[idx % 2]; pkk = ('pk', idx % 2)
                        for pl in range(4):
                            pair = d * 16 + q * 4 + pl
                            if pl < 3:
                                osl = pkt[32 * pl:32 * pl + 32, 32 * pl:32 * pl + 32]; csl = slice(32 * pl, 32 * pl + 32)
                            else:
                                osl = pkt[64:128, 96:128]; csl = slice(96, 160)
                            S.op('pe', lambda e: e.matmul(osl, lhsT=Dbr[:, d * 4 + q, csl], rhs=Cbr[:, pair, :], start=True, stop=False),
                                 reads=['Dbr', 'Cbr'], writes=[pkk])
                            S.op('pe', lambda e: e.matmul(osl, lhsT=Dbi[:, d * 4 + q, csl], rhs=Cbni[:, pair, :], start=False, stop=True),
                                 reads=['Dbi', 'Cbni'], writes=[pkk])
                        for pl in range(4):
                            if pl < 3:
                                rs_, cs_ = slice(32 * pl, 32 * pl + 32), slice(32 * pl, 32 * pl + 32)
                            else:
                                rs_, cs_ = slice(64, 128), slice(96, 128)
                            S.op('act', lambda e: e.activation(out=Kt_sb[rs_, idx, cs_], in_=pkt[rs_, cs_], func=AF.Copy),
                                 reads=[pkk], writes=['Kt_sb'])
                Et = Es[j % 2]; ek = ('Es', j % 2)
                n_e = 0
                for reim, X in ((0, Dr), (1, Di)):
                    for d in range(2):
                        for q in range(4):
                            pet = pE[n_e % 4]; pek = ('pE', n_e % 4); n_e += 1
                            src = X[:, d * 16 + q * 4:d * 16 + q * 4 + 4, :].rearrange("p a b -> p (a b)")
                            S.op('pe', lambda e: e.transpose(out=pet[:], in_=src, identity=identf[:]), reads=[kn(X), 'identf'], writes=[pek])
                            S.op('act', lambda e: e.activation(out=Et[:, d, q, reim, 0, :], in_=pet[:], func=AF.Copy), reads=[pek], writes=[ek])
                            S.op('dve', lambda e: e.tensor_scalar(out=Et[64:128, d, q, reim, 1, :], in0=pet[64:128, :], scalar1=m3[64:128, 0:1],
                                                                  scalar2=None, op0=ALU.mult), reads=[pek, 'm3'], writes=[ek])
                for d in range(2):
                    S.dma('sp', E_d[:, d, :, j, :, :, :], Et[:, d], reads=[ek], writes=['E_d'])
                cmul(Gr, Gi, 'pool')
                to5(Gbr, Gr, 1.0, 'Gbr'); to5(Gbni, Gi, -1.0, 'Gbni')
                for d in range(2):
                    S.dma('sp', G_d[:, (d * 16 + j) * 2 + 0, :], Gbr[:, d * 4:d * 4 + 4, :].rearrange("p a b -> p (a b)"), reads=['Gbr'], writes=['G_d'])
                    S.dma('sp', G_d[:, (d * 16 + j) * 2 + 1, :], Gbni[:, d * 4:d * 4 + 4, :].rearrange("p a b -> p (a b)"), reads=['Gbni'], writes=['G_d'])
            S.barrier()
        if 's5prep' in dbg:
            dbg_out['Kt'] = dout("dbg_Kt", [128, 128, 128], BF16)
            S.dma('sp', dbg_out['Kt'], Kt_sb[:], reads=['Kt_sb'])
            dbg_out['aL'] = dout("dbg_aL", [128, 2, 32])
            S.dma('sp', dbg_out['aL'][:, 0, :], aL[:], reads=['aL']); S.dma('sp', dbg_out['aL'][:, 1, :], bL[:], reads=['bL'])
            dbg_out['G'] = dout("dbg_G", [128, 64, 640], BF16)
            S.dma('sp', dbg_out['G'], G_d, reads=['G_d'])
            dbg_out['E'] = dout("dbg_E", [128, 2, 4, 16, 2, 2, 128], BF16)
            for d in range(2):
                S.dma('sp', dbg_out['E'][:, d], E_d[:, d], reads=['E_d'])
            return 'stop'
        return s5_main(nc, S, es, sb, ps, din, dscr, dout, dbg, dbg_out, identf, identb, uT_d, yaT_d, nbanks,
                       s5, Kt_sb, aL, bL, diagD, E_d, G_d, H_d, wglu_in, bglu_in)


def s5_main(nc, S, es, sb, ps, din, dscr, dout, dbg, dbg_out, identf, identb, uT_d, yaT_d, nbanks,
            s5, Kt_sb, aL, bL, diagD, E_d, G_d, H_d, wglu_in, bglu_in):
    NCH = 528
    with ExitStack() as pb:
        uTb = [sb("s_uTb%d" % i, [128, 2048], BF16, pb) for i in range(3)]
        Esl = [sb("s_Esl%d" % i, [128, 16, 2, 2, 128], BF16, pb) for i in range(2)]
        Ssb = [[sb("s_Ssb%d_%d" % (d, i), [128, 16, 2, 128], F32, pb) for i in range(2)] for d in range(2)]
        hist = [sb("s_hist%d" % d, [128, 16, 2, 129], F32, pb) for d in range(2)]
        histb = [sb("s_histb%d" % d, [128, 16, 2, 128], BF16, pb) for d in range(2)]
        tP = [sb("s_tP%d" % d, [128, 16, 2], F32, pb) for d in range(2)]
        tQ = [sb("s_tQ%d" % d, [128, 16, 2], F32, pb) for d in range(2)]
        pS = [ps("s_pS%d" % i, [128, 4, 128], F32, pb) for i in range(4)]
        for d in range(2):
            S.op('dve' if d == 0 else 'pool', lambda e: e.memset(hist[d][:], 0.0), writes=[('hist', d)])
        fblocks = [(0, 128), (128, 128), (256, 128), (384, 128), (512, 16)]
        bblocks = [(512, 16), (384, 128), (256, 128), (128, 128), (0, 128)]
        esl_ctr = 0
        ps_ctr = 0
        for step in range(5):
            for d in range(2):
                k0, nk = (fblocks if d == 0 else bblocks)[step]
                Sb = Ssb[d][step % 2]; skey = ('Ssb', d, step % 2)
                base = 0 if d == 0 else 256
                for q in range(4):
                    et = Esl[esl_ctr % 2]; ekey = ('Esl', esl_ctr % 2); esl_ctr += 1
                    S.dma('sp', et[:], E_d[:, d, q], reads=['E_d'], writes=[ekey])
                    ut = uTb[(esl_ctr - 1) % 3]; ukey = ('uTb', (esl_ctr - 1) % 3)
                    S.dma('sp', ut[:, 0:16 * nk], uT_d[q * 128:(q + 1) * 128, base + 16 * k0: base + 16 * k0 + 16 * nk], reads=['uT_d'], writes=[ukey])
                    for pl in range(4):
                        pst = pS[ps_ctr % 4]; pskey = ('pS', ps_ctr % 4); ps_ctr += 1
                        if pl < 3:
                            rsl, alt = slice(32 * pl, 32 * pl + 32), 0
                        else:
                            rsl, alt = slice(64, 128), 1
                        for reim in range(2):
                            for i in range(16):
                                j = 15 - i if d == 0 else i
                                rhs = ut[rsl, i:i + 16 * (nk - 1) + 1:16]
                                S.op('pe', lambda e: e.matmul(pst[:, reim, 0:nk], lhsT=et[rsl, j, reim, alt, :], rhs=rhs,
                                                              start=(i == 0), stop=(i == 15)),
                                     reads=[ekey, ukey], writes=[pskey])
                        S.op('act', lambda e: e.activation(out=Sb[:, q * 4 + pl, :, 0:nk], in_=pst[:, 0:2, 0:nk], func=AF.Copy),
                             reads=[pskey], writes=[skey])
            def chain_ops(d, kk):
                k0, nk = (fblocks if d == 0 else bblocks)[step]
                Sb = Ssb[d][step % 2]; skey = ('Ssb', d, step % 2)
                hk = ('hist', d)
                A2 = aL[:, d * 16:(d + 1) * 16].unsqueeze(2).to_broadcast([128, 16, 2])
                B2 = bL[:, d * 16:(d + 1) * 16].unsqueeze(2).to_broadcast([128, 16, 2])
                H = hist[d]
                if d == 0:
                    cprev, ccur, scol = kk, kk + 1, kk
                else:
                    cprev, ccur, scol = nk - kk, nk - 1 - kk, nk - 1 - kk
                prev = H[:, :, :, cprev]
                P_, Q_ = tP[d], tQ[d]
                eng = 'dve'
                return [
                    lambda: S.op(eng, lambda e: e.tensor_tensor(out=P_[:], in0=prev, in1=A2, op=ALU.mult), reads=[hk, 'aL'], writes=[('tP', d)]),
                    lambda: S.op(eng, lambda e: e.tensor_tensor(out=Q_[:], in0=prev, in1=B2, op=ALU.mult), reads=[hk, 'bL'], writes=[('tQ', d)]),
                    lambda: S.op(eng, lambda e: e.tensor_tensor(out=P_[:], in0=P_[:], in1=Sb[:, :, :, scol], op=ALU.add),
                                 reads=[('tP', d), skey], writes=[('tP', d)]),
                    lambda: S.op(eng, lambda e: e.tensor_tensor(out=H[:, :, 0, ccur], in0=P_[:, :, 0], in1=Q_[:, :, 1], op=ALU.subtract),
                                 reads=[('tP', d), ('tQ', d)], writes=[hk]),
                    lambda: S.op(eng, lambda e: e.tensor_tensor(out=H[:, :, 1, ccur], in0=P_[:, :, 1], in1=Q_[:, :, 0], op=ALU.add),
                                 reads=[('tP', d), ('tQ', d)], writes=[hk]),
                ]
            nks = [(fblocks if d == 0 else bblocks)[step][1] for d in range(2)]
            for kk in range(max(nks)):
                lists = [chain_ops(d, kk) for d in range(2) if kk < nks[d]]
                for oi in range(5):
                    for l in lists:
                        l[oi]()
            for d in range(2):
                eng = 'dve' if d == 0 else 'pool'
                k0, nk = (fblocks if d == 0 else bblocks)[step]
                hk = ('hist', d)
                H = hist[d]
                if d == 0:
                    S.op(eng, lambda e: e.tensor_copy(out=histb[d][:, :, :, 0:nk], in_=H[:, :, :, 1:nk + 1]), reads=[hk], writes=[('histb', d)])
                    S.op(eng, lambda e: e.tensor_copy(out=H[:, :, :, 0], in_=H[:, :, :, nk]), reads=[hk], writes=[hk])
                else:
                    S.op(eng, lambda e: e.tensor_copy(out=histb[d][:, :, :, 0:nk], in_=H[:, :, :, 0:nk]), reads=[hk], writes=[('histb', d)])
                    S.op(eng, lambda e: e.tensor_copy(out=H[:, :, :, 128], in_=H[:, :, :, 0]), reads=[hk], writes=[hk])
                S.dma('pool', H_d[d, :, :, :, k0:k0 + nk], histb[d][:, :, :, 0:nk], reads=[('histb', d)], writes=['H_d'])
        S.barrier()
    if 's5lvl3' in dbg:
        dbg_out['H'] = dout("dbg_H", [2, 128, 16, 2, 544], BF16)
        for d in range(2):
            S.dma('sp', dbg_out['H'][d], H_d[d], reads=['H_d'])
        return 'stop'

    with ExitStack() as pc:
        Gsb = sb("s_Gsb", [128, 64, 640], BF16, pc)
        wglu = sb("s_wglu", [128, 4, 512], BF16, pc)
        bglu = sb("s_bglu", [128, 4], F32, pc)
        ub = [sb("s_ub%d" % i, [128, 4, 544], BF16, pc) for i in range(2)]
        Hf = [sb("s_Hf%d" % i, [128, 16, 2, 32], BF16, pc) for i in range(2)]
        Hb = [sb("s_Hb%d" % i, [128, 16, 2, 32], BF16, pc) for i in range(2)]
        aT = [sb("s_aT%d" % i, [128, 4, 512], BF16, pc) for i in range(2)]
        sig = [sb("s_sig%d" % i, [128, 512], BF16, pc) for i in range(2)]
        yo = [sb("s_yo%d" % i, [128, 512], BF16, pc) for i in range(4)]
        py = [ps("s_py%d" % i, [128, 512], F32, pc) for i in range(4)]
        pg = [ps("s_pg%d" % i, [128, 512], F32, pc) for i in range(2)]
        for c in range(4):
            S.dma('sp', Gsb[:, c * 16:(c + 1) * 16, :], G_d[:, c * 16:(c + 1) * 16, :], reads=['G_d'], writes=[('Gsb', c)])
        gkeys = [('Gsb', c) for c in range(4)]
        for q in range(4):
            S.dma('pool', wglu[:, q, :], wglu_in[q * 128:(q + 1) * 128, :], writes=['wglu'])
        S.dma('sp', bglu[:], bglu_in, writes=['bglu'])
        yo_ctr = 0
        for n in range(nbanks):
            i2 = n % 2
            e0 = 256 + 512 * n
            S.dma('sp', ub[i2][:], uT_d[:, e0 - 16:e0 + 528].rearrange("(q p) t -> p q t", p=128), reads=['uT_d'], writes=[('ub', i2)])
            kf = 16 + 32 * n
            kb = 32 * n
            S.dma('sp', Hf[i2][:], H_d[0, :, :, :, kf - 1:kf + 31], reads=['H_d'], writes=[('Hf', i2)])
            S.dma('sp', Hb[i2][:], H_d[1, :, :, :, kb + 1:kb + 33], reads=['H_d'], writes=[('Hb', i2)])
            for q in range(4):
                pyt = py[q]; pk_ = ('py', q)
                pyv = pyt[:].rearrange("p (k j) -> p k j", j=16)
                uc = ub[i2][:, q, 16:528]
                ucv = uc.rearrange("p (k j) -> p k j", j=16)
                rd = [('ub', i2), 'Kt_sb', 'diagD']
                S.op('pe', lambda e: e.matmul(pyt[:], lhsT=diagD[:, q, :], rhs=uc, start=True, stop=False), reads=rd, writes=[pk_])
                S.op('pe', lambda e: e.matmul(pyt[:], lhsT=Kt_sb[:, (0 * 4 + q) * 16 + 0, :], rhs=uc, start=False, stop=False), reads=rd, writes=[pk_])
                for tau in range(1, 16):
                    S.op('pe', lambda e: e.matmul(pyv[:, :, tau:16], lhsT=Kt_sb[:, (0 * 4 + q) * 16 + tau, :], rhs=ucv[:, :, 0:16 - tau],
                                                  start=False, stop=False), reads=rd, writes=[pk_])
                S.op('pe', lambda e: e.matmul(pyt[:], lhsT=Kt_sb[:, (1 * 4 + q) * 16 + 0, :], rhs=uc, start=False, stop=False), reads=rd, writes=[pk_])
                for tau in range(1, 16):
                    S.op('pe', lambda e: e.matmul(pyv[:, :, 0:16 - tau], lhsT=Kt_sb[:, (1 * 4 + q) * 16 + tau, :], rhs=ucv[:, :, tau:16],
                                                  start=False, stop=False), reads=rd, writes=[pk_])
                for d in range(2):
                    Hx = Hf[i2] if d == 0 else Hb[i2]
                    hkey = ('Hf', i2) if d == 0 else ('Hb', i2)
                    for j in range(16):
                        m = j + 1 if d == 0 else 16 - j
                        for pl in range(4):
                            pair = q * 4 + pl
                            if pl < 3:
                                osl = pyv[32 * pl:32 * pl + 32, :, j]; csl = slice(q * 160 + 32 * pl, q * 160 + 32 * pl + 32)
                            else:
                                osl = pyv[64:128, :, j]; csl = slice(q * 160 + 96, q * 160 + 160)
                            for reim in range(2):
                                last = (d == 1 and j == 15 and pl == 3 and reim == 1)
                                S.op('pe', lambda e: e.matmul(osl, lhsT=Gsb[:, (d * 16 + m - 1) * 2 + reim, csl], rhs=Hx[:, pair, reim, :],
                                                              start=False, stop=last), reads=gkeys + [hkey], writes=[pk_])
                S.op('act', lambda e: e.activation(out=aT[i2][:, q, :], in_=pyt[:], func=AF.Gelu), reads=[pk_], writes=[('aT', i2, q)])
            for oc in range(4):
                pgt = pg[oc % 2]; pgk = ('pg', oc % 2)
                for q in range(4):
                    S.op('pe', lambda e: e.matmul(pgt[:], lhsT=wglu[:, q, oc * 128:(oc + 1) * 128], rhs=aT[i2][:, q, :],
                                                  start=(q == 0), stop=(q == 3)), reads=['wglu', ('aT', i2, q)], writes=[pgk])
                sg = sig[oc % 2]; sgk = ('sig', oc % 2)
                S.op('act', lambda e: e.activation(out=sg[:], in_=pgt[:], func=AF.Sigmoid, bias=bglu[:, oc:oc + 1]),
                     reads=[pgk, 'bglu'], writes=[sgk])
                yt = yo[yo_ctr % 4]; yk = ('yo', yo_ctr % 4); yo_ctr += 1
                S.op('dve', lambda e: e.tensor_tensor(out=yt[:], in0=aT[i2][:, oc, :], in1=sg[:], op=ALU.mult),
                     reads=[('aT', i2, oc), sgk], writes=[yk])
                S.dma('pool', yaT_d[oc * 128:(oc + 1) * 128, 512 * n:512 * n + 512], yt[:], reads=[yk], writes=['yaT_d'])
        S.barrier()
    if 's5' in dbg:
        dbg_out['yaT'] = dout("dbg_yaT", [512, 8192], BF16)
        for r0 in range(0, 512, 128):
            S.dma('sp', dbg_out['yaT'][r0:r0 + 128, :], yaT_d[r0:r0 + 128, :], reads=['yaT_d'])
    return 'ok'

NEG = -30000.0


def na_phase(nc, S, es, sb, ps, din, dscr, dout, dbg, dbg_out, identb, qT_d, kT_d, v_d, ybT_d, nblocks=16):
    rpbT_in = din("rpbT", [128, 8 * 14 * 64])
    maskT_in = din("maskT", [128, 64])
    with ExitStack() as pa:
        Tb = sb("n_Tb", [128, 8, 14, 64], F32, pa)
        mk = sb("n_mk", [128, 64], F32, pa)
        kctx = sb("n_kctx", [128, 4, 256], BF16, pa)
        vctx = sb("n_vctx", [128, 2, 8, 65], BF16, pa)
        qb = [sb("n_qb%d" % i, [128, 4, 512], BF16, pa) for i in range(2)]
        kw = [sb("n_kw%d" % i, [128, 4, 960], BF16, pa) for i in range(2)]
        vw = [sb("n_vw%d" % i, [128, 15, 8, 65], BF16, pa) for i in range(2)]
        sS = [sb("n_sS%d" % i, [128, 4, 64], F32, pa) for i in range(3)]
        P = [sb("n_Pn%d" % i, [128, 6, 64], BF16, pa) for i in range(3)]
        rc = [sb("n_rc%d" % i, [128, 8], F32, pa) for i in range(2)]
        ybt = [sb("n_ybt%d" % i, [128, 8, 64], BF16, pa) for i in range(2)]
        ybT = [sb("n_ybT%d" % i, [128, 4, 512], BF16, pa) for i in range(2)]
        pS = [ps("n_pS%d" % i, [128, 6, 64], F32, pa) for i in range(3)]
        pO = [ps("n_pO%d" % i, [128, 4, 65], F32, pa) for i in range(4)]
        pT = ps("n_pTn", [128, 4, 128], BF16, pa)

        S.dma('sp', Tb[:].rearrange("p h a q -> p (h a q)"), rpbT_in, writes=['Tb'])
        S.dma('sp', mk[:], maskT_in, writes=['mk'])
        S.dma('sp', kctx[:], kT_d[:, 0:256].rearrange("(c p) t -> p c t", p=128), reads=['kT_d'], writes=['kctx'])
        S.dma('sp', vctx[:], v_d[0:256].rearrange("(m p) h d -> p m h d", p=128), reads=['v_d'], writes=['vctx'])
        S.op('dve', lambda e: e.tensor_tensor(out=Tb[:].rearrange("p h a q -> p (h a) q"), in0=Tb[:].rearrange("p h a q -> p (h a) q"),
                                              in1=mk[:].unsqueeze(1).to_broadcast([128, 112, 64]), op=ALU.add),
             reads=['Tb', 'mk'], writes=['Tb'])
        item = 0
        pend = None
        for b in range(nblocks):
            i2 = b % 2
            r0 = 8 * b
            lo = min(max(r0 - 4, 0), 120)
            hi = min(max(r0 + 7 - 4, 0), 120) + 8
            nrows = hi - lo
            S.dma('sp', qb[i2][:], qT_d[:, 512 * b:512 * b + 512].rearrange("(c p) t -> p c t", p=128), reads=['qT_d'], writes=[('qb', i2)])
            S.dma('sp', kw[i2][:, :, 0:64 * nrows], kT_d[:, 256 + 64 * lo:256 + 64 * hi].rearrange("(c p) t -> p c t", p=128),
                  reads=['kT_d'], writes=[('kw', i2)])
            for s in range(nrows - 1):
                S.dma('sp', vw[i2][:, s], v_d[256 + 64 * (lo + s):256 + 64 * (lo + s) + 128], reads=['v_d'], writes=[('vw', i2)])
            for rr in range(8):
                r = r0 + rr
                krs = min(max(r - 4, 0), 120)
                a0 = krs - r + 7
                half = 64 * (r % 2)
                pp = (r // 2) % 2
                for h in range(8):
                    hc, hb = h // 2, 64 * (h % 2)
                    it = item % 3
                    item += 1
                    pst = pS[it]; psk = ('pS', it)
                    for c in range(4):
                        kc0 = 64 * (krs + 2 * c - lo)
                        S.op('pe', lambda e: e.matmul(pst[:, c, :], lhsT=kw[i2][hb:hb + 64, hc, kc0:kc0 + 128],
                                                      rhs=qb[i2][hb:hb + 64, hc, 64 * rr:64 * rr + 64], start=True, stop=True),
                             reads=[('kw', i2), ('qb', i2)], writes=[psk])
                    for c in range(2):
                        S.op('pe', lambda e: e.matmul(pst[:, 4 + c, :], lhsT=kctx[hb:hb + 64, hc, 128 * c:128 * c + 128],
                                                      rhs=qb[i2][hb:hb + 64, hc, 64 * rr:64 * rr + 64], start=True, stop=True),
                             reads=['kctx', ('qb', i2)], writes=[psk])
                    S.op('dve', lambda e: e.tensor_tensor(out=sS[it][:], in0=pst[:, 0:4, :], in1=Tb[:, h, a0:a0 + 7:2, :], op=ALU.add),
                         reads=[psk, 'Tb'], writes=[('sS', it)])
                    S.op('act', lambda e: e.activation(out=P[it][:, 0:4, :], in_=sS[it][:], func=AF.Exp), reads=[('sS', it)], writes=[('P', it)])
                    S.op('act', lambda e: e.activation(out=P[it][:, 4:6, :], in_=pst[:, 4:6, :], func=AF.Exp), reads=[psk], writes=[('P', it)])
                    if pend is not None:
                        pend()
                    def mk_pv(it=it, i2=i2, krs=krs, lo=lo, h=h, half=half, pp=pp, r=r, rr=rr, b=b):
                        def pv():
                            pot = pO[pp * 2 + h // 4]; pok = ('pO', pp * 2 + h // 4)
                            for c in range(6):
                                if c < 4:
                                    rhs = vw[i2][:, krs + 2 * c - lo, h, :]
                                    rk = ('vw', i2)
                                else:
                                    rhs = vctx[:, c - 4, h, :]
                                    rk = 'vctx'
                                S.op('pe', lambda e: e.matmul(pot[half:half + 64, h % 4, :], lhsT=P[it][:, c, :], rhs=rhs,
                                                              start=(c == 0), stop=(c == 5)), reads=[('P', it), rk], writes=[pok])
                            if h % 4 == 3:
                                hh = h // 4
                                yk = ('ybt', pp)
                                S.op('dve', lambda e: e.reciprocal(out=rc[pp][half:half + 64, 4 * hh:4 * hh + 4], in_=pot[half:half + 64, :, 64]),
                                     reads=[pok], writes=[('rc', pp)])
                                S.op('dve', lambda e: e.tensor_tensor(out=ybt[pp][half:half + 64, 4 * hh:4 * hh + 4, :], in0=pot[half:half + 64, :, 0:64],
                                                                      in1=rc[pp][half:half + 64, 4 * hh:4 * hh + 4].unsqueeze(2).to_broadcast([64, 4, 64]),
                                                                      op=ALU.mult), reads=[pok, ('rc', pp)], writes=[yk])
                            if h == 7 and r % 2 == 1:
                                tk = ('ybT', b % 2)
                                for c4 in range(4):
                                    S.op('pe', lambda e: e.transpose(out=pT[:, c4, :], in_=ybt[pp][:, 2 * c4:2 * c4 + 2, :].rearrange("p a d -> p (a d)"),
                                                                     identity=identb[:]), reads=[('ybt', pp), 'identb'], writes=['pTn'])
                                S.op('act', lambda e: e.activation(out=ybT[b % 2][:, :, 64 * (rr - 1):64 * (rr - 1) + 128], in_=pT[:], func=AF.Copy),
                                     reads=['pTn'], writes=[tk])
                                if rr == 7:
                                    S.dma('act', ybT_d[:, 512 * b:512 * b + 512].rearrange("(c p) t -> p c t", p=128), ybT[b % 2][:],
                                          reads=[tk], writes=['ybT_d'])
                        return pv
                    pend = mk_pv()
        pend()
        S.barrier()
    if 'na' in dbg:
        dbg_out['ybT'] = dout("dbg_ybT", [512, 8192], BF16)
        for r0 in range(0, 512, 128):
            S.dma('sp', dbg_out['ybT'][r0:r0 + 128, :], ybT_d[r0:r0 + 128, :], reads=['ybT_d'])


def na_host(inp):
    rpb = inp['na_rpb'][0].astype(np.float32)
    i = np.arange(2)[:, None, None, None]
    kc = np.arange(64)[None, :, None, None]
    a = np.arange(14)[None, None, :, None]
    qc = np.arange(64)[None, None, None, :]
    dcol = np.clip(kc - qc + 15, 0, 30)
    drow = a + i
    T = rpb[:, drow, dcol]
    T = np.broadcast_to(T, (8, 2, 64, 14, 64)).transpose(1, 2, 0, 3, 4).reshape(128, 8 * 14 * 64)
    q = np.arange(64)
    qcs = np.clip(q - 8, 0, 48)
    k = np.arange(64)[:, None]
    inw = (k >= qcs[None, :]) & (k < qcs[None, :] + 16)
    m = np.where(inw, 0.0, NEG).astype(np.float32)
    m = np.concatenate([m, m], axis=0)
    return {'rpbT': np.ascontiguousarray(T.astype(np.float32)), 'maskT': np.ascontiguousarray(m)}

D = 1024


def merge_phase(nc, S, es, sb, ps, din, dscr, dout, dbg, dbg_out, identf, identb, epsc, bc_g1, bc_mul2, bc_add2,
                xcat, out, yaT_d, ybT_d, sgaT_d, sgbT_d, h2_d, aff_d, affT_d, nblocks=16):
    wpa_in = din("w_proj_a", [512, D]); wpb_in = din("w_proj_b", [512, D]); wout_in = din("w_out", [D, D])
    wr_in = din("w_routerT", [128, 8, 16])
    with ExitStack() as pa:
        wpa = sb("g_wpa", [128, 4, D], BF16, pa); wpb = sb("g_wpb", [128, 4, D], BF16, pa)
        wout = sb("g_wout", [128, 8, D], BF16, pa)
        wr = sb("g_wr", [128, 8, 16], F32, pa)
        ya = [sb("g_mya%d" % i, [128, 4, 512], BF16, pa) for i in range(2)]
        yb = [sb("g_myb%d" % i, [128, 4, 512], BF16, pa) for i in range(2)]
        sga = [sb("g_msga%d" % i, [128, 8, 512], BF16, pa) for i in range(2)]
        sgb = [sb("g_msgb%d" % i, [128, 8, 512], BF16, pa) for i in range(2)]
        mT = [sb("g_mT%d" % i, [128, 8, 512], BF16, pa) for i in range(2)]
        t1 = [sb("g_mt1_%d" % i, [128, 512], F32, pa) for i in range(2)]
        t2 = [sb("g_mt2_%d" % i, [128, 512], F32, pa) for i in range(2)]
        xt = [sb("g_mxt%d" % i, [128, D], F32, pa) for i in range(2)]
        x1 = [sb("g_mx1_%d" % i, [128, D], F32, pa) for i in range(3)]
        h2 = [sb("g_mh2_%d" % i, [128, D], F32, pa) for i in range(3)]
        h2b = [sb("g_mh2b_%d" % i, [128, D], BF16, pa) for i in range(3)]
        h2T = [sb("g_mh2T_%d" % i, [128, 8, 128], F32, pa) for i in range(3)]
        junk = sb("g_mjunk", [128, D], BF16, pa)
        st = [sb("g_mst%d" % i, [128, 8], F32, pa) for i in range(3)]
        lg = [sb("g_mlg%d" % i, [128, 16], F32, pa) for i in range(3)]
        af = [sb("g_maf%d" % i, [128, 16], F32, pa) for i in range(3)]
        A16 = sb("g_A16", [16, 8192], F32, pa)
        pab = [ps("g_mpab%d" % i, [128, 512], F32, pa) for i in range(4)]
        pyo = ps("g_mpyo", [128, D], F32, pa)
        phT = ps("g_mphT", [128, 4, 128], F32, pa)
        pmisc = ps("g_mpmisc", [128, 256], F32, pa)
        plg = pmisc[:, 0:16]
        paT = pmisc[0:16, 128:256]

        for q in range(4):
            S.dma('pool', wpa[:, q, :], wpa_in[q * 128:(q + 1) * 128, :], writes=['wpa'])
            S.dma('pool', wpb[:, q, :], wpb_in[q * 128:(q + 1) * 128, :], writes=['wpb'])
        for dc in range(8):
            S.dma('pool', wout[:, dc, :], wout_in[dc * 128:(dc + 1) * 128, :], writes=['wout'])
        S.dma('sp', wr[:], wr_in, writes=['wr'])
        tile_ctr = 0
        pend = []
        for b in range(nblocks):
            i2 = b % 2
            cs = slice(512 * b, 512 * b + 512)
            S.dma('sp', ya[i2][:], yaT_d[:, cs].rearrange("(c p) t -> p c t", p=128), reads=['yaT_d'], writes=[('ya', i2)])
            S.dma('sp', yb[i2][:], ybT_d[:, cs].rearrange("(c p) t -> p c t", p=128), reads=['ybT_d'], writes=[('yb', i2)])
            S.dma('sp', sga[i2][:], sgaT_d[:, cs].rearrange("(c p) t -> p c t", p=128), reads=['sgaT_d'], writes=[('sga', i2)])
            S.dma('sp', sgb[i2][:], sgbT_d[:, cs].rearrange("(c p) t -> p c t", p=128), reads=['sgbT_d'], writes=[('sgb', i2)])
            for dc in range(8):
                j2 = dc % 2
                pA_, pB_ = pab[2 * j2], pab[2 * j2 + 1]
                for q in range(4):
                    S.op('pe', lambda e: e.matmul(pA_[:], lhsT=wpa[:, q, dc * 128:(dc + 1) * 128], rhs=ya[i2][:, q, :],
                                                  start=(q == 0), stop=(q == 3)), reads=['wpa', ('ya', i2)], writes=[('pA', j2)])
                for q in range(4):
                    S.op('pe', lambda e: e.matmul(pB_[:], lhsT=wpb[:, q, dc * 128:(dc + 1) * 128], rhs=yb[i2][:, q, :],
                                                  start=(q == 0), stop=(q == 3)), reads=['wpb', ('yb', i2)], writes=[('pB', j2)])
                S.op('dve', lambda e: e.tensor_tensor(out=t1[j2][:], in0=pA_[:], in1=sga[i2][:, dc, :], op=ALU.mult),
                     reads=[('pA', j2), ('sga', i2)], writes=[('t1', j2)])
                S.op('dve', lambda e: e.tensor_tensor(out=t2[j2][:], in0=pB_[:], in1=sgb[i2][:, dc, :], op=ALU.mult),
                     reads=[('pB', j2), ('sgb', i2)], writes=[('t2', j2)])
                S.op('pool', lambda e: e.tensor_tensor(out=mT[i2][:, dc, :], in0=t1[j2][:], in1=t2[j2][:], op=ALU.add),
                     reads=[('t1', j2), ('t2', j2)], writes=[('mT', i2)])
            for ti in range(4):
                k2 = tile_ctr % 3
                kx = tile_ctr % 2
                tile_ctr += 1
                tok0 = 512 * b + 128 * ti
                S.dma('sp', xt[kx][:], xcat[256 + tok0:256 + tok0 + 128, :], writes=[('xt', kx)])
                for hf in range(2):
                    for dc in range(8):
                        S.op('pe', lambda e: e.matmul(pyo[:, hf * 512:(hf + 1) * 512], lhsT=mT[i2][:, dc, ti * 128:(ti + 1) * 128],
                                                      rhs=wout[:, dc, hf * 512:(hf + 1) * 512], start=(dc == 0), stop=(dc == 7)),
                             reads=[('mT', i2), 'wout'], writes=['pyo'])
                S.op('dve', lambda e: e.tensor_tensor(out=x1[k2][:], in0=pyo[:], in1=bc_g1[:], op=ALU.mult),
                     reads=['pyo', 'bc_g1'], writes=[('x1', k2)])
                S.op('dve', lambda e: e.tensor_tensor(out=x1[k2][:], in0=x1[k2][:], in1=xt[kx][:], op=ALU.add),
                     reads=[('x1', k2), ('xt', kx)], writes=[('x1', k2)])
                S.dma('pool', out[tok0:tok0 + 128, :], x1[k2][:], reads=[('x1', k2)], writes=['out'])
                s_ = st[k2]; sk = ('st', k2)
                S.op('act', lambda e: e.activation(out=junk[:], in_=x1[k2][:], func=AF.Square, accum_out=s_[:, 0:1]),
                     reads=[('x1', k2)], writes=['mjunk', sk])
                S.op('act', lambda e: e.activation(out=s_[:, 6:7], in_=s_[:, 0:1], func=AF.Ln, scale=1.0 / D, bias=epsc[:, 0:1]),
                     reads=[sk, 'epsc'], writes=[sk])
                S.op('act', lambda e: e.activation(out=s_[:, 1:2], in_=s_[:, 6:7], func=AF.Exp, scale=-0.5), reads=[sk], writes=[sk])
                S.op('dve', lambda e: e.scalar_tensor_tensor(out=h2[k2][:], in0=x1[k2][:], scalar=s_[:, 1:2], in1=bc_mul2[:],
                                                             op0=ALU.mult, op1=ALU.mult), reads=[('x1', k2), sk, 'bc_mul2'], writes=[('h2', k2)])
                S.op('dve', lambda e: e.tensor_tensor(out=h2[k2][:], in0=h2[k2][:], in1=bc_add2[:], op=ALU.add),
                     reads=[('h2', k2), 'bc_add2'], writes=[('h2', k2)])
                S.op('act', lambda e: e.activation(out=h2b[k2][:], in_=h2[k2][:], func=AF.Copy), reads=[('h2', k2)], writes=[('h2b', k2)])
                S.dma('act', h2_d[tok0:tok0 + 128, :], h2b[k2][:], reads=[('h2b', k2)], writes=['h2_d'])
                def mk_tail(k2=k2, tok0=tok0, s_=s_, sk=sk):
                    def tail():
                        for hh in range(2):
                            for dc in range(4):
                                S.op('pe', lambda e: e.transpose(out=phT[:, dc, :], in_=h2[k2][:, (4 * hh + dc) * 128:(4 * hh + dc + 1) * 128],
                                                                 identity=identf[:]), reads=[('h2', k2), 'identf'], writes=['phT'])
                            S.op('act', lambda e: e.activation(out=h2T[k2][:, 4 * hh:4 * hh + 4, :], in_=phT[:], func=AF.Copy),
                                 reads=['phT'], writes=[('h2T', k2)])
                        for dc in range(8):
                            S.op('pe', lambda e: e.matmul(plg, lhsT=h2T[k2][:, dc, :], rhs=wr[:, dc, :], start=(dc == 0), stop=(dc == 7)),
                                 reads=[('h2T', k2), 'wr'], writes=['plg'])
                        S.op('dve', lambda e: e.tensor_copy(out=lg[k2][:], in_=plg), reads=['plg'], writes=[('lg', k2)])
                        S.op('dve', lambda e: e.reduce_max(out=s_[:, 2:3], in_=lg[k2][:], axis=AX.X), reads=[('lg', k2)], writes=[sk])
                        S.op('dve', lambda e: e.tensor_scalar(out=s_[:, 3:4], in0=s_[:, 2:3], scalar1=-1.0, scalar2=None, op0=ALU.mult),
                             reads=[sk], writes=[sk])
                        S.op('act', lambda e: e.activation(out=af[k2][:], in_=lg[k2][:], func=AF.Exp, bias=s_[:, 3:4], accum_out=s_[:, 4:5]),
                             reads=[('lg', k2), sk], writes=[('af', k2), sk])
                        S.op('dve', lambda e: e.reciprocal(out=s_[:, 5:6], in_=s_[:, 4:5]), reads=[sk], writes=[sk])
                        S.op('dve', lambda e: e.tensor_scalar(out=af[k2][:], in0=af[k2][:], scalar1=s_[:, 5:6], scalar2=None, op0=ALU.mult),
                             reads=[('af', k2), sk], writes=[('af', k2)])
                        S.dma('pool', aff_d[tok0:tok0 + 128, :], af[k2][:], reads=[('af', k2)], writes=['aff_d'])
                        S.op('pe', lambda e: e.transpose(out=paT, in_=af[k2][:], identity=identf[:]), reads=[('af', k2), 'identf'], writes=['paT'])
                        S.op('act', lambda e: e.activation(out=A16[:, tok0:tok0 + 128], in_=paT, func=AF.Copy), reads=['paT'], writes=['A16'])
                    return tail
                if len(pend) == 2:
                    pend.pop(0)()
                pend.append(mk_tail())
        for t_ in pend:
            t_()
        S.dma('sp', affT_d, A16[:], reads=['A16'], writes=['affT_d'])
        S.barrier()
        if 'mg2' in dbg:
            for nm, t, shp, dt in (('bcg1', bc_g1, [128, D], F32), ('mT0', mT[(nblocks - 1) % 2], [128, 8, 512], BF16), ('t1', t1[1], [128, 512], F32),
                                   ('sga0', sga[(nblocks - 1) % 2], [128, 8, 512], BF16), ('ya0', ya[(nblocks - 1) % 2], [128, 4, 512], BF16),
                                   ('yb0', yb[(nblocks - 1) % 2], [128, 4, 512], BF16), ('wpa', wpa, [128, 4, D], BF16), ('x1t', x1[1], [128, D], F32),
                                   ('xt', xt[1], [128, D], F32)):
                dbg_out[nm] = dout("dbg_" + nm, shp, dt)
                S.dma('sp', dbg_out[nm], t[:], reads=[])
    if 'mg' in dbg:
        dbg_out['aff'] = dout("dbg_aff", [8192, 16])
        S.dma('sp', dbg_out['aff'], aff_d, reads=['aff_d'])
        dbg_out['affT'] = dout("dbg_affT", [16, 8192])
        S.dma('sp', dbg_out['affT'], affT_d, reads=['affT_d'])
        dbg_out['h2'] = dout("dbg_h2", [8192, D], BF16)
        for r0 in range(0, 8192, 1024):
            S.dma('sp', dbg_out['h2'][r0:r0 + 1024, :], h2_d[r0:r0 + 1024, :], reads=['h2_d'])

D = 1024
CAP = 1024
NT = 8192


def moe_phase(nc, S, es, sb, ps, din, dscr, dout, dbg, dbg_out, identf, identb, bc_g2, out, h2_d, aff_d, affT_d, nexp=16):
    wg_in = din("w_e_gate", [16, D, D]); wu_in = din("w_e_up", [16, D, D]); wd_in = din("w_e_down", [16, D, D])
    m16_in = din("m16", [128, 128])
    with ExitStack() as pm:
        cum_d = dscr("cum_d", [16, NT], F32)
        slotv = sb("e_slotv", [128, 8], F32, pm)
        thr_dbg = sb("e_thr", [128, 1], F32, pm)
        S.op('pool', lambda e: e.iota(slotv[:], pattern=[[128, 8]], base=0, channel_multiplier=1, allow_small_or_imprecise_dtypes=True),
             writes=['slotv'])
        with ExitStack() as pa:
            A128 = sb("e_A128", [128, 1024], F32, pa)
            A16 = sb("e_A16", [16, NT], F32, pa)
            C16 = sb("e_C16", [16, NT], F32, pa)
            M16 = sb("e_M16", [128, 128], F32, pa)
            junk = sb("e_junk", [128, 1024], BF16, pa)
            lo = sb("e_lo", [128, 1], F32, pa); mid = sb("e_mid", [128, 1], F32, pa)
            cnt = sb("e_cnt", [128, 1], F32, pa); g = sb("e_g", [128, 1], F32, pa)
            one16 = sb("e_one16", [16, 1], F32, pa)
            ptot = ps("e_ptot", [128, 1], F32, pa)
            for s in range(8):
                S.dma('sp', A128[16 * s:16 * s + 16, :], affT_d[:, 1024 * s:1024 * s + 1024], reads=['affT_d'], writes=['A128'])
            S.dma('sp', A16[:], affT_d, reads=['affT_d'], writes=['A16'])
            S.dma('sp', M16[:], m16_in, writes=['M16'])
            S.op('dve', lambda e: e.memset(lo[:], 0.0), writes=['lo'])
            S.op('dve', lambda e: e.memset(one16[:], 1.0), writes=['one16'])
            for k in range(30):
                hk = 2.0 ** -(k + 1)
                S.op('dve', lambda e: e.tensor_scalar(out=mid[:], in0=lo[:], scalar1=hk, scalar2=None, op0=ALU.add), reads=['lo'], writes=['mid'])
                S.op('dve', lambda e: e.tensor_scalar(out=junk[:], in0=A128[:], scalar1=mid[:, 0:1], scalar2=0.0, op0=ALU.is_gt, op1=ALU.add,
                                                      accum_out=cnt[:]), reads=['A128', 'mid'], writes=['junk', 'cnt'])
                S.op('pe', lambda e: e.matmul(ptot[:], lhsT=M16[:], rhs=cnt[:], start=True, stop=True), reads=['M16', 'cnt'], writes=['ptot'])
                S.op('dve', lambda e: e.tensor_scalar(out=g[:], in0=ptot[:], scalar1=float(CAP), scalar2=hk, op0=ALU.is_ge, op1=ALU.mult),
                     reads=['ptot'], writes=['g'])
                S.op('dve', lambda e: e.tensor_tensor(out=lo[:], in0=lo[:], in1=g[:], op=ALU.add), reads=['lo', 'g'], writes=['lo'])
            S.op('dve', lambda e: e.tensor_copy(out=thr_dbg[:], in_=lo[:]), reads=['lo'], writes=['thr'])
            S.op('dve', lambda e: e.tensor_scalar(out=A16[:], in0=A16[:], scalar1=lo[0:16, 0:1], scalar2=None, op0=ALU.is_gt),
                 reads=['A16', 'lo'], writes=['A16'])
            S.op('dve', lambda e: e.tensor_tensor_scan(out=C16[:], data0=one16[:, 0:1].to_broadcast([16, NT]), data1=A16[:], initial=0.0,
                                                       op0=ALU.mult, op1=ALU.add), reads=['A16', 'one16'], writes=['C16'])
            S.dma('sp', cum_d, C16[:], reads=['C16'], writes=['cum_d'])
            S.barrier()
        if 'moe_thr' in dbg:
            dbg_out['thr'] = dout("dbg_thr", [128, 1])
            S.dma('sp', dbg_out['thr'], thr_dbg[:], reads=['thr'])
        with ExitStack() as pe_:
            Wg = [sb("e_Wg%d" % i, [128, 8, D], BF16, pe_) for i in range(2)]
            Wu = [sb("e_Wu%d" % i, [128, 8, D], BF16, pe_) for i in range(2)]
            Wd = [sb("e_Wd%d" % i, [128, 8, D], BF16, pe_) for i in range(2)]
            xe = [sb("e_xe%d" % i, [128, D], BF16, pe_) for i in range(8)]
            ag = [[sb("e_ag%d_%d" % (p_, i), [128, 16], F32, pe_) for i in range(8)] for p_ in range(2)]
            cq = [sb("e_cq%d" % i, [128, 2048], F32, pe_) for i in range(2)]
            junkq = sb("e_junkq", [128, 2048], BF16, pe_)
            acc = [sb("e_acc%d" % i, [128, 4, 8], F32, pe_) for i in range(3)]
            idxs = sb("e_idxs", [128, 8], F32, pe_)
            idxi = [sb("e_idxi%d" % i, [128, 8], I32, pe_) for i in range(3)]
            xeT = sb("e_xeT", [128, 8, CAP], BF16, pe_)
            hidT = sb("e_hidT", [128, 8, CAP], BF16, pe_)
            sg = [sb("e_sg%d" % i, [128, 512], F32, pe_) for i in range(2)]
            ye = [sb("e_ye%d" % i, [128, D], F32, pe_) for i in range(2)]
            pGU = [ps("e_pGU%d" % i, [128, 512], F32, pe_) for i in range(3)]
            pY = ps("e_pY", [128, D], F32, pe_)
            pT = ps("e_pT", [128, 8, 128], BF16, pe_)
            pTi = pT[:].bitcast(F32) if False else None

            def load_w(e):
                i2 = e % 2
                for dc in range(8):
                    S.dma('pool', Wg[i2][:, dc, :], wg_in[e, dc * 128:(dc + 1) * 128, :], writes=[('Wg', i2)])
                    S.dma('pool', Wu[i2][:, dc, :], wu_in[e, dc * 128:(dc + 1) * 128, :], writes=[('Wu', i2)])
                for dc in range(8):
                    S.dma('pool', Wd[i2][:, dc, :], wd_in[e, dc * 128:(dc + 1) * 128, :], writes=[('Wd', i2)])

            def load_cq(n):
                e_, qd = n // 4, n % 4
                if e_ >= nexp:
                    return
                S.dma('sp', cq[n % 2][:], cum_d[e_:e_ + 1, qd * 2048:(qd + 1) * 2048].to_broadcast([128, 2048]),
                      reads=['cum_d'], writes=[('cq', n % 2)])

            def idx_piece(e, piece):
                j3 = e % 3
                n = e * 4 + piece // 2
                if piece % 2 == 0:
                    if n == 0:
                        load_cq(0)
                    load_cq(n + 1)
                for sc in range(4 * (piece % 2), 4 * (piece % 2) + 4):
                    S.op('dve', lambda e_: e_.tensor_scalar(out=junkq[:], in0=cq[n % 2][:], scalar1=slotv[:, sc:sc + 1], scalar2=0.0,
                                                            op0=ALU.is_le, op1=ALU.add, accum_out=acc[j3][:, piece // 2, sc:sc + 1]),
                         reads=[('cq', n % 2), 'slotv'], writes=['junkq', ('acc', j3)])
                if piece == 7:
                    S.op('dve', lambda e_: e_.tensor_tensor(out=idxs[:], in0=acc[j3][:, 0, :], in1=acc[j3][:, 1, :], op=ALU.add),
                         reads=[('acc', j3)], writes=['idxs'])
                    S.op('dve', lambda e_: e_.tensor_tensor(out=idxs[:], in0=idxs[:], in1=acc[j3][:, 2, :], op=ALU.add),
                         reads=[('acc', j3), 'idxs'], writes=['idxs'])
                    S.op('dve', lambda e_: e_.tensor_tensor(out=idxs[:], in0=idxs[:], in1=acc[j3][:, 3, :], op=ALU.add),
                         reads=[('acc', j3), 'idxs'], writes=['idxs'])
                    S.op('dve', lambda e_: e_.tensor_copy(out=idxi[j3][:], in_=idxs[:]), reads=['idxs'], writes=[('idxi', j3)])

            def gather(e):
                j2 = e % 3
                for sc in range(8):
                    S.custom_dma('pool', lambda e_: e_.indirect_dma_start(out=xe[sc][:, :], out_offset=None, in_=h2_d[:, :],
                                 in_offset=bass.IndirectOffsetOnAxis(ap=idxi[j2][:, sc:sc + 1], axis=0)),
                                 reads=[('idxi', j2), 'h2_d'], writes=[('xe', sc)])
                    S.custom_dma('pool', lambda e_: e_.indirect_dma_start(out=ag[e % 2][sc][:, :], out_offset=None, in_=aff_d[:, :],
                                 in_offset=bass.IndirectOffsetOnAxis(ap=idxi[j2][:, sc:sc + 1], axis=0)),
                                 reads=[('idxi', j2), 'aff_d'], writes=[('ag', e % 2, sc)])

            gu_ctr = [0]

            def xpose(e):
                for sc in range(8):
                    for dc in range(8):
                        S.op('pe', lambda e_: e_.transpose(out=pT[:, dc, :], in_=xe[sc][:, dc * 128:(dc + 1) * 128], identity=identb[:]),
                             reads=[('xe', sc), 'identb'], writes=['pT'])
                    S.op('act', lambda e_: e_.activation(out=xeT[:, :, sc * 128:(sc + 1) * 128], in_=pT[:], func=AF.Copy),
                         reads=['pT'], writes=['xeT'])
            def ffn(e, nxt):
                i2 = e % 2; j2 = e % 3
                for fc in range(8):
                    for hf in range(2):
                        a = gu_ctr[0] % 3; gu_ctr[0] += 1
                        b = gu_ctr[0] % 3; gu_ctr[0] += 1
                        for dc in range(8):
                            S.op('pe', lambda e_: e_.matmul(pGU[a][:], lhsT=Wg[i2][:, dc, fc * 128:(fc + 1) * 128], rhs=xeT[:, dc, hf * 512:(hf + 1) * 512],
                                                            start=(dc == 0), stop=(dc == 7)), reads=[('Wg', i2), 'xeT'], writes=[('pGU', a)])
                        for dc in range(8):
                            S.op('pe', lambda e_: e_.matmul(pGU[b][:], lhsT=Wu[i2][:, dc, fc * 128:(fc + 1) * 128], rhs=xeT[:, dc, hf * 512:(hf + 1) * 512],
                                                            start=(dc == 0), stop=(dc == 7)), reads=[('Wu', i2), 'xeT'], writes=[('pGU', b)])
                        s2 = (fc * 2 + hf) % 2
                        S.op('act', lambda e_: e_.activation(out=sg[s2][:], in_=pGU[a][:], func=AF.Silu), reads=[('pGU', a)], writes=[('sg', s2)])
                        S.op('dve', lambda e_: e_.tensor_tensor(out=hidT[:, fc, hf * 512:(hf + 1) * 512], in0=pGU[b][:], in1=sg[s2][:], op=ALU.mult),
                             reads=[('pGU', b), ('sg', s2)], writes=['hidT'])
                    if nxt is not None:
                        idx_piece(nxt, fc)
                for sc in range(8):
                    for hf in range(2):
                        for fc in range(8):
                            S.op('pe', lambda e_: e_.matmul(pY[:, hf * 512:(hf + 1) * 512], lhsT=hidT[:, fc, sc * 128:(sc + 1) * 128],
                                                            rhs=Wd[i2][:, fc, hf * 512:(hf + 1) * 512], start=(fc == 0), stop=(fc == 7)),
                                 reads=['hidT', ('Wd', i2)], writes=['pY'])
                    y2 = sc % 2
                    S.op('dve', lambda e_: e_.scalar_tensor_tensor(out=ye[y2][:], in0=pY[:], scalar=ag[e % 2][sc][:, e:e + 1], in1=bc_g2[:],
                                                                   op0=ALU.mult, op1=ALU.mult), reads=['pY', ('ag', e % 2, sc), 'bc_g2'], writes=[('ye', y2)])
                    S.custom_dma('pool', lambda e_: e_.indirect_dma_start(out=out[:, :], out_offset=bass.IndirectOffsetOnAxis(ap=idxi[j2][:, sc:sc + 1], axis=0),
                                 in_=ye[y2][:, :], in_offset=None, compute_op=ALU.add), reads=[('ye', y2), ('idxi', j2)], writes=['out'])

            load_w(0)
            for p in range(8):
                idx_piece(0, p)
            gather(0)
            if nexp > 1:
                load_w(1)
                for p in range(8):
                    idx_piece(1, p)
            for e in range(nexp):
                xpose(e)
                if e + 1 < nexp:
                    gather(e + 1)
                    if e >= 1:
                        load_w(e + 1)
                ffn(e, e + 2 if e + 2 < nexp else None)
            if 'moe_idx' in dbg:
                dbg_out['idxi'] = dout("dbg_idxi", [128, 8], I32)
                S.dma('sp', dbg_out['idxi'], idxi[(nexp - 1) % 3][:], reads=[('idxi', (nexp - 1) % 3)])
            S.barrier()


def moe_host(inp):
    f = np.float32
    m16 = np.zeros((128, 128), f)
    for s in range(8):
        for s2 in range(8):
            m16[16 * s:16 * s + 16, 16 * s2:16 * s2 + 16] = np.eye(16, dtype=f)
    return {'w_e_gate': np.ascontiguousarray(inp['w_e_gate'][0]), 'w_e_up': np.ascontiguousarray(inp['w_e_up'][0]),
            'w_e_down': np.ascontiguousarray(inp['w_e_down'][0]), 'm16': m16}


D = 1024
NLAT = 8192
NCTX = 256
NEXT = NLAT + NCTX
NE2 = NEXT + NCTX
EPS = 1e-6
INCOLS = 4096


def build(stop_after=99, dbg=(), nblk_lim=17, skip_s5=False, mg_blocks=16, nexp=16):
    nc = bass.Bass("TRN2", target_bir_lowering=False)
    es = ExitStack()
    S = Sched(nc, es)
    dbg_out = {}

    def din(name, shape, dt=F32):
        return nc.dram_tensor(name, list(shape), dt, kind="ExternalInput").ap()

    def dscr(name, shape, dt):
        return nc.dram_tensor(name, list(shape), dt, kind="Internal").ap()

    def dout(name, shape, dt=F32):
        return nc.dram_tensor(name, list(shape), dt, kind="ExternalOutput").ap()

    def sb(name, shape, dt, stack=None):
        return (stack or es).enter_context(nc.sbuf_tensor(name, list(shape), dt))

    def ps(name, shape, dt, stack=None):
        return (stack or es).enter_context(nc.psum_tensor(name, list(shape), dt))

    xcat = din("xcat", [NEXT, D])
    cT = din("cT", [128, 8, 2])
    w_ada = din("w_ada", [D, 6 * D])
    b_ada = din("b_ada", [1, 6 * D])
    nmixT = din("nmixT", [128, 8])
    nffn = din("nffn", [1, D])
    w_in = din("w_in", [D, INCOLS])
    ident_in = din("ident", [128, 128])
    qkg = din("qkg", [128, 2])

    out = dout("out", [NLAT, D])

    uT_d = dscr("uT_d", [512, NE2], BF16)
    qT_d = dscr("qT_d", [512, NLAT], BF16)
    kT_d = dscr("kT_d", [512, NEXT], BF16)
    v_d = dscr("v_d", [NEXT, 8, 65], BF16)
    sgaT_d = dscr("sgaT_d", [D, NLAT], BF16)
    sgbT_d = dscr("sgbT_d", [D, NLAT], BF16)

    identf = sb("identf", [128, 128], F32)
    identb = sb("identb", [128, 128], BF16)
    mul1 = sb("mul1", [128, 8, 2], F32)
    add1 = sb("add1", [128, 8, 2], F32)
    bc_g1 = sb("bc_g1", [128, D], F32)
    bc_mul2 = sb("bc_mul2", [128, D], F32)
    bc_add2 = sb("bc_add2", [128, D], F32)
    bc_g2 = sb("bc_g2", [128, D], F32)

    epsc = sb("epsc", [128, 1], F32)
    S.op('dve', lambda e: e.memset(epsc[:], EPS), writes=['epsc'])
    S.dma('sp', identf[:], ident_in, writes=['identf'])
    S.dma('pool', identb[:], ident_in, writes=['identb'])

    with ExitStack() as p0:
        wada = sb("wada", [128, 8, 6 * D], BF16, p0)
        cs_f = sb("cs_f", [128, 8, 2], F32, p0)
        cs = sb("cs", [128, 8, 2], BF16, p0)
        brow = sb("brow", [2, 6 * D], F32, p0)
        modrow = sb("modrow", [2, 6 * D], F32, p0)
        sel = sb("sel", [2, 128], F32, p0)
        nmix = sb("nmix", [128, 8], F32, p0)
        colsb = sb("colsb", [128, 16, 2], F32, p0)
        pm = [ps("pm%d" % i, [2, 512], F32, p0) for i in range(2)]
        pcol = ps("pcol", [128, 16, 2], F32, p0)
        pbc = [ps("pbc%d" % i, [128, 512], F32, p0) for i in range(2)]

        S.dma('sp', cs_f[:], cT, writes=['cs_f'])
        S.dma('sp', brow[0:1, :], b_ada, writes=['brow0'])
        S.dma('sp', brow[1:2, :], b_ada, writes=['brow1'])
        S.dma('sp', nmix[:], nmixT, writes=['nmix'])
        S.dma('sp', bc_mul2[:], nffn.to_broadcast([128, D]), writes=['bc_mul2'])
        for dc in range(8):
            S.dma('pool', wada[:, dc, :].rearrange("p (a b) -> p a b", b=2048),
                  w_ada[dc * 128:(dc + 1) * 128, :].rearrange("p (a b) -> p a b", b=2048),
                  writes=[('wada', dc)])
        S.op('act', lambda e: e.activation(out=cs[:], in_=cs_f[:], func=AF.Silu), reads=['cs_f'], writes=['cs'])
        S.op('dve', lambda e: e.memset(sel[:], 0.0), writes=['sel'])
        S.op('dve', lambda e: e.memset(sel[0:1, :], 1.0), writes=['sel'])
        for cc in range(12):
            pt = pm[cc % 2]
            for dc in range(8):
                S.op('pe', lambda e: e.matmul(pt[:], lhsT=cs[:, dc, :], rhs=wada[:, dc, cc * 512:(cc + 1) * 512],
                                              start=(dc == 0), stop=(dc == 7)),
                     reads=['cs', ('wada', dc)], writes=[('pm', cc % 2)])
            S.op('dve', lambda e: e.tensor_tensor(out=modrow[:, cc * 512:(cc + 1) * 512], in0=pt[:],
                                                  in1=brow[:, cc * 512:(cc + 1) * 512], op=ALU.add),
                 reads=[('pm', cc % 2), 'brow0', 'brow1'], writes=[('modrow', cc)])
        for i in range(16):
            S.op('pe', lambda e: e.transpose(out=pcol[:, i, :], in_=modrow[0:2, i * 128:(i + 1) * 128],
                                             identity=identf[0:2, 0:2]),
                 reads=[('modrow', i // 4), 'identf'], writes=['pcol'])
        S.op('dve', lambda e: e.tensor_copy(out=colsb[:], in_=pcol[:]), reads=['pcol'], writes=['colsb'])
        S.op('dve', lambda e: e.tensor_copy(out=add1[:], in_=colsb[:, 0:8, :]), reads=['colsb'], writes=['add1'])
        S.op('dve', lambda e: e.tensor_scalar(out=colsb[:, 8:16, :], in0=colsb[:, 8:16, :], scalar1=1.0, scalar2=None,
                                              op0=ALU.add), reads=['colsb'], writes=['colsb'])
        S.op('dve', lambda e: e.tensor_tensor(out=mul1[:], in0=colsb[:, 8:16, :],
                                              in1=nmix[:].unsqueeze(2).to_broadcast([128, 8, 2]), op=ALU.mult),
             reads=['colsb', 'nmix'], writes=['mul1'])

        def bcast(dst, col0, mode, dkey):
            for h in range(2):
                pt = pbc[h]
                S.op('pe', lambda e: e.matmul(pt[:], lhsT=sel[:], rhs=modrow[:, col0 + h * 512: col0 + (h + 1) * 512],
                                              start=True, stop=True),
                     reads=['sel'] + [('modrow', c) for c in range(12)], writes=[('pbc', h)])
                dsl = dst[:, h * 512:(h + 1) * 512]
                if mode == 'copy':
                    S.op('dve', lambda e: e.tensor_copy(out=dsl, in_=pt[:]), reads=[('pbc', h)], writes=[dkey])
                else:
                    S.op('dve', lambda e: e.scalar_tensor_tensor(out=dsl, in0=pt[:], scalar=1.0, in1=dsl,
                                                                 op0=ALU.add, op1=ALU.mult),
                         reads=[('pbc', h), dkey], writes=[dkey])
        bcast(bc_g1, 2048, 'copy', 'bc_g1')
        bcast(bc_add2, 3072, 'copy', 'bc_add2')
        bcast(bc_mul2, 4096, 'mul1p', 'bc_mul2')
        bcast(bc_g2, 5120, 'copy', 'bc_g2')
        if 'mod' in dbg:
            dbg_out['mod'] = dout("dbg_mod", [2, 6 * D])
            S.dma('sp', dbg_out['mod'], modrow[:], reads=[('modrow', c) for c in range(12)])
            dbg_out['mul1'] = dout("dbg_mul1", [128, 8, 2])
            S.dma('sp', dbg_out['mul1'], mul1[:], reads=['mul1'])
            dbg_out['bcm2'] = dout("dbg_bcm2", [128, D])
            S.dma('sp', dbg_out['bcm2'], bc_mul2[:], reads=['bc_mul2'])
        S.barrier()

    if stop_after <= 0:
        return finish(nc, S, es, out, dbg_out)

    with ExitStack() as p1:
        win = sb("win", [128, 8, INCOLS], BF16, p1)
        xt = [sb("xt%d" % i, [128, D], F32, p1) for i in range(2)]
        junk = sb("junk", [128, D], BF16, p1)
        xn = [sb("xn%d" % i, [128, D], BF16, p1) for i in range(4)]
        ss = [sb("ss%d" % i, [128, 1], F32, p1) for i in range(4)]
        rstd = [sb("rstd%d" % i, [128, 1], F32, p1) for i in range(4)]
        hT = [sb("hT%d" % i, [128, 8, 512], BF16, p1) for i in range(2)]
        stg = [sb("stg%d" % i, [128, 512], BF16, p1) for i in range(4)]
        sq = [sb("sq%d" % i, [128, 512], BF16, p1) for i in range(2)]
        qk32 = [sb("qk32_%d" % i, [128, 512], F32, p1) for i in range(2)]
        rs = [sb("rs%d" % i, [128, 512], F32, p1) for i in range(2)]
        vst = [sb("vst%d" % i, [128, 8, 65], BF16, p1) for i in range(2)]
        bones = sb("bones", [128, 128], BF16, p1)
        gq = sb("gq", [128, 2], F32, p1)
        pT = [ps("pT%d" % i, [128, 8, 128], BF16, p1) for i in range(2)]
        pz = [ps("pz%d" % i, [128, 512], F32, p1) for i in range(4)]
        pms = [ps("pms%d" % i, [128, 512], F32, p1) for i in range(2)]

        for dc in range(8):
            S.dma('pool', win[:, dc, :].rearrange("p (a b) -> p a b", b=2048),
                  w_in[dc * 128:(dc + 1) * 128, :].rearrange("p (a b) -> p a b", b=2048),
                  writes=[('win', dc)])
        S.dma('sp', gq[:], qkg, writes=['gq'])
        S.op('dve', lambda e: e.tensor_scalar(out=gq[:, 0:1], in0=gq[:, 0:1], scalar1=0.125, scalar2=None, op0=ALU.mult), reads=['gq'], writes=['gq'])
        for i in range(2):
            S.op('dve', lambda e: e.memset(vst[i][:], 1.0), writes=[('vst', i)])
        S.op('dve', lambda e: e.memset(bones[:], 0.0), writes=['bones'])
        S.op('dve', lambda e: e.memset(bones[0:64, 0:64], 1.0 / 64), writes=['bones'])
        S.op('dve', lambda e: e.memset(bones[64:128, 64:128], 1.0 / 64), writes=['bones'])

        nblk = (NEXT + 511) // 512
        ctr = {'tile': 0, 'tpe': 0, 'stg': 0, 'z': 0, 'v': 0}

        def blk_info(b):
            if b == 0:
                return 0, 256
            return 256 + (b - 1) * 512, 512

        def head_pre(b):
            t0, nt = blk_info(b)
            for ti in range(nt // 128):
                i2 = ctr['tile'] % 2
                i4 = ctr['tile'] % 4
                ctr['tile'] += 1
                r0 = t0 + ti * 128
                S.dma('sp', xt[i2][:], xcat[r0:r0 + 128, :], writes=[('xt', i2)])
                S.op('act', lambda e: e.activation(out=junk[:], in_=xt[i2][:], func=AF.Square, accum_out=ss[i4][:]),
                     reads=[('xt', i2)], writes=['junk', ('ss', i4)])
                S.op('act', lambda e: e.activation(out=rstd[i4][:], in_=ss[i4][:], func=AF.Ln, scale=1.0 / D, bias=epsc[:, 0:1]),
                     reads=[('ss', i4), 'epsc'], writes=[('rstd', i4)])
                S.op('act', lambda e: e.activation(out=rstd[i4][:], in_=rstd[i4][:], func=AF.Exp, scale=-0.5),
                     reads=[('rstd', i4)], writes=[('rstd', i4)])
                S.op('act', lambda e: e.activation(out=xn[i4][:], in_=xt[i2][:], func=AF.Copy, scale=rstd[i4][:, 0:1]),
                     reads=[('xt', i2), ('rstd', i4)], writes=[('xn', i4)])

        def head_pe(b):
            t0, nt = blk_info(b)
            j = 1 if b == 0 else 0
            hb = hT[b % 2]
            hkey = ('hT', b % 2)
            for ti in range(nt // 128):
                i2 = ctr['tpe'] % 2
                i4 = ctr['tpe'] % 4
                ctr['tpe'] += 1
                for dc in range(8):
                    S.op('pe', lambda e: e.transpose(out=pT[i2][:, dc, :], in_=xn[i4][:, dc * 128:(dc + 1) * 128],
                                                     identity=identb[:]),
                         reads=[('xn', i4), 'identb'], writes=[('pT', i2)])
                for dc in range(8):
                    S.op('dve', lambda e: e.tensor_scalar(out=hb[:, dc, ti * 128:(ti + 1) * 128], in0=pT[i2][:, dc, :],
                                                          scalar1=mul1[:, dc, j:j + 1], scalar2=add1[:, dc, j:j + 1],
                                                          op0=ALU.mult, op1=ALU.add),
                         reads=[('pT', i2), 'mul1', 'add1'], writes=[hkey])

        pend_qk = []

        def chunk(b, cc):
            t0, nt = blk_info(b)
            is_ctx = (b == 0)
            hb = hT[b % 2]
            hkey = ('hT', b % 2)
            zi = ctr['z'] % 4
            ctr['z'] += 1
            pzt = pz[zi]
            for dc in range(8):
                S.op('pe', lambda e: e.matmul(pzt[:, 0:nt], lhsT=win[:, dc, cc * 128:(cc + 1) * 128], rhs=hb[:, dc, 0:nt],
                                              start=(dc == 0), stop=(dc == 7)),
                     reads=[('win', dc), hkey], writes=[('pz', zi)], sig=(dc == 7))
            si = ctr['stg'] % 4
            ctr['stg'] += 1
            st = stg[si]
            skey = ('stg', si)
            if not (4 <= cc < 12):
                while pend_qk:
                    pend_qk.pop(0)()
            if cc < 4:
                S.op('act', lambda e: e.activation(out=st[:, 0:nt], in_=pzt[:, 0:nt], func=AF.Copy),
                     reads=[('pz', zi)], writes=[skey])
                S.dma('sp', uT_d[cc * 128:(cc + 1) * 128, t0:t0 + nt], st[:, 0:nt], reads=[skey], writes=['uT_d'])
                if is_ctx:
                    S.dma('sp', uT_d[cc * 128:(cc + 1) * 128, NEXT:NEXT + nt], st[:, 0:nt], reads=[skey], writes=['uT_d'])
            elif cc < 12:
                isq = cc < 8
                qi = ctr['z'] % 2
                S.op('act', lambda e: e.activation(out=sq[qi][:, 0:nt], in_=pzt[:, 0:nt], func=AF.Square),
                     reads=[('pz', zi)], writes=[('sq', qi)])
                while pend_qk:
                    pend_qk.pop(0)()

                def tail(qi=qi, zi=zi, pzt=pzt, st=st, skey=skey, isq=isq, cc=cc, t0=t0, nt=nt):
                    S.op('pe', lambda e: e.matmul(pms[qi][:, 0:nt], lhsT=bones[:], rhs=sq[qi][:, 0:nt], start=True, stop=True),
                         reads=['bones', ('sq', qi)], writes=[('pms', qi)])
                    S.op('act', lambda e: e.activation(out=rs[qi][:, 0:nt], in_=pms[qi][:, 0:nt], func=AF.Ln, bias=epsc[:, 0:1]),
                         reads=[('pms', qi), 'epsc'], writes=[('rs', qi)])
                    S.op('act', lambda e: e.activation(out=rs[qi][:, 0:nt], in_=rs[qi][:, 0:nt], func=AF.Exp, scale=-0.5),
                         reads=[('rs', qi)], writes=[('rs', qi)])
                    gcol = gq[:, 0:1] if isq else gq[:, 1:2]
                    S.op('dve', lambda e: e.scalar_tensor_tensor(out=st[:, 0:nt], in0=pzt[:, 0:nt], scalar=gcol,
                                                                 in1=rs[qi][:, 0:nt], op0=ALU.mult, op1=ALU.mult),
                         reads=[('pz', zi), ('rs', qi), 'gq'], writes=[skey])
                    if isq:
                        S.dma('sp', qT_d[(cc - 4) * 128:(cc - 3) * 128, t0 - NCTX:t0 - NCTX + nt], st[:, 0:nt],
                              reads=[skey], writes=['qT_d'])
                    else:
                        S.dma('sp', kT_d[(cc - 8) * 128:(cc - 7) * 128, t0:t0 + nt], st[:, 0:nt], reads=[skey], writes=['kT_d'])
                pend_qk.append(tail)
            else:
                S.op('act', lambda e: e.activation(out=st[:, 0:nt], in_=pzt[:, 0:nt], func=AF.Sigmoid),
                     reads=[('pz', zi)], writes=[skey])
                if cc < 24:
                    dst = sgaT_d[(cc - 16) * 128:(cc - 15) * 128, t0 - NCTX:t0 - NCTX + nt]
                    dk = 'sgaT_d'
                else:
                    dst = sgbT_d[(cc - 24) * 128:(cc - 23) * 128, t0 - NCTX:t0 - NCTX + nt]
                    dk = 'sgbT_d'
                S.dma('sp', dst, st[:, 0:nt], reads=[skey], writes=[dk])

        def vpart(b):
            t0, nt = blk_info(b)
            hb = hT[b % 2]
            hkey = ('hT', b % 2)
            for ti in range(nt // 128):
                zi = ctr['z'] % 4
                ctr['z'] += 1
                pzt = pz[zi]
                for dc in range(8):
                    S.op('pe', lambda e: e.matmul(pzt[:], lhsT=hb[:, dc, ti * 128:(ti + 1) * 128], rhs=win[:, dc, 1536:2048],
                                                  start=(dc == 0), stop=(dc == 7)),
                         reads=[('win', dc), hkey], writes=[('pz', zi)], sig=(dc == 7))
                while pend_qk:
                    pend_qk.pop(0)()
                vi = ctr['v'] % 2
                ctr['v'] += 1
                S.op('act', lambda e: e.activation(out=vst[vi][:, :, 0:64], in_=pzt[:].rearrange("p (h d) -> p h d", d=64), func=AF.Copy),
                     reads=[('pz', zi)], writes=[('vst', vi)])
                S.dma('sp', v_d[t0 + ti * 128:t0 + (ti + 1) * 128], vst[vi][:], reads=[('vst', vi)], writes=['v_d'])

        head_pre(0)
        head_pe(0)
        for b in range(nblk_lim):
            if b == 0:
                ccs = list(range(0, 4)) + list(range(8, 12))
            else:
                ccs = list(range(0, 12)) + list(range(16, 32))
            n1, n2 = len(ccs) // 4, (3 * len(ccs)) // 4
            for cc in ccs[:n1]:
                chunk(b, cc)
            if b + 1 < nblk_lim:
                head_pre(b + 1)
            for cc in ccs[n1:n2]:
                chunk(b, cc)
            if b + 1 < nblk_lim:
                head_pe(b + 1)
            for cc in ccs[n2:]:
                chunk(b, cc)
            vpart(b)
        S.barrier()
    if 'p1' in dbg:
        for nm, t in (('uT', uT_d), ('qT', qT_d), ('kT', kT_d), ('sgaT', sgaT_d), ('sgbT', sgbT_d)):
            dbg_out[nm] = dout("dbg_" + nm, list(t.shape), BF16)
            for r0 in range(0, t.shape[0], 128):
                S.dma('sp', dbg_out[nm][r0:r0 + 128, :], t[r0:r0 + 128, :], reads=[])
    if stop_after <= 1:
        return finish(nc, S, es, out, dbg_out)
    yaT_d = dscr("yaT_d", [512, NLAT], BF16)
    r = 'ok' if skip_s5 else s5_phase(nc, S, es, sb, ps, din, dscr, dout, dbg, dbg_out, identf, identb, uT_d, yaT_d)
    if r == 'stop' or stop_after <= 2:
        return finish(nc, S, es, out, dbg_out)
    ybT_d = dscr("ybT_d", [512, NLAT], BF16)
    na_phase(nc, S, es, sb, ps, din, dscr, dout, dbg, dbg_out, identb, qT_d, kT_d, v_d, ybT_d)
    if stop_after <= 3:
        return finish(nc, S, es, out, dbg_out)
    h2_d = dscr("h2_d", [NLAT, D], BF16)
    aff_d = dscr("aff_d", [NLAT, 16], F32)
    affT_d = dscr("affT_d", [16, NLAT], F32)
    merge_phase(nc, S, es, sb, ps, din, dscr, dout, dbg, dbg_out, identf, identb, epsc, bc_g1, bc_mul2, bc_add2,
                xcat, out, yaT_d, ybT_d, sgaT_d, sgbT_d, h2_d, aff_d, affT_d, nblocks=mg_blocks)
    if stop_after <= 4:
        return finish(nc, S, es, out, dbg_out)
    moe_phase(nc, S, es, sb, ps, din, dscr, dout, dbg, dbg_out, identf, identb, bc_g2, out, h2_d, aff_d, affT_d, nexp=nexp)
    return finish(nc, S, es, out, dbg_out)


def finish(nc, S, es, out, dbg_out):
    S.barrier(['sp'])
    print("instructions", S.nins, "waits", S.nwaits)
    es.close()
    return nc, dbg_out


def host_inputs(inp, b):
    f = np.float32
    xcat = np.concatenate([inp['ctx'][b], inp['x'][b]], axis=0).astype(f)
    cT = np.stack([inp['c'][b].reshape(8, 128).T, inp['c_ctx'].reshape(8, 128).T], axis=-1).astype(f)
    qkg = np.stack([np.tile(inp['q_norm'][0], 2), np.tile(inp['k_norm'][0], 2)], axis=-1).astype(f)
    m = {
        'xcat': np.ascontiguousarray(xcat),
        'cT': np.ascontiguousarray(cT),
        'w_ada': np.ascontiguousarray(inp['w_ada'][0]),
        'b_ada': np.ascontiguousarray(inp['b_ada'][0][None, :]),
        'nmixT': np.ascontiguousarray(inp['norm_mix'][0].reshape(8, 128).T),
        'nffn': np.ascontiguousarray(inp['norm_ffn'][0][None, :]),
        'w_in': np.ascontiguousarray(inp['w_in'][0]),
        'ident': np.eye(128, dtype=f),
        'qkg': np.ascontiguousarray(qkg),
    }
    f32c = lambda a: np.ascontiguousarray(a.astype(f))
    lr = inp['ssm_lam_re'][0].reshape(2, 16, 2, 64)
    m['lamre_l'] = f32c(lr.transpose(2, 3, 0, 1).reshape(128, 32))
    li = inp['ssm_lam_im'][0].reshape(2, 16, 2, 64)
    m['lamim_l'] = f32c(li.transpose(2, 3, 0, 1).reshape(128, 32))
    ld = inp['ssm_log_dt'][0].reshape(2, 16, 2)
    m['logdt_l'] = f32c(np.broadcast_to(ld.transpose(2, 0, 1)[:, None, :, :], (2, 64, 2, 16)).reshape(128, 32))
    for nm, key in (('Bre_c', 'ssm_b_re'), ('Bim_c', 'ssm_b_im')):
        bb = inp[key][0].reshape(2, 16, 2, 64, 16)
        m[nm] = f32c(bb.transpose(2, 3, 0, 1, 4).reshape(128, 32, 16))
    for nm, key in (('Cre_c', 'ssm_c_re'), ('Cim_c', 'ssm_c_im')):
        cc = inp[key][0].reshape(2, 16, 2, 16, 64)
        m[nm] = f32c(cc.transpose(2, 4, 0, 1, 3).reshape(128, 32, 16))
    m['dskipT'] = f32c(inp['ssm_d'][0].reshape(4, 128).T)
    m['w_glu'] = f32c(inp['w_glu'][0])
    m['bgluT'] = f32c(inp['b_glu'][0].reshape(4, 128).T)
    m.update(na_host(inp))
    m['w_proj_a'] = f32c(inp['w_proj_a'][0]); m['w_proj_b'] = f32c(inp['w_proj_b'][0]); m['w_out'] = f32c(inp['w_out'][0])
    m.update(moe_host(inp))
    m['w_routerT'] = f32c(inp['w_router'][0].reshape(8, 128, 16).transpose(1, 0, 2))
    return m


_NC_CACHE = {}


def kernel(**inputs):
    inp = {k_: np.asarray(v_) for k_, v_ in inputs.items()}
    if 'nc' not in _NC_CACHE:
        nc, _ = build()
        _NC_CACHE['nc'] = nc
    nc = _NC_CACHE['nc']
    nb = inp['x'].shape[0]
    in_maps = [host_inputs(inp, b) for b in range(nb)]
    res = run_bass_kernel_spmd(nc, in_maps, core_ids=list(range(nb)))
    outs = [np.asarray(res.results[b]["out"], dtype=np.float32) for b in range(nb)]
    return np.stack(outs, axis=0)
```

```python
import numpy as np
from contextlib import ExitStack
import concourse.bass as bass
import concourse.mybir as mybir
from concourse.bass_utils import run_bass_kernel_spmd

F32 = mybir.dt.float32
BF16 = mybir.dt.bfloat16
I32 = mybir.dt.int32
U32 = mybir.dt.uint32
ALU = mybir.AluOpType
AF = mybir.ActivationFunctionType
AX = mybir.AxisListType
STRICT = False


class Sched:
    def __init__(self, nc, es, n_dma_sems=24):
        self.nc = nc
        self.e = {'pe': nc.tensor, 'act': nc.scalar, 'dve': nc.vector, 'pool': nc.gpsimd, 'sp': nc.sync}
        self.sems = {}
        self.cnt = {}
        for k in self.e:
            self.sems[k] = es.enter_context(nc.semaphore("sem_" + k))
            self.cnt[k] = 0
        self.nd = n_dma_sems
        self.qsems = {}
        for q in ('sp', 'pool', 'act'):
            n = n_dma_sems if q != 'act' else 16
            self.qsems[q] = []
            for i in range(n):
                sk = 'd_%s%d' % (q, i)
                self.sems[sk] = es.enter_context(nc.semaphore("dsem_%s%d" % (q, i)))
                self.cnt[sk] = 0
                self.qsems[q].append(sk)
        self.qrr = {'sp': 0, 'pool': 0, 'act': 0}
        self.drr = 0
        self.waited = {}
        self.lastw = {}
        self.readers = {}
        self.nwaits = 0
        self.nins = 0

    def _wait(self, eng, tok):
        sk, val, src = tok
        if self.waited.get((eng, sk), 0) >= val:
            return
        self.e[eng].wait_ge(self.sems[sk], val)
        self.waited[(eng, sk)] = val
        self.nwaits += 1

    def _deps(self, eng, reads, writes):
        for k in reads:
            t = self.lastw.get(k)
            if t is not None:
                self._wait(eng, t)
        for k in writes:
            t = self.lastw.get(k)
            if t is not None and (t[2] != eng or (STRICT and eng != 'pe')):
                self._wait(eng, t)
            for sk, (val, src) in self.readers.get(k, {}).items():
                if src != eng or (STRICT and eng != 'pe'):
                    self._wait(eng, (sk, val, src))

    def _record(self, tok, reads, writes):
        sk, val, src = tok
        for k in reads:
            self.readers.setdefault(k, {})[sk] = (val, src)
        for k in writes:
            self.lastw[k] = tok
            self.readers[k] = {}

    def op(self, eng, fn, reads=(), writes=(), sig=True):
        self._deps(eng, reads, writes)
        ins = fn(self.e[eng])
        if sig:
            self.cnt[eng] += 1
            ins.then_inc(self.sems[eng], 1)
            tok = (eng, self.cnt[eng], eng)
        else:
            tok = (eng, self.cnt[eng] + 1, eng)
        self._record(tok, reads, writes)
        self.nins += 1
        return tok

    def _dma_common(self, q, fn, reads, writes):
        self._deps(q, reads, writes)
        i = self.qrr[q]
        self.qrr[q] = (i + 1) % len(self.qsems[q])
        sk = self.qsems[q][i]
        if self.cnt[sk] > 0:
            self._wait(q, (sk, self.cnt[sk], None))
        ins = fn(self.e[q])
        self.cnt[sk] += 16
        ins.then_inc(self.sems[sk], 16)
        tok = (sk, self.cnt[sk], None)
        self._record(tok, reads, writes)
        self.nins += 1
        return tok

    def dma(self, q, out, in_, reads=(), writes=(), **kw):
        return self._dma_common(q, lambda e: e.dma_start(out=out, in_=in_, **kw), reads, writes)

    def custom_dma(self, q, fn, reads=(), writes=()):
        return self._dma_common(q, fn, reads, writes)

    def barrier(self, engines=None):
        for e in (engines or self.e):
            for sk, c in self.cnt.items():
                if c > 0:
                    self._wait(e, (sk, c, None))
        self.lastw = {}
        self.readers = {}

PI = float(np.pi)


def kn(t):
    n = t.name
    return n[2:] if n.startswith('s_') else n


def s5_phase(nc, S, es, sb, ps, din, dscr, dout, dbg, dbg_out, identf, identb, uT_d, yaT_d, nbanks=16):
    NEXT, NE2 = 8448, 8704
    lre_in = din("lamre_l", [128, 32]); lim_in = din("lamim_l", [128, 32]); ldt_in = din("logdt_l", [128, 32])
    Bre_in = din("Bre_c", [128, 32, 16]); Bim_in = din("Bim_c", [128, 32, 16])
    Cre_in = din("Cre_c", [128, 32, 16]); Cim_in = din("Cim_c", [128, 32, 16])
    dsk_in = din("dskipT", [128, 4]); wglu_in = din("w_glu", [512, 512]); bglu_in = din("bgluT", [128, 4])
    E_d = dscr("E_d", [128, 2, 4, 16, 2, 2, 128], BF16)
    G_d = dscr("G_d", [128, 64, 640], BF16)
    H_d = dscr("H_d", [2, 128, 16, 2, 544], BF16)

    with ExitStack() as s5:
        Kt_sb = sb("s_Kt_sb", [128, 128, 128], BF16, s5)
        aL = sb("s_aL", [128, 32], F32, s5); bL = sb("s_bL", [128, 32], F32, s5)
        diagD = sb("s_diagD", [128, 4, 128], BF16, s5)
        with ExitStack() as pa:
            def t32(name):
                return sb(name, [128, 32], F32, pa)
            lre, lim, ldt, dt, a, b = (t32(n) for n in ("lre", "lim", "ldt", "dt", "a_", "b_"))
            u1, u2, u3, u4, c16, s16, wre, wim = (t32(n) for n in ("u1", "u2", "u3", "u4", "c16", "s16", "wre", "wim"))
            Bcr = sb("s_Bcr", [128, 32, 16], F32, pa); Bci = sb("s_Bci", [128, 32, 16], F32, pa)
            Dr = sb("s_Dr", [128, 32, 32], F32, pa); Di = sb("s_Di", [128, 32, 32], F32, pa)
            Dr2 = sb("s_Dr2", [128, 32, 32], F32, pa); Di2 = sb("s_Di2", [128, 32, 32], F32, pa)
            Gr = sb("s_Gr", [128, 32, 32], F32, pa); Gi = sb("s_Gi", [128, 32, 32], F32, pa)
            T1 = sb("s_T1", [128, 32, 32], F32, pa); T2 = sb("s_T2", [128, 32, 32], F32, pa)
            T3 = sb("s_T3", [128, 32, 32], F32, pa); T4 = sb("s_T4", [128, 32, 32], F32, pa)
            P1 = sb("s_P1", [128, 32, 32], F32, pa); P2 = sb("s_P2", [128, 32, 32], F32, pa)
            P3 = sb("s_P3", [128, 32, 32], F32, pa); P4 = sb("s_P4", [128, 32, 32], F32, pa)
            Cbr = sb("s_Cbr", [128, 32, 32], BF16, pa); Cbni = sb("s_Cbni", [128, 32, 32], BF16, pa)
            Dbr = sb("s_Dbr", [128, 8, 160], BF16, pa); Dbi = sb("s_Dbi", [128, 8, 160], BF16, pa)
            Gbr = sb("s_Gbr", [128, 8, 160], BF16, pa); Gbni = sb("s_Gbni", [128, 8, 160], BF16, pa)
            Es = [sb("s_Es%d" % i, [128, 2, 4, 2, 2, 128], BF16, pa) for i in range(2)]
            m3 = sb("s_m3", [128, 1], F32, pa)
            dsk = sb("s_dsk", [128, 4], F32, pa)
            pk = [ps("s_pk%d" % i, [128, 128], F32, pa) for i in range(2)]
            pE = [ps("s_pE%d" % i, [128, 128], F32, pa) for i in range(4)]

            S.dma('sp', lre[:], lre_in, writes=['lre']); S.dma('sp', lim[:], lim_in, writes=['lim'])
            S.dma('sp', ldt[:], ldt_in, writes=['ldt']); S.dma('sp', dsk[:], dsk_in, writes=['dsk'])
            for X in (Dr, Di, Gr, Gi, Dr2, Di2):
                S.op('dve', lambda e: e.memset(X[:], 0.0), writes=[kn(X)])
            for X in (Dbr, Dbi, Gbr, Gbni):
                S.op('pool', lambda e: e.memset(X[:], 0.0), writes=[kn(X)])
            S.op('pool', lambda e: e.memset(Kt_sb[:], 0.0), writes=['Kt_sb'])
            for i in range(2):
                S.op('pool', lambda e: e.memset(Es[i][:], 0.0), writes=[('Es', i)])
            S.op('dve', lambda e: e.memset(m3[:], 1.0), writes=['m3'])
            S.op('dve', lambda e: e.memset(m3[64:96, :], 0.0), writes=['m3'])
            S.dma('sp', Bcr[:], Bre_in, writes=['Bcr']); S.dma('sp', Bci[:], Bim_in, writes=['Bci'])
            for h in range(2):
                S.dma('sp', Gr[64 * h:64 * h + 64, :, 16 * h:16 * h + 16], Cre_in[64 * h:64 * h + 64], reads=[], writes=['Gr'])
                S.dma('sp', Gi[64 * h:64 * h + 64, :, 16 * h:16 * h + 16], Cim_in[64 * h:64 * h + 64], reads=[], writes=['Gi'])
            for q in range(4):
                S.op('dve', lambda e: e.tensor_scalar(out=diagD[:, q, :], in0=identf[:], scalar1=dsk[:, q:q + 1], scalar2=None,
                                                      op0=ALU.mult), reads=['identf', 'dsk'], writes=['diagD'])
            V = lambda e: e
            def tt(out, in0, in1, op, r, w, eng='dve'):
                S.op(eng, lambda e: e.tensor_tensor(out=out, in0=in0, in1=in1, op=op), reads=r, writes=w)
            S.op('act', lambda e: e.activation(out=dt[:], in_=ldt[:], func=AF.Exp), reads=['ldt'], writes=['dt'])
            tt(u1[:], lre[:], dt[:], ALU.mult, ['lre', 'dt'], ['u1'])
            S.op('act', lambda e: e.activation(out=u1[:], in_=u1[:], func=AF.Exp), reads=['u1'], writes=['u1'])
            tt(u2[:], lim[:], dt[:], ALU.mult, ['lim', 'dt'], ['u2'])
            S.op('act', lambda e: e.activation(out=s16[:], in_=u2[:], func=AF.Sin, scale=1.0 / 16), reads=['u2'], writes=['s16'])
            S.op('act', lambda e: e.activation(out=u3[:], in_=u2[:], func=AF.Sin, scale=1.0 / 32), reads=['u2'], writes=['u3'])
            tt(u3[:], u3[:], u3[:], ALU.mult, ['u3'], ['u3'])
            S.op('dve', lambda e: e.tensor_scalar(out=c16[:], in0=u3[:], scalar1=-2.0, scalar2=1.0, op0=ALU.mult, op1=ALU.add),
                 reads=['u3'], writes=['c16'])

            def csquare(cr, ci, n):
                for _ in range(n):
                    tt(u3[:], cr[:], cr[:], ALU.mult, [kn(cr)], ['u3'])
                    tt(u4[:], ci[:], ci[:], ALU.mult, [kn(ci)], ['u4'])
                    tt(ci[:], cr[:], ci[:], ALU.mult, [kn(cr), kn(ci)], [kn(ci)])
                    S.op('dve', lambda e: e.tensor_scalar(out=ci[:], in0=ci[:], scalar1=2.0, scalar2=None, op0=ALU.mult),
                         reads=[kn(ci)], writes=[kn(ci)])
                    tt(cr[:], u3[:], u4[:], ALU.subtract, ['u3', 'u4'], [kn(cr)])
            csquare(c16, s16, 4)
            tt(a[:], u1[:], c16[:], ALU.mult, ['u1', 'c16'], ['a_'])
            tt(b[:], u1[:], s16[:], ALU.mult, ['u1', 's16'], ['b_'])
            S.op('dve', lambda e: e.tensor_copy(out=aL[:], in_=c16[:]), reads=['c16'], writes=['aL'])
            S.op('dve', lambda e: e.tensor_copy(out=bL[:], in_=s16[:]), reads=['s16'], writes=['bL'])
            csquare(aL, bL, 4)
            tt(u3[:], lre[:], dt[:], ALU.mult, ['lre', 'dt'], ['u3'])
            S.op('act', lambda e: e.activation(out=u3[:], in_=u3[:], func=AF.Exp, scale=16.0), reads=['u3'], writes=['u3'])
            tt(aL[:], aL[:], u3[:], ALU.mult, ['aL', 'u3'], ['aL'])
            tt(bL[:], bL[:], u3[:], ALU.mult, ['bL', 'u3'], ['bL'])
            S.op('dve', lambda e: e.tensor_scalar(out=u1[:], in0=a[:], scalar1=-1.0, scalar2=None, op0=ALU.add), reads=['a_'], writes=['u1'])
            tt(u2[:], lre[:], lre[:], ALU.mult, ['lre'], ['u2'])
            tt(u3[:], lim[:], lim[:], ALU.mult, ['lim'], ['u3'])
            tt(u2[:], u2[:], u3[:], ALU.add, ['u2', 'u3'], ['u2'])
            S.op('dve', lambda e: e.reciprocal(out=u2[:], in_=u2[:]), reads=['u2'], writes=['u2'])
            tt(u3[:], u1[:], lre[:], ALU.mult, ['u1', 'lre'], ['u3'])
            tt(u4[:], b[:], lim[:], ALU.mult, ['b_', 'lim'], ['u4'])
            tt(u3[:], u3[:], u4[:], ALU.add, ['u3', 'u4'], ['u3'])
            tt(wre[:], u3[:], u2[:], ALU.mult, ['u3', 'u2'], ['wre'])
            tt(u3[:], b[:], lre[:], ALU.mult, ['b_', 'lre'], ['u3'])
            tt(u4[:], u1[:], lim[:], ALU.mult, ['u1', 'lim'], ['u4'])
            tt(u3[:], u3[:], u4[:], ALU.subtract, ['u3', 'u4'], ['u3'])
            tt(wim[:], u3[:], u2[:], ALU.mult, ['u3', 'u2'], ['wim'])
            bc16 = lambda t: t[:].unsqueeze(2).to_broadcast([128, 32, 16])
            Tc = [T1[:, :, 0:16], T2[:, :, 0:16], T3[:, :, 0:16], T4[:, :, 0:16]]
            tt(Tc[0], Bcr[:], bc16(wre), ALU.mult, ['Bcr', 'wre'], ['T1'])
            tt(Tc[1], Bci[:], bc16(wim), ALU.mult, ['Bci', 'wim'], ['T2'])
            tt(Tc[2], Bci[:], bc16(wre), ALU.mult, ['Bci', 'wre'], ['T3'])
            tt(Tc[3], Bcr[:], bc16(wim), ALU.mult, ['Bcr', 'wim'], ['T4'])
            for h in range(2):
                sl = slice(64 * h, 64 * h + 64)
                tt(Dr[sl, :, 16 * h:16 * h + 16], T1[sl, :, 0:16], T2[sl, :, 0:16], ALU.subtract, ['T1', 'T2'], ['Dr'])
                tt(Di[sl, :, 16 * h:16 * h + 16], T3[sl, :, 0:16], T4[sl, :, 0:16], ALU.add, ['T3', 'T4'], ['Di'])
            S.op('dve', lambda e: e.tensor_copy(out=Cbr[:], in_=Gr[:]), reads=['Gr'], writes=['Cbr'])
            S.op('dve', lambda e: e.tensor_scalar(out=Cbni[:], in0=Gi[:], scalar1=-1.0, scalar2=None, op0=ALU.mult),
                 reads=['Gi'], writes=['Cbni'])
            bc32 = lambda t: t[:].unsqueeze(2).to_broadcast([128, 32, 32])

            def cmul(Xr, Xi, eng, Yr=None, Yi=None):
                Yr = Yr or Xr; Yi = Yi or Xi
                A1, A2, A3, A4 = (T1, T2, T3, T4) if eng == 'dve' else (P1, P2, P3, P4)
                tt(A1[:], Xr[:], bc32(a), ALU.mult, [kn(Xr), 'a_'], [kn(A1)], eng)
                tt(A2[:], Xi[:], bc32(b), ALU.mult, [kn(Xi), 'b_'], [kn(A2)], eng)
                tt(A3[:], Xi[:], bc32(a), ALU.mult, [kn(Xi), 'a_'], [kn(A3)], eng)
                tt(A4[:], Xr[:], bc32(b), ALU.mult, [kn(Xr), 'b_'], [kn(A4)], eng)
                tt(Yr[:], A1[:], A2[:], ALU.subtract, [kn(A1), kn(A2)], [kn(Yr)], eng)
                tt(Yi[:], A3[:], A4[:], ALU.add, [kn(A3), kn(A4)], [kn(Yi)], eng)

            def to5(dst, src, scale, key):
                d4 = dst[:].rearrange("p g (s c) -> p g s c", c=32)
                s4 = src[:].rearrange("p (g s) c -> p g s c", s=4)
                S.op('dve', lambda e: e.tensor_scalar(out=d4[:, :, 0:3, :], in0=s4[:, :, 0:3, :], scalar1=scale, scalar2=None, op0=ALU.mult),
                     reads=[kn(src)], writes=[key])
                S.op('dve', lambda e: e.tensor_scalar(out=d4[:, :, 4, :], in0=s4[:, :, 3, :], scalar1=scale, scalar2=None, op0=ALU.mult),
                     reads=[kn(src)], writes=[key])

            Dpairs = [(Dr, Di), (Dr2, Di2)]
            for j in range(16):
                Dr, Di = Dpairs[j % 2]
                to5(Dbr, Dr, 1.0, 'Dbr'); to5(Dbi, Di, 1.0, 'Dbi')
                if j < 15:
                    cmul(Dr, Di, 'dve', Dpairs[(j + 1) % 2][0], Dpairs[(j + 1) % 2][1])
                for d in range(2):
                    for q in range(4):
                        idx = (d * 4 + q) * 16 + j
                        pkt = pk[idx % 2]; pkk = ('pk', idx % 2)
                        for pl in range(4):
                            pair = d * 16 + q * 4 + pl
                            if pl < 3:
                                osl = pkt[32 * pl:32 * pl + 32, 32 * pl:32 * pl + 32]; csl = slice(32 * pl, 32 * pl + 32)
                            else:
                                osl = pkt[64:128, 96:128]; csl = slice(96, 160)
                            S.op('pe', lambda e: e.matmul(osl, lhsT=Dbr[:, d * 4 + q, csl], rhs=Cbr[:, pair, :], start=True, stop=False),
                                 reads=['Dbr', 'Cbr'], writes=[pkk])
                            S.op('pe', lambda e: e.matmul(osl, lhsT=Dbi[:, d * 4 + q, csl], rhs=Cbni[:, pair, :], start=False, stop=True),
                                 reads=['Dbi', 'Cbni'], writes=[pkk])
                        for pl in range(4):
                            if pl < 3:
                                rs_, cs_ = slice(32 * pl, 32 * pl + 32), slice(32 * pl, 32 * pl + 32)
                            else:
                                rs_, cs_ = slice(64, 128), slice(96, 128)
                            S.op('act', lambda e: e.activation(out=Kt_sb[rs_, idx, cs_], in_=pkt[rs_, cs_], func=AF.Copy),
                                 reads=[pkk], writes=['Kt_sb'])
                Et = Es[j % 2]; ek = ('Es', j % 2)
                n_e = 0
                for reim, X in ((0, Dr), (1, Di)):
                    for d in range(2):
                        for q in range(4):
                            pet = pE[n_e % 4]; pek = ('pE', n_e % 4); n_e += 1
                            src = X[:, d * 16 + q * 4:d * 16 + q * 4 + 4, :].rearrange("p a b -> p (a b)")
                            S.op('pe', lambda e: e.transpose(out=pet[:], in_=src, identity=identf[:]), reads=[kn(X), 'identf'], writes=[pek])
                            S.op('act', lambda e: e.activation(out=Et[:, d, q, reim, 0, :], in_=pet[:], func=AF.Copy), reads=[pek], writes=[ek])
                            S.op('dve', lambda e: e.tensor_scalar(out=Et[64:128, d, q, reim, 1, :], in0=pet[64:128, :], scalar1=m3[64:128, 0:1],
                                                                  scalar2=None, op0=ALU.mult), reads=[pek, 'm3'], writes=[ek])
                for d in range(2):
                    S.dma('sp', E_d[:, d, :, j, :, :, :], Et[:, d], reads=[ek], writes=['E_d'])
                cmul(Gr, Gi, 'pool')
                to5(Gbr, Gr, 1.0, 'Gbr'); to5(Gbni, Gi, -1.0, 'Gbni')
                for d in range(2):
                    S.dma('sp', G_d[:, (d * 16 + j) * 2 + 0, :], Gbr[:, d * 4:d * 4 + 4, :].rearrange("p a b -> p (a b)"), reads=['Gbr'], writes=['G_d'])
                    S.dma('sp', G_d[:, (d * 16 + j) * 2 + 1, :], Gbni[:, d * 4:d * 4 + 4, :].rearrange("p a b -> p (a b)"), reads=['Gbni'], writes=['G_d'])
            S.barrier()
        if 's5prep' in dbg:
            dbg_out['Kt'] = dout("dbg_Kt", [128, 128, 128], BF16)
            S.dma('sp', dbg_out['Kt'], Kt_sb[:], reads=['Kt_sb'])
            dbg_out['aL'] = dout("dbg_aL", [128, 2, 32])
            S.dma('sp', dbg_out['aL'][:, 0, :], aL[:], reads=['aL']); S.dma('sp', dbg_out['aL'][:, 1, :], bL[:], reads=['bL'])
            dbg_out['G'] = dout("dbg_G", [128, 64, 640], BF16)
            S.dma('sp', dbg_out['G'], G_d, reads=['G_d'])
            dbg_out['E'] = dout("dbg_E", [128, 2, 4, 16, 2, 2, 128], BF16)
            for d in range(2):
                S.dma('sp', dbg_out['E'][:, d], E_d[:, d], reads=['E_d'])
            return 'stop'
        return s5_main(nc, S, es, sb, ps, din, dscr, dout, dbg, dbg_out, identf, identb, uT_d, yaT_d, nbanks,
                       s5, Kt_sb, aL, bL, diagD, E_d, G_d, H_d, wglu_in, bglu_in)


def s5_main(nc, S, es, sb, ps, din, dscr, dout, dbg, dbg_out, identf, identb, uT_d, yaT_d, nbanks,
            s5, Kt_sb, aL, bL, diagD, E_d, G_d, H_d, wglu_in, bglu_in):
    NCH = 528
    with ExitStack() as pb:
        uTb = [sb("s_uTb%d" % i, [128, 2048], BF16, pb) for i in range(3)]
        Esl = [sb("s_Esl%d" % i, [128, 16, 2, 2, 128], BF16, pb) for i in range(2)]
        Ssb = [[sb("s_Ssb%d_%d" % (d, i), [128, 16, 2, 128], F32, pb) for i in range(2)] for d in range(2)]
        hist = [sb("s_hist%d" % d, [128, 16, 2, 129], F32, pb) for d in range(2)]
        histb = [sb("s_histb%d" % d, [128, 16, 2, 128], BF16, pb) for d in range(2)]
        tP = [sb("s_tP%d" % d, [128, 16, 2], F32, pb) for d in range(2)]
        tQ = [sb("s_tQ%d" % d, [128, 16, 2], F32, pb) for d in range(2)]
        pS = [ps("s_pS%d" % i, [128, 4, 128], F32, pb) for i in range(4)]
        for d in range(2):
            S.op('dve' if d == 0 else 'pool', lambda e: e.memset(hist[d][:], 0.0), writes=[('hist', d)])
        fblocks = [(0, 128), (128, 128), (256, 128), (384, 128), (512, 16)]
        bblocks = [(512, 16), (384, 128), (256, 128), (128, 128), (0, 128)]
        esl_ctr = 0
        ps_ctr = 0
        for step in range(5):
            for d in range(2):
                k0, nk = (fblocks if d == 0 else bblocks)[step]
                Sb = Ssb[d][step % 2]; skey = ('Ssb', d, step % 2)
                base = 0 if d == 0 else 256
                for q in range(4):
                    et = Esl[esl_ctr % 2]; ekey = ('Esl', esl_ctr % 2); esl_ctr += 1
                    S.dma('sp', et[:], E_d[:, d, q], reads=['E_d'], writes=[ekey])
                    ut = uTb[(esl_ctr - 1) % 3]; ukey = ('uTb', (esl_ctr - 1) % 3)
                    S.dma('sp', ut[:, 0:16 * nk], uT_d[q * 128:(q + 1) * 128, base + 16 * k0: base + 16 * k0 + 16 * nk], reads=['uT_d'], writes=[ukey])
                    for pl in range(4):
                        pst = pS[ps_ctr % 4]; pskey = ('pS', ps_ctr % 4); ps_ctr += 1
                        if pl < 3:
                            rsl, alt = slice(32 * pl, 32 * pl + 32), 0
                        else:
                            rsl, alt = slice(64, 128), 1
                        for reim in range(2):
                            for i in range(16):
                                j = 15 - i if d == 0 else i
                                rhs = ut[rsl, i:i + 16 * (nk - 1) + 1:16]
                                S.op('pe', lambda e: e.matmul(pst[:, reim, 0:nk], lhsT=et[rsl, j, reim, alt, :], rhs=rhs,
                                                              start=(i == 0), stop=(i == 15)),
                                     reads=[ekey, ukey], writes=[pskey])
                        S.op('act', lambda e: e.activation(out=Sb[:, q * 4 + pl, :, 0:nk], in_=pst[:, 0:2, 0:nk], func=AF.Copy),
                             reads=[pskey], writes=[skey])
            def chain_ops(d, kk):
                k0, nk = (fblocks if d == 0 else bblocks)[step]
                Sb = Ssb[d][step % 2]; skey = ('Ssb', d, step % 2)
                hk = ('hist', d)
                A2 = aL[:, d * 16:(d + 1) * 16].unsqueeze(2).to_broadcast([128, 16, 2])
                B2 = bL[:, d * 16:(d + 1) * 16].unsqueeze(2).to_broadcast([128, 16, 2])
                H = hist[d]
                if d == 0:
                    cprev, ccur, scol = kk, kk + 1, kk
                else:
                    cprev, ccur, scol = nk - kk, nk - 1 - kk, nk - 1 - kk
                prev = H[:, :, :, cprev]
                P_, Q_ = tP[d], tQ[d]
                eng = 'dve'
                return [
                    lambda: S.op(eng, lambda e: e.tensor_tensor(out=P_[:], in0=prev, in1=A2, op=ALU.mult), reads=[hk, 'aL'], writes=[('tP', d)]),
                    lambda: S.op(eng, lambda e: e.tensor_tensor(out=Q_[:], in0=prev, in1=B2, op=ALU.mult), reads=[hk, 'bL'], writes=[('tQ', d)]),
                    lambda: S.op(eng, lambda e: e.tensor_tensor(out=P_[:], in0=P_[:], in1=Sb[:, :, :, scol], op=ALU.add),
                                 reads=[('tP', d), skey], writes=[('tP', d)]),
                    lambda: S.op(eng, lambda e: e.tensor_tensor(out=H[:, :, 0, ccur], in0=P_[:, :, 0], in1=Q_[:, :, 1], op=ALU.subtract),
                                 reads=[('tP', d), ('tQ', d)], writes=[hk]),
                    lambda: S.op(eng, lambda e: e.tensor_tensor(out=H[:, :, 1, ccur], in0=P_[:, :, 1], in1=Q_[:, :, 0], op=ALU.add),
                                 reads=[('tP', d), ('tQ', d)], writes=[hk]),
                ]
            nks = [(fblocks if d == 0 else bblocks)[step][1] for d in range(2)]
            for kk in range(max(nks)):
                lists = [chain_ops(d, kk) for d in range(2) if kk < nks[d]]
                for oi in range(5):
                    for l in lists:
                        l[oi]()
            for d in range(2):
                eng = 'dve' if d == 0 else 'pool'
                k0, nk = (fblocks if d == 0 else bblocks)[step]
                hk = ('hist', d)
                H = hist[d]
                if d == 0:
                    S.op(eng, lambda e: e.tensor_copy(out=histb[d][:, :, :, 0:nk], in_=H[:, :, :, 1:nk + 1]), reads=[hk], writes=[('histb', d)])
                    S.op(eng, lambda e: e.tensor_copy(out=H[:, :, :, 0], in_=H[:, :, :, nk]), reads=[hk], writes=[hk])
                else:
                    S.op(eng, lambda e: e.tensor_copy(out=histb[d][:, :, :, 0:nk], in_=H[:, :, :, 0:nk]), reads=[hk], writes=[('histb', d)])
                    S.op(eng, lambda e: e.tensor_copy(out=H[:, :, :, 128], in_=H[:, :, :, 0]), reads=[hk], writes=[hk])
                S.dma('pool', H_d[d, :, :, :, k0:k0 + nk], histb[d][:, :, :, 0:nk], reads=[('histb', d)], writes=['H_d'])
        S.barrier()
    if 's5lvl3' in dbg:
        dbg_out['H'] = dout("dbg_H", [2, 128, 16, 2, 544], BF16)
        for d in range(2):
            S.dma('sp', dbg_out['H'][d], H_d[d], reads=['H_d'])
        return 'stop'

    with ExitStack() as pc:
        Gsb = sb("s_Gsb", [128, 64, 640], BF16, pc)
        wglu = sb("s_wglu", [128, 4, 512], BF16, pc)
        bglu = sb("s_bglu", [128, 4], F32, pc)
        ub = [sb("s_ub%d" % i, [128, 4, 544], BF16, pc) for i in range(2)]
        Hf = [sb("s_Hf%d" % i, [128, 16, 2, 32], BF16, pc) for i in range(2)]
        Hb = [sb("s_Hb%d" % i, [128, 16, 2, 32], BF16, pc) for i in range(2)]
        aT = [sb("s_aT%d" % i, [128, 4, 512], BF16, pc) for i in range(2)]
        sig = [sb("s_sig%d" % i, [128, 512], BF16, pc) for i in range(2)]
        yo = [sb("s_yo%d" % i, [128, 512], BF16, pc) for i in range(4)]
        py = [ps("s_py%d" % i, [128, 512], F32, pc) for i in range(4)]
        pg = [ps("s_pg%d" % i, [128, 512], F32, pc) for i in range(2)]
        for c in range(4):
            S.dma('sp', Gsb[:, c * 16:(c + 1) * 16, :], G_d[:, c * 16:(c + 1) * 16, :], reads=['G_d'], writes=[('Gsb', c)])
        gkeys = [('Gsb', c) for c in range(4)]
        for q in range(4):
            S.dma('pool', wglu[:, q, :], wglu_in[q * 128:(q + 1) * 128, :], writes=['wglu'])
        S.dma('sp', bglu[:], bglu_in, writes=['bglu'])
        yo_ctr = 0
        for n in range(nbanks):
            i2 = n % 2
            e0 = 256 + 512 * n
            S.dma('sp', ub[i2][:], uT_d[:, e0 - 16:e0 + 528].rearrange("(q p) t -> p q t", p=128), reads=['uT_d'], writes=[('ub', i2)])
            kf = 16 + 32 * n
            kb = 32 * n
            S.dma('sp', Hf[i2][:], H_d[0, :, :, :, kf - 1:kf + 31], reads=['H_d'], writes=[('Hf', i2)])
            S.dma('sp', Hb[i2][:], H_d[1, :, :, :, kb + 1:kb + 33], reads=['H_d'], writes=[('Hb', i2)])
            for q in range(4):
                pyt = py[q]; pk_ = ('py', q)
                pyv = pyt[:].rearrange("p (k j) -> p k j", j=16)
                uc = ub[i2][:, q, 16:528]
                ucv = uc.rearrange("p (k j) -> p k j", j=16)
                rd = [('ub', i2), 'Kt_sb', 'diagD']
                S.op('pe', lambda e: e.matmul(pyt[:], lhsT=diagD[:, q, :], rhs=uc, start=True, stop=False), reads=rd, writes=[pk_])
                S.op('pe', lambda e: e.matmul(pyt[:], lhsT=Kt_sb[:, (0 * 4 + q) * 16 + 0, :], rhs=uc, start=False, stop=False), reads=rd, writes=[pk_])
                for tau in range(1, 16):
                    S.op('pe', lambda e: e.matmul(pyv[:, :, tau:16], lhsT=Kt_sb[:, (0 * 4 + q) * 16 + tau, :], rhs=ucv[:, :, 0:16 - tau],
                                                  start=False, stop=False), reads=rd, writes=[pk_])
                S.op('pe', lambda e: e.matmul(pyt[:], lhsT=Kt_sb[:, (1 * 4 + q) * 16 + 0, :], rhs=uc, start=False, stop=False), reads=rd, writes=[pk_])
                for tau in range(1, 16):
                    S.op('pe', lambda e: e.matmul(pyv[:, :, 0:16 - tau], lhsT=Kt_sb[:, (1 * 4 + q) * 16 + tau, :], rhs=ucv[:, :, tau:16],
                                                  start=False, stop=False), reads=rd, writes=[pk_])
                for d in range(2):
                    Hx = Hf[i2] if d == 0 else Hb[i2]
                    hkey = ('Hf', i2) if d == 0 else ('Hb', i2)
                    for j in range(16):
                        m = j + 1 if d == 0 else 16 - j
                        for pl in range(4):
                            pair = q * 4 + pl
                            if pl < 3:
                                osl = pyv[32 * pl:32 * pl + 32, :, j]; csl = slice(q * 160 + 32 * pl, q * 160 + 32 * pl + 32)
                            else:
                                osl = pyv[64:128, :, j]; csl = slice(q * 160 + 96, q * 160 + 160)
                            for reim in range(2):
                                last = (d == 1 and j == 15 and pl == 3 and reim == 1)
                                S.op('pe', lambda e: e.matmul(osl, lhsT=Gsb[:, (d * 16 + m - 1) * 2 + reim, csl], rhs=Hx[:, pair, reim, :],
                                                              start=False, stop=last), reads=gkeys + [hkey], writes=[pk_])
                S.op('act', lambda e: e.activation(out=aT[i2][:, q, :], in_=pyt[:], func=AF.Gelu), reads=[pk_], writes=[('aT', i2, q)])
            for oc in range(4):
                pgt = pg[oc % 2]; pgk = ('pg', oc % 2)
                for q in range(4):
                    S.op('pe', lambda e: e.matmul(pgt[:], lhsT=wglu[:, q, oc * 128:(oc + 1) * 128], rhs=aT[i2][:, q, :],
                                                  start=(q == 0), stop=(q == 3)), reads=['wglu', ('aT', i2, q)], writes=[pgk])
                sg = sig[oc % 2]; sgk = ('sig', oc % 2)
                S.op('act', lambda e: e.activation(out=sg[:], in_=pgt[:], func=AF.Sigmoid, bias=bglu[:, oc:oc + 1]),
                     reads=[pgk, 'bglu'], writes=[sgk])
                yt = yo[yo_ctr % 4]; yk = ('yo', yo_ctr % 4); yo_ctr += 1
                S.op('dve', lambda e: e.tensor_tensor(out=yt[:], in0=aT[i2][:, oc, :], in1=sg[:], op=ALU.mult),
                     reads=[('aT', i2, oc), sgk], writes=[yk])
                S.dma('pool', yaT_d[oc * 128:(oc + 1) * 128, 512 * n:512 * n + 512], yt[:], reads=[yk], writes=['yaT_d'])
        S.barrier()
    if 's5' in dbg:
        dbg_out['yaT'] = dout("dbg_yaT", [512, 8192], BF16)
        for r0 in range(0, 512, 128):
            S.dma('sp', dbg_out['yaT'][r0:r0 + 128, :], yaT_d[r0:r0 + 128, :], reads=['yaT_d'])
    return 'ok'

NEG = -30000.0


def na_phase(nc, S, es, sb, ps, din, dscr, dout, dbg, dbg_out, identb, qT_d, kT_d, v_d, ybT_d, nblocks=16):
    rpbT_in = din("rpbT", [128, 8 * 14 * 64])
    maskT_in = din("maskT", [128, 64])
    with ExitStack() as pa:
        Tb = sb("n_Tb", [128, 8, 14, 64], F32, pa)
        mk = sb("n_mk", [128, 64], F32, pa)
        kctx = sb("n_kctx", [128, 4, 256], BF16, pa)
        vctx = sb("n_vctx", [128, 2, 8, 65], BF16, pa)
        qb = [sb("n_qb%d" % i, [128, 4, 512], BF16, pa) for i in range(2)]
        kw = [sb("n_kw%d" % i, [128, 4, 960], BF16, pa) for i in range(2)]
        vw = [sb("n_vw%d" % i, [128, 15, 8, 65], BF16, pa) for i in range(2)]
        sS = [sb("n_sS%d" % i, [128, 4, 64], F32, pa) for i in range(3)]
        P = [sb("n_Pn%d" % i, [128, 6, 64], BF16, pa) for i in range(3)]
        rc = [sb("n_rc%d" % i, [128, 8], F32, pa) for i in range(2)]
        ybt = [sb("n_ybt%d" % i, [128, 8, 64], BF16, pa) for i in range(2)]
        ybT = [sb("n_ybT%d" % i, [128, 4, 512], BF16, pa) for i in range(2)]
        pS = [ps("n_pS%d" % i, [128, 6, 64], F32, pa) for i in range(3)]
        pO = [ps("n_pO%d" % i, [128, 4, 65], F32, pa) for i in range(4)]
        pT = ps("n_pTn", [128, 4, 128], BF16, pa)

        S.dma('sp', Tb[:].rearrange("p h a q -> p (h a q)"), rpbT_in, writes=['Tb'])
        S.dma('sp', mk[:], maskT_in, writes=['mk'])
        S.dma('sp', kctx[:], kT_d[:, 0:256].rearrange("(c p) t -> p c t", p=128), reads=['kT_d'], writes=['kctx'])
        S.dma('sp', vctx[:], v_d[0:256].rearrange("(m p) h d -> p m h d", p=128), reads=['v_d'], writes=['vctx'])
        S.op('dve', lambda e: e.tensor_tensor(out=Tb[:].rearrange("p h a q -> p (h a) q"), in0=Tb[:].rearrange("p h a q -> p (h a) q"),
                                              in1=mk[:].unsqueeze(1).to_broadcast([128, 112, 64]), op=ALU.add),
             reads=['Tb', 'mk'], writes=['Tb'])
        item = 0
        pend = None
        for b in range(nblocks):
            i2 = b % 2
            r0 = 8 * b
            lo = min(max(r0 - 4, 0), 120)
            hi = min(max(r0 + 7 - 4, 0), 120) + 8
            nrows = hi - lo
            S.dma('sp', qb[i2][:], qT_d[:, 512 * b:512 * b + 512].rearrange("(c p) t -> p c t", p=128), reads=['qT_d'], writes=[('qb', i2)])
            S.dma('sp', kw[i2][:, :, 0:64 * nrows], kT_d[:, 256 + 64 * lo:256 + 64 * hi].rearrange("(c p) t -> p c t", p=128),
                  reads=['kT_d'], writes=[('kw', i2)])
            for s in range(nrows - 1):
                S.dma('sp', vw[i2][:, s], v_d[256 + 64 * (lo + s):256 + 64 * (lo + s) + 128], reads=['v_d'], writes=[('vw', i2)])
            for rr in range(8):
                r = r0 + rr
                krs = min(max(r - 4, 0), 120)
                a0 = krs - r + 7
                half = 64 * (r % 2)
                pp = (r // 2) % 2
                for h in range(8):
                    hc, hb = h // 2, 64 * (h % 2)
                    it = item % 3
                    item += 1
                    pst = pS[it]; psk = ('pS', it)
                    for c in range(4):
                        kc0 = 64 * (krs + 2 * c - lo)
                        S.op('pe', lambda e: e.matmul(pst[:, c, :], lhsT=kw[i2][hb:hb + 64, hc, kc0:kc0 + 128],
                                                      rhs=qb[i2][hb:hb + 64, hc, 64 * rr:64 * rr + 64], start=True, stop=True),
                             reads=[('kw', i2), ('qb', i2)], writes=[psk])
                    for c in range(2):
                        S.op('pe', lambda e: e.matmul(pst[:, 4 + c, :], lhsT=kctx[hb:hb + 64, hc, 128 * c:128 * c + 128],
                                                      rhs=qb[i2][hb:hb + 64, hc, 64 * rr:64 * rr + 64], start=True, stop=True),
                             reads=['kctx', ('qb', i2)], writes=[psk])
                    S.op('dve', lambda e: e.tensor_tensor(out=sS[it][:], in0=pst[:, 0:4, :], in1=Tb[:, h, a0:a0 + 7:2, :], op=ALU.add),
                         reads=[psk, 'Tb'], writes=[('sS', it)])
                    S.op('act', lambda e: e.activation(out=P[it][:, 0:4, :], in_=sS[it][:], func=AF.Exp), reads=[('sS', it)], writes=[('P', it)])
                    S.op('act', lambda e: e.activation(out=P[it][:, 4:6, :], in_=pst[:, 4:6, :], func=AF.Exp), reads=[psk], writes=[('P', it)])
                    if pend is not None:
                        pend()
                    def mk_pv(it=it, i2=i2, krs=krs, lo=lo, h=h, half=half, pp=pp, r=r, rr=rr, b=b):
                        def pv():
                            pot = pO[pp * 2 + h // 4]; pok = ('pO', pp * 2 + h // 4)
                            for c in range(6):
                                if c < 4:
                                    rhs = vw[i2][:, krs + 2 * c - lo, h, :]
                                    rk = ('vw', i2)
                                else:
                                    rhs = vctx[:, c - 4, h, :]
                                    rk = 'vctx'
                                S.op('pe', lambda e: e.matmul(pot[half:half + 64, h % 4, :], lhsT=P[it][:, c, :], rhs=rhs,
                                                              start=(c == 0), stop=(c == 5)), reads=[('P', it), rk], writes=[pok])
                            if h % 4 == 3:
                                hh = h // 4
                                yk = ('ybt', pp)
                                S.op('dve', lambda e: e.reciprocal(out=rc[pp][half:half + 64, 4 * hh:4 * hh + 4], in_=pot[half:half + 64, :, 64]),
                                     reads=[pok], writes=[('rc', pp)])
                                S.op('dve', lambda e: e.tensor_tensor(out=ybt[pp][half:half + 64, 4 * hh:4 * hh + 4, :], in0=pot[half:half + 64, :, 0:64],
                                                                      in1=rc[pp][half:half + 64, 4 * hh:4 * hh + 4].unsqueeze(2).to_broadcast([64, 4, 64]),
                                                                      op=ALU.mult), reads=[pok, ('rc', pp)], writes=[yk])
                            if h == 7 and r % 2 == 1:
                                tk = ('ybT', b % 2)
                                for c4 in range(4):
                                    S.op('pe', lambda e: e.transpose(out=pT[:, c4, :], in_=ybt[pp][:, 2 * c4:2 * c4 + 2, :].rearrange("p a d -> p (a d)"),
                                                                     identity=identb[:]), reads=[('ybt', pp), 'identb'], writes=['pTn'])
                                S.op('act', lambda e: e.activation(out=ybT[b % 2][:, :, 64 * (rr - 1):64 * (rr - 1) + 128], in_=pT[:], func=AF.Copy),
                                     reads=['pTn'], writes=[tk])
                                if rr == 7:
                                    S.dma('act', ybT_d[:, 512 * b:512 * b + 512].rearrange("(c p) t -> p c t", p=128), ybT[b % 2][:],
                                          reads=[tk], writes=['ybT_d'])
                        return pv
                    pend = mk_pv()
        pend()
        S.barrier()
    if 'na' in dbg:
        dbg_out['ybT'] = dout("dbg_ybT", [512, 8192], BF16)
        for r0 in range(0, 512, 128):
            S.dma('sp', dbg_out['ybT'][r0:r0 + 128, :], ybT_d[r0:r0 + 128, :], reads=['ybT_d'])


def na_host(inp):
    rpb = inp['na_rpb'][0].astype(np.float32)
    i = np.arange(2)[:, None, None, None]
    kc = np.arange(64)[None, :, None, None]
    a = np.arange(14)[None, None, :, None]
    qc = np.arange(64)[None, None, None, :]
    dcol = np.clip(kc - qc + 15, 0, 30)
    drow = a + i
    T = rpb[:, drow, dcol]
    T = np.broadcast_to(T, (8, 2, 64, 14, 64)).transpose(1, 2, 0, 3, 4).reshape(128, 8 * 14 * 64)
    q = np.arange(64)
    qcs = np.clip(q - 8, 0, 48)
    k = np.arange(64)[:, None]
    inw = (k >= qcs[None, :]) & (k < qcs[None, :] + 16)
    m = np.where(inw, 0.0, NEG).astype(np.float32)
    m = np.concatenate([m, m], axis=0)
    return {'rpbT': np.ascontiguousarray(T.astype(np.float32)), 'maskT': np.ascontiguousarray(m)}

D = 1024


def merge_phase(nc, S, es, sb, ps, din, dscr, dout, dbg, dbg_out, identf, identb, epsc, bc_g1, bc_mul2, bc_add2,
                xcat, out, yaT_d, ybT_d, sgaT_d, sgbT_d, h2_d, aff_d, affT_d, nblocks=16):
    wpa_in = din("w_proj_a", [512, D]); wpb_in = din("w_proj_b", [512, D]); wout_in = din("w_out", [D, D])
    wr_in = din("w_routerT", [128, 8, 16])
    with ExitStack() as pa:
        wpa = sb("g_wpa", [128, 4, D], BF16, pa); wpb = sb("g_wpb", [128, 4, D], BF16, pa)
        wout = sb("g_wout", [128, 8, D], BF16, pa)
        wr = sb("g_wr", [128, 8, 16], BF16, pa)
        ya = [sb("g_mya%d" % i, [128, 4, 512], BF16, pa) for i in range(2)]
        yb = [sb("g_myb%d" % i, [128, 4, 512], BF16, pa) for i in range(2)]
        sga = [sb("g_msga%d" % i, [128, 8, 512], BF16, pa) for i in range(2)]
        sgb = [sb("g_msgb%d" % i, [128, 8, 512], BF16, pa) for i in range(2)]
        mT = [sb("g_mT%d" % i, [128, 8, 512], BF16, pa) for i in range(2)]
        t1 = [sb("g_mt1_%d" % i, [128, 512], F32, pa) for i in range(2)]
        t2 = [sb("g_mt2_%d" % i, [128, 512], F32, pa) for i in range(2)]
        xt = [sb("g_mxt%d" % i, [128, D], F32, pa) for i in range(2)]
        x1 = [sb("g_mx1_%d" % i, [128, D], F32, pa) for i in range(3)]
        h2 = [sb("g_mh2_%d" % i, [128, D], F32, pa) for i in range(3)]
        h2b = [sb("g_mh2b_%d" % i, [128, D], BF16, pa) for i in range(3)]
        h2T = [sb("g_mh2T_%d" % i, [128, 8, 128], BF16, pa) for i in range(3)]
        junk = sb("g_mjunk", [128, D], BF16, pa)
        st = [sb("g_mst%d" % i, [128, 8], F32, pa) for i in range(3)]
        lg = [sb("g_mlg%d" % i, [128, 16], F32, pa) for i in range(3)]
        af = [sb("g_maf%d" % i, [128, 16], F32, pa) for i in range(3)]
        A16 = sb("g_A16", [16, 8192], F32, pa)
        pab = [ps("g_mpab%d" % i, [128, 512], F32, pa) for i in range(4)]
        pyo = ps("g_mpyo", [128, D], F32, pa)
        phT = ps("g_mphT", [128, 8, 128], BF16, pa)
        pmisc = ps("g_mpmisc", [128, 256], F32, pa)
        plg = pmisc[:, 0:16]
        paT = pmisc[0:16, 128:256]

        for q in range(4):
            S.dma('pool', wpa[:, q, :], wpa_in[q * 128:(q + 1) * 128, :], writes=['wpa'])
            S.dma('pool', wpb[:, q, :], wpb_in[q * 128:(q + 1) * 128, :], writes=['wpb'])
        for dc in range(8):
            S.dma('pool', wout[:, dc, :], wout_in[dc * 128:(dc + 1) * 128, :], writes=['wout'])
        S.dma('pool', wr[:], wr_in, writes=['wr'])
        tile_ctr = 0
        pend = []
        for b in range(nblocks):
            i2 = b % 2
            cs = slice(512 * b, 512 * b + 512)
            S.dma('sp', ya[i2][:], yaT_d[:, cs].rearrange("(c p) t -> p c t", p=128), reads=['yaT_d'], writes=[('ya', i2)])
            S.dma('sp', yb[i2][:], ybT_d[:, cs].rearrange("(c p) t -> p c t", p=128), reads=['ybT_d'], writes=[('yb', i2)])
            S.dma('sp', sga[i2][:], sgaT_d[:, cs].rearrange("(c p) t -> p c t", p=128), reads=['sgaT_d'], writes=[('sga', i2)])
            S.dma('sp', sgb[i2][:], sgbT_d[:, cs].rearrange("(c p) t -> p c t", p=128), reads=['sgbT_d'], writes=[('sgb', i2)])
            for dc in range(8):
                j2 = dc % 2
                pA_, pB_ = pab[2 * j2], pab[2 * j2 + 1]
                for q in range(4):
                    S.op('pe', lambda e: e.matmul(pA_[:], lhsT=wpa[:, q, dc * 128:(dc + 1) * 128], rhs=ya[i2][:, q, :],
                                                  start=(q == 0), stop=(q == 3)), reads=['wpa', ('ya', i2)], writes=[('pA', j2)])
                for q in range(4):
                    S.op('pe', lambda e: e.matmul(pB_[:], lhsT=wpb[:, q, dc * 128:(dc + 1) * 128], rhs=yb[i2][:, q, :],
                                                  start=(q == 0), stop=(q == 3)), reads=['wpb', ('yb', i2)], writes=[('pB', j2)])
                S.op('dve', lambda e: e.tensor_tensor(out=t1[j2][:], in0=pA_[:], in1=sga[i2][:, dc, :], op=ALU.mult),
                     reads=[('pA', j2), ('sga', i2)], writes=[('t1', j2)])
                S.op('dve', lambda e: e.tensor_tensor(out=t2[j2][:], in0=pB_[:], in1=sgb[i2][:, dc, :], op=ALU.mult),
                     reads=[('pB', j2), ('sgb', i2)], writes=[('t2', j2)])
                S.op('pool', lambda e: e.tensor_tensor(out=mT[i2][:, dc, :], in0=t1[j2][:], in1=t2[j2][:], op=ALU.add),
                     reads=[('t1', j2), ('t2', j2)], writes=[('mT', i2)])
            for ti in range(4):
                k2 = tile_ctr % 3
                kx = tile_ctr % 2
                tile_ctr += 1
                tok0 = 512 * b + 128 * ti
                S.dma('sp', xt[kx][:], xcat[256 + tok0:256 + tok0 + 128, :], writes=[('xt', kx)])
                for hf in range(2):
                    for dc in range(8):
                        S.op('pe', lambda e: e.matmul(pyo[:, hf * 512:(hf + 1) * 512], lhsT=mT[i2][:, dc, ti * 128:(ti + 1) * 128],
                                                      rhs=wout[:, dc, hf * 512:(hf + 1) * 512], start=(dc == 0), stop=(dc == 7)),
                             reads=[('mT', i2), 'wout'], writes=['pyo'])
                S.op('dve', lambda e: e.tensor_tensor(out=x1[k2][:], in0=pyo[:], in1=bc_g1[:], op=ALU.mult),
                     reads=['pyo', 'bc_g1'], writes=[('x1', k2)])
                S.op('dve', lambda e: e.tensor_tensor(out=x1[k2][:], in0=x1[k2][:], in1=xt[kx][:], op=ALU.add),
                     reads=[('x1', k2), ('xt', kx)], writes=[('x1', k2)])
                S.dma('pool', out[tok0:tok0 + 128, :], x1[k2][:], reads=[('x1', k2)], writes=['out'])
                s_ = st[k2]; sk = ('st', k2)
                S.op('act', lambda e: e.activation(out=junk[:], in_=x1[k2][:], func=AF.Square, accum_out=s_[:, 0:1]),
                     reads=[('x1', k2)], writes=['mjunk', sk])
                S.op('act', lambda e: e.activation(out=s_[:, 6:7], in_=s_[:, 0:1], func=AF.Ln, scale=1.0 / D, bias=epsc[:, 0:1]),
                     reads=[sk, 'epsc'], writes=[sk])
                S.op('act', lambda e: e.activation(out=s_[:, 1:2], in_=s_[:, 6:7], func=AF.Exp, scale=-0.5), reads=[sk], writes=[sk])
                S.op('dve', lambda e: e.scalar_tensor_tensor(out=h2[k2][:], in0=x1[k2][:], scalar=s_[:, 1:2], in1=bc_mul2[:],
                                                             op0=ALU.mult, op1=ALU.mult), reads=[('x1', k2), sk, 'bc_mul2'], writes=[('h2', k2)])
                S.op('dve', lambda e: e.tensor_tensor(out=h2[k2][:], in0=h2[k2][:], in1=bc_add2[:], op=ALU.add),
                     reads=[('h2', k2), 'bc_add2'], writes=[('h2', k2)])
                S.op('act', lambda e: e.activation(out=h2b[k2][:], in_=h2[k2][:], func=AF.Copy), reads=[('h2', k2)], writes=[('h2b', k2)])
                S.dma('act', h2_d[tok0:tok0 + 128, :], h2b[k2][:], reads=[('h2b', k2)], writes=['h2_d'])
                def mk_tail(k2=k2, tok0=tok0, s_=s_, sk=sk):
                    def tail():
                        for dc in range(8):
                            S.op('pe', lambda e: e.transpose(out=phT[:, dc, :], in_=h2b[k2][:, dc * 128:(dc + 1) * 128], identity=identb[:]),
                                 reads=[('h2b', k2), 'identb'], writes=['phT'])
                        S.op('act', lambda e: e.activation(out=h2T[k2][:], in_=phT[:], func=AF.Copy), reads=['phT'], writes=[('h2T', k2)])
                        for dc in range(8):
                            S.op('pe', lambda e: e.matmul(plg, lhsT=h2T[k2][:, dc, :], rhs=wr[:, dc, :], start=(dc == 0), stop=(dc == 7)),
                                 reads=[('h2T', k2), 'wr'], writes=['plg'])
                        S.op('dve', lambda e: e.tensor_copy(out=lg[k2][:], in_=plg), reads=['plg'], writes=[('lg', k2)])
                        S.op('dve', lambda e: e.reduce_max(out=s_[:, 2:3], in_=lg[k2][:], axis=AX.X), reads=[('lg', k2)], writes=[sk])
                        S.op('dve', lambda e: e.tensor_scalar(out=s_[:, 3:4], in0=s_[:, 2:3], scalar1=-1.0, scalar2=None, op0=ALU.mult),
                             reads=[sk], writes=[sk])
                        S.op('act', lambda e: e.activation(out=af[k2][:], in_=lg[k2][:], func=AF.Exp, bias=s_[:, 3:4], accum_out=s_[:, 4:5]),
                             reads=[('lg', k2), sk], writes=[('af', k2), sk])
                        S.op('dve', lambda e: e.reciprocal(out=s_[:, 5:6], in_=s_[:, 4:5]), reads=[sk], writes=[sk])
                        S.op('dve', lambda e: e.tensor_scalar(out=af[k2][:], in0=af[k2][:], scalar1=s_[:, 5:6], scalar2=None, op0=ALU.mult),
                             reads=[('af', k2), sk], writes=[('af', k2)])
                        S.dma('pool', aff_d[tok0:tok0 + 128, :], af[k2][:], reads=[('af', k2)], writes=['aff_d'])
                        S.op('pe', lambda e: e.transpose(out=paT, in_=af[k2][:], identity=identf[:]), reads=[('af', k2), 'identf'], writes=['paT'])
                        S.op('act', lambda e: e.activation(out=A16[:, tok0:tok0 + 128], in_=paT, func=AF.Copy), reads=['paT'], writes=['A16'])
                    return tail
                if len(pend) == 2:
                    pend.pop(0)()
                pend.append(mk_tail())
        for t_ in pend:
            t_()
        S.dma('sp', affT_d, A16[:], reads=['A16'], writes=['affT_d'])
        S.barrier()
        if 'mg2' in dbg:
            for nm, t, shp, dt in (('bcg1', bc_g1, [128, D], F32), ('mT0', mT[(nblocks - 1) % 2], [128, 8, 512], BF16), ('t1', t1[1], [128, 512], F32),
                                   ('sga0', sga[(nblocks - 1) % 2], [128, 8, 512], BF16), ('ya0', ya[(nblocks - 1) % 2], [128, 4, 512], BF16),
                                   ('yb0', yb[(nblocks - 1) % 2], [128, 4, 512], BF16), ('wpa', wpa, [128, 4, D], BF16), ('x1t', x1[1], [128, D], F32),
                                   ('xt', xt[1], [128, D], F32)):
                dbg_out[nm] = dout("dbg_" + nm, shp, dt)
                S.dma('sp', dbg_out[nm], t[:], reads=[])
    if 'mg' in dbg:
        dbg_out['aff'] = dout("dbg_aff", [8192, 16])
        S.dma('sp', dbg_out['aff'], aff_d, reads=['aff_d'])
        dbg_out['affT'] = dout("dbg_affT", [16, 8192])
        S.dma('sp', dbg_out['affT'], affT_d, reads=['affT_d'])
        dbg_out['h2'] = dout("dbg_h2", [8192, D], BF16)
        for r0 in range(0, 8192, 1024):
            S.dma('sp', dbg_out['h2'][r0:r0 + 1024, :], h2_d[r0:r0 + 1024, :], reads=['h2_d'])

D = 1024
CAP = 1024
NT = 8192


def moe_phase(nc, S, es, sb, ps, din, dscr, dout, dbg, dbg_out, identf, identb, bc_g2, out, h2_d, aff_d, affT_d, nexp=16):
    wg_in = din("w_e_gate", [16, D, D]); wu_in = din("w_e_up", [16, D, D]); wd_in = din("w_e_down", [16, D, D])
    m16_in = din("m16", [128, 128])
    with ExitStack() as pm:
        cum_d = dscr("cum_d", [16, NT], F32)
        slotv = sb("e_slotv", [128, 8], F32, pm)
        thr_dbg = sb("e_thr", [128, 1], F32, pm)
        S.op('pool', lambda e: e.iota(slotv[:], pattern=[[128, 8]], base=0, channel_multiplier=1, allow_small_or_imprecise_dtypes=True),
             writes=['slotv'])
        with ExitStack() as pa:
            A128 = sb("e_A128", [128, 1024], F32, pa)
            A16 = sb("e_A16", [16, NT], F32, pa)
            C16 = sb("e_C16", [16, NT], F32, pa)
            M16 = sb("e_M16", [128, 128], F32, pa)
            junk = sb("e_junk", [128, 1024], BF16, pa)
            lo = sb("e_lo", [128, 1], F32, pa); mid = sb("e_mid", [128, 1], F32, pa)
            cnt = sb("e_cnt", [128, 1], F32, pa); g = sb("e_g", [128, 1], F32, pa)
            one16 = sb("e_one16", [16, 1], F32, pa)
            ptot = ps("e_ptot", [128, 1], F32, pa)
            for s in range(8):
                S.dma('sp', A128[16 * s:16 * s + 16, :], affT_d[:, 1024 * s:1024 * s + 1024], reads=['affT_d'], writes=['A128'])
            S.dma('sp', A16[:], affT_d, reads=['affT_d'], writes=['A16'])
            S.dma('sp', M16[:], m16_in, writes=['M16'])
            S.op('dve', lambda e: e.memset(lo[:], 0.0), writes=['lo'])
            S.op('dve', lambda e: e.memset(one16[:], 1.0), writes=['one16'])
            for k in range(30):
                hk = 2.0 ** -(k + 1)
                S.op('dve', lambda e: e.tensor_scalar(out=mid[:], in0=lo[:], scalar1=hk, scalar2=None, op0=ALU.add), reads=['lo'], writes=['mid'])
                S.op('dve', lambda e: e.tensor_scalar(out=junk[:], in0=A128[:], scalar1=mid[:, 0:1], scalar2=0.0, op0=ALU.is_gt, op1=ALU.add,
                                                      accum_out=cnt[:]), reads=['A128', 'mid'], writes=['junk', 'cnt'])
                S.op('pe', lambda e: e.matmul(ptot[:], lhsT=M16[:], rhs=cnt[:], start=True, stop=True), reads=['M16', 'cnt'], writes=['ptot'])
                S.op('dve', lambda e: e.tensor_scalar(out=g[:], in0=ptot[:], scalar1=float(CAP), scalar2=hk, op0=ALU.is_ge, op1=ALU.mult),
                     reads=['ptot'], writes=['g'])
                S.op('dve', lambda e: e.tensor_tensor(out=lo[:], in0=lo[:], in1=g[:], op=ALU.add), reads=['lo', 'g'], writes=['lo'])
            S.op('dve', lambda e: e.tensor_copy(out=thr_dbg[:], in_=lo[:]), reads=['lo'], writes=['thr'])
            S.op('dve', lambda e: e.tensor_scalar(out=A16[:], in0=A16[:], scalar1=lo[0:16, 0:1], scalar2=None, op0=ALU.is_gt),
                 reads=['A16', 'lo'], writes=['A16'])
            S.op('dve', lambda e: e.tensor_tensor_scan(out=C16[:], data0=one16[:, 0:1].to_broadcast([16, NT]), data1=A16[:], initial=0.0,
                                                       op0=ALU.mult, op1=ALU.add), reads=['A16', 'one16'], writes=['C16'])
            S.dma('sp', cum_d, C16[:], reads=['C16'], writes=['cum_d'])
            S.barrier()
        if 'moe_thr' in dbg:
            dbg_out['thr'] = dout("dbg_thr", [128, 1])
            S.dma('sp', dbg_out['thr'], thr_dbg[:], reads=['thr'])
        with ExitStack() as pe_:
            Wg = [sb("e_Wg%d" % i, [128, 8, D], BF16, pe_) for i in range(2)]
            Wu = [sb("e_Wu%d" % i, [128, 8, D], BF16, pe_) for i in range(2)]
            Wd = [sb("e_Wd%d" % i, [128, 8, D], BF16, pe_) for i in range(2)]
            xe = [sb("e_xe%d" % i, [128, D], BF16, pe_) for i in range(8)]
            ag = [[sb("e_ag%d_%d" % (p_, i), [128, 16], F32, pe_) for i in range(8)] for p_ in range(2)]
            cq = [sb("e_cq%d" % i, [128, 2048], F32, pe_) for i in range(2)]
            junkq = sb("e_junkq", [128, 2048], BF16, pe_)
            acc = [sb("e_acc%d" % i, [128, 4, 8], F32, pe_) for i in range(3)]
            idxs = sb("e_idxs", [128, 8], F32, pe_)
            idxi = [sb("e_idxi%d" % i, [128, 8], I32, pe_) for i in range(3)]
            xeT = sb("e_xeT", [128, 8, CAP], BF16, pe_)
            hidT = sb("e_hidT", [128, 8, CAP], BF16, pe_)
            sg = [sb("e_sg%d" % i, [128, 512], F32, pe_) for i in range(2)]
            ye = [sb("e_ye%d" % i, [128, D], F32, pe_) for i in range(2)]
            pGU = [ps("e_pGU%d" % i, [128, 512], F32, pe_) for i in range(3)]
            pY = ps("e_pY", [128, D], F32, pe_)
            pT = ps("e_pT", [128, 8, 128], BF16, pe_)
            pTi = pT[:].bitcast(F32) if False else None

            def load_w(e):
                i2 = e % 2
                for dc in range(8):
                    S.dma('pool', Wg[i2][:, dc, :], wg_in[e, dc * 128:(dc + 1) * 128, :], writes=[('Wg', i2)])
                    S.dma('pool', Wu[i2][:, dc, :], wu_in[e, dc * 128:(dc + 1) * 128, :], writes=[('Wu', i2)])
                for dc in range(8):
                    S.dma('pool', Wd[i2][:, dc, :], wd_in[e, dc * 128:(dc + 1) * 128, :], writes=[('Wd', i2)])

            def load_cq(n):
                e_, qd = n // 4, n % 4
                if e_ >= nexp:
                    return
                S.dma('sp', cq[n % 2][:], cum_d[e_:e_ + 1, qd * 2048:(qd + 1) * 2048].to_broadcast([128, 2048]),
                      reads=['cum_d'], writes=[('cq', n % 2)])

            def idx_piece(e, piece):
                j3 = e % 3
                n = e * 4 + piece // 2
                if piece % 2 == 0:
                    if n == 0:
                        load_cq(0)
                    load_cq(n + 1)
                for sc in range(4 * (piece % 2), 4 * (piece % 2) + 4):
                    S.op('dve', lambda e_: e_.tensor_scalar(out=junkq[:], in0=cq[n % 2][:], scalar1=slotv[:, sc:sc + 1], scalar2=0.0,
                                                            op0=ALU.is_le, op1=ALU.add, accum_out=acc[j3][:, piece // 2, sc:sc + 1]),
                         reads=[('cq', n % 2), 'slotv'], writes=['junkq', ('acc', j3)])
                if piece == 7:
                    S.op('dve', lambda e_: e_.tensor_tensor(out=idxs[:], in0=acc[j3][:, 0, :], in1=acc[j3][:, 1, :], op=ALU.add),
                         reads=[('acc', j3)], writes=['idxs'])
                    S.op('dve', lambda e_: e_.tensor_tensor(out=idxs[:], in0=idxs[:], in1=acc[j3][:, 2, :], op=ALU.add),
                         reads=[('acc', j3), 'idxs'], writes=['idxs'])
                    S.op('dve', lambda e_: e_.tensor_tensor(out=idxs[:], in0=idxs[:], in1=acc[j3][:, 3, :], op=ALU.add),
                         reads=[('acc', j3), 'idxs'], writes=['idxs'])
                    S.op('dve', lambda e_: e_.tensor_copy(out=idxi[j3][:], in_=idxs[:]), reads=['idxs'], writes=[('idxi', j3)])

            def gather(e):
                j2 = e % 3
                for sc in range(8):
                    S.custom_dma('pool', lambda e_: e_.indirect_dma_start(out=xe[sc][:, :], out_offset=None, in_=h2_d[:, :],
                                 in_offset=bass.IndirectOffsetOnAxis(ap=idxi[j2][:, sc:sc + 1], axis=0)),
                                 reads=[('idxi', j2), 'h2_d'], writes=[('xe', sc)])
                    S.custom_dma('pool', lambda e_: e_.indirect_dma_start(out=ag[e % 2][sc][:, :], out_offset=None, in_=aff_d[:, :],
                                 in_offset=bass.IndirectOffsetOnAxis(ap=idxi[j2][:, sc:sc + 1], axis=0)),
                                 reads=[('idxi', j2), 'aff_d'], writes=[('ag', e % 2, sc)])

            gu_ctr = [0]

            def xpose(e):
                for sc in range(8):
                    for dc in range(8):
                        S.op('pe', lambda e_: e_.transpose(out=pT[:, dc, :], in_=xe[sc][:, dc * 128:(dc + 1) * 128], identity=identb[:]),
                             reads=[('xe', sc), 'identb'], writes=['pT'])
                    S.op('act', lambda e_: e_.activation(out=xeT[:, :, sc * 128:(sc + 1) * 128], in_=pT[:], func=AF.Copy),
                         reads=['pT'], writes=['xeT'])
            def ffn(e, nxt):
                i2 = e % 2; j2 = e % 3
                for fc in range(8):
                    for hf in range(2):
                        a = gu_ctr[0] % 3; gu_ctr[0] += 1
                        b = gu_ctr[0] % 3; gu_ctr[0] += 1
                        for dc in range(8):
                            S.op('pe', lambda e_: e_.matmul(pGU[a][:], lhsT=Wg[i2][:, dc, fc * 128:(fc + 1) * 128], rhs=xeT[:, dc, hf * 512:(hf + 1) * 512],
                                                            start=(dc == 0), stop=(dc == 7)), reads=[('Wg', i2), 'xeT'], writes=[('pGU', a)])
                        for dc in range(8):
                            S.op('pe', lambda e_: e_.matmul(pGU[b][:], lhsT=Wu[i2][:, dc, fc * 128:(fc + 1) * 128], rhs=xeT[:, dc, hf * 512:(hf + 1) * 512],
                                                            start=(dc == 0), stop=(dc == 7)), reads=[('Wu', i2), 'xeT'], writes=[('pGU', b)])
                        s2 = (fc * 2 + hf) % 2
                        S.op('act', lambda e_: e_.activation(out=sg[s2][:], in_=pGU[a][:], func=AF.Silu), reads=[('pGU', a)], writes=[('sg', s2)])
                        S.op('dve', lambda e_: e_.tensor_tensor(out=hidT[:, fc, hf * 512:(hf + 1) * 512], in0=pGU[b][:], in1=sg[s2][:], op=ALU.mult),
                             reads=[('pGU', b), ('sg', s2)], writes=['hidT'])
                    if nxt is not None:
                        idx_piece(nxt, fc)
                for sc in range(8):
                    for hf in range(2):
                        for fc in range(8):
                            S.op('pe', lambda e_: e_.matmul(pY[:, hf * 512:(hf + 1) * 512], lhsT=hidT[:, fc, sc * 128:(sc + 1) * 128],
                                                            rhs=Wd[i2][:, fc, hf * 512:(hf + 1) * 512], start=(fc == 0), stop=(fc == 7)),
                                 reads=['hidT', ('Wd', i2)], writes=['pY'])
                    y2 = sc % 2
                    S.op('dve', lambda e_: e_.scalar_tensor_tensor(out=ye[y2][:], in0=pY[:], scalar=ag[e % 2][sc][:, e:e + 1], in1=bc_g2[:],
                                                                   op0=ALU.mult, op1=ALU.mult), reads=['pY', ('ag', e % 2, sc), 'bc_g2'], writes=[('ye', y2)])
                    S.custom_dma('pool', lambda e_: e_.indirect_dma_start(out=out[:, :], out_offset=bass.IndirectOffsetOnAxis(ap=idxi[j2][:, sc:sc + 1], axis=0),
                                 in_=ye[y2][:, :], in_offset=None, compute_op=ALU.add), reads=[('ye', y2), ('idxi', j2)], writes=['out'])

            load_w(0)
            for p in range(8):
                idx_piece(0, p)
            gather(0)
            if nexp > 1:
                load_w(1)
                for p in range(8):
                    idx_piece(1, p)
            for e in range(nexp):
                xpose(e)
                if e + 1 < nexp:
                    gather(e + 1)
                    if e >= 1:
                        load_w(e + 1)
                ffn(e, e + 2 if e + 2 < nexp else None)
            if 'moe_idx' in dbg:
                dbg_out['idxi'] = dout("dbg_idxi", [128, 8], I32)
                S.dma('sp', dbg_out['idxi'], idxi[(nexp - 1) % 3][:], reads=[('idxi', (nexp - 1) % 3)])
            S.barrier()


def moe_host(inp):
    f = np.float32
    m16 = np.zeros((128, 128), f)
    for s in range(8):
        for s2 in range(8):
            m16[16 * s:16 * s + 16, 16 * s2:16 * s2 + 16] = np.eye(16, dtype=f)
    return {'w_e_gate': np.ascontiguousarray(inp['w_e_gate'][0]), 'w_e_up': np.ascontiguousarray(inp['w_e_up'][0]),
            'w_e_down': np.ascontiguousarray(inp['w_e_down'][0]), 'm16': m16}


D = 1024
NLAT = 8192
NCTX = 256
NEXT = NLAT + NCTX
NE2 = NEXT + NCTX
EPS = 1e-6
INCOLS = 4096


def build(stop_after=99, dbg=(), nblk_lim=17, skip_s5=False, mg_blocks=16, nexp=16):
    nc = bass.Bass("TRN2", target_bir_lowering=False)
    es = ExitStack()
    S = Sched(nc, es)
    dbg_out = {}

    def din(name, shape, dt=F32):
        return nc.dram_tensor(name, list(shape), dt, kind="ExternalInput").ap()

    def dscr(name, shape, dt):
        return nc.dram_tensor(name, list(shape), dt, kind="Internal").ap()

    def dout(name, shape, dt=F32):
        return nc.dram_tensor(name, list(shape), dt, kind="ExternalOutput").ap()

    def sb(name, shape, dt, stack=None):
        return (stack or es).enter_context(nc.sbuf_tensor(name, list(shape), dt))

    def ps(name, shape, dt, stack=None):
        return (stack or es).enter_context(nc.psum_tensor(name, list(shape), dt))

    xcat = din("xcat", [NEXT, D])
    cT = din("cT", [128, 8, 2])
    w_ada = din("w_ada", [D, 6 * D])
    b_ada = din("b_ada", [1, 6 * D])
    nmixT = din("nmixT", [128, 8])
    nffn = din("nffn", [1, D])
    w_in = din("w_in", [D, INCOLS])
    ident_in = din("ident", [128, 128])
    qkg = din("qkg", [128, 2])

    out = dout("out", [NLAT, D])

    uT_d = dscr("uT_d", [512, NE2], BF16)
    qT_d = dscr("qT_d", [512, NLAT], BF16)
    kT_d = dscr("kT_d", [512, NEXT], BF16)
    v_d = dscr("v_d", [NEXT, 8, 65], BF16)
    sgaT_d = dscr("sgaT_d", [D, NLAT], BF16)
    sgbT_d = dscr("sgbT_d", [D, NLAT], BF16)

    identf = sb("identf", [128, 128], F32)
    identb = sb("identb", [128, 128], BF16)
    mul1 = sb("mul1", [128, 8, 2], F32)
    add1 = sb("add1", [128, 8, 2], F32)
    bc_g1 = sb("bc_g1", [128, D], F32)
    bc_mul2 = sb("bc_mul2", [128, D], F32)
    bc_add2 = sb("bc_add2", [128, D], F32)
    bc_g2 = sb("bc_g2", [128, D], F32)

    epsc = sb("epsc", [128, 1], F32)
    S.op('dve', lambda e: e.memset(epsc[:], EPS), writes=['epsc'])
    S.dma('sp', identf[:], ident_in, writes=['identf'])
    S.dma('pool', identb[:], ident_in, writes=['identb'])

    with ExitStack() as p0:
        wada = sb("wada", [128, 8, 6 * D], BF16, p0)
        cs_f = sb("cs_f", [128, 8, 2], F32, p0)
        cs = sb("cs", [128, 8, 2], BF16, p0)
        brow = sb("brow", [2, 6 * D], F32, p0)
        modrow = sb("modrow", [2, 6 * D], F32, p0)
        sel = sb("sel", [2, 128], F32, p0)
        nmix = sb("nmix", [128, 8], F32, p0)
        colsb = sb("colsb", [128, 16, 2], F32, p0)
        pm = [ps("pm%d" % i, [2, 512], F32, p0) for i in range(2)]
        pcol = ps("pcol", [128, 16, 2], F32, p0)
        pbc = [ps("pbc%d" % i, [128, 512], F32, p0) for i in range(2)]

        S.dma('sp', cs_f[:], cT, writes=['cs_f'])
        S.dma('sp', brow[0:1, :], b_ada, writes=['brow0'])
        S.dma('sp', brow[1:2, :], b_ada, writes=['brow1'])
        S.dma('sp', nmix[:], nmixT, writes=['nmix'])
        S.dma('sp', bc_mul2[:], nffn.to_broadcast([128, D]), writes=['bc_mul2'])
        for dc in range(8):
            S.dma('pool', wada[:, dc, :].rearrange("p (a b) -> p a b", b=2048),
                  w_ada[dc * 128:(dc + 1) * 128, :].rearrange("p (a b) -> p a b", b=2048),
                  writes=[('wada', dc)])
        S.op('act', lambda e: e.activation(out=cs[:], in_=cs_f[:], func=AF.Silu), reads=['cs_f'], writes=['cs'])
        S.op('dve', lambda e: e.memset(sel[:], 0.0), writes=['sel'])
        S.op('dve', lambda e: e.memset(sel[0:1, :], 1.0), writes=['sel'])
        for cc in range(12):
            pt = pm[cc % 2]
            for dc in range(8):
                S.op('pe', lambda e: e.matmul(pt[:], lhsT=cs[:, dc, :], rhs=wada[:, dc, cc * 512:(cc + 1) * 512],
                                              start=(dc == 0), stop=(dc == 7)),
                     reads=['cs', ('wada', dc)], writes=[('pm', cc % 2)])
            S.op('dve', lambda e: e.tensor_tensor(out=modrow[:, cc * 512:(cc + 1) * 512], in0=pt[:],
                                                  in1=brow[:, cc * 512:(cc + 1) * 512], op=ALU.add),
                 reads=[('pm', cc % 2), 'brow0', 'brow1'], writes=[('modrow', cc)])
        for i in range(16):
            S.op('pe', lambda e: e.transpose(out=pcol[:, i, :], in_=modrow[0:2, i * 128:(i + 1) * 128],
                                             identity=identf[0:2, 0:2]),
                 reads=[('modrow', i // 4), 'identf'], writes=['pcol'])
        S.op('dve', lambda e: e.tensor_copy(out=colsb[:], in_=pcol[:]), reads=['pcol'], writes=['colsb'])
        S.op('dve', lambda e: e.tensor_copy(out=add1[:], in_=colsb[:, 0:8, :]), reads=['colsb'], writes=['add1'])
        S.op('dve', lambda e: e.tensor_scalar(out=colsb[:, 8:16, :], in0=colsb[:, 8:16, :], scalar1=1.0, scalar2=None,
                                              op0=ALU.add), reads=['colsb'], writes=['colsb'])
        S.op('dve', lambda e: e.tensor_tensor(out=mul1[:], in0=colsb[:, 8:16, :],
                                              in1=nmix[:].unsqueeze(2).to_broadcast([128, 8, 2]), op=ALU.mult),
             reads=['colsb', 'nmix'], writes=['mul1'])

        def bcast(dst, col0, mode, dkey):
            for h in range(2):
                pt = pbc[h]
                S.op('pe', lambda e: e.matmul(pt[:], lhsT=sel[:], rhs=modrow[:, col0 + h * 512: col0 + (h + 1) * 512],
                                              start=True, stop=True),
                     reads=['sel'] + [('modrow', c) for c in range(12)], writes=[('pbc', h)])
                dsl = dst[:, h * 512:(h + 1) * 512]
                if mode == 'copy':
                    S.op('dve', lambda e: e.tensor_copy(out=dsl, in_=pt[:]), reads=[('pbc', h)], writes=[dkey])
                else:
                    S.op('dve', lambda e: e.scalar_tensor_tensor(out=dsl, in0=pt[:], scalar=1.0, in1=dsl,
                                                                 op0=ALU.add, op1=ALU.mult),
                         reads=[('pbc', h), dkey], writes=[dkey])
        bcast(bc_g1, 2048, 'copy', 'bc_g1')
        bcast(bc_add2, 3072, 'copy', 'bc_add2')
        bcast(bc_mul2, 4096, 'mul1p', 'bc_mul2')
        bcast(bc_g2, 5120, 'copy', 'bc_g2')
        if 'mod' in dbg:
            dbg_out['mod'] = dout("dbg_mod", [2, 6 * D])
            S.dma('sp', dbg_out['mod'], modrow[:], reads=[('modrow', c) for c in range(12)])
            dbg_out['mul1'] = dout("dbg_mul1", [128, 8, 2])
            S.dma('sp', dbg_out['mul1'], mul1[:], reads=['mul1'])
            dbg_out['bcm2'] = dout("dbg_bcm2", [128, D])
            S.dma('sp', dbg_out['bcm2'], bc_mul2[:], reads=['bc_mul2'])
        S.barrier()

    if stop_after <= 0:
        return finish(nc, S, es, out, dbg_out)

    with ExitStack() as p1:
        win = sb("win", [128, 8, INCOLS], BF16, p1)
        xt = [sb("xt%d" % i, [128, D], F32, p1) for i in range(2)]
        junk = sb("junk", [128, D], BF16, p1)
        xn = [sb("xn%d" % i, [128, D], BF16, p1) for i in range(4)]
        ss = [sb("ss%d" % i, [128, 1], F32, p1) for i in range(4)]
        rstd = [sb("rstd%d" % i, [128, 1], F32, p1) for i in range(4)]
        hT = [sb("hT%d" % i, [128, 8, 512], BF16, p1) for i in range(2)]
        stg = [sb("stg%d" % i, [128, 512], BF16, p1) for i in range(4)]
        sq = [sb("sq%d" % i, [128, 512], BF16, p1) for i in range(2)]
        qk32 = [sb("qk32_%d" % i, [128, 512], F32, p1) for i in range(2)]
        rs = [sb("rs%d" % i, [128, 512], F32, p1) for i in range(2)]
        vst = [sb("vst%d" % i, [128, 8, 65], BF16, p1) for i in range(2)]
        bones = sb("bones", [128, 128], BF16, p1)
        gq = sb("gq", [128, 2], F32, p1)
        pT = [ps("pT%d" % i, [128, 8, 128], BF16, p1) for i in range(2)]
        pz = [ps("pz%d" % i, [128, 512], F32, p1) for i in range(4)]
        pms = [ps("pms%d" % i, [128, 512], F32, p1) for i in range(2)]

        for dc in range(8):
            S.dma('pool', win[:, dc, :].rearrange("p (a b) -> p a b", b=2048),
                  w_in[dc * 128:(dc + 1) * 128, :].rearrange("p (a b) -> p a b", b=2048),
                  writes=[('win', dc)])
        S.dma('sp', gq[:], qkg, writes=['gq'])
        S.op('dve', lambda e: e.tensor_scalar(out=gq[:, 0:1], in0=gq[:, 0:1], scalar1=0.125, scalar2=None, op0=ALU.mult), reads=['gq'], writes=['gq'])
        for i in range(2):
            S.op('dve', lambda e: e.memset(vst[i][:], 1.0), writes=[('vst', i)])
        S.op('dve', lambda e: e.memset(bones[:], 0.0), writes=['bones'])
        S.op('dve', lambda e: e.memset(bones[0:64, 0:64], 1.0 / 64), writes=['bones'])
        S.op('dve', lambda e: e.memset(bones[64:128, 64:128], 1.0 / 64), writes=['bones'])

        nblk = (NEXT + 511) // 512
        ctr = {'tile': 0, 'tpe': 0, 'stg': 0, 'z': 0, 'v': 0}

        def blk_info(b):
            if b == 0:
                return 0, 256
            return 256 + (b - 1) * 512, 512

        def head_pre(b):
            t0, nt = blk_info(b)
            for ti in range(nt // 128):
                i2 = ctr['tile'] % 2
                i4 = ctr['tile'] % 4
                ctr['tile'] += 1
                r0 = t0 + ti * 128
                S.dma('sp', xt[i2][:], xcat[r0:r0 + 128, :], writes=[('xt', i2)])
                S.op('act', lambda e: e.activation(out=junk[:], in_=xt[i2][:], func=AF.Square, accum_out=ss[i4][:]),
                     reads=[('xt', i2)], writes=['junk', ('ss', i4)])
                S.op('act', lambda e: e.activation(out=rstd[i4][:], in_=ss[i4][:], func=AF.Ln, scale=1.0 / D, bias=epsc[:, 0:1]),
                     reads=[('ss', i4), 'epsc'], writes=[('rstd', i4)])
                S.op('act', lambda e: e.activation(out=rstd[i4][:], in_=rstd[i4][:], func=AF.Exp, scale=-0.5),
                     reads=[('rstd', i4)], writes=[('rstd', i4)])
                S.op('act', lambda e: e.activation(out=xn[i4][:], in_=xt[i2][:], func=AF.Copy, scale=rstd[i4][:, 0:1]),
                     reads=[('xt', i2), ('rstd', i4)], writes=[('xn', i4)])

        def head_pe(b):
            t0, nt = blk_info(b)
            j = 1 if b == 0 else 0
            hb = hT[b % 2]
            hkey = ('hT', b % 2)
            for ti in range(nt // 128):
                i2 = ctr['tpe'] % 2
                i4 = ctr['tpe'] % 4
                ctr['tpe'] += 1
                for dc in range(8):
                    S.op('pe', lambda e: e.transpose(out=pT[i2][:, dc, :], in_=xn[i4][:, dc * 128:(dc + 1) * 128],
                                                     identity=identb[:]),
                         reads=[('xn', i4), 'identb'], writes=[('pT', i2)])
                for dc in range(8):
                    S.op('dve', lambda e: e.tensor_scalar(out=hb[:, dc, ti * 128:(ti + 1) * 128], in0=pT[i2][:, dc, :],
                                                          scalar1=mul1[:, dc, j:j + 1], scalar2=add1[:, dc, j:j + 1],
                                                          op0=ALU.mult, op1=ALU.add),
                         reads=[('pT', i2), 'mul1', 'add1'], writes=[hkey])

        pend_qk = []

        def chunk(b, cc):
            t0, nt = blk_info(b)
            is_ctx = (b == 0)
            hb = hT[b % 2]
            hkey = ('hT', b % 2)
            zi = ctr['z'] % 4
            ctr['z'] += 1
            pzt = pz[zi]
            for dc in range(8):
                S.op('pe', lambda e: e.matmul(pzt[:, 0:nt], lhsT=win[:, dc, cc * 128:(cc + 1) * 128], rhs=hb[:, dc, 0:nt],
                                              start=(dc == 0), stop=(dc == 7)),
                     reads=[('win', dc), hkey], writes=[('pz', zi)], sig=(dc == 7))
            si = ctr['stg'] % 4
            ctr['stg'] += 1
            st = stg[si]
            skey = ('stg', si)
            if not (4 <= cc < 12):
                while pend_qk:
                    pend_qk.pop(0)()
            if cc < 4:
                S.op('act', lambda e: e.activation(out=st[:, 0:nt], in_=pzt[:, 0:nt], func=AF.Copy),
                     reads=[('pz', zi)], writes=[skey])
                S.dma('sp', uT_d[cc * 128:(cc + 1) * 128, t0:t0 + nt], st[:, 0:nt], reads=[skey], writes=['uT_d'])
                if is_ctx:
                    S.dma('sp', uT_d[cc * 128:(cc + 1) * 128, NEXT:NEXT + nt], st[:, 0:nt], reads=[skey], writes=['uT_d'])
            elif cc < 12:
                isq = cc < 8
                qi = ctr['z'] % 2
                S.op('act', lambda e: e.activation(out=sq[qi][:, 0:nt], in_=pzt[:, 0:nt], func=AF.Square),
                     reads=[('pz', zi)], writes=[('sq', qi)])
                while pend_qk:
                    pend_qk.pop(0)()

                def tail(qi=qi, zi=zi, pzt=pzt, st=st, skey=skey, isq=isq, cc=cc, t0=t0, nt=nt):
                    S.op('pe', lambda e: e.matmul(pms[qi][:, 0:nt], lhsT=bones[:], rhs=sq[qi][:, 0:nt], start=True, stop=True),
                         reads=['bones', ('sq', qi)], writes=[('pms', qi)])
                    S.op('act', lambda e: e.activation(out=rs[qi][:, 0:nt], in_=pms[qi][:, 0:nt], func=AF.Ln, bias=epsc[:, 0:1]),
                         reads=[('pms', qi), 'epsc'], writes=[('rs', qi)])
                    S.op('act', lambda e: e.activation(out=rs[qi][:, 0:nt], in_=rs[qi][:, 0:nt], func=AF.Exp, scale=-0.5),
                         reads=[('rs', qi)], writes=[('rs', qi)])
                    gcol = gq[:, 0:1] if isq else gq[:, 1:2]
                    S.op('dve', lambda e: e.scalar_tensor_tensor(out=st[:, 0:nt], in0=pzt[:, 0:nt], scalar=gcol,
                                                                 in1=rs[qi][:, 0:nt], op0=ALU.mult, op1=ALU.mult),
                         reads=[('pz', zi), ('rs', qi), 'gq'], writes=[skey])
                    if isq:
                        S.dma('sp', qT_d[(cc - 4) * 128:(cc - 3) * 128, t0 - NCTX:t0 - NCTX + nt], st[:, 0:nt],
                              reads=[skey], writes=['qT_d'])
                    else:
                        S.dma('sp', kT_d[(cc - 8) * 128:(cc - 7) * 128, t0:t0 + nt], st[:, 0:nt], reads=[skey], writes=['kT_d'])
                pend_qk.append(tail)
            else:
                S.op('act', lambda e: e.activation(out=st[:, 0:nt], in_=pzt[:, 0:nt], func=AF.Sigmoid),
                     reads=[('pz', zi)], writes=[skey])
                if cc < 24:
                    dst = sgaT_d[(cc - 16) * 128:(cc - 15) * 128, t0 - NCTX:t0 - NCTX + nt]
                    dk = 'sgaT_d'
                else:
                    dst = sgbT_d[(cc - 24) * 128:(cc - 23) * 128, t0 - NCTX:t0 - NCTX + nt]
                    dk = 'sgbT_d'
                S.dma('sp', dst, st[:, 0:nt], reads=[skey], writes=[dk])

        def vpart(b):
            t0, nt = blk_info(b)
            hb = hT[b % 2]
            hkey = ('hT', b % 2)
            for ti in range(nt // 128):
                zi = ctr['z'] % 4
                ctr['z'] += 1
                pzt = pz[zi]
                for dc in range(8):
                    S.op('pe', lambda e: e.matmul(pzt[:], lhsT=hb[:, dc, ti * 128:(ti + 1) * 128], rhs=win[:, dc, 1536:2048],
                                                  start=(dc == 0), stop=(dc == 7)),
                         reads=[('win', dc), hkey], writes=[('pz', zi)], sig=(dc == 7))
                while pend_qk:
                    pend_qk.pop(0)()
                vi = ctr['v'] % 2
                ctr['v'] += 1
                S.op('act', lambda e: e.activation(out=vst[vi][:, :, 0:64], in_=pzt[:].rearrange("p (h d) -> p h d", d=64), func=AF.Copy),
                     reads=[('pz', zi)], writes=[('vst', vi)])
                S.dma('sp', v_d[t0 + ti * 128:t0 + (ti + 1) * 128], vst[vi][:], reads=[('vst', vi)], writes=['v_d'])

        head_pre(0)
        head_pe(0)
        for b in range(nblk_lim):
            if b == 0:
                ccs = list(range(0, 4)) + list(range(8, 12))
            else:
                ccs = list(range(0, 12)) + list(range(16, 32))
            n1, n2 = len(ccs) // 4, (3 * len(ccs)) // 4
            for cc in ccs[:n1]:
                chunk(b, cc)
            if b + 1 < nblk_lim:
                head_pre(b + 1)
            for cc in ccs[n1:n2]:
                chunk(b, cc)
            if b + 1 < nblk_lim:
                head_pe(b + 1)
            for cc in ccs[n2:]:
                chunk(b, cc)
            vpart(b)
        S.barrier()
    if 'p1' in dbg:
        for nm, t in (('uT', uT_d), ('qT', qT_d), ('kT', kT_d), ('sgaT', sgaT_d), ('sgbT', sgbT_d)):
            dbg_out[nm] = dout("dbg_" + nm, list(t.shape), BF16)
            for r0 in range(0, t.shape[0], 128):
                S.dma('sp', dbg_out[nm][r0:r0 + 128, :], t[r0:r0 + 128, :], reads=[])
    if stop_after <= 1:
        return finish(nc, S, es, out, dbg_out)
    yaT_d = dscr("yaT_d", [512, NLAT], BF16)
    r = 'ok' if skip_s5 else s5_phase(nc, S, es, sb, ps, din, dscr, dout, dbg, dbg_out, identf, identb, uT_d, yaT_d)
    if r == 'stop' or stop_after <= 2:
        return finish(nc, S, es, out, dbg_out)
    ybT_d = dscr("ybT_d", [512, NLAT], BF16)
    na_phase(nc, S, es, sb, ps, din, dscr, dout, dbg, dbg_out, identb, qT_d, kT_d, v_d, ybT_d)
    if stop_after <= 3:
        return finish(nc, S, es, out, dbg_out)
    h2_d = dscr("h2_d", [NLAT, D], BF16)
    aff_d = dscr("aff_d", [NLAT, 16], F32)
    affT_d = dscr("affT_d", [16, NLAT], F32)
    merge_phase(nc, S, es, sb, ps, din, dscr, dout, dbg, dbg_out, identf, identb, epsc, bc_g1, bc_mul2, bc_add2,
                xcat, out, yaT_d, ybT_d, sgaT_d, sgbT_d, h2_d, aff_d, affT_d, nblocks=mg_blocks)
    if stop_after <= 4:
        return finish(nc, S, es, out, dbg_out)
    moe_phase(nc, S, es, sb, ps, din, dscr, dout, dbg, dbg_out, identf, identb, bc_g2, out, h2_d, aff_d, affT_d, nexp=nexp)
    return finish(nc, S, es, out, dbg_out)


def finish(nc, S, es, out, dbg_out):
    S.barrier(['sp'])
    print("instructions", S.nins, "waits", S.nwaits)
    es.close()
    return nc, dbg_out


def host_inputs(inp, b):
    f = np.float32
    xcat = np.concatenate([inp['ctx'][b], inp['x'][b]], axis=0).astype(f)
    cT = np.stack([inp['c'][b].reshape(8, 128).T, inp['c_ctx'].reshape(8, 128).T], axis=-1).astype(f)
    qkg = np.stack([np.tile(inp['q_norm'][0], 2), np.tile(inp['k_norm'][0], 2)], axis=-1).astype(f)
    m = {
        'xcat': np.ascontiguousarray(xcat),
        'cT': np.ascontiguousarray(cT),
        'w_ada': np.ascontiguousarray(inp['w_ada'][0]),
        'b_ada': np.ascontiguousarray(inp['b_ada'][0][None, :]),
        'nmixT': np.ascontiguousarray(inp['norm_mix'][0].reshape(8, 128).T),
        'nffn': np.ascontiguousarray(inp['norm_ffn'][0][None, :]),
        'w_in': np.ascontiguousarray(inp['w_in'][0]),
        'ident': np.eye(128, dtype=f),
        'qkg': np.ascontiguousarray(qkg),
    }
    f32c = lambda a: np.ascontiguousarray(a.astype(f))
    lr = inp['ssm_lam_re'][0].reshape(2, 16, 2, 64)
    m['lamre_l'] = f32c(lr.transpose(2, 3, 0, 1).reshape(128, 32))
    li = inp['ssm_lam_im'][0].reshape(2, 16, 2, 64)
    m['lamim_l'] = f32c(li.transpose(2, 3, 0, 1).reshape(128, 32))
    ld = inp['ssm_log_dt'][0].reshape(2, 16, 2)
    m['logdt_l'] = f32c(np.broadcast_to(ld.transpose(2, 0, 1)[:, None, :, :], (2, 64, 2, 16)).reshape(128, 32))
    for nm, key in (('Bre_c', 'ssm_b_re'), ('Bim_c', 'ssm_b_im')):
        bb = inp[key][0].reshape(2, 16, 2, 64, 16)
        m[nm] = f32c(bb.transpose(2, 3, 0, 1, 4).reshape(128, 32, 16))
    for nm, key in (('Cre_c', 'ssm_c_re'), ('Cim_c', 'ssm_c_im')):
        cc = inp[key][0].reshape(2, 16, 2, 16, 64)
        m[nm] = f32c(cc.transpose(2, 4, 0, 1, 3).reshape(128, 32, 16))
    m['dskipT'] = f32c(inp['ssm_d'][0].reshape(4, 128).T)
    m['w_glu'] = f32c(inp['w_glu'][0])
    m['bgluT'] = f32c(inp['b_glu'][0].reshape(4, 128).T)
    m.update(na_host(inp))
    m['w_proj_a'] = f32c(inp['w_proj_a'][0]); m['w_proj_b'] = f32c(inp['w_proj_b'][0]); m['w_out'] = f32c(inp['w_out'][0])
    m.update(moe_host(inp))
    m['w_routerT'] = f32c(inp['w_router'][0].reshape(8, 128, 16).transpose(1, 0, 2))
    return m


_NC_CACHE = {}


def kernel(**inputs):
    inp = {k_: np.asarray(v_) for k_, v_ in inputs.items()}
    if 'nc' not in _NC_CACHE:
        nc, _ = build()
        _NC_CACHE['nc'] = nc
    nc = _NC_CACHE['nc']
    nb = inp['x'].shape[0]
    in_maps = [host_inputs(inp, b) for b in range(nb)]
    res = run_bass_kernel_spmd(nc, in_maps, core_ids=list(range(nb)))
    outs = [np.asarray(res.results[b]["out"], dtype=np.float32) for b in range(nb)]
    return np.stack(outs, axis=0)
```

```python
import numpy as np
from contextlib import ExitStack
import concourse.bass as bass
import concourse.mybir as mybir
from concourse.bass_utils import run_bass_kernel_spmd

F32 = mybir.dt.float32
BF16 = mybir.dt.bfloat16
I32 = mybir.dt.int32
U32 = mybir.dt.uint32
ALU = mybir.AluOpType
AF = mybir.ActivationFunctionType
AX = mybir.AxisListType
STRICT = False


class Sched:
    def __init__(self, nc, es, n_dma_sems=24):
        self.nc = nc
        self.e = {'pe': nc.tensor, 'act': nc.scalar, 'dve': nc.vector, 'pool': nc.gpsimd, 'sp': nc.sync}
        self.sems = {}
        self.cnt = {}
        for k in self.e:
            self.sems[k] = es.enter_context(nc.semaphore("sem_" + k))
            self.cnt[k] = 0
        self.nd = n_dma_sems
        self.qsems = {}
        for q in ('sp', 'pool', 'act'):
            n = n_dma_sems if q != 'act' else 16
            self.qsems[q] = []
            for i in range(n):
                sk = 'd_%s%d' % (q, i)
                self.sems[sk] = es.enter_context(nc.semaphore("dsem_%s%d" % (q, i)))
                self.cnt[sk] = 0
                self.qsems[q].append(sk)
        self.qrr = {'sp': 0, 'pool': 0, 'act': 0}
        self.drr = 0
        self.waited = {}
        self.lastw = {}
        self.readers = {}
        self.nwaits = 0
        self.nins = 0

    def _wait(self, eng, tok):
        sk, val, src = tok
        if self.waited.get((eng, sk), 0) >= val:
            return
        self.e[eng].wait_ge(self.sems[sk], val)
        self.waited[(eng, sk)] = val
        self.nwaits += 1

    def _deps(self, eng, reads, writes):
        for k in reads:
            t = self.lastw.get(k)
            if t is not None:
                self._wait(eng, t)
        for k in writes:
            t = self.lastw.get(k)
            if t is not None and (t[2] != eng or (STRICT and eng != 'pe')):
                self._wait(eng, t)
            for sk, (val, src) in self.readers.get(k, {}).items():
                if src != eng or (STRICT and eng != 'pe'):
                    self._wait(eng, (sk, val, src))

    def _record(self, tok, reads, writes):
        sk, val, src = tok
        for k in reads:
            self.readers.setdefault(k, {})[sk] = (val, src)
        for k in writes:
            self.lastw[k] = tok
            self.readers[k] = {}

    def op(self, eng, fn, reads=(), writes=(), sig=True):
        self._deps(eng, reads, writes)
        ins = fn(self.e[eng])
        if sig:
            self.cnt[eng] += 1
            ins.then_inc(self.sems[eng], 1)
            tok = (eng, self.cnt[eng], eng)
        else:
            tok = (eng, self.cnt[eng] + 1, eng)
        self._record(tok, reads, writes)
        self.nins += 1
        return tok

    def _dma_common(self, q, fn, reads, writes):
        self._deps(q, reads, writes)
        i = self.qrr[q]
        self.qrr[q] = (i + 1) % len(self.qsems[q])
        sk = self.qsems[q][i]
        if self.cnt[sk] > 0:
            self._wait(q, (sk, self.cnt[sk], None))
        ins = fn(self.e[q])
        self.cnt[sk] += 16
        ins.then_inc(self.sems[sk], 16)
        tok = (sk, self.cnt[sk], None)
        self._record(tok, reads, writes)
        self.nins += 1
        return tok

    def dma(self, q, out, in_, reads=(), writes=(), **kw):
        return self._dma_common(q, lambda e: e.dma_start(out=out, in_=in_, **kw), reads, writes)

    def custom_dma(self, q, fn, reads=(), writes=()):
        return self._dma_common(q, fn, reads, writes)

    def barrier(self, engines=None):
        for e in (engines or self.e):
            for sk, c in self.cnt.items():
                if c > 0:
                    self._wait(e, (sk, c, None))
        self.lastw = {}
        self.readers = {}

PI = float(np.pi)


def kn(t):
    n = t.name
    return n[2:] if n.startswith('s_') else n


def s5_phase(nc, S, es, sb, ps, din, dscr, dout, dbg, dbg_out, identf, identb, uT_d, yaT_d, nbanks=16):
    NEXT, NE2 = 8448, 8704
    lre_in = din("lamre_l", [128, 32]); lim_in = din("lamim_l", [128, 32]); ldt_in = din("logdt_l", [128, 32])
    Bre_in = din("Bre_c", [128, 32, 16]); Bim_in = din("Bim_c", [128, 32, 16])
    Cre_in = din("Cre_c", [128, 32, 16]); Cim_in = din("Cim_c", [128, 32, 16])
    dsk_in = din("dskipT", [128, 4]); wglu_in = din("w_glu", [512, 512]); bglu_in = din("bgluT", [128, 4])
    E_d = dscr("E_d", [128, 2, 4, 16, 2, 2, 128], BF16)
    G_d = dscr("G_d", [128, 64, 640], BF16)
    H_d = dscr("H_d", [2, 128, 16, 2, 544], BF16)

    with ExitStack() as s5:
        Kt_sb = sb("s_Kt_sb", [128, 128, 128], BF16, s5)
        aL = sb("s_aL", [128, 32], F32, s5); bL = sb("s_bL", [128, 32], F32, s5)
        diagD = sb("s_diagD", [128, 4, 128], BF16, s5)
        with ExitStack() as pa:
            def t32(name):
                return sb(name, [128, 32], F32, pa)
            lre, lim, ldt, dt, a, b = (t32(n) for n in ("lre", "lim", "ldt", "dt", "a_", "b_"))
            u1, u2, u3, u4, c16, s16, wre, wim = (t32(n) for n in ("u1", "u2", "u3", "u4", "c16", "s16", "wre", "wim"))
            Bcr = sb("s_Bcr", [128, 32, 16], F32, pa); Bci = sb("s_Bci", [128, 32, 16], F32, pa)
            Dr = sb("s_Dr", [128, 32, 32], F32, pa); Di = sb("s_Di", [128, 32, 32], F32, pa)
            Dr2 = sb("s_Dr2", [128, 32, 32], F32, pa); Di2 = sb("s_Di2", [128, 32, 32], F32, pa)
            Gr = sb("s_Gr", [128, 32, 32], F32, pa); Gi = sb("s_Gi", [128, 32, 32], F32, pa)
            T1 = sb("s_T1", [128, 32, 32], F32, pa); T2 = sb("s_T2", [128, 32, 32], F32, pa)
            T3 = sb("s_T3", [128, 32, 32], F32, pa); T4 = sb("s_T4", [128, 32, 32], F32, pa)
            P1 = sb("s_P1", [128, 32, 32], F32, pa); P2 = sb("s_P2", [128, 32, 32], F32, pa)
            P3 = sb("s_P3", [128, 32, 32], F32, pa); P4 = sb("s_P4", [128, 32, 32], F32, pa)
            Cbr = sb("s_Cbr", [128, 32, 32], BF16, pa); Cbni = sb("s_Cbni", [128, 32, 32], BF16, pa)
            Dbr = sb("s_Dbr", [128, 8, 160], BF16, pa); Dbi = sb("s_Dbi", [128, 8, 160], BF16, pa)
            Gbr = sb("s_Gbr", [128, 8, 160], BF16, pa); Gbni = sb("s_Gbni", [128, 8, 160], BF16, pa)
            Es = [sb("s_Es%d" % i, [128, 2, 4, 2, 2, 128], BF16, pa) for i in range(2)]
            m3 = sb("s_m3", [128, 1], F32, pa)
            dsk = sb("s_dsk", [128, 4], F32, pa)
            pk = [ps("s_pk%d" % i, [128, 128], F32, pa) for i in range(2)]
            pE = [ps("s_pE%d" % i, [128, 128], F32, pa) for i in range(4)]

            S.dma('sp', lre[:], lre_in, writes=['lre']); S.dma('sp', lim[:], lim_in, writes=['lim'])
            S.dma('sp', ldt[:], ldt_in, writes=['ldt']); S.dma('sp', dsk[:], dsk_in, writes=['dsk'])
            for X in (Dr, Di, Gr, Gi, Dr2, Di2):
                S.op('dve', lambda e: e.memset(X[:], 0.0), writes=[kn(X)])
            for X in (Dbr, Dbi, Gbr, Gbni):
                S.op('pool', lambda e: e.memset(X[:], 0.0), writes=[kn(X)])
            S.op('pool', lambda e: e.memset(Kt_sb[:], 0.0), writes=['Kt_sb'])
            for i in range(2):
                S.op('pool', lambda e: e.memset(Es[i][:], 0.0), writes=[('Es', i)])
            S.op('dve', lambda e: e.memset(m3[:], 1.0), writes=['m3'])
            S.op('dve', lambda e: e.memset(m3[64:96, :], 0.0), writes=['m3'])
            S.dma('sp', Bcr[:], Bre_in, writes=['Bcr']); S.dma('sp', Bci[:], Bim_in, writes=['Bci'])
            for h in range(2):
                S.dma('sp', Gr[64 * h:64 * h + 64, :, 16 * h:16 * h + 16], Cre_in[64 * h:64 * h + 64], reads=[], writes=['Gr'])
                S.dma('sp', Gi[64 * h:64 * h + 64, :, 16 * h:16 * h + 16], Cim_in[64 * h:64 * h + 64], reads=[], writes=['Gi'])
            for q in range(4):
                S.op('dve', lambda e: e.tensor_scalar(out=diagD[:, q, :], in0=identf[:], scalar1=dsk[:, q:q + 1], scalar2=None,
                                                      op0=ALU.mult), reads=['identf', 'dsk'], writes=['diagD'])
            V = lambda e: e
            def tt(out, in0, in1, op, r, w, eng='dve'):
                S.op(eng, lambda e: e.tensor_tensor(out=out, in0=in0, in1=in1, op=op), reads=r, writes=w)
            S.op('act', lambda e: e.activation(out=dt[:], in_=ldt[:], func=AF.Exp), reads=['ldt'], writes=['dt'])
            tt(u1[:], lre[:], dt[:], ALU.mult, ['lre', 'dt'], ['u1'])
            S.op('act', lambda e: e.activation(out=u1[:], in_=u1[:], func=AF.Exp), reads=['u1'], writes=['u1'])
            tt(u2[:], lim[:], dt[:], ALU.mult, ['lim', 'dt'], ['u2'])
            S.op('act', lambda e: e.activation(out=s16[:], in_=u2[:], func=AF.Sin, scale=1.0 / 16), reads=['u2'], writes=['s16'])
            S.op('act', lambda e: e.activation(out=u3[:], in_=u2[:], func=AF.Sin, scale=1.0 / 32), reads=['u2'], writes=['u3'])
            tt(u3[:], u3[:], u3[:], ALU.mult, ['u3'], ['u3'])
            S.op('dve', lambda e: e.tensor_scalar(out=c16[:], in0=u3[:], scalar1=-2.0, scalar2=1.0, op0=ALU.mult, op1=ALU.add),
                 reads=['u3'], writes=['c16'])

            def csquare(cr, ci, n):
                for _ in range(n):
                    tt(u3[:], cr[:], cr[:], ALU.mult, [kn(cr)], ['u3'])
                    tt(u4[:], ci[:], ci[:], ALU.mult, [kn(ci)], ['u4'])
                    tt(ci[:], cr[:], ci[:], ALU.mult, [kn(cr), kn(ci)], [kn(ci)])
                    S.op('dve', lambda e: e.tensor_scalar(out=ci[:], in0=ci[:], scalar1=2.0, scalar2=None, op0=ALU.mult),
                         reads=[kn(ci)], writes=[kn(ci)])
                    tt(cr[:], u3[:], u4[:], ALU.subtract, ['u3', 'u4'], [kn(cr)])
            csquare(c16, s16, 4)
            tt(a[:], u1[:], c16[:], ALU.mult, ['u1', 'c16'], ['a_'])
            tt(b[:], u1[:], s16[:], ALU.mult, ['u1', 's16'], ['b_'])
            S.op('dve', lambda e: e.tensor_copy(out=aL[:], in_=c16[:]), reads=['c16'], writes=['aL'])
            S.op('dve', lambda e: e.tensor_copy(out=bL[:], in_=s16[:]), reads=['s16'], writes=['bL'])
            csquare(aL, bL, 4)
            tt(u3[:], lre[:], dt[:], ALU.mult, ['lre', 'dt'], ['u3'])
            S.op('act', lambda e: e.activation(out=u3[:], in_=u3[:], func=AF.Exp, scale=16.0), reads=['u3'], writes=['u3'])
            tt(aL[:], aL[:], u3[:], ALU.mult, ['aL', 'u3'], ['aL'])
            tt(bL[:], bL[:], u3[:], ALU.mult, ['bL', 'u3'], ['bL'])
            S.op('dve', lambda e: e.tensor_scalar(out=u1[:], in0=a[:], scalar1=-1.0, scalar2=None, op0=ALU.add), reads=['a_'], writes=['u1'])
            tt(u2[:], lre[:], lre[:], ALU.mult, ['lre'], ['u2'])
            tt(u3[:], lim[:], lim[:], ALU.mult, ['lim'], ['u3'])
            tt(u2[:], u2[:], u3[:], ALU.add, ['u2', 'u3'], ['u2'])
            S.op('dve', lambda e: e.reciprocal(out=u2[:], in_=u2[:]), reads=['u2'], writes=['u2'])
            tt(u3[:], u1[:], lre[:], ALU.mult, ['u1', 'lre'], ['u3'])
            tt(u4[:], b[:], lim[:], ALU.mult, ['b_', 'lim'], ['u4'])
            tt(u3[:], u3[:], u4[:], ALU.add, ['u3', 'u4'], ['u3'])
            tt(wre[:], u3[:], u2[:], ALU.mult, ['u3', 'u2'], ['wre'])
            tt(u3[:], b[:], lre[:], ALU.mult, ['b_', 'lre'], ['u3'])
            tt(u4[:], u1[:], lim[:], ALU.mult, ['u1', 'lim'], ['u4'])
            tt(u3[:], u3[:], u4[:], ALU.subtract, ['u3', 'u4'], ['u3'])
            tt(wim[:], u3[:], u2[:], ALU.mult, ['u3', 'u2'], ['wim'])
            bc16 = lambda t: t[:].unsqueeze(2).to_broadcast([128, 32, 16])
            Tc = [T1[:, :, 0:16], T2[:, :, 0:16], T3[:, :, 0:16], T4[:, :, 0:16]]
            tt(Tc[0], Bcr[:], bc16(wre), ALU.mult, ['Bcr', 'wre'], ['T1'])
            tt(Tc[1], Bci[:], bc16(wim), ALU.mult, ['Bci', 'wim'], ['T2'])
            tt(Tc[2], Bci[:], bc16(wre), ALU.mult, ['Bci', 'wre'], ['T3'])
            tt(Tc[3], Bcr[:], bc16(wim), ALU.mult, ['Bcr', 'wim'], ['T4'])
            for h in range(2):
                sl = slice(64 * h, 64 * h + 64)
                tt(Dr[sl, :, 16 * h:16 * h + 16], T1[sl, :, 0:16], T2[sl, :, 0:16], ALU.subtract, ['T1', 'T2'], ['Dr'])
                tt(Di[sl, :, 16 * h:16 * h + 16], T3[sl, :, 0:16], T4[sl, :, 0:16], ALU.add, ['T3', 'T4'], ['Di'])
            S.op('dve', lambda e: e.tensor_copy(out=Cbr[:], in_=Gr[:]), reads=['Gr'], writes=['Cbr'])
            S.op('dve', lambda e: e.tensor_scalar(out=Cbni[:], in0=Gi[:], scalar1=-1.0, scalar2=None, op0=ALU.mult),
                 reads=['Gi'], writes=['Cbni'])
            bc32 = lambda t: t[:].unsqueeze(2).to_broadcast([128, 32, 32])

            def cmul(Xr, Xi, eng, Yr=None, Yi=None):
                Yr = Yr or Xr; Yi = Yi or Xi
                A1, A2, A3, A4 = (T1, T2, T3, T4) if eng == 'dve' else (P1, P2, P3, P4)
                tt(A1[:], Xr[:], bc32(a), ALU.mult, [kn(Xr), 'a_'], [kn(A1)], eng)
                tt(A2[:], Xi[:], bc32(b), ALU.mult, [kn(Xi), 'b_'], [kn(A2)], eng)
                tt(A3[:], Xi[:], bc32(a), ALU.mult, [kn(Xi), 'a_'], [kn(A3)], eng)
                tt(A4[:], Xr[:], bc32(b), ALU.mult, [kn(Xr), 'b_'], [kn(A4)], eng)
                tt(Yr[:], A1[:], A2[:], ALU.subtract, [kn(A1), kn(A2)], [kn(Yr)], eng)
                tt(Yi[:], A3[:], A4[:], ALU.add, [kn(A3), kn(A4)], [kn(Yi)], eng)

            def to5(dst, src, scale, key):
                d4 = dst[:].rearrange("p g (s c) -> p g s c", c=32)
                s4 = src[:].rearrange("p (g s) c -> p g s c", s=4)
                S.op('dve', lambda e: e.tensor_scalar(out=d4[:, :, 0:3, :], in0=s4[:, :, 0:3, :], scalar1=scale, scalar2=None, op0=ALU.mult),
                     reads=[kn(src)], writes=[key])
                S.op('dve', lambda e: e.tensor_scalar(out=d4[:, :, 4, :], in0=s4[:, :, 3, :], scalar1=scale, scalar2=None, op0=ALU.mult),
                     reads=[kn(src)], writes=[key])

            Dpairs = [(Dr, Di), (Dr2, Di2)]
            for j in range(16):
                Dr, Di = Dpairs[j % 2]
                to5(Dbr, Dr, 1.0, 'Dbr'); to5(Dbi, Di, 1.0, 'Dbi')
                if j < 15:
                    cmul(Dr, Di, 'dve', Dpairs[(j + 1) % 2][0], Dpairs[(j + 1) % 2][1])
                for d in range(2):
                    for q in range(4):
                        idx = (d * 4 + q) * 16 + j
                        pkt = pk[idx % 2]; pkk = ('pk', idx % 2)
                        for pl in range(4):
                            pair = d * 16 + q * 4 + pl
                            if pl < 3:
                                osl = pkt[32 * pl:32 * pl + 32, 32 * pl:32 * pl + 32]; csl = slice(32 * pl, 32 * pl + 32)
                            else:
                                osl = pkt[64:128, 96:128]; csl = slice(96, 160)
                            S.op('pe', lambda e: e.matmul(osl, lhsT=Dbr[:, d * 4 + q, csl], rhs=Cbr[:, pair, :], start=True, stop=False),
                                 reads=['Dbr', 'Cbr'], writes=[pkk])
                            S.op('pe', lambda e: e.matmul(osl, lhsT=Dbi[:, d * 4 + q, csl], rhs=Cbni[:, pair, :], start=False, stop=True),
                                 reads=['Dbi', 'Cbni'], writes=[pkk])
                        for pl in range(4):
                            if pl < 3:
                                rs_, cs_ = slice(32 * pl, 32 * pl + 32), slice(32 * pl, 32 * pl + 32)
                            else:
                                rs_, cs_ = slice(64, 128), slice(96, 128)
                            S.op('act', lambda e: e.activation(out=Kt_sb[rs_, idx, cs_], in_=pkt[rs_, cs_], func=AF.Copy),
                                 reads=[pkk], writes=['Kt_sb'])
                Et = Es[j % 2]; ek = ('Es', j % 2)
                n_e = 0
                for reim, X in ((0, Dr), (1, Di)):
                    for d in range(2):
                        for q in range(4):
                            pet = pE[n_e % 4]; pek = ('pE', n_e % 4); n_e += 1
                            src = X[:, d * 16 + q * 4:d * 16 + q * 4 + 4, :].rearrange("p a b -> p (a b)")
                            S.op('pe', lambda e: e.transpose(out=pet[:], in_=src, identity=identf[:]), reads=[kn(X), 'identf'], writes=[pek])
                            S.op('act', lambda e: e.activation(out=Et[:, d, q, reim, 0, :], in_=pet[:], func=AF.Copy), reads=[pek], writes=[ek])
                            S.op('dve', lambda e: e.tensor_scalar(out=Et[64:128, d, q, reim, 1, :], in0=pet[64:128, :], scalar1=m3[64:128, 0:1],
                                                                  scalar2=None, op0=ALU.mult), reads=[pek, 'm3'], writes=[ek])
                for d in range(2):
                    S.dma('sp', E_d[:, d, :, j, :, :, :], Et[:, d], reads=[ek], writes=['E_d'])
                cmul(Gr, Gi, 'pool')
                to5(Gbr, Gr, 1.0, 'Gbr'); to5(Gbni, Gi, -1.0, 'Gbni')
                for d in range(2):
                    S.dma('sp', G_d[:, (d * 16 + j) * 2 + 0, :], Gbr[:, d * 4:d * 4 + 4, :].rearrange("p a b -> p (a b)"), reads=['Gbr'], writes=['G_d'])
                    S.dma('sp', G_d[:, (d * 16 + j) * 2 + 1, :], Gbni[:, d * 4:d * 4 + 4, :].rearrange("p a b -> p (a b)"), reads=['Gbni'], writes=['G_d'])
            S.barrier()
        if 's5prep' in dbg:
            dbg_out['Kt'] = dout("dbg_Kt", [128, 128, 128], BF16)
            S.dma('sp', dbg_out['Kt'], Kt_sb[:], reads=['Kt_sb'])
            dbg_out['aL'] = dout("dbg_aL", [128, 2, 32])
            S.dma('sp', dbg_out['aL'][:, 0, :], aL[:], reads=['aL']); S.dma('sp', dbg_out['aL'][:, 1, :], bL[:], reads=['bL'])
            dbg_out['G'] = dout("dbg_G", [128, 64, 640], BF16)
            S.dma('sp', dbg_out['G'], G_d, reads=['G_d'])
            dbg_out['E'] = dout("dbg_E", [128, 2, 4, 16, 2, 2, 128], BF16)
            for d in range(2):
                S.dma('sp', dbg_out['E'][:, d], E_d[:, d], reads=['E_d'])
            return 'stop'
        return s5_main(nc, S, es, sb, ps, din, dscr, dout, dbg, dbg_out, identf, identb, uT_d, yaT_d, nbanks,
                       s5, Kt_sb, aL, bL, diagD, E_d, G_d, H_d, wglu_in, bglu_in)


def s5_main(nc, S, es, sb, ps, din, dscr, dout, dbg, dbg_out, identf, identb, uT_d, yaT_d, nbanks,
            s5, Kt_sb, aL, bL, diagD, E_d, G_d, H_d, wglu_in, bglu_in):
    NCH = 528
    with ExitStack() as pb:
        uTb = [sb("s_uTb%d" % i, [128, 2048], BF16, pb) for i in range(3)]
        Esl = [sb("s_Esl%d" % i, [128, 16, 2, 2, 128], BF16, pb) for i in range(2)]
        Ssb = [[sb("s_Ssb%d_%d" % (d, i), [128, 16, 2, 128], F32, pb) for i in range(2)] for d in range(2)]
        hist = [sb("s_hist%d" % d, [128, 16, 2, 129], F32, pb) for d in range(2)]
        histb = [sb("s_histb%d" % d, [128, 16, 2, 128], BF16, pb) for d in range(2)]
        tP = [sb("s_tP%d" % d, [128, 16, 2], F32, pb) for d in range(2)]
        tQ = [sb("s_tQ%d" % d, [128, 16, 2], F32, pb) for d in range(2)]
        pS = [ps("s_pS%d" % i, [128, 4, 128], F32, pb) for i in range(4)]
        for d in range(2):
            S.op('dve' if d == 0 else 'pool', lambda e: e.memset(hist[d][:], 0.0), writes=[('hist', d)])
        fblocks = [(0, 128), (128, 128), (256, 128), (384, 128), (512, 16)]
        bblocks = [(512, 16), (384, 128), (256, 128), (128, 128), (0, 128)]
        esl_ctr = 0
        ps_ctr = 0
        for step in range(5):
            for d in range(2):
                k0, nk = (fblocks if d == 0 else bblocks)[step]
                Sb = Ssb[d][step % 2]; skey = ('Ssb', d, step % 2)
                base = 0 if d == 0 else 256
                for q in range(4):
                    et = Esl[esl_ctr % 2]; ekey = ('Esl', esl_ctr % 2); esl_ctr += 1
                    S.dma('sp', et[:], E_d[:, d, q], reads=['E_d'], writes=[ekey])
                    ut = uTb[(esl_ctr - 1) % 3]; ukey = ('uTb', (esl_ctr - 1) % 3)
                    S.dma('sp', ut[:, 0:16 * nk], uT_d[q * 128:(q + 1) * 128, base + 16 * k0: base + 16 * k0 + 16 * nk], reads=['uT_d'], writes=[ukey])
                    for pl in range(4):
                        pst = pS[ps_ctr % 4]; pskey = ('pS', ps_ctr % 4); ps_ctr += 1
                        if pl < 3:
                            rsl, alt = slice(32 * pl, 32 * pl + 32), 0
                        else:
                            rsl, alt = slice(64, 128), 1
                        for reim in range(2):
                            for i in range(16):
                                j = 15 - i if d == 0 else i
                                rhs = ut[rsl, i:i + 16 * (nk - 1) + 1:16]
                                S.op('pe', lambda e: e.matmul(pst[:, reim, 0:nk], lhsT=et[rsl, j, reim, alt, :], rhs=rhs,
                                                              start=(i == 0), stop=(i == 15)),
                                     reads=[ekey, ukey], writes=[pskey])
                        S.op('act', lambda e: e.activation(out=Sb[:, q * 4 + pl, :, 0:nk], in_=pst[:, 0:2, 0:nk], func=AF.Copy),
                             reads=[pskey], writes=[skey])
            def chain_ops(d, kk):
                k0, nk = (fblocks if d == 0 else bblocks)[step]
                Sb = Ssb[d][step % 2]; skey = ('Ssb', d, step % 2)
                hk = ('hist', d)
                A2 = aL[:, d * 16:(d + 1) * 16].unsqueeze(2).to_broadcast([128, 16, 2])
                B2 = bL[:, d * 16:(d + 1) * 16].unsqueeze(2).to_broadcast([128, 16, 2])
                H = hist[d]
                if d == 0:
                    cprev, ccur, scol = kk, kk + 1, kk
                else:
                    cprev, ccur, scol = nk - kk, nk - 1 - kk, nk - 1 - kk
                prev = H[:, :, :, cprev]
                P_, Q_ = tP[d], tQ[d]
                eng = 'dve'
                return [
                    lambda: S.op(eng, lambda e: e.tensor_tensor(out=P_[:], in0=prev, in1=A2, op=ALU.mult), reads=[hk, 'aL'], writes=[('tP', d)]),
                    lambda: S.op(eng, lambda e: e.tensor_tensor(out=Q_[:], in0=prev, in1=B2, op=ALU.mult), reads=[hk, 'bL'], writes=[('tQ', d)]),
                    lambda: S.op(eng, lambda e: e.tensor_tensor(out=P_[:], in0=P_[:], in1=Sb[:, :, :, scol], op=ALU.add),
                                 reads=[('tP', d), skey], writes=[('tP', d)]),
                    lambda: S.op(eng, lambda e: e.tensor_tensor(out=H[:, :, 0, ccur], in0=P_[:, :, 0], in1=Q_[:, :, 1], op=ALU.subtract),
                                 reads=[('tP', d), ('tQ', d)], writes=[hk]),
                    lambda: S.op(eng, lambda e: e.tensor_tensor(out=H[:, :, 1, ccur], in0=P_[:, :, 1], in1=Q_[:, :, 0], op=ALU.add),
                                 reads=[('tP', d), ('tQ', d)], writes=[hk]),
                ]
            nks = [(fblocks if d == 0 else bblocks)[step][1] for d in range(2)]
            for kk in range(max(nks)):
                lists = [chain_ops(d, kk) for d in range(2) if kk < nks[d]]
                for oi in range(5):
                    for l in lists:
                        l[oi]()
            for d in range(2):
                eng = 'dve' if d == 0 else 'pool'
                k0, nk = (fblocks if d == 0 else bblocks)[step]
                hk = ('hist', d)
                H = hist[d]
                if d == 0:
                    S.op(eng, lambda e: e.tensor_copy(out=histb[d][:, :, :, 0:nk], in_=H[:, :, :, 1:nk + 1]), reads=[hk], writes=[('histb', d)])
                    S.op(eng, lambda e: e.tensor_copy(out=H[:, :, :, 0], in_=H[:, :, :, nk]), reads=[hk], writes=[hk])
                else:
                    S.op(eng, lambda e: e.tensor_copy(out=histb[d][:, :, :, 0:nk], in_=H[:, :, :, 0:nk]), reads=[hk], writes=[('histb', d)])
                    S.op(eng, lambda e: e.tensor_copy(out=H[:, :, :, 128], in_=H[:, :, :, 0]), reads=[hk], writes=[hk])
                S.dma('pool', H_d[d, :, :, :, k0:k0 + nk], histb[d][:, :, :, 0:nk], reads=[('histb', d)], writes=['H_d'])
        S.barrier()
    if 's5lvl3' in dbg:
        dbg_out['H'] = dout("dbg_H", [2, 128, 16, 2, 544], BF16)
        for d in range(2):
            S.dma('sp', dbg_out['H'][d], H_d[d], reads=['H_d'])
        return 'stop'

    with ExitStack() as pc:
        Gsb = sb("s_Gsb", [128, 64, 640], BF16, pc)
        wglu = sb("s_wglu", [128, 4, 512], BF16, pc)
        bglu = sb("s_bglu", [128, 4], F32, pc)
        ub = [sb("s_ub%d" % i, [128, 4, 544], BF16, pc) for i in range(2)]
        Hf = [sb("s_Hf%d" % i, [128, 16, 2, 32], BF16, pc) for i in range(2)]
        Hb = [sb("s_Hb%d" % i, [128, 16, 2, 32], BF16, pc) for i in range(2)]
        aT = [sb("s_aT%d" % i, [128, 4, 512], BF16, pc) for i in range(2)]
        sig = [sb("s_sig%d" % i, [128, 512], BF16, pc) for i in range(2)]
        yo = [sb("s_yo%d" % i, [128, 512], BF16, pc) for i in range(4)]
        py = [ps("s_py%d" % i, [128, 512], F32, pc) for i in range(4)]
        pg = [ps("s_pg%d" % i, [128, 512], F32, pc) for i in range(2)]
        for c in range(4):
            S.dma('sp', Gsb[:, c * 16:(c + 1) * 16, :], G_d[:, c * 16:(c + 1) * 16, :], reads=['G_d'], writes=[('Gsb', c)])
        gkeys = [('Gsb', c) for c in range(4)]
        for q in range(4):
            S.dma('pool', wglu[:, q, :], wglu_in[q * 128:(q + 1) * 128, :], writes=['wglu'])
        S.dma('sp', bglu[:], bglu_in, writes=['bglu'])
        yo_ctr = 0
        for n in range(nbanks):
            i2 = n % 2
            e0 = 256 + 512 * n
            S.dma('sp', ub[i2][:], uT_d[:, e0 - 16:e0 + 528].rearrange("(q p) t -> p q t", p=128), reads=['uT_d'], writes=[('ub', i2)])
            kf = 16 + 32 * n
            kb = 32 * n
            S.dma('sp', Hf[i2][:], H_d[0, :, :, :, kf - 1:kf + 31], reads=['H_d'], writes=[('Hf', i2)])
            S.dma('sp', Hb[i2][:], H_d[1, :, :, :, kb + 1:kb + 33], reads=['H_d'], writes=[('Hb', i2)])
            for q in range(4):
                pyt = py[q]; pk_ = ('py', q)
                pyv = pyt[:].rearrange("p (k j) -> p k j", j=16)
                uc = ub[i2][:, q, 16:528]
                ucv = uc.rearrange("p (k j) -> p k j", j=16)
                rd = [('ub', i2), 'Kt_sb', 'diagD']
                S.op('pe', lambda e: e.matmul(pyt[:], lhsT=diagD[:, q, :], rhs=uc, start=True, stop=False), reads=rd, writes=[pk_])
                S.op('pe', lambda e: e.matmul(pyt[:], lhsT=Kt_sb[:, (0 * 4 + q) * 16 + 0, :], rhs=uc, start=False, stop=False), reads=rd, writes=[pk_])
                for tau in range(1, 16):
                    S.op('pe', lambda e: e.matmul(pyv[:, :, tau:16], lhsT=Kt_sb[:, (0 * 4 + q) * 16 + tau, :], rhs=ucv[:, :, 0:16 - tau],
                                                  start=False, stop=False), reads=rd, writes=[pk_])
                S.op('pe', lambda e: e.matmul(pyt[:], lhsT=Kt_sb[:, (1 * 4 + q) * 16 + 0, :], rhs=uc, start=False, stop=False), reads=rd, writes=[pk_])
                for tau in range(1, 16):
                    S.op('pe', lambda e: e.matmul(pyv[:, :, 0:16 - tau], lhsT=Kt_sb[:, (1 * 4 + q) * 16 + tau, :], rhs=ucv[:, :, tau:16],
                                                  start=False, stop=False), reads=rd, writes=[pk_])
                for d in range(2):
                    Hx = Hf[i2] if d == 0 else Hb[i2]
                    hkey = ('Hf', i2) if d == 0 else ('Hb', i2)
                    for j in range(16):
                        m = j + 1 if d == 0 else 16 - j
                        for pl in range(4):
                            pair = q * 4 + pl
                            if pl < 3:
                                osl = pyv[32 * pl:32 * pl + 32, :, j]; csl = slice(q * 160 + 32 * pl, q * 160 + 32 * pl + 32)
                            else:
                                osl = pyv[64:128, :, j]; csl = slice(q * 160 + 96, q * 160 + 160)
                            for reim in range(2):
                                last = (d == 1 and j == 15 and pl == 3 and reim == 1)
                                S.op('pe', lambda e: e.matmul(osl, lhsT=Gsb[:, (d * 16 + m - 1) * 2 + reim, csl], rhs=Hx[:, pair, reim, :],
                                                              start=False, stop=last), reads=gkeys + [hkey], writes=[pk_])
                S.op('act', lambda e: e.activation(out=aT[i2][:, q, :], in_=pyt[:], func=AF.Gelu), reads=[pk_], writes=[('aT', i2, q)])
            for oc in range(4):
                pgt = pg[oc % 2]; pgk = ('pg', oc % 2)
                for q in range(4):
                    S.op('pe', lambda e: e.matmul(pgt[:], lhsT=wglu[:, q, oc * 128:(oc + 1) * 128], rhs=aT[i2][:, q, :],
                                                  start=(q == 0), stop=(q == 3)), reads=['wglu', ('aT', i2, q)], writes=[pgk])
                sg = sig[oc % 2]; sgk = ('sig', oc % 2)
                S.op('act', lambda e: e.activation(out=sg[:], in_=pgt[:], func=AF.Sigmoid, bias=bglu[:, oc:oc + 1]),
                     reads=[pgk, 'bglu'], writes=[sgk])
                yt = yo[yo_ctr % 4]; yk = ('yo', yo_ctr % 4); yo_ctr += 1
                S.op('dve', lambda e: e.tensor_tensor(out=yt[:], in0=aT[i2][:, oc, :], in1=sg[:], op=ALU.mult),
                     reads=[('aT', i2, oc), sgk], writes=[yk])
                S.dma('pool', yaT_d[oc * 128:(oc + 1) * 128, 512 * n:512 * n + 512], yt[:], reads=[yk], writes=['yaT_d'])
        S.barrier()
    if 's5' in dbg:
        dbg_out['yaT'] = dout("dbg_yaT", [512, 8192], BF16)
        for r0 in range(0, 512, 128):
            S.dma('sp', dbg_out['yaT'][r0:r0 + 128, :], yaT_d[r0:r0 + 128, :], reads=['yaT_d'])
    return 'ok'

NEG = -30000.0


def na_phase(nc, S, es, sb, ps, din, dscr, dout, dbg, dbg_out, identb, qT_d, kT_d, v_d, ybT_d, nblocks=16):
    rpbT_in = din("rpbT", [128, 8 * 14 * 64])
    maskT_in = din("maskT", [128, 64])
    with ExitStack() as pa:
        Tb = sb("n_Tb", [128, 8, 14, 64], F32, pa)
        mk = sb("n_mk", [128, 64], F32, pa)
        kctx = sb("n_kctx", [128, 4, 256], BF16, pa)
        vctx = sb("n_vctx", [128, 2, 8, 65], BF16, pa)
        qb = [sb("n_qb%d" % i, [128, 4, 512], BF16, pa) for i in range(2)]
        kw = [sb("n_kw%d" % i, [128, 4, 960], BF16, pa) for i in range(2)]
        vw = [sb("n_vw%d" % i, [128, 15, 8, 65], BF16, pa) for i in range(2)]
        sS = [sb("n_sS%d" % i, [128, 4, 64], F32, pa) for i in range(3)]
        P = [sb("n_Pn%d" % i, [128, 6, 64], BF16, pa) for i in range(4)]
        rc = [sb("n_rc%d" % i, [128, 8], F32, pa) for i in range(2)]
        ybt = [sb("n_ybt%d" % i, [128, 8, 64], BF16, pa) for i in range(2)]
        ybT = [sb("n_ybT%d" % i, [128, 4, 512], BF16, pa) for i in range(2)]
        pS = [ps("n_pS%d" % i, [128, 6, 64], F32, pa) for i in range(3)]
        pO = [ps("n_pO%d" % i, [128, 4, 65], F32, pa) for i in range(4)]
        pT = ps("n_pTn", [128, 4, 128], BF16, pa)

        S.dma('sp', Tb[:].rearrange("p h a q -> p (h a q)"), rpbT_in, writes=['Tb'])
        S.dma('sp', mk[:], maskT_in, writes=['mk'])
        S.dma('sp', kctx[:], kT_d[:, 0:256].rearrange("(c p) t -> p c t", p=128), reads=['kT_d'], writes=['kctx'])
        S.dma('sp', vctx[:], v_d[0:256].rearrange("(m p) h d -> p m h d", p=128), reads=['v_d'], writes=['vctx'])
        S.op('dve', lambda e: e.tensor_tensor(out=Tb[:].rearrange("p h a q -> p (h a) q"), in0=Tb[:].rearrange("p h a q -> p (h a) q"),
                                              in1=mk[:].unsqueeze(1).to_broadcast([128, 112, 64]), op=ALU.add),
             reads=['Tb', 'mk'], writes=['Tb'])
        item = 0
        pend = []
        for b in range(nblocks):
            i2 = b % 2
            r0 = 8 * b
            lo = min(max(r0 - 4, 0), 120)
            hi = min(max(r0 + 7 - 4, 0), 120) + 8
            nrows = hi - lo
            S.dma('sp', qb[i2][:], qT_d[:, 512 * b:512 * b + 512].rearrange("(c p) t -> p c t", p=128), reads=['qT_d'], writes=[('qb', i2)])
            S.dma('sp', kw[i2][:, :, 0:64 * nrows], kT_d[:, 256 + 64 * lo:256 + 64 * hi].rearrange("(c p) t -> p c t", p=128),
                  reads=['kT_d'], writes=[('kw', i2)])
            for s in range(nrows - 1):
                S.dma('sp', vw[i2][:, s], v_d[256 + 64 * (lo + s):256 + 64 * (lo + s) + 128], reads=['v_d'], writes=[('vw', i2)])
            for rr in range(8):
                r = r0 + rr
                krs = min(max(r - 4, 0), 120)
                a0 = krs - r + 7
                half = 64 * (r % 2)
                pp = (r // 2) % 2
                for h in range(8):
                    hc, hb = h // 2, 64 * (h % 2)
                    it = item % 3
                    ip = item % 4
                    item += 1
                    pst = pS[it]; psk = ('pS', it)
                    for c in range(4):
                        kc0 = 64 * (krs + 2 * c - lo)
                        S.op('pe', lambda e: e.matmul(pst[:, c, :], lhsT=kw[i2][hb:hb + 64, hc, kc0:kc0 + 128],
                                                      rhs=qb[i2][hb:hb + 64, hc, 64 * rr:64 * rr + 64], start=True, stop=True),
                             reads=[('kw', i2), ('qb', i2)], writes=[psk])
                    for c in range(2):
                        S.op('pe', lambda e: e.matmul(pst[:, 4 + c, :], lhsT=kctx[hb:hb + 64, hc, 128 * c:128 * c + 128],
                                                      rhs=qb[i2][hb:hb + 64, hc, 64 * rr:64 * rr + 64], start=True, stop=True),
                             reads=['kctx', ('qb', i2)], writes=[psk])
                    S.op('dve', lambda e: e.tensor_tensor(out=sS[it][:], in0=pst[:, 0:4, :], in1=Tb[:, h, a0:a0 + 7:2, :], op=ALU.add),
                         reads=[psk, 'Tb'], writes=[('sS', it)])
                    S.op('act', lambda e: e.activation(out=P[ip][:, 0:4, :], in_=sS[it][:], func=AF.Exp), reads=[('sS', it)], writes=[('P', ip)])
                    S.op('act', lambda e: e.activation(out=P[ip][:, 4:6, :], in_=pst[:, 4:6, :], func=AF.Exp), reads=[psk], writes=[('P', ip)])
                    if len(pend) == 2:
                        pend.pop(0)()
                    def mk_pv(it=ip, i2=i2, krs=krs, lo=lo, h=h, half=half, pp=pp, r=r, rr=rr, b=b):
                        def pv():
                            pot = pO[pp * 2 + h // 4]; pok = ('pO', pp * 2 + h // 4)
                            for c in range(6):
                                if c < 4:
                                    rhs = vw[i2][:, krs + 2 * c - lo, h, :]
                                    rk = ('vw', i2)
                                else:
                                    rhs = vctx[:, c - 4, h, :]
                                    rk = 'vctx'
                                S.op('pe', lambda e: e.matmul(pot[half:half + 64, h % 4, :], lhsT=P[it][:, c, :], rhs=rhs,
                                                              start=(c == 0), stop=(c == 5)), reads=[('P', it), rk], writes=[pok])
                            if h % 4 == 3:
                                hh = h // 4
                                yk = ('ybt', pp)
                                S.op('dve', lambda e: e.reciprocal(out=rc[pp][half:half + 64, 4 * hh:4 * hh + 4], in_=pot[half:half + 64, :, 64]),
                                     reads=[pok], writes=[('rc', pp)])
                                S.op('dve', lambda e: e.tensor_tensor(out=ybt[pp][half:half + 64, 4 * hh:4 * hh + 4, :], in0=pot[half:half + 64, :, 0:64],
                                                                      in1=rc[pp][half:half + 64, 4 * hh:4 * hh + 4].unsqueeze(2).to_broadcast([64, 4, 64]),
                                                                      op=ALU.mult), reads=[pok, ('rc', pp)], writes=[yk])
                            if h == 7 and r % 2 == 1:
                                tk = ('ybT', b % 2)
                                for c4 in range(4):
                                    S.op('pe', lambda e: e.transpose(out=pT[:, c4, :], in_=ybt[pp][:, 2 * c4:2 * c4 + 2, :].rearrange("p a d -> p (a d)"),
                                                                     identity=identb[:]), reads=[('ybt', pp), 'identb'], writes=['pTn'])
                                S.op('act', lambda e: e.activation(out=ybT[b % 2][:, :, 64 * (rr - 1):64 * (rr - 1) + 128], in_=pT[:], func=AF.Copy),
                                     reads=['pTn'], writes=[tk])
                                if rr == 7:
                                    S.dma('act', ybT_d[:, 512 * b:512 * b + 512].rearrange("(c p) t -> p c t", p=128), ybT[b % 2][:],
                                          reads=[tk], writes=['ybT_d'])
                        return pv
                    pend.append(mk_pv())
        for p_ in pend:
            p_()
        S.barrier()
    if 'na' in dbg:
        dbg_out['ybT'] = dout("dbg_ybT", [512, 8192], BF16)
        for r0 in range(0, 512, 128):
            S.dma('sp', dbg_out['ybT'][r0:r0 + 128, :], ybT_d[r0:r0 + 128, :], reads=['ybT_d'])


def na_host(inp):
    rpb = inp['na_rpb'][0].astype(np.float32)
    i = np.arange(2)[:, None, None, None]
    kc = np.arange(64)[None, :, None, None]
    a = np.arange(14)[None, None, :, None]
    qc = np.arange(64)[None, None, None, :]
    dcol = np.clip(kc - qc + 15, 0, 30)
    drow = a + i
    T = rpb[:, drow, dcol]
    T = np.broadcast_to(T, (8, 2, 64, 14, 64)).transpose(1, 2, 0, 3, 4).reshape(128, 8 * 14 * 64)
    q = np.arange(64)
    qcs = np.clip(q - 8, 0, 48)
    k = np.arange(64)[:, None]
    inw = (k >= qcs[None, :]) & (k < qcs[None, :] + 16)
    m = np.where(inw, 0.0, NEG).astype(np.float32)
    m = np.concatenate([m, m], axis=0)
    return {'rpbT': np.ascontiguousarray(T.astype(np.float32)), 'maskT': np.ascontiguousarray(m)}

D = 1024


def merge_phase(nc, S, es, sb, ps, din, dscr, dout, dbg, dbg_out, identf, identb, epsc, bc_g1, bc_mul2, bc_add2,
                xcat, out, yaT_d, ybT_d, sgaT_d, sgbT_d, h2_d, aff_d, affT_d, nblocks=16):
    wpa_in = din("w_proj_a", [512, D]); wpb_in = din("w_proj_b", [512, D]); wout_in = din("w_out", [D, D])
    wr_in = din("w_routerT", [128, 8, 16])
    with ExitStack() as pa:
        wpa = sb("g_wpa", [128, 4, D], BF16, pa); wpb = sb("g_wpb", [128, 4, D], BF16, pa)
        wout = sb("g_wout", [128, 8, D], BF16, pa)
        wr = sb("g_wr", [128, 8, 16], BF16, pa)
        ya = [sb("g_mya%d" % i, [128, 4, 512], BF16, pa) for i in range(2)]
        yb = [sb("g_myb%d" % i, [128, 4, 512], BF16, pa) for i in range(2)]
        sga = [sb("g_msga%d" % i, [128, 8, 512], BF16, pa) for i in range(2)]
        sgb = [sb("g_msgb%d" % i, [128, 8, 512], BF16, pa) for i in range(2)]
        mT = [sb("g_mT%d" % i, [128, 8, 512], BF16, pa) for i in range(2)]
        t1 = [sb("g_mt1_%d" % i, [128, 512], F32, pa) for i in range(2)]
        t2 = [sb("g_mt2_%d" % i, [128, 512], F32, pa) for i in range(2)]
        xt = [sb("g_mxt%d" % i, [128, D], F32, pa) for i in range(2)]
        x1 = [sb("g_mx1_%d" % i, [128, D], F32, pa) for i in range(3)]
        h2 = [sb("g_mh2_%d" % i, [128, D], F32, pa) for i in range(3)]
        h2b = [sb("g_mh2b_%d" % i, [128, D], BF16, pa) for i in range(3)]
        h2T = [sb("g_mh2T_%d" % i, [128, 8, 128], BF16, pa) for i in range(3)]
        junk = sb("g_mjunk", [128, D], BF16, pa)
        st = [sb("g_mst%d" % i, [128, 8], F32, pa) for i in range(3)]
        lg = [sb("g_mlg%d" % i, [128, 16], F32, pa) for i in range(3)]
        af = [sb("g_maf%d" % i, [128, 16], F32, pa) for i in range(3)]
        A16 = sb("g_A16", [16, 8192], F32, pa)
        pab = [ps("g_mpab%d" % i, [128, 512], F32, pa) for i in range(4)]
        pyo = ps("g_mpyo", [128, D], F32, pa)
        phT = ps("g_mphT", [128, 8, 128], BF16, pa)
        pmisc = ps("g_mpmisc", [128, 256], F32, pa)
        plg = pmisc[:, 0:16]
        paT = pmisc[0:16, 128:256]

        for q in range(4):
            S.dma('pool', wpa[:, q, :], wpa_in[q * 128:(q + 1) * 128, :], writes=['wpa'])
            S.dma('pool', wpb[:, q, :], wpb_in[q * 128:(q + 1) * 128, :], writes=['wpb'])
        for dc in range(8):
            S.dma('pool', wout[:, dc, :], wout_in[dc * 128:(dc + 1) * 128, :], writes=['wout'])
        S.dma('pool', wr[:], wr_in, writes=['wr'])
        tile_ctr = 0
        pend = []
        for b in range(nblocks):
            i2 = b % 2
            cs = slice(512 * b, 512 * b + 512)
            S.dma('sp', ya[i2][:], yaT_d[:, cs].rearrange("(c p) t -> p c t", p=128), reads=['yaT_d'], writes=[('ya', i2)])
            S.dma('sp', yb[i2][:], ybT_d[:, cs].rearrange("(c p) t -> p c t", p=128), reads=['ybT_d'], writes=[('yb', i2)])
            S.dma('sp', sga[i2][:], sgaT_d[:, cs].rearrange("(c p) t -> p c t", p=128), reads=['sgaT_d'], writes=[('sga', i2)])
            S.dma('sp', sgb[i2][:], sgbT_d[:, cs].rearrange("(c p) t -> p c t", p=128), reads=['sgbT_d'], writes=[('sgb', i2)])
            for dc in range(8):
                j2 = dc % 2
                pA_, pB_ = pab[2 * j2], pab[2 * j2 + 1]
                for q in range(4):
                    S.op('pe', lambda e: e.matmul(pA_[:], lhsT=wpa[:, q, dc * 128:(dc + 1) * 128], rhs=ya[i2][:, q, :],
                                                  start=(q == 0), stop=(q == 3)), reads=['wpa', ('ya', i2)], writes=[('pA', j2)])
                for q in range(4):
                    S.op('pe', lambda e: e.matmul(pB_[:], lhsT=wpb[:, q, dc * 128:(dc + 1) * 128], rhs=yb[i2][:, q, :],
                                                  start=(q == 0), stop=(q == 3)), reads=['wpb', ('yb', i2)], writes=[('pB', j2)])
                S.op('dve', lambda e: e.tensor_tensor(out=t1[j2][:], in0=pA_[:], in1=sga[i2][:, dc, :], op=ALU.mult),
                     reads=[('pA', j2), ('sga', i2)], writes=[('t1', j2)])
                S.op('dve', lambda e: e.tensor_tensor(out=t2[j2][:], in0=pB_[:], in1=sgb[i2][:, dc, :], op=ALU.mult),
                     reads=[('pB', j2), ('sgb', i2)], writes=[('t2', j2)])
                S.op('pool', lambda e: e.tensor_tensor(out=mT[i2][:, dc, :], in0=t1[j2][:], in1=t2[j2][:], op=ALU.add),
                     reads=[('t1', j2), ('t2', j2)], writes=[('mT', i2)])
            for ti in range(4):
                k2 = tile_ctr % 3
                kx = tile_ctr % 2
                tile_ctr += 1
                tok0 = 512 * b + 128 * ti
                S.dma('sp', xt[kx][:], xcat[256 + tok0:256 + tok0 + 128, :], writes=[('xt', kx)])
                for hf in range(2):
                    for dc in range(8):
                        S.op('pe', lambda e: e.matmul(pyo[:, hf * 512:(hf + 1) * 512], lhsT=mT[i2][:, dc, ti * 128:(ti + 1) * 128],
                                                      rhs=wout[:, dc, hf * 512:(hf + 1) * 512], start=(dc == 0), stop=(dc == 7)),
                             reads=[('mT', i2), 'wout'], writes=['pyo'])
                S.op('dve', lambda e: e.tensor_tensor(out=x1[k2][:], in0=pyo[:], in1=bc_g1[:], op=ALU.mult),
                     reads=['pyo', 'bc_g1'], writes=[('x1', k2)])
                S.op('dve', lambda e: e.tensor_tensor(out=x1[k2][:], in0=x1[k2][:], in1=xt[kx][:], op=ALU.add),
                     reads=[('x1', k2), ('xt', kx)], writes=[('x1', k2)])
                S.dma('pool', out[tok0:tok0 + 128, :], x1[k2][:], reads=[('x1', k2)], writes=['out'])
                s_ = st[k2]; sk = ('st', k2)
                S.op('act', lambda e: e.activation(out=junk[:], in_=x1[k2][:], func=AF.Square, accum_out=s_[:, 0:1]),
                     reads=[('x1', k2)], writes=['mjunk', sk])
                S.op('act', lambda e: e.activation(out=s_[:, 6:7], in_=s_[:, 0:1], func=AF.Ln, scale=1.0 / D, bias=epsc[:, 0:1]),
                     reads=[sk, 'epsc'], writes=[sk])
                S.op('act', lambda e: e.activation(out=s_[:, 1:2], in_=s_[:, 6:7], func=AF.Exp, scale=-0.5), reads=[sk], writes=[sk])
                S.op('dve', lambda e: e.scalar_tensor_tensor(out=h2[k2][:], in0=x1[k2][:], scalar=s_[:, 1:2], in1=bc_mul2[:],
                                                             op0=ALU.mult, op1=ALU.mult), reads=[('x1', k2), sk, 'bc_mul2'], writes=[('h2', k2)])
                S.op('dve', lambda e: e.tensor_tensor(out=h2[k2][:], in0=h2[k2][:], in1=bc_add2[:], op=ALU.add),
                     reads=[('h2', k2), 'bc_add2'], writes=[('h2', k2)])
                S.op('act', lambda e: e.activation(out=h2b[k2][:], in_=h2[k2][:], func=AF.Copy), reads=[('h2', k2)], writes=[('h2b', k2)])
                S.dma('act', h2_d[tok0:tok0 + 128, :], h2b[k2][:], reads=[('h2b', k2)], writes=['h2_d'])
                def mk_tail(k2=k2, tok0=tok0, s_=s_, sk=sk):
                    def tail():
                        for dc in range(8):
                            S.op('pe', lambda e: e.transpose(out=phT[:, dc, :], in_=h2b[k2][:, dc * 128:(dc + 1) * 128], identity=identb[:]),
                                 reads=[('h2b', k2), 'identb'], writes=['phT'])
                        S.op('act', lambda e: e.activation(out=h2T[k2][:], in_=phT[:], func=AF.Copy), reads=['phT'], writes=[('h2T', k2)])
                        for dc in range(8):
                            S.op('pe', lambda e: e.matmul(plg, lhsT=h2T[k2][:, dc, :], rhs=wr[:, dc, :], start=(dc == 0), stop=(dc == 7)),
                                 reads=[('h2T', k2), 'wr'], writes=['plg'])
                        S.op('dve', lambda e: e.tensor_copy(out=lg[k2][:], in_=plg), reads=['plg'], writes=[('lg', k2)])
                        S.op('dve', lambda e: e.reduce_max(out=s_[:, 2:3], in_=lg[k2][:], axis=AX.X), reads=[('lg', k2)], writes=[sk])
                        S.op('dve', lambda e: e.tensor_scalar(out=s_[:, 3:4], in0=s_[:, 2:3], scalar1=-1.0, scalar2=None, op0=ALU.mult),
                             reads=[sk], writes=[sk])
                        S.op('act', lambda e: e.activation(out=af[k2][:], in_=lg[k2][:], func=AF.Exp, bias=s_[:, 3:4], accum_out=s_[:, 4:5]),
                             reads=[('lg', k2), sk], writes=[('af', k2), sk])
                        S.op('dve', lambda e: e.reciprocal(out=s_[:, 5:6], in_=s_[:, 4:5]), reads=[sk], writes=[sk])
                        S.op('dve', lambda e: e.tensor_scalar(out=af[k2][:], in0=af[k2][:], scalar1=s_[:, 5:6], scalar2=None, op0=ALU.mult),
                             reads=[('af', k2), sk], writes=[('af', k2)])
                        S.dma('pool', aff_d[tok0:tok0 + 128, :], af[k2][:], reads=[('af', k2)], writes=['aff_d'])
                        S.op('pe', lambda e: e.transpose(out=paT, in_=af[k2][:], identity=identf[:]), reads=[('af', k2), 'identf'], writes=['paT'])
                        S.op('act', lambda e: e.activation(out=A16[:, tok0:tok0 + 128], in_=paT, func=AF.Copy), reads=['paT'], writes=['A16'])
                    return tail
                if len(pend) == 2:
                    pend.pop(0)()
                pend.append(mk_tail())
        for t_ in pend:
            t_()
        S.dma('sp', affT_d, A16[:], reads=['A16'], writes=['affT_d'])
        S.barrier()
        if 'mg2' in dbg:
            for nm, t, shp, dt in (('bcg1', bc_g1, [128, D], F32), ('mT0', mT[(nblocks - 1) % 2], [128, 8, 512], BF16), ('t1', t1[1], [128, 512], F32),
                                   ('sga0', sga[(nblocks - 1) % 2], [128, 8, 512], BF16), ('ya0', ya[(nblocks - 1) % 2], [128, 4, 512], BF16),
                                   ('yb0', yb[(nblocks - 1) % 2], [128, 4, 512], BF16), ('wpa', wpa, [128, 4, D], BF16), ('x1t', x1[1], [128, D], F32),
                                   ('xt', xt[1], [128, D], F32)):
                dbg_out[nm] = dout("dbg_" + nm, shp, dt)
                S.dma('sp', dbg_out[nm], t[:], reads=[])
    if 'mg' in dbg:
        dbg_out['aff'] = dout("dbg_aff", [8192, 16])
        S.dma('sp', dbg_out['aff'], aff_d, reads=['aff_d'])
        dbg_out['affT'] = dout("dbg_affT", [16, 8192])
        S.dma('sp', dbg_out['affT'], affT_d, reads=['affT_d'])
        dbg_out['h2'] = dout("dbg_h2", [8192, D], BF16)
        for r0 in range(0, 8192, 1024):
            S.dma('sp', dbg_out['h2'][r0:r0 + 1024, :], h2_d[r0:r0 + 1024, :], reads=['h2_d'])

D = 1024
CAP = 1024
NT = 8192


def moe_phase(nc, S, es, sb, ps, din, dscr, dout, dbg, dbg_out, identf, identb, bc_g2, out, h2_d, aff_d, affT_d, nexp=16):
    wg_in = din("w_e_gate", [16, D, D]); wu_in = din("w_e_up", [16, D, D]); wd_in = din("w_e_down", [16, D, D])
    m16_in = din("m16", [128, 128])
    with ExitStack() as pm:
        cum_d = dscr("cum_d", [16, NT], F32)
        slotv = sb("e_slotv", [128, 8], F32, pm)
        thr_dbg = sb("e_thr", [128, 1], F32, pm)
        S.op('pool', lambda e: e.iota(slotv[:], pattern=[[128, 8]], base=0, channel_multiplier=1, allow_small_or_imprecise_dtypes=True),
             writes=['slotv'])
        with ExitStack() as pa:
            A128 = sb("e_A128", [128, 1024], F32, pa)
            A16 = sb("e_A16", [16, NT], F32, pa)
            C16 = sb("e_C16", [16, NT], F32, pa)
            M16 = sb("e_M16", [128, 128], F32, pa)
            junk = sb("e_junk", [128, 1024], BF16, pa)
            lo = sb("e_lo", [128, 1], F32, pa); mid = sb("e_mid", [128, 1], F32, pa)
            cnt = sb("e_cnt", [128, 1], F32, pa); g = sb("e_g", [128, 1], F32, pa)
            one16 = sb("e_one16", [16, 1], F32, pa)
            ptot = ps("e_ptot", [128, 1], F32, pa)
            for s in range(8):
                S.dma('sp', A128[16 * s:16 * s + 16, :], affT_d[:, 1024 * s:1024 * s + 1024], reads=['affT_d'], writes=['A128'])
            S.dma('sp', A16[:], affT_d, reads=['affT_d'], writes=['A16'])
            S.dma('sp', M16[:], m16_in, writes=['M16'])
            S.op('dve', lambda e: e.memset(lo[:], 0.0), writes=['lo'])
            S.op('dve', lambda e: e.memset(one16[:], 1.0), writes=['one16'])
            for k in range(30):
                hk = 2.0 ** -(k + 1)
                S.op('dve', lambda e: e.tensor_scalar(out=mid[:], in0=lo[:], scalar1=hk, scalar2=None, op0=ALU.add), reads=['lo'], writes=['mid'])
                S.op('dve', lambda e: e.tensor_scalar(out=junk[:], in0=A128[:], scalar1=mid[:, 0:1], scalar2=0.0, op0=ALU.is_gt, op1=ALU.add,
                                                      accum_out=cnt[:]), reads=['A128', 'mid'], writes=['junk', 'cnt'])
                S.op('pe', lambda e: e.matmul(ptot[:], lhsT=M16[:], rhs=cnt[:], start=True, stop=True), reads=['M16', 'cnt'], writes=['ptot'])
                S.op('dve', lambda e: e.tensor_scalar(out=g[:], in0=ptot[:], scalar1=float(CAP), scalar2=hk, op0=ALU.is_ge, op1=ALU.mult),
                     reads=['ptot'], writes=['g'])
                S.op('dve', lambda e: e.tensor_tensor(out=lo[:], in0=lo[:], in1=g[:], op=ALU.add), reads=['lo', 'g'], writes=['lo'])
            S.op('dve', lambda e: e.tensor_copy(out=thr_dbg[:], in_=lo[:]), reads=['lo'], writes=['thr'])
            S.op('dve', lambda e: e.tensor_scalar(out=A16[:], in0=A16[:], scalar1=lo[0:16, 0:1], scalar2=None, op0=ALU.is_gt),
                 reads=['A16', 'lo'], writes=['A16'])
            S.op('dve', lambda e: e.tensor_tensor_scan(out=C16[:], data0=one16[:, 0:1].to_broadcast([16, NT]), data1=A16[:], initial=0.0,
                                                       op0=ALU.mult, op1=ALU.add), reads=['A16', 'one16'], writes=['C16'])
            S.dma('sp', cum_d, C16[:], reads=['C16'], writes=['cum_d'])
            S.barrier()
        if 'moe_thr' in dbg:
            dbg_out['thr'] = dout("dbg_thr", [128, 1])
            S.dma('sp', dbg_out['thr'], thr_dbg[:], reads=['thr'])
        with ExitStack() as pe_:
            Wg = [sb("e_Wg%d" % i, [128, 8, D], BF16, pe_) for i in range(2)]
            Wu = [sb("e_Wu%d" % i, [128, 8, D], BF16, pe_) for i in range(2)]
            Wd = [sb("e_Wd%d" % i, [128, 8, D], BF16, pe_) for i in range(2)]
            xe = [sb("e_xe%d" % i, [128, D], BF16, pe_) for i in range(8)]
            ag = [[sb("e_ag%d_%d" % (p_, i), [128, 16], F32, pe_) for i in range(8)] for p_ in range(2)]
            cq = [sb("e_cq%d" % i, [128, 2048], F32, pe_) for i in range(2)]
            junkq = sb("e_junkq", [128, 2048], BF16, pe_)
            acc = [sb("e_acc%d" % i, [128, 4, 8], F32, pe_) for i in range(3)]
            idxs = sb("e_idxs", [128, 8], F32, pe_)
            idxi = [sb("e_idxi%d" % i, [128, 8], I32, pe_) for i in range(3)]
            xeT = sb("e_xeT", [128, 8, CAP], BF16, pe_)
            hidT = sb("e_hidT", [128, 8, CAP], BF16, pe_)
            sg = [sb("e_sg%d" % i, [128, 512], F32, pe_) for i in range(2)]
            ye = [sb("e_ye%d" % i, [128, D], F32, pe_) for i in range(2)]
            pGU = [ps("e_pGU%d" % i, [128, 512], F32, pe_) for i in range(3)]
            pY = ps("e_pY", [128, D], F32, pe_)
            pT = ps("e_pT", [128, 8, 128], BF16, pe_)
            pTi = pT[:].bitcast(F32) if False else None

            def load_w(e):
                i2 = e % 2
                for dc in range(8):
                    S.dma('pool', Wg[i2][:, dc, :], wg_in[e, dc * 128:(dc + 1) * 128, :], writes=[('Wg', i2)])
                    S.dma('pool', Wu[i2][:, dc, :], wu_in[e, dc * 128:(dc + 1) * 128, :], writes=[('Wu', i2)])
                for dc in range(8):
                    S.dma('pool', Wd[i2][:, dc, :], wd_in[e, dc * 128:(dc + 1) * 128, :], writes=[('Wd', i2)])

            def load_cq(n):
                e_, qd = n // 4, n % 4
                if e_ >= nexp:
                    return
                S.dma('sp', cq[n % 2][:], cum_d[e_:e_ + 1, qd * 2048:(qd + 1) * 2048].to_broadcast([128, 2048]),
                      reads=['cum_d'], writes=[('cq', n % 2)])

            def idx_piece(e, piece):
                j3 = e % 3
                n = e * 4 + piece // 2
                if piece % 2 == 0:
                    if n == 0:
                        load_cq(0)
                    load_cq(n + 1)
                for sc in range(4 * (piece % 2), 4 * (piece % 2) + 4):
                    S.op('dve', lambda e_: e_.tensor_scalar(out=junkq[:], in0=cq[n % 2][:], scalar1=slotv[:, sc:sc + 1], scalar2=0.0,
                                                            op0=ALU.is_le, op1=ALU.add, accum_out=acc[j3][:, piece // 2, sc:sc + 1]),
                         reads=[('cq', n % 2), 'slotv'], writes=['junkq', ('acc', j3)])
                if piece == 7:
                    S.op('dve', lambda e_: e_.tensor_tensor(out=idxs[:], in0=acc[j3][:, 0, :], in1=acc[j3][:, 1, :], op=ALU.add),
                         reads=[('acc', j3)], writes=['idxs'])
                    S.op('dve', lambda e_: e_.tensor_tensor(out=idxs[:], in0=idxs[:], in1=acc[j3][:, 2, :], op=ALU.add),
                         reads=[('acc', j3), 'idxs'], writes=['idxs'])
                    S.op('dve', lambda e_: e_.tensor_tensor(out=idxs[:], in0=idxs[:], in1=acc[j3][:, 3, :], op=ALU.add),
                         reads=[('acc', j3), 'idxs'], writes=['idxs'])
                    S.op('dve', lambda e_: e_.tensor_copy(out=idxi[j3][:], in_=idxs[:]), reads=['idxs'], writes=[('idxi', j3)])

            def gather(e):
                j2 = e % 3
                for sc in range(8):
                    S.custom_dma('pool', lambda e_: e_.indirect_dma_start(out=xe[sc][:, :], out_offset=None, in_=h2_d[:, :],
                                 in_offset=bass.IndirectOffsetOnAxis(ap=idxi[j2][:, sc:sc + 1], axis=0)),
                                 reads=[('idxi', j2), 'h2_d'], writes=[('xe', sc)])
                    S.custom_dma('pool', lambda e_: e_.indirect_dma_start(out=ag[e % 2][sc][:, :], out_offset=None, in_=aff_d[:, :],
                                 in_offset=bass.IndirectOffsetOnAxis(ap=idxi[j2][:, sc:sc + 1], axis=0)),
                                 reads=[('idxi', j2), 'aff_d'], writes=[('ag', e % 2, sc)])

            gu_ctr = [0]

            def xpose(e):
                for sc in range(8):
                    for dc in range(8):
                        S.op('pe', lambda e_: e_.transpose(out=pT[:, dc, :], in_=xe[sc][:, dc * 128:(dc + 1) * 128], identity=identb[:]),
                             reads=[('xe', sc), 'identb'], writes=['pT'])
                    S.op('act', lambda e_: e_.activation(out=xeT[:, :, sc * 128:(sc + 1) * 128], in_=pT[:], func=AF.Copy),
                         reads=['pT'], writes=['xeT'])
            def ffn(e, nxt):
                i2 = e % 2; j2 = e % 3
                for fc in range(8):
                    for hf in range(2):
                        a = gu_ctr[0] % 3; gu_ctr[0] += 1
                        b = gu_ctr[0] % 3; gu_ctr[0] += 1
                        for dc in range(8):
                            S.op('pe', lambda e_: e_.matmul(pGU[a][:], lhsT=Wg[i2][:, dc, fc * 128:(fc + 1) * 128], rhs=xeT[:, dc, hf * 512:(hf + 1) * 512],
                                                            start=(dc == 0), stop=(dc == 7)), reads=[('Wg', i2), 'xeT'], writes=[('pGU', a)])
                        for dc in range(8):
                            S.op('pe', lambda e_: e_.matmul(pGU[b][:], lhsT=Wu[i2][:, dc, fc * 128:(fc + 1) * 128], rhs=xeT[:, dc, hf * 512:(hf + 1) * 512],
                                                            start=(dc == 0), stop=(dc == 7)), reads=[('Wu', i2), 'xeT'], writes=[('pGU', b)])
                        s2 = (fc * 2 + hf) % 2
                        S.op('act', lambda e_: e_.activation(out=sg[s2][:], in_=pGU[a][:], func=AF.Silu), reads=[('pGU', a)], writes=[('sg', s2)])
                        S.op('dve', lambda e_: e_.tensor_tensor(out=hidT[:, fc, hf * 512:(hf + 1) * 512], in0=pGU[b][:], in1=sg[s2][:], op=ALU.mult),
                             reads=[('pGU', b), ('sg', s2)], writes=['hidT'])
                    if nxt is not None:
                        idx_piece(nxt, fc)
                for sc in range(8):
                    for hf in range(2):
                        for fc in range(8):
                            S.op('pe', lambda e_: e_.matmul(pY[:, hf * 512:(hf + 1) * 512], lhsT=hidT[:, fc, sc * 128:(sc + 1) * 128],
                                                            rhs=Wd[i2][:, fc, hf * 512:(hf + 1) * 512], start=(fc == 0), stop=(fc == 7)),
                                 reads=['hidT', ('Wd', i2)], writes=['pY'])
                    y2 = sc % 2
                    S.op('dve', lambda e_: e_.scalar_tensor_tensor(out=ye[y2][:], in0=pY[:], scalar=ag[e % 2][sc][:, e:e + 1], in1=bc_g2[:],
                                                                   op0=ALU.mult, op1=ALU.mult), reads=['pY', ('ag', e % 2, sc), 'bc_g2'], writes=[('ye', y2)])
                    S.custom_dma('pool', lambda e_: e_.indirect_dma_start(out=out[:, :], out_offset=bass.IndirectOffsetOnAxis(ap=idxi[j2][:, sc:sc + 1], axis=0),
                                 in_=ye[y2][:, :], in_offset=None, compute_op=ALU.add), reads=[('ye', y2), ('idxi', j2)], writes=['out'])

            load_w(0)
            for p in range(8):
                idx_piece(0, p)
            gather(0)
            if nexp > 1:
                load_w(1)
                for p in range(8):
                    idx_piece(1, p)
            for e in range(nexp):
                xpose(e)
                if e + 1 < nexp:
                    gather(e + 1)
                    if e >= 1:
                        load_w(e + 1)
                ffn(e, e + 2 if e + 2 < nexp else None)
            if 'moe_idx' in dbg:
                dbg_out['idxi'] = dout("dbg_idxi", [128, 8], I32)
                S.dma('sp', dbg_out['idxi'], idxi[(nexp - 1) % 3][:], reads=[('idxi', (nexp - 1) % 3)])
            S.barrier()


def moe_host(inp):
    f = np.float32
    m16 = np.zeros((128, 128), f)
    for s in range(8):
        for s2 in range(8):
            m16[16 * s:16 * s + 16, 16 * s2:16 * s2 + 16] = np.eye(16, dtype=f)
    return {'w_e_gate': np.ascontiguousarray(inp['w_e_gate'][0]), 'w_e_up': np.ascontiguousarray(inp['w_e_up'][0]),
            'w_e_down': np.ascontiguousarray(inp['w_e_down'][0]), 'm16': m16}


D = 1024
NLAT = 8192
NCTX = 256
NEXT = NLAT + NCTX
NE2 = NEXT + NCTX
EPS = 1e-6
INCOLS = 4096


def build(stop_after=99, dbg=(), nblk_lim=17, skip_s5=False, mg_blocks=16, nexp=16):
    nc = bass.Bass("TRN2", target_bir_lowering=False)
    es = ExitStack()
    S = Sched(nc, es)
    dbg_out = {}

    def din(name, shape, dt=F32):
        return nc.dram_tensor(name, list(shape), dt, kind="ExternalInput").ap()

    def dscr(name, shape, dt):
        return nc.dram_tensor(name, list(shape), dt, kind="Internal").ap()

    def dout(name, shape, dt=F32):
        return nc.dram_tensor(name, list(shape), dt, kind="ExternalOutput").ap()

    def sb(name, shape, dt, stack=None):
        return (stack or es).enter_context(nc.sbuf_tensor(name, list(shape), dt))

    def ps(name, shape, dt, stack=None):
        return (stack or es).enter_context(nc.psum_tensor(name, list(shape), dt))

    xcat = din("xcat", [NEXT, D])
    cT = din("cT", [128, 8, 2])
    w_ada = din("w_ada", [D, 6 * D])
    b_ada = din("b_ada", [1, 6 * D])
    nmixT = din("nmixT", [128, 8])
    nffn = din("nffn", [1, D])
    w_in = din("w_in", [D, INCOLS])
    ident_in = din("ident", [128, 128])
    qkg = din("qkg", [128, 2])

    out = dout("out", [NLAT, D])

    uT_d = dscr("uT_d", [512, NE2], BF16)
    qT_d = dscr("qT_d", [512, NLAT], BF16)
    kT_d = dscr("kT_d", [512, NEXT], BF16)
    v_d = dscr("v_d", [NEXT, 8, 65], BF16)
    sgaT_d = dscr("sgaT_d", [D, NLAT], BF16)
    sgbT_d = dscr("sgbT_d", [D, NLAT], BF16)

    identf = sb("identf", [128, 128], F32)
    identb = sb("identb", [128, 128], BF16)
    mul1 = sb("mul1", [128, 8, 2], F32)
    add1 = sb("add1", [128, 8, 2], F32)
    bc_g1 = sb("bc_g1", [128, D], F32)
    bc_mul2 = sb("bc_mul2", [128, D], F32)
    bc_add2 = sb("bc_add2", [128, D], F32)
    bc_g2 = sb("bc_g2", [128, D], F32)

    epsc = sb("epsc", [128, 1], F32)
    S.op('dve', lambda e: e.memset(epsc[:], EPS), writes=['epsc'])
    S.dma('sp', identf[:], ident_in, writes=['identf'])
    S.dma('pool', identb[:], ident_in, writes=['identb'])

    with ExitStack() as p0:
        wada = sb("wada", [128, 8, 6 * D], BF16, p0)
        cs_f = sb("cs_f", [128, 8, 2], F32, p0)
        cs = sb("cs", [128, 8, 2], BF16, p0)
        brow = sb("brow", [2, 6 * D], F32, p0)
        modrow = sb("modrow", [2, 6 * D], F32, p0)
        sel = sb("sel", [2, 128], F32, p0)
        nmix = sb("nmix", [128, 8], F32, p0)
        colsb = sb("colsb", [128, 16, 2], F32, p0)
        pm = [ps("pm%d" % i, [2, 512], F32, p0) for i in range(2)]
        pcol = ps("pcol", [128, 16, 2], F32, p0)
        pbc = [ps("pbc%d" % i, [128, 512], F32, p0) for i in range(2)]

        S.dma('sp', cs_f[:], cT, writes=['cs_f'])
        S.dma('sp', brow[0:1, :], b_ada, writes=['brow0'])
        S.dma('sp', brow[1:2, :], b_ada, writes=['brow1'])
        S.dma('sp', nmix[:], nmixT, writes=['nmix'])
        S.dma('sp', bc_mul2[:], nffn.to_broadcast([128, D]), writes=['bc_mul2'])
        for dc in range(8):
            S.dma('pool', wada[:, dc, :].rearrange("p (a b) -> p a b", b=2048),
                  w_ada[dc * 128:(dc + 1) * 128, :].rearrange("p (a b) -> p a b", b=2048),
                  writes=[('wada', dc)])
        S.op('act', lambda e: e.activation(out=cs[:], in_=cs_f[:], func=AF.Silu), reads=['cs_f'], writes=['cs'])
        S.op('dve', lambda e: e.memset(sel[:], 0.0), writes=['sel'])
        S.op('dve', lambda e: e.memset(sel[0:1, :], 1.0), writes=['sel'])
        for cc in range(12):
            pt = pm[cc % 2]
            for dc in range(8):
                S.op('pe', lambda e: e.matmul(pt[:], lhsT=cs[:, dc, :], rhs=wada[:, dc, cc * 512:(cc + 1) * 512],
                                              start=(dc == 0), stop=(dc == 7)),
                     reads=['cs', ('wada', dc)], writes=[('pm', cc % 2)])
            S.op('dve', lambda e: e.tensor_tensor(out=modrow[:, cc * 512:(cc + 1) * 512], in0=pt[:],
                                                  in1=brow[:, cc * 512:(cc + 1) * 512], op=ALU.add),
                 reads=[('pm', cc % 2), 'brow0', 'brow1'], writes=[('modrow', cc)])
        for i in range(16):
            S.op('pe', lambda e: e.transpose(out=pcol[:, i, :], in_=modrow[0:2, i * 128:(i + 1) * 128],
                                             identity=identf[0:2, 0:2]),
                 reads=[('modrow', i // 4), 'identf'], writes=['pcol'])
        S.op('dve', lambda e: e.tensor_copy(out=colsb[:], in_=pcol[:]), reads=['pcol'], writes=['colsb'])
        S.op('dve', lambda e: e.tensor_copy(out=add1[:], in_=colsb[:, 0:8, :]), reads=['colsb'], writes=['add1'])
        S.op('dve', lambda e: e.tensor_scalar(out=colsb[:, 8:16, :], in0=colsb[:, 8:16, :], scalar1=1.0, scalar2=None,
                                              op0=ALU.add), reads=['colsb'], writes=['colsb'])
        S.op('dve', lambda e: e.tensor_tensor(out=mul1[:], in0=colsb[:, 8:16, :],
                                              in1=nmix[:].unsqueeze(2).to_broadcast([128, 8, 2]), op=ALU.mult),
             reads=['colsb', 'nmix'], writes=['mul1'])

        def bcast(dst, col0, mode, dkey):
            for h in range(2):
                pt = pbc[h]
                S.op('pe', lambda e: e.matmul(pt[:], lhsT=sel[:], rhs=modrow[:, col0 + h * 512: col0 + (h + 1) * 512],
                                              start=True, stop=True),
                     reads=['sel'] + [('modrow', c) for c in range(12)], writes=[('pbc', h)])
                dsl = dst[:, h * 512:(h + 1) * 512]
                if mode == 'copy':
                    S.op('dve', lambda e: e.tensor_copy(out=dsl, in_=pt[:]), reads=[('pbc', h)], writes=[dkey])
                else:
                    S.op('dve', lambda e: e.scalar_tensor_tensor(out=dsl, in0=pt[:], scalar=1.0, in1=dsl,
                                                                 op0=ALU.add, op1=ALU.mult),
                         reads=[('pbc', h), dkey], writes=[dkey])
        bcast(bc_g1, 2048, 'copy', 'bc_g1')
        bcast(bc_add2, 3072, 'copy', 'bc_add2')
        bcast(bc_mul2, 4096, 'mul1p', 'bc_mul2')
        bcast(bc_g2, 5120, 'copy', 'bc_g2')
        if 'mod' in dbg:
            dbg_out['mod'] = dout("dbg_mod", [2, 6 * D])
            S.dma('sp', dbg_out['mod'], modrow[:], reads=[('modrow', c) for c in range(12)])
            dbg_out['mul1'] = dout("dbg_mul1", [128, 8, 2])
            S.dma('sp', dbg_out['mul1'], mul1[:], reads=['mul1'])
            dbg_out['bcm2'] = dout("dbg_bcm2", [128, D])
            S.dma('sp', dbg_out['bcm2'], bc_mul2[:], reads=['bc_mul2'])
        S.barrier()

    if stop_after <= 0:
        return finish(nc, S, es, out, dbg_out)

    with ExitStack() as p1:
        win = sb("win", [128, 8, INCOLS], BF16, p1)
        xt = [sb("xt%d" % i, [128, D], F32, p1) for i in range(2)]
        junk = sb("junk", [128, D], BF16, p1)
        xn = [sb("xn%d" % i, [128, D], BF16, p1) for i in range(4)]
        ss = [sb("ss%d" % i, [128, 1], F32, p1) for i in range(4)]
        rstd = [sb("rstd%d" % i, [128, 1], F32, p1) for i in range(4)]
        hT = [sb("hT%d" % i, [128, 8, 512], BF16, p1) for i in range(2)]
        stg = [sb("stg%d" % i, [128, 512], BF16, p1) for i in range(4)]
        sq = [sb("sq%d" % i, [128, 512], BF16, p1) for i in range(2)]
        qk32 = [sb("qk32_%d" % i, [128, 512], F32, p1) for i in range(2)]
        rs = [sb("rs%d" % i, [128, 512], F32, p1) for i in range(2)]
        vst = [sb("vst%d" % i, [128, 8, 65], BF16, p1) for i in range(2)]
        bones = sb("bones", [128, 128], BF16, p1)
        gq = sb("gq", [128, 2], F32, p1)
        pT = [ps("pT%d" % i, [128, 8, 128], BF16, p1) for i in range(2)]
        pz = [ps("pz%d" % i, [128, 512], F32, p1) for i in range(4)]
        pms = [ps("pms%d" % i, [128, 512], F32, p1) for i in range(2)]

        for dc in range(8):
            S.dma('pool', win[:, dc, :].rearrange("p (a b) -> p a b", b=2048),
                  w_in[dc * 128:(dc + 1) * 128, :].rearrange("p (a b) -> p a b", b=2048),
                  writes=[('win', dc)])
        S.dma('sp', gq[:], qkg, writes=['gq'])
        S.op('dve', lambda e: e.tensor_scalar(out=gq[:, 0:1], in0=gq[:, 0:1], scalar1=0.125, scalar2=None, op0=ALU.mult), reads=['gq'], writes=['gq'])
        for i in range(2):
            S.op('dve', lambda e: e.memset(vst[i][:], 1.0), writes=[('vst', i)])
        S.op('dve', lambda e: e.memset(bones[:], 0.0), writes=['bones'])
        S.op('dve', lambda e: e.memset(bones[0:64, 0:64], 1.0 / 64), writes=['bones'])
        S.op('dve', lambda e: e.memset(bones[64:128, 64:128], 1.0 / 64), writes=['bones'])

        nblk = (NEXT + 511) // 512
        ctr = {'tile': 0, 'tpe': 0, 'stg': 0, 'z': 0, 'v': 0}

        def blk_info(b):
            if b == 0:
                return 0, 256
            return 256 + (b - 1) * 512, 512

        def head_pre(b):
            t0, nt = blk_info(b)
            for ti in range(nt // 128):
                i2 = ctr['tile'] % 2
                i4 = ctr['tile'] % 4
                ctr['tile'] += 1
                r0 = t0 + ti * 128
                S.dma('sp', xt[i2][:], xcat[r0:r0 + 128, :], writes=[('xt', i2)])
                S.op('act', lambda e: e.activation(out=junk[:], in_=xt[i2][:], func=AF.Square, accum_out=ss[i4][:]),
                     reads=[('xt', i2)], writes=['junk', ('ss', i4)])
                S.op('act', lambda e: e.activation(out=rstd[i4][:], in_=ss[i4][:], func=AF.Ln, scale=1.0 / D, bias=epsc[:, 0:1]),
                     reads=[('ss', i4), 'epsc'], writes=[('rstd', i4)])
                S.op('act', lambda e: e.activation(out=rstd[i4][:], in_=rstd[i4][:], func=AF.Exp, scale=-0.5),
                     reads=[('rstd', i4)], writes=[('rstd', i4)])
                S.op('act', lambda e: e.activation(out=xn[i4][:], in_=xt[i2][:], func=AF.Copy, scale=rstd[i4][:, 0:1]),
                     reads=[('xt', i2), ('rstd', i4)], writes=[('xn', i4)])

        def head_pe(b):
            t0, nt = blk_info(b)
            j = 1 if b == 0 else 0
            hb = hT[b % 2]
            hkey = ('hT', b % 2)
            for ti in range(nt // 128):
                i2 = ctr['tpe'] % 2
                i4 = ctr['tpe'] % 4
                ctr['tpe'] += 1
                for dc in range(8):
                    S.op('pe', lambda e: e.transpose(out=pT[i2][:, dc, :], in_=xn[i4][:, dc * 128:(dc + 1) * 128],
                                                     identity=identb[:]),
                         reads=[('xn', i4), 'identb'], writes=[('pT', i2)])
                for dc in range(8):
                    S.op('dve', lambda e: e.tensor_scalar(out=hb[:, dc, ti * 128:(ti + 1) * 128], in0=pT[i2][:, dc, :],
                                                          scalar1=mul1[:, dc, j:j + 1], scalar2=add1[:, dc, j:j + 1],
                                                          op0=ALU.mult, op1=ALU.add),
                         reads=[('pT', i2), 'mul1', 'add1'], writes=[hkey])

        pend_qk = []

        def chunk(b, cc):
            t0, nt = blk_info(b)
            is_ctx = (b == 0)
            hb = hT[b % 2]
            hkey = ('hT', b % 2)
            zi = ctr['z'] % 4
            ctr['z'] += 1
            pzt = pz[zi]
            for dc in range(8):
                S.op('pe', lambda e: e.matmul(pzt[:, 0:nt], lhsT=win[:, dc, cc * 128:(cc + 1) * 128], rhs=hb[:, dc, 0:nt],
                                              start=(dc == 0), stop=(dc == 7)),
                     reads=[('win', dc), hkey], writes=[('pz', zi)], sig=(dc == 7))
            si = ctr['stg'] % 4
            ctr['stg'] += 1
            st = stg[si]
            skey = ('stg', si)
            if not (4 <= cc < 12):
                while pend_qk:
                    pend_qk.pop(0)()
            if cc < 4:
                S.op('act', lambda e: e.activation(out=st[:, 0:nt], in_=pzt[:, 0:nt], func=AF.Copy),
                     reads=[('pz', zi)], writes=[skey])
                S.dma('sp', uT_d[cc * 128:(cc + 1) * 128, t0:t0 + nt], st[:, 0:nt], reads=[skey], writes=['uT_d'])
                if is_ctx:
                    S.dma('sp', uT_d[cc * 128:(cc + 1) * 128, NEXT:NEXT + nt], st[:, 0:nt], reads=[skey], writes=['uT_d'])
            elif cc < 12:
                isq = cc < 8
                qi = ctr['z'] % 2
                S.op('act', lambda e: e.activation(out=sq[qi][:, 0:nt], in_=pzt[:, 0:nt], func=AF.Square),
                     reads=[('pz', zi)], writes=[('sq', qi)])
                while pend_qk:
                    pend_qk.pop(0)()

                def tail(qi=qi, zi=zi, pzt=pzt, st=st, skey=skey, isq=isq, cc=cc, t0=t0, nt=nt):
                    S.op('pe', lambda e: e.matmul(pms[qi][:, 0:nt], lhsT=bones[:], rhs=sq[qi][:, 0:nt], start=True, stop=True),
                         reads=['bones', ('sq', qi)], writes=[('pms', qi)])
                    S.op('act', lambda e: e.activation(out=rs[qi][:, 0:nt], in_=pms[qi][:, 0:nt], func=AF.Ln, bias=epsc[:, 0:1]),
                         reads=[('pms', qi), 'epsc'], writes=[('rs', qi)])
                    S.op('act', lambda e: e.activation(out=rs[qi][:, 0:nt], in_=rs[qi][:, 0:nt], func=AF.Exp, scale=-0.5),
                         reads=[('rs', qi)], writes=[('rs', qi)])
                    gcol = gq[:, 0:1] if isq else gq[:, 1:2]
                    S.op('dve', lambda e: e.scalar_tensor_tensor(out=st[:, 0:nt], in0=pzt[:, 0:nt], scalar=gcol,
                                                                 in1=rs[qi][:, 0:nt], op0=ALU.mult, op1=ALU.mult),
                         reads=[('pz', zi), ('rs', qi), 'gq'], writes=[skey])
                    if isq:
                        S.dma('sp', qT_d[(cc - 4) * 128:(cc - 3) * 128, t0 - NCTX:t0 - NCTX + nt], st[:, 0:nt],
                              reads=[skey], writes=['qT_d'])
                    else:
                        S.dma('sp', kT_d[(cc - 8) * 128:(cc - 7) * 128, t0:t0 + nt], st[:, 0:nt], reads=[skey], writes=['kT_d'])
                pend_qk.append(tail)
            else:
                S.op('act', lambda e: e.activation(out=st[:, 0:nt], in_=pzt[:, 0:nt], func=AF.Sigmoid),
                     reads=[('pz', zi)], writes=[skey])
                if cc < 24:
                    dst = sgaT_d[(cc - 16) * 128:(cc - 15) * 128, t0 - NCTX:t0 - NCTX + nt]
                    dk = 'sgaT_d'
                else:
                    dst = sgbT_d[(cc - 24) * 128:(cc - 23) * 128, t0 - NCTX:t0 - NCTX + nt]
                    dk = 'sgbT_d'
                S.dma('sp', dst, st[:, 0:nt], reads=[skey], writes=[dk])

        def vpart(b):
            t0, nt = blk_info(b)
            hb = hT[b % 2]
            hkey = ('hT', b % 2)
            for ti in range(nt // 128):
                zi = ctr['z'] % 4
                ctr['z'] += 1
                pzt = pz[zi]
                for dc in range(8):
                    S.op('pe', lambda e: e.matmul(pzt[:], lhsT=hb[:, dc, ti * 128:(ti + 1) * 128], rhs=win[:, dc, 1536:2048],
                                                  start=(dc == 0), stop=(dc == 7)),
                         reads=[('win', dc), hkey], writes=[('pz', zi)], sig=(dc == 7))
                while pend_qk:
                    pend_qk.pop(0)()
                vi = ctr['v'] % 2
                ctr['v'] += 1
                S.op('act', lambda e: e.activation(out=vst[vi][:, :, 0:64], in_=pzt[:].rearrange("p (h d) -> p h d", d=64), func=AF.Copy),
                     reads=[('pz', zi)], writes=[('vst', vi)])
                S.dma('sp', v_d[t0 + ti * 128:t0 + (ti + 1) * 128], vst[vi][:], reads=[('vst', vi)], writes=['v_d'])

        head_pre(0)
        head_pe(0)
        for b in range(nblk_lim):
            if b == 0:
                ccs = list(range(0, 4)) + list(range(8, 12))
            else:
                ccs = list(range(0, 12)) + list(range(16, 32))
            n1, n2 = len(ccs) // 4, (3 * len(ccs)) // 4
            for cc in ccs[:n1]:
                chunk(b, cc)
            if b + 1 < nblk_lim:
                head_pre(b + 1)
            for cc in ccs[n1:n2]:
                chunk(b, cc)
            if b + 1 < nblk_lim:
                head_pe(b + 1)
            for cc in ccs[n2:]:
                chunk(b, cc)
            vpart(b)
        S.barrier()
    if 'p1' in dbg:
        for nm, t in (('uT', uT_d), ('qT', qT_d), ('kT', kT_d), ('sgaT', sgaT_d), ('sgbT', sgbT_d)):
            dbg_out[nm] = dout("dbg_" + nm, list(t.shape), BF16)
            for r0 in range(0, t.shape[0], 128):
                S.dma('sp', dbg_out[nm][r0:r0 + 128, :], t[r0:r0 + 128, :], reads=[])
    if stop_after <= 1:
        return finish(nc, S, es, out, dbg_out)
    yaT_d = dscr("yaT_d", [512, NLAT], BF16)
    r = 'ok' if skip_s5 else s5_phase(nc, S, es, sb, ps, din, dscr, dout, dbg, dbg_out, identf, identb, uT_d, yaT_d)
    if r == 'stop' or stop_after <= 2:
        return finish(nc, S, es, out, dbg_out)
    ybT_d = dscr("ybT_d", [512, NLAT], BF16)
    na_phase(nc, S, es, sb, ps, din, dscr, dout, dbg, dbg_out, identb, qT_d, kT_d, v_d, ybT_d)
    if stop_after <= 3:
        return finish(nc, S, es, out, dbg_out)
    h2_d = dscr("h2_d", [NLAT, D], BF16)
    aff_d = dscr("aff_d", [NLAT, 16], F32)
    affT_d = dscr("affT_d", [16, NLAT], F32)
    merge_phase(nc, S, es, sb, ps, din, dscr, dout, dbg, dbg_out, identf, identb, epsc, bc_g1, bc_mul2, bc_add2,
                xcat, out, yaT_d, ybT_d, sgaT_d, sgbT_d, h2_d, aff_d, affT_d, nblocks=mg_blocks)
    if stop_after <= 4:
        return finish(nc, S, es, out, dbg_out)
    moe_phase(nc, S, es, sb, ps, din, dscr, dout, dbg, dbg_out, identf, identb, bc_g2, out, h2_d, aff_d, affT_d, nexp=nexp)
    return finish(nc, S, es, out, dbg_out)


def finish(nc, S, es, out, dbg_out):
    S.barrier(['sp'])
    print("instructions", S.nins, "waits", S.nwaits)
    es.close()
    return nc, dbg_out


def host_inputs(inp, b):
    f = np.float32
    xcat = np.concatenate([inp['ctx'][b], inp['x'][b]], axis=0).astype(f)
    cT = np.stack([inp['c'][b].reshape(8, 128).T, inp['c_ctx'].reshape(8, 128).T], axis=-1).astype(f)
    qkg = np.stack([np.tile(inp['q_norm'][0], 2), np.tile(inp['k_norm'][0], 2)], axis=-1).astype(f)
    m = {
        'xcat': np.ascontiguousarray(xcat),
        'cT': np.ascontiguousarray(cT),
        'w_ada': np.ascontiguousarray(inp['w_ada'][0]),
        'b_ada': np.ascontiguousarray(inp['b_ada'][0][None, :]),
        'nmixT': np.ascontiguousarray(inp['norm_mix'][0].reshape(8, 128).T),
        'nffn': np.ascontiguousarray(inp['norm_ffn'][0][None, :]),
        'w_in': np.ascontiguousarray(inp['w_in'][0]),
        'ident': np.eye(128, dtype=f),
        'qkg': np.ascontiguousarray(qkg),
    }
    f32c = lambda a: np.ascontiguousarray(a.astype(f))
    lr = inp['ssm_lam_re'][0].reshape(2, 16, 2, 64)
    m['lamre_l'] = f32c(lr.transpose(2, 3, 0, 1).reshape(128, 32))
    li = inp['ssm_lam_im'][0].reshape(2, 16, 2, 64)
    m['lamim_l'] = f32c(li.transpose(2, 3, 0, 1).reshape(128, 32))
    ld = inp['ssm_log_dt'][0].reshape(2, 16, 2)
    m['logdt_l'] = f32c(np.broadcast_to(ld.transpose(2, 0, 1)[:, None, :, :], (2, 64, 2, 16)).reshape(128, 32))
    for nm, key in (('Bre_c', 'ssm_b_re'), ('Bim_c', 'ssm_b_im')):
        bb = inp[key][0].reshape(2, 16, 2, 64, 16)
        m[nm] = f32c(bb.transpose(2, 3, 0, 1, 4).reshape(128, 32, 16))
    for nm, key in (('Cre_c', 'ssm_c_re'), ('Cim_c', 'ssm_c_im')):
        cc = inp[key][0].reshape(2, 16, 2, 16, 64)
        m[nm] = f32c(cc.transpose(2, 4, 0, 1, 3).reshape(128, 32, 16))
    m['dskipT'] = f32c(inp['ssm_d'][0].reshape(4, 128).T)
    m['w_glu'] = f32c(inp['w_glu'][0])
    m['bgluT'] = f32c(inp['b_glu'][0].reshape(4, 128).T)
    m.update(na_host(inp))
    m['w_proj_a'] = f32c(inp['w_proj_a'][0]); m['w_proj_b'] = f32c(inp['w_proj_b'][0]); m['w_out'] = f32c(inp['w_out'][0])
    m.update(moe_host(inp))
    m['w_routerT'] = f32c(inp['w_router'][0].reshape(8, 128, 16).transpose(1, 0, 2))
    return m


_NC_CACHE = {}


def kernel(**inputs):
    inp = {k_: np.asarray(v_) for k_, v_ in inputs.items()}
    if 'nc' not in _NC_CACHE:
        nc, _ = build()
        _NC_CACHE['nc'] = nc
    nc = _NC_CACHE['nc']
    nb = inp['x'].shape[0]
    in_maps = [host_inputs(inp, b) for b in range(nb)]
    res = run_bass_kernel_spmd(nc, in_maps, core_ids=list(range(nb)))
    outs = [np.asarray(res.results[b]["out"], dtype=np.float32) for b in range(nb)]
    return np.stack(outs, axis=0)
```

```python
import numpy as np
from contextlib import ExitStack
import concourse.bass as bass
import concourse.mybir as mybir
from concourse.bass_utils import run_bass_kernel_spmd

F32 = mybir.dt.float32
BF16 = mybir.dt.bfloat16
I32 = mybir.dt.int32
U32 = mybir.dt.uint32
ALU = mybir.AluOpType
AF = mybir.ActivationFunctionType
AX = mybir.AxisListType
STRICT = False


class Sched:
    def __init__(self, nc, es, n_dma_sems=24):
        self.nc = nc
        self.e = {'pe': nc.tensor, 'act': nc.scalar, 'dve': nc.vector, 'pool': nc.gpsimd, 'sp': nc.sync}
        self.sems = {}
        self.cnt = {}
        for k in self.e:
            self.sems[k] = es.enter_context(nc.semaphore("sem_" + k))
            self.cnt[k] = 0
        self.nd = n_dma_sems
        self.qsems = {}
        for q in ('sp', 'pool', 'act'):
            n = n_dma_sems if q != 'act' else 16
            self.qsems[q] = []
            for i in range(n):
                sk = 'd_%s%d' % (q, i)
                self.sems[sk] = es.enter_context(nc.semaphore("dsem_%s%d" % (q, i)))
                self.cnt[sk] = 0
                self.qsems[q].append(sk)
        self.qrr = {'sp': 0, 'pool': 0, 'act': 0}
        self.drr = 0
        self.waited = {}
        self.lastw = {}
        self.readers = {}
        self.nwaits = 0
        self.nins = 0

    def _wait(self, eng, tok):
        sk, val, src = tok
        if self.waited.get((eng, sk), 0) >= val:
            return
        self.e[eng].wait_ge(self.sems[sk], val)
        self.waited[(eng, sk)] = val
        self.nwaits += 1

    def _deps(self, eng, reads, writes):
        for k in reads:
            t = self.lastw.get(k)
            if t is not None:
                self._wait(eng, t)
        for k in writes:
            t = self.lastw.get(k)
            if t is not None and (t[2] != eng or (STRICT and eng != 'pe')):
                self._wait(eng, t)
            for sk, (val, src) in self.readers.get(k, {}).items():
                if src != eng or (STRICT and eng != 'pe'):
                    self._wait(eng, (sk, val, src))

    def _record(self, tok, reads, writes):
        sk, val, src = tok
        for k in reads:
            self.readers.setdefault(k, {})[sk] = (val, src)
        for k in writes:
            self.lastw[k] = tok
            self.readers[k] = {}

    def op(self, eng, fn, reads=(), writes=(), sig=True):
        self._deps(eng, reads, writes)
        ins = fn(self.e[eng])
        if sig:
            self.cnt[eng] += 1
            ins.then_inc(self.sems[eng], 1)
            tok = (eng, self.cnt[eng], eng)
        else:
            tok = (eng, self.cnt[eng] + 1, eng)
        self._record(tok, reads, writes)
        self.nins += 1
        return tok

    def _dma_common(self, q, fn, reads, writes):
        self._deps(q, reads, writes)
        i = self.qrr[q]
        self.qrr[q] = (i + 1) % len(self.qsems[q])
        sk = self.qsems[q][i]
        if self.cnt[sk] > 0:
            self._wait(q, (sk, self.cnt[sk], None))
        ins = fn(self.e[q])
        self.cnt[sk] += 16
        ins.then_inc(self.sems[sk], 16)
        tok = (sk, self.cnt[sk], None)
        self._record(tok, reads, writes)
        self.nins += 1
        return tok

    def dma(self, q, out, in_, reads=(), writes=(), **kw):
        return self._dma_common(q, lambda e: e.dma_start(out=out, in_=in_, **kw), reads, writes)

    def custom_dma(self, q, fn, reads=(), writes=()):
        return self._dma_common(q, fn, reads, writes)

    def barrier(self, engines=None):
        for e in (engines or self.e):
            for sk, c in self.cnt.items():
                if c > 0:
                    self._wait(e, (sk, c, None))
        self.lastw = {}
        self.readers = {}

PI = float(np.pi)


def kn(t):
    n = t.name
    return n[2:] if n.startswith('s_') else n


def s5_phase(nc, S, es, sb, ps, din, dscr, dout, dbg, dbg_out, identf, identb, uT_d, yaT_d, nbanks=16):
    NEXT, NE2 = 8448, 8704
    lre_in = din("lamre_l", [128, 32]); lim_in = din("lamim_l", [128, 32]); ldt_in = din("logdt_l", [128, 32])
    Bre_in = din("Bre_c", [128, 32, 16]); Bim_in = din("Bim_c", [128, 32, 16])
    Cre_in = din("Cre_c", [128, 32, 16]); Cim_in = din("Cim_c", [128, 32, 16])
    dsk_in = din("dskipT", [128, 4]); wglu_in = din("w_glu", [512, 512]); bglu_in = din("bgluT", [128, 4])
    E_d = dscr("E_d", [128, 2, 4, 16, 2, 2, 128], BF16)
    G_d = dscr("G_d", [128, 64, 640], BF16)
    H_d = dscr("H_d", [2, 128, 16, 2, 544], BF16)

    with ExitStack() as s5:
        Kt_sb = sb("s_Kt_sb", [128, 128, 128], BF16, s5)
        aL = sb("s_aL", [128, 32], F32, s5); bL = sb("s_bL", [128, 32], F32, s5)
        diagD = sb("s_diagD", [128, 4, 128], BF16, s5)
        with ExitStack() as pa:
            def t32(name):
                return sb(name, [128, 32], F32, pa)
            lre, lim, ldt, dt, a, b = (t32(n) for n in ("lre", "lim", "ldt", "dt", "a_", "b_"))
            u1, u2, u3, u4, c16, s16, wre, wim = (t32(n) for n in ("u1", "u2", "u3", "u4", "c16", "s16", "wre", "wim"))
            Bcr = sb("s_Bcr", [128, 32, 16], F32, pa); Bci = sb("s_Bci", [128, 32, 16], F32, pa)
            Dr = sb("s_Dr", [128, 32, 32], F32, pa); Di = sb("s_Di", [128, 32, 32], F32, pa)
            Dr2 = sb("s_Dr2", [128, 32, 32], F32, pa); Di2 = sb("s_Di2", [128, 32, 32], F32, pa)
            Gr = sb("s_Gr", [128, 32, 32], F32, pa); Gi = sb("s_Gi", [128, 32, 32], F32, pa)
            T1 = sb("s_T1", [128, 32, 32], F32, pa); T2 = sb("s_T2", [128, 32, 32], F32, pa)
            T3 = sb("s_T3", [128, 32, 32], F32, pa); T4 = sb("s_T4", [128, 32, 32], F32, pa)
            P1 = sb("s_P1", [128, 32, 32], F32, pa); P2 = sb("s_P2", [128, 32, 32], F32, pa)
            P3 = sb("s_P3", [128, 32, 32], F32, pa); P4 = sb("s_P4", [128, 32, 32], F32, pa)
            Cbr = sb("s_Cbr", [128, 32, 32], BF16, pa); Cbni = sb("s_Cbni", [128, 32, 32], BF16, pa)
            Dbr = sb("s_Dbr", [128, 8, 160], BF16, pa); Dbi = sb("s_Dbi", [128, 8, 160], BF16, pa)
            Gbr = sb("s_Gbr", [128, 8, 160], BF16, pa); Gbni = sb("s_Gbni", [128, 8, 160], BF16, pa)
            Es = [sb("s_Es%d" % i, [128, 2, 4, 2, 2, 128], BF16, pa) for i in range(2)]
            m3 = sb("s_m3", [128, 1], F32, pa)
            dsk = sb("s_dsk", [128, 4], F32, pa)
            pk = [ps("s_pk%d" % i, [128, 128], F32, pa) for i in range(2)]
            pE = [ps("s_pE%d" % i, [128, 128], F32, pa) for i in range(4)]

            S.dma('sp', lre[:], lre_in, writes=['lre']); S.dma('sp', lim[:], lim_in, writes=['lim'])
            S.dma('sp', ldt[:], ldt_in, writes=['ldt']); S.dma('sp', dsk[:], dsk_in, writes=['dsk'])
            for X in (Dr, Di, Gr, Gi, Dr2, Di2):
                S.op('dve', lambda e: e.memset(X[:], 0.0), writes=[kn(X)])
            for X in (Dbr, Dbi, Gbr, Gbni):
                S.op('pool', lambda e: e.memset(X[:], 0.0), writes=[kn(X)])
            S.op('pool', lambda e: e.memset(Kt_sb[:], 0.0), writes=['Kt_sb'])
            for i in range(2):
                S.op('pool', lambda e: e.memset(Es[i][:], 0.0), writes=[('Es', i)])
            S.op('dve', lambda e: e.memset(m3[:], 1.0), writes=['m3'])
            S.op('dve', lambda e: e.memset(m3[64:96, :], 0.0), writes=['m3'])
            S.dma('sp', Bcr[:], Bre_in, writes=['Bcr']); S.dma('sp', Bci[:], Bim_in, writes=['Bci'])
            for h in range(2):
                S.dma('sp', Gr[64 * h:64 * h + 64, :, 16 * h:16 * h + 16], Cre_in[64 * h:64 * h + 64], reads=[], writes=['Gr'])
                S.dma('sp', Gi[64 * h:64 * h + 64, :, 16 * h:16 * h + 16], Cim_in[64 * h:64 * h + 64], reads=[], writes=['Gi'])
            for q in range(4):
                S.op('dve', lambda e: e.tensor_scalar(out=diagD[:, q, :], in0=identf[:], scalar1=dsk[:, q:q + 1], scalar2=None,
                                                      op0=ALU.mult), reads=['identf', 'dsk'], writes=['diagD'])
            V = lambda e: e
            def tt(out, in0, in1, op, r, w, eng='dve'):
                S.op(eng, lambda e: e.tensor_tensor(out=out, in0=in0, in1=in1, op=op), reads=r, writes=w)
            S.op('act', lambda e: e.activation(out=dt[:], in_=ldt[:], func=AF.Exp), reads=['ldt'], writes=['dt'])
            tt(u1[:], lre[:], dt[:], ALU.mult, ['lre', 'dt'], ['u1'])
            S.op('act', lambda e: e.activation(out=u1[:], in_=u1[:], func=AF.Exp), reads=['u1'], writes=['u1'])
            tt(u2[:], lim[:], dt[:], ALU.mult, ['lim', 'dt'], ['u2'])
            S.op('act', lambda e: e.activation(out=s16[:], in_=u2[:], func=AF.Sin, scale=1.0 / 16), reads=['u2'], writes=['s16'])
            S.op('act', lambda e: e.activation(out=u3[:], in_=u2[:], func=AF.Sin, scale=1.0 / 32), reads=['u2'], writes=['u3'])
            tt(u3[:], u3[:], u3[:], ALU.mult, ['u3'], ['u3'])
            S.op('dve', lambda e: e.tensor_scalar(out=c16[:], in0=u3[:], scalar1=-2.0, scalar2=1.0, op0=ALU.mult, op1=ALU.add),
                 reads=['u3'], writes=['c16'])

            def csquare(cr, ci, n):
                for _ in range(n):
                    tt(u3[:], cr[:], cr[:], ALU.mult, [kn(cr)], ['u3'])
                    tt(u4[:], ci[:], ci[:], ALU.mult, [kn(ci)], ['u4'])
                    tt(ci[:], cr[:], ci[:], ALU.mult, [kn(cr), kn(ci)], [kn(ci)])
                    S.op('dve', lambda e: e.tensor_scalar(out=ci[:], in0=ci[:], scalar1=2.0, scalar2=None, op0=ALU.mult),
                         reads=[kn(ci)], writes=[kn(ci)])
                    tt(cr[:], u3[:], u4[:], ALU.subtract, ['u3', 'u4'], [kn(cr)])
            csquare(c16, s16, 4)
            tt(a[:], u1[:], c16[:], ALU.mult, ['u1', 'c16'], ['a_'])
            tt(b[:], u1[:], s16[:], ALU.mult, ['u1', 's16'], ['b_'])
            S.op('dve', lambda e: e.tensor_copy(out=aL[:], in_=c16[:]), reads=['c16'], writes=['aL'])
            S.op('dve', lambda e: e.tensor_copy(out=bL[:], in_=s16[:]), reads=['s16'], writes=['bL'])
            csquare(aL, bL, 4)
            tt(u3[:], lre[:], dt[:], ALU.mult, ['lre', 'dt'], ['u3'])
            S.op('act', lambda e: e.activation(out=u3[:], in_=u3[:], func=AF.Exp, scale=16.0), reads=['u3'], writes=['u3'])
            tt(aL[:], aL[:], u3[:], ALU.mult, ['aL', 'u3'], ['aL'])
            tt(bL[:], bL[:], u3[:], ALU.mult, ['bL', 'u3'], ['bL'])
            S.op('dve', lambda e: e.tensor_scalar(out=u1[:], in0=a[:], scalar1=-1.0, scalar2=None, op0=ALU.add), reads=['a_'], writes=['u1'])
            tt(u2[:], lre[:], lre[:], ALU.mult, ['lre'], ['u2'])
            tt(u3[:], lim[:], lim[:], ALU.mult, ['lim'], ['u3'])
            tt(u2[:], u2[:], u3[:], ALU.add, ['u2', 'u3'], ['u2'])
            S.op('dve', lambda e: e.reciprocal(out=u2[:], in_=u2[:]), reads=['u2'], writes=['u2'])
            tt(u3[:], u1[:], lre[:], ALU.mult, ['u1', 'lre'], ['u3'])
            tt(u4[:], b[:], lim[:], ALU.mult, ['b_', 'lim'], ['u4'])
            tt(u3[:], u3[:], u4[:], ALU.add, ['u3', 'u4'], ['u3'])
            tt(wre[:], u3[:], u2[:], ALU.mult, ['u3', 'u2'], ['wre'])
            tt(u3[:], b[:], lre[:], ALU.mult, ['b_', 'lre'], ['u3'])
            tt(u4[:], u1[:], lim[:], ALU.mult, ['u1', 'lim'], ['u4'])
            tt(u3[:], u3[:], u4[:], ALU.subtract, ['u3', 'u4'], ['u3'])
            tt(wim[:], u3[:], u2[:], ALU.mult, ['u3', 'u2'], ['wim'])
            bc16 = lambda t: t[:].unsqueeze(2).to_broadcast([128, 32, 16])
            Tc = [T1[:, :, 0:16], T2[:, :, 0:16], T3[:, :, 0:16], T4[:, :, 0:16]]
            tt(Tc[0], Bcr[:], bc16(wre), ALU.mult, ['Bcr', 'wre'], ['T1'])
            tt(Tc[1], Bci[:], bc16(wim), ALU.mult, ['Bci', 'wim'], ['T2'])
            tt(Tc[2], Bci[:], bc16(wre), ALU.mult, ['Bci', 'wre'], ['T3'])
            tt(Tc[3], Bcr[:], bc16(wim), ALU.mult, ['Bcr', 'wim'], ['T4'])
            for h in range(2):
                sl = slice(64 * h, 64 * h + 64)
                tt(Dr[sl, :, 16 * h:16 * h + 16], T1[sl, :, 0:16], T2[sl, :, 0:16], ALU.subtract, ['T1', 'T2'], ['Dr'])
                tt(Di[sl, :, 16 * h:16 * h + 16], T3[sl, :, 0:16], T4[sl, :, 0:16], ALU.add, ['T3', 'T4'], ['Di'])
            S.op('dve', lambda e: e.tensor_copy(out=Cbr[:], in_=Gr[:]), reads=['Gr'], writes=['Cbr'])
            S.op('dve', lambda e: e.tensor_scalar(out=Cbni[:], in0=Gi[:], scalar1=-1.0, scalar2=None, op0=ALU.mult),
                 reads=['Gi'], writes=['Cbni'])
            bc32 = lambda t: t[:].unsqueeze(2).to_broadcast([128, 32, 32])

            def cmul(Xr, Xi, eng, Yr=None, Yi=None):
                Yr = Yr or Xr; Yi = Yi or Xi
                A1, A2, A3, A4 = (T1, T2, T3, T4) if eng == 'dve' else (P1, P2, P3, P4)
                tt(A1[:], Xr[:], bc32(a), ALU.mult, [kn(Xr), 'a_'], [kn(A1)], eng)
                tt(A2[:], Xi[:], bc32(b), ALU.mult, [kn(Xi), 'b_'], [kn(A2)], eng)
                tt(A3[:], Xi[:], bc32(a), ALU.mult, [kn(Xi), 'a_'], [kn(A3)], eng)
                tt(A4[:], Xr[:], bc32(b), ALU.mult, [kn(Xr), 'b_'], [kn(A4)], eng)
                tt(Yr[:], A1[:], A2[:], ALU.subtract, [kn(A1), kn(A2)], [kn(Yr)], eng)
                tt(Yi[:], A3[:], A4[:], ALU.add, [kn(A3), kn(A4)], [kn(Yi)], eng)

            def to5(dst, src, scale, key, eng='dve'):
                d4 = dst[:].rearrange("p g (s c) -> p g s c", c=32)
                s4 = src[:].rearrange("p (g s) c -> p g s c", s=4)
                S.op(eng, lambda e: e.tensor_scalar(out=d4[:, :, 0:3, :], in0=s4[:, :, 0:3, :], scalar1=scale, scalar2=None, op0=ALU.mult),
                     reads=[kn(src)], writes=[key])
                S.op(eng, lambda e: e.tensor_scalar(out=d4[:, :, 4, :], in0=s4[:, :, 3, :], scalar1=scale, scalar2=None, op0=ALU.mult),
                     reads=[kn(src)], writes=[key])

            Dpairs = [(Dr, Di), (Dr2, Di2)]
            for j in range(16):
                Dr, Di = Dpairs[j % 2]
                to5(Dbr, Dr, 1.0, 'Dbr'); to5(Dbi, Di, 1.0, 'Dbi')
                if j < 15:
                    cmul(Dr, Di, 'dve', Dpairs[(j + 1) % 2][0], Dpairs[(j + 1) % 2][1])
                for d in range(2):
                    for q in range(4):
                        idx = (d * 4 + q) * 16 + j
                        pkt = pk[idx % 2]; pkk = ('pk', idx % 2)
                        for pl in range(4):
                            pair = d * 16 + q * 4 + pl
                            if pl < 3:
                                osl = pkt[32 * pl:32 * pl + 32, 32 * pl:32 * pl + 32]; csl = slice(32 * pl, 32 * pl + 32)
                            else:
                                osl = pkt[64:128, 96:128]; csl = slice(96, 160)
                            S.op('pe', lambda e: e.matmul(osl, lhsT=Dbr[:, d * 4 + q, csl], rhs=Cbr[:, pair, :], start=True, stop=False),
                                 reads=['Dbr', 'Cbr'], writes=[pkk])
                            S.op('pe', lambda e: e.matmul(osl, lhsT=Dbi[:, d * 4 + q, csl], rhs=Cbni[:, pair, :], start=False, stop=True),
                                 reads=['Dbi', 'Cbni'], writes=[pkk])
                        for pl in range(4):
                            if pl < 3:
                                rs_, cs_ = slice(32 * pl, 32 * pl + 32), slice(32 * pl, 32 * pl + 32)
                            else:
                                rs_, cs_ = slice(64, 128), slice(96, 128)
                            S.op('act', lambda e: e.activation(out=Kt_sb[rs_, idx, cs_], in_=pkt[rs_, cs_], func=AF.Copy),
                                 reads=[pkk], writes=['Kt_sb'])
                Et = Es[j % 2]; ek = ('Es', j % 2)
                n_e = 0
                for reim, X in ((0, Dr), (1, Di)):
                    for d in range(2):
                        for q in range(4):
                            pet = pE[n_e % 4]; pek = ('pE', n_e % 4); n_e += 1
                            src = X[:, d * 16 + q * 4:d * 16 + q * 4 + 4, :].rearrange("p a b -> p (a b)")
                            S.op('pe', lambda e: e.transpose(out=pet[:], in_=src, identity=identf[:]), reads=[kn(X), 'identf'], writes=[pek])
                            S.op('act', lambda e: e.activation(out=Et[:, d, q, reim, 0, :], in_=pet[:], func=AF.Copy), reads=[pek], writes=[ek])
                            S.op('dve', lambda e: e.tensor_scalar(out=Et[64:128, d, q, reim, 1, :], in0=pet[64:128, :], scalar1=m3[64:128, 0:1],
                                                                  scalar2=None, op0=ALU.mult), reads=[pek, 'm3'], writes=[ek])
                for d in range(2):
                    S.dma('sp', E_d[:, d, :, j, :, :, :], Et[:, d], reads=[ek], writes=['E_d'])
                cmul(Gr, Gi, 'pool')
                to5(Gbr, Gr, 1.0, 'Gbr'); to5(Gbni, Gi, -1.0, 'Gbni')
                for d in range(2):
                    S.dma('sp', G_d[:, (d * 16 + j) * 2 + 0, :], Gbr[:, d * 4:d * 4 + 4, :].rearrange("p a b -> p (a b)"), reads=['Gbr'], writes=['G_d'])
                    S.dma('sp', G_d[:, (d * 16 + j) * 2 + 1, :], Gbni[:, d * 4:d * 4 + 4, :].rearrange("p a b -> p (a b)"), reads=['Gbni'], writes=['G_d'])
            S.barrier()
        if 's5prep' in dbg:
            dbg_out['Kt'] = dout("dbg_Kt", [128, 128, 128], BF16)
            S.dma('sp', dbg_out['Kt'], Kt_sb[:], reads=['Kt_sb'])
            dbg_out['aL'] = dout("dbg_aL", [128, 2, 32])
            S.dma('sp', dbg_out['aL'][:, 0, :], aL[:], reads=['aL']); S.dma('sp', dbg_out['aL'][:, 1, :], bL[:], reads=['bL'])
            dbg_out['G'] = dout("dbg_G", [128, 64, 640], BF16)
            S.dma('sp', dbg_out['G'], G_d, reads=['G_d'])
            dbg_out['E'] = dout("dbg_E", [128, 2, 4, 16, 2, 2, 128], BF16)
            for d in range(2):
                S.dma('sp', dbg_out['E'][:, d], E_d[:, d], reads=['E_d'])
            return 'stop'
        return s5_main(nc, S, es, sb, ps, din, dscr, dout, dbg, dbg_out, identf, identb, uT_d, yaT_d, nbanks,
                       s5, Kt_sb, aL, bL, diagD, E_d, G_d, H_d, wglu_in, bglu_in)


def s5_main(nc, S, es, sb, ps, din, dscr, dout, dbg, dbg_out, identf, identb, uT_d, yaT_d, nbanks,
            s5, Kt_sb, aL, bL, diagD, E_d, G_d, H_d, wglu_in, bglu_in):
    NCH = 528
    with ExitStack() as pb:
        uTb = [sb("s_uTb%d" % i, [128, 2048], BF16, pb) for i in range(3)]
        Esl = [sb("s_Esl%d" % i, [128, 16, 2, 2, 128], BF16, pb) for i in range(2)]
        Ssb = [[sb("s_Ssb%d_%d" % (d, i), [128, 16, 2, 128], F32, pb) for i in range(2)] for d in range(2)]
        hist = [sb("s_hist%d" % d, [128, 16, 2, 129], F32, pb) for d in range(2)]
        histb = [sb("s_histb%d" % d, [128, 16, 2, 128], BF16, pb) for d in range(2)]
        tP = [sb("s_tP%d" % d, [128, 16, 2], F32, pb) for d in range(2)]
        tQ = [sb("s_tQ%d" % d, [128, 16, 2], F32, pb) for d in range(2)]
        pS = [ps("s_pS%d" % i, [128, 4, 128], F32, pb) for i in range(4)]
        for d in range(2):
            S.op('dve' if d == 0 else 'pool', lambda e: e.memset(hist[d][:], 0.0), writes=[('hist', d)])
        fblocks = [(0, 128), (128, 128), (256, 128), (384, 128), (512, 16)]
        bblocks = [(512, 16), (384, 128), (256, 128), (128, 128), (0, 128)]
        esl_ctr = 0
        ps_ctr = 0
        for step in range(5):
            for d in range(2):
                k0, nk = (fblocks if d == 0 else bblocks)[step]
                Sb = Ssb[d][step % 2]; skey = ('Ssb', d, step % 2)
                base = 0 if d == 0 else 256
                for q in range(4):
                    et = Esl[esl_ctr % 2]; ekey = ('Esl', esl_ctr % 2); esl_ctr += 1
                    S.dma('sp', et[:], E_d[:, d, q], reads=['E_d'], writes=[ekey])
                    ut = uTb[(esl_ctr - 1) % 3]; ukey = ('uTb', (esl_ctr - 1) % 3)
                    S.dma('sp', ut[:, 0:16 * nk], uT_d[q * 128:(q + 1) * 128, base + 16 * k0: base + 16 * k0 + 16 * nk], reads=['uT_d'], writes=[ukey])
                    for pl in range(4):
                        pst = pS[ps_ctr % 4]; pskey = ('pS', ps_ctr % 4); ps_ctr += 1
                        if pl < 3:
                            rsl, alt = slice(32 * pl, 32 * pl + 32), 0
                        else:
                            rsl, alt = slice(64, 128), 1
                        for reim in range(2):
                            for i in range(16):
                                j = 15 - i if d == 0 else i
                                rhs = ut[rsl, i:i + 16 * (nk - 1) + 1:16]
                                S.op('pe', lambda e: e.matmul(pst[:, reim, 0:nk], lhsT=et[rsl, j, reim, alt, :], rhs=rhs,
                                                              start=(i == 0), stop=(i == 15)),
                                     reads=[ekey, ukey], writes=[pskey])
                        S.op('act', lambda e: e.activation(out=Sb[:, q * 4 + pl, :, 0:nk], in_=pst[:, 0:2, 0:nk], func=AF.Copy),
                             reads=[pskey], writes=[skey])
            def chain_ops(d, kk):
                k0, nk = (fblocks if d == 0 else bblocks)[step]
                Sb = Ssb[d][step % 2]; skey = ('Ssb', d, step % 2)
                hk = ('hist', d)
                A2 = aL[:, d * 16:(d + 1) * 16].unsqueeze(2).to_broadcast([128, 16, 2])
                B2 = bL[:, d * 16:(d + 1) * 16].unsqueeze(2).to_broadcast([128, 16, 2])
                H = hist[d]
                if d == 0:
                    cprev, ccur, scol = kk, kk + 1, kk
                else:
                    cprev, ccur, scol = nk - kk, nk - 1 - kk, nk - 1 - kk
                prev = H[:, :, :, cprev]
                P_, Q_ = tP[d], tQ[d]
                eng = 'dve'
                return [
                    lambda: S.op(eng, lambda e: e.tensor_tensor(out=P_[:], in0=prev, in1=A2, op=ALU.mult), reads=[hk, 'aL'], writes=[('tP', d)]),
                    lambda: S.op(eng, lambda e: e.tensor_tensor(out=Q_[:], in0=prev, in1=B2, op=ALU.mult), reads=[hk, 'bL'], writes=[('tQ', d)]),
                    lambda: S.op(eng, lambda e: e.tensor_tensor(out=P_[:], in0=P_[:], in1=Sb[:, :, :, scol], op=ALU.add),
                                 reads=[('tP', d), skey], writes=[('tP', d)]),
                    lambda: S.op(eng, lambda e: e.tensor_tensor(out=H[:, :, 0, ccur], in0=P_[:, :, 0], in1=Q_[:, :, 1], op=ALU.subtract),
                                 reads=[('tP', d), ('tQ', d)], writes=[hk]),
                    lambda: S.op(eng, lambda e: e.tensor_tensor(out=H[:, :, 1, ccur], in0=P_[:, :, 1], in1=Q_[:, :, 0], op=ALU.add),
                                 reads=[('tP', d), ('tQ', d)], writes=[hk]),
                ]
            nks = [(fblocks if d == 0 else bblocks)[step][1] for d in range(2)]
            for kk in range(max(nks)):
                lists = [chain_ops(d, kk) for d in range(2) if kk < nks[d]]
                for oi in range(5):
                    for l in lists:
                        l[oi]()
            for d in range(2):
                eng = 'dve' if d == 0 else 'pool'
                k0, nk = (fblocks if d == 0 else bblocks)[step]
                hk = ('hist', d)
                H = hist[d]
                if d == 0:
                    S.op(eng, lambda e: e.tensor_copy(out=histb[d][:, :, :, 0:nk], in_=H[:, :, :, 1:nk + 1]), reads=[hk], writes=[('histb', d)])
                    S.op(eng, lambda e: e.tensor_copy(out=H[:, :, :, 0], in_=H[:, :, :, nk]), reads=[hk], writes=[hk])
                else:
                    S.op(eng, lambda e: e.tensor_copy(out=histb[d][:, :, :, 0:nk], in_=H[:, :, :, 0:nk]), reads=[hk], writes=[('histb', d)])
                    S.op(eng, lambda e: e.tensor_copy(out=H[:, :, :, 128], in_=H[:, :, :, 0]), reads=[hk], writes=[hk])
                S.dma('pool', H_d[d, :, :, :, k0:k0 + nk], histb[d][:, :, :, 0:nk], reads=[('histb', d)], writes=['H_d'])
        S.barrier()
    if 's5lvl3' in dbg:
        dbg_out['H'] = dout("dbg_H", [2, 128, 16, 2, 544], BF16)
        for d in range(2):
            S.dma('sp', dbg_out['H'][d], H_d[d], reads=['H_d'])
        return 'stop'

    with ExitStack() as pc:
        Gsb = sb("s_Gsb", [128, 64, 640], BF16, pc)
        wglu = sb("s_wglu", [128, 4, 512], BF16, pc)
        bglu = sb("s_bglu", [128, 4], F32, pc)
        ub = [sb("s_ub%d" % i, [128, 4, 544], BF16, pc) for i in range(2)]
        Hf = [sb("s_Hf%d" % i, [128, 16, 2, 32], BF16, pc) for i in range(2)]
        Hb = [sb("s_Hb%d" % i, [128, 16, 2, 32], BF16, pc) for i in range(2)]
        aT = [sb("s_aT%d" % i, [128, 4, 512], BF16, pc) for i in range(2)]
        sig = [sb("s_sig%d" % i, [128, 512], BF16, pc) for i in range(2)]
        yo = [sb("s_yo%d" % i, [128, 512], BF16, pc) for i in range(4)]
        py = [ps("s_py%d" % i, [128, 512], F32, pc) for i in range(2)]
        py4 = [ps("s_py4_%d" % i, [128, 16, 32], F32, pc) for i in range(2)]
        y4 = [sb("s_y4_%d" % i, [128, 512], F32, pc) for i in range(2)]
        ysum = [sb("s_ysum%d" % i, [128, 512], F32, pc) for i in range(2)]
        zlhs = sb("s_zlhs", [128, 128], BF16, pc)
        S.op('dve', lambda e: e.memset(zlhs[:], 0.0), writes=['zlhs'])
        pg = [ps("s_pg%d" % i, [128, 512], F32, pc) for i in range(2)]
        for c in range(4):
            S.dma('sp', Gsb[:, c * 16:(c + 1) * 16, :], G_d[:, c * 16:(c + 1) * 16, :], reads=['G_d'], writes=[('Gsb', c)])
        gkeys = [('Gsb', c) for c in range(4)]
        for q in range(4):
            S.dma('pool', wglu[:, q, :], wglu_in[q * 128:(q + 1) * 128, :], writes=['wglu'])
        S.dma('sp', bglu[:], bglu_in, writes=['bglu'])
        yo_ctr = 0
        for n in range(nbanks):
            i2 = n % 2
            e0 = 256 + 512 * n
            S.dma('sp', ub[i2][:], uT_d[:, e0 - 16:e0 + 528].rearrange("(q p) t -> p q t", p=128), reads=['uT_d'], writes=[('ub', i2)])
            kf = 16 + 32 * n
            kb = 32 * n
            S.dma('sp', Hf[i2][:], H_d[0, :, :, :, kf - 1:kf + 31], reads=['H_d'], writes=[('Hf', i2)])
            S.dma('sp', Hb[i2][:], H_d[1, :, :, :, kb + 1:kb + 33], reads=['H_d'], writes=[('Hb', i2)])
            for q in range(4):
                pyt = py[q % 2]; pk_ = ('py', q % 2)
                p4 = py4[q % 2]; p4k = ('py4', q % 2)
                pyv = pyt[:].rearrange("p (k j) -> p k j", j=16)
                uc = ub[i2][:, q, 16:528]
                ucv = uc.rearrange("p (k j) -> p k j", j=16)
                rd = [('ub', i2), 'Kt_sb', 'diagD']
                S.op('pe', lambda e: e.matmul(pyt[:], lhsT=diagD[:, q, :], rhs=uc, start=True, stop=False), reads=rd, writes=[pk_])
                S.op('pe', lambda e: e.matmul(pyt[:], lhsT=Kt_sb[:, (0 * 4 + q) * 16 + 0, :], rhs=uc, start=False, stop=False), reads=rd, writes=[pk_])
                for tau in range(1, 16):
                    S.op('pe', lambda e: e.matmul(pyv[:, :, tau:16], lhsT=Kt_sb[:, (0 * 4 + q) * 16 + tau, :], rhs=ucv[:, :, 0:16 - tau],
                                                  start=False, stop=False), reads=rd, writes=[pk_])
                S.op('pe', lambda e: e.matmul(pyt[:], lhsT=Kt_sb[:, (1 * 4 + q) * 16 + 0, :], rhs=uc, start=False, stop=False), reads=rd, writes=[pk_])
                for tau in range(1, 16):
                    S.op('pe', lambda e: e.matmul(pyv[:, :, 0:16 - tau], lhsT=Kt_sb[:, (1 * 4 + q) * 16 + tau, :], rhs=ucv[:, :, tau:16],
                                                  start=False, stop=(tau == 15)), reads=rd, writes=[pk_])
                S.op('pe', lambda e: e.matmul(p4[:].rearrange("p j k -> p (j k)"), lhsT=zlhs[:], rhs=uc, start=True, stop=False),
                     reads=['zlhs', ('ub', i2)], writes=[p4k])
                for d in range(2):
                    Hx = Hf[i2] if d == 0 else Hb[i2]
                    hkey = ('Hf', i2) if d == 0 else ('Hb', i2)
                    for j in range(16):
                        m = j + 1 if d == 0 else 16 - j
                        for pl in range(4):
                            pair = q * 4 + pl
                            if pl < 3:
                                osl = p4[32 * pl:32 * pl + 32, j, :]; csl = slice(q * 160 + 32 * pl, q * 160 + 32 * pl + 32)
                            else:
                                osl = p4[64:128, j, :]; csl = slice(q * 160 + 96, q * 160 + 160)
                            for reim in range(2):
                                last = (d == 1 and j == 15 and pl == 3 and reim == 1)
                                S.op('pe', lambda e: e.matmul(osl, lhsT=Gsb[:, (d * 16 + m - 1) * 2 + reim, csl], rhs=Hx[:, pair, reim, :],
                                                              start=False, stop=last), reads=gkeys + [hkey], writes=[p4k])
                S.op('act', lambda e: e.activation(out=y4[q % 2][:].rearrange("p (k j) -> p j k", j=16), in_=p4[:], func=AF.Copy),
                     reads=[p4k], writes=[('y4', q % 2)])
                S.op('dve', lambda e: e.tensor_tensor(out=ysum[q % 2][:], in0=pyt[:], in1=y4[q % 2][:], op=ALU.add),
                     reads=[pk_, ('y4', q % 2)], writes=[('ysum', q % 2)])
                S.op('act', lambda e: e.activation(out=aT[i2][:, q, :], in_=ysum[q % 2][:], func=AF.Gelu), reads=[('ysum', q % 2)], writes=[('aT', i2, q)])
            for oc in range(4):
                pgt = pg[oc % 2]; pgk = ('pg', oc % 2)
                for q in range(4):
                    S.op('pe', lambda e: e.matmul(pgt[:], lhsT=wglu[:, q, oc * 128:(oc + 1) * 128], rhs=aT[i2][:, q, :],
                                                  start=(q == 0), stop=(q == 3)), reads=['wglu', ('aT', i2, q)], writes=[pgk])
                sg = sig[oc % 2]; sgk = ('sig', oc % 2)
                S.op('act', lambda e: e.activation(out=sg[:], in_=pgt[:], func=AF.Sigmoid, bias=bglu[:, oc:oc + 1]),
                     reads=[pgk, 'bglu'], writes=[sgk])
                yt = yo[yo_ctr % 4]; yk = ('yo', yo_ctr % 4); yo_ctr += 1
                S.op('dve', lambda e: e.tensor_tensor(out=yt[:], in0=aT[i2][:, oc, :], in1=sg[:], op=ALU.mult),
                     reads=[('aT', i2, oc), sgk], writes=[yk])
                S.dma('pool', yaT_d[oc * 128:(oc + 1) * 128, 512 * n:512 * n + 512], yt[:], reads=[yk], writes=['yaT_d'])
        S.barrier()
    if 's5' in dbg:
        dbg_out['yaT'] = dout("dbg_yaT", [512, 8192], BF16)
        for r0 in range(0, 512, 128):
            S.dma('sp', dbg_out['yaT'][r0:r0 + 128, :], yaT_d[r0:r0 + 128, :], reads=['yaT_d'])
    return 'ok'

NEG = -30000.0


def na_phase(nc, S, es, sb, ps, din, dscr, dout, dbg, dbg_out, identb, qT_d, kT_d, v_d, ybT_d, nblocks=16):
    rpbT_in = din("rpbT", [128, 8 * 14 * 64])
    maskT_in = din("maskT", [128, 64])
    with ExitStack() as pa:
        Tb = sb("n_Tb", [128, 8, 14, 64], F32, pa)
        mk = sb("n_mk", [128, 64], F32, pa)
        kctx = sb("n_kctx", [128, 4, 256], BF16, pa)
        vctx = sb("n_vctx", [128, 2, 8, 65], BF16, pa)
        qb = [sb("n_qb%d" % i, [128, 4, 512], BF16, pa) for i in range(2)]
        kw = [sb("n_kw%d" % i, [128, 4, 960], BF16, pa) for i in range(2)]
        vw = [sb("n_vw%d" % i, [128, 15, 8, 65], BF16, pa) for i in range(2)]
        sS = [sb("n_sS%d" % i, [128, 4, 64], F32, pa) for i in range(3)]
        P = [sb("n_Pn%d" % i, [128, 6, 64], BF16, pa) for i in range(4)]
        rc = [sb("n_rc%d" % i, [128, 8], F32, pa) for i in range(2)]
        ybt = [sb("n_ybt%d" % i, [128, 8, 64], BF16, pa) for i in range(2)]
        ybT = [sb("n_ybT%d" % i, [128, 4, 512], BF16, pa) for i in range(2)]
        pS = [ps("n_pS%d" % i, [128, 6, 64], F32, pa) for i in range(3)]
        pO = [ps("n_pO%d" % i, [128, 4, 65], F32, pa) for i in range(4)]
        pT = ps("n_pTn", [128, 4, 128], BF16, pa)

        S.dma('sp', Tb[:].rearrange("p h a q -> p (h a q)"), rpbT_in, writes=['Tb'])
        S.dma('sp', mk[:], maskT_in, writes=['mk'])
        S.dma('sp', kctx[:], kT_d[:, 0:256].rearrange("(c p) t -> p c t", p=128), reads=['kT_d'], writes=['kctx'])
        S.dma('sp', vctx[:], v_d[0:256].rearrange("(m p) h d -> p m h d", p=128), reads=['v_d'], writes=['vctx'])
        S.op('dve', lambda e: e.tensor_tensor(out=Tb[:].rearrange("p h a q -> p (h a) q"), in0=Tb[:].rearrange("p h a q -> p (h a) q"),
                                              in1=mk[:].unsqueeze(1).to_broadcast([128, 112, 64]), op=ALU.add),
             reads=['Tb', 'mk'], writes=['Tb'])
        item = 0
        pend = []
        for b in range(nblocks):
            i2 = b % 2
            r0 = 8 * b
            lo = min(max(r0 - 4, 0), 120)
            hi = min(max(r0 + 7 - 4, 0), 120) + 8
            nrows = hi - lo
            S.dma('sp', qb[i2][:], qT_d[:, 512 * b:512 * b + 512].rearrange("(c p) t -> p c t", p=128), reads=['qT_d'], writes=[('qb', i2)])
            S.dma('sp', kw[i2][:, :, 0:64 * nrows], kT_d[:, 256 + 64 * lo:256 + 64 * hi].rearrange("(c p) t -> p c t", p=128),
                  reads=['kT_d'], writes=[('kw', i2)])
            for s in range(nrows - 1):
                S.dma('sp', vw[i2][:, s], v_d[256 + 64 * (lo + s):256 + 64 * (lo + s) + 128], reads=['v_d'], writes=[('vw', i2)])
            for rr in range(8):
                r = r0 + rr
                krs = min(max(r - 4, 0), 120)
                a0 = krs - r + 7
                half = 64 * (r % 2)
                pp = (r // 2) % 2
                for h in range(8):
                    hc, hb = h // 2, 64 * (h % 2)
                    it = item % 3
                    ip = item % 4
                    item += 1
                    pst = pS[it]; psk = ('pS', it)
                    for c in range(4):
                        kc0 = 64 * (krs + 2 * c - lo)
                        S.op('pe', lambda e: e.matmul(pst[:, c, :], lhsT=kw[i2][hb:hb + 64, hc, kc0:kc0 + 128],
                                                      rhs=qb[i2][hb:hb + 64, hc, 64 * rr:64 * rr + 64], start=True, stop=True),
                             reads=[('kw', i2), ('qb', i2)], writes=[psk])
                    for c in range(2):
                        S.op('pe', lambda e: e.matmul(pst[:, 4 + c, :], lhsT=kctx[hb:hb + 64, hc, 128 * c:128 * c + 128],
                                                      rhs=qb[i2][hb:hb + 64, hc, 64 * rr:64 * rr + 64], start=True, stop=True),
                             reads=['kctx', ('qb', i2)], writes=[psk])
                    S.op('dve', lambda e: e.tensor_tensor(out=sS[it][:], in0=pst[:, 0:4, :], in1=Tb[:, h, a0:a0 + 7:2, :], op=ALU.add),
                         reads=[psk, 'Tb'], writes=[('sS', it)])
                    S.op('act', lambda e: e.activation(out=P[ip][:, 0:4, :], in_=sS[it][:], func=AF.Exp), reads=[('sS', it)], writes=[('P', ip)])
                    S.op('act', lambda e: e.activation(out=P[ip][:, 4:6, :], in_=pst[:, 4:6, :], func=AF.Exp), reads=[psk], writes=[('P', ip)])
                    if len(pend) == 2:
                        pend.pop(0)()
                    def mk_pv(it=ip, i2=i2, krs=krs, lo=lo, h=h, half=half, pp=pp, r=r, rr=rr, b=b):
                        def pv():
                            pot = pO[pp * 2 + h // 4]; pok = ('pO', pp * 2 + h // 4)
                            for c in range(6):
                                if c < 4:
                                    rhs = vw[i2][:, krs + 2 * c - lo, h, :]
                                    rk = ('vw', i2)
                                else:
                                    rhs = vctx[:, c - 4, h, :]
                                    rk = 'vctx'
                                S.op('pe', lambda e: e.matmul(pot[half:half + 64, h % 4, :], lhsT=P[it][:, c, :], rhs=rhs,
                                                              start=(c == 0), stop=(c == 5)), reads=[('P', it), rk], writes=[pok])
                            if h % 4 == 3:
                                hh = h // 4
                                yk = ('ybt', pp)
                                S.op('dve', lambda e: e.reciprocal(out=rc[pp][half:half + 64, 4 * hh:4 * hh + 4], in_=pot[half:half + 64, :, 64]),
                                     reads=[pok], writes=[('rc', pp)])
                                S.op('dve', lambda e: e.tensor_tensor(out=ybt[pp][half:half + 64, 4 * hh:4 * hh + 4, :], in0=pot[half:half + 64, :, 0:64],
                                                                      in1=rc[pp][half:half + 64, 4 * hh:4 * hh + 4].unsqueeze(2).to_broadcast([64, 4, 64]),
                                                                      op=ALU.mult), reads=[pok, ('rc', pp)], writes=[yk])
                            if h == 7 and r % 2 == 1:
                                tk = ('ybT', b % 2)
                                for c4 in range(4):
                                    S.op('pe', lambda e: e.transpose(out=pT[:, c4, :], in_=ybt[pp][:, 2 * c4:2 * c4 + 2, :].rearrange("p a d -> p (a d)"),
                                                                     identity=identb[:]), reads=[('ybt', pp), 'identb'], writes=['pTn'])
                                S.op('act', lambda e: e.activation(out=ybT[b % 2][:, :, 64 * (rr - 1):64 * (rr - 1) + 128], in_=pT[:], func=AF.Copy),
                                     reads=['pTn'], writes=[tk])
                                if rr == 7:
                                    S.dma('act', ybT_d[:, 512 * b:512 * b + 512].rearrange("(c p) t -> p c t", p=128), ybT[b % 2][:],
                                          reads=[tk], writes=['ybT_d'])
                        return pv
                    pend.append(mk_pv())
        for p_ in pend:
            p_()
        S.barrier()
    if 'na' in dbg:
        dbg_out['ybT'] = dout("dbg_ybT", [512, 8192], BF16)
        for r0 in range(0, 512, 128):
            S.dma('sp', dbg_out['ybT'][r0:r0 + 128, :], ybT_d[r0:r0 + 128, :], reads=['ybT_d'])


def na_host(inp):
    rpb = inp['na_rpb'][0].astype(np.float32)
    i = np.arange(2)[:, None, None, None]
    kc = np.arange(64)[None, :, None, None]
    a = np.arange(14)[None, None, :, None]
    qc = np.arange(64)[None, None, None, :]
    dcol = np.clip(kc - qc + 15, 0, 30)
    drow = a + i
    T = rpb[:, drow, dcol]
    T = np.broadcast_to(T, (8, 2, 64, 14, 64)).transpose(1, 2, 0, 3, 4).reshape(128, 8 * 14 * 64)
    q = np.arange(64)
    qcs = np.clip(q - 8, 0, 48)
    k = np.arange(64)[:, None]
    inw = (k >= qcs[None, :]) & (k < qcs[None, :] + 16)
    m = np.where(inw, 0.0, NEG).astype(np.float32)
    m = np.concatenate([m, m], axis=0)
    return {'rpbT': np.ascontiguousarray(T.astype(np.float32)), 'maskT': np.ascontiguousarray(m)}

D = 1024


def merge_phase(nc, S, es, sb, ps, din, dscr, dout, dbg, dbg_out, identf, identb, epsc, bc_g1, bc_mul2, bc_add2,
                xcat, out, yaT_d, ybT_d, sgaT_d, sgbT_d, h2_d, aff_d, affT_d, nblocks=16):
    wpa_in = din("w_proj_a", [512, D]); wpb_in = din("w_proj_b", [512, D]); wout_in = din("w_out", [D, D])
    wr_in = din("w_routerT", [128, 8, 16])
    with ExitStack() as pa:
        wpa = sb("g_wpa", [128, 4, D], BF16, pa); wpb = sb("g_wpb", [128, 4, D], BF16, pa)
        wout = sb("g_wout", [128, 8, D], BF16, pa)
        wr = sb("g_wr", [128, 8, 16], BF16, pa)
        ya = [sb("g_mya%d" % i, [128, 4, 512], BF16, pa) for i in range(2)]
        yb = [sb("g_myb%d" % i, [128, 4, 512], BF16, pa) for i in range(2)]
        sga = [sb("g_msga%d" % i, [128, 8, 512], BF16, pa) for i in range(2)]
        sgb = [sb("g_msgb%d" % i, [128, 8, 512], BF16, pa) for i in range(2)]
        mT = [sb("g_mT%d" % i, [128, 8, 512], BF16, pa) for i in range(2)]
        t1 = [sb("g_mt1_%d" % i, [128, 512], F32, pa) for i in range(2)]
        t2 = [sb("g_mt2_%d" % i, [128, 512], F32, pa) for i in range(2)]
        xt = [sb("g_mxt%d" % i, [128, D], F32, pa) for i in range(2)]
        x1 = [sb("g_mx1_%d" % i, [128, D], F32, pa) for i in range(3)]
        h2 = [sb("g_mh2_%d" % i, [128, D], F32, pa) for i in range(3)]
        h2b = [sb("g_mh2b_%d" % i, [128, D], BF16, pa) for i in range(3)]
        h2T = [sb("g_mh2T_%d" % i, [128, 8, 128], BF16, pa) for i in range(3)]
        junk = sb("g_mjunk", [128, D], BF16, pa)
        st = [sb("g_mst%d" % i, [128, 8], F32, pa) for i in range(3)]
        lg = [sb("g_mlg%d" % i, [128, 16], F32, pa) for i in range(3)]
        af = [sb("g_maf%d" % i, [128, 16], F32, pa) for i in range(3)]
        A16 = sb("g_A16", [16, 8192], F32, pa)
        pab = [ps("g_mpab%d" % i, [128, 512], F32, pa) for i in range(4)]
        pyo = ps("g_mpyo", [128, D], F32, pa)
        phT = ps("g_mphT", [128, 8, 128], BF16, pa)
        pmisc = ps("g_mpmisc", [128, 256], F32, pa)
        plg = pmisc[:, 0:16]
        paT = pmisc[0:16, 128:256]

        for q in range(4):
            S.dma('pool', wpa[:, q, :], wpa_in[q * 128:(q + 1) * 128, :], writes=['wpa'])
            S.dma('pool', wpb[:, q, :], wpb_in[q * 128:(q + 1) * 128, :], writes=['wpb'])
        for dc in range(8):
            S.dma('pool', wout[:, dc, :], wout_in[dc * 128:(dc + 1) * 128, :], writes=['wout'])
        S.dma('pool', wr[:], wr_in, writes=['wr'])
        tile_ctr = 0
        pend = []
        for b in range(nblocks):
            i2 = b % 2
            cs = slice(512 * b, 512 * b + 512)
            S.dma('sp', ya[i2][:], yaT_d[:, cs].rearrange("(c p) t -> p c t", p=128), reads=['yaT_d'], writes=[('ya', i2)])
            S.dma('sp', yb[i2][:], ybT_d[:, cs].rearrange("(c p) t -> p c t", p=128), reads=['ybT_d'], writes=[('yb', i2)])
            S.dma('sp', sga[i2][:], sgaT_d[:, cs].rearrange("(c p) t -> p c t", p=128), reads=['sgaT_d'], writes=[('sga', i2)])
            S.dma('sp', sgb[i2][:], sgbT_d[:, cs].rearrange("(c p) t -> p c t", p=128), reads=['sgbT_d'], writes=[('sgb', i2)])
            for dc in range(8):
                j2 = dc % 2
                pA_, pB_ = pab[2 * j2], pab[2 * j2 + 1]
                for q in range(4):
                    S.op('pe', lambda e: e.matmul(pA_[:], lhsT=wpa[:, q, dc * 128:(dc + 1) * 128], rhs=ya[i2][:, q, :],
                                                  start=(q == 0), stop=(q == 3)), reads=['wpa', ('ya', i2)], writes=[('pA', j2)])
                for q in range(4):
                    S.op('pe', lambda e: e.matmul(pB_[:], lhsT=wpb[:, q, dc * 128:(dc + 1) * 128], rhs=yb[i2][:, q, :],
                                                  start=(q == 0), stop=(q == 3)), reads=['wpb', ('yb', i2)], writes=[('pB', j2)])
                S.op('dve', lambda e: e.tensor_tensor(out=t1[j2][:], in0=pA_[:], in1=sga[i2][:, dc, :], op=ALU.mult),
                     reads=[('pA', j2), ('sga', i2)], writes=[('t1', j2)])
                S.op('dve', lambda e: e.tensor_tensor(out=t2[j2][:], in0=pB_[:], in1=sgb[i2][:, dc, :], op=ALU.mult),
                     reads=[('pB', j2), ('sgb', i2)], writes=[('t2', j2)])
                S.op('pool', lambda e: e.tensor_tensor(out=mT[i2][:, dc, :], in0=t1[j2][:], in1=t2[j2][:], op=ALU.add),
                     reads=[('t1', j2), ('t2', j2)], writes=[('mT', i2)])
            for ti in range(4):
                k2 = tile_ctr % 3
                kx = tile_ctr % 2
                tile_ctr += 1
                tok0 = 512 * b + 128 * ti
                S.dma('sp', xt[kx][:], xcat[256 + tok0:256 + tok0 + 128, :], writes=[('xt', kx)])
                for hf in range(2):
                    for dc in range(8):
                        S.op('pe', lambda e: e.matmul(pyo[:, hf * 512:(hf + 1) * 512], lhsT=mT[i2][:, dc, ti * 128:(ti + 1) * 128],
                                                      rhs=wout[:, dc, hf * 512:(hf + 1) * 512], start=(dc == 0), stop=(dc == 7)),
                             reads=[('mT', i2), 'wout'], writes=['pyo'])
                S.op('dve', lambda e: e.tensor_tensor(out=x1[k2][:], in0=pyo[:], in1=bc_g1[:], op=ALU.mult),
                     reads=['pyo', 'bc_g1'], writes=[('x1', k2)])
                S.op('dve', lambda e: e.tensor_tensor(out=x1[k2][:], in0=x1[k2][:], in1=xt[kx][:], op=ALU.add),
                     reads=[('x1', k2), ('xt', kx)], writes=[('x1', k2)])
                S.dma('pool', out[tok0:tok0 + 128, :], x1[k2][:], reads=[('x1', k2)], writes=['out'])
                s_ = st[k2]; sk = ('st', k2)
                S.op('act', lambda e: e.activation(out=junk[:], in_=x1[k2][:], func=AF.Square, accum_out=s_[:, 0:1]),
                     reads=[('x1', k2)], writes=['mjunk', sk])
                S.op('act', lambda e: e.activation(out=s_[:, 6:7], in_=s_[:, 0:1], func=AF.Ln, scale=1.0 / D, bias=epsc[:, 0:1]),
                     reads=[sk, 'epsc'], writes=[sk])
                S.op('act', lambda e: e.activation(out=s_[:, 1:2], in_=s_[:, 6:7], func=AF.Exp, scale=-0.5), reads=[sk], writes=[sk])
                S.op('dve', lambda e: e.scalar_tensor_tensor(out=h2[k2][:], in0=x1[k2][:], scalar=s_[:, 1:2], in1=bc_mul2[:],
                                                             op0=ALU.mult, op1=ALU.mult), reads=[('x1', k2), sk, 'bc_mul2'], writes=[('h2', k2)])
                S.op('dve', lambda e: e.tensor_tensor(out=h2[k2][:], in0=h2[k2][:], in1=bc_add2[:], op=ALU.add),
                     reads=[('h2', k2), 'bc_add2'], writes=[('h2', k2)])
                S.op('act', lambda e: e.activation(out=h2b[k2][:], in_=h2[k2][:], func=AF.Copy), reads=[('h2', k2)], writes=[('h2b', k2)])
                S.dma('act', h2_d[tok0:tok0 + 128, :], h2b[k2][:], reads=[('h2b', k2)], writes=['h2_d'])
                def mk_tail(k2=k2, tok0=tok0, s_=s_, sk=sk):
                    def tail():
                        for dc in range(8):
                            S.op('pe', lambda e: e.transpose(out=phT[:, dc, :], in_=h2b[k2][:, dc * 128:(dc + 1) * 128], identity=identb[:]),
                                 reads=[('h2b', k2), 'identb'], writes=['phT'])
                        S.op('act', lambda e: e.activation(out=h2T[k2][:], in_=phT[:], func=AF.Copy), reads=['phT'], writes=[('h2T', k2)])
                        for dc in range(8):
                            S.op('pe', lambda e: e.matmul(plg, lhsT=h2T[k2][:, dc, :], rhs=wr[:, dc, :], start=(dc == 0), stop=(dc == 7)),
                                 reads=[('h2T', k2), 'wr'], writes=['plg'])
                        S.op('dve', lambda e: e.tensor_copy(out=lg[k2][:], in_=plg), reads=['plg'], writes=[('lg', k2)])
                        S.op('dve', lambda e: e.reduce_max(out=s_[:, 2:3], in_=lg[k2][:], axis=AX.X), reads=[('lg', k2)], writes=[sk])
                        S.op('dve', lambda e: e.tensor_scalar(out=s_[:, 3:4], in0=s_[:, 2:3], scalar1=-1.0, scalar2=None, op0=ALU.mult),
                             reads=[sk], writes=[sk])
                        S.op('act', lambda e: e.activation(out=af[k2][:], in_=lg[k2][:], func=AF.Exp, bias=s_[:, 3:4], accum_out=s_[:, 4:5]),
                             reads=[('lg', k2), sk], writes=[('af', k2), sk])
                        S.op('dve', lambda e: e.reciprocal(out=s_[:, 5:6], in_=s_[:, 4:5]), reads=[sk], writes=[sk])
                        S.op('dve', lambda e: e.tensor_scalar(out=af[k2][:], in0=af[k2][:], scalar1=s_[:, 5:6], scalar2=None, op0=ALU.mult),
                             reads=[('af', k2), sk], writes=[('af', k2)])
                        S.dma('pool', aff_d[tok0:tok0 + 128, :], af[k2][:], reads=[('af', k2)], writes=['aff_d'])
                        S.op('pe', lambda e: e.transpose(out=paT, in_=af[k2][:], identity=identf[:]), reads=[('af', k2), 'identf'], writes=['paT'])
                        S.op('act', lambda e: e.activation(out=A16[:, tok0:tok0 + 128], in_=paT, func=AF.Copy), reads=['paT'], writes=['A16'])
                    return tail
                if len(pend) == 2:
                    pend.pop(0)()
                pend.append(mk_tail())
        for t_ in pend:
            t_()
        S.dma('sp', affT_d, A16[:], reads=['A16'], writes=['affT_d'])
        S.barrier()
        if 'mg2' in dbg:
            for nm, t, shp, dt in (('bcg1', bc_g1, [128, D], F32), ('mT0', mT[(nblocks - 1) % 2], [128, 8, 512], BF16), ('t1', t1[1], [128, 512], F32),
                                   ('sga0', sga[(nblocks - 1) % 2], [128, 8, 512], BF16), ('ya0', ya[(nblocks - 1) % 2], [128, 4, 512], BF16),
                                   ('yb0', yb[(nblocks - 1) % 2], [128, 4, 512], BF16), ('wpa', wpa, [128, 4, D], BF16), ('x1t', x1[1], [128, D], F32),
                                   ('xt', xt[1], [128, D], F32)):
                dbg_out[nm] = dout("dbg_" + nm, shp, dt)
                S.dma('sp', dbg_out[nm], t[:], reads=[])
    if 'mg' in dbg:
        dbg_out['aff'] = dout("dbg_aff", [8192, 16])
        S.dma('sp', dbg_out['aff'], aff_d, reads=['aff_d'])
        dbg_out['affT'] = dout("dbg_affT", [16, 8192])
        S.dma('sp', dbg_out['affT'], affT_d, reads=['affT_d'])
        dbg_out['h2'] = dout("dbg_h2", [8192, D], BF16)
        for r0 in range(0, 8192, 1024):
            S.dma('sp', dbg_out['h2'][r0:r0 + 1024, :], h2_d[r0:r0 + 1024, :], reads=['h2_d'])

D = 1024
CAP = 1024
NT = 8192


def moe_phase(nc, S, es, sb, ps, din, dscr, dout, dbg, dbg_out, identf, identb, bc_g2, out, h2_d, aff_d, affT_d, nexp=16):
    wg_in = din("w_e_gate", [16, D, D]); wu_in = din("w_e_up", [16, D, D]); wd_in = din("w_e_down", [16, D, D])
    m16_in = din("m16", [128, 128])
    with ExitStack() as pm:
        cum_d = dscr("cum_d", [16, NT], F32)
        slotv = sb("e_slotv", [128, 8], F32, pm)
        thr_dbg = sb("e_thr", [128, 1], F32, pm)
        S.op('pool', lambda e: e.iota(slotv[:], pattern=[[128, 8]], base=0, channel_multiplier=1, allow_small_or_imprecise_dtypes=True),
             writes=['slotv'])
        with ExitStack() as pa:
            A128 = sb("e_A128", [128, 1024], F32, pa)
            A16 = sb("e_A16", [16, NT], F32, pa)
            C16 = sb("e_C16", [16, NT], F32, pa)
            M16 = sb("e_M16", [128, 128], F32, pa)
            junk = sb("e_junk", [128, 1024], BF16, pa)
            lo = sb("e_lo", [128, 1], F32, pa); mid = sb("e_mid", [128, 1], F32, pa)
            cnt = sb("e_cnt", [128, 1], F32, pa); g = sb("e_g", [128, 1], F32, pa)
            one16 = sb("e_one16", [16, 1], F32, pa)
            ptot = ps("e_ptot", [128, 1], F32, pa)
            for s in range(8):
                S.dma('sp', A128[16 * s:16 * s + 16, :], affT_d[:, 1024 * s:1024 * s + 1024], reads=['affT_d'], writes=['A128'])
            S.dma('sp', A16[:], affT_d, reads=['affT_d'], writes=['A16'])
            S.dma('sp', M16[:], m16_in, writes=['M16'])
            S.op('dve', lambda e: e.memset(lo[:], 0.0), writes=['lo'])
            S.op('dve', lambda e: e.memset(one16[:], 1.0), writes=['one16'])
            for k in range(30):
                hk = 2.0 ** -(k + 1)
                S.op('dve', lambda e: e.tensor_scalar(out=mid[:], in0=lo[:], scalar1=hk, scalar2=None, op0=ALU.add), reads=['lo'], writes=['mid'])
                S.op('dve', lambda e: e.tensor_scalar(out=junk[:], in0=A128[:], scalar1=mid[:, 0:1], scalar2=0.0, op0=ALU.is_gt, op1=ALU.add,
                                                      accum_out=cnt[:]), reads=['A128', 'mid'], writes=['junk', 'cnt'])
                S.op('pe', lambda e: e.matmul(ptot[:], lhsT=M16[:], rhs=cnt[:], start=True, stop=True), reads=['M16', 'cnt'], writes=['ptot'])
                S.op('dve', lambda e: e.tensor_scalar(out=g[:], in0=ptot[:], scalar1=float(CAP), scalar2=hk, op0=ALU.is_ge, op1=ALU.mult),
                     reads=['ptot'], writes=['g'])
                S.op('dve', lambda e: e.tensor_tensor(out=lo[:], in0=lo[:], in1=g[:], op=ALU.add), reads=['lo', 'g'], writes=['lo'])
            S.op('dve', lambda e: e.tensor_copy(out=thr_dbg[:], in_=lo[:]), reads=['lo'], writes=['thr'])
            S.op('dve', lambda e: e.tensor_scalar(out=A16[:], in0=A16[:], scalar1=lo[0:16, 0:1], scalar2=None, op0=ALU.is_gt),
                 reads=['A16', 'lo'], writes=['A16'])
            S.op('dve', lambda e: e.tensor_tensor_scan(out=C16[:], data0=one16[:, 0:1].to_broadcast([16, NT]), data1=A16[:], initial=0.0,
                                                       op0=ALU.mult, op1=ALU.add), reads=['A16', 'one16'], writes=['C16'])
            S.dma('sp', cum_d, C16[:], reads=['C16'], writes=['cum_d'])
            S.barrier()
        if 'moe_thr' in dbg:
            dbg_out['thr'] = dout("dbg_thr", [128, 1])
            S.dma('sp', dbg_out['thr'], thr_dbg[:], reads=['thr'])
        with ExitStack() as pe_:
            Wg = [sb("e_Wg%d" % i, [128, 8, D], BF16, pe_) for i in range(2)]
            Wu = [sb("e_Wu%d" % i, [128, 8, D], BF16, pe_) for i in range(2)]
            Wd = [sb("e_Wd%d" % i, [128, 8, D], BF16, pe_) for i in range(2)]
            xe = [sb("e_xe%d" % i, [128, D], BF16, pe_) for i in range(8)]
            ag = [[sb("e_ag%d_%d" % (p_, i), [128, 16], F32, pe_) for i in range(8)] for p_ in range(2)]
            cq = [sb("e_cq%d" % i, [128, 2048], F32, pe_) for i in range(2)]
            junkq = sb("e_junkq", [128, 2048], BF16, pe_)
            acc = [sb("e_acc%d" % i, [128, 4, 8], F32, pe_) for i in range(3)]
            idxs = sb("e_idxs", [128, 8], F32, pe_)
            idxi = [sb("e_idxi%d" % i, [128, 8], I32, pe_) for i in range(3)]
            xeT = sb("e_xeT", [128, 8, CAP], BF16, pe_)
            hidT = sb("e_hidT", [128, 8, CAP], BF16, pe_)
            sg = [sb("e_sg%d" % i, [128, 512], F32, pe_) for i in range(2)]
            ye = [sb("e_ye%d" % i, [128, D], F32, pe_) for i in range(2)]
            pGU = [ps("e_pGU%d" % i, [128, 512], F32, pe_) for i in range(3)]
            pY = ps("e_pY", [128, D], F32, pe_)
            pT = ps("e_pT", [128, 8, 128], BF16, pe_)
            pTi = pT[:].bitcast(F32) if False else None

            def load_w(e):
                i2 = e % 2
                for dc in range(8):
                    S.dma('pool', Wg[i2][:, dc, :], wg_in[e, dc * 128:(dc + 1) * 128, :], writes=[('Wg', i2)])
                    S.dma('pool', Wu[i2][:, dc, :], wu_in[e, dc * 128:(dc + 1) * 128, :], writes=[('Wu', i2)])
                for dc in range(8):
                    S.dma('pool', Wd[i2][:, dc, :], wd_in[e, dc * 128:(dc + 1) * 128, :], writes=[('Wd', i2)])

            def load_cq(n):
                e_, qd = n // 4, n % 4
                if e_ >= nexp:
                    return
                S.dma('sp', cq[n % 2][:], cum_d[e_:e_ + 1, qd * 2048:(qd + 1) * 2048].to_broadcast([128, 2048]),
                      reads=['cum_d'], writes=[('cq', n % 2)])

            def idx_piece(e, piece):
                j3 = e % 3
                n = e * 4 + piece // 2
                if piece % 2 == 0:
                    if n == 0:
                        load_cq(0)
                    load_cq(n + 1)
                for sc in range(4 * (piece % 2), 4 * (piece % 2) + 4):
                    S.op('dve', lambda e_: e_.tensor_scalar(out=junkq[:], in0=cq[n % 2][:], scalar1=slotv[:, sc:sc + 1], scalar2=0.0,
                                                            op0=ALU.is_le, op1=ALU.add, accum_out=acc[j3][:, piece // 2, sc:sc + 1]),
                         reads=[('cq', n % 2), 'slotv'], writes=['junkq', ('acc', j3)])
                if piece == 7:
                    S.op('dve', lambda e_: e_.tensor_tensor(out=idxs[:], in0=acc[j3][:, 0, :], in1=acc[j3][:, 1, :], op=ALU.add),
                         reads=[('acc', j3)], writes=['idxs'])
                    S.op('dve', lambda e_: e_.tensor_tensor(out=idxs[:], in0=idxs[:], in1=acc[j3][:, 2, :], op=ALU.add),
                         reads=[('acc', j3), 'idxs'], writes=['idxs'])
                    S.op('dve', lambda e_: e_.tensor_tensor(out=idxs[:], in0=idxs[:], in1=acc[j3][:, 3, :], op=ALU.add),
                         reads=[('acc', j3), 'idxs'], writes=['idxs'])
                    S.op('dve', lambda e_: e_.tensor_copy(out=idxi[j3][:], in_=idxs[:]), reads=['idxs'], writes=[('idxi', j3)])

            def gather(e):
                j2 = e % 3
                for sc in range(8):
                    S.custom_dma('pool', lambda e_: e_.indirect_dma_start(out=xe[sc][:, :], out_offset=None, in_=h2_d[:, :],
                                 in_offset=bass.IndirectOffsetOnAxis(ap=idxi[j2][:, sc:sc + 1], axis=0)),
                                 reads=[('idxi', j2), 'h2_d'], writes=[('xe', sc)])
                    S.custom_dma('pool', lambda e_: e_.indirect_dma_start(out=ag[e % 2][sc][:, :], out_offset=None, in_=aff_d[:, :],
                                 in_offset=bass.IndirectOffsetOnAxis(ap=idxi[j2][:, sc:sc + 1], axis=0)),
                                 reads=[('idxi', j2), 'aff_d'], writes=[('ag', e % 2, sc)])

            gu_ctr = [0]

            def xpose(e):
                for sc in range(8):
                    for dc in range(8):
                        S.op('pe', lambda e_: e_.transpose(out=pT[:, dc, :], in_=xe[sc][:, dc * 128:(dc + 1) * 128], identity=identb[:]),
                             reads=[('xe', sc), 'identb'], writes=['pT'])
                    S.op('act', lambda e_: e_.activation(out=xeT[:, :, sc * 128:(sc + 1) * 128], in_=pT[:], func=AF.Copy),
                         reads=['pT'], writes=['xeT'])
            def ffn(e, nxt):
                i2 = e % 2; j2 = e % 3
                for fc in range(8):
                    for hf in range(2):
                        a = gu_ctr[0] % 3; gu_ctr[0] += 1
                        b = gu_ctr[0] % 3; gu_ctr[0] += 1
                        for dc in range(8):
                            S.op('pe', lambda e_: e_.matmul(pGU[a][:], lhsT=Wg[i2][:, dc, fc * 128:(fc + 1) * 128], rhs=xeT[:, dc, hf * 512:(hf + 1) * 512],
                                                            start=(dc == 0), stop=(dc == 7)), reads=[('Wg', i2), 'xeT'], writes=[('pGU', a)])
                        for dc in range(8):
                            S.op('pe', lambda e_: e_.matmul(pGU[b][:], lhsT=Wu[i2][:, dc, fc * 128:(fc + 1) * 128], rhs=xeT[:, dc, hf * 512:(hf + 1) * 512],
                                                            start=(dc == 0), stop=(dc == 7)), reads=[('Wu', i2), 'xeT'], writes=[('pGU', b)])
                        s2 = (fc * 2 + hf) % 2
                        S.op('act', lambda e_: e_.activation(out=sg[s2][:], in_=pGU[a][:], func=AF.Silu), reads=[('pGU', a)], writes=[('sg', s2)])
                        S.op('dve', lambda e_: e_.tensor_tensor(out=hidT[:, fc, hf * 512:(hf + 1) * 512], in0=pGU[b][:], in1=sg[s2][:], op=ALU.mult),
                             reads=[('pGU', b), ('sg', s2)], writes=['hidT'])
                    if nxt is not None:
                        idx_piece(nxt, fc)
                for sc in range(8):
                    for hf in range(2):
                        for fc in range(8):
                            S.op('pe', lambda e_: e_.matmul(pY[:, hf * 512:(hf + 1) * 512], lhsT=hidT[:, fc, sc * 128:(sc + 1) * 128],
                                                            rhs=Wd[i2][:, fc, hf * 512:(hf + 1) * 512], start=(fc == 0), stop=(fc == 7)),
                                 reads=['hidT', ('Wd', i2)], writes=['pY'])
                    y2 = sc % 2
                    S.op('dve', lambda e_: e_.scalar_tensor_tensor(out=ye[y2][:], in0=pY[:], scalar=ag[e % 2][sc][:, e:e + 1], in1=bc_g2[:],
                                                                   op0=ALU.mult, op1=ALU.mult), reads=['pY', ('ag', e % 2, sc), 'bc_g2'], writes=[('ye', y2)])
                    S.custom_dma('pool', lambda e_: e_.indirect_dma_start(out=out[:, :], out_offset=bass.IndirectOffsetOnAxis(ap=idxi[j2][:, sc:sc + 1], axis=0),
                                 in_=ye[y2][:, :], in_offset=None, compute_op=ALU.add), reads=[('ye', y2), ('idxi', j2)], writes=['out'])

            load_w(0)
            for p in range(8):
                idx_piece(0, p)
            gather(0)
            if nexp > 1:
                load_w(1)
                for p in range(8):
                    idx_piece(1, p)
            for e in range(nexp):
                xpose(e)
                if e + 1 < nexp:
                    gather(e + 1)
                    if e >= 1:
                        load_w(e + 1)
                ffn(e, e + 2 if e + 2 < nexp else None)
            if 'moe_idx' in dbg:
                dbg_out['idxi'] = dout("dbg_idxi", [128, 8], I32)
                S.dma('sp', dbg_out['idxi'], idxi[(nexp - 1) % 3][:], reads=[('idxi', (nexp - 1) % 3)])
            S.barrier()


def moe_host(inp):
    f = np.float32
    m16 = np.zeros((128, 128), f)
    for s in range(8):
        for s2 in range(8):
            m16[16 * s:16 * s + 16, 16 * s2:16 * s2 + 16] = np.eye(16, dtype=f)
    return {'w_e_gate': np.ascontiguousarray(inp['w_e_gate'][0]), 'w_e_up': np.ascontiguousarray(inp['w_e_up'][0]),
            'w_e_down': np.ascontiguousarray(inp['w_e_down'][0]), 'm16': m16}


D = 1024
NLAT = 8192
NCTX = 256
NEXT = NLAT + NCTX
NE2 = NEXT + NCTX
EPS = 1e-6
INCOLS = 4096


def build(stop_after=99, dbg=(), nblk_lim=17, skip_s5=False, mg_blocks=16, nexp=16):
    nc = bass.Bass("TRN2", target_bir_lowering=False)
    es = ExitStack()
    S = Sched(nc, es)
    dbg_out = {}

    def din(name, shape, dt=F32):
        return nc.dram_tensor(name, list(shape), dt, kind="ExternalInput").ap()

    def dscr(name, shape, dt):
        return nc.dram_tensor(name, list(shape), dt, kind="Internal").ap()

    def dout(name, shape, dt=F32):
        return nc.dram_tensor(name, list(shape), dt, kind="ExternalOutput").ap()

    def sb(name, shape, dt, stack=None):
        return (stack or es).enter_context(nc.sbuf_tensor(name, list(shape), dt))

    def ps(name, shape, dt, stack=None):
        return (stack or es).enter_context(nc.psum_tensor(name, list(shape), dt))

    xcat = din("xcat", [NEXT, D])
    cT = din("cT", [128, 8, 2])
    w_ada = din("w_ada", [D, 6 * D])
    b_ada = din("b_ada", [1, 6 * D])
    nmixT = din("nmixT", [128, 8])
    nffn = din("nffn", [1, D])
    w_in = din("w_in", [D, INCOLS])
    ident_in = din("ident", [128, 128])
    qkg = din("qkg", [128, 2])

    out = dout("out", [NLAT, D])

    uT_d = dscr("uT_d", [512, NE2], BF16)
    qT_d = dscr("qT_d", [512, NLAT], BF16)
    kT_d = dscr("kT_d", [512, NEXT], BF16)
    v_d = dscr("v_d", [NEXT, 8, 65], BF16)
    sgaT_d = dscr("sgaT_d", [D, NLAT], BF16)
    sgbT_d = dscr("sgbT_d", [D, NLAT], BF16)

    identf = sb("identf", [128, 128], F32)
    identb = sb("identb", [128, 128], BF16)
    mul1 = sb("mul1", [128, 8, 2], F32)
    add1 = sb("add1", [128, 8, 2], F32)
    bc_g1 = sb("bc_g1", [128, D], F32)
    bc_mul2 = sb("bc_mul2", [128, D], F32)
    bc_add2 = sb("bc_add2", [128, D], F32)
    bc_g2 = sb("bc_g2", [128, D], F32)

    epsc = sb("epsc", [128, 1], F32)
    S.op('dve', lambda e: e.memset(epsc[:], EPS), writes=['epsc'])
    S.dma('sp', identf[:], ident_in, writes=['identf'])
    S.dma('pool', identb[:], ident_in, writes=['identb'])

    with ExitStack() as p0:
        wada = sb("wada", [128, 8, 6 * D], BF16, p0)
        cs_f = sb("cs_f", [128, 8, 2], F32, p0)
        cs = sb("cs", [128, 8, 2], BF16, p0)
        brow = sb("brow", [2, 6 * D], F32, p0)
        modrow = sb("modrow", [2, 6 * D], F32, p0)
        sel = sb("sel", [2, 128], F32, p0)
        nmix = sb("nmix", [128, 8], F32, p0)
        colsb = sb("colsb", [128, 16, 2], F32, p0)
        pm = [ps("pm%d" % i, [2, 512], F32, p0) for i in range(2)]
        pcol = ps("pcol", [128, 16, 2], F32, p0)
        pbc = [ps("pbc%d" % i, [128, 512], F32, p0) for i in range(2)]

        S.dma('sp', cs_f[:], cT, writes=['cs_f'])
        S.dma('sp', brow[0:1, :], b_ada, writes=['brow0'])
        S.dma('sp', brow[1:2, :], b_ada, writes=['brow1'])
        S.dma('sp', nmix[:], nmixT, writes=['nmix'])
        S.dma('sp', bc_mul2[:], nffn.to_broadcast([128, D]), writes=['bc_mul2'])
        for dc in range(8):
            S.dma('pool', wada[:, dc, :].rearrange("p (a b) -> p a b", b=2048),
                  w_ada[dc * 128:(dc + 1) * 128, :].rearrange("p (a b) -> p a b", b=2048),
                  writes=[('wada', dc)])
        S.op('act', lambda e: e.activation(out=cs[:], in_=cs_f[:], func=AF.Silu), reads=['cs_f'], writes=['cs'])
        S.op('dve', lambda e: e.memset(sel[:], 0.0), writes=['sel'])
        S.op('dve', lambda e: e.memset(sel[0:1, :], 1.0), writes=['sel'])
        for cc in range(12):
            pt = pm[cc % 2]
            for dc in range(8):
                S.op('pe', lambda e: e.matmul(pt[:], lhsT=cs[:, dc, :], rhs=wada[:, dc, cc * 512:(cc + 1) * 512],
                                              start=(dc == 0), stop=(dc == 7)),
                     reads=['cs', ('wada', dc)], writes=[('pm', cc % 2)])
            S.op('dve', lambda e: e.tensor_tensor(out=modrow[:, cc * 512:(cc + 1) * 512], in0=pt[:],
                                                  in1=brow[:, cc * 512:(cc + 1) * 512], op=ALU.add),
                 reads=[('pm', cc % 2), 'brow0', 'brow1'], writes=[('modrow', cc)])
        for i in range(16):
            S.op('pe', lambda e: e.transpose(out=pcol[:, i, :], in_=modrow[0:2, i * 128:(i + 1) * 128],
                                             identity=identf[0:2, 0:2]),
                 reads=[('modrow', i // 4), 'identf'], writes=['pcol'])
        S.op('dve', lambda e: e.tensor_copy(out=colsb[:], in_=pcol[:]), reads=['pcol'], writes=['colsb'])
        S.op('dve', lambda e: e.tensor_copy(out=add1[:], in_=colsb[:, 0:8, :]), reads=['colsb'], writes=['add1'])
        S.op('dve', lambda e: e.tensor_scalar(out=colsb[:, 8:16, :], in0=colsb[:, 8:16, :], scalar1=1.0, scalar2=None,
                                              op0=ALU.add), reads=['colsb'], writes=['colsb'])
        S.op('dve', lambda e: e.tensor_tensor(out=mul1[:], in0=colsb[:, 8:16, :],
                                              in1=nmix[:].unsqueeze(2).to_broadcast([128, 8, 2]), op=ALU.mult),
             reads=['colsb', 'nmix'], writes=['mul1'])

        def bcast(dst, col0, mode, dkey):
            for h in range(2):
                pt = pbc[h]
                S.op('pe', lambda e: e.matmul(pt[:], lhsT=sel[:], rhs=modrow[:, col0 + h * 512: col0 + (h + 1) * 512],
                                              start=True, stop=True),
                     reads=['sel'] + [('modrow', c) for c in range(12)], writes=[('pbc', h)])
                dsl = dst[:, h * 512:(h + 1) * 512]
                if mode == 'copy':
                    S.op('dve', lambda e: e.tensor_copy(out=dsl, in_=pt[:]), reads=[('pbc', h)], writes=[dkey])
                else:
                    S.op('dve', lambda e: e.scalar_tensor_tensor(out=dsl, in0=pt[:], scalar=1.0, in1=dsl,
                                                                 op0=ALU.add, op1=ALU.mult),
                         reads=[('pbc', h), dkey], writes=[dkey])
        bcast(bc_g1, 2048, 'copy', 'bc_g1')
        bcast(bc_add2, 3072, 'copy', 'bc_add2')
        bcast(bc_mul2, 4096, 'mul1p', 'bc_mul2')
        bcast(bc_g2, 5120, 'copy', 'bc_g2')
        if 'mod' in dbg:
            dbg_out['mod'] = dout("dbg_mod", [2, 6 * D])
            S.dma('sp', dbg_out['mod'], modrow[:], reads=[('modrow', c) for c in range(12)])
            dbg_out['mul1'] = dout("dbg_mul1", [128, 8, 2])
            S.dma('sp', dbg_out['mul1'], mul1[:], reads=['mul1'])
            dbg_out['bcm2'] = dout("dbg_bcm2", [128, D])
            S.dma('sp', dbg_out['bcm2'], bc_mul2[:], reads=['bc_mul2'])
        S.barrier()

    if stop_after <= 0:
        return finish(nc, S, es, out, dbg_out)

    with ExitStack() as p1:
        win = sb("win", [128, 8, INCOLS], BF16, p1)
        xt = [sb("xt%d" % i, [128, D], F32, p1) for i in range(2)]
        junk = sb("junk", [128, D], BF16, p1)
        xn = [sb("xn%d" % i, [128, D], BF16, p1) for i in range(4)]
        ss = [sb("ss%d" % i, [128, 1], F32, p1) for i in range(4)]
        rstd = [sb("rstd%d" % i, [128, 1], F32, p1) for i in range(4)]
        hT = [sb("hT%d" % i, [128, 8, 512], BF16, p1) for i in range(2)]
        stg = [sb("stg%d" % i, [128, 512], BF16, p1) for i in range(4)]
        sq = [sb("sq%d" % i, [128, 512], BF16, p1) for i in range(2)]
        qk32 = [sb("qk32_%d" % i, [128, 512], F32, p1) for i in range(2)]
        rs = [sb("rs%d" % i, [128, 512], F32, p1) for i in range(2)]
        vst = [sb("vst%d" % i, [128, 8, 65], BF16, p1) for i in range(2)]
        bones = sb("bones", [128, 128], BF16, p1)
        gq = sb("gq", [128, 2], F32, p1)
        pT = [ps("pT%d" % i, [128, 8, 128], BF16, p1) for i in range(2)]
        pz = [ps("pz%d" % i, [128, 512], F32, p1) for i in range(4)]
        pms = [ps("pms%d" % i, [128, 512], F32, p1) for i in range(2)]

        for dc in range(8):
            S.dma('pool', win[:, dc, :].rearrange("p (a b) -> p a b", b=2048),
                  w_in[dc * 128:(dc + 1) * 128, :].rearrange("p (a b) -> p a b", b=2048),
                  writes=[('win', dc)])
        S.dma('sp', gq[:], qkg, writes=['gq'])
        S.op('dve', lambda e: e.tensor_scalar(out=gq[:, 0:1], in0=gq[:, 0:1], scalar1=0.125, scalar2=None, op0=ALU.mult), reads=['gq'], writes=['gq'])
        for i in range(2):
            S.op('dve', lambda e: e.memset(vst[i][:], 1.0), writes=[('vst', i)])
        S.op('dve', lambda e: e.memset(bones[:], 0.0), writes=['bones'])
        S.op('dve', lambda e: e.memset(bones[0:64, 0:64], 1.0 / 64), writes=['bones'])
        S.op('dve', lambda e: e.memset(bones[64:128, 64:128], 1.0 / 64), writes=['bones'])

        nblk = (NEXT + 511) // 512
        ctr = {'tile': 0, 'tpe': 0, 'stg': 0, 'z': 0, 'v': 0}

        def blk_info(b):
            if b == 0:
                return 0, 256
            return 256 + (b - 1) * 512, 512

        def head_pre(b):
            t0, nt = blk_info(b)
            for ti in range(nt // 128):
                i2 = ctr['tile'] % 2
                i4 = ctr['tile'] % 4
                ctr['tile'] += 1
                r0 = t0 + ti * 128
                S.dma('sp', xt[i2][:], xcat[r0:r0 + 128, :], writes=[('xt', i2)])
                S.op('act', lambda e: e.activation(out=junk[:], in_=xt[i2][:], func=AF.Square, accum_out=ss[i4][:]),
                     reads=[('xt', i2)], writes=['junk', ('ss', i4)])
                S.op('act', lambda e: e.activation(out=rstd[i4][:], in_=ss[i4][:], func=AF.Ln, scale=1.0 / D, bias=epsc[:, 0:1]),
                     reads=[('ss', i4), 'epsc'], writes=[('rstd', i4)])
                S.op('act', lambda e: e.activation(out=rstd[i4][:], in_=rstd[i4][:], func=AF.Exp, scale=-0.5),
                     reads=[('rstd', i4)], writes=[('rstd', i4)])
                S.op('act', lambda e: e.activation(out=xn[i4][:], in_=xt[i2][:], func=AF.Copy, scale=rstd[i4][:, 0:1]),
                     reads=[('xt', i2), ('rstd', i4)], writes=[('xn', i4)])

        def head_pe(b):
            t0, nt = blk_info(b)
            j = 1 if b == 0 else 0
            hb = hT[b % 2]
            hkey = ('hT', b % 2)
            for ti in range(nt // 128):
                i2 = ctr['tpe'] % 2
                i4 = ctr['tpe'] % 4
                ctr['tpe'] += 1
                for dc in range(8):
                    S.op('pe', lambda e: e.transpose(out=pT[i2][:, dc, :], in_=xn[i4][:, dc * 128:(dc + 1) * 128],
                                                     identity=identb[:]),
                         reads=[('xn', i4), 'identb'], writes=[('pT', i2)])
                for dc in range(8):
                    S.op('dve', lambda e: e.tensor_scalar(out=hb[:, dc, ti * 128:(ti + 1) * 128], in0=pT[i2][:, dc, :],
                                                          scalar1=mul1[:, dc, j:j + 1], scalar2=add1[:, dc, j:j + 1],
                                                          op0=ALU.mult, op1=ALU.add),
                         reads=[('pT', i2), 'mul1', 'add1'], writes=[hkey])

        pend_qk = []

        def chunk(b, cc):
            t0, nt = blk_info(b)
            is_ctx = (b == 0)
            hb = hT[b % 2]
            hkey = ('hT', b % 2)
            zi = ctr['z'] % 4
            ctr['z'] += 1
            pzt = pz[zi]
            for dc in range(8):
                S.op('pe', lambda e: e.matmul(pzt[:, 0:nt], lhsT=win[:, dc, cc * 128:(cc + 1) * 128], rhs=hb[:, dc, 0:nt],
                                              start=(dc == 0), stop=(dc == 7)),
                     reads=[('win', dc), hkey], writes=[('pz', zi)], sig=(dc == 7))
            si = ctr['stg'] % 4
            ctr['stg'] += 1
            st = stg[si]
            skey = ('stg', si)
            if not (4 <= cc < 12):
                while pend_qk:
                    pend_qk.pop(0)()
            if cc < 4:
                S.op('act', lambda e: e.activation(out=st[:, 0:nt], in_=pzt[:, 0:nt], func=AF.Copy),
                     reads=[('pz', zi)], writes=[skey])
                S.dma('sp', uT_d[cc * 128:(cc + 1) * 128, t0:t0 + nt], st[:, 0:nt], reads=[skey], writes=['uT_d'])
                if is_ctx:
                    S.dma('sp', uT_d[cc * 128:(cc + 1) * 128, NEXT:NEXT + nt], st[:, 0:nt], reads=[skey], writes=['uT_d'])
            elif cc < 12:
                isq = cc < 8
                qi = ctr['z'] % 2
                S.op('act', lambda e: e.activation(out=sq[qi][:, 0:nt], in_=pzt[:, 0:nt], func=AF.Square),
                     reads=[('pz', zi)], writes=[('sq', qi)])
                while pend_qk:
                    pend_qk.pop(0)()

                def tail(qi=qi, zi=zi, pzt=pzt, st=st, skey=skey, isq=isq, cc=cc, t0=t0, nt=nt):
                    S.op('pe', lambda e: e.matmul(pms[qi][:, 0:nt], lhsT=bones[:], rhs=sq[qi][:, 0:nt], start=True, stop=True),
                         reads=['bones', ('sq', qi)], writes=[('pms', qi)])
                    S.op('act', lambda e: e.activation(out=rs[qi][:, 0:nt], in_=pms[qi][:, 0:nt], func=AF.Ln, bias=epsc[:, 0:1]),
                         reads=[('pms', qi), 'epsc'], writes=[('rs', qi)])
                    S.op('act', lambda e: e.activation(out=rs[qi][:, 0:nt], in_=rs[qi][:, 0:nt], func=AF.Exp, scale=-0.5),
                         reads=[('rs', qi)], writes=[('rs', qi)])
                    gcol = gq[:, 0:1] if isq else gq[:, 1:2]
                    S.op('dve', lambda e: e.scalar_tensor_tensor(out=st[:, 0:nt], in0=pzt[:, 0:nt], scalar=gcol,
                                                                 in1=rs[qi][:, 0:nt], op0=ALU.mult, op1=ALU.mult),
                         reads=[('pz', zi), ('rs', qi), 'gq'], writes=[skey])
                    if isq:
                        S.dma('sp', qT_d[(cc - 4) * 128:(cc - 3) * 128, t0 - NCTX:t0 - NCTX + nt], st[:, 0:nt],
                              reads=[skey], writes=['qT_d'])
                    else:
                        S.dma('sp', kT_d[(cc - 8) * 128:(cc - 7) * 128, t0:t0 + nt], st[:, 0:nt], reads=[skey], writes=['kT_d'])
                pend_qk.append(tail)
            else:
                S.op('act', lambda e: e.activation(out=st[:, 0:nt], in_=pzt[:, 0:nt], func=AF.Sigmoid),
                     reads=[('pz', zi)], writes=[skey])
                if cc < 24:
                    dst = sgaT_d[(cc - 16) * 128:(cc - 15) * 128, t0 - NCTX:t0 - NCTX + nt]
                    dk = 'sgaT_d'
                else:
                    dst = sgbT_d[(cc - 24) * 128:(cc - 23) * 128, t0 - NCTX:t0 - NCTX + nt]
                    dk = 'sgbT_d'
                S.dma('sp', dst, st[:, 0:nt], reads=[skey], writes=[dk])

        def vpart(b):
            t0, nt = blk_info(b)
            hb = hT[b % 2]
            hkey = ('hT', b % 2)
            for ti in range(nt // 128):
                zi = ctr['z'] % 4
                ctr['z'] += 1
                pzt = pz[zi]
                for dc in range(8):
                    S.op('pe', lambda e: e.matmul(pzt[:], lhsT=hb[:, dc, ti * 128:(ti + 1) * 128], rhs=win[:, dc, 1536:2048],
                                                  start=(dc == 0), stop=(dc == 7)),
                         reads=[('win', dc), hkey], writes=[('pz', zi)], sig=(dc == 7))
                while pend_qk:
                    pend_qk.pop(0)()
                vi = ctr['v'] % 2
                ctr['v'] += 1
                S.op('act', lambda e: e.activation(out=vst[vi][:, :, 0:64], in_=pzt[:].rearrange("p (h d) -> p h d", d=64), func=AF.Copy),
                     reads=[('pz', zi)], writes=[('vst', vi)])
                S.dma('sp', v_d[t0 + ti * 128:t0 + (ti + 1) * 128], vst[vi][:], reads=[('vst', vi)], writes=['v_d'])

        head_pre(0)
        head_pe(0)
        for b in range(nblk_lim):
            if b == 0:
                ccs = list(range(0, 4)) + list(range(8, 12))
            else:
                ccs = list(range(0, 12)) + list(range(16, 32))
            n1, n2 = len(ccs) // 4, (3 * len(ccs)) // 4
            for cc in ccs[:n1]:
                chunk(b, cc)
            if b + 1 < nblk_lim:
                head_pre(b + 1)
            for cc in ccs[n1:n2]:
                chunk(b, cc)
            if b + 1 < nblk_lim:
                head_pe(b + 1)
            for cc in ccs[n2:]:
                chunk(b, cc)
            vpart(b)
        S.barrier()
    if 'p1' in dbg:
        for nm, t in (('uT', uT_d), ('qT', qT_d), ('kT', kT_d), ('sgaT', sgaT_d), ('sgbT', sgbT_d)):
            dbg_out[nm] = dout("dbg_" + nm, list(t.shape), BF16)
            for r0 in range(0, t.shape[0], 128):
                S.dma('sp', dbg_out[nm][r0:r0 + 128, :], t[r0:r0 + 128, :], reads=[])
    if stop_after <= 1:
        return finish(nc, S, es, out, dbg_out)
    yaT_d = dscr("yaT_d", [512, NLAT], BF16)
    r = 'ok' if skip_s5 else s5_phase(nc, S, es, sb, ps, din, dscr, dout, dbg, dbg_out, identf, identb, uT_d, yaT_d)
    if r == 'stop' or stop_after <= 2:
        return finish(nc, S, es, out, dbg_out)
    ybT_d = dscr("ybT_d", [512, NLAT], BF16)
    na_phase(nc, S, es, sb, ps, din, dscr, dout, dbg, dbg_out, identb, qT_d, kT_d, v_d, ybT_d)
    if stop_after <= 3:
        return finish(nc, S, es, out, dbg_out)
    h2_d = dscr("h2_d", [NLAT, D], BF16)
    aff_d = dscr("aff_d", [NLAT, 16], F32)
    affT_d = dscr("affT_d", [16, NLAT], F32)
    merge_phase(nc, S, es, sb, ps, din, dscr, dout, dbg, dbg_out, identf, identb, epsc, bc_g1, bc_mul2, bc_add2,
                xcat, out, yaT_d, ybT_d, sgaT_d, sgbT_d, h2_d, aff_d, affT_d, nblocks=mg_blocks)
    if stop_after <= 4:
        return finish(nc, S, es, out, dbg_out)
    moe_phase(nc, S, es, sb, ps, din, dscr, dout, dbg, dbg_out, identf, identb, bc_g2, out, h2_d, aff_d, affT_d, nexp=nexp)
    return finish(nc, S, es, out, dbg_out)


def finish(nc, S, es, out, dbg_out):
    S.barrier(['sp'])
    print("instructions", S.nins, "waits", S.nwaits)
    es.close()
    return nc, dbg_out


def host_inputs(inp, b):
    f = np.float32
    xcat = np.concatenate([inp['ctx'][b], inp['x'][b]], axis=0).astype(f)
    cT = np.stack([inp['c'][b].reshape(8, 128).T, inp['c_ctx'].reshape(8, 128).T], axis=-1).astype(f)
    qkg = np.stack([np.tile(inp['q_norm'][0], 2), np.tile(inp['k_norm'][0], 2)], axis=-1).astype(f)
    m = {
        'xcat': np.ascontiguousarray(xcat),
        'cT': np.ascontiguousarray(cT),
        'w_ada': np.ascontiguousarray(inp['w_ada'][0]),
        'b_ada': np.ascontiguousarray(inp['b_ada'][0][None, :]),
        'nmixT': np.ascontiguousarray(inp['norm_mix'][0].reshape(8, 128).T),
        'nffn': np.ascontiguousarray(inp['norm_ffn'][0][None, :]),
        'w_in': np.ascontiguousarray(inp['w_in'][0]),
        'ident': np.eye(128, dtype=f),
        'qkg': np.ascontiguousarray(qkg),
    }
    f32c = lambda a: np.ascontiguousarray(a.astype(f))
    lr = inp['ssm_lam_re'][0].reshape(2, 16, 2, 64)
    m['lamre_l'] = f32c(lr.transpose(2, 3, 0, 1).reshape(128, 32))
    li = inp['ssm_lam_im'][0].reshape(2, 16, 2, 64)
    m['lamim_l'] = f32c(li.transpose(2, 3, 0, 1).reshape(128, 32))
    ld = inp['ssm_log_dt'][0].reshape(2, 16, 2)
    m['logdt_l'] = f32c(np.broadcast_to(ld.transpose(2, 0, 1)[:, None, :, :], (2, 64, 2, 16)).reshape(128, 32))
    for nm, key in (('Bre_c', 'ssm_b_re'), ('Bim_c', 'ssm_b_im')):
        bb = inp[key][0].reshape(2, 16, 2, 64, 16)
        m[nm] = f32c(bb.transpose(2, 3, 0, 1, 4).reshape(128, 32, 16))
    for nm, key in (('Cre_c', 'ssm_c_re'), ('Cim_c', 'ssm_c_im')):
        cc = inp[key][0].reshape(2, 16, 2, 16, 64)
        m[nm] = f32c(cc.transpose(2, 4, 0, 1, 3).reshape(128, 32, 16))
    m['dskipT'] = f32c(inp['ssm_d'][0].reshape(4, 128).T)
    m['w_glu'] = f32c(inp['w_glu'][0])
    m['bgluT'] = f32c(inp['b_glu'][0].reshape(4, 128).T)
    m.update(na_host(inp))
    m['w_proj_a'] = f32c(inp['w_proj_a'][0]); m['w_proj_b'] = f32c(inp['w_proj_b'][0]); m['w_out'] = f32c(inp['w_out'][0])
    m.update(moe_host(inp))
    m['w_routerT'] = f32c(inp['w_router'][0].reshape(8, 128, 16).transpose(1, 0, 2))
    return m


_NC_CACHE = {}


def kernel(**inputs):
    inp = {k_: np.asarray(v_) for k_, v_ in inputs.items()}
    if 'nc' not in _NC_CACHE:
        nc, _ = build()
        _NC_CACHE['nc'] = nc
    nc = _NC_CACHE['nc']
    nb = inp['x'].shape[0]
    in_maps = [host_inputs(inp, b) for b in range(nb)]
    res = run_bass_kernel_spmd(nc, in_maps, core_ids=list(range(nb)))
    outs = [np.asarray(res.results[b]["out"], dtype=np.float32) for b in range(nb)]
    return np.stack(outs, axis=0)
```

```python
import numpy as np
from contextlib import ExitStack
import concourse.bass as bass
import concourse.mybir as mybir
from concourse.bass_utils import run_bass_kernel_spmd

F32 = mybir.dt.float32
BF16 = mybir.dt.bfloat16
I32 = mybir.dt.int32
U32 = mybir.dt.uint32
ALU = mybir.AluOpType
AF = mybir.ActivationFunctionType
AX = mybir.AxisListType
STRICT = False


class Sched:
    def __init__(self, nc, es, n_dma_sems=24):
        self.nc = nc
        self.e = {'pe': nc.tensor, 'act': nc.scalar, 'dve': nc.vector, 'pool': nc.gpsimd, 'sp': nc.sync}
        self.sems = {}
        self.cnt = {}
        for k in self.e:
            self.sems[k] = es.enter_context(nc.semaphore("sem_" + k))
            self.cnt[k] = 0
        self.nd = n_dma_sems
        self.qsems = {}
        for q in ('sp', 'pool', 'act'):
            n = n_dma_sems if q != 'act' else 16
            self.qsems[q] = []
            for i in range(n):
                sk = 'd_%s%d' % (q, i)
                self.sems[sk] = es.enter_context(nc.semaphore("dsem_%s%d" % (q, i)))
                self.cnt[sk] = 0
                self.qsems[q].append(sk)
        self.qrr = {'sp': 0, 'pool': 0, 'act': 0}
        self.drr = 0
        self.waited = {}
        self.lastw = {}
        self.readers = {}
        self.nwaits = 0
        self.nins = 0

    def _wait(self, eng, tok):
        sk, val, src = tok
        if self.waited.get((eng, sk), 0) >= val:
            return
        self.e[eng].wait_ge(self.sems[sk], val)
        self.waited[(eng, sk)] = val
        self.nwaits += 1

    def _deps(self, eng, reads, writes):
        for k in reads:
            t = self.lastw.get(k)
            if t is not None:
                self._wait(eng, t)
        for k in writes:
            t = self.lastw.get(k)
            if t is not None and (t[2] != eng or (STRICT and eng != 'pe')):
                self._wait(eng, t)
            for sk, (val, src) in self.readers.get(k, {}).items():
                if src != eng or (STRICT and eng != 'pe'):
                    self._wait(eng, (sk, val, src))

    def _record(self, tok, reads, writes):
        sk, val, src = tok
        for k in reads:
            self.readers.setdefault(k, {})[sk] = (val, src)
        for k in writes:
            self.lastw[k] = tok
            self.readers[k] = {}

    def op(self, eng, fn, reads=(), writes=(), sig=True):
        self._deps(eng, reads, writes)
        ins = fn(self.e[eng])
        if sig:
            self.cnt[eng] += 1
            ins.then_inc(self.sems[eng], 1)
            tok = (eng, self.cnt[eng], eng)
        else:
            tok = (eng, self.cnt[eng] + 1, eng)
        self._record(tok, reads, writes)
        self.nins += 1
        return tok

    def _dma_common(self, q, fn, reads, writes):
        self._deps(q, reads, writes)
        i = self.qrr[q]
        self.qrr[q] = (i + 1) % len(self.qsems[q])
        sk = self.qsems[q][i]
        if self.cnt[sk] > 0:
            self._wait(q, (sk, self.cnt[sk], None))
        ins = fn(self.e[q])
        self.cnt[sk] += 16
        ins.then_inc(self.sems[sk], 16)
        tok = (sk, self.cnt[sk], None)
        self._record(tok, reads, writes)
        self.nins += 1
        return tok

    def dma(self, q, out, in_, reads=(), writes=(), **kw):
        return self._dma_common(q, lambda e: e.dma_start(out=out, in_=in_, **kw), reads, writes)

    def custom_dma(self, q, fn, reads=(), writes=()):
        return self._dma_common(q, fn, reads, writes)

    def barrier(self, engines=None):
        for e in (engines or self.e):
            for sk, c in self.cnt.items():
                if c > 0:
                    self._wait(e, (sk, c, None))
        self.lastw = {}
        self.readers = {}

PI = float(np.pi)


def kn(t):
    n = t.name
    return n[2:] if n.startswith('s_') else n


def s5_phase(nc, S, es, sb, ps, din, dscr, dout, dbg, dbg_out, identf, identb, uT_d, yaT_d, nbanks=16):
    NEXT, NE2 = 8448, 8704
    lre_in = din("lamre_l", [128, 32]); lim_in = din("lamim_l", [128, 32]); ldt_in = din("logdt_l", [128, 32])
    Bre_in = din("Bre_c", [128, 32, 16]); Bim_in = din("Bim_c", [128, 32, 16])
    Cre_in = din("Cre_c", [128, 32, 16]); Cim_in = din("Cim_c", [128, 32, 16])
    dsk_in = din("dskipT", [128, 4]); wglu_in = din("w_glu", [512, 512]); bglu_in = din("bgluT", [128, 4])
    E_d = dscr("E_d", [128, 2, 4, 16, 2, 2, 128], BF16)
    G_d = dscr("G_d", [128, 64, 640], BF16)
    H_d = dscr("H_d", [2, 128, 16, 2, 544], BF16)

    with ExitStack() as s5:
        Kt_sb = sb("s_Kt_sb", [128, 128, 128], BF16, s5)
        aL = sb("s_aL", [128, 32], F32, s5); bL = sb("s_bL", [128, 32], F32, s5)
        diagD = sb("s_diagD", [128, 4, 128], BF16, s5)
        with ExitStack() as pa:
            def t32(name):
                return sb(name, [128, 32], F32, pa)
            lre, lim, ldt, dt, a, b = (t32(n) for n in ("lre", "lim", "ldt", "dt", "a_", "b_"))
            u1, u2, u3, u4, c16, s16, wre, wim = (t32(n) for n in ("u1", "u2", "u3", "u4", "c16", "s16", "wre", "wim"))
            Bcr = sb("s_Bcr", [128, 32, 16], F32, pa); Bci = sb("s_Bci", [128, 32, 16], F32, pa)
            Dr = sb("s_Dr", [128, 32, 32], F32, pa); Di = sb("s_Di", [128, 32, 32], F32, pa)
            Dr2 = sb("s_Dr2", [128, 32, 32], F32, pa); Di2 = sb("s_Di2", [128, 32, 32], F32, pa)
            Gr = sb("s_Gr", [128, 32, 32], F32, pa); Gi = sb("s_Gi", [128, 32, 32], F32, pa)
            T1 = sb("s_T1", [128, 32, 32], F32, pa); T2 = sb("s_T2", [128, 32, 32], F32, pa)
            T3 = sb("s_T3", [128, 32, 32], F32, pa); T4 = sb("s_T4", [128, 32, 32], F32, pa)
            P1 = sb("s_P1", [128, 32, 32], F32, pa); P2 = sb("s_P2", [128, 32, 32], F32, pa)
            P3 = sb("s_P3", [128, 32, 32], F32, pa); P4 = sb("s_P4", [128, 32, 32], F32, pa)
            Cbr = sb("s_Cbr", [128, 32, 32], BF16, pa); Cbni = sb("s_Cbni", [128, 32, 32], BF16, pa)
            Dbr = sb("s_Dbr", [128, 8, 160], BF16, pa); Dbi = sb("s_Dbi", [128, 8, 160], BF16, pa)
            Gbr = sb("s_Gbr", [128, 8, 160], BF16, pa); Gbni = sb("s_Gbni", [128, 8, 160], BF16, pa)
            Es = [sb("s_Es%d" % i, [128, 2, 4, 2, 2, 128], BF16, pa) for i in range(2)]
            m3 = sb("s_m3", [128, 1], F32, pa)
            dsk = sb("s_dsk", [128, 4], F32, pa)
            pk = [ps("s_pk%d" % i, [128, 128], F32, pa) for i in range(2)]
            pE = [ps("s_pE%d" % i, [128, 128], F32, pa) for i in range(4)]

            S.dma('sp', lre[:], lre_in, writes=['lre']); S.dma('sp', lim[:], lim_in, writes=['lim'])
            S.dma('sp', ldt[:], ldt_in, writes=['ldt']); S.dma('sp', dsk[:], dsk_in, writes=['dsk'])
            for X in (Dr, Di, Gr, Gi, Dr2, Di2):
                S.op('dve', lambda e: e.memset(X[:], 0.0), writes=[kn(X)])
            for X in (Dbr, Dbi, Gbr, Gbni):
                S.op('pool', lambda e: e.memset(X[:], 0.0), writes=[kn(X)])
            S.op('pool', lambda e: e.memset(Kt_sb[:], 0.0), writes=['Kt_sb'])
            for i in range(2):
                S.op('pool', lambda e: e.memset(Es[i][:], 0.0), writes=[('Es', i)])
            S.op('dve', lambda e: e.memset(m3[:], 1.0), writes=['m3'])
            S.op('dve', lambda e: e.memset(m3[64:96, :], 0.0), writes=['m3'])
            S.dma('sp', Bcr[:], Bre_in, writes=['Bcr']); S.dma('sp', Bci[:], Bim_in, writes=['Bci'])
            for h in range(2):
                S.dma('sp', Gr[64 * h:64 * h + 64, :, 16 * h:16 * h + 16], Cre_in[64 * h:64 * h + 64], reads=[], writes=['Gr'])
                S.dma('sp', Gi[64 * h:64 * h + 64, :, 16 * h:16 * h + 16], Cim_in[64 * h:64 * h + 64], reads=[], writes=['Gi'])
            for q in range(4):
                S.op('dve', lambda e: e.tensor_scalar(out=diagD[:, q, :], in0=identf[:], scalar1=dsk[:, q:q + 1], scalar2=None,
                                                      op0=ALU.mult), reads=['identf', 'dsk'], writes=['diagD'])
            V = lambda e: e
            def tt(out, in0, in1, op, r, w, eng='dve'):
                S.op(eng, lambda e: e.tensor_tensor(out=out, in0=in0, in1=in1, op=op), reads=r, writes=w)
            S.op('act', lambda e: e.activation(out=dt[:], in_=ldt[:], func=AF.Exp), reads=['ldt'], writes=['dt'])
            tt(u1[:], lre[:], dt[:], ALU.mult, ['lre', 'dt'], ['u1'])
            S.op('act', lambda e: e.activation(out=u1[:], in_=u1[:], func=AF.Exp), reads=['u1'], writes=['u1'])
            tt(u2[:], lim[:], dt[:], ALU.mult, ['lim', 'dt'], ['u2'])
            S.op('act', lambda e: e.activation(out=s16[:], in_=u2[:], func=AF.Sin, scale=1.0 / 16), reads=['u2'], writes=['s16'])
            S.op('act', lambda e: e.activation(out=u3[:], in_=u2[:], func=AF.Sin, scale=1.0 / 32), reads=['u2'], writes=['u3'])
            tt(u3[:], u3[:], u3[:], ALU.mult, ['u3'], ['u3'])
            S.op('dve', lambda e: e.tensor_scalar(out=c16[:], in0=u3[:], scalar1=-2.0, scalar2=1.0, op0=ALU.mult, op1=ALU.add),
                 reads=['u3'], writes=['c16'])

            def csquare(cr, ci, n):
                for _ in range(n):
                    tt(u3[:], cr[:], cr[:], ALU.mult, [kn(cr)], ['u3'])
                    tt(u4[:], ci[:], ci[:], ALU.mult, [kn(ci)], ['u4'])
                    tt(ci[:], cr[:], ci[:], ALU.mult, [kn(cr), kn(ci)], [kn(ci)])
                    S.op('dve', lambda e: e.tensor_scalar(out=ci[:], in0=ci[:], scalar1=2.0, scalar2=None, op0=ALU.mult),
                         reads=[kn(ci)], writes=[kn(ci)])
                    tt(cr[:], u3[:], u4[:], ALU.subtract, ['u3', 'u4'], [kn(cr)])
            csquare(c16, s16, 4)
            tt(a[:], u1[:], c16[:], ALU.mult, ['u1', 'c16'], ['a_'])
            tt(b[:], u1[:], s16[:], ALU.mult, ['u1', 's16'], ['b_'])
            S.op('dve', lambda e: e.tensor_copy(out=aL[:], in_=c16[:]), reads=['c16'], writes=['aL'])
            S.op('dve', lambda e: e.tensor_copy(out=bL[:], in_=s16[:]), reads=['s16'], writes=['bL'])
            csquare(aL, bL, 4)
            tt(u3[:], lre[:], dt[:], ALU.mult, ['lre', 'dt'], ['u3'])
            S.op('act', lambda e: e.activation(out=u3[:], in_=u3[:], func=AF.Exp, scale=16.0), reads=['u3'], writes=['u3'])
            tt(aL[:], aL[:], u3[:], ALU.mult, ['aL', 'u3'], ['aL'])
            tt(bL[:], bL[:], u3[:], ALU.mult, ['bL', 'u3'], ['bL'])
            S.op('dve', lambda e: e.tensor_scalar(out=u1[:], in0=a[:], scalar1=-1.0, scalar2=None, op0=ALU.add), reads=['a_'], writes=['u1'])
            tt(u2[:], lre[:], lre[:], ALU.mult, ['lre'], ['u2'])
            tt(u3[:], lim[:], lim[:], ALU.mult, ['lim'], ['u3'])
            tt(u2[:], u2[:], u3[:], ALU.add, ['u2', 'u3'], ['u2'])
            S.op('dve', lambda e: e.reciprocal(out=u2[:], in_=u2[:]), reads=['u2'], writes=['u2'])
            tt(u3[:], u1[:], lre[:], ALU.mult, ['u1', 'lre'], ['u3'])
            tt(u4[:], b[:], lim[:], ALU.mult, ['b_', 'lim'], ['u4'])
            tt(u3[:], u3[:], u4[:], ALU.add, ['u3', 'u4'], ['u3'])
            tt(wre[:], u3[:], u2[:], ALU.mult, ['u3', 'u2'], ['wre'])
            tt(u3[:], b[:], lre[:], ALU.mult, ['b_', 'lre'], ['u3'])
            tt(u4[:], u1[:], lim[:], ALU.mult, ['u1', 'lim'], ['u4'])
            tt(u3[:], u3[:], u4[:], ALU.subtract, ['u3', 'u4'], ['u3'])
            tt(wim[:], u3[:], u2[:], ALU.mult, ['u3', 'u2'], ['wim'])
            bc16 = lambda t: t[:].unsqueeze(2).to_broadcast([128, 32, 16])
            Tc = [T1[:, :, 0:16], T2[:, :, 0:16], T3[:, :, 0:16], T4[:, :, 0:16]]
            tt(Tc[0], Bcr[:], bc16(wre), ALU.mult, ['Bcr', 'wre'], ['T1'])
            tt(Tc[1], Bci[:], bc16(wim), ALU.mult, ['Bci', 'wim'], ['T2'])
            tt(Tc[2], Bci[:], bc16(wre), ALU.mult, ['Bci', 'wre'], ['T3'])
            tt(Tc[3], Bcr[:], bc16(wim), ALU.mult, ['Bcr', 'wim'], ['T4'])
            for h in range(2):
                sl = slice(64 * h, 64 * h + 64)
                tt(Dr[sl, :, 16 * h:16 * h + 16], T1[sl, :, 0:16], T2[sl, :, 0:16], ALU.subtract, ['T1', 'T2'], ['Dr'])
                tt(Di[sl, :, 16 * h:16 * h + 16], T3[sl, :, 0:16], T4[sl, :, 0:16], ALU.add, ['T3', 'T4'], ['Di'])
            S.op('dve', lambda e: e.tensor_copy(out=Cbr[:], in_=Gr[:]), reads=['Gr'], writes=['Cbr'])
            S.op('dve', lambda e: e.tensor_scalar(out=Cbni[:], in0=Gi[:], scalar1=-1.0, scalar2=None, op0=ALU.mult),
                 reads=['Gi'], writes=['Cbni'])
            bc32 = lambda t: t[:].unsqueeze(2).to_broadcast([128, 32, 32])

            def cmul(Xr, Xi, eng, Yr=None, Yi=None):
                Yr = Yr or Xr; Yi = Yi or Xi
                A1, A2, A3, A4 = (T1, T2, T3, T4) if eng == 'dve' else (P1, P2, P3, P4)
                tt(A1[:], Xr[:], bc32(a), ALU.mult, [kn(Xr), 'a_'], [kn(A1)], eng)
                tt(A2[:], Xi[:], bc32(b), ALU.mult, [kn(Xi), 'b_'], [kn(A2)], eng)
                tt(A3[:], Xi[:], bc32(a), ALU.mult, [kn(Xi), 'a_'], [kn(A3)], eng)
                tt(A4[:], Xr[:], bc32(b), ALU.mult, [kn(Xr), 'b_'], [kn(A4)], eng)
                tt(Yr[:], A1[:], A2[:], ALU.subtract, [kn(A1), kn(A2)], [kn(Yr)], eng)
                tt(Yi[:], A3[:], A4[:], ALU.add, [kn(A3), kn(A4)], [kn(Yi)], eng)

            def to5(dst, src, scale, key, eng='dve'):
                d4 = dst[:].rearrange("p g (s c) -> p g s c", c=32)
                s4 = src[:].rearrange("p (g s) c -> p g s c", s=4)
                S.op(eng, lambda e: e.tensor_scalar(out=d4[:, :, 0:3, :], in0=s4[:, :, 0:3, :], scalar1=scale, scalar2=None, op0=ALU.mult),
                     reads=[kn(src)], writes=[key])
                S.op(eng, lambda e: e.tensor_scalar(out=d4[:, :, 4, :], in0=s4[:, :, 3, :], scalar1=scale, scalar2=None, op0=ALU.mult),
                     reads=[kn(src)], writes=[key])

            Dpairs = [(Dr, Di), (Dr2, Di2)]
            for j in range(16):
                Dr, Di = Dpairs[j % 2]
                to5(Dbr, Dr, 1.0, 'Dbr'); to5(Dbi, Di, 1.0, 'Dbi')
                if j < 15:
                    cmul(Dr, Di, 'dve', Dpairs[(j + 1) % 2][0], Dpairs[(j + 1) % 2][1])
                for d in range(2):
                    for q in range(4):
                        idx = (d * 4 + q) * 16 + j
                        pkt = pk[idx % 2]; pkk = ('pk', idx % 2)
                        for pl in range(4):
                            pair = d * 16 + q * 4 + pl
                            if pl < 3:
                                osl = pkt[32 * pl:32 * pl + 32, 32 * pl:32 * pl + 32]; csl = slice(32 * pl, 32 * pl + 32)
                            else:
                                osl = pkt[64:128, 96:128]; csl = slice(96, 160)
                            S.op('pe', lambda e: e.matmul(osl, lhsT=Dbr[:, d * 4 + q, csl], rhs=Cbr[:, pair, :], start=True, stop=False),
                                 reads=['Dbr', 'Cbr'], writes=[pkk])
                            S.op('pe', lambda e: e.matmul(osl, lhsT=Dbi[:, d * 4 + q, csl], rhs=Cbni[:, pair, :], start=False, stop=True),
                                 reads=['Dbi', 'Cbni'], writes=[pkk])
                        for pl in range(4):
                            if pl < 3:
                                rs_, cs_ = slice(32 * pl, 32 * pl + 32), slice(32 * pl, 32 * pl + 32)
                            else:
                                rs_, cs_ = slice(64, 128), slice(96, 128)
                            S.op('act', lambda e: e.activation(out=Kt_sb[rs_, idx, cs_], in_=pkt[rs_, cs_], func=AF.Copy),
                                 reads=[pkk], writes=['Kt_sb'])
                Et = Es[j % 2]; ek = ('Es', j % 2)
                n_e = 0
                for reim, X in ((0, Dr), (1, Di)):
                    for d in range(2):
                        for q in range(4):
                            pet = pE[n_e % 4]; pek = ('pE', n_e % 4); n_e += 1
                            src = X[:, d * 16 + q * 4:d * 16 + q * 4 + 4, :].rearrange("p a b -> p (a b)")
                            S.op('pe', lambda e: e.transpose(out=pet[:], in_=src, identity=identf[:]), reads=[kn(X), 'identf'], writes=[pek])
                            S.op('act', lambda e: e.activation(out=Et[:, d, q, reim, 0, :], in_=pet[:], func=AF.Copy), reads=[pek], writes=[ek])
                            S.op('dve', lambda e: e.tensor_scalar(out=Et[64:128, d, q, reim, 1, :], in0=pet[64:128, :], scalar1=m3[64:128, 0:1],
                                                                  scalar2=None, op0=ALU.mult), reads=[pek, 'm3'], writes=[ek])
                for d in range(2):
                    S.dma('sp', E_d[:, d, :, j, :, :, :], Et[:, d], reads=[ek], writes=['E_d'])
                cmul(Gr, Gi, 'pool')
                to5(Gbr, Gr, 1.0, 'Gbr'); to5(Gbni, Gi, -1.0, 'Gbni')
                for d in range(2):
                    S.dma('sp', G_d[:, (d * 16 + j) * 2 + 0, :], Gbr[:, d * 4:d * 4 + 4, :].rearrange("p a b -> p (a b)"), reads=['Gbr'], writes=['G_d'])
                    S.dma('sp', G_d[:, (d * 16 + j) * 2 + 1, :], Gbni[:, d * 4:d * 4 + 4, :].rearrange("p a b -> p (a b)"), reads=['Gbni'], writes=['G_d'])
            S.barrier()
        if 's5prep' in dbg:
            dbg_out['Kt'] = dout("dbg_Kt", [128, 128, 128], BF16)
            S.dma('sp', dbg_out['Kt'], Kt_sb[:], reads=['Kt_sb'])
            dbg_out['aL'] = dout("dbg_aL", [128, 2, 32])
            S.dma('sp', dbg_out['aL'][:, 0, :], aL[:], reads=['aL']); S.dma('sp', dbg_out['aL'][:, 1, :], bL[:], reads=['bL'])
            dbg_out['G'] = dout("dbg_G", [128, 64, 640], BF16)
            S.dma('sp', dbg_out['G'], G_d, reads=['G_d'])
            dbg_out['E'] = dout("dbg_E", [128, 2, 4, 16, 2, 2, 128], BF16)
            for d in range(2):
                S.dma('sp', dbg_out['E'][:, d], E_d[:, d], reads=['E_d'])
            return 'stop'
        return s5_main(nc, S, es, sb, ps, din, dscr, dout, dbg, dbg_out, identf, identb, uT_d, yaT_d, nbanks,
                       s5, Kt_sb, aL, bL, diagD, E_d, G_d, H_d, wglu_in, bglu_in)


def s5_main(nc, S, es, sb, ps, din, dscr, dout, dbg, dbg_out, identf, identb, uT_d, yaT_d, nbanks,
            s5, Kt_sb, aL, bL, diagD, E_d, G_d, H_d, wglu_in, bglu_in):
    NCH = 528
    with ExitStack() as pb:
        uTb = [sb("s_uTb%d" % i, [128, 2048], BF16, pb) for i in range(3)]
        Esl = [sb("s_Esl%d" % i, [128, 16, 2, 2, 128], BF16, pb) for i in range(2)]
        Ssb = [[sb("s_Ssb%d_%d" % (d, i), [128, 16, 2, 128], F32, pb) for i in range(2)] for d in range(2)]
        hist = [sb("s_hist%d" % d, [128, 16, 2, 129], F32, pb) for d in range(2)]
        histb = [sb("s_histb%d" % d, [128, 16, 2, 128], BF16, pb) for d in range(2)]
        tP = [sb("s_tP%d" % d, [128, 16, 2], F32, pb) for d in range(2)]
        tQ = [sb("s_tQ%d" % d, [128, 16, 2], F32, pb) for d in range(2)]
        pS = [ps("s_pS%d" % i, [128, 4, 128], F32, pb) for i in range(4)]
        for d in range(2):
            S.op('dve' if d == 0 else 'pool', lambda e: e.memset(hist[d][:], 0.0), writes=[('hist', d)])
        fblocks = [(0, 128), (128, 128), (256, 128), (384, 128), (512, 16)]
        bblocks = [(512, 16), (384, 128), (256, 128), (128, 128), (0, 128)]
        esl_ctr = 0
        ps_ctr = 0
        for step in range(5):
            for d in range(2):
                k0, nk = (fblocks if d == 0 else bblocks)[step]
                Sb = Ssb[d][step % 2]; skey = ('Ssb', d, step % 2)
                base = 0 if d == 0 else 256
                for q in range(4):
                    et = Esl[esl_ctr % 2]; ekey = ('Esl', esl_ctr % 2); esl_ctr += 1
                    S.dma('sp', et[:], E_d[:, d, q], reads=['E_d'], writes=[ekey])
                    ut = uTb[(esl_ctr - 1) % 3]; ukey = ('uTb', (esl_ctr - 1) % 3)
                    S.dma('sp', ut[:, 0:16 * nk], uT_d[q * 128:(q + 1) * 128, base + 16 * k0: base + 16 * k0 + 16 * nk], reads=['uT_d'], writes=[ukey])
                    for pl in range(4):
                        pst = pS[ps_ctr % 4]; pskey = ('pS', ps_ctr % 4); ps_ctr += 1
                        if pl < 3:
                            rsl, alt = slice(32 * pl, 32 * pl + 32), 0
                        else:
                            rsl, alt = slice(64, 128), 1
                        for reim in range(2):
                            for i in range(16):
                                j = 15 - i if d == 0 else i
                                rhs = ut[rsl, i:i + 16 * (nk - 1) + 1:16]
                                S.op('pe', lambda e: e.matmul(pst[:, reim, 0:nk], lhsT=et[rsl, j, reim, alt, :], rhs=rhs,
                                                              start=(i == 0), stop=(i == 15)),
                                     reads=[ekey, ukey], writes=[pskey])
                        S.op('act', lambda e: e.activation(out=Sb[:, q * 4 + pl, :, 0:nk], in_=pst[:, 0:2, 0:nk], func=AF.Copy),
                             reads=[pskey], writes=[skey])
            def chain_ops(d, kk):
                k0, nk = (fblocks if d == 0 else bblocks)[step]
                Sb = Ssb[d][step % 2]; skey = ('Ssb', d, step % 2)
                hk = ('hist', d)
                A2 = aL[:, d * 16:(d + 1) * 16].unsqueeze(2).to_broadcast([128, 16, 2])
                B2 = bL[:, d * 16:(d + 1) * 16].unsqueeze(2).to_broadcast([128, 16, 2])
                H = hist[d]
                if d == 0:
                    cprev, ccur, scol = kk, kk + 1, kk
                else:
                    cprev, ccur, scol = nk - kk, nk - 1 - kk, nk - 1 - kk
                prev = H[:, :, :, cprev]
                P_, Q_ = tP[d], tQ[d]
                eng = 'dve'
                return [
                    lambda: S.op(eng, lambda e: e.tensor_tensor(out=P_[:], in0=prev, in1=A2, op=ALU.mult), reads=[hk, 'aL'], writes=[('tP', d)]),
                    lambda: S.op(eng, lambda e: e.tensor_tensor(out=Q_[:], in0=prev, in1=B2, op=ALU.mult), reads=[hk, 'bL'], writes=[('tQ', d)]),
                    lambda: S.op(eng, lambda e: e.tensor_tensor(out=P_[:], in0=P_[:], in1=Sb[:, :, :, scol], op=ALU.add),
                                 reads=[('tP', d), skey], writes=[('tP', d)]),
                    lambda: S.op(eng, lambda e: e.tensor_tensor(out=H[:, :, 0, ccur], in0=P_[:, :, 0], in1=Q_[:, :, 1], op=ALU.subtract),
                                 reads=[('tP', d), ('tQ', d)], writes=[hk]),
                    lambda: S.op(eng, lambda e: e.tensor_tensor(out=H[:, :, 1, ccur], in0=P_[:, :, 1], in1=Q_[:, :, 0], op=ALU.add),
                                 reads=[('tP', d), ('tQ', d)], writes=[hk]),
                ]
            nks = [(fblocks if d == 0 else bblocks)[step][1] for d in range(2)]
            for kk in range(max(nks)):
                lists = [chain_ops(d, kk) for d in range(2) if kk < nks[d]]
                for oi in range(5):
                    for l in lists:
                        l[oi]()
            for d in range(2):
                eng = 'dve' if d == 0 else 'pool'
                k0, nk = (fblocks if d == 0 else bblocks)[step]
                hk = ('hist', d)
                H = hist[d]
                if d == 0:
                    S.op(eng, lambda e: e.tensor_copy(out=histb[d][:, :, :, 0:nk], in_=H[:, :, :, 1:nk + 1]), reads=[hk], writes=[('histb', d)])
                    S.op(eng, lambda e: e.tensor_copy(out=H[:, :, :, 0], in_=H[:, :, :, nk]), reads=[hk], writes=[hk])
                else:
                    S.op(eng, lambda e: e.tensor_copy(out=histb[d][:, :, :, 0:nk], in_=H[:, :, :, 0:nk]), reads=[hk], writes=[('histb', d)])
                    S.op(eng, lambda e: e.tensor_copy(out=H[:, :, :, 128], in_=H[:, :, :, 0]), reads=[hk], writes=[hk])
                S.dma('pool', H_d[d, :, :, :, k0:k0 + nk], histb[d][:, :, :, 0:nk], reads=[('histb', d)], writes=['H_d'])
        S.barrier()
    if 's5lvl3' in dbg:
        dbg_out['H'] = dout("dbg_H", [2, 128, 16, 2, 544], BF16)
        for d in range(2):
            S.dma('sp', dbg_out['H'][d], H_d[d], reads=['H_d'])
        return 'stop'

    with ExitStack() as pc:
        Gsb = sb("s_Gsb", [128, 64, 640], BF16, pc)
        wglu = sb("s_wglu", [128, 4, 512], BF16, pc)
        bglu = sb("s_bglu", [128, 4], F32, pc)
        ub = [sb("s_ub%d" % i, [128, 4, 544], BF16, pc) for i in range(2)]
        Hf = [sb("s_Hf%d" % i, [128, 16, 2, 32], BF16, pc) for i in range(2)]
        Hb = [sb("s_Hb%d" % i, [128, 16, 2, 32], BF16, pc) for i in range(2)]
        aT = [sb("s_aT%d" % i, [128, 4, 512], BF16, pc) for i in range(2)]
        sig = [sb("s_sig%d" % i, [128, 512], BF16, pc) for i in range(2)]
        yo = [sb("s_yo%d" % i, [128, 512], BF16, pc) for i in range(4)]
        py = [ps("s_py%d" % i, [128, 512], F32, pc) for i in range(2)]
        py4 = [ps("s_py4_%d" % i, [128, 16, 32], F32, pc) for i in range(2)]
        y4 = [sb("s_y4_%d" % i, [128, 512], F32, pc) for i in range(2)]
        ysum = [sb("s_ysum%d" % i, [128, 512], F32, pc) for i in range(2)]
        zlhs = sb("s_zlhs", [128, 128], BF16, pc)
        S.op('dve', lambda e: e.memset(zlhs[:], 0.0), writes=['zlhs'])
        pg = [ps("s_pg%d" % i, [128, 512], F32, pc) for i in range(2)]
        for c in range(4):
            S.dma('sp', Gsb[:, c * 16:(c + 1) * 16, :], G_d[:, c * 16:(c + 1) * 16, :], reads=['G_d'], writes=[('Gsb', c)])
        gkeys = [('Gsb', c) for c in range(4)]
        for q in range(4):
            S.dma('pool', wglu[:, q, :], wglu_in[q * 128:(q + 1) * 128, :], writes=['wglu'])
        S.dma('sp', bglu[:], bglu_in, writes=['bglu'])
        yo_ctr = 0
        for n in range(nbanks):
            i2 = n % 2
            e0 = 256 + 512 * n
            S.dma('sp', ub[i2][:], uT_d[:, e0 - 16:e0 + 528].rearrange("(q p) t -> p q t", p=128), reads=['uT_d'], writes=[('ub', i2)])
            kf = 16 + 32 * n
            kb = 32 * n
            S.dma('sp', Hf[i2][:], H_d[0, :, :, :, kf - 1:kf + 31], reads=['H_d'], writes=[('Hf', i2)])
            S.dma('sp', Hb[i2][:], H_d[1, :, :, :, kb + 1:kb + 33], reads=['H_d'], writes=[('Hb', i2)])
            for q in range(4):
                pyt = py[q % 2]; pk_ = ('py', q % 2)
                p4 = py4[q % 2]; p4k = ('py4', q % 2)
                pyv = pyt[:].rearrange("p (k j) -> p k j", j=16)
                uc = ub[i2][:, q, 16:528]
                ucv = uc.rearrange("p (k j) -> p k j", j=16)
                rd = [('ub', i2), 'Kt_sb', 'diagD']
                S.op('pe', lambda e: e.matmul(pyt[:], lhsT=diagD[:, q, :], rhs=uc, start=True, stop=False), reads=rd, writes=[pk_])
                S.op('pe', lambda e: e.matmul(pyt[:], lhsT=Kt_sb[:, (0 * 4 + q) * 16 + 0, :], rhs=uc, start=False, stop=False), reads=rd, writes=[pk_])
                for tau in range(1, 16):
                    S.op('pe', lambda e: e.matmul(pyv[:, :, tau:16], lhsT=Kt_sb[:, (0 * 4 + q) * 16 + tau, :], rhs=ucv[:, :, 0:16 - tau],
                                                  start=False, stop=False), reads=rd, writes=[pk_])
                S.op('pe', lambda e: e.matmul(pyt[:], lhsT=Kt_sb[:, (1 * 4 + q) * 16 + 0, :], rhs=uc, start=False, stop=False), reads=rd, writes=[pk_])
                for tau in range(1, 16):
                    S.op('pe', lambda e: e.matmul(pyv[:, :, 0:16 - tau], lhsT=Kt_sb[:, (1 * 4 + q) * 16 + tau, :], rhs=ucv[:, :, tau:16],
                                                  start=False, stop=(tau == 15)), reads=rd, writes=[pk_])
                S.op('pe', lambda e: e.matmul(p4[:].rearrange("p j k -> p (j k)"), lhsT=zlhs[:], rhs=uc, start=True, stop=False),
                     reads=['zlhs', ('ub', i2)], writes=[p4k])
                for pl in range(4):
                    pair = q * 4 + pl
                    for d in range(2):
                        Hx = Hf[i2] if d == 0 else Hb[i2]
                        hkey = ('Hf', i2) if d == 0 else ('Hb', i2)
                        for j in range(16):
                            m = j + 1 if d == 0 else 16 - j
                            if pl < 3:
                                osl = p4[32 * pl:32 * pl + 32, j, :]; csl = slice(q * 160 + 32 * pl, q * 160 + 32 * pl + 32)
                            else:
                                osl = p4[64:128, j, :]; csl = slice(q * 160 + 96, q * 160 + 160)
                            for reim in range(2):
                                last = (pl == 3 and d == 1 and j == 15 and reim == 1)
                                S.op('pe', lambda e: e.matmul(osl, lhsT=Gsb[:, (d * 16 + m - 1) * 2 + reim, csl], rhs=Hx[:, pair, reim, :],
                                                              start=False, stop=last), reads=gkeys + [hkey], writes=[p4k])
                S.op('act', lambda e: e.activation(out=y4[q % 2][:].rearrange("p (k j) -> p j k", j=16), in_=p4[:], func=AF.Copy),
                     reads=[p4k], writes=[('y4', q % 2)])
                S.op('dve', lambda e: e.tensor_tensor(out=ysum[q % 2][:], in0=pyt[:], in1=y4[q % 2][:], op=ALU.add),
                     reads=[pk_, ('y4', q % 2)], writes=[('ysum', q % 2)])
                S.op('act', lambda e: e.activation(out=aT[i2][:, q, :], in_=ysum[q % 2][:], func=AF.Gelu), reads=[('ysum', q % 2)], writes=[('aT', i2, q)])
            for oc in range(4):
                pgt = pg[oc % 2]; pgk = ('pg', oc % 2)
                for q in range(4):
                    S.op('pe', lambda e: e.matmul(pgt[:], lhsT=wglu[:, q, oc * 128:(oc + 1) * 128], rhs=aT[i2][:, q, :],
                                                  start=(q == 0), stop=(q == 3)), reads=['wglu', ('aT', i2, q)], writes=[pgk])
                sg = sig[oc % 2]; sgk = ('sig', oc % 2)
                S.op('act', lambda e: e.activation(out=sg[:], in_=pgt[:], func=AF.Sigmoid, bias=bglu[:, oc:oc + 1]),
                     reads=[pgk, 'bglu'], writes=[sgk])
                yt = yo[yo_ctr % 4]; yk = ('yo', yo_ctr % 4); yo_ctr += 1
                S.op('dve', lambda e: e.tensor_tensor(out=yt[:], in0=aT[i2][:, oc, :], in1=sg[:], op=ALU.mult),
                     reads=[('aT', i2, oc), sgk], writes=[yk])
                S.dma('pool', yaT_d[oc * 128:(oc + 1) * 128, 512 * n:512 * n + 512], yt[:], reads=[yk], writes=['yaT_d'])
        S.barrier()
    if 's5' in dbg:
        dbg_out['yaT'] = dout("dbg_yaT", [512, 8192], BF16)
        for r0 in range(0, 512, 128):
            S.dma('sp', dbg_out['yaT'][r0:r0 + 128, :], yaT_d[r0:r0 + 128, :], reads=['yaT_d'])
    return 'ok'

NEG = -30000.0


def na_phase(nc, S, es, sb, ps, din, dscr, dout, dbg, dbg_out, identb, qT_d, kT_d, v_d, ybT_d, nblocks=16):
    rpbT_in = din("rpbT", [128, 8 * 14 * 64])
    maskT_in = din("maskT", [128, 64])
    with ExitStack() as pa:
        Tb = sb("n_Tb", [128, 8, 14, 64], F32, pa)
        mk = sb("n_mk", [128, 64], F32, pa)
        kctx = sb("n_kctx", [128, 4, 256], BF16, pa)
        vctx = sb("n_vctx", [128, 2, 8, 65], BF16, pa)
        qb = [sb("n_qb%d" % i, [128, 4, 512], BF16, pa) for i in range(2)]
        kw = [sb("n_kw%d" % i, [128, 4, 960], BF16, pa) for i in range(2)]
        vw = [sb("n_vw%d" % i, [128, 15, 8, 65], BF16, pa) for i in range(2)]
        sS = [sb("n_sS%d" % i, [128, 4, 64], F32, pa) for i in range(3)]
        P = [sb("n_Pn%d" % i, [128, 6, 64], BF16, pa) for i in range(4)]
        rc = [sb("n_rc%d" % i, [128, 8], F32, pa) for i in range(2)]
        ybt = [sb("n_ybt%d" % i, [128, 8, 64], BF16, pa) for i in range(2)]
        ybT = [sb("n_ybT%d" % i, [128, 4, 512], BF16, pa) for i in range(2)]
        pS = [ps("n_pS%d" % i, [128, 6, 64], F32, pa) for i in range(3)]
        pO = [ps("n_pO%d" % i, [128, 4, 65], F32, pa) for i in range(4)]
        pT = ps("n_pTn", [128, 4, 128], BF16, pa)

        S.dma('sp', Tb[:].rearrange("p h a q -> p (h a q)"), rpbT_in, writes=['Tb'])
        S.dma('sp', mk[:], maskT_in, writes=['mk'])
        S.dma('sp', kctx[:], kT_d[:, 0:256].rearrange("(c p) t -> p c t", p=128), reads=['kT_d'], writes=['kctx'])
        S.dma('sp', vctx[:], v_d[0:256].rearrange("(m p) h d -> p m h d", p=128), reads=['v_d'], writes=['vctx'])
        S.op('dve', lambda e: e.tensor_tensor(out=Tb[:].rearrange("p h a q -> p (h a) q"), in0=Tb[:].rearrange("p h a q -> p (h a) q"),
                                              in1=mk[:].unsqueeze(1).to_broadcast([128, 112, 64]), op=ALU.add),
             reads=['Tb', 'mk'], writes=['Tb'])
        item = 0
        pend = []
        for b in range(nblocks):
            i2 = b % 2
            r0 = 8 * b
            lo = min(max(r0 - 4, 0), 120)
            hi = min(max(r0 + 7 - 4, 0), 120) + 8
            nrows = hi - lo
            S.dma('sp', qb[i2][:], qT_d[:, 512 * b:512 * b + 512].rearrange("(c p) t -> p c t", p=128), reads=['qT_d'], writes=[('qb', i2)])
            S.dma('sp', kw[i2][:, :, 0:64 * nrows], kT_d[:, 256 + 64 * lo:256 + 64 * hi].rearrange("(c p) t -> p c t", p=128),
                  reads=['kT_d'], writes=[('kw', i2)])
            for s in range(nrows - 1):
                S.dma('sp', vw[i2][:, s], v_d[256 + 64 * (lo + s):256 + 64 * (lo + s) + 128], reads=['v_d'], writes=[('vw', i2)])
            for rr in range(8):
                r = r0 + rr
                krs = min(max(r - 4, 0), 120)
                a0 = krs - r + 7
                half = 64 * (r % 2)
                pp = (r // 2) % 2
                for h in range(8):
                    hc, hb = h // 2, 64 * (h % 2)
                    it = item % 3
                    ip = item % 4
                    item += 1
                    pst = pS[it]; psk = ('pS', it)
                    for c in range(4):
                        kc0 = 64 * (krs + 2 * c - lo)
                        S.op('pe', lambda e: e.matmul(pst[:, c, :], lhsT=kw[i2][hb:hb + 64, hc, kc0:kc0 + 128],
                                                      rhs=qb[i2][hb:hb + 64, hc, 64 * rr:64 * rr + 64], start=True, stop=True),
                             reads=[('kw', i2), ('qb', i2)], writes=[psk])
                    for c in range(2):
                        S.op('pe', lambda e: e.matmul(pst[:, 4 + c, :], lhsT=kctx[hb:hb + 64, hc, 128 * c:128 * c + 128],
                                                      rhs=qb[i2][hb:hb + 64, hc, 64 * rr:64 * rr + 64], start=True, stop=True),
                             reads=['kctx', ('qb', i2)], writes=[psk])
                    S.op('dve', lambda e: e.tensor_tensor(out=sS[it][:], in0=pst[:, 0:4, :], in1=Tb[:, h, a0:a0 + 7:2, :], op=ALU.add),
                         reads=[psk, 'Tb'], writes=[('sS', it)])
                    S.op('act', lambda e: e.activation(out=P[ip][:, 0:4, :], in_=sS[it][:], func=AF.Exp), reads=[('sS', it)], writes=[('P', ip)])
                    S.op('act', lambda e: e.activation(out=P[ip][:, 4:6, :], in_=pst[:, 4:6, :], func=AF.Exp), reads=[psk], writes=[('P', ip)])
                    if len(pend) == 2:
                        pend.pop(0)()
                    def mk_pv(it=ip, i2=i2, krs=krs, lo=lo, h=h, half=half, pp=pp, r=r, rr=rr, b=b):
                        def pv():
                            pot = pO[pp * 2 + h // 4]; pok = ('pO', pp * 2 + h // 4)
                            for c in range(6):
                                if c < 4:
                                    rhs = vw[i2][:, krs + 2 * c - lo, h, :]
                                    rk = ('vw', i2)
                                else:
                                    rhs = vctx[:, c - 4, h, :]
                                    rk = 'vctx'
                                S.op('pe', lambda e: e.matmul(pot[half:half + 64, h % 4, :], lhsT=P[it][:, c, :], rhs=rhs,
                                                              start=(c == 0), stop=(c == 5)), reads=[('P', it), rk], writes=[pok])
                            if h % 4 == 3:
                                hh = h // 4
                                yk = ('ybt', pp)
                                S.op('dve', lambda e: e.reciprocal(out=rc[pp][half:half + 64, 4 * hh:4 * hh + 4], in_=pot[half:half + 64, :, 64]),
                                     reads=[pok], writes=[('rc', pp)])
                                S.op('dve', lambda e: e.tensor_tensor(out=ybt[pp][half:half + 64, 4 * hh:4 * hh + 4, :], in0=pot[half:half + 64, :, 0:64],
                                                                      in1=rc[pp][half:half + 64, 4 * hh:4 * hh + 4].unsqueeze(2).to_broadcast([64, 4, 64]),
                                                                      op=ALU.mult), reads=[pok, ('rc', pp)], writes=[yk])
                            if h == 7 and r % 2 == 1:
                                tk = ('ybT', b % 2)
                                for c4 in range(4):
                                    S.op('pe', lambda e: e.transpose(out=pT[:, c4, :], in_=ybt[pp][:, 2 * c4:2 * c4 + 2, :].rearrange("p a d -> p (a d)"),
                                                                     identity=identb[:]), reads=[('ybt', pp), 'identb'], writes=['pTn'])
                                S.op('act', lambda e: e.activation(out=ybT[b % 2][:, :, 64 * (rr - 1):64 * (rr - 1) + 128], in_=pT[:], func=AF.Copy),
                                     reads=['pTn'], writes=[tk])
                                if rr == 7:
                                    S.dma('act', ybT_d[:, 512 * b:512 * b + 512].rearrange("(c p) t -> p c t", p=128), ybT[b % 2][:],
                                          reads=[tk], writes=['ybT_d'])
                        return pv
                    pend.append(mk_pv())
        for p_ in pend:
            p_()
        S.barrier()
    if 'na' in dbg:
        dbg_out['ybT'] = dout("dbg_ybT", [512, 8192], BF16)
        for r0 in range(0, 512, 128):
            S.dma('sp', dbg_out['ybT'][r0:r0 + 128, :], ybT_d[r0:r0 + 128, :], reads=['ybT_d'])


def na_host(inp):
    rpb = inp['na_rpb'][0].astype(np.float32)
    i = np.arange(2)[:, None, None, None]
    kc = np.arange(64)[None, :, None, None]
    a = np.arange(14)[None, None, :, None]
    qc = np.arange(64)[None, None, None, :]
    dcol = np.clip(kc - qc + 15, 0, 30)
    drow = a + i
    T = rpb[:, drow, dcol]
    T = np.broadcast_to(T, (8, 2, 64, 14, 64)).transpose(1, 2, 0, 3, 4).reshape(128, 8 * 14 * 64)
    q = np.arange(64)
    qcs = np.clip(q - 8, 0, 48)
    k = np.arange(64)[:, None]
    inw = (k >= qcs[None, :]) & (k < qcs[None, :] + 16)
    m = np.where(inw, 0.0, NEG).astype(np.float32)
    m = np.concatenate([m, m], axis=0)
    return {'rpbT': np.ascontiguousarray(T.astype(np.float32)), 'maskT': np.ascontiguousarray(m)}

D = 1024


def merge_phase(nc, S, es, sb, ps, din, dscr, dout, dbg, dbg_out, identf, identb, epsc, bc_g1, bc_mul2, bc_add2,
                xcat, out, yaT_d, ybT_d, sgaT_d, sgbT_d, h2_d, aff_d, affT_d, nblocks=16):
    wpa_in = din("w_proj_a", [512, D]); wpb_in = din("w_proj_b", [512, D]); wout_in = din("w_out", [D, D])
    wr_in = din("w_routerT", [128, 8, 16])
    with ExitStack() as pa:
        wpa = sb("g_wpa", [128, 4, D], BF16, pa); wpb = sb("g_wpb", [128, 4, D], BF16, pa)
        wout = sb("g_wout", [128, 8, D], BF16, pa)
        wr = sb("g_wr", [128, 8, 16], BF16, pa)
        ya = [sb("g_mya%d" % i, [128, 4, 512], BF16, pa) for i in range(2)]
        yb = [sb("g_myb%d" % i, [128, 4, 512], BF16, pa) for i in range(2)]
        sga = [sb("g_msga%d" % i, [128, 8, 512], BF16, pa) for i in range(2)]
        sgb = [sb("g_msgb%d" % i, [128, 8, 512], BF16, pa) for i in range(2)]
        mT = [sb("g_mT%d" % i, [128, 8, 512], BF16, pa) for i in range(2)]
        t1 = [sb("g_mt1_%d" % i, [128, 512], F32, pa) for i in range(2)]
        t2 = [sb("g_mt2_%d" % i, [128, 512], F32, pa) for i in range(2)]
        xt = [sb("g_mxt%d" % i, [128, D], F32, pa) for i in range(2)]
        x1 = [sb("g_mx1_%d" % i, [128, D], F32, pa) for i in range(3)]
        h2 = [sb("g_mh2_%d" % i, [128, D], F32, pa) for i in range(3)]
        h2b = [sb("g_mh2b_%d" % i, [128, D], BF16, pa) for i in range(3)]
        h2T = [sb("g_mh2T_%d" % i, [128, 8, 128], BF16, pa) for i in range(3)]
        junk = sb("g_mjunk", [128, D], BF16, pa)
        st = [sb("g_mst%d" % i, [128, 8], F32, pa) for i in range(3)]
        lg = [sb("g_mlg%d" % i, [128, 16], F32, pa) for i in range(3)]
        af = [sb("g_maf%d" % i, [128, 16], F32, pa) for i in range(3)]
        A16 = sb("g_A16", [16, 8192], F32, pa)
        pab = [ps("g_mpab%d" % i, [128, 512], F32, pa) for i in range(4)]
        pyo = ps("g_mpyo", [128, D], F32, pa)
        phT = ps("g_mphT", [128, 8, 128], BF16, pa)
        pmisc = ps("g_mpmisc", [128, 256], F32, pa)
        plg = pmisc[:, 0:16]
        paT = pmisc[0:16, 128:256]

        for q in range(4):
            S.dma('pool', wpa[:, q, :], wpa_in[q * 128:(q + 1) * 128, :], writes=['wpa'])
            S.dma('pool', wpb[:, q, :], wpb_in[q * 128:(q + 1) * 128, :], writes=['wpb'])
        for dc in range(8):
            S.dma('pool', wout[:, dc, :], wout_in[dc * 128:(dc + 1) * 128, :], writes=['wout'])
        S.dma('pool', wr[:], wr_in, writes=['wr'])
        tile_ctr = 0
        pend = []
        for b in range(nblocks):
            i2 = b % 2
            cs = slice(512 * b, 512 * b + 512)
            S.dma('sp', ya[i2][:], yaT_d[:, cs].rearrange("(c p) t -> p c t", p=128), reads=['yaT_d'], writes=[('ya', i2)])
            S.dma('sp', yb[i2][:], ybT_d[:, cs].rearrange("(c p) t -> p c t", p=128), reads=['ybT_d'], writes=[('yb', i2)])
            S.dma('sp', sga[i2][:], sgaT_d[:, cs].rearrange("(c p) t -> p c t", p=128), reads=['sgaT_d'], writes=[('sga', i2)])
            S.dma('sp', sgb[i2][:], sgbT_d[:, cs].rearrange("(c p) t -> p c t", p=128), reads=['sgbT_d'], writes=[('sgb', i2)])
            for dc in range(8):
                j2 = dc % 2
                pA_, pB_ = pab[2 * j2], pab[2 * j2 + 1]
                for q in range(4):
                    S.op('pe', lambda e: e.matmul(pA_[:], lhsT=wpa[:, q, dc * 128:(dc + 1) * 128], rhs=ya[i2][:, q, :],
                                                  start=(q == 0), stop=(q == 3)), reads=['wpa', ('ya', i2)], writes=[('pA', j2)])
                for q in range(4):
                    S.op('pe', lambda e: e.matmul(pB_[:], lhsT=wpb[:, q, dc * 128:(dc + 1) * 128], rhs=yb[i2][:, q, :],
                                                  start=(q == 0), stop=(q == 3)), reads=['wpb', ('yb', i2)], writes=[('pB', j2)])
                S.op('dve', lambda e: e.tensor_tensor(out=t1[j2][:], in0=pA_[:], in1=sga[i2][:, dc, :], op=ALU.mult),
                     reads=[('pA', j2), ('sga', i2)], writes=[('t1', j2)])
                S.op('dve', lambda e: e.tensor_tensor(out=t2[j2][:], in0=pB_[:], in1=sgb[i2][:, dc, :], op=ALU.mult),
                     reads=[('pB', j2), ('sgb', i2)], writes=[('t2', j2)])
                S.op('pool', lambda e: e.tensor_tensor(out=mT[i2][:, dc, :], in0=t1[j2][:], in1=t2[j2][:], op=ALU.add),
                     reads=[('t1', j2), ('t2', j2)], writes=[('mT', i2)])
            for ti in range(4):
                k2 = tile_ctr % 3
                kx = tile_ctr % 2
                tile_ctr += 1
                tok0 = 512 * b + 128 * ti
                S.dma('sp', xt[kx][:], xcat[256 + tok0:256 + tok0 + 128, :], writes=[('xt', kx)])
                for hf in range(2):
                    for dc in range(8):
                        S.op('pe', lambda e: e.matmul(pyo[:, hf * 512:(hf + 1) * 512], lhsT=mT[i2][:, dc, ti * 128:(ti + 1) * 128],
                                                      rhs=wout[:, dc, hf * 512:(hf + 1) * 512], start=(dc == 0), stop=(dc == 7)),
                             reads=[('mT', i2), 'wout'], writes=['pyo'])
                S.op('dve', lambda e: e.tensor_tensor(out=x1[k2][:], in0=pyo[:], in1=bc_g1[:], op=ALU.mult),
                     reads=['pyo', 'bc_g1'], writes=[('x1', k2)])
                S.op('dve', lambda e: e.tensor_tensor(out=x1[k2][:], in0=x1[k2][:], in1=xt[kx][:], op=ALU.add),
                     reads=[('x1', k2), ('xt', kx)], writes=[('x1', k2)])
                S.dma('pool', out[tok0:tok0 + 128, :], x1[k2][:], reads=[('x1', k2)], writes=['out'])
                s_ = st[k2]; sk = ('st', k2)
                S.op('act', lambda e: e.activation(out=junk[:], in_=x1[k2][:], func=AF.Square, accum_out=s_[:, 0:1]),
                     reads=[('x1', k2)], writes=['mjunk', sk])
                S.op('act', lambda e: e.activation(out=s_[:, 6:7], in_=s_[:, 0:1], func=AF.Ln, scale=1.0 / D, bias=epsc[:, 0:1]),
                     reads=[sk, 'epsc'], writes=[sk])
                S.op('act', lambda e: e.activation(out=s_[:, 1:2], in_=s_[:, 6:7], func=AF.Exp, scale=-0.5), reads=[sk], writes=[sk])
                S.op('dve', lambda e: e.scalar_tensor_tensor(out=h2[k2][:], in0=x1[k2][:], scalar=s_[:, 1:2], in1=bc_mul2[:],
                                                             op0=ALU.mult, op1=ALU.mult), reads=[('x1', k2), sk, 'bc_mul2'], writes=[('h2', k2)])
                S.op('dve', lambda e: e.tensor_tensor(out=h2[k2][:], in0=h2[k2][:], in1=bc_add2[:], op=ALU.add),
                     reads=[('h2', k2), 'bc_add2'], writes=[('h2', k2)])
                S.op('act', lambda e: e.activation(out=h2b[k2][:], in_=h2[k2][:], func=AF.Copy), reads=[('h2', k2)], writes=[('h2b', k2)])
                S.dma('act', h2_d[tok0:tok0 + 128, :], h2b[k2][:], reads=[('h2b', k2)], writes=['h2_d'])
                def mk_tail(k2=k2, tok0=tok0, s_=s_, sk=sk):
                    def tail():
                        for dc in range(8):
                            S.op('pe', lambda e: e.transpose(out=phT[:, dc, :], in_=h2b[k2][:, dc * 128:(dc + 1) * 128], identity=identb[:]),
                                 reads=[('h2b', k2), 'identb'], writes=['phT'])
                        S.op('act', lambda e: e.activation(out=h2T[k2][:], in_=phT[:], func=AF.Copy), reads=['phT'], writes=[('h2T', k2)])
                        for dc in range(8):
                            S.op('pe', lambda e: e.matmul(plg, lhsT=h2T[k2][:, dc, :], rhs=wr[:, dc, :], start=(dc == 0), stop=(dc == 7)),
                                 reads=[('h2T', k2), 'wr'], writes=['plg'])
                        S.op('dve', lambda e: e.tensor_copy(out=lg[k2][:], in_=plg), reads=['plg'], writes=[('lg', k2)])
                        S.op('dve', lambda e: e.reduce_max(out=s_[:, 2:3], in_=lg[k2][:], axis=AX.X), reads=[('lg', k2)], writes=[sk])
                        S.op('dve', lambda e: e.tensor_scalar(out=s_[:, 3:4], in0=s_[:, 2:3], scalar1=-1.0, scalar2=None, op0=ALU.mult),
                             reads=[sk], writes=[sk])
                        S.op('act', lambda e: e.activation(out=af[k2][:], in_=lg[k2][:], func=AF.Exp, bias=s_[:, 3:4], accum_out=s_[:, 4:5]),
                             reads=[('lg', k2), sk], writes=[('af', k2), sk])
                        S.op('dve', lambda e: e.reciprocal(out=s_[:, 5:6], in_=s_[:, 4:5]), reads=[sk], writes=[sk])
                        S.op('dve', lambda e: e.tensor_scalar(out=af[k2][:], in0=af[k2][:], scalar1=s_[:, 5:6], scalar2=None, op0=ALU.mult),
                             reads=[('af', k2), sk], writes=[('af', k2)])
                        S.dma('pool', aff_d[tok0:tok0 + 128, :], af[k2][:], reads=[('af', k2)], writes=['aff_d'])
                        S.op('pe', lambda e: e.transpose(out=paT, in_=af[k2][:], identity=identf[:]), reads=[('af', k2), 'identf'], writes=['paT'])
                        S.op('act', lambda e: e.activation(out=A16[:, tok0:tok0 + 128], in_=paT, func=AF.Copy), reads=['paT'], writes=['A16'])
                    return tail
                if len(pend) == 2:
                    pend.pop(0)()
                pend.append(mk_tail())
        for t_ in pend:
            t_()
        S.dma('sp', affT_d, A16[:], reads=['A16'], writes=['affT_d'])
        S.barrier()
        if 'mg2' in dbg:
            for nm, t, shp, dt in (('bcg1', bc_g1, [128, D], F32), ('mT0', mT[(nblocks - 1) % 2], [128, 8, 512], BF16), ('t1', t1[1], [128, 512], F32),
                                   ('sga0', sga[(nblocks - 1) % 2], [128, 8, 512], BF16), ('ya0', ya[(nblocks - 1) % 2], [128, 4, 512], BF16),
                                   ('yb0', yb[(nblocks - 1) % 2], [128, 4, 512], BF16), ('wpa', wpa, [128, 4, D], BF16), ('x1t', x1[1], [128, D], F32),
                                   ('xt', xt[1], [128, D], F32)):
                dbg_out[nm] = dout("dbg_" + nm, shp, dt)
                S.dma('sp', dbg_out[nm], t[:], reads=[])
    if 'mg' in dbg:
        dbg_out['aff'] = dout("dbg_aff", [8192, 16])
        S.dma('sp', dbg_out['aff'], aff_d, reads=['aff_d'])
        dbg_out['affT'] = dout("dbg_affT", [16, 8192])
        S.dma('sp', dbg_out['affT'], affT_d, reads=['affT_d'])
        dbg_out['h2'] = dout("dbg_h2", [8192, D], BF16)
        for r0 in range(0, 8192, 1024):
            S.dma('sp', dbg_out['h2'][r0:r0 + 1024, :], h2_d[r0:r0 + 1024, :], reads=['h2_d'])

D = 1024
CAP = 1024
NT = 8192


def moe_phase(nc, S, es, sb, ps, din, dscr, dout, dbg, dbg_out, identf, identb, bc_g2, out, h2_d, aff_d, affT_d, nexp=16):
    wg_in = din("w_e_gate", [16, D, D]); wu_in = din("w_e_up", [16, D, D]); wd_in = din("w_e_down", [16, D, D])
    m16_in = din("m16", [128, 128])
    with ExitStack() as pm:
        cum_d = dscr("cum_d", [16, NT], F32)
        slotv = sb("e_slotv", [128, 8], F32, pm)
        thr_dbg = sb("e_thr", [128, 1], F32, pm)
        S.op('pool', lambda e: e.iota(slotv[:], pattern=[[128, 8]], base=0, channel_multiplier=1, allow_small_or_imprecise_dtypes=True),
             writes=['slotv'])
        with ExitStack() as pa:
            A128 = sb("e_A128", [128, 1024], F32, pa)
            A16 = sb("e_A16", [16, NT], F32, pa)
            C16 = sb("e_C16", [16, NT], F32, pa)
            M16 = sb("e_M16", [128, 128], F32, pa)
            junk = sb("e_junk", [128, 1024], BF16, pa)
            lo = sb("e_lo", [128, 1], F32, pa); mid = sb("e_mid", [128, 1], F32, pa)
            cnt = sb("e_cnt", [128, 1], F32, pa); g = sb("e_g", [128, 1], F32, pa)
            one16 = sb("e_one16", [16, 1], F32, pa)
            ptot = ps("e_ptot", [128, 1], F32, pa)
            for s in range(8):
                S.dma('sp', A128[16 * s:16 * s + 16, :], affT_d[:, 1024 * s:1024 * s + 1024], reads=['affT_d'], writes=['A128'])
            S.dma('sp', A16[:], affT_d, reads=['affT_d'], writes=['A16'])
            S.dma('sp', M16[:], m16_in, writes=['M16'])
            S.op('dve', lambda e: e.memset(lo[:], 0.0), writes=['lo'])
            S.op('dve', lambda e: e.memset(one16[:], 1.0), writes=['one16'])
            for k in range(30):
                hk = 2.0 ** -(k + 1)
                S.op('dve', lambda e: e.tensor_scalar(out=mid[:], in0=lo[:], scalar1=hk, scalar2=None, op0=ALU.add), reads=['lo'], writes=['mid'])
                S.op('dve', lambda e: e.tensor_scalar(out=junk[:], in0=A128[:], scalar1=mid[:, 0:1], scalar2=0.0, op0=ALU.is_gt, op1=ALU.add,
                                                      accum_out=cnt[:]), reads=['A128', 'mid'], writes=['junk', 'cnt'])
                S.op('pe', lambda e: e.matmul(ptot[:], lhsT=M16[:], rhs=cnt[:], start=True, stop=True), reads=['M16', 'cnt'], writes=['ptot'])
                S.op('dve', lambda e: e.tensor_scalar(out=g[:], in0=ptot[:], scalar1=float(CAP), scalar2=hk, op0=ALU.is_ge, op1=ALU.mult),
                     reads=['ptot'], writes=['g'])
                S.op('dve', lambda e: e.tensor_tensor(out=lo[:], in0=lo[:], in1=g[:], op=ALU.add), reads=['lo', 'g'], writes=['lo'])
            S.op('dve', lambda e: e.tensor_copy(out=thr_dbg[:], in_=lo[:]), reads=['lo'], writes=['thr'])
            S.op('dve', lambda e: e.tensor_scalar(out=A16[:], in0=A16[:], scalar1=lo[0:16, 0:1], scalar2=None, op0=ALU.is_gt),
                 reads=['A16', 'lo'], writes=['A16'])
            S.op('dve', lambda e: e.tensor_tensor_scan(out=C16[:], data0=one16[:, 0:1].to_broadcast([16, NT]), data1=A16[:], initial=0.0,
                                                       op0=ALU.mult, op1=ALU.add), reads=['A16', 'one16'], writes=['C16'])
            S.dma('sp', cum_d, C16[:], reads=['C16'], writes=['cum_d'])
            S.barrier()
        if 'moe_thr' in dbg:
            dbg_out['thr'] = dout("dbg_thr", [128, 1])
            S.dma('sp', dbg_out['thr'], thr_dbg[:], reads=['thr'])
        with ExitStack() as pe_:
            Wg = [sb("e_Wg%d" % i, [128, 8, D], BF16, pe_) for i in range(2)]
            Wu = [sb("e_Wu%d" % i, [128, 8, D], BF16, pe_) for i in range(2)]
            Wd = [sb("e_Wd%d" % i, [128, 8, D], BF16, pe_) for i in range(2)]
            xe = [sb("e_xe%d" % i, [128, D], BF16, pe_) for i in range(8)]
            ag = [[sb("e_ag%d_%d" % (p_, i), [128, 16], F32, pe_) for i in range(8)] for p_ in range(2)]
            cq = [sb("e_cq%d" % i, [128, 2048], F32, pe_) for i in range(2)]
            junkq = sb("e_junkq", [128, 2048], BF16, pe_)
            acc = [sb("e_acc%d" % i, [128, 4, 8], F32, pe_) for i in range(3)]
            idxs = sb("e_idxs", [128, 8], F32, pe_)
            idxi = [sb("e_idxi%d" % i, [128, 8], I32, pe_) for i in range(3)]
            xeT = sb("e_xeT", [128, 8, CAP], BF16, pe_)
            hidT = sb("e_hidT", [128, 8, CAP], BF16, pe_)
            sg = [sb("e_sg%d" % i, [128, 512], F32, pe_) for i in range(2)]
            ye = [sb("e_ye%d" % i, [128, D], F32, pe_) for i in range(2)]
            pGU = [ps("e_pGU%d" % i, [128, 512], F32, pe_) for i in range(3)]
            pY = ps("e_pY", [128, D], F32, pe_)
            pT = ps("e_pT", [128, 8, 128], BF16, pe_)
            pTi = pT[:].bitcast(F32) if False else None

            def load_w(e):
                i2 = e % 2
                for dc in range(8):
                    S.dma('pool', Wg[i2][:, dc, :], wg_in[e, dc * 128:(dc + 1) * 128, :], writes=[('Wg', i2)])
                    S.dma('pool', Wu[i2][:, dc, :], wu_in[e, dc * 128:(dc + 1) * 128, :], writes=[('Wu', i2)])
                for dc in range(8):
                    S.dma('pool', Wd[i2][:, dc, :], wd_in[e, dc * 128:(dc + 1) * 128, :], writes=[('Wd', i2)])

            def load_cq(n):
                e_, qd = n // 4, n % 4
                if e_ >= nexp:
                    return
                S.dma('sp', cq[n % 2][:], cum_d[e_:e_ + 1, qd * 2048:(qd + 1) * 2048].to_broadcast([128, 2048]),
                      reads=['cum_d'], writes=[('cq', n % 2)])

            def idx_piece(e, piece):
                j3 = e % 3
                n = e * 4 + piece // 2
                if piece % 2 == 0:
                    if n == 0:
                        load_cq(0)
                    load_cq(n + 1)
                for sc in range(4 * (piece % 2), 4 * (piece % 2) + 4):
                    S.op('dve', lambda e_: e_.tensor_scalar(out=junkq[:], in0=cq[n % 2][:], scalar1=slotv[:, sc:sc + 1], scalar2=0.0,
                                                            op0=ALU.is_le, op1=ALU.add, accum_out=acc[j3][:, piece // 2, sc:sc + 1]),
                         reads=[('cq', n % 2), 'slotv'], writes=['junkq', ('acc', j3)])
                if piece == 7:
                    S.op('dve', lambda e_: e_.tensor_tensor(out=idxs[:], in0=acc[j3][:, 0, :], in1=acc[j3][:, 1, :], op=ALU.add),
                         reads=[('acc', j3)], writes=['idxs'])
                    S.op('dve', lambda e_: e_.tensor_tensor(out=idxs[:], in0=idxs[:], in1=acc[j3][:, 2, :], op=ALU.add),
                         reads=[('acc', j3), 'idxs'], writes=['idxs'])
                    S.op('dve', lambda e_: e_.tensor_tensor(out=idxs[:], in0=idxs[:], in1=acc[j3][:, 3, :], op=ALU.add),
                         reads=[('acc', j3), 'idxs'], writes=['idxs'])
                    S.op('dve', lambda e_: e_.tensor_copy(out=idxi[j3][:], in_=idxs[:]), reads=['idxs'], writes=[('idxi', j3)])

            def gather(e):
                j2 = e % 3
                for sc in range(8):
                    S.custom_dma('pool', lambda e_: e_.indirect_dma_start(out=xe[sc][:, :], out_offset=None, in_=h2_d[:, :],
                                 in_offset=bass.IndirectOffsetOnAxis(ap=idxi[j2][:, sc:sc + 1], axis=0)),
                                 reads=[('idxi', j2), 'h2_d'], writes=[('xe', sc)])
                    S.custom_dma('pool', lambda e_: e_.indirect_dma_start(out=ag[e % 2][sc][:, :], out_offset=None, in_=aff_d[:, :],
                                 in_offset=bass.IndirectOffsetOnAxis(ap=idxi[j2][:, sc:sc + 1], axis=0)),
                                 reads=[('idxi', j2), 'aff_d'], writes=[('ag', e % 2, sc)])

            gu_ctr = [0]

            def xpose(e):
                for sc in range(8):
                    for dc in range(8):
                        S.op('pe', lambda e_: e_.transpose(out=pT[:, dc, :], in_=xe[sc][:, dc * 128:(dc + 1) * 128], identity=identb[:]),
                             reads=[('xe', sc), 'identb'], writes=['pT'])
                    S.op('act', lambda e_: e_.activation(out=xeT[:, :, sc * 128:(sc + 1) * 128], in_=pT[:], func=AF.Copy),
                         reads=['pT'], writes=['xeT'])
            def ffn(e, nxt):
                i2 = e % 2; j2 = e % 3
                for fc in range(8):
                    for hf in range(2):
                        a = gu_ctr[0] % 3; gu_ctr[0] += 1
                        b = gu_ctr[0] % 3; gu_ctr[0] += 1
                        for dc in range(8):
                            S.op('pe', lambda e_: e_.matmul(pGU[a][:], lhsT=Wg[i2][:, dc, fc * 128:(fc + 1) * 128], rhs=xeT[:, dc, hf * 512:(hf + 1) * 512],
                                                            start=(dc == 0), stop=(dc == 7)), reads=[('Wg', i2), 'xeT'], writes=[('pGU', a)])
                        for dc in range(8):
                            S.op('pe', lambda e_: e_.matmul(pGU[b][:], lhsT=Wu[i2][:, dc, fc * 128:(fc + 1) * 128], rhs=xeT[:, dc, hf * 512:(hf + 1) * 512],
                                                            start=(dc == 0), stop=(dc == 7)), reads=[('Wu', i2), 'xeT'], writes=[('pGU', b)])
                        s2 = (fc * 2 + hf) % 2
                        S.op('act', lambda e_: e_.activation(out=sg[s2][:], in_=pGU[a][:], func=AF.Silu), reads=[('pGU', a)], writes=[('sg', s2)])
                        S.op('dve', lambda e_: e_.tensor_tensor(out=hidT[:, fc, hf * 512:(hf + 1) * 512], in0=pGU[b][:], in1=sg[s2][:], op=ALU.mult),
                             reads=[('pGU', b), ('sg', s2)], writes=['hidT'])
                    if nxt is not None:
                        idx_piece(nxt, fc)
                for sc in range(8):
                    for hf in range(2):
                        for fc in range(8):
                            S.op('pe', lambda e_: e_.matmul(pY[:, hf * 512:(hf + 1) * 512], lhsT=hidT[:, fc, sc * 128:(sc + 1) * 128],
                                                            rhs=Wd[i2][:, fc, hf * 512:(hf + 1) * 512], start=(fc == 0), stop=(fc == 7)),
                                 reads=['hidT', ('Wd', i2)], writes=['pY'])
                    y2 = sc % 2
                    S.op('dve', lambda e_: e_.scalar_tensor_tensor(out=ye[y2][:], in0=pY[:], scalar=ag[e % 2][sc][:, e:e + 1], in1=bc_g2[:],
                                                                   op0=ALU.mult, op1=ALU.mult), reads=['pY', ('ag', e % 2, sc), 'bc_g2'], writes=[('ye', y2)])
                    S.custom_dma('pool', lambda e_: e_.indirect_dma_start(out=out[:, :], out_offset=bass.IndirectOffsetOnAxis(ap=idxi[j2][:, sc:sc + 1], axis=0),
                                 in_=ye[y2][:, :], in_offset=None, compute_op=ALU.add), reads=[('ye', y2), ('idxi', j2)], writes=['out'])

            load_w(0)
            for p in range(8):
                idx_piece(0, p)
            gather(0)
            if nexp > 1:
                load_w(1)
                for p in range(8):
                    idx_piece(1, p)
            for e in range(nexp):
                xpose(e)
                if e + 1 < nexp:
                    gather(e + 1)
                    if e >= 1:
                        load_w(e + 1)
                ffn(e, e + 2 if e + 2 < nexp else None)
            if 'moe_idx' in dbg:
                dbg_out['idxi'] = dout("dbg_idxi", [128, 8], I32)
                S.dma('sp', dbg_out['idxi'], idxi[(nexp - 1) % 3][:], reads=[('idxi', (nexp - 1) % 3)])
            S.barrier()


def moe_host(inp):
    f = np.float32
    m16 = np.zeros((128, 128), f)
    for s in range(8):
        for s2 in range(8):
            m16[16 * s:16 * s + 16, 16 * s2:16 * s2 + 16] = np.eye(16, dtype=f)
    return {'w_e_gate': np.ascontiguousarray(inp['w_e_gate'][0]), 'w_e_up': np.ascontiguousarray(inp['w_e_up'][0]),
            'w_e_down': np.ascontiguousarray(inp['w_e_down'][0]), 'm16': m16}


D = 1024
NLAT = 8192
NCTX = 256
NEXT = NLAT + NCTX
NE2 = NEXT + NCTX
EPS = 1e-6
INCOLS = 4096


def build(stop_after=99, dbg=(), nblk_lim=17, skip_s5=False, mg_blocks=16, nexp=16):
    nc = bass.Bass("TRN2", target_bir_lowering=False)
    es = ExitStack()
    S = Sched(nc, es)
    dbg_out = {}

    def din(name, shape, dt=F32):
        return nc.dram_tensor(name, list(shape), dt, kind="ExternalInput").ap()

    def dscr(name, shape, dt):
        return nc.dram_tensor(name, list(shape), dt, kind="Internal").ap()

    def dout(name, shape, dt=F32):
        return nc.dram_tensor(name, list(shape), dt, kind="ExternalOutput").ap()

    def sb(name, shape, dt, stack=None):
        return (stack or es).enter_context(nc.sbuf_tensor(name, list(shape), dt))

    def ps(name, shape, dt, stack=None):
        return (stack or es).enter_context(nc.psum_tensor(name, list(shape), dt))

    xcat = din("xcat", [NEXT, D])
    cT = din("cT", [128, 8, 2])
    w_ada = din("w_ada", [D, 6 * D])
    b_ada = din("b_ada", [1, 6 * D])
    nmixT = din("nmixT", [128, 8])
    nffn = din("nffn", [1, D])
    w_in = din("w_in", [D, INCOLS])
    ident_in = din("ident", [128, 128])
    qkg = din("qkg", [128, 2])

    out = dout("out", [NLAT, D])

    uT_d = dscr("uT_d", [512, NE2], BF16)
    qT_d = dscr("qT_d", [512, NLAT], BF16)
    kT_d = dscr("kT_d", [512, NEXT], BF16)
    v_d = dscr("v_d", [NEXT, 8, 65], BF16)
    sgaT_d = dscr("sgaT_d", [D, NLAT], BF16)
    sgbT_d = dscr("sgbT_d", [D, NLAT], BF16)

    identf = sb("identf", [128, 128], F32)
    identb = sb("identb", [128, 128], BF16)
    mul1 = sb("mul1", [128, 8, 2], F32)
    add1 = sb("add1", [128, 8, 2], F32)
    bc_g1 = sb("bc_g1", [128, D], F32)
    bc_mul2 = sb("bc_mul2", [128, D], F32)
    bc_add2 = sb("bc_add2", [128, D], F32)
    bc_g2 = sb("bc_g2", [128, D], F32)

    epsc = sb("epsc", [128, 1], F32)
    S.op('dve', lambda e: e.memset(epsc[:], EPS), writes=['epsc'])
    S.dma('sp', identf[:], ident_in, writes=['identf'])
    S.dma('pool', identb[:], ident_in, writes=['identb'])

    with ExitStack() as p0:
        wada = sb("wada", [128, 8, 6 * D], BF16, p0)
        cs_f = sb("cs_f", [128, 8, 2], F32, p0)
        cs = sb("cs", [128, 8, 2], BF16, p0)
        brow = sb("brow", [2, 6 * D], F32, p0)
        modrow = sb("modrow", [2, 6 * D], F32, p0)
        sel = sb("sel", [2, 128], F32, p0)
        nmix = sb("nmix", [128, 8], F32, p0)
        colsb = sb("colsb", [128, 16, 2], F32, p0)
        pm = [ps("pm%d" % i, [2, 512], F32, p0) for i in range(2)]
        pcol = ps("pcol", [128, 16, 2], F32, p0)
        pbc = [ps("pbc%d" % i, [128, 512], F32, p0) for i in range(2)]

        S.dma('sp', cs_f[:], cT, writes=['cs_f'])
        S.dma('sp', brow[0:1, :], b_ada, writes=['brow0'])
        S.dma('sp', brow[1:2, :], b_ada, writes=['brow1'])
        S.dma('sp', nmix[:], nmixT, writes=['nmix'])
        S.dma('sp', bc_mul2[:], nffn.to_broadcast([128, D]), writes=['bc_mul2'])
        for dc in range(8):
            S.dma('pool', wada[:, dc, :].rearrange("p (a b) -> p a b", b=2048),
                  w_ada[dc * 128:(dc + 1) * 128, :].rearrange("p (a b) -> p a b", b=2048),
                  writes=[('wada', dc)])
        S.op('act', lambda e: e.activation(out=cs[:], in_=cs_f[:], func=AF.Silu), reads=['cs_f'], writes=['cs'])
        S.op('dve', lambda e: e.memset(sel[:], 0.0), writes=['sel'])
        S.op('dve', lambda e: e.memset(sel[0:1, :], 1.0), writes=['sel'])
        for cc in range(12):
            pt = pm[cc % 2]
            for dc in range(8):
                S.op('pe', lambda e: e.matmul(pt[:], lhsT=cs[:, dc, :], rhs=wada[:, dc, cc * 512:(cc + 1) * 512],
                                              start=(dc == 0), stop=(dc == 7)),
                     reads=['cs', ('wada', dc)], writes=[('pm', cc % 2)])
            S.op('dve', lambda e: e.tensor_tensor(out=modrow[:, cc * 512:(cc + 1) * 512], in0=pt[:],
                                                  in1=brow[:, cc * 512:(cc + 1) * 512], op=ALU.add),
                 reads=[('pm', cc % 2), 'brow0', 'brow1'], writes=[('modrow', cc)])
        for i in range(16):
            S.op('pe', lambda e: e.transpose(out=pcol[:, i, :], in_=modrow[0:2, i * 128:(i + 1) * 128],
                                             identity=identf[0:2, 0:2]),
                 reads=[('modrow', i // 4), 'identf'], writes=['pcol'])
        S.op('dve', lambda e: e.tensor_copy(out=colsb[:], in_=pcol[:]), reads=['pcol'], writes=['colsb'])
        S.op('dve', lambda e: e.tensor_copy(out=add1[:], in_=colsb[:, 0:8, :]), reads=['colsb'], writes=['add1'])
        S.op('dve', lambda e: e.tensor_scalar(out=colsb[:, 8:16, :], in0=colsb[:, 8:16, :], scalar1=1.0, scalar2=None,
                                              op0=ALU.add), reads=['colsb'], writes=['colsb'])
        S.op('dve', lambda e: e.tensor_tensor(out=mul1[:], in0=colsb[:, 8:16, :],
                                              in1=nmix[:].unsqueeze(2).to_broadcast([128, 8, 2]), op=ALU.mult),
             reads=['colsb', 'nmix'], writes=['mul1'])

        def bcast(dst, col0, mode, dkey):
            for h in range(2):
                pt = pbc[h]
                S.op('pe', lambda e: e.matmul(pt[:], lhsT=sel[:], rhs=modrow[:, col0 + h * 512: col0 + (h + 1) * 512],
                                              start=True, stop=True),
                     reads=['sel'] + [('modrow', c) for c in range(12)], writes=[('pbc', h)])
                dsl = dst[:, h * 512:(h + 1) * 512]
                if mode == 'copy':
                    S.op('dve', lambda e: e.tensor_copy(out=dsl, in_=pt[:]), reads=[('pbc', h)], writes=[dkey])
                else:
                    S.op('dve', lambda e: e.scalar_tensor_tensor(out=dsl, in0=pt[:], scalar=1.0, in1=dsl,
                                                                 op0=ALU.add, op1=ALU.mult),
                         reads=[('pbc', h), dkey], writes=[dkey])
        bcast(bc_g1, 2048, 'copy', 'bc_g1')
        bcast(bc_add2, 3072, 'copy', 'bc_add2')
        bcast(bc_mul2, 4096, 'mul1p', 'bc_mul2')
        bcast(bc_g2, 5120, 'copy', 'bc_g2')
        if 'mod' in dbg:
            dbg_out['mod'] = dout("dbg_mod", [2, 6 * D])
            S.dma('sp', dbg_out['mod'], modrow[:], reads=[('modrow', c) for c in range(12)])
            dbg_out['mul1'] = dout("dbg_mul1", [128, 8, 2])
            S.dma('sp', dbg_out['mul1'], mul1[:], reads=['mul1'])
            dbg_out['bcm2'] = dout("dbg_bcm2", [128, D])
            S.dma('sp', dbg_out['bcm2'], bc_mul2[:], reads=['bc_mul2'])
        S.barrier()

    if stop_after <= 0:
        return finish(nc, S, es, out, dbg_out)

    with ExitStack() as p1:
        win = sb("win", [128, 8, INCOLS], BF16, p1)
        xt = [sb("xt%d" % i, [128, D], F32, p1) for i in range(2)]
        junk = sb("junk", [128, D], BF16, p1)
        xn = [sb("xn%d" % i, [128, D], BF16, p1) for i in range(4)]
        ss = [sb("ss%d" % i, [128, 1], F32, p1) for i in range(4)]
        rstd = [sb("rstd%d" % i, [128, 1], F32, p1) for i in range(4)]
        hT = [sb("hT%d" % i, [128, 8, 512], BF16, p1) for i in range(2)]
        stg = [sb("stg%d" % i, [128, 512], BF16, p1) for i in range(4)]
        sq = [sb("sq%d" % i, [128, 512], BF16, p1) for i in range(2)]
        qk32 = [sb("qk32_%d" % i, [128, 512], F32, p1) for i in range(2)]
        rs = [sb("rs%d" % i, [128, 512], F32, p1) for i in range(2)]
        vst = [sb("vst%d" % i, [128, 8, 65], BF16, p1) for i in range(2)]
        bones = sb("bones", [128, 128], BF16, p1)
        gq = sb("gq", [128, 2], F32, p1)
        pT = [ps("pT%d" % i, [128, 8, 128], BF16, p1) for i in range(2)]
        pz = [ps("pz%d" % i, [128, 512], F32, p1) for i in range(4)]
        pms = [ps("pms%d" % i, [128, 512], F32, p1) for i in range(2)]

        for dc in range(8):
            S.dma('pool', win[:, dc, :].rearrange("p (a b) -> p a b", b=2048),
                  w_in[dc * 128:(dc + 1) * 128, :].rearrange("p (a b) -> p a b", b=2048),
                  writes=[('win', dc)])
        S.dma('sp', gq[:], qkg, writes=['gq'])
        S.op('dve', lambda e: e.tensor_scalar(out=gq[:, 0:1], in0=gq[:, 0:1], scalar1=0.125, scalar2=None, op0=ALU.mult), reads=['gq'], writes=['gq'])
        for i in range(2):
            S.op('dve', lambda e: e.memset(vst[i][:], 1.0), writes=[('vst', i)])
        S.op('dve', lambda e: e.memset(bones[:], 0.0), writes=['bones'])
        S.op('dve', lambda e: e.memset(bones[0:64, 0:64], 1.0 / 64), writes=['bones'])
        S.op('dve', lambda e: e.memset(bones[64:128, 64:128], 1.0 / 64), writes=['bones'])

        nblk = (NEXT + 511) // 512
        ctr = {'tile': 0, 'tpe': 0, 'stg': 0, 'z': 0, 'v': 0}

        def blk_info(b):
            if b == 0:
                return 0, 256
            return 256 + (b - 1) * 512, 512

        def head_pre(b):
            t0, nt = blk_info(b)
            for ti in range(nt // 128):
                i2 = ctr['tile'] % 2
                i4 = ctr['tile'] % 4
                ctr['tile'] += 1
                r0 = t0 + ti * 128
                S.dma('sp', xt[i2][:], xcat[r0:r0 + 128, :], writes=[('xt', i2)])
                S.op('act', lambda e: e.activation(out=junk[:], in_=xt[i2][:], func=AF.Square, accum_out=ss[i4][:]),
                     reads=[('xt', i2)], writes=['junk', ('ss', i4)])
                S.op('act', lambda e: e.activation(out=rstd[i4][:], in_=ss[i4][:], func=AF.Ln, scale=1.0 / D, bias=epsc[:, 0:1]),
                     reads=[('ss', i4), 'epsc'], writes=[('rstd', i4)])
                S.op('act', lambda e: e.activation(out=rstd[i4][:], in_=rstd[i4][:], func=AF.Exp, scale=-0.5),
                     reads=[('rstd', i4)], writes=[('rstd', i4)])
                S.op('act', lambda e: e.activation(out=xn[i4][:], in_=xt[i2][:], func=AF.Copy, scale=rstd[i4][:, 0:1]),
                     reads=[('xt', i2), ('rstd', i4)], writes=[('xn', i4)])

        def head_pe(b):
            t0, nt = blk_info(b)
            j = 1 if b == 0 else 0
            hb = hT[b % 2]
            hkey = ('hT', b % 2)
            for ti in range(nt // 128):
                i2 = ctr['tpe'] % 2
                i4 = ctr['tpe'] % 4
                ctr['tpe'] += 1
                for dc in range(8):
                    S.op('pe', lambda e: e.transpose(out=pT[i2][:, dc, :], in_=xn[i4][:, dc * 128:(dc + 1) * 128],
                                                     identity=identb[:]),
                         reads=[('xn', i4), 'identb'], writes=[('pT', i2)])
                for dc in range(8):
                    S.op('dve', lambda e: e.tensor_scalar(out=hb[:, dc, ti * 128:(ti + 1) * 128], in0=pT[i2][:, dc, :],
                                                          scalar1=mul1[:, dc, j:j + 1], scalar2=add1[:, dc, j:j + 1],
                                                          op0=ALU.mult, op1=ALU.add),
                         reads=[('pT', i2), 'mul1', 'add1'], writes=[hkey])

        pend_qk = []

        def chunk(b, cc):
            t0, nt = blk_info(b)
            is_ctx = (b == 0)
            hb = hT[b % 2]
            hkey = ('hT', b % 2)
            zi = ctr['z'] % 4
            ctr['z'] += 1
            pzt = pz[zi]
            for dc in range(8):
                S.op('pe', lambda e: e.matmul(pzt[:, 0:nt], lhsT=win[:, dc, cc * 128:(cc + 1) * 128], rhs=hb[:, dc, 0:nt],
                                              start=(dc == 0), stop=(dc == 7)),
                     reads=[('win', dc), hkey], writes=[('pz', zi)], sig=(dc == 7))
            si = ctr['stg'] % 4
            ctr['stg'] += 1
            st = stg[si]
            skey = ('stg', si)
            if not (4 <= cc < 12):
                while pend_qk:
                    pend_qk.pop(0)()
            if cc < 4:
                S.op('act', lambda e: e.activation(out=st[:, 0:nt], in_=pzt[:, 0:nt], func=AF.Copy),
                     reads=[('pz', zi)], writes=[skey])
                S.dma('sp', uT_d[cc * 128:(cc + 1) * 128, t0:t0 + nt], st[:, 0:nt], reads=[skey], writes=['uT_d'])
                if is_ctx:
                    S.dma('sp', uT_d[cc * 128:(cc + 1) * 128, NEXT:NEXT + nt], st[:, 0:nt], reads=[skey], writes=['uT_d'])
            elif cc < 12:
                isq = cc < 8
                qi = ctr['z'] % 2
                S.op('act', lambda e: e.activation(out=sq[qi][:, 0:nt], in_=pzt[:, 0:nt], func=AF.Square),
                     reads=[('pz', zi)], writes=[('sq', qi)])
                while pend_qk:
                    pend_qk.pop(0)()

                def tail(qi=qi, zi=zi, pzt=pzt, st=st, skey=skey, isq=isq, cc=cc, t0=t0, nt=nt):
                    S.op('pe', lambda e: e.matmul(pms[qi][:, 0:nt], lhsT=bones[:], rhs=sq[qi][:, 0:nt], start=True, stop=True),
                         reads=['bones', ('sq', qi)], writes=[('pms', qi)])
                    S.op('act', lambda e: e.activation(out=rs[qi][:, 0:nt], in_=pms[qi][:, 0:nt], func=AF.Ln, bias=epsc[:, 0:1]),
                         reads=[('pms', qi), 'epsc'], writes=[('rs', qi)])
                    S.op('act', lambda e: e.activation(out=rs[qi][:, 0:nt], in_=rs[qi][:, 0:nt], func=AF.Exp, scale=-0.5),
                         reads=[('rs', qi)], writes=[('rs', qi)])
                    gcol = gq[:, 0:1] if isq else gq[:, 1:2]
                    S.op('dve', lambda e: e.scalar_tensor_tensor(out=st[:, 0:nt], in0=pzt[:, 0:nt], scalar=gcol,
                                                                 in1=rs[qi][:, 0:nt], op0=ALU.mult, op1=ALU.mult),
                         reads=[('pz', zi), ('rs', qi), 'gq'], writes=[skey])
                    if isq:
                        S.dma('sp', qT_d[(cc - 4) * 128:(cc - 3) * 128, t0 - NCTX:t0 - NCTX + nt], st[:, 0:nt],
                              reads=[skey], writes=['qT_d'])
                    else:
                        S.dma('sp', kT_d[(cc - 8) * 128:(cc - 7) * 128, t0:t0 + nt], st[:, 0:nt], reads=[skey], writes=['kT_d'])
                pend_qk.append(tail)
            else:
                S.op('act', lambda e: e.activation(out=st[:, 0:nt], in_=pzt[:, 0:nt], func=AF.Sigmoid),
                     reads=[('pz', zi)], writes=[skey])
                if cc < 24:
                    dst = sgaT_d[(cc - 16) * 128:(cc - 15) * 128, t0 - NCTX:t0 - NCTX + nt]
                    dk = 'sgaT_d'
                else:
                    dst = sgbT_d[(cc - 24) * 128:(cc - 23) * 128, t0 - NCTX:t0 - NCTX + nt]
                    dk = 'sgbT_d'
                S.dma('sp', dst, st[:, 0:nt], reads=[skey], writes=[dk])

        def vpart(b):
            t0, nt = blk_info(b)
            hb = hT[b % 2]
            hkey = ('hT', b % 2)
            for ti in range(nt // 128):
                zi = ctr['z'] % 4
                ctr['z'] += 1
                pzt = pz[zi]
                for dc in range(8):
                    S.op('pe', lambda e: e.matmul(pzt[:], lhsT=hb[:, dc, ti * 128:(ti + 1) * 128], rhs=win[:, dc, 1536:2048],
                                                  start=(dc == 0), stop=(dc == 7)),
                         reads=[('win', dc), hkey], writes=[('pz', zi)], sig=(dc == 7))
                while pend_qk:
                    pend_qk.pop(0)()
                vi = ctr['v'] % 2
                ctr['v'] += 1
                S.op('act', lambda e: e.activation(out=vst[vi][:, :, 0:64], in_=pzt[:].rearrange("p (h d) -> p h d", d=64), func=AF.Copy),
                     reads=[('pz', zi)], writes=[('vst', vi)])
                S.dma('sp', v_d[t0 + ti * 128:t0 + (ti + 1) * 128], vst[vi][:], reads=[('vst', vi)], writes=['v_d'])

        head_pre(0)
        head_pe(0)
        for b in range(nblk_lim):
            if b == 0:
                ccs = list(range(0, 4)) + list(range(8, 12))
            else:
                ccs = list(range(0, 12)) + list(range(16, 32))
            n1, n2 = len(ccs) // 4, (3 * len(ccs)) // 4
            for cc in ccs[:n1]:
                chunk(b, cc)
            if b + 1 < nblk_lim:
                head_pre(b + 1)
            for cc in ccs[n1:n2]:
                chunk(b, cc)
            if b + 1 < nblk_lim:
                head_pe(b + 1)
            for cc in ccs[n2:]:
                chunk(b, cc)
            vpart(b)
        S.barrier()
    if 'p1' in dbg:
        for nm, t in (('uT', uT_d), ('qT', qT_d), ('kT', kT_d), ('sgaT', sgaT_d), ('sgbT', sgbT_d)):
            dbg_out[nm] = dout("dbg_" + nm, list(t.shape), BF16)
            for r0 in range(0, t.shape[0], 128):
                S.dma('sp', dbg_out[nm][r0:r0 + 128, :], t[r0:r0 + 128, :], reads=[])
    if stop_after <= 1:
        return finish(nc, S, es, out, dbg_out)
    yaT_d = dscr("yaT_d", [512, NLAT], BF16)
    r = 'ok' if skip_s5 else s5_phase(nc, S, es, sb, ps, din, dscr, dout, dbg, dbg_out, identf, identb, uT_d, yaT_d)
    if r == 'stop' or stop_after <= 2:
        return finish(nc, S, es, out, dbg_out)
    ybT_d = dscr("ybT_d", [512, NLAT], BF16)
    na_phase(nc, S, es, sb, ps, din, dscr, dout, dbg, dbg_out, identb, qT_d, kT_d, v_d, ybT_d)
    if stop_after <= 3:
        return finish(nc, S, es, out, dbg_out)
    h2_d = dscr("h2_d", [NLAT, D], BF16)
    aff_d = dscr("aff_d", [NLAT, 16], F32)
    affT_d = dscr("affT_d", [16, NLAT], F32)
    merge_phase(nc, S, es, sb, ps, din, dscr, dout, dbg, dbg_out, identf, identb, epsc, bc_g1, bc_mul2, bc_add2,
                xcat, out, yaT_d, ybT_d, sgaT_d, sgbT_d, h2_d, aff_d, affT_d, nblocks=mg_blocks)
    if stop_after <= 4:
        return finish(nc, S, es, out, dbg_out)
    moe_phase(nc, S, es, sb, ps, din, dscr, dout, dbg, dbg_out, identf, identb, bc_g2, out, h2_d, aff_d, affT_d, nexp=nexp)
    return finish(nc, S, es, out, dbg_out)


def finish(nc, S, es, out, dbg_out):
    S.barrier(['sp'])
    print("instructions", S.nins, "waits", S.nwaits)
    es.close()
    return nc, dbg_out


def host_inputs(inp, b):
    f = np.float32
    xcat = np.concatenate([inp['ctx'][b], inp['x'][b]], axis=0).astype(f)
    cT = np.stack([inp['c'][b].reshape(8, 128).T, inp['c_ctx'].reshape(8, 128).T], axis=-1).astype(f)
    qkg = np.stack([np.tile(inp['q_norm'][0], 2), np.tile(inp['k_norm'][0], 2)], axis=-1).astype(f)
    m = {
        'xcat': np.ascontiguousarray(xcat),
        'cT': np.ascontiguousarray(cT),
        'w_ada': np.ascontiguousarray(inp['w_ada'][0]),
        'b_ada': np.ascontiguousarray(inp['b_ada'][0][None, :]),
        'nmixT': np.ascontiguousarray(inp['norm_mix'][0].reshape(8, 128).T),
        'nffn': np.ascontiguousarray(inp['norm_ffn'][0][None, :]),
        'w_in': np.ascontiguousarray(inp['w_in'][0]),
        'ident': np.eye(128, dtype=f),
        'qkg': np.ascontiguousarray(qkg),
    }
    f32c = lambda a: np.ascontiguousarray(a.astype(f))
    lr = inp['ssm_lam_re'][0].reshape(2, 16, 2, 64)
    m['lamre_l'] = f32c(lr.transpose(2, 3, 0, 1).reshape(128, 32))
    li = inp['ssm_lam_im'][0].reshape(2, 16, 2, 64)
    m['lamim_l'] = f32c(li.transpose(2, 3, 0, 1).reshape(128, 32))
    ld = inp['ssm_log_dt'][0].reshape(2, 16, 2)
    m['logdt_l'] = f32c(np.broadcast_to(ld.transpose(2, 0, 1)[:, None, :, :], (2, 64, 2, 16)).reshape(128, 32))
    for nm, key in (('Bre_c', 'ssm_b_re'), ('Bim_c', 'ssm_b_im')):
        bb = inp[key][0].reshape(2, 16, 2, 64, 16)
        m[nm] = f32c(bb.transpose(2, 3, 0, 1, 4).reshape(128, 32, 16))
    for nm, key in (('Cre_c', 'ssm_c_re'), ('Cim_c', 'ssm_c_im')):
        cc = inp[key][0].reshape(2, 16, 2, 16, 64)
        m[nm] = f32c(cc.transpose(2, 4, 0, 1, 3).reshape(128, 32, 16))
    m['dskipT'] = f32c(inp['ssm_d'][0].reshape(4, 128).T)
    m['w_glu'] = f32c(inp['w_glu'][0])
    m['bgluT'] = f32c(inp['b_glu'][0].reshape(4, 128).T)
    m.update(na_host(inp))
    m['w_proj_a'] = f32c(inp['w_proj_a'][0]); m['w_proj_b'] = f32c(inp['w_proj_b'][0]); m['w_out'] = f32c(inp['w_out'][0])
    m.update(moe_host(inp))
    m['w_routerT'] = f32c(inp['w_router'][0].reshape(8, 128, 16).transpose(1, 0, 2))
    return m


_NC_CACHE = {}


def kernel(**inputs):
    inp = {k_: np.asarray(v_) for k_, v_ in inputs.items()}
    if 'nc' not in _NC_CACHE:
        nc, _ = build()
        _NC_CACHE['nc'] = nc
    nc = _NC_CACHE['nc']
    nb = inp['x'].shape[0]
    in_maps = [host_inputs(inp, b) for b in range(nb)]
    res = run_bass_kernel_spmd(nc, in_maps, core_ids=list(range(nb)))
    outs = [np.asarray(res.results[b]["out"], dtype=np.float32) for b in range(nb)]
    return np.stack(outs, axis=0)
```

```python
import numpy as np
from contextlib import ExitStack
import concourse.bass as bass
import concourse.mybir as mybir
from concourse.bass_utils import run_bass_kernel_spmd

F32 = mybir.dt.float32
BF16 = mybir.dt.bfloat16
I32 = mybir.dt.int32
U32 = mybir.dt.uint32
ALU = mybir.AluOpType
AF = mybir.ActivationFunctionType
AX = mybir.AxisListType
STRICT = False


class Sched:
    def __init__(self, nc, es, n_dma_sems=24):
        self.nc = nc
        self.e = {'pe': nc.tensor, 'act': nc.scalar, 'dve': nc.vector, 'pool': nc.gpsimd, 'sp': nc.sync}
        self.sems = {}
        self.cnt = {}
        for k in self.e:
            self.sems[k] = es.enter_context(nc.semaphore("sem_" + k))
            self.cnt[k] = 0
        self.nd = n_dma_sems
        self.qsems = {}
        for q in ('sp', 'pool', 'act'):
            n = n_dma_sems if q != 'act' else 16
            self.qsems[q] = []
            for i in range(n):
                sk = 'd_%s%d' % (q, i)
                self.sems[sk] = es.enter_context(nc.semaphore("dsem_%s%d" % (q, i)))
                self.cnt[sk] = 0
                self.qsems[q].append(sk)
        self.qrr = {'sp': 0, 'pool': 0, 'act': 0}
        self.drr = 0
        self.waited = {}
        self.lastw = {}
        self.readers = {}
        self.nwaits = 0
        self.nins = 0

    def _wait(self, eng, tok):
        sk, val, src = tok
        if self.waited.get((eng, sk), 0) >= val:
            return
        self.e[eng].wait_ge(self.sems[sk], val)
        self.waited[(eng, sk)] = val
        self.nwaits += 1

    def _deps(self, eng, reads, writes):
        for k in reads:
            t = self.lastw.get(k)
            if t is not None:
                self._wait(eng, t)
        for k in writes:
            t = self.lastw.get(k)
            if t is not None and (t[2] != eng or (STRICT and eng != 'pe')):
                self._wait(eng, t)
            for sk, (val, src) in self.readers.get(k, {}).items():
                if src != eng or (STRICT and eng != 'pe'):
                    self._wait(eng, (sk, val, src))

    def _record(self, tok, reads, writes):
        sk, val, src = tok
        for k in reads:
            self.readers.setdefault(k, {})[sk] = (val, src)
        for k in writes:
            self.lastw[k] = tok
            self.readers[k] = {}

    def op(self, eng, fn, reads=(), writes=(), sig=True):
        self._deps(eng, reads, writes)
        ins = fn(self.e[eng])
        if sig:
            self.cnt[eng] += 1
            ins.then_inc(self.sems[eng], 1)
            tok = (eng, self.cnt[eng], eng)
        else:
            tok = (eng, self.cnt[eng] + 1, eng)
        self._record(tok, reads, writes)
        self.nins += 1
        return tok

    def _dma_common(self, q, fn, reads, writes):
        self._deps(q, reads, writes)
        i = self.qrr[q]
        self.qrr[q] = (i + 1) % len(self.qsems[q])
        sk = self.qsems[q][i]
        if self.cnt[sk] > 0:
            self._wait(q, (sk, self.cnt[sk], None))
        ins = fn(self.e[q])
        self.cnt[sk] += 16
        ins.then_inc(self.sems[sk], 16)
        tok = (sk, self.cnt[sk], None)
        self._record(tok, reads, writes)
        self.nins += 1
        return tok

    def dma(self, q, out, in_, reads=(), writes=(), **kw):
        return self._dma_common(q, lambda e: e.dma_start(out=out, in_=in_, **kw), reads, writes)

    def custom_dma(self, q, fn, reads=(), writes=()):
        return self._dma_common(q, fn, reads, writes)

    def barrier(self, engines=None):
        for e in (engines or self.e):
            for sk, c in self.cnt.items():
                if c > 0:
                    self._wait(e, (sk, c, None))
        self.lastw = {}
        self.readers = {}

PI = float(np.pi)


def kn(t):
    n = t.name
    return n[2:] if n.startswith('s_') else n


def s5_phase(nc, S, es, sb, ps, din, dscr, dout, dbg, dbg_out, identf, identb, uT_d, yaT_d, nbanks=16):
    NEXT, NE2 = 8448, 8704
    lre_in = din("lamre_l", [128, 32]); lim_in = din("lamim_l", [128, 32]); ldt_in = din("logdt_l", [128, 32])
    Bre_in = din("Bre_c", [128, 32, 16]); Bim_in = din("Bim_c", [128, 32, 16])
    Cre_in = din("Cre_c", [128, 32, 16]); Cim_in = din("Cim_c", [128, 32, 16])
    dsk_in = din("dskipT", [128, 4]); wglu_in = din("w_glu", [512, 512]); bglu_in = din("bgluT", [128, 4])
    E_d = dscr("E_d", [128, 2, 4, 16, 2, 2, 128], BF16)
    G_d = dscr("G_d", [128, 64, 640], BF16)
    H_d = dscr("H_d", [2, 128, 16, 2, 544], BF16)

    with ExitStack() as s5:
        Kt_sb = sb("s_Kt_sb", [128, 128, 128], BF16, s5)
        aL = sb("s_aL", [128, 32], F32, s5); bL = sb("s_bL", [128, 32], F32, s5)
        diagD = sb("s_diagD", [128, 4, 128], BF16, s5)
        with ExitStack() as pa:
            def t32(name):
                return sb(name, [128, 32], F32, pa)
            lre, lim, ldt, dt, a, b = (t32(n) for n in ("lre", "lim", "ldt", "dt", "a_", "b_"))
            u1, u2, u3, u4, c16, s16, wre, wim = (t32(n) for n in ("u1", "u2", "u3", "u4", "c16", "s16", "wre", "wim"))
            Bcr = sb("s_Bcr", [128, 32, 16], F32, pa); Bci = sb("s_Bci", [128, 32, 16], F32, pa)
            Dr = sb("s_Dr", [128, 32, 32], F32, pa); Di = sb("s_Di", [128, 32, 32], F32, pa)
            Dr2 = sb("s_Dr2", [128, 32, 32], F32, pa); Di2 = sb("s_Di2", [128, 32, 32], F32, pa)
            Gr = sb("s_Gr", [128, 32, 32], F32, pa); Gi = sb("s_Gi", [128, 32, 32], F32, pa)
            T1 = sb("s_T1", [128, 32, 32], F32, pa); T2 = sb("s_T2", [128, 32, 32], F32, pa)
            T3 = sb("s_T3", [128, 32, 32], F32, pa); T4 = sb("s_T4", [128, 32, 32], F32, pa)
            P1 = sb("s_P1", [128, 32, 32], F32, pa); P2 = sb("s_P2", [128, 32, 32], F32, pa)
            P3 = sb("s_P3", [128, 32, 32], F32, pa); P4 = sb("s_P4", [128, 32, 32], F32, pa)
            Cbr = sb("s_Cbr", [128, 32, 32], BF16, pa); Cbni = sb("s_Cbni", [128, 32, 32], BF16, pa)
            Dbr = sb("s_Dbr", [128, 8, 160], BF16, pa); Dbi = sb("s_Dbi", [128, 8, 160], BF16, pa)
            Gbr = sb("s_Gbr", [128, 8, 160], BF16, pa); Gbni = sb("s_Gbni", [128, 8, 160], BF16, pa)
            Es = [sb("s_Es%d" % i, [128, 2, 4, 2, 2, 128], BF16, pa) for i in range(2)]
            m3 = sb("s_m3", [128, 1], F32, pa)
            dsk = sb("s_dsk", [128, 4], F32, pa)
            pk = [ps("s_pk%d" % i, [128, 128], F32, pa) for i in range(2)]
            pE = [ps("s_pE%d" % i, [128, 128], F32, pa) for i in range(4)]

            S.dma('sp', lre[:], lre_in, writes=['lre']); S.dma('sp', lim[:], lim_in, writes=['lim'])
            S.dma('sp', ldt[:], ldt_in, writes=['ldt']); S.dma('sp', dsk[:], dsk_in, writes=['dsk'])
            for X in (Dr, Di, Gr, Gi, Dr2, Di2):
                S.op('dve', lambda e: e.memset(X[:], 0.0), writes=[kn(X)])
            for X in (Dbr, Dbi, Gbr, Gbni):
                S.op('pool', lambda e: e.memset(X[:], 0.0), writes=[kn(X)])
            S.op('pool', lambda e: e.memset(Kt_sb[:], 0.0), writes=['Kt_sb'])
            for i in range(2):
                S.op('pool', lambda e: e.memset(Es[i][:], 0.0), writes=[('Es', i)])
            S.op('dve', lambda e: e.memset(m3[:], 1.0), writes=['m3'])
            S.op('dve', lambda e: e.memset(m3[64:96, :], 0.0), writes=['m3'])
            S.dma('sp', Bcr[:], Bre_in, writes=['Bcr']); S.dma('sp', Bci[:], Bim_in, writes=['Bci'])
            for h in range(2):
                S.dma('sp', Gr[64 * h:64 * h + 64, :, 16 * h:16 * h + 16], Cre_in[64 * h:64 * h + 64], reads=[], writes=['Gr'])
                S.dma('sp', Gi[64 * h:64 * h + 64, :, 16 * h:16 * h + 16], Cim_in[64 * h:64 * h + 64], reads=[], writes=['Gi'])
            for q in range(4):
                S.op('dve', lambda e: e.tensor_scalar(out=diagD[:, q, :], in0=identf[:], scalar1=dsk[:, q:q + 1], scalar2=None,
                                                      op0=ALU.mult), reads=['identf', 'dsk'], writes=['diagD'])
            V = lambda e: e
            def tt(out, in0, in1, op, r, w, eng='dve'):
                S.op(eng, lambda e: e.tensor_tensor(out=out, in0=in0, in1=in1, op=op), reads=r, writes=w)
            S.op('act', lambda e: e.activation(out=dt[:], in_=ldt[:], func=AF.Exp), reads=['ldt'], writes=['dt'])
            tt(u1[:], lre[:], dt[:], ALU.mult, ['lre', 'dt'], ['u1'])
            S.op('act', lambda e: e.activation(out=u1[:], in_=u1[:], func=AF.Exp), reads=['u1'], writes=['u1'])
            tt(u2[:], lim[:], dt[:], ALU.mult, ['lim', 'dt'], ['u2'])
            S.op('act', lambda e: e.activation(out=s16[:], in_=u2[:], func=AF.Sin, scale=1.0 / 16), reads=['u2'], writes=['s16'])
            S.op('act', lambda e: e.activation(out=u3[:], in_=u2[:], func=AF.Sin, scale=1.0 / 32), reads=['u2'], writes=['u3'])
            tt(u3[:], u3[:], u3[:], ALU.mult, ['u3'], ['u3'])
            S.op('dve', lambda e: e.tensor_scalar(out=c16[:], in0=u3[:], scalar1=-2.0, scalar2=1.0, op0=ALU.mult, op1=ALU.add),
                 reads=['u3'], writes=['c16'])

            def csquare(cr, ci, n):
                for _ in range(n):
                    tt(u3[:], cr[:], cr[:], ALU.mult, [kn(cr)], ['u3'])
                    tt(u4[:], ci[:], ci[:], ALU.mult, [kn(ci)], ['u4'])
                    tt(ci[:], cr[:], ci[:], ALU.mult, [kn(cr), kn(ci)], [kn(ci)])
                    S.op('dve', lambda e: e.tensor_scalar(out=ci[:], in0=ci[:], scalar1=2.0, scalar2=None, op0=ALU.mult),
                         reads=[kn(ci)], writes=[kn(ci)])
                    tt(cr[:], u3[:], u4[:], ALU.subtract, ['u3', 'u4'], [kn(cr)])
            csquare(c16, s16, 4)
            tt(a[:], u1[:], c16[:], ALU.mult, ['u1', 'c16'], ['a_'])
            tt(b[:], u1[:], s16[:], ALU.mult, ['u1', 's16'], ['b_'])
            S.op('dve', lambda e: e.tensor_copy(out=aL[:], in_=c16[:]), reads=['c16'], writes=['aL'])
            S.op('dve', lambda e: e.tensor_copy(out=bL[:], in_=s16[:]), reads=['s16'], writes=['bL'])
            csquare(aL, bL, 4)
            tt(u3[:], lre[:], dt[:], ALU.mult, ['lre', 'dt'], ['u3'])
            S.op('act', lambda e: e.activation(out=u3[:], in_=u3[:], func=AF.Exp, scale=16.0), reads=['u3'], writes=['u3'])
            tt(aL[:], aL[:], u3[:], ALU.mult, ['aL', 'u3'], ['aL'])
            tt(bL[:], bL[:], u3[:], ALU.mult, ['bL', 'u3'], ['bL'])
            S.op('dve', lambda e: e.tensor_scalar(out=u1[:], in0=a[:], scalar1=-1.0, scalar2=None, op0=ALU.add), reads=['a_'], writes=['u1'])
            tt(u2[:], lre[:], lre[:], ALU.mult, ['lre'], ['u2'])
            tt(u3[:], lim[:], lim[:], ALU.mult, ['lim'], ['u3'])
            tt(u2[:], u2[:], u3[:], ALU.add, ['u2', 'u3'], ['u2'])
            S.op('dve', lambda e: e.reciprocal(out=u2[:], in_=u2[:]), reads=['u2'], writes=['u2'])
            tt(u3[:], u1[:], lre[:], ALU.mult, ['u1', 'lre'], ['u3'])
            tt(u4[:], b[:], lim[:], ALU.mult, ['b_', 'lim'], ['u4'])
            tt(u3[:], u3[:], u4[:], ALU.add, ['u3', 'u4'], ['u3'])
            tt(wre[:], u3[:], u2[:], ALU.mult, ['u3', 'u2'], ['wre'])
            tt(u3[:], b[:], lre[:], ALU.mult, ['b_', 'lre'], ['u3'])
            tt(u4[:], u1[:], lim[:], ALU.mult, ['u1', 'lim'], ['u4'])
            tt(u3[:], u3[:], u4[:], ALU.subtract, ['u3', 'u4'], ['u3'])
            tt(wim[:], u3[:], u2[:], ALU.mult, ['u3', 'u2'], ['wim'])
            bc16 = lambda t: t[:].unsqueeze(2).to_broadcast([128, 32, 16])
            Tc = [T1[:, :, 0:16], T2[:, :, 0:16], T3[:, :, 0:16], T4[:, :, 0:16]]
            tt(Tc[0], Bcr[:], bc16(wre), ALU.mult, ['Bcr', 'wre'], ['T1'])
            tt(Tc[1], Bci[:], bc16(wim), ALU.mult, ['Bci', 'wim'], ['T2'])
            tt(Tc[2], Bci[:], bc16(wre), ALU.mult, ['Bci', 'wre'], ['T3'])
            tt(Tc[3], Bcr[:], bc16(wim), ALU.mult, ['Bcr', 'wim'], ['T4'])
            for h in range(2):
                sl = slice(64 * h, 64 * h + 64)
                tt(Dr[sl, :, 16 * h:16 * h + 16], T1[sl, :, 0:16], T2[sl, :, 0:16], ALU.subtract, ['T1', 'T2'], ['Dr'])
                tt(Di[sl, :, 16 * h:16 * h + 16], T3[sl, :, 0:16], T4[sl, :, 0:16], ALU.add, ['T3', 'T4'], ['Di'])
            S.op('dve', lambda e: e.tensor_copy(out=Cbr[:], in_=Gr[:]), reads=['Gr'], writes=['Cbr'])
            S.op('dve', lambda e: e.tensor_scalar(out=Cbni[:], in0=Gi[:], scalar1=-1.0, scalar2=None, op0=ALU.mult),
                 reads=['Gi'], writes=['Cbni'])
            bc32 = lambda t: t[:].unsqueeze(2).to_broadcast([128, 32, 32])

            def cmul(Xr, Xi, eng, Yr=None, Yi=None):
                Yr = Yr or Xr; Yi = Yi or Xi
                A1, A2, A3, A4 = (T1, T2, T3, T4) if eng == 'dve' else (P1, P2, P3, P4)
                tt(A1[:], Xr[:], bc32(a), ALU.mult, [kn(Xr), 'a_'], [kn(A1)], eng)
                tt(A2[:], Xi[:], bc32(b), ALU.mult, [kn(Xi), 'b_'], [kn(A2)], eng)
                tt(A3[:], Xi[:], bc32(a), ALU.mult, [kn(Xi), 'a_'], [kn(A3)], eng)
                tt(A4[:], Xr[:], bc32(b), ALU.mult, [kn(Xr), 'b_'], [kn(A4)], eng)
                tt(Yr[:], A1[:], A2[:], ALU.subtract, [kn(A1), kn(A2)], [kn(Yr)], eng)
                tt(Yi[:], A3[:], A4[:], ALU.add, [kn(A3), kn(A4)], [kn(Yi)], eng)

            def to5(dst, src, scale, key, eng='dve'):
                d4 = dst[:].rearrange("p g (s c) -> p g s c", c=32)
                s4 = src[:].rearrange("p (g s) c -> p g s c", s=4)
                S.op(eng, lambda e: e.tensor_scalar(out=d4[:, :, 0:3, :], in0=s4[:, :, 0:3, :], scalar1=scale, scalar2=None, op0=ALU.mult),
                     reads=[kn(src)], writes=[key])
                S.op(eng, lambda e: e.tensor_scalar(out=d4[:, :, 4, :], in0=s4[:, :, 3, :], scalar1=scale, scalar2=None, op0=ALU.mult),
                     reads=[kn(src)], writes=[key])

            Dpairs = [(Dr, Di), (Dr2, Di2)]
            for j in range(16):
                Dr, Di = Dpairs[j % 2]
                to5(Dbr, Dr, 1.0, 'Dbr'); to5(Dbi, Di, 1.0, 'Dbi')
                if j < 15:
                    cmul(Dr, Di, 'dve', Dpairs[(j + 1) % 2][0], Dpairs[(j + 1) % 2][1])
                for d in range(2):
                    for q in range(4):
                        idx = (d * 4 + q) * 16 + j
                        pkt = pk[idx % 2]; pkk = ('pk', idx % 2)
                        for pl in range(4):
                            pair = d * 16 + q * 4 + pl
                            if pl < 3:
                                osl = pkt[32 * pl:32 * pl + 32, 32 * pl:32 * pl + 32]; csl = slice(32 * pl, 32 * pl + 32)
                            else:
                                osl = pkt[64:128, 96:128]; csl = slice(96, 160)
                            S.op('pe', lambda e: e.matmul(osl, lhsT=Dbr[:, d * 4 + q, csl], rhs=Cbr[:, pair, :], start=True, stop=False),
                                 reads=['Dbr', 'Cbr'], writes=[pkk])
                            S.op('pe', lambda e: e.matmul(osl, lhsT=Dbi[:, d * 4 + q, csl], rhs=Cbni[:, pair, :], start=False, stop=True),
                                 reads=['Dbi', 'Cbni'], writes=[pkk])
                        for pl in range(4):
                            if pl < 3:
                                rs_, cs_ = slice(32 * pl, 32 * pl + 32), slice(32 * pl, 32 * pl + 32)
                            else:
                                rs_, cs_ = slice(64, 128), slice(96, 128)
                            S.op('act', lambda e: e.activation(out=Kt_sb[rs_, idx, cs_], in_=pkt[rs_, cs_], func=AF.Copy),
                                 reads=[pkk], writes=['Kt_sb'])
                Et = Es[j % 2]; ek = ('Es', j % 2)
                n_e = 0
                for reim, X in ((0, Dr), (1, Di)):
                    for d in range(2):
                        for q in range(4):
                            pet = pE[n_e % 4]; pek = ('pE', n_e % 4); n_e += 1
                            src = X[:, d * 16 + q * 4:d * 16 + q * 4 + 4, :].rearrange("p a b -> p (a b)")
                            S.op('pe', lambda e: e.transpose(out=pet[:], in_=src, identity=identf[:]), reads=[kn(X), 'identf'], writes=[pek])
                            S.op('act', lambda e: e.activation(out=Et[:, d, q, reim, 0, :], in_=pet[:], func=AF.Copy), reads=[pek], writes=[ek])
                            S.op('dve', lambda e: e.tensor_scalar(out=Et[64:128, d, q, reim, 1, :], in0=pet[64:128, :], scalar1=m3[64:128, 0:1],
                                                                  scalar2=None, op0=ALU.mult), reads=[pek, 'm3'], writes=[ek])
                for d in range(2):
                    S.dma('sp', E_d[:, d, :, j, :, :, :], Et[:, d], reads=[ek], writes=['E_d'])
                cmul(Gr, Gi, 'pool')
                to5(Gbr, Gr, 1.0, 'Gbr'); to5(Gbni, Gi, -1.0, 'Gbni')
                for d in range(2):
                    S.dma('sp', G_d[:, (d * 16 + j) * 2 + 0, :], Gbr[:, d * 4:d * 4 + 4, :].rearrange("p a b -> p (a b)"), reads=['Gbr'], writes=['G_d'])
                    S.dma('sp', G_d[:, (d * 16 + j) * 2 + 1, :], Gbni[:, d * 4:d * 4 + 4, :].rearrange("p a b -> p (a b)"), reads=['Gbni'], writes=['G_d'])
            S.barrier()
        if 's5prep' in dbg:
            dbg_out['Kt'] = dout("dbg_Kt", [128, 128, 128], BF16)
            S.dma('sp', dbg_out['Kt'], Kt_sb[:], reads=['Kt_sb'])
            dbg_out['aL'] = dout("dbg_aL", [128, 2, 32])
            S.dma('sp', dbg_out['aL'][:, 0, :], aL[:], reads=['aL']); S.dma('sp', dbg_out['aL'][:, 1, :], bL[:], reads=['bL'])
            dbg_out['G'] = dout("dbg_G", [128, 64, 640], BF16)
            S.dma('sp', dbg_out['G'], G_d, reads=['G_d'])
            dbg_out['E'] = dout("dbg_E", [128, 2, 4, 16, 2, 2, 128], BF16)
            for d in range(2):
                S.dma('sp', dbg_out['E'][:, d], E_d[:, d], reads=['E_d'])
            return 'stop'
        return s5_main(nc, S, es, sb, ps, din, dscr, dout, dbg, dbg_out, identf, identb, uT_d, yaT_d, nbanks,
                       s5, Kt_sb, aL, bL, diagD, E_d, G_d, H_d, wglu_in, bglu_in)


def s5_main(nc, S, es, sb, ps, din, dscr, dout, dbg, dbg_out, identf, identb, uT_d, yaT_d, nbanks,
            s5, Kt_sb, aL, bL, diagD, E_d, G_d, H_d, wglu_in, bglu_in):
    NCH = 528
    with ExitStack() as pb:
        uTb = [sb("s_uTb%d" % i, [128, 2048], BF16, pb) for i in range(1)]
        uTc = [sb("s_uTc%d" % i, [128, 16, 128], BF16, pb) for i in range(2)]
        Esl = [sb("s_Esl%d" % i, [128, 16, 2, 2, 128], BF16, pb) for i in range(2)]
        Ssb = [[sb("s_Ssb%d_%d" % (d, i), [128, 16, 2, 128], F32, pb) for i in range(2)] for d in range(2)]
        hist = [sb("s_hist%d" % d, [128, 16, 2, 129], F32, pb) for d in range(2)]
        histb = [sb("s_histb%d" % d, [128, 16, 2, 128], BF16, pb) for d in range(2)]
        tP = [sb("s_tP%d" % d, [128, 16, 2], F32, pb) for d in range(2)]
        tQ = [sb("s_tQ%d" % d, [128, 16, 2], F32, pb) for d in range(2)]
        pS = [ps("s_pS%d" % i, [128, 4, 128], F32, pb) for i in range(4)]
        for d in range(2):
            S.op('dve' if d == 0 else 'pool', lambda e: e.memset(hist[d][:], 0.0), writes=[('hist', d)])
        fblocks = [(0, 128), (128, 128), (256, 128), (384, 128), (512, 16)]
        bblocks = [(512, 16), (384, 128), (256, 128), (128, 128), (0, 128)]
        esl_ctr = 0
        ps_ctr = 0
        for step in range(5):
            for d in range(2):
                k0, nk = (fblocks if d == 0 else bblocks)[step]
                Sb = Ssb[d][step % 2]; skey = ('Ssb', d, step % 2)
                base = 0 if d == 0 else 256
                for q in range(4):
                    et = Esl[esl_ctr % 2]; ekey = ('Esl', esl_ctr % 2); esl_ctr += 1
                    S.dma('sp', et[:], E_d[:, d, q], reads=['E_d'], writes=[ekey])
                    ut0 = uTb[0]; u0key = ('uTb', 0)
                    S.dma('sp', ut0[:, 0:16 * nk], uT_d[q * 128:(q + 1) * 128, base + 16 * k0: base + 16 * k0 + 16 * nk], reads=['uT_d'], writes=[u0key])
                    ut = uTc[(esl_ctr - 1) % 2]; ukey = ('uTc', (esl_ctr - 1) % 2)
                    S.op('act', lambda e: e.activation(out=ut[:, :, 0:nk], in_=ut0[:, 0:16 * nk].rearrange("p (k i) -> p i k", i=16), func=AF.Copy),
                         reads=[u0key], writes=[ukey])
                    for pl in range(4):
                        pst = pS[ps_ctr % 4]; pskey = ('pS', ps_ctr % 4); ps_ctr += 1
                        if pl < 3:
                            rsl, alt = slice(32 * pl, 32 * pl + 32), 0
                        else:
                            rsl, alt = slice(64, 128), 1
                        for reim in range(2):
                            for i in range(16):
                                j = 15 - i if d == 0 else i
                                rhs = ut[rsl, i, 0:nk]
                                S.op('pe', lambda e: e.matmul(pst[:, reim, 0:nk], lhsT=et[rsl, j, reim, alt, :], rhs=rhs,
                                                              start=(i == 0), stop=(i == 15)),
                                     reads=[ekey, ukey], writes=[pskey])
                        S.op('act', lambda e: e.activation(out=Sb[:, q * 4 + pl, :, 0:nk], in_=pst[:, 0:2, 0:nk], func=AF.Copy),
                             reads=[pskey], writes=[skey])
            def chain_ops(d, kk):
                k0, nk = (fblocks if d == 0 else bblocks)[step]
                Sb = Ssb[d][step % 2]; skey = ('Ssb', d, step % 2)
                hk = ('hist', d)
                A2 = aL[:, d * 16:(d + 1) * 16].unsqueeze(2).to_broadcast([128, 16, 2])
                B2 = bL[:, d * 16:(d + 1) * 16].unsqueeze(2).to_broadcast([128, 16, 2])
                H = hist[d]
                if d == 0:
                    cprev, ccur, scol = kk, kk + 1, kk
                else:
                    cprev, ccur, scol = nk - kk, nk - 1 - kk, nk - 1 - kk
                prev = H[:, :, :, cprev]
                P_, Q_ = tP[d], tQ[d]
                eng = 'dve'
                return [
                    lambda: S.op(eng, lambda e: e.tensor_tensor(out=P_[:], in0=prev, in1=A2, op=ALU.mult), reads=[hk, 'aL'], writes=[('tP', d)]),
                    lambda: S.op(eng, lambda e: e.tensor_tensor(out=Q_[:], in0=prev, in1=B2, op=ALU.mult), reads=[hk, 'bL'], writes=[('tQ', d)]),
                    lambda: S.op(eng, lambda e: e.tensor_tensor(out=P_[:], in0=P_[:], in1=Sb[:, :, :, scol], op=ALU.add),
                                 reads=[('tP', d), skey], writes=[('tP', d)]),
                    lambda: S.op(eng, lambda e: e.tensor_tensor(out=H[:, :, 0, ccur], in0=P_[:, :, 0], in1=Q_[:, :, 1], op=ALU.subtract),
                                 reads=[('tP', d), ('tQ', d)], writes=[hk]),
                    lambda: S.op(eng, lambda e: e.tensor_tensor(out=H[:, :, 1, ccur], in0=P_[:, :, 1], in1=Q_[:, :, 0], op=ALU.add),
                                 reads=[('tP', d), ('tQ', d)], writes=[hk]),
                ]
            nks = [(fblocks if d == 0 else bblocks)[step][1] for d in range(2)]
            for kk in range(max(nks)):
                lists = [chain_ops(d, kk) for d in range(2) if kk < nks[d]]
                for oi in range(5):
                    for l in lists:
                        l[oi]()
            for d in range(2):
                eng = 'dve' if d == 0 else 'pool'
                k0, nk = (fblocks if d == 0 else bblocks)[step]
                hk = ('hist', d)
                H = hist[d]
                if d == 0:
                    S.op(eng, lambda e: e.tensor_copy(out=histb[d][:, :, :, 0:nk], in_=H[:, :, :, 1:nk + 1]), reads=[hk], writes=[('histb', d)])
                    S.op(eng, lambda e: e.tensor_copy(out=H[:, :, :, 0], in_=H[:, :, :, nk]), reads=[hk], writes=[hk])
                else:
                    S.op(eng, lambda e: e.tensor_copy(out=histb[d][:, :, :, 0:nk], in_=H[:, :, :, 0:nk]), reads=[hk], writes=[('histb', d)])
                    S.op(eng, lambda e: e.tensor_copy(out=H[:, :, :, 128], in_=H[:, :, :, 0]), reads=[hk], writes=[hk])
                S.dma('pool', H_d[d, :, :, :, k0:k0 + nk], histb[d][:, :, :, 0:nk], reads=[('histb', d)], writes=['H_d'])
        S.barrier()
    if 's5lvl3' in dbg:
        dbg_out['H'] = dout("dbg_H", [2, 128, 16, 2, 544], BF16)
        for d in range(2):
            S.dma('sp', dbg_out['H'][d], H_d[d], reads=['H_d'])
        return 'stop'

    with ExitStack() as pc:
        Gsb = sb("s_Gsb", [128, 64, 640], BF16, pc)
        wglu = sb("s_wglu", [128, 4, 512], BF16, pc)
        bglu = sb("s_bglu", [128, 4], F32, pc)
        ub = [sb("s_ub%d" % i, [128, 4, 544], BF16, pc) for i in range(2)]
        Hf = [sb("s_Hf%d" % i, [128, 16, 2, 32], BF16, pc) for i in range(2)]
        Hb = [sb("s_Hb%d" % i, [128, 16, 2, 32], BF16, pc) for i in range(2)]
        aT = [sb("s_aT%d" % i, [128, 4, 512], BF16, pc) for i in range(2)]
        sig = [sb("s_sig%d" % i, [128, 512], BF16, pc) for i in range(2)]
        yo = [sb("s_yo%d" % i, [128, 512], BF16, pc) for i in range(4)]
        py = [ps("s_py%d" % i, [128, 512], F32, pc) for i in range(2)]
        py4 = [ps("s_py4_%d" % i, [128, 16, 32], F32, pc) for i in range(2)]
        y4 = [sb("s_y4_%d" % i, [128, 512], F32, pc) for i in range(2)]
        ysum = [sb("s_ysum%d" % i, [128, 512], F32, pc) for i in range(2)]
        zlhs = sb("s_zlhs", [128, 128], BF16, pc)
        S.op('dve', lambda e: e.memset(zlhs[:], 0.0), writes=['zlhs'])
        pg = [ps("s_pg%d" % i, [128, 512], F32, pc) for i in range(2)]
        for c in range(4):
            S.dma('sp', Gsb[:, c * 16:(c + 1) * 16, :], G_d[:, c * 16:(c + 1) * 16, :], reads=['G_d'], writes=[('Gsb', c)])
        gkeys = [('Gsb', c) for c in range(4)]
        for q in range(4):
            S.dma('pool', wglu[:, q, :], wglu_in[q * 128:(q + 1) * 128, :], writes=['wglu'])
        S.dma('sp', bglu[:], bglu_in, writes=['bglu'])
        yo_ctr = 0
        for n in range(nbanks):
            i2 = n % 2
            e0 = 256 + 512 * n
            S.dma('sp', ub[i2][:], uT_d[:, e0 - 16:e0 + 528].rearrange("(q p) t -> p q t", p=128), reads=['uT_d'], writes=[('ub', i2)])
            kf = 16 + 32 * n
            kb = 32 * n
            S.dma('sp', Hf[i2][:], H_d[0, :, :, :, kf - 1:kf + 31], reads=['H_d'], writes=[('Hf', i2)])
            S.dma('sp', Hb[i2][:], H_d[1, :, :, :, kb + 1:kb + 33], reads=['H_d'], writes=[('Hb', i2)])
            for q in range(4):
                pyt = py[q % 2]; pk_ = ('py', q % 2)
                p4 = py4[q % 2]; p4k = ('py4', q % 2)
                pyv = pyt[:].rearrange("p (k j) -> p k j", j=16)
                uc = ub[i2][:, q, 16:528]
                ucv = uc.rearrange("p (k j) -> p k j", j=16)
                rd = [('ub', i2), 'Kt_sb', 'diagD']
                S.op('pe', lambda e: e.matmul(pyt[:], lhsT=diagD[:, q, :], rhs=uc, start=True, stop=False), reads=rd, writes=[pk_])
                S.op('pe', lambda e: e.matmul(pyt[:], lhsT=Kt_sb[:, (0 * 4 + q) * 16 + 0, :], rhs=uc, start=False, stop=False), reads=rd, writes=[pk_])
                for tau in range(1, 16):
                    S.op('pe', lambda e: e.matmul(pyv[:, :, tau:16], lhsT=Kt_sb[:, (0 * 4 + q) * 16 + tau, :], rhs=ucv[:, :, 0:16 - tau],
                                                  start=False, stop=False), reads=rd, writes=[pk_])
                S.op('pe', lambda e: e.matmul(pyt[:], lhsT=Kt_sb[:, (1 * 4 + q) * 16 + 0, :], rhs=uc, start=False, stop=False), reads=rd, writes=[pk_])
                for tau in range(1, 16):
                    S.op('pe', lambda e: e.matmul(pyv[:, :, 0:16 - tau], lhsT=Kt_sb[:, (1 * 4 + q) * 16 + tau, :], rhs=ucv[:, :, tau:16],
                                                  start=False, stop=(tau == 15)), reads=rd, writes=[pk_])
                S.op('pe', lambda e: e.matmul(p4[:].rearrange("p j k -> p (j k)"), lhsT=zlhs[:], rhs=uc, start=True, stop=False),
                     reads=['zlhs', ('ub', i2)], writes=[p4k])
                for pl in range(4):
                    pair = q * 4 + pl
                    for d in range(2):
                        Hx = Hf[i2] if d == 0 else Hb[i2]
                        hkey = ('Hf', i2) if d == 0 else ('Hb', i2)
                        for j in range(16):
                            m = j + 1 if d == 0 else 16 - j
                            if pl < 3:
                                osl = p4[32 * pl:32 * pl + 32, j, :]; csl = slice(q * 160 + 32 * pl, q * 160 + 32 * pl + 32)
                            else:
                                osl = p4[64:128, j, :]; csl = slice(q * 160 + 96, q * 160 + 160)
                            for reim in range(2):
                                last = (pl == 3 and d == 1 and j == 15 and reim == 1)
                                S.op('pe', lambda e: e.matmul(osl, lhsT=Gsb[:, (d * 16 + m - 1) * 2 + reim, csl], rhs=Hx[:, pair, reim, :],
                                                              start=False, stop=last), reads=gkeys + [hkey], writes=[p4k])
                S.op('act', lambda e: e.activation(out=y4[q % 2][:].rearrange("p (k j) -> p j k", j=16), in_=p4[:], func=AF.Copy),
                     reads=[p4k], writes=[('y4', q % 2)])
                S.op('dve', lambda e: e.tensor_tensor(out=ysum[q % 2][:], in0=pyt[:], in1=y4[q % 2][:], op=ALU.add),
                     reads=[pk_, ('y4', q % 2)], writes=[('ysum', q % 2)])
                S.op('act', lambda e: e.activation(out=aT[i2][:, q, :], in_=ysum[q % 2][:], func=AF.Gelu), reads=[('ysum', q % 2)], writes=[('aT', i2, q)])
            for oc in range(4):
                pgt = pg[oc % 2]; pgk = ('pg', oc % 2)
                for q in range(4):
                    S.op('pe', lambda e: e.matmul(pgt[:], lhsT=wglu[:, q, oc * 128:(oc + 1) * 128], rhs=aT[i2][:, q, :],
                                                  start=(q == 0), stop=(q == 3)), reads=['wglu', ('aT', i2, q)], writes=[pgk])
                sg = sig[oc % 2]; sgk = ('sig', oc % 2)
                S.op('act', lambda e: e.activation(out=sg[:], in_=pgt[:], func=AF.Sigmoid, bias=bglu[:, oc:oc + 1]),
                     reads=[pgk, 'bglu'], writes=[sgk])
                yt = yo[yo_ctr % 4]; yk = ('yo', yo_ctr % 4); yo_ctr += 1
                S.op('dve', lambda e: e.tensor_tensor(out=yt[:], in0=aT[i2][:, oc, :], in1=sg[:], op=ALU.mult),
                     reads=[('aT', i2, oc), sgk], writes=[yk])
                S.dma('pool', yaT_d[oc * 128:(oc + 1) * 128, 512 * n:512 * n + 512], yt[:], reads=[yk], writes=['yaT_d'])
        S.barrier()
    if 's5' in dbg:
        dbg_out['yaT'] = dout("dbg_yaT", [512, 8192], BF16)
        for r0 in range(0, 512, 128):
            S.dma('sp', dbg_out['yaT'][r0:r0 + 128, :], yaT_d[r0:r0 + 128, :], reads=['yaT_d'])
    return 'ok'

NEG = -30000.0


def na_phase(nc, S, es, sb, ps, din, dscr, dout, dbg, dbg_out, identb, qT_d, kT_d, v_d, ybT_d, nblocks=16):
    rpbT_in = din("rpbT", [128, 8 * 14 * 64])
    maskT_in = din("maskT", [128, 64])
    with ExitStack() as pa:
        Tb = sb("n_Tb", [128, 8, 14, 64], F32, pa)
        mk = sb("n_mk", [128, 64], F32, pa)
        kctx = sb("n_kctx", [128, 4, 256], BF16, pa)
        vctx = sb("n_vctx", [128, 2, 8, 65], BF16, pa)
        qb = [sb("n_qb%d" % i, [128, 4, 512], BF16, pa) for i in range(2)]
        kw = [sb("n_kw%d" % i, [128, 4, 960], BF16, pa) for i in range(2)]
        vw = [sb("n_vw%d" % i, [128, 15, 8, 65], BF16, pa) for i in range(2)]
        sS = [sb("n_sS%d" % i, [128, 4, 64], F32, pa) for i in range(3)]
        P = [sb("n_Pn%d" % i, [128, 6, 64], BF16, pa) for i in range(4)]
        rc = [sb("n_rc%d" % i, [128, 8], F32, pa) for i in range(2)]
        ybt = [sb("n_ybt%d" % i, [128, 8, 64], BF16, pa) for i in range(2)]
        ybT = [sb("n_ybT%d" % i, [128, 4, 512], BF16, pa) for i in range(2)]
        pS = [ps("n_pS%d" % i, [128, 6, 64], F32, pa) for i in range(3)]
        pO = [ps("n_pO%d" % i, [128, 4, 65], F32, pa) for i in range(4)]
        pT = ps("n_pTn", [128, 4, 128], BF16, pa)

        S.dma('sp', Tb[:].rearrange("p h a q -> p (h a q)"), rpbT_in, writes=['Tb'])
        S.dma('sp', mk[:], maskT_in, writes=['mk'])
        S.dma('sp', kctx[:], kT_d[:, 0:256].rearrange("(c p) t -> p c t", p=128), reads=['kT_d'], writes=['kctx'])
        S.dma('sp', vctx[:], v_d[0:256].rearrange("(m p) h d -> p m h d", p=128), reads=['v_d'], writes=['vctx'])
        S.op('dve', lambda e: e.tensor_tensor(out=Tb[:].rearrange("p h a q -> p (h a) q"), in0=Tb[:].rearrange("p h a q -> p (h a) q"),
                                              in1=mk[:].unsqueeze(1).to_broadcast([128, 112, 64]), op=ALU.add),
             reads=['Tb', 'mk'], writes=['Tb'])
        item = 0
        pend = []
        for b in range(nblocks):
            i2 = b % 2
            r0 = 8 * b
            lo = min(max(r0 - 4, 0), 120)
            hi = min(max(r0 + 7 - 4, 0), 120) + 8
            nrows = hi - lo
            S.dma('sp', qb[i2][:], qT_d[:, 512 * b:512 * b + 512].rearrange("(c p) t -> p c t", p=128), reads=['qT_d'], writes=[('qb', i2)])
            S.dma('sp', kw[i2][:, :, 0:64 * nrows], kT_d[:, 256 + 64 * lo:256 + 64 * hi].rearrange("(c p) t -> p c t", p=128),
                  reads=['kT_d'], writes=[('kw', i2)])
            for s in range(nrows - 1):
                S.dma('sp', vw[i2][:, s], v_d[256 + 64 * (lo + s):256 + 64 * (lo + s) + 128], reads=['v_d'], writes=[('vw', i2)])
            for rr in range(8):
                r = r0 + rr
                krs = min(max(r - 4, 0), 120)
                a0 = krs - r + 7
                half = 64 * (r % 2)
                pp = (r // 2) % 2
                for h in range(8):
                    hc, hb = h // 2, 64 * (h % 2)
                    it = item % 3
                    ip = item % 4
                    item += 1
                    pst = pS[it]; psk = ('pS', it)
                    for c in range(4):
                        kc0 = 64 * (krs + 2 * c - lo)
                        S.op('pe', lambda e: e.matmul(pst[:, c, :], lhsT=kw[i2][hb:hb + 64, hc, kc0:kc0 + 128],
                                                      rhs=qb[i2][hb:hb + 64, hc, 64 * rr:64 * rr + 64], start=True, stop=True),
                             reads=[('kw', i2), ('qb', i2)], writes=[psk])
                    for c in range(2):
                        S.op('pe', lambda e: e.matmul(pst[:, 4 + c, :], lhsT=kctx[hb:hb + 64, hc, 128 * c:128 * c + 128],
                                                      rhs=qb[i2][hb:hb + 64, hc, 64 * rr:64 * rr + 64], start=True, stop=True),
                             reads=['kctx', ('qb', i2)], writes=[psk])
                    S.op('dve', lambda e: e.tensor_tensor(out=sS[it][:], in0=pst[:, 0:4, :], in1=Tb[:, h, a0:a0 + 7:2, :], op=ALU.add),
                         reads=[psk, 'Tb'], writes=[('sS', it)])
                    S.op('act', lambda e: e.activation(out=P[ip][:, 0:4, :], in_=sS[it][:], func=AF.Exp), reads=[('sS', it)], writes=[('P', ip)])
                    S.op('act', lambda e: e.activation(out=P[ip][:, 4:6, :], in_=pst[:, 4:6, :], func=AF.Exp), reads=[psk], writes=[('P', ip)])
                    if len(pend) == 2:
                        pend.pop(0)()
                    def mk_pv(it=ip, i2=i2, krs=krs, lo=lo, h=h, half=half, pp=pp, r=r, rr=rr, b=b):
                        def pv():
                            pot = pO[pp * 2 + h // 4]; pok = ('pO', pp * 2 + h // 4)
                            for c in range(6):
                                if c < 4:
                                    rhs = vw[i2][:, krs + 2 * c - lo, h, :]
                                    rk = ('vw', i2)
                                else:
                                    rhs = vctx[:, c - 4, h, :]
                                    rk = 'vctx'
                                S.op('pe', lambda e: e.matmul(pot[half:half + 64, h % 4, :], lhsT=P[it][:, c, :], rhs=rhs,
                                                              start=(c == 0), stop=(c == 5)), reads=[('P', it), rk], writes=[pok])
                            if h % 4 == 3:
                                hh = h // 4
                                yk = ('ybt', pp)
                                S.op('dve', lambda e: e.reciprocal(out=rc[pp][half:half + 64, 4 * hh:4 * hh + 4], in_=pot[half:half + 64, :, 64]),
                                     reads=[pok], writes=[('rc', pp)])
                                S.op('dve', lambda e: e.tensor_tensor(out=ybt[pp][half:half + 64, 4 * hh:4 * hh + 4, :], in0=pot[half:half + 64, :, 0:64],
                                                                      in1=rc[pp][half:half + 64, 4 * hh:4 * hh + 4].unsqueeze(2).to_broadcast([64, 4, 64]),
                                                                      op=ALU.mult), reads=[pok, ('rc', pp)], writes=[yk])
                            if h == 7 and r % 2 == 1:
                                tk = ('ybT', b % 2)
                                for c4 in range(4):
                                    S.op('pe', lambda e: e.transpose(out=pT[:, c4, :], in_=ybt[pp][:, 2 * c4:2 * c4 + 2, :].rearrange("p a d -> p (a d)"),
                                                                     identity=identb[:]), reads=[('ybt', pp), 'identb'], writes=['pTn'])
                                S.op('act', lambda e: e.activation(out=ybT[b % 2][:, :, 64 * (rr - 1):64 * (rr - 1) + 128], in_=pT[:], func=AF.Copy),
                                     reads=['pTn'], writes=[tk])
                                if rr == 7:
                                    S.dma('act', ybT_d[:, 512 * b:512 * b + 512].rearrange("(c p) t -> p c t", p=128), ybT[b % 2][:],
                                          reads=[tk], writes=['ybT_d'])
                        return pv
                    pend.append(mk_pv())
        for p_ in pend:
            p_()
        S.barrier()
    if 'na' in dbg:
        dbg_out['ybT'] = dout("dbg_ybT", [512, 8192], BF16)
        for r0 in range(0, 512, 128):
            S.dma('sp', dbg_out['ybT'][r0:r0 + 128, :], ybT_d[r0:r0 + 128, :], reads=['ybT_d'])


def na_host(inp):
    rpb = inp['na_rpb'][0].astype(np.float32)
    i = np.arange(2)[:, None, None, None]
    kc = np.arange(64)[None, :, None, None]
    a = np.arange(14)[None, None, :, None]
    qc = np.arange(64)[None, None, None, :]
    dcol = np.clip(kc - qc + 15, 0, 30)
    drow = a + i
    T = rpb[:, drow, dcol]
    T = np.broadcast_to(T, (8, 2, 64, 14, 64)).transpose(1, 2, 0, 3, 4).reshape(128, 8 * 14 * 64)
    q = np.arange(64)
    qcs = np.clip(q - 8, 0, 48)
    k = np.arange(64)[:, None]
    inw = (k >= qcs[None, :]) & (k < qcs[None, :] + 16)
    m = np.where(inw, 0.0, NEG).astype(np.float32)
    m = np.concatenate([m, m], axis=0)
    return {'rpbT': np.ascontiguousarray(T.astype(np.float32)), 'maskT': np.ascontiguousarray(m)}

D = 1024


def merge_phase(nc, S, es, sb, ps, din, dscr, dout, dbg, dbg_out, identf, identb, epsc, bc_g1, bc_mul2, bc_add2,
                xcat, out, yaT_d, ybT_d, sgaT_d, sgbT_d, h2_d, aff_d, affT_d, nblocks=16):
    wpa_in = din("w_proj_a", [512, D]); wpb_in = din("w_proj_b", [512, D]); wout_in = din("w_out", [D, D])
    wr_in = din("w_routerT", [128, 8, 16])
    with ExitStack() as pa:
        wpa = sb("g_wpa", [128, 4, D], BF16, pa); wpb = sb("g_wpb", [128, 4, D], BF16, pa)
        wout = sb("g_wout", [128, 8, D], BF16, pa)
        wr = sb("g_wr", [128, 8, 16], BF16, pa)
        ya = [sb("g_mya%d" % i, [128, 4, 512], BF16, pa) for i in range(2)]
        yb = [sb("g_myb%d" % i, [128, 4, 512], BF16, pa) for i in range(2)]
        sga = [sb("g_msga%d" % i, [128, 8, 512], BF16, pa) for i in range(2)]
        sgb = [sb("g_msgb%d" % i, [128, 8, 512], BF16, pa) for i in range(2)]
        mT = [sb("g_mT%d" % i, [128, 8, 512], BF16, pa) for i in range(2)]
        t1 = [sb("g_mt1_%d" % i, [128, 512], F32, pa) for i in range(2)]
        t2 = [sb("g_mt2_%d" % i, [128, 512], F32, pa) for i in range(2)]
        xt = [sb("g_mxt%d" % i, [128, D], F32, pa) for i in range(2)]
        x1 = [sb("g_mx1_%d" % i, [128, D], F32, pa) for i in range(3)]
        h2 = [sb("g_mh2_%d" % i, [128, D], F32, pa) for i in range(3)]
        h2b = [sb("g_mh2b_%d" % i, [128, D], BF16, pa) for i in range(3)]
        h2T = [sb("g_mh2T_%d" % i, [128, 8, 128], BF16, pa) for i in range(3)]
        junk = sb("g_mjunk", [128, D], BF16, pa)
        st = [sb("g_mst%d" % i, [128, 8], F32, pa) for i in range(3)]
        lg = [sb("g_mlg%d" % i, [128, 16], F32, pa) for i in range(3)]
        af = [sb("g_maf%d" % i, [128, 16], F32, pa) for i in range(3)]
        A16 = sb("g_A16", [16, 8192], F32, pa)
        pab = [ps("g_mpab%d" % i, [128, 512], F32, pa) for i in range(4)]
        pyo = ps("g_mpyo", [128, D], F32, pa)
        phT = ps("g_mphT", [128, 8, 128], BF16, pa)
        pmisc = ps("g_mpmisc", [128, 256], F32, pa)
        plg = pmisc[:, 0:16]
        paT = pmisc[0:16, 128:256]

        for q in range(4):
            S.dma('pool', wpa[:, q, :], wpa_in[q * 128:(q + 1) * 128, :], writes=['wpa'])
            S.dma('pool', wpb[:, q, :], wpb_in[q * 128:(q + 1) * 128, :], writes=['wpb'])
        for dc in range(8):
            S.dma('pool', wout[:, dc, :], wout_in[dc * 128:(dc + 1) * 128, :], writes=['wout'])
        S.dma('pool', wr[:], wr_in, writes=['wr'])
        tile_ctr = 0
        pend = []
        for b in range(nblocks):
            i2 = b % 2
            cs = slice(512 * b, 512 * b + 512)
            S.dma('sp', ya[i2][:], yaT_d[:, cs].rearrange("(c p) t -> p c t", p=128), reads=['yaT_d'], writes=[('ya', i2)])
            S.dma('sp', yb[i2][:], ybT_d[:, cs].rearrange("(c p) t -> p c t", p=128), reads=['ybT_d'], writes=[('yb', i2)])
            S.dma('sp', sga[i2][:], sgaT_d[:, cs].rearrange("(c p) t -> p c t", p=128), reads=['sgaT_d'], writes=[('sga', i2)])
            S.dma('sp', sgb[i2][:], sgbT_d[:, cs].rearrange("(c p) t -> p c t", p=128), reads=['sgbT_d'], writes=[('sgb', i2)])
            for dc in range(8):
                j2 = dc % 2
                pA_, pB_ = pab[2 * j2], pab[2 * j2 + 1]
                for q in range(4):
                    S.op('pe', lambda e: e.matmul(pA_[:], lhsT=wpa[:, q, dc * 128:(dc + 1) * 128], rhs=ya[i2][:, q, :],
                                                  start=(q == 0), stop=(q == 3)), reads=['wpa', ('ya', i2)], writes=[('pA', j2)])
                for q in range(4):
                    S.op('pe', lambda e: e.matmul(pB_[:], lhsT=wpb[:, q, dc * 128:(dc + 1) * 128], rhs=yb[i2][:, q, :],
                                                  start=(q == 0), stop=(q == 3)), reads=['wpb', ('yb', i2)], writes=[('pB', j2)])
                S.op('dve', lambda e: e.tensor_tensor(out=t1[j2][:], in0=pA_[:], in1=sga[i2][:, dc, :], op=ALU.mult),
                     reads=[('pA', j2), ('sga', i2)], writes=[('t1', j2)])
                S.op('dve', lambda e: e.tensor_tensor(out=t2[j2][:], in0=pB_[:], in1=sgb[i2][:, dc, :], op=ALU.mult),
                     reads=[('pB', j2), ('sgb', i2)], writes=[('t2', j2)])
                S.op('pool', lambda e: e.tensor_tensor(out=mT[i2][:, dc, :], in0=t1[j2][:], in1=t2[j2][:], op=ALU.add),
                     reads=[('t1', j2), ('t2', j2)], writes=[('mT', i2)])
            for ti in range(4):
                k2 = tile_ctr % 3
                kx = tile_ctr % 2
                tile_ctr += 1
                tok0 = 512 * b + 128 * ti
                S.dma('sp', xt[kx][:], xcat[256 + tok0:256 + tok0 + 128, :], writes=[('xt', kx)])
                for hf in range(2):
                    for dc in range(8):
                        S.op('pe', lambda e: e.matmul(pyo[:, hf * 512:(hf + 1) * 512], lhsT=mT[i2][:, dc, ti * 128:(ti + 1) * 128],
                                                      rhs=wout[:, dc, hf * 512:(hf + 1) * 512], start=(dc == 0), stop=(dc == 7)),
                             reads=[('mT', i2), 'wout'], writes=['pyo'])
                S.op('dve', lambda e: e.tensor_tensor(out=x1[k2][:], in0=pyo[:], in1=bc_g1[:], op=ALU.mult),
                     reads=['pyo', 'bc_g1'], writes=[('x1', k2)])
                S.op('dve', lambda e: e.tensor_tensor(out=x1[k2][:], in0=x1[k2][:], in1=xt[kx][:], op=ALU.add),
                     reads=[('x1', k2), ('xt', kx)], writes=[('x1', k2)])
                S.dma('pool', out[tok0:tok0 + 128, :], x1[k2][:], reads=[('x1', k2)], writes=['out'])
                s_ = st[k2]; sk = ('st', k2)
                S.op('act', lambda e: e.activation(out=junk[:], in_=x1[k2][:], func=AF.Square, accum_out=s_[:, 0:1]),
                     reads=[('x1', k2)], writes=['mjunk', sk])
                S.op('act', lambda e: e.activation(out=s_[:, 6:7], in_=s_[:, 0:1], func=AF.Ln, scale=1.0 / D, bias=epsc[:, 0:1]),
                     reads=[sk, 'epsc'], writes=[sk])
                S.op('act', lambda e: e.activation(out=s_[:, 1:2], in_=s_[:, 6:7], func=AF.Exp, scale=-0.5), reads=[sk], writes=[sk])
                S.op('dve', lambda e: e.scalar_tensor_tensor(out=h2[k2][:], in0=x1[k2][:], scalar=s_[:, 1:2], in1=bc_mul2[:],
                                                             op0=ALU.mult, op1=ALU.mult), reads=[('x1', k2), sk, 'bc_mul2'], writes=[('h2', k2)])
                S.op('dve', lambda e: e.tensor_tensor(out=h2[k2][:], in0=h2[k2][:], in1=bc_add2[:], op=ALU.add),
                     reads=[('h2', k2), 'bc_add2'], writes=[('h2', k2)])
                S.op('act', lambda e: e.activation(out=h2b[k2][:], in_=h2[k2][:], func=AF.Copy), reads=[('h2', k2)], writes=[('h2b', k2)])
                S.dma('act', h2_d[tok0:tok0 + 128, :], h2b[k2][:], reads=[('h2b', k2)], writes=['h2_d'])
                def mk_tail(k2=k2, tok0=tok0, s_=s_, sk=sk):
                    def tail():
                        for dc in range(8):
                            S.op('pe', lambda e: e.transpose(out=phT[:, dc, :], in_=h2b[k2][:, dc * 128:(dc + 1) * 128], identity=identb[:]),
                                 reads=[('h2b', k2), 'identb'], writes=['phT'])
                        S.op('act', lambda e: e.activation(out=h2T[k2][:], in_=phT[:], func=AF.Copy), reads=['phT'], writes=[('h2T', k2)])
                        for dc in range(8):
                            S.op('pe', lambda e: e.matmul(plg, lhsT=h2T[k2][:, dc, :], rhs=wr[:, dc, :], start=(dc == 0), stop=(dc == 7)),
                                 reads=[('h2T', k2), 'wr'], writes=['plg'])
                        S.op('dve', lambda e: e.tensor_copy(out=lg[k2][:], in_=plg), reads=['plg'], writes=[('lg', k2)])
                        S.op('dve', lambda e: e.reduce_max(out=s_[:, 2:3], in_=lg[k2][:], axis=AX.X), reads=[('lg', k2)], writes=[sk])
                        S.op('dve', lambda e: e.tensor_scalar(out=s_[:, 3:4], in0=s_[:, 2:3], scalar1=-1.0, scalar2=None, op0=ALU.mult),
                             reads=[sk], writes=[sk])
                        S.op('act', lambda e: e.activation(out=af[k2][:], in_=lg[k2][:], func=AF.Exp, bias=s_[:, 3:4], accum_out=s_[:, 4:5]),
                             reads=[('lg', k2), sk], writes=[('af', k2), sk])
                        S.op('dve', lambda e: e.reciprocal(out=s_[:, 5:6], in_=s_[:, 4:5]), reads=[sk], writes=[sk])
                        S.op('dve', lambda e: e.tensor_scalar(out=af[k2][:], in0=af[k2][:], scalar1=s_[:, 5:6], scalar2=None, op0=ALU.mult),
                             reads=[('af', k2), sk], writes=[('af', k2)])
                        S.dma('pool', aff_d[tok0:tok0 + 128, :], af[k2][:], reads=[('af', k2)], writes=['aff_d'])
                        S.op('pe', lambda e: e.transpose(out=paT, in_=af[k2][:], identity=identf[:]), reads=[('af', k2), 'identf'], writes=['paT'])
                        S.op('act', lambda e: e.activation(out=A16[:, tok0:tok0 + 128], in_=paT, func=AF.Copy), reads=['paT'], writes=['A16'])
                    return tail
                if len(pend) == 2:
                    pend.pop(0)()
                pend.append(mk_tail())
        for t_ in pend:
            t_()
        S.dma('sp', affT_d, A16[:], reads=['A16'], writes=['affT_d'])
        S.barrier()
        if 'mg2' in dbg:
            for nm, t, shp, dt in (('bcg1', bc_g1, [128, D], F32), ('mT0', mT[(nblocks - 1) % 2], [128, 8, 512], BF16), ('t1', t1[1], [128, 512], F32),
                                   ('sga0', sga[(nblocks - 1) % 2], [128, 8, 512], BF16), ('ya0', ya[(nblocks - 1) % 2], [128, 4, 512], BF16),
                                   ('yb0', yb[(nblocks - 1) % 2], [128, 4, 512], BF16), ('wpa', wpa, [128, 4, D], BF16), ('x1t', x1[1], [128, D], F32),
                                   ('xt', xt[1], [128, D], F32)):
                dbg_out[nm] = dout("dbg_" + nm, shp, dt)
                S.dma('sp', dbg_out[nm], t[:], reads=[])
    if 'mg' in dbg:
        dbg_out['aff'] = dout("dbg_aff", [8192, 16])
        S.dma('sp', dbg_out['aff'], aff_d, reads=['aff_d'])
        dbg_out['affT'] = dout("dbg_affT", [16, 8192])
        S.dma('sp', dbg_out['affT'], affT_d, reads=['affT_d'])
        dbg_out['h2'] = dout("dbg_h2", [8192, D], BF16)
        for r0 in range(0, 8192, 1024):
            S.dma('sp', dbg_out['h2'][r0:r0 + 1024, :], h2_d[r0:r0 + 1024, :], reads=['h2_d'])

D = 1024
CAP = 1024
NT = 8192


def moe_phase(nc, S, es, sb, ps, din, dscr, dout, dbg, dbg_out, identf, identb, bc_g2, out, h2_d, aff_d, affT_d, nexp=16):
    wg_in = din("w_e_gate", [16, D, D]); wu_in = din("w_e_up", [16, D, D]); wd_in = din("w_e_down", [16, D, D])
    m16_in = din("m16", [128, 128])
    with ExitStack() as pm:
        cum_d = dscr("cum_d", [16, NT], F32)
        slotv = sb("e_slotv", [128, 8], F32, pm)
        thr_dbg = sb("e_thr", [128, 1], F32, pm)
        S.op('pool', lambda e: e.iota(slotv[:], pattern=[[128, 8]], base=0, channel_multiplier=1, allow_small_or_imprecise_dtypes=True),
             writes=['slotv'])
        with ExitStack() as pa:
            A128 = sb("e_A128", [128, 1024], F32, pa)
            A16 = sb("e_A16", [16, NT], F32, pa)
            C16 = sb("e_C16", [16, NT], F32, pa)
            M16 = sb("e_M16", [128, 128], F32, pa)
            junk = sb("e_junk", [128, 1024], BF16, pa)
            lo = sb("e_lo", [128, 1], F32, pa); mid = sb("e_mid", [128, 1], F32, pa)
            cnt = sb("e_cnt", [128, 1], F32, pa); g = sb("e_g", [128, 1], F32, pa)
            one16 = sb("e_one16", [16, 1], F32, pa)
            ptot = ps("e_ptot", [128, 1], F32, pa)
            for s in range(8):
                S.dma('sp', A128[16 * s:16 * s + 16, :], affT_d[:, 1024 * s:1024 * s + 1024], reads=['affT_d'], writes=['A128'])
            S.dma('sp', A16[:], affT_d, reads=['affT_d'], writes=['A16'])
            S.dma('sp', M16[:], m16_in, writes=['M16'])
            S.op('dve', lambda e: e.memset(lo[:], 0.0), writes=['lo'])
            S.op('dve', lambda e: e.memset(one16[:], 1.0), writes=['one16'])
            for k in range(30):
                hk = 2.0 ** -(k + 1)
                S.op('dve', lambda e: e.tensor_scalar(out=mid[:], in0=lo[:], scalar1=hk, scalar2=None, op0=ALU.add), reads=['lo'], writes=['mid'])
                S.op('dve', lambda e: e.tensor_scalar(out=junk[:], in0=A128[:], scalar1=mid[:, 0:1], scalar2=0.0, op0=ALU.is_gt, op1=ALU.add,
                                                      accum_out=cnt[:]), reads=['A128', 'mid'], writes=['junk', 'cnt'])
                S.op('pe', lambda e: e.matmul(ptot[:], lhsT=M16[:], rhs=cnt[:], start=True, stop=True), reads=['M16', 'cnt'], writes=['ptot'])
                S.op('dve', lambda e: e.tensor_scalar(out=g[:], in0=ptot[:], scalar1=float(CAP), scalar2=hk, op0=ALU.is_ge, op1=ALU.mult),
                     reads=['ptot'], writes=['g'])
                S.op('dve', lambda e: e.tensor_tensor(out=lo[:], in0=lo[:], in1=g[:], op=ALU.add), reads=['lo', 'g'], writes=['lo'])
            S.op('dve', lambda e: e.tensor_copy(out=thr_dbg[:], in_=lo[:]), reads=['lo'], writes=['thr'])
            S.op('dve', lambda e: e.tensor_scalar(out=A16[:], in0=A16[:], scalar1=lo[0:16, 0:1], scalar2=None, op0=ALU.is_gt),
                 reads=['A16', 'lo'], writes=['A16'])
            S.op('dve', lambda e: e.tensor_tensor_scan(out=C16[:], data0=one16[:, 0:1].to_broadcast([16, NT]), data1=A16[:], initial=0.0,
                                                       op0=ALU.mult, op1=ALU.add), reads=['A16', 'one16'], writes=['C16'])
            S.dma('sp', cum_d, C16[:], reads=['C16'], writes=['cum_d'])
            S.barrier()
        if 'moe_thr' in dbg:
            dbg_out['thr'] = dout("dbg_thr", [128, 1])
            S.dma('sp', dbg_out['thr'], thr_dbg[:], reads=['thr'])
        with ExitStack() as pe_:
            Wg = [sb("e_Wg%d" % i, [128, 8, D], BF16, pe_) for i in range(2)]
            Wu = [sb("e_Wu%d" % i, [128, 8, D], BF16, pe_) for i in range(2)]
            Wd = [sb("e_Wd%d" % i, [128, 8, D], BF16, pe_) for i in range(2)]
            xe = [sb("e_xe%d" % i, [128, D], BF16, pe_) for i in range(8)]
            ag = [[sb("e_ag%d_%d" % (p_, i), [128, 16], F32, pe_) for i in range(8)] for p_ in range(2)]
            cq = [sb("e_cq%d" % i, [128, 2048], F32, pe_) for i in range(2)]
            junkq = sb("e_junkq", [128, 2048], BF16, pe_)
            acc = [sb("e_acc%d" % i, [128, 4, 8], F32, pe_) for i in range(3)]
            idxs = sb("e_idxs", [128, 8], F32, pe_)
            idxi = [sb("e_idxi%d" % i, [128, 8], I32, pe_) for i in range(3)]
            xeT = sb("e_xeT", [128, 8, CAP], BF16, pe_)
            hidT = sb("e_hidT", [128, 8, CAP], BF16, pe_)
            sg = [sb("e_sg%d" % i, [128, 512], F32, pe_) for i in range(2)]
            ye = [sb("e_ye%d" % i, [128, D], F32, pe_) for i in range(2)]
            pGU = [ps("e_pGU%d" % i, [128, 512], F32, pe_) for i in range(3)]
            pY = ps("e_pY", [128, D], F32, pe_)
            pT = ps("e_pT", [128, 8, 128], BF16, pe_)
            pTi = pT[:].bitcast(F32) if False else None

            def load_w(e):
                i2 = e % 2
                for dc in range(8):
                    S.dma('pool', Wg[i2][:, dc, :], wg_in[e, dc * 128:(dc + 1) * 128, :], writes=[('Wg', i2)])
                    S.dma('pool', Wu[i2][:, dc, :], wu_in[e, dc * 128:(dc + 1) * 128, :], writes=[('Wu', i2)])
                for dc in range(8):
                    S.dma('pool', Wd[i2][:, dc, :], wd_in[e, dc * 128:(dc + 1) * 128, :], writes=[('Wd', i2)])

            def load_cq(n):
                e_, qd = n // 4, n % 4
                if e_ >= nexp:
                    return
                S.dma('sp', cq[n % 2][:], cum_d[e_:e_ + 1, qd * 2048:(qd + 1) * 2048].to_broadcast([128, 2048]),
                      reads=['cum_d'], writes=[('cq', n % 2)])

            def idx_piece(e, piece):
                j3 = e % 3
                n = e * 4 + piece // 2
                if piece % 2 == 0:
                    if n == 0:
                        load_cq(0)
                    load_cq(n + 1)
                for sc in range(4 * (piece % 2), 4 * (piece % 2) + 4):
                    S.op('dve', lambda e_: e_.tensor_scalar(out=junkq[:], in0=cq[n % 2][:], scalar1=slotv[:, sc:sc + 1], scalar2=0.0,
                                                            op0=ALU.is_le, op1=ALU.add, accum_out=acc[j3][:, piece // 2, sc:sc + 1]),
                         reads=[('cq', n % 2), 'slotv'], writes=['junkq', ('acc', j3)])
                if piece == 7:
                    S.op('dve', lambda e_: e_.tensor_tensor(out=idxs[:], in0=acc[j3][:, 0, :], in1=acc[j3][:, 1, :], op=ALU.add),
                         reads=[('acc', j3)], writes=['idxs'])
                    S.op('dve', lambda e_: e_.tensor_tensor(out=idxs[:], in0=idxs[:], in1=acc[j3][:, 2, :], op=ALU.add),
                         reads=[('acc', j3), 'idxs'], writes=['idxs'])
                    S.op('dve', lambda e_: e_.tensor_tensor(out=idxs[:], in0=idxs[:], in1=acc[j3][:, 3, :], op=ALU.add),
                         reads=[('acc', j3), 'idxs'], writes=['idxs'])
                    S.op('dve', lambda e_: e_.tensor_copy(out=idxi[j3][:], in_=idxs[:]), reads=['idxs'], writes=[('idxi', j3)])

            def gather(e):
                j2 = e % 3
                for sc in range(8):
                    S.custom_dma('pool', lambda e_: e_.indirect_dma_start(out=xe[sc][:, :], out_offset=None, in_=h2_d[:, :],
                                 in_offset=bass.IndirectOffsetOnAxis(ap=idxi[j2][:, sc:sc + 1], axis=0)),
                                 reads=[('idxi', j2), 'h2_d'], writes=[('xe', sc)])
                    S.custom_dma('pool', lambda e_: e_.indirect_dma_start(out=ag[e % 2][sc][:, :], out_offset=None, in_=aff_d[:, :],
                                 in_offset=bass.IndirectOffsetOnAxis(ap=idxi[j2][:, sc:sc + 1], axis=0)),
                                 reads=[('idxi', j2), 'aff_d'], writes=[('ag', e % 2, sc)])

            gu_ctr = [0]

            def xpose(e):
                for sc in range(8):
                    for dc in range(8):
                        S.op('pe', lambda e_: e_.transpose(out=pT[:, dc, :], in_=xe[sc][:, dc * 128:(dc + 1) * 128], identity=identb[:]),
                             reads=[('xe', sc), 'identb'], writes=['pT'])
                    S.op('act', lambda e_: e_.activation(out=xeT[:, :, sc * 128:(sc + 1) * 128], in_=pT[:], func=AF.Copy),
                         reads=['pT'], writes=['xeT'])
            def ffn(e, nxt):
                i2 = e % 2; j2 = e % 3
                for fc in range(8):
                    for hf in range(2):
                        a = gu_ctr[0] % 3; gu_ctr[0] += 1
                        b = gu_ctr[0] % 3; gu_ctr[0] += 1
                        for dc in range(8):
                            S.op('pe', lambda e_: e_.matmul(pGU[a][:], lhsT=Wg[i2][:, dc, fc * 128:(fc + 1) * 128], rhs=xeT[:, dc, hf * 512:(hf + 1) * 512],
                                                            start=(dc == 0), stop=(dc == 7)), reads=[('Wg', i2), 'xeT'], writes=[('pGU', a)])
                        for dc in range(8):
                            S.op('pe', lambda e_: e_.matmul(pGU[b][:], lhsT=Wu[i2][:, dc, fc * 128:(fc + 1) * 128], rhs=xeT[:, dc, hf * 512:(hf + 1) * 512],
                                                            start=(dc == 0), stop=(dc == 7)), reads=[('Wu', i2), 'xeT'], writes=[('pGU', b)])
                        s2 = (fc * 2 + hf) % 2
                        S.op('act', lambda e_: e_.activation(out=sg[s2][:], in_=pGU[a][:], func=AF.Silu), reads=[('pGU', a)], writes=[('sg', s2)])
                        S.op('dve', lambda e_: e_.tensor_tensor(out=hidT[:, fc, hf * 512:(hf + 1) * 512], in0=pGU[b][:], in1=sg[s2][:], op=ALU.mult),
                             reads=[('pGU', b), ('sg', s2)], writes=['hidT'])
                    if nxt is not None:
                        idx_piece(nxt, fc)
                for sc in range(8):
                    for hf in range(2):
                        for fc in range(8):
                            S.op('pe', lambda e_: e_.matmul(pY[:, hf * 512:(hf + 1) * 512], lhsT=hidT[:, fc, sc * 128:(sc + 1) * 128],
                                                            rhs=Wd[i2][:, fc, hf * 512:(hf + 1) * 512], start=(fc == 0), stop=(fc == 7)),
                                 reads=['hidT', ('Wd', i2)], writes=['pY'])
                    y2 = sc % 2
                    S.op('dve', lambda e_: e_.scalar_tensor_tensor(out=ye[y2][:], in0=pY[:], scalar=ag[e % 2][sc][:, e:e + 1], in1=bc_g2[:],
                                                                   op0=ALU.mult, op1=ALU.mult), reads=['pY', ('ag', e % 2, sc), 'bc_g2'], writes=[('ye', y2)])
                    S.custom_dma('pool', lambda e_: e_.indirect_dma_start(out=out[:, :], out_offset=bass.IndirectOffsetOnAxis(ap=idxi[j2][:, sc:sc + 1], axis=0),
                                 in_=ye[y2][:, :], in_offset=None, compute_op=ALU.add), reads=[('ye', y2), ('idxi', j2)], writes=['out'])

            load_w(0)
            for p in range(8):
                idx_piece(0, p)
            gather(0)
            if nexp > 1:
                load_w(1)
                for p in range(8):
                    idx_piece(1, p)
            for e in range(nexp):
                xpose(e)
                if e + 1 < nexp:
                    gather(e + 1)
                    if e >= 1:
                        load_w(e + 1)
                ffn(e, e + 2 if e + 2 < nexp else None)
            if 'moe_idx' in dbg:
                dbg_out['idxi'] = dout("dbg_idxi", [128, 8], I32)
                S.dma('sp', dbg_out['idxi'], idxi[(nexp - 1) % 3][:], reads=[('idxi', (nexp - 1) % 3)])
            S.barrier()


def moe_host(inp):
    f = np.float32
    m16 = np.zeros((128, 128), f)
    for s in range(8):
        for s2 in range(8):
            m16[16 * s:16 * s + 16, 16 * s2:16 * s2 + 16] = np.eye(16, dtype=f)
    return {'w_e_gate': np.ascontiguousarray(inp['w_e_gate'][0]), 'w_e_up': np.ascontiguousarray(inp['w_e_up'][0]),
            'w_e_down': np.ascontiguousarray(inp['w_e_down'][0]), 'm16': m16}


D = 1024
NLAT = 8192
NCTX = 256
NEXT = NLAT + NCTX
NE2 = NEXT + NCTX
EPS = 1e-6
INCOLS = 4096


def build(stop_after=99, dbg=(), nblk_lim=17, skip_s5=False, mg_blocks=16, nexp=16):
    nc = bass.Bass("TRN2", target_bir_lowering=False)
    es = ExitStack()
    S = Sched(nc, es)
    dbg_out = {}

    def din(name, shape, dt=F32):
        return nc.dram_tensor(name, list(shape), dt, kind="ExternalInput").ap()

    def dscr(name, shape, dt):
        return nc.dram_tensor(name, list(shape), dt, kind="Internal").ap()

    def dout(name, shape, dt=F32):
        return nc.dram_tensor(name, list(shape), dt, kind="ExternalOutput").ap()

    def sb(name, shape, dt, stack=None):
        return (stack or es).enter_context(nc.sbuf_tensor(name, list(shape), dt))

    def ps(name, shape, dt, stack=None):
        return (stack or es).enter_context(nc.psum_tensor(name, list(shape), dt))

    xcat = din("xcat", [NEXT, D])
    cT = din("cT", [128, 8, 2])
    w_ada = din("w_ada", [D, 6 * D])
    b_ada = din("b_ada", [1, 6 * D])
    nmixT = din("nmixT", [128, 8])
    nffn = din("nffn", [1, D])
    w_in = din("w_in", [D, INCOLS])
    ident_in = din("ident", [128, 128])
    qkg = din("qkg", [128, 2])

    out = dout("out", [NLAT, D])

    uT_d = dscr("uT_d", [512, NE2], BF16)
    qT_d = dscr("qT_d", [512, NLAT], BF16)
    kT_d = dscr("kT_d", [512, NEXT], BF16)
    v_d = dscr("v_d", [NEXT, 8, 65], BF16)
    sgaT_d = dscr("sgaT_d", [D, NLAT], BF16)
    sgbT_d = dscr("sgbT_d", [D, NLAT], BF16)

    identf = sb("identf", [128, 128], F32)
    identb = sb("identb", [128, 128], BF16)
    mul1 = sb("mul1", [128, 8, 2], F32)
    add1 = sb("add1", [128, 8, 2], F32)
    bc_g1 = sb("bc_g1", [128, D], F32)
    bc_mul2 = sb("bc_mul2", [128, D], F32)
    bc_add2 = sb("bc_add2", [128, D], F32)
    bc_g2 = sb("bc_g2", [128, D], F32)

    epsc = sb("epsc", [128, 1], F32)
    S.op('dve', lambda e: e.memset(epsc[:], EPS), writes=['epsc'])
    S.dma('sp', identf[:], ident_in, writes=['identf'])
    S.dma('pool', identb[:], ident_in, writes=['identb'])

    with ExitStack() as p0:
        wada = sb("wada", [128, 8, 6 * D], BF16, p0)
        cs_f = sb("cs_f", [128, 8, 2], F32, p0)
        cs = sb("cs", [128, 8, 2], BF16, p0)
        brow = sb("brow", [2, 6 * D], F32, p0)
        modrow = sb("modrow", [2, 6 * D], F32, p0)
        sel = sb("sel", [2, 128], F32, p0)
        nmix = sb("nmix", [128, 8], F32, p0)
        colsb = sb("colsb", [128, 16, 2], F32, p0)
        pm = [ps("pm%d" % i, [2, 512], F32, p0) for i in range(2)]
        pcol = ps("pcol", [128, 16, 2], F32, p0)
        pbc = [ps("pbc%d" % i, [128, 512], F32, p0) for i in range(2)]

        S.dma('sp', cs_f[:], cT, writes=['cs_f'])
        S.dma('sp', brow[0:1, :], b_ada, writes=['brow0'])
        S.dma('sp', brow[1:2, :], b_ada, writes=['brow1'])
        S.dma('sp', nmix[:], nmixT, writes=['nmix'])
        S.dma('sp', bc_mul2[:], nffn.to_broadcast([128, D]), writes=['bc_mul2'])
        for dc in range(8):
            S.dma('pool', wada[:, dc, :].rearrange("p (a b) -> p a b", b=2048),
                  w_ada[dc * 128:(dc + 1) * 128, :].rearrange("p (a b) -> p a b", b=2048),
                  writes=[('wada', dc)])
        S.op('act', lambda e: e.activation(out=cs[:], in_=cs_f[:], func=AF.Silu), reads=['cs_f'], writes=['cs'])
        S.op('dve', lambda e: e.memset(sel[:], 0.0), writes=['sel'])
        S.op('dve', lambda e: e.memset(sel[0:1, :], 1.0), writes=['sel'])
        for cc in range(12):
            pt = pm[cc % 2]
            for dc in range(8):
                S.op('pe', lambda e: e.matmul(pt[:], lhsT=cs[:, dc, :], rhs=wada[:, dc, cc * 512:(cc + 1) * 512],
                                              start=(dc == 0), stop=(dc == 7)),
                     reads=['cs', ('wada', dc)], writes=[('pm', cc % 2)])
            S.op('dve', lambda e: e.tensor_tensor(out=modrow[:, cc * 512:(cc + 1) * 512], in0=pt[:],
                                                  in1=brow[:, cc * 512:(cc + 1) * 512], op=ALU.add),
                 reads=[('pm', cc % 2), 'brow0', 'brow1'], writes=[('modrow', cc)])
        for i in range(16):
            S.op('pe', lambda e: e.transpose(out=pcol[:, i, :], in_=modrow[0:2, i * 128:(i + 1) * 128],
                                             identity=identf[0:2, 0:2]),
                 reads=[('modrow', i // 4), 'identf'], writes=['pcol'])
        S.op('dve', lambda e: e.tensor_copy(out=colsb[:], in_=pcol[:]), reads=['pcol'], writes=['colsb'])
        S.op('dve', lambda e: e.tensor_copy(out=add1[:], in_=colsb[:, 0:8, :]), reads=['colsb'], writes=['add1'])
        S.op('dve', lambda e: e.tensor_scalar(out=colsb[:, 8:16, :], in0=colsb[:, 8:16, :], scalar1=1.0, scalar2=None,
                                              op0=ALU.add), reads=['colsb'], writes=['colsb'])
        S.op('dve', lambda e: e.tensor_tensor(out=mul1[:], in0=colsb[:, 8:16, :],
                                              in1=nmix[:].unsqueeze(2).to_broadcast([128, 8, 2]), op=ALU.mult),
             reads=['colsb', 'nmix'], writes=['mul1'])

        def bcast(dst, col0, mode, dkey):
            for h in range(2):
                pt = pbc[h]
                S.op('pe', lambda e: e.matmul(pt[:], lhsT=sel[:], rhs=modrow[:, col0 + h * 512: col0 + (h + 1) * 512],
                                              start=True, stop=True),
                     reads=['sel'] + [('modrow', c) for c in range(12)], writes=[('pbc', h)])
                dsl = dst[:, h * 512:(h + 1) * 512]
                if mode == 'copy':
                    S.op('dve', lambda e: e.tensor_copy(out=dsl, in_=pt[:]), reads=[('pbc', h)], writes=[dkey])
                else:
                    S.op('dve', lambda e: e.scalar_tensor_tensor(out=dsl, in0=pt[:], scalar=1.0, in1=dsl,
                                                                 op0=ALU.add, op1=ALU.mult),
                         reads=[('pbc', h), dkey], writes=[dkey])
        bcast(bc_g1, 2048, 'copy', 'bc_g1')
        bcast(bc_add2, 3072, 'copy', 'bc_add2')
        bcast(bc_mul2, 4096, 'mul1p', 'bc_mul2')
        bcast(bc_g2, 5120, 'copy', 'bc_g2')
        if 'mod' in dbg:
            dbg_out['mod'] = dout("dbg_mod", [2, 6 * D])
            S.dma('sp', dbg_out['mod'], modrow[:], reads=[('modrow', c) for c in range(12)])
            dbg_out['mul1'] = dout("dbg_mul1", [128, 8, 2])
            S.dma('sp', dbg_out['mul1'], mul1[:], reads=['mul1'])
            dbg_out['bcm2'] = dout("dbg_bcm2", [128, D])
            S.dma('sp', dbg_out['bcm2'], bc_mul2[:], reads=['bc_mul2'])
        S.barrier()

    if stop_after <= 0:
        return finish(nc, S, es, out, dbg_out)

    with ExitStack() as p1:
        win = sb("win", [128, 8, INCOLS], BF16, p1)
        xt = [sb("xt%d" % i, [128, D], F32, p1) for i in range(2)]
        junk = sb("junk", [128, D], BF16, p1)
        xn = [sb("xn%d" % i, [128, D], BF16, p1) for i in range(4)]
        ss = [sb("ss%d" % i, [128, 1], F32, p1) for i in range(4)]
        rstd = [sb("rstd%d" % i, [128, 1], F32, p1) for i in range(4)]
        hT = [sb("hT%d" % i, [128, 8, 512], BF16, p1) for i in range(2)]
        stg = [sb("stg%d" % i, [128, 512], BF16, p1) for i in range(4)]
        sq = [sb("sq%d" % i, [128, 512], BF16, p1) for i in range(2)]
        qk32 = [sb("qk32_%d" % i, [128, 512], F32, p1) for i in range(2)]
        rs = [sb("rs%d" % i, [128, 512], F32, p1) for i in range(2)]
        vst = [sb("vst%d" % i, [128, 8, 65], BF16, p1) for i in range(2)]
        bones = sb("bones", [128, 128], BF16, p1)
        gq = sb("gq", [128, 2], F32, p1)
        pT = [ps("pT%d" % i, [128, 8, 128], BF16, p1) for i in range(2)]
        pz = [ps("pz%d" % i, [128, 512], F32, p1) for i in range(4)]
        pms = [ps("pms%d" % i, [128, 512], F32, p1) for i in range(2)]

        for dc in range(8):
            S.dma('pool', win[:, dc, :].rearrange("p (a b) -> p a b", b=2048),
                  w_in[dc * 128:(dc + 1) * 128, :].rearrange("p (a b) -> p a b", b=2048),
                  writes=[('win', dc)])
        S.dma('sp', gq[:], qkg, writes=['gq'])
        S.op('dve', lambda e: e.tensor_scalar(out=gq[:, 0:1], in0=gq[:, 0:1], scalar1=0.125, scalar2=None, op0=ALU.mult), reads=['gq'], writes=['gq'])
        for i in range(2):
            S.op('dve', lambda e: e.memset(vst[i][:], 1.0), writes=[('vst', i)])
        S.op('dve', lambda e: e.memset(bones[:], 0.0), writes=['bones'])
        S.op('dve', lambda e: e.memset(bones[0:64, 0:64], 1.0 / 64), writes=['bones'])
        S.op('dve', lambda e: e.memset(bones[64:128, 64:128], 1.0 / 64), writes=['bones'])

        nblk = (NEXT + 511) // 512
        ctr = {'tile': 0, 'tpe': 0, 'stg': 0, 'z': 0, 'v': 0}

        def blk_info(b):
            if b == 0:
                return 0, 256
            return 256 + (b - 1) * 512, 512

        def head_pre(b):
            t0, nt = blk_info(b)
            for ti in range(nt // 128):
                i2 = ctr['tile'] % 2
                i4 = ctr['tile'] % 4
                ctr['tile'] += 1
                r0 = t0 + ti * 128
                S.dma('sp', xt[i2][:], xcat[r0:r0 + 128, :], writes=[('xt', i2)])
                S.op('act', lambda e: e.activation(out=junk[:], in_=xt[i2][:], func=AF.Square, accum_out=ss[i4][:]),
                     reads=[('xt', i2)], writes=['junk', ('ss', i4)])
                S.op('act', lambda e: e.activation(out=rstd[i4][:], in_=ss[i4][:], func=AF.Ln, scale=1.0 / D, bias=epsc[:, 0:1]),
                     reads=[('ss', i4), 'epsc'], writes=[('rstd', i4)])
                S.op('act', lambda e: e.activation(out=rstd[i4][:], in_=rstd[i4][:], func=AF.Exp, scale=-0.5),
                     reads=[('rstd', i4)], writes=[('rstd', i4)])
                S.op('act', lambda e: e.activation(out=xn[i4][:], in_=xt[i2][:], func=AF.Copy, scale=rstd[i4][:, 0:1]),
                     reads=[('xt', i2), ('rstd', i4)], writes=[('xn', i4)])

        def head_pe(b):
            t0, nt = blk_info(b)
            j = 1 if b == 0 else 0
            hb = hT[b % 2]
            hkey = ('hT', b % 2)
            for ti in range(nt // 128):
                i2 = ctr['tpe'] % 2
                i4 = ctr['tpe'] % 4
                ctr['tpe'] += 1
                for dc in range(8):
                    S.op('pe', lambda e: e.transpose(out=pT[i2][:, dc, :], in_=xn[i4][:, dc * 128:(dc + 1) * 128],
                                                     identity=identb[:]),
                         reads=[('xn', i4), 'identb'], writes=[('pT', i2)])
                for dc in range(8):
                    S.op('dve', lambda e: e.tensor_scalar(out=hb[:, dc, ti * 128:(ti + 1) * 128], in0=pT[i2][:, dc, :],
                                                          scalar1=mul1[:, dc, j:j + 1], scalar2=add1[:, dc, j:j + 1],
                                                          op0=ALU.mult, op1=ALU.add),
                         reads=[('pT', i2), 'mul1', 'add1'], writes=[hkey])

        pend_qk = []

        def chunk(b, cc):
            t0, nt = blk_info(b)
            is_ctx = (b == 0)
            hb = hT[b % 2]
            hkey = ('hT', b % 2)
            zi = ctr['z'] % 4
            ctr['z'] += 1
            pzt = pz[zi]
            for dc in range(8):
                S.op('pe', lambda e: e.matmul(pzt[:, 0:nt], lhsT=win[:, dc, cc * 128:(cc + 1) * 128], rhs=hb[:, dc, 0:nt],
                                              start=(dc == 0), stop=(dc == 7)),
                     reads=[('win', dc), hkey], writes=[('pz', zi)], sig=(dc == 7))
            si = ctr['stg'] % 4
            ctr['stg'] += 1
            st = stg[si]
            skey = ('stg', si)
            if not (4 <= cc < 12):
                while pend_qk:
                    pend_qk.pop(0)()
            if cc < 4:
                S.op('act', lambda e: e.activation(out=st[:, 0:nt], in_=pzt[:, 0:nt], func=AF.Copy),
                     reads=[('pz', zi)], writes=[skey])
                S.dma('sp', uT_d[cc * 128:(cc + 1) * 128, t0:t0 + nt], st[:, 0:nt], reads=[skey], writes=['uT_d'])
                if is_ctx:
                    S.dma('sp', uT_d[cc * 128:(cc + 1) * 128, NEXT:NEXT + nt], st[:, 0:nt], reads=[skey], writes=['uT_d'])
            elif cc < 12:
                isq = cc < 8
                qi = ctr['z'] % 2
                S.op('act', lambda e: e.activation(out=sq[qi][:, 0:nt], in_=pzt[:, 0:nt], func=AF.Square),
                     reads=[('pz', zi)], writes=[('sq', qi)])
                while pend_qk:
                    pend_qk.pop(0)()

                def tail(qi=qi, zi=zi, pzt=pzt, st=st, skey=skey, isq=isq, cc=cc, t0=t0, nt=nt):
                    S.op('pe', lambda e: e.matmul(pms[qi][:, 0:nt], lhsT=bones[:], rhs=sq[qi][:, 0:nt], start=True, stop=True),
                         reads=['bones', ('sq', qi)], writes=[('pms', qi)])
                    S.op('act', lambda e: e.activation(out=rs[qi][:, 0:nt], in_=pms[qi][:, 0:nt], func=AF.Ln, bias=epsc[:, 0:1]),
                         reads=[('pms', qi), 'epsc'], writes=[('rs', qi)])
                    S.op('act', lambda e: e.activation(out=rs[qi][:, 0:nt], in_=rs[qi][:, 0:nt], func=AF.Exp, scale=-0.5),
                         reads=[('rs', qi)], writes=[('rs', qi)])
                    gcol = gq[:, 0:1] if isq else gq[:, 1:2]
                    S.op('dve', lambda e: e.scalar_tensor_tensor(out=st[:, 0:nt], in0=pzt[:, 0:nt], scalar=gcol,
                                                                 in1=rs[qi][:, 0:nt], op0=ALU.mult, op1=ALU.mult),
                         reads=[('pz', zi), ('rs', qi), 'gq'], writes=[skey])
                    if isq:
                        S.dma('sp', qT_d[(cc - 4) * 128:(cc - 3) * 128, t0 - NCTX:t0 - NCTX + nt], st[:, 0:nt],
                              reads=[skey], writes=['qT_d'])
                    else:
                        S.dma('sp', kT_d[(cc - 8) * 128:(cc - 7) * 128, t0:t0 + nt], st[:, 0:nt], reads=[skey], writes=['kT_d'])
                pend_qk.append(tail)
            else:
                S.op('act', lambda e: e.activation(out=st[:, 0:nt], in_=pzt[:, 0:nt], func=AF.Sigmoid),
                     reads=[('pz', zi)], writes=[skey])
                if cc < 24:
                    dst = sgaT_d[(cc - 16) * 128:(cc - 15) * 128, t0 - NCTX:t0 - NCTX + nt]
                    dk = 'sgaT_d'
                else:
                    dst = sgbT_d[(cc - 24) * 128:(cc - 23) * 128, t0 - NCTX:t0 - NCTX + nt]
                    dk = 'sgbT_d'
                S.dma('sp', dst, st[:, 0:nt], reads=[skey], writes=[dk])

        def vpart(b):
            t0, nt = blk_info(b)
            hb = hT[b % 2]
            hkey = ('hT', b % 2)
            for ti in range(nt // 128):
                zi = ctr['z'] % 4
                ctr['z'] += 1
                pzt = pz[zi]
                for dc in range(8):
                    S.op('pe', lambda e: e.matmul(pzt[:], lhsT=hb[:, dc, ti * 128:(ti + 1) * 128], rhs=win[:, dc, 1536:2048],
                                                  start=(dc == 0), stop=(dc == 7)),
                         reads=[('win', dc), hkey], writes=[('pz', zi)], sig=(dc == 7))
                while pend_qk:
                    pend_qk.pop(0)()
                vi = ctr['v'] % 2
                ctr['v'] += 1
                S.op('act', lambda e: e.activation(out=vst[vi][:, :, 0:64], in_=pzt[:].rearrange("p (h d) -> p h d", d=64), func=AF.Copy),
                     reads=[('pz', zi)], writes=[('vst', vi)])
                S.dma('sp', v_d[t0 + ti * 128:t0 + (ti + 1) * 128], vst[vi][:], reads=[('vst', vi)], writes=['v_d'])

        head_pre(0)
        head_pe(0)
        for b in range(nblk_lim):
            if b == 0:
                ccs = list(range(0, 4)) + list(range(8, 12))
            else:
                ccs = list(range(0, 12)) + list(range(16, 32))
            n1, n2 = len(ccs) // 4, (3 * len(ccs)) // 4
            for cc in ccs[:n1]:
                chunk(b, cc)
            if b + 1 < nblk_lim:
                head_pre(b + 1)
            for cc in ccs[n1:n2]:
                chunk(b, cc)
            if b + 1 < nblk_lim:
                head_pe(b + 1)
            for cc in ccs[n2:]:
                chunk(b, cc)
            vpart(b)
        S.barrier()
    if 'p1' in dbg:
        for nm, t in (('uT', uT_d), ('qT', qT_d), ('kT', kT_d), ('sgaT', sgaT_d), ('sgbT', sgbT_d)):
            dbg_out[nm] = dout("dbg_" + nm, list(t.shape), BF16)
            for r0 in range(0, t.shape[0], 128):
                S.dma('sp', dbg_out[nm][r0:r0 + 128, :], t[r0:r0 + 128, :], reads=[])
    if stop_after <= 1:
        return finish(nc, S, es, out, dbg_out)
    yaT_d = dscr("yaT_d", [512, NLAT], BF16)
    r = 'ok' if skip_s5 else s5_phase(nc, S, es, sb, ps, din, dscr, dout, dbg, dbg_out, identf, identb, uT_d, yaT_d)
    if r == 'stop' or stop_after <= 2:
        return finish(nc, S, es, out, dbg_out)
    ybT_d = dscr("ybT_d", [512, NLAT], BF16)
    na_phase(nc, S, es, sb, ps, din, dscr, dout, dbg, dbg_out, identb, qT_d, kT_d, v_d, ybT_d)
    if stop_after <= 3:
        return finish(nc, S, es, out, dbg_out)
    h2_d = dscr("h2_d", [NLAT, D], BF16)
    aff_d = dscr("aff_d", [NLAT, 16], F32)
    affT_d = dscr("affT_d", [16, NLAT], F32)
    merge_phase(nc, S, es, sb, ps, din, dscr, dout, dbg, dbg_out, identf, identb, epsc, bc_g1, bc_mul2, bc_add2,
                xcat, out, yaT_d, ybT_d, sgaT_d, sgbT_d, h2_d, aff_d, affT_d, nblocks=mg_blocks)
    if stop_after <= 4:
        return finish(nc, S, es, out, dbg_out)
    moe_phase(nc, S, es, sb, ps, din, dscr, dout, dbg, dbg_out, identf, identb, bc_g2, out, h2_d, aff_d, affT_d, nexp=nexp)
    return finish(nc, S, es, out, dbg_out)


def finish(nc, S, es, out, dbg_out):
    S.barrier(['sp'])
    print("instructions", S.nins, "waits", S.nwaits)
    es.close()
    return nc, dbg_out


def host_inputs(inp, b):
    f = np.float32
    xcat = np.concatenate([inp['ctx'][b], inp['x'][b]], axis=0).astype(f)
    cT = np.stack([inp['c'][b].reshape(8, 128).T, inp['c_ctx'].reshape(8, 128).T], axis=-1).astype(f)
    qkg = np.stack([np.tile(inp['q_norm'][0], 2), np.tile(inp['k_norm'][0], 2)], axis=-1).astype(f)
    m = {
        'xcat': np.ascontiguousarray(xcat),
        'cT': np.ascontiguousarray(cT),
        'w_ada': np.ascontiguousarray(inp['w_ada'][0]),
        'b_ada': np.ascontiguousarray(inp['b_ada'][0][None, :]),
        'nmixT': np.ascontiguousarray(inp['norm_mix'][0].reshape(8, 128).T),
        'nffn': np.ascontiguousarray(inp['norm_ffn'][0][None, :]),
        'w_in': np.ascontiguousarray(inp['w_in'][0]),
        'ident': np.eye(128, dtype=f),
        'qkg': np.ascontiguousarray(qkg),
    }
    f32c = lambda a: np.ascontiguousarray(a.astype(f))
    lr = inp['ssm_lam_re'][0].reshape(2, 16, 2, 64)
    m['lamre_l'] = f32c(lr.transpose(2, 3, 0, 1).reshape(128, 32))
    li = inp['ssm_lam_im'][0].reshape(2, 16, 2, 64)
    m['lamim_l'] = f32c(li.transpose(2, 3, 0, 1).reshape(128, 32))
    ld = inp['ssm_log_dt'][0].reshape(2, 16, 2)
    m['logdt_l'] = f32c(np.broadcast_to(ld.transpose(2, 0, 1)[:, None, :, :], (2, 64, 2, 16)).reshape(128, 32))
    for nm, key in (('Bre_c', 'ssm_b_re'), ('Bim_c', 'ssm_b_im')):
        bb = inp[key][0].reshape(2, 16, 2, 64, 16)
        m[nm] = f32c(bb.transpose(2, 3, 0, 1, 4).reshape(128, 32, 16))
    for nm, key in (('Cre_c', 'ssm_c_re'), ('Cim_c', 'ssm_c_im')):
        cc = inp[key][0].reshape(2, 16, 2, 16, 64)
        m[nm] = f32c(cc.transpose(2, 4, 0, 1, 3).reshape(128, 32, 16))
    m['dskipT'] = f32c(inp['ssm_d'][0].reshape(4, 128).T)
    m['w_glu'] = f32c(inp['w_glu'][0])
    m['bgluT'] = f32c(inp['b_glu'][0].reshape(4, 128).T)
    m.update(na_host(inp))
    m['w_proj_a'] = f32c(inp['w_proj_a'][0]); m['w_proj_b'] = f32c(inp['w_proj_b'][0]); m['w_out'] = f32c(inp['w_out'][0])
    m.update(moe_host(inp))
    m['w_routerT'] = f32c(inp['w_router'][0].reshape(8, 128, 16).transpose(1, 0, 2))
    return m


_NC_CACHE = {}


def kernel(**inputs):
    inp = {k_: np.asarray(v_) for k_, v_ in inputs.items()}
    if 'nc' not in _NC_CACHE:
        nc, _ = build()
        _NC_CACHE['nc'] = nc
    nc = _NC_CACHE['nc']
    nb = inp['x'].shape[0]
    in_maps = [host_inputs(inp, b) for b in range(nb)]
    res = run_bass_kernel_spmd(nc, in_maps, core_ids=list(range(nb)))
    outs = [np.asarray(res.results[b]["out"], dtype=np.float32) for b in range(nb)]
    return np.stack(outs, axis=0)
```

```python
import numpy as np
from contextlib import ExitStack
import concourse.bass as bass
import concourse.mybir as mybir
from concourse.bass_utils import run_bass_kernel_spmd

F32 = mybir.dt.float32
BF16 = mybir.dt.bfloat16
I32 = mybir.dt.int32
U32 = mybir.dt.uint32
ALU = mybir.AluOpType
AF = mybir.ActivationFunctionType
AX = mybir.AxisListType
STRICT = False


class Sched:
    def __init__(self, nc, es, n_dma_sems=24):
        self.nc = nc
        self.e = {'pe': nc.tensor, 'act': nc.scalar, 'dve': nc.vector, 'pool': nc.gpsimd, 'sp': nc.sync}
        self.sems = {}
        self.cnt = {}
        for k in self.e:
            self.sems[k] = es.enter_context(nc.semaphore("sem_" + k))
            self.cnt[k] = 0
        self.nd = n_dma_sems
        self.qsems = {}
        for q in ('sp', 'pool', 'act'):
            n = n_dma_sems if q != 'act' else 16
            self.qsems[q] = []
            for i in range(n):
                sk = 'd_%s%d' % (q, i)
                self.sems[sk] = es.enter_context(nc.semaphore("dsem_%s%d" % (q, i)))
                self.cnt[sk] = 0
                self.qsems[q].append(sk)
        self.qrr = {'sp': 0, 'pool': 0, 'act': 0}
        self.drr = 0
        self.waited = {}
        self.lastw = {}
        self.readers = {}
        self.nwaits = 0
        self.nins = 0

    def _wait(self, eng, tok):
        sk, val, src = tok
        if self.waited.get((eng, sk), 0) >= val:
            return
        self.e[eng].wait_ge(self.sems[sk], val)
        self.waited[(eng, sk)] = val
        self.nwaits += 1

    def _deps(self, eng, reads, writes):
        for k in reads:
            t = self.lastw.get(k)
            if t is not None:
                self._wait(eng, t)
        for k in writes:
            t = self.lastw.get(k)
            if t is not None and (t[2] != eng or (STRICT and eng != 'pe')):
                self._wait(eng, t)
            for sk, (val, src) in self.readers.get(k, {}).items():
                if src != eng or (STRICT and eng != 'pe'):
                    self._wait(eng, (sk, val, src))

    def _record(self, tok, reads, writes):
        sk, val, src = tok
        for k in reads:
            self.readers.setdefault(k, {})[sk] = (val, src)
        for k in writes:
            self.lastw[k] = tok
            self.readers[k] = {}

    def op(self, eng, fn, reads=(), writes=(), sig=True):
        self._deps(eng, reads, writes)
        ins = fn(self.e[eng])
        if sig:
            self.cnt[eng] += 1
            ins.then_inc(self.sems[eng], 1)
            tok = (eng, self.cnt[eng], eng)
        else:
            tok = (eng, self.cnt[eng] + 1, eng)
        self._record(tok, reads, writes)
        self.nins += 1
        return tok

    def _dma_common(self, q, fn, reads, writes):
        self._deps(q, reads, writes)
        i = self.qrr[q]
        self.qrr[q] = (i + 1) % len(self.qsems[q])
        sk = self.qsems[q][i]
        if self.cnt[sk] > 0:
            self._wait(q, (sk, self.cnt[sk], None))
        ins = fn(self.e[q])
        self.cnt[sk] += 16
        ins.then_inc(self.sems[sk], 16)
        tok = (sk, self.cnt[sk], None)
        self._record(tok, reads, writes)
        self.nins += 1
        return tok

    def dma(self, q, out, in_, reads=(), writes=(), **kw):
        return self._dma_common(q, lambda e: e.dma_start(out=out, in_=in_, **kw), reads, writes)

    def custom_dma(self, q, fn, reads=(), writes=()):
        return self._dma_common(q, fn, reads, writes)

    def barrier(self, engines=None):
        for e in (engines or self.e):
            for sk, c in self.cnt.items():
                if c > 0:
                    self._wait(e, (sk, c, None))
        self.lastw = {}
        self.readers = {}

PI = float(np.pi)


def kn(t):
    n = t.name
    return n[2:] if n.startswith('s_') else n


def s5_phase(nc, S, es, sb, ps, din, dscr, dout, dbg, dbg_out, identf, identb, uT_d, yaT_d, nbanks=16):
    NEXT, NE2 = 8448, 8704
    lre_in = din("lamre_l", [128, 32]); lim_in = din("lamim_l", [128, 32]); ldt_in = din("logdt_l", [128, 32])
    Bre_in = din("Bre_c", [128, 32, 16]); Bim_in = din("Bim_c", [128, 32, 16])
    Cre_in = din("Cre_c", [128, 32, 16]); Cim_in = din("Cim_c", [128, 32, 16])
    dsk_in = din("dskipT", [128, 4]); wglu_in = din("w_glu", [512, 512]); bglu_in = din("bgluT", [128, 4])
    E_d = dscr("E_d", [128, 2, 4, 16, 2, 2, 128], BF16)
    G_d = dscr("G_d", [128, 64, 640], BF16)
    H_d = dscr("H_d", [2, 128, 16, 2, 544], BF16)

    with ExitStack() as s5:
        Kt_sb = sb("s_Kt_sb", [128, 128, 128], BF16, s5)
        aL = sb("s_aL", [128, 32], F32, s5); bL = sb("s_bL", [128, 32], F32, s5)
        diagD = sb("s_diagD", [128, 4, 128], BF16, s5)
        with ExitStack() as pa:
            def t32(name):
                return sb(name, [128, 32], F32, pa)
            lre, lim, ldt, dt, a, b = (t32(n) for n in ("lre", "lim", "ldt", "dt", "a_", "b_"))
            u1, u2, u3, u4, c16, s16, wre, wim = (t32(n) for n in ("u1", "u2", "u3", "u4", "c16", "s16", "wre", "wim"))
            Bcr = sb("s_Bcr", [128, 32, 16], F32, pa); Bci = sb("s_Bci", [128, 32, 16], F32, pa)
            Dr = sb("s_Dr", [128, 32, 32], F32, pa); Di = sb("s_Di", [128, 32, 32], F32, pa)
            Dr2 = sb("s_Dr2", [128, 32, 32], F32, pa); Di2 = sb("s_Di2", [128, 32, 32], F32, pa)
            Gr = sb("s_Gr", [128, 32, 32], F32, pa); Gi = sb("s_Gi", [128, 32, 32], F32, pa)
            T1 = sb("s_T1", [128, 32, 32], F32, pa); T2 = sb("s_T2", [128, 32, 32], F32, pa)
            T3 = sb("s_T3", [128, 32, 32], F32, pa); T4 = sb("s_T4", [128, 32, 32], F32, pa)
            P1 = sb("s_P1", [128, 32, 32], F32, pa); P2 = sb("s_P2", [128, 32, 32], F32, pa)
            P3 = sb("s_P3", [128, 32, 32], F32, pa); P4 = sb("s_P4", [128, 32, 32], F32, pa)
            Cbr = sb("s_Cbr", [128, 32, 32], BF16, pa); Cbni = sb("s_Cbni", [128, 32, 32], BF16, pa)
            Dbr = sb("s_Dbr", [128, 8, 160], BF16, pa); Dbi = sb("s_Dbi", [128, 8, 160], BF16, pa)
            Gbr = sb("s_Gbr", [128, 8, 160], BF16, pa); Gbni = sb("s_Gbni", [128, 8, 160], BF16, pa)
            Es = [sb("s_Es%d" % i, [128, 2, 4, 2, 2, 128], BF16, pa) for i in range(2)]
            m3 = sb("s_m3", [128, 1], F32, pa)
            dsk = sb("s_dsk", [128, 4], F32, pa)
            pk = [ps("s_pk%d" % i, [128, 128], F32, pa) for i in range(2)]
            pE = [ps("s_pE%d" % i, [128, 128], F32, pa) for i in range(4)]

            S.dma('sp', lre[:], lre_in, writes=['lre']); S.dma('sp', lim[:], lim_in, writes=['lim'])
            S.dma('sp', ldt[:], ldt_in, writes=['ldt']); S.dma('sp', dsk[:], dsk_in, writes=['dsk'])
            for X in (Dr, Di, Gr, Gi, Dr2, Di2):
                S.op('dve', lambda e: e.memset(X[:], 0.0), writes=[kn(X)])
            for X in (Dbr, Dbi, Gbr, Gbni):
                S.op('pool', lambda e: e.memset(X[:], 0.0), writes=[kn(X)])
            S.op('pool', lambda e: e.memset(Kt_sb[:], 0.0), writes=['Kt_sb'])
            for i in range(2):
                S.op('pool', lambda e: e.memset(Es[i][:], 0.0), writes=[('Es', i)])
            S.op('dve', lambda e: e.memset(m3[:], 1.0), writes=['m3'])
            S.op('dve', lambda e: e.memset(m3[64:96, :], 0.0), writes=['m3'])
            S.dma('sp', Bcr[:], Bre_in, writes=['Bcr']); S.dma('sp', Bci[:], Bim_in, writes=['Bci'])
            for h in range(2):
                S.dma('sp', Gr[64 * h:64 * h + 64, :, 16 * h:16 * h + 16], Cre_in[64 * h:64 * h + 64], reads=[], writes=['Gr'])
                S.dma('sp', Gi[64 * h:64 * h + 64, :, 16 * h:16 * h + 16], Cim_in[64 * h:64 * h + 64], reads=[], writes=['Gi'])
            for q in range(4):
                S.op('dve', lambda e: e.tensor_scalar(out=diagD[:, q, :], in0=identf[:], scalar1=dsk[:, q:q + 1], scalar2=None,
                                                      op0=ALU.mult), reads=['identf', 'dsk'], writes=['diagD'])
            V = lambda e: e
            def tt(out, in0, in1, op, r, w, eng='dve'):
                S.op(eng, lambda e: e.tensor_tensor(out=out, in0=in0, in1=in1, op=op), reads=r, writes=w)
            S.op('act', lambda e: e.activation(out=dt[:], in_=ldt[:], func=AF.Exp), reads=['ldt'], writes=['dt'])
            tt(u1[:], lre[:], dt[:], ALU.mult, ['lre', 'dt'], ['u1'])
            S.op('act', lambda e: e.activation(out=u1[:], in_=u1[:], func=AF.Exp), reads=['u1'], writes=['u1'])
            tt(u2[:], lim[:], dt[:], ALU.mult, ['lim', 'dt'], ['u2'])
            S.op('act', lambda e: e.activation(out=s16[:], in_=u2[:], func=AF.Sin, scale=1.0 / 16), reads=['u2'], writes=['s16'])
            S.op('act', lambda e: e.activation(out=u3[:], in_=u2[:], func=AF.Sin, scale=1.0 / 32), reads=['u2'], writes=['u3'])
            tt(u3[:], u3[:], u3[:], ALU.mult, ['u3'], ['u3'])
            S.op('dve', lambda e: e.tensor_scalar(out=c16[:], in0=u3[:], scalar1=-2.0, scalar2=1.0, op0=ALU.mult, op1=ALU.add),
                 reads=['u3'], writes=['c16'])

            def csquare(cr, ci, n):
                for _ in range(n):
                    tt(u3[:], cr[:], cr[:], ALU.mult, [kn(cr)], ['u3'])
                    tt(u4[:], ci[:], ci[:], ALU.mult, [kn(ci)], ['u4'])
                    tt(ci[:], cr[:], ci[:], ALU.mult, [kn(cr), kn(ci)], [kn(ci)])
                    S.op('dve', lambda e: e.tensor_scalar(out=ci[:], in0=ci[:], scalar1=2.0, scalar2=None, op0=ALU.mult),
                         reads=[kn(ci)], writes=[kn(ci)])
                    tt(cr[:], u3[:], u4[:], ALU.subtract, ['u3', 'u4'], [kn(cr)])
            csquare(c16, s16, 4)
            tt(a[:], u1[:], c16[:], ALU.mult, ['u1', 'c16'], ['a_'])
            tt(b[:], u1[:], s16[:], ALU.mult, ['u1', 's16'], ['b_'])
            S.op('dve', lambda e: e.tensor_copy(out=aL[:], in_=c16[:]), reads=['c16'], writes=['aL'])
            S.op('dve', lambda e: e.tensor_copy(out=bL[:], in_=s16[:]), reads=['s16'], writes=['bL'])
            csquare(aL, bL, 4)
            tt(u3[:], lre[:], dt[:], ALU.mult, ['lre', 'dt'], ['u3'])
            S.op('act', lambda e: e.activation(out=u3[:], in_=u3[:], func=AF.Exp, scale=16.0), reads=['u3'], writes=['u3'])
            tt(aL[:], aL[:], u3[:], ALU.mult, ['aL', 'u3'], ['aL'])
            tt(bL[:], bL[:], u3[:], ALU.mult, ['bL', 'u3'], ['bL'])
            S.op('dve', lambda e: e.tensor_scalar(out=u1[:], in0=a[:], scalar1=-1.0, scalar2=None, op0=ALU.add), reads=['a_'], writes=['u1'])
            tt(u2[:], lre[:], lre[:], ALU.mult, ['lre'], ['u2'])
            tt(u3[:], lim[:], lim[:], ALU.mult, ['lim'], ['u3'])
            tt(u2[:], u2[:], u3[:], ALU.add, ['u2', 'u3'], ['u2'])
            S.op('dve', lambda e: e.reciprocal(out=u2[:], in_=u2[:]), reads=['u2'], writes=['u2'])
            tt(u3[:], u1[:], lre[:], ALU.mult, ['u1', 'lre'], ['u3'])
            tt(u4[:], b[:], lim[:], ALU.mult, ['b_', 'lim'], ['u4'])
            tt(u3[:], u3[:], u4[:], ALU.add, ['u3', 'u4'], ['u3'])
            tt(wre[:], u3[:], u2[:], ALU.mult, ['u3', 'u2'], ['wre'])
            tt(u3[:], b[:], lre[:], ALU.mult, ['b_', 'lre'], ['u3'])
            tt(u4[:], u1[:], lim[:], ALU.mult, ['u1', 'lim'], ['u4'])
            tt(u3[:], u3[:], u4[:], ALU.subtract, ['u3', 'u4'], ['u3'])
            tt(wim[:], u3[:], u2[:], ALU.mult, ['u3', 'u2'], ['wim'])
            bc16 = lambda t: t[:].unsqueeze(2).to_broadcast([128, 32, 16])
            Tc = [T1[:, :, 0:16], T2[:, :, 0:16], T3[:, :, 0:16], T4[:, :, 0:16]]
            tt(Tc[0], Bcr[:], bc16(wre), ALU.mult, ['Bcr', 'wre'], ['T1'])
            tt(Tc[1], Bci[:], bc16(wim), ALU.mult, ['Bci', 'wim'], ['T2'])
            tt(Tc[2], Bci[:], bc16(wre), ALU.mult, ['Bci', 'wre'], ['T3'])
            tt(Tc[3], Bcr[:], bc16(wim), ALU.mult, ['Bcr', 'wim'], ['T4'])
            for h in range(2):
                sl = slice(64 * h, 64 * h + 64)
                tt(Dr[sl, :, 16 * h:16 * h + 16], T1[sl, :, 0:16], T2[sl, :, 0:16], ALU.subtract, ['T1', 'T2'], ['Dr'])
                tt(Di[sl, :, 16 * h:16 * h + 16], T3[sl, :, 0:16], T4[sl, :, 0:16], ALU.add, ['T3', 'T4'], ['Di'])
            S.op('dve', lambda e: e.tensor_copy(out=Cbr[:], in_=Gr[:]), reads=['Gr'], writes=['Cbr'])
            S.op('dve', lambda e: e.tensor_scalar(out=Cbni[:], in0=Gi[:], scalar1=-1.0, scalar2=None, op0=ALU.mult),
                 reads=['Gi'], writes=['Cbni'])
            bc32 = lambda t: t[:].unsqueeze(2).to_broadcast([128, 32, 32])

            def cmul(Xr, Xi, eng, Yr=None, Yi=None):
                Yr = Yr or Xr; Yi = Yi or Xi
                A1, A2, A3, A4 = (T1, T2, T3, T4) if eng == 'dve' else (P1, P2, P3, P4)
                tt(A1[:], Xr[:], bc32(a), ALU.mult, [kn(Xr), 'a_'], [kn(A1)], eng)
                tt(A2[:], Xi[:], bc32(b), ALU.mult, [kn(Xi), 'b_'], [kn(A2)], eng)
                tt(A3[:], Xi[:], bc32(a), ALU.mult, [kn(Xi), 'a_'], [kn(A3)], eng)
                tt(A4[:], Xr[:], bc32(b), ALU.mult, [kn(Xr), 'b_'], [kn(A4)], eng)
                tt(Yr[:], A1[:], A2[:], ALU.subtract, [kn(A1), kn(A2)], [kn(Yr)], eng)
                tt(Yi[:], A3[:], A4[:], ALU.add, [kn(A3), kn(A4)], [kn(Yi)], eng)

            def to5(dst, src, scale, key, eng='dve'):
                d4 = dst[:].rearrange("p g (s c) -> p g s c", c=32)
                s4 = src[:].rearrange("p (g s) c -> p g s c", s=4)
                if eng == 'act':
                    S.op('act', lambda e: e.activation(out=d4[:, :, 0:3, :], in_=s4[:, :, 0:3, :], func=AF.Copy, scale=scale),
                         reads=[kn(src)], writes=[key])
                    S.op('act', lambda e: e.activation(out=d4[:, :, 4, :], in_=s4[:, :, 3, :], func=AF.Copy, scale=scale),
                         reads=[kn(src)], writes=[key])
                    return
                S.op(eng, lambda e: e.tensor_scalar(out=d4[:, :, 0:3, :], in0=s4[:, :, 0:3, :], scalar1=scale, scalar2=None, op0=ALU.mult),
                     reads=[kn(src)], writes=[key])
                S.op(eng, lambda e: e.tensor_scalar(out=d4[:, :, 4, :], in0=s4[:, :, 3, :], scalar1=scale, scalar2=None, op0=ALU.mult),
                     reads=[kn(src)], writes=[key])

            Dpairs = [(Dr, Di), (Dr2, Di2)]
            for j in range(16):
                Dr, Di = Dpairs[j % 2]
                to5(Dbr, Dr, 1.0, 'Dbr', 'act'); to5(Dbi, Di, 1.0, 'Dbi', 'act')
                if j < 15:
                    cmul(Dr, Di, 'dve', Dpairs[(j + 1) % 2][0], Dpairs[(j + 1) % 2][1])
                for d in range(2):
                    for q in range(4):
                        idx = (d * 4 + q) * 16 + j
                        pkt = pk[idx % 2]; pkk = ('pk', idx % 2)
                        for pl in range(4):
                            pair = d * 16 + q * 4 + pl
                            if pl < 3:
                                osl = pkt[32 * pl:32 * pl + 32, 32 * pl:32 * pl + 32]; csl = slice(32 * pl, 32 * pl + 32)
                            else:
                                osl = pkt[64:128, 96:128]; csl = slice(96, 160)
                            S.op('pe', lambda e: e.matmul(osl, lhsT=Dbr[:, d * 4 + q, csl], rhs=Cbr[:, pair, :], start=True, stop=False),
                                 reads=['Dbr', 'Cbr'], writes=[pkk])
                            S.op('pe', lambda e: e.matmul(osl, lhsT=Dbi[:, d * 4 + q, csl], rhs=Cbni[:, pair, :], start=False, stop=True),
                                 reads=['Dbi', 'Cbni'], writes=[pkk])
                        for pl in range(4):
                            if pl < 3:
                                rs_, cs_ = slice(32 * pl, 32 * pl + 32), slice(32 * pl, 32 * pl + 32)
                            else:
                                rs_, cs_ = slice(64, 128), slice(96, 128)
                            S.op('act', lambda e: e.activation(out=Kt_sb[rs_, idx, cs_], in_=pkt[rs_, cs_], func=AF.Copy),
                                 reads=[pkk], writes=['Kt_sb'])
                Et = Es[j % 2]; ek = ('Es', j % 2)
                n_e = 0
                for reim, X in ((0, Dr), (1, Di)):
                    for d in range(2):
                        for q in range(4):
                            pet = pE[n_e % 4]; pek = ('pE', n_e % 4); n_e += 1
                            src = X[:, d * 16 + q * 4:d * 16 + q * 4 + 4, :].rearrange("p a b -> p (a b)")
                            S.op('pe', lambda e: e.transpose(out=pet[:], in_=src, identity=identf[:]), reads=[kn(X), 'identf'], writes=[pek])
                            S.op('act', lambda e: e.activation(out=Et[:, d, q, reim, 0, :], in_=pet[:], func=AF.Copy), reads=[pek], writes=[ek])
                            S.op('dve', lambda e: e.tensor_scalar(out=Et[64:128, d, q, reim, 1, :], in0=pet[64:128, :], scalar1=m3[64:128, 0:1],
                                                                  scalar2=None, op0=ALU.mult), reads=[pek, 'm3'], writes=[ek])
                for d in range(2):
                    S.dma('sp', E_d[:, d, :, j, :, :, :], Et[:, d], reads=[ek], writes=['E_d'])
                cmul(Gr, Gi, 'pool')
                to5(Gbr, Gr, 1.0, 'Gbr'); to5(Gbni, Gi, -1.0, 'Gbni')
                for d in range(2):
                    S.dma('sp', G_d[:, (d * 16 + j) * 2 + 0, :], Gbr[:, d * 4:d * 4 + 4, :].rearrange("p a b -> p (a b)"), reads=['Gbr'], writes=['G_d'])
                    S.dma('sp', G_d[:, (d * 16 + j) * 2 + 1, :], Gbni[:, d * 4:d * 4 + 4, :].rearrange("p a b -> p (a b)"), reads=['Gbni'], writes=['G_d'])
            S.barrier()
        if 's5prep' in dbg:
            dbg_out['Kt'] = dout("dbg_Kt", [128, 128, 128], BF16)
            S.dma('sp', dbg_out['Kt'], Kt_sb[:], reads=['Kt_sb'])
            dbg_out['aL'] = dout("dbg_aL", [128, 2, 32])
            S.dma('sp', dbg_out['aL'][:, 0, :], aL[:], reads=['aL']); S.dma('sp', dbg_out['aL'][:, 1, :], bL[:], reads=['bL'])
            dbg_out['G'] = dout("dbg_G", [128, 64, 640], BF16)
            S.dma('sp', dbg_out['G'], G_d, reads=['G_d'])
            dbg_out['E'] = dout("dbg_E", [128, 2, 4, 16, 2, 2, 128], BF16)
            for d in range(2):
                S.dma('sp', dbg_out['E'][:, d], E_d[:, d], reads=['E_d'])
            return 'stop'
        return s5_main(nc, S, es, sb, ps, din, dscr, dout, dbg, dbg_out, identf, identb, uT_d, yaT_d, nbanks,
                       s5, Kt_sb, aL, bL, diagD, E_d, G_d, H_d, wglu_in, bglu_in)


def s5_main(nc, S, es, sb, ps, din, dscr, dout, dbg, dbg_out, identf, identb, uT_d, yaT_d, nbanks,
            s5, Kt_sb, aL, bL, diagD, E_d, G_d, H_d, wglu_in, bglu_in):
    NCH = 528
    with ExitStack() as pb:
        uTb = [sb("s_uTb%d" % i, [128, 2048], BF16, pb) for i in range(1)]
        uTc = [sb("s_uTc%d" % i, [128, 16, 128], BF16, pb) for i in range(2)]
        Esl = [sb("s_Esl%d" % i, [128, 16, 2, 2, 128], BF16, pb) for i in range(2)]
        Ssb = [[sb("s_Ssb%d_%d" % (d, i), [128, 16, 2, 128], F32, pb) for i in range(2)] for d in range(2)]
        hist = [sb("s_hist%d" % d, [128, 16, 2, 129], F32, pb) for d in range(2)]
        histb = [sb("s_histb%d" % d, [128, 16, 2, 128], BF16, pb) for d in range(2)]
        tP = [sb("s_tP%d" % d, [128, 16, 2], F32, pb) for d in range(2)]
        tQ = [sb("s_tQ%d" % d, [128, 16, 2], F32, pb) for d in range(2)]
        pS = [ps("s_pS%d" % i, [128, 4, 128], F32, pb) for i in range(4)]
        for d in range(2):
            S.op('dve' if d == 0 else 'pool', lambda e: e.memset(hist[d][:], 0.0), writes=[('hist', d)])
        fblocks = [(0, 128), (128, 128), (256, 128), (384, 128), (512, 16)]
        bblocks = [(512, 16), (384, 128), (256, 128), (128, 128), (0, 128)]
        esl_ctr = 0
        ps_ctr = 0
        for step in range(5):
            for d in range(2):
                k0, nk = (fblocks if d == 0 else bblocks)[step]
                Sb = Ssb[d][step % 2]; skey = ('Ssb', d, step % 2)
                base = 0 if d == 0 else 256
                for q in range(4):
                    et = Esl[esl_ctr % 2]; ekey = ('Esl', esl_ctr % 2); esl_ctr += 1
                    S.dma('sp', et[:], E_d[:, d, q], reads=['E_d'], writes=[ekey])
                    ut0 = uTb[0]; u0key = ('uTb', 0)
                    S.dma('sp', ut0[:, 0:16 * nk], uT_d[q * 128:(q + 1) * 128, base + 16 * k0: base + 16 * k0 + 16 * nk], reads=['uT_d'], writes=[u0key])
                    ut = uTc[(esl_ctr - 1) % 2]; ukey = ('uTc', (esl_ctr - 1) % 2)
                    S.op('act', lambda e: e.activation(out=ut[:, :, 0:nk], in_=ut0[:, 0:16 * nk].rearrange("p (k i) -> p i k", i=16), func=AF.Copy),
                         reads=[u0key], writes=[ukey])
                    for pl in range(4):
                        pst = pS[ps_ctr % 4]; pskey = ('pS', ps_ctr % 4); ps_ctr += 1
                        if pl < 3:
                            rsl, alt = slice(32 * pl, 32 * pl + 32), 0
                        else:
                            rsl, alt = slice(64, 128), 1
                        for reim in range(2):
                            for i in range(16):
                                j = 15 - i if d == 0 else i
                                rhs = ut[rsl, i, 0:nk]
                                S.op('pe', lambda e: e.matmul(pst[:, reim, 0:nk], lhsT=et[rsl, j, reim, alt, :], rhs=rhs,
                                                              start=(i == 0), stop=(i == 15)),
                                     reads=[ekey, ukey], writes=[pskey])
                        S.op('act', lambda e: e.activation(out=Sb[:, q * 4 + pl, :, 0:nk], in_=pst[:, 0:2, 0:nk], func=AF.Copy),
                             reads=[pskey], writes=[skey])
            def chain_ops(d, kk):
                k0, nk = (fblocks if d == 0 else bblocks)[step]
                Sb = Ssb[d][step % 2]; skey = ('Ssb', d, step % 2)
                hk = ('hist', d)
                A2 = aL[:, d * 16:(d + 1) * 16].unsqueeze(2).to_broadcast([128, 16, 2])
                B2 = bL[:, d * 16:(d + 1) * 16].unsqueeze(2).to_broadcast([128, 16, 2])
                H = hist[d]
                if d == 0:
                    cprev, ccur, scol = kk, kk + 1, kk
                else:
                    cprev, ccur, scol = nk - kk, nk - 1 - kk, nk - 1 - kk
                prev = H[:, :, :, cprev]
                P_, Q_ = tP[d], tQ[d]
                eng = 'dve'
                return [
                    lambda: S.op(eng, lambda e: e.tensor_tensor(out=P_[:], in0=prev, in1=A2, op=ALU.mult), reads=[hk, 'aL'], writes=[('tP', d)]),
                    lambda: S.op(eng, lambda e: e.tensor_tensor(out=Q_[:], in0=prev, in1=B2, op=ALU.mult), reads=[hk, 'bL'], writes=[('tQ', d)]),
                    lambda: S.op(eng, lambda e: e.tensor_tensor(out=P_[:], in0=P_[:], in1=Sb[:, :, :, scol], op=ALU.add),
                                 reads=[('tP', d), skey], writes=[('tP', d)]),
                    lambda: S.op(eng, lambda e: e.tensor_tensor(out=H[:, :, 0, ccur], in0=P_[:, :, 0], in1=Q_[:, :, 1], op=ALU.subtract),
                                 reads=[('tP', d), ('tQ', d)], writes=[hk]),
                    lambda: S.op(eng, lambda e: e.tensor_tensor(out=H[:, :, 1, ccur], in0=P_[:, :, 1], in1=Q_[:, :, 0], op=ALU.add),
                                 reads=[('tP', d), ('tQ', d)], writes=[hk]),
                ]
            nks = [(fblocks if d == 0 else bblocks)[step][1] for d in range(2)]
            for kk in range(max(nks)):
                lists = [chain_ops(d, kk) for d in range(2) if kk < nks[d]]
                for oi in range(5):
                    for l in lists:
                        l[oi]()
            for d in range(2):
                eng = 'dve' if d == 0 else 'pool'
                k0, nk = (fblocks if d == 0 else bblocks)[step]
                hk = ('hist', d)
                H = hist[d]
                if d == 0:
                    S.op(eng, lambda e: e.tensor_copy(out=histb[d][:, :, :, 0:nk], in_=H[:, :, :, 1:nk + 1]), reads=[hk], writes=[('histb', d)])
                    S.op(eng, lambda e: e.tensor_copy(out=H[:, :, :, 0], in_=H[:, :, :, nk]), reads=[hk], writes=[hk])
                else:
                    S.op(eng, lambda e: e.tensor_copy(out=histb[d][:, :, :, 0:nk], in_=H[:, :, :, 0:nk]), reads=[hk], writes=[('histb', d)])
                    S.op(eng, lambda e: e.tensor_copy(out=H[:, :, :, 128], in_=H[:, :, :, 0]), reads=[hk], writes=[hk])
                S.dma('pool', H_d[d, :, :, :, k0:k0 + nk], histb[d][:, :, :, 0:nk], reads=[('histb', d)], writes=['H_d'])
        S.barrier()
    if 's5lvl3' in dbg:
        dbg_out['H'] = dout("dbg_H", [2, 128, 16, 2, 544], BF16)
        for d in range(2):
            S.dma('sp', dbg_out['H'][d], H_d[d], reads=['H_d'])
        return 'stop'

    with ExitStack() as pc:
        Gsb = sb("s_Gsb", [128, 64, 640], BF16, pc)
        wglu = sb("s_wglu", [128, 4, 512], BF16, pc)
        bglu = sb("s_bglu", [128, 4], F32, pc)
        ub = [sb("s_ub%d" % i, [128, 4, 544], BF16, pc) for i in range(2)]
        Hf = [sb("s_Hf%d" % i, [128, 16, 2, 32], BF16, pc) for i in range(2)]
        Hb = [sb("s_Hb%d" % i, [128, 16, 2, 32], BF16, pc) for i in range(2)]
        aT = [sb("s_aT%d" % i, [128, 4, 512], BF16, pc) for i in range(2)]
        sig = [sb("s_sig%d" % i, [128, 512], BF16, pc) for i in range(2)]
        yo = [sb("s_yo%d" % i, [128, 512], BF16, pc) for i in range(4)]
        py = [ps("s_py%d" % i, [128, 512], F32, pc) for i in range(2)]
        py4 = [ps("s_py4_%d" % i, [128, 16, 32], F32, pc) for i in range(2)]
        y4 = [sb("s_y4_%d" % i, [128, 512], F32, pc) for i in range(2)]
        ysum = [sb("s_ysum%d" % i, [128, 512], F32, pc) for i in range(2)]
        zlhs = sb("s_zlhs", [128, 128], BF16, pc)
        S.op('dve', lambda e: e.memset(zlhs[:], 0.0), writes=['zlhs'])
        pg = [ps("s_pg%d" % i, [128, 512], F32, pc) for i in range(2)]
        for c in range(4):
            S.dma('sp', Gsb[:, c * 16:(c + 1) * 16, :], G_d[:, c * 16:(c + 1) * 16, :], reads=['G_d'], writes=[('Gsb', c)])
        gkeys = [('Gsb', c) for c in range(4)]
        for q in range(4):
            S.dma('pool', wglu[:, q, :], wglu_in[q * 128:(q + 1) * 128, :], writes=['wglu'])
        S.dma('sp', bglu[:], bglu_in, writes=['bglu'])
        yo_ctr = 0
        for n in range(nbanks):
            i2 = n % 2
            e0 = 256 + 512 * n
            S.dma('sp', ub[i2][:], uT_d[:, e0 - 16:e0 + 528].rearrange("(q p) t -> p q t", p=128), reads=['uT_d'], writes=[('ub', i2)])
            kf = 16 + 32 * n
            kb = 32 * n
            S.dma('sp', Hf[i2][:], H_d[0, :, :, :, kf - 1:kf + 31], reads=['H_d'], writes=[('Hf', i2)])
            S.dma('sp', Hb[i2][:], H_d[1, :, :, :, kb + 1:kb + 33], reads=['H_d'], writes=[('Hb', i2)])
            for q in range(4):
                pyt = py[q % 2]; pk_ = ('py', q % 2)
                p4 = py4[q % 2]; p4k = ('py4', q % 2)
                pyv = pyt[:].rearrange("p (k j) -> p k j", j=16)
                uc = ub[i2][:, q, 16:528]
                ucv = uc.rearrange("p (k j) -> p k j", j=16)
                rd = [('ub', i2), 'Kt_sb', 'diagD']
                S.op('pe', lambda e: e.matmul(pyt[:], lhsT=diagD[:, q, :], rhs=uc, start=True, stop=False), reads=rd, writes=[pk_])
                S.op('pe', lambda e: e.matmul(pyt[:], lhsT=Kt_sb[:, (0 * 4 + q) * 16 + 0, :], rhs=uc, start=False, stop=False), reads=rd, writes=[pk_])
                for tau in range(1, 16):
                    S.op('pe', lambda e: e.matmul(pyv[:, :, tau:16], lhsT=Kt_sb[:, (0 * 4 + q) * 16 + tau, :], rhs=ucv[:, :, 0:16 - tau],
                                                  start=False, stop=False), reads=rd, writes=[pk_])
                S.op('pe', lambda e: e.matmul(pyt[:], lhsT=Kt_sb[:, (1 * 4 + q) * 16 + 0, :], rhs=uc, start=False, stop=False), reads=rd, writes=[pk_])
                for tau in range(1, 16):
                    S.op('pe', lambda e: e.matmul(pyv[:, :, 0:16 - tau], lhsT=Kt_sb[:, (1 * 4 + q) * 16 + tau, :], rhs=ucv[:, :, tau:16],
                                                  start=False, stop=(tau == 15)), reads=rd, writes=[pk_])
                S.op('pe', lambda e: e.matmul(p4[:].rearrange("p j k -> p (j k)"), lhsT=zlhs[:], rhs=uc, start=True, stop=False),
                     reads=['zlhs', ('ub', i2)], writes=[p4k])
                for pl in range(4):
                    pair = q * 4 + pl
                    for d in range(2):
                        Hx = Hf[i2] if d == 0 else Hb[i2]
                        hkey = ('Hf', i2) if d == 0 else ('Hb', i2)
                        for j in range(16):
                            m = j + 1 if d == 0 else 16 - j
                            if pl < 3:
                                osl = p4[32 * pl:32 * pl + 32, j, :]; csl = slice(q * 160 + 32 * pl, q * 160 + 32 * pl + 32)
                            else:
                                osl = p4[64:128, j, :]; csl = slice(q * 160 + 96, q * 160 + 160)
                            for reim in range(2):
                                last = (pl == 3 and d == 1 and j == 15 and reim == 1)
                                S.op('pe', lambda e: e.matmul(osl, lhsT=Gsb[:, (d * 16 + m - 1) * 2 + reim, csl], rhs=Hx[:, pair, reim, :],
                                                              start=False, stop=last), reads=gkeys + [hkey], writes=[p4k])
                S.op('act', lambda e: e.activation(out=y4[q % 2][:].rearrange("p (k j) -> p j k", j=16), in_=p4[:], func=AF.Copy),
                     reads=[p4k], writes=[('y4', q % 2)])
                S.op('dve', lambda e: e.tensor_tensor(out=ysum[q % 2][:], in0=pyt[:], in1=y4[q % 2][:], op=ALU.add),
                     reads=[pk_, ('y4', q % 2)], writes=[('ysum', q % 2)])
                S.op('act', lambda e: e.activation(out=aT[i2][:, q, :], in_=ysum[q % 2][:], func=AF.Gelu), reads=[('ysum', q % 2)], writes=[('aT', i2, q)])
            for oc in range(4):
                pgt = pg[oc % 2]; pgk = ('pg', oc % 2)
                for q in range(4):
                    S.op('pe', lambda e: e.matmul(pgt[:], lhsT=wglu[:, q, oc * 128:(oc + 1) * 128], rhs=aT[i2][:, q, :],
                                                  start=(q == 0), stop=(q == 3)), reads=['wglu', ('aT', i2, q)], writes=[pgk])
                sg = sig[oc % 2]; sgk = ('sig', oc % 2)
                S.op('act', lambda e: e.activation(out=sg[:], in_=pgt[:], func=AF.Sigmoid, bias=bglu[:, oc:oc + 1]),
                     reads=[pgk, 'bglu'], writes=[sgk])
                yt = yo[yo_ctr % 4]; yk = ('yo', yo_ctr % 4); yo_ctr += 1
                S.op('dve', lambda e: e.tensor_tensor(out=yt[:], in0=aT[i2][:, oc, :], in1=sg[:], op=ALU.mult),
                     reads=[('aT', i2, oc), sgk], writes=[yk])
                S.dma('pool', yaT_d[oc * 128:(oc + 1) * 128, 512 * n:512 * n + 512], yt[:], reads=[yk], writes=['yaT_d'])
        S.barrier()
    if 's5' in dbg:
        dbg_out['yaT'] = dout("dbg_yaT", [512, 8192], BF16)
        for r0 in range(0, 512, 128):
            S.dma('sp', dbg_out['yaT'][r0:r0 + 128, :], yaT_d[r0:r0 + 128, :], reads=['yaT_d'])
    return 'ok'

NEG = -30000.0


def na_phase(nc, S, es, sb, ps, din, dscr, dout, dbg, dbg_out, identb, qT_d, kT_d, v_d, ybT_d, nblocks=16):
    rpbT_in = din("rpbT", [128, 8 * 14 * 64])
    maskT_in = din("maskT", [128, 64])
    with ExitStack() as pa:
        Tb = sb("n_Tb", [128, 8, 14, 64], F32, pa)
        mk = sb("n_mk", [128, 64], F32, pa)
        kctx = sb("n_kctx", [128, 4, 256], BF16, pa)
        vctx = sb("n_vctx", [128, 2, 8, 65], BF16, pa)
        qb = [sb("n_qb%d" % i, [128, 4, 512], BF16, pa) for i in range(2)]
        kw = [sb("n_kw%d" % i, [128, 4, 960], BF16, pa) for i in range(2)]
        vw = [sb("n_vw%d" % i, [128, 15, 8, 65], BF16, pa) for i in range(2)]
        sS = [sb("n_sS%d" % i, [128, 4, 64], F32, pa) for i in range(3)]
        P = [sb("n_Pn%d" % i, [128, 6, 64], BF16, pa) for i in range(4)]
        rc = [sb("n_rc%d" % i, [128, 8], F32, pa) for i in range(2)]
        ybt = [sb("n_ybt%d" % i, [128, 8, 64], BF16, pa) for i in range(2)]
        ybT = [sb("n_ybT%d" % i, [128, 4, 512], BF16, pa) for i in range(2)]
        pS = [ps("n_pS%d" % i, [128, 6, 64], F32, pa) for i in range(3)]
        pO = [ps("n_pO%d" % i, [128, 4, 65], F32, pa) for i in range(4)]
        pT = ps("n_pTn", [128, 4, 128], BF16, pa)

        S.dma('sp', Tb[:].rearrange("p h a q -> p (h a q)"), rpbT_in, writes=['Tb'])
        S.dma('sp', mk[:], maskT_in, writes=['mk'])
        S.dma('sp', kctx[:], kT_d[:, 0:256].rearrange("(c p) t -> p c t", p=128), reads=['kT_d'], writes=['kctx'])
        S.dma('sp', vctx[:], v_d[0:256].rearrange("(m p) h d -> p m h d", p=128), reads=['v_d'], writes=['vctx'])
        S.op('dve', lambda e: e.tensor_tensor(out=Tb[:].rearrange("p h a q -> p (h a) q"), in0=Tb[:].rearrange("p h a q -> p (h a) q"),
                                              in1=mk[:].unsqueeze(1).to_broadcast([128, 112, 64]), op=ALU.add),
             reads=['Tb', 'mk'], writes=['Tb'])
        item = 0
        pend = []
        for b in range(nblocks):
            i2 = b % 2
            r0 = 8 * b
            lo = min(max(r0 - 4, 0), 120)
            hi = min(max(r0 + 7 - 4, 0), 120) + 8
            nrows = hi - lo
            S.dma('sp', qb[i2][:], qT_d[:, 512 * b:512 * b + 512].rearrange("(c p) t -> p c t", p=128), reads=['qT_d'], writes=[('qb', i2)])
            S.dma('sp', kw[i2][:, :, 0:64 * nrows], kT_d[:, 256 + 64 * lo:256 + 64 * hi].rearrange("(c p) t -> p c t", p=128),
                  reads=['kT_d'], writes=[('kw', i2)])
            for s in range(nrows - 1):
                S.dma('sp', vw[i2][:, s], v_d[256 + 64 * (lo + s):256 + 64 * (lo + s) + 128], reads=['v_d'], writes=[('vw', i2)])
            for rr in range(8):
                r = r0 + rr
                krs = min(max(r - 4, 0), 120)
                a0 = krs - r + 7
                half = 64 * (r % 2)
                pp = (r // 2) % 2
                for h in range(8):
                    hc, hb = h // 2, 64 * (h % 2)
                    it = item % 3
                    ip = item % 4
                    item += 1
                    pst = pS[it]; psk = ('pS', it)
                    for c in range(4):
                        kc0 = 64 * (krs + 2 * c - lo)
                        S.op('pe', lambda e: e.matmul(pst[:, c, :], lhsT=kw[i2][hb:hb + 64, hc, kc0:kc0 + 128],
                                                      rhs=qb[i2][hb:hb + 64, hc, 64 * rr:64 * rr + 64], start=True, stop=True),
                             reads=[('kw', i2), ('qb', i2)], writes=[psk])
                    for c in range(2):
                        S.op('pe', lambda e: e.matmul(pst[:, 4 + c, :], lhsT=kctx[hb:hb + 64, hc, 128 * c:128 * c + 128],
                                                      rhs=qb[i2][hb:hb + 64, hc, 64 * rr:64 * rr + 64], start=True, stop=True),
                             reads=['kctx', ('qb', i2)], writes=[psk])
                    S.op('dve', lambda e: e.tensor_tensor(out=sS[it][:], in0=pst[:, 0:4, :], in1=Tb[:, h, a0:a0 + 7:2, :], op=ALU.add),
                         reads=[psk, 'Tb'], writes=[('sS', it)])
                    S.op('act', lambda e: e.activation(out=P[ip][:, 0:4, :], in_=sS[it][:], func=AF.Exp), reads=[('sS', it)], writes=[('P', ip)])
                    S.op('act', lambda e: e.activation(out=P[ip][:, 4:6, :], in_=pst[:, 4:6, :], func=AF.Exp), reads=[psk], writes=[('P', ip)])
                    if len(pend) == 2:
                        pend.pop(0)()
                    def mk_pv(it=ip, i2=i2, krs=krs, lo=lo, h=h, half=half, pp=pp, r=r, rr=rr, b=b):
                        def pv():
                            pot = pO[pp * 2 + h // 4]; pok = ('pO', pp * 2 + h // 4)
                            for c in range(6):
                                if c < 4:
                                    rhs = vw[i2][:, krs + 2 * c - lo, h, :]
                                    rk = ('vw', i2)
                                else:
                                    rhs = vctx[:, c - 4, h, :]
                                    rk = 'vctx'
                                S.op('pe', lambda e: e.matmul(pot[half:half + 64, h % 4, :], lhsT=P[it][:, c, :], rhs=rhs,
                                                              start=(c == 0), stop=(c == 5)), reads=[('P', it), rk], writes=[pok])
                            if h % 4 == 3:
                                hh = h // 4
                                yk = ('ybt', pp)
                                S.op('dve', lambda e: e.reciprocal(out=rc[pp][half:half + 64, 4 * hh:4 * hh + 4], in_=pot[half:half + 64, :, 64]),
                                     reads=[pok], writes=[('rc', pp)])
                                S.op('dve', lambda e: e.tensor_tensor(out=ybt[pp][half:half + 64, 4 * hh:4 * hh + 4, :], in0=pot[half:half + 64, :, 0:64],
                                                                      in1=rc[pp][half:half + 64, 4 * hh:4 * hh + 4].unsqueeze(2).to_broadcast([64, 4, 64]),
                                                                      op=ALU.mult), reads=[pok, ('rc', pp)], writes=[yk])
                            if h == 7 and r % 2 == 1:
                                tk = ('ybT', b % 2)
                                for c4 in range(4):
                                    S.op('pe', lambda e: e.transpose(out=pT[:, c4, :], in_=ybt[pp][:, 2 * c4:2 * c4 + 2, :].rearrange("p a d -> p (a d)"),
                                                                     identity=identb[:]), reads=[('ybt', pp), 'identb'], writes=['pTn'])
                                S.op('act', lambda e: e.activation(out=ybT[b % 2][:, :, 64 * (rr - 1):64 * (rr - 1) + 128], in_=pT[:], func=AF.Copy),
                                     reads=['pTn'], writes=[tk])
                                if rr == 7:
                                    S.dma('act', ybT_d[:, 512 * b:512 * b + 512].rearrange("(c p) t -> p c t", p=128), ybT[b % 2][:],
                                          reads=[tk], writes=['ybT_d'])
                        return pv
                    pend.append(mk_pv())
        for p_ in pend:
            p_()
        S.barrier()
    if 'na' in dbg:
        dbg_out['ybT'] = dout("dbg_ybT", [512, 8192], BF16)
        for r0 in range(0, 512, 128):
            S.dma('sp', dbg_out['ybT'][r0:r0 + 128, :], ybT_d[r0:r0 + 128, :], reads=['ybT_d'])


def na_host(inp):
    rpb = inp['na_rpb'][0].astype(np.float32)
    i = np.arange(2)[:, None, None, None]
    kc = np.arange(64)[None, :, None, None]
    a = np.arange(14)[None, None, :, None]
    qc = np.arange(64)[None, None, None, :]
    dcol = np.clip(kc - qc + 15, 0, 30)
    drow = a + i
    T = rpb[:, drow, dcol]
    T = np.broadcast_to(T, (8, 2, 64, 14, 64)).transpose(1, 2, 0, 3, 4).reshape(128, 8 * 14 * 64)
    q = np.arange(64)
    qcs = np.clip(q - 8, 0, 48)
    k = np.arange(64)[:, None]
    inw = (k >= qcs[None, :]) & (k < qcs[None, :] + 16)
    m = np.where(inw, 0.0, NEG).astype(np.float32)
    m = np.concatenate([m, m], axis=0)
    return {'rpbT': np.ascontiguousarray(T.astype(np.float32)), 'maskT': np.ascontiguousarray(m)}

D = 1024


def merge_phase(nc, S, es, sb, ps, din, dscr, dout, dbg, dbg_out, identf, identb, epsc, bc_g1, bc_mul2, bc_add2,
                xcat, out, yaT_d, ybT_d, sgaT_d, sgbT_d, h2_d, aff_d, affT_d, nblocks=16):
    wpa_in = din("w_proj_a", [512, D]); wpb_in = din("w_proj_b", [512, D]); wout_in = din("w_out", [D, D])
    wr_in = din("w_routerT", [128, 8, 16])
    with ExitStack() as pa:
        wpa = sb("g_wpa", [128, 4, D], BF16, pa); wpb = sb("g_wpb", [128, 4, D], BF16, pa)
        wout = sb("g_wout", [128, 8, D], BF16, pa)
        wr = sb("g_wr", [128, 8, 16], BF16, pa)
        ya = [sb("g_mya%d" % i, [128, 4, 512], BF16, pa) for i in range(2)]
        yb = [sb("g_myb%d" % i, [128, 4, 512], BF16, pa) for i in range(2)]
        sga = [sb("g_msga%d" % i, [128, 8, 512], BF16, pa) for i in range(2)]
        sgb = [sb("g_msgb%d" % i, [128, 8, 512], BF16, pa) for i in range(2)]
        mT = [sb("g_mT%d" % i, [128, 8, 512], BF16, pa) for i in range(2)]
        t1 = [sb("g_mt1_%d" % i, [128, 512], F32, pa) for i in range(2)]
        t2 = [sb("g_mt2_%d" % i, [128, 512], F32, pa) for i in range(2)]
        xt = [sb("g_mxt%d" % i, [128, D], F32, pa) for i in range(2)]
        x1 = [sb("g_mx1_%d" % i, [128, D], F32, pa) for i in range(3)]
        h2 = [sb("g_mh2_%d" % i, [128, D], F32, pa) for i in range(3)]
        h2b = [sb("g_mh2b_%d" % i, [128, D], BF16, pa) for i in range(3)]
        h2T = [sb("g_mh2T_%d" % i, [128, 8, 128], BF16, pa) for i in range(3)]
        junk = sb("g_mjunk", [128, D], BF16, pa)
        st = [sb("g_mst%d" % i, [128, 8], F32, pa) for i in range(3)]
        lg = [sb("g_mlg%d" % i, [128, 16], F32, pa) for i in range(3)]
        af = [sb("g_maf%d" % i, [128, 16], F32, pa) for i in range(3)]
        A16 = sb("g_A16", [16, 8192], F32, pa)
        pab = [ps("g_mpab%d" % i, [128, 512], F32, pa) for i in range(4)]
        pyo = ps("g_mpyo", [128, D], F32, pa)
        phT = ps("g_mphT", [128, 8, 128], BF16, pa)
        pmisc = ps("g_mpmisc", [128, 256], F32, pa)
        plg = pmisc[:, 0:16]
        paT = pmisc[0:16, 128:256]

        for q in range(4):
            S.dma('pool', wpa[:, q, :], wpa_in[q * 128:(q + 1) * 128, :], writes=['wpa'])
            S.dma('pool', wpb[:, q, :], wpb_in[q * 128:(q + 1) * 128, :], writes=['wpb'])
        for dc in range(8):
            S.dma('pool', wout[:, dc, :], wout_in[dc * 128:(dc + 1) * 128, :], writes=['wout'])
        S.dma('pool', wr[:], wr_in, writes=['wr'])
        tile_ctr = 0
        pend = []
        for b in range(nblocks):
            i2 = b % 2
            cs = slice(512 * b, 512 * b + 512)
            S.dma('sp', ya[i2][:], yaT_d[:, cs].rearrange("(c p) t -> p c t", p=128), reads=['yaT_d'], writes=[('ya', i2)])
            S.dma('sp', yb[i2][:], ybT_d[:, cs].rearrange("(c p) t -> p c t", p=128), reads=['ybT_d'], writes=[('yb', i2)])
            S.dma('sp', sga[i2][:], sgaT_d[:, cs].rearrange("(c p) t -> p c t", p=128), reads=['sgaT_d'], writes=[('sga', i2)])
            S.dma('sp', sgb[i2][:], sgbT_d[:, cs].rearrange("(c p) t -> p c t", p=128), reads=['sgbT_d'], writes=[('sgb', i2)])
            for dc in range(8):
                j2 = dc % 2
                pA_, pB_ = pab[2 * j2], pab[2 * j2 + 1]
                for q in range(4):
                    S.op('pe', lambda e: e.matmul(pA_[:], lhsT=wpa[:, q, dc * 128:(dc + 1) * 128], rhs=ya[i2][:, q, :],
                                                  start=(q == 0), stop=(q == 3)), reads=['wpa', ('ya', i2)], writes=[('pA', j2)])
                for q in range(4):
                    S.op('pe', lambda e: e.matmul(pB_[:], lhsT=wpb[:, q, dc * 128:(dc + 1) * 128], rhs=yb[i2][:, q, :],
                                                  start=(q == 0), stop=(q == 3)), reads=['wpb', ('yb', i2)], writes=[('pB', j2)])
                S.op('dve', lambda e: e.tensor_tensor(out=t1[j2][:], in0=pA_[:], in1=sga[i2][:, dc, :], op=ALU.mult),
                     reads=[('pA', j2), ('sga', i2)], writes=[('t1', j2)])
                S.op('dve', lambda e: e.tensor_tensor(out=t2[j2][:], in0=pB_[:], in1=sgb[i2][:, dc, :], op=ALU.mult),
                     reads=[('pB', j2), ('sgb', i2)], writes=[('t2', j2)])
                S.op('pool', lambda e: e.tensor_tensor(out=mT[i2][:, dc, :], in0=t1[j2][:], in1=t2[j2][:], op=ALU.add),
                     reads=[('t1', j2), ('t2', j2)], writes=[('mT', i2)])
            for ti in range(4):
                k2 = tile_ctr % 3
                kx = tile_ctr % 2
                tile_ctr += 1
                tok0 = 512 * b + 128 * ti
                S.dma('sp', xt[kx][:], xcat[256 + tok0:256 + tok0 + 128, :], writes=[('xt', kx)])
                for hf in range(2):
                    for dc in range(8):
                        S.op('pe', lambda e: e.matmul(pyo[:, hf * 512:(hf + 1) * 512], lhsT=mT[i2][:, dc, ti * 128:(ti + 1) * 128],
                                                      rhs=wout[:, dc, hf * 512:(hf + 1) * 512], start=(dc == 0), stop=(dc == 7)),
                             reads=[('mT', i2), 'wout'], writes=['pyo'])
                S.op('dve', lambda e: e.tensor_tensor(out=x1[k2][:], in0=pyo[:], in1=bc_g1[:], op=ALU.mult),
                     reads=['pyo', 'bc_g1'], writes=[('x1', k2)])
                S.op('dve', lambda e: e.tensor_tensor(out=x1[k2][:], in0=x1[k2][:], in1=xt[kx][:], op=ALU.add),
                     reads=[('x1', k2), ('xt', kx)], writes=[('x1', k2)])
                S.dma('pool', out[tok0:tok0 + 128, :], x1[k2][:], reads=[('x1', k2)], writes=['out'])
                s_ = st[k2]; sk = ('st', k2)
                S.op('act', lambda e: e.activation(out=junk[:], in_=x1[k2][:], func=AF.Square, accum_out=s_[:, 0:1]),
                     reads=[('x1', k2)], writes=['mjunk', sk])
                S.op('act', lambda e: e.activation(out=s_[:, 6:7], in_=s_[:, 0:1], func=AF.Ln, scale=1.0 / D, bias=epsc[:, 0:1]),
                     reads=[sk, 'epsc'], writes=[sk])
                S.op('act', lambda e: e.activation(out=s_[:, 1:2], in_=s_[:, 6:7], func=AF.Exp, scale=-0.5), reads=[sk], writes=[sk])
                S.op('dve', lambda e: e.scalar_tensor_tensor(out=h2[k2][:], in0=x1[k2][:], scalar=s_[:, 1:2], in1=bc_mul2[:],
                                                             op0=ALU.mult, op1=ALU.mult), reads=[('x1', k2), sk, 'bc_mul2'], writes=[('h2', k2)])
                S.op('dve', lambda e: e.tensor_tensor(out=h2[k2][:], in0=h2[k2][:], in1=bc_add2[:], op=ALU.add),
                     reads=[('h2', k2), 'bc_add2'], writes=[('h2', k2)])
                S.op('act', lambda e: e.activation(out=h2b[k2][:], in_=h2[k2][:], func=AF.Copy), reads=[('h2', k2)], writes=[('h2b', k2)])
                S.dma('act', h2_d[tok0:tok0 + 128, :], h2b[k2][:], reads=[('h2b', k2)], writes=['h2_d'])
                def mk_tail(k2=k2, tok0=tok0, s_=s_, sk=sk):
                    def tail():
                        for dc in range(8):
                            S.op('pe', lambda e: e.transpose(out=phT[:, dc, :], in_=h2b[k2][:, dc * 128:(dc + 1) * 128], identity=identb[:]),
                                 reads=[('h2b', k2), 'identb'], writes=['phT'])
                        S.op('act', lambda e: e.activation(out=h2T[k2][:], in_=phT[:], func=AF.Copy), reads=['phT'], writes=[('h2T', k2)])
                        for dc in range(8):
                            S.op('pe', lambda e: e.matmul(plg, lhsT=h2T[k2][:, dc, :], rhs=wr[:, dc, :], start=(dc == 0), stop=(dc == 7)),
                                 reads=[('h2T', k2), 'wr'], writes=['plg'])
                        S.op('dve', lambda e: e.tensor_copy(out=lg[k2][:], in_=plg), reads=['plg'], writes=[('lg', k2)])
                        S.op('dve', lambda e: e.reduce_max(out=s_[:, 2:3], in_=lg[k2][:], axis=AX.X), reads=[('lg', k2)], writes=[sk])
                        S.op('dve', lambda e: e.tensor_scalar(out=s_[:, 3:4], in0=s_[:, 2:3], scalar1=-1.0, scalar2=None, op0=ALU.mult),
                             reads=[sk], writes=[sk])
                        S.op('act', lambda e: e.activation(out=af[k2][:], in_=lg[k2][:], func=AF.Exp, bias=s_[:, 3:4], accum_out=s_[:, 4:5]),
                             reads=[('lg', k2), sk], writes=[('af', k2), sk])
                        S.op('dve', lambda e: e.reciprocal(out=s_[:, 5:6], in_=s_[:, 4:5]), reads=[sk], writes=[sk])
                        S.op('dve', lambda e: e.tensor_scalar(out=af[k2][:], in0=af[k2][:], scalar1=s_[:, 5:6], scalar2=None, op0=ALU.mult),
                             reads=[('af', k2), sk], writes=[('af', k2)])
                        S.dma('pool', aff_d[tok0:tok0 + 128, :], af[k2][:], reads=[('af', k2)], writes=['aff_d'])
                        S.op('pe', lambda e: e.transpose(out=paT, in_=af[k2][:], identity=identf[:]), reads=[('af', k2), 'identf'], writes=['paT'])
                        S.op('act', lambda e: e.activation(out=A16[:, tok0:tok0 + 128], in_=paT, func=AF.Copy), reads=['paT'], writes=['A16'])
                    return tail
                if len(pend) == 2:
                    pend.pop(0)()
                pend.append(mk_tail())
        for t_ in pend:
            t_()
        S.dma('sp', affT_d, A16[:], reads=['A16'], writes=['affT_d'])
        S.barrier()
        if 'mg2' in dbg:
            for nm, t, shp, dt in (('bcg1', bc_g1, [128, D], F32), ('mT0', mT[(nblocks - 1) % 2], [128, 8, 512], BF16), ('t1', t1[1], [128, 512], F32),
                                   ('sga0', sga[(nblocks - 1) % 2], [128, 8, 512], BF16), ('ya0', ya[(nblocks - 1) % 2], [128, 4, 512], BF16),
                                   ('yb0', yb[(nblocks - 1) % 2], [128, 4, 512], BF16), ('wpa', wpa, [128, 4, D], BF16), ('x1t', x1[1], [128, D], F32),
                                   ('xt', xt[1], [128, D], F32)):
                dbg_out[nm] = dout("dbg_" + nm, shp, dt)
                S.dma('sp', dbg_out[nm], t[:], reads=[])
    if 'mg' in dbg:
        dbg_out['aff'] = dout("dbg_aff", [8192, 16])
        S.dma('sp', dbg_out['aff'], aff_d, reads=['aff_d'])
        dbg_out['affT'] = dout("dbg_affT", [16, 8192])
        S.dma('sp', dbg_out['affT'], affT_d, reads=['affT_d'])
        dbg_out['h2'] = dout("dbg_h2", [8192, D], BF16)
        for r0 in range(0, 8192, 1024):
            S.dma('sp', dbg_out['h2'][r0:r0 + 1024, :], h2_d[r0:r0 + 1024, :], reads=['h2_d'])

D = 1024
CAP = 1024
NT = 8192


def moe_phase(nc, S, es, sb, ps, din, dscr, dout, dbg, dbg_out, identf, identb, bc_g2, out, h2_d, aff_d, affT_d, nexp=16):
    wg_in = din("w_e_gate", [16, D, D]); wu_in = din("w_e_up", [16, D, D]); wd_in = din("w_e_down", [16, D, D])
    m16_in = din("m16", [128, 128])
    with ExitStack() as pm:
        cum_d = dscr("cum_d", [16, NT], F32)
        slotv = sb("e_slotv", [128, 8], F32, pm)
        thr_dbg = sb("e_thr", [128, 1], F32, pm)
        S.op('pool', lambda e: e.iota(slotv[:], pattern=[[128, 8]], base=0, channel_multiplier=1, allow_small_or_imprecise_dtypes=True),
             writes=['slotv'])
        with ExitStack() as pa:
            A128 = sb("e_A128", [128, 1024], F32, pa)
            A16 = sb("e_A16", [16, NT], F32, pa)
            C16 = sb("e_C16", [16, NT], F32, pa)
            M16 = sb("e_M16", [128, 128], F32, pa)
            junk = sb("e_junk", [128, 1024], BF16, pa)
            lo = sb("e_lo", [128, 1], F32, pa); mid = sb("e_mid", [128, 1], F32, pa)
            cnt = sb("e_cnt", [128, 1], F32, pa); g = sb("e_g", [128, 1], F32, pa)
            one16 = sb("e_one16", [16, 1], F32, pa)
            ptot = ps("e_ptot", [128, 1], F32, pa)
            for s in range(8):
                S.dma('sp', A128[16 * s:16 * s + 16, :], affT_d[:, 1024 * s:1024 * s + 1024], reads=['affT_d'], writes=['A128'])
            S.dma('sp', A16[:], affT_d, reads=['affT_d'], writes=['A16'])
            S.dma('sp', M16[:], m16_in, writes=['M16'])
            S.op('dve', lambda e: e.memset(lo[:], 0.0), writes=['lo'])
            S.op('dve', lambda e: e.memset(one16[:], 1.0), writes=['one16'])
            for k in range(30):
                hk = 2.0 ** -(k + 1)
                S.op('dve', lambda e: e.tensor_scalar(out=mid[:], in0=lo[:], scalar1=hk, scalar2=None, op0=ALU.add), reads=['lo'], writes=['mid'])
                S.op('dve', lambda e: e.tensor_scalar(out=junk[:], in0=A128[:], scalar1=mid[:, 0:1], scalar2=0.0, op0=ALU.is_gt, op1=ALU.add,
                                                      accum_out=cnt[:]), reads=['A128', 'mid'], writes=['junk', 'cnt'])
                S.op('pe', lambda e: e.matmul(ptot[:], lhsT=M16[:], rhs=cnt[:], start=True, stop=True), reads=['M16', 'cnt'], writes=['ptot'])
                S.op('dve', lambda e: e.tensor_scalar(out=g[:], in0=ptot[:], scalar1=float(CAP), scalar2=hk, op0=ALU.is_ge, op1=ALU.mult),
                     reads=['ptot'], writes=['g'])
                S.op('dve', lambda e: e.tensor_tensor(out=lo[:], in0=lo[:], in1=g[:], op=ALU.add), reads=['lo', 'g'], writes=['lo'])
            S.op('dve', lambda e: e.tensor_copy(out=thr_dbg[:], in_=lo[:]), reads=['lo'], writes=['thr'])
            S.op('dve', lambda e: e.tensor_scalar(out=A16[:], in0=A16[:], scalar1=lo[0:16, 0:1], scalar2=None, op0=ALU.is_gt),
                 reads=['A16', 'lo'], writes=['A16'])
            S.op('dve', lambda e: e.tensor_tensor_scan(out=C16[:], data0=one16[:, 0:1].to_broadcast([16, NT]), data1=A16[:], initial=0.0,
                                                       op0=ALU.mult, op1=ALU.add), reads=['A16', 'one16'], writes=['C16'])
            S.dma('sp', cum_d, C16[:], reads=['C16'], writes=['cum_d'])
            S.barrier()
        if 'moe_thr' in dbg:
            dbg_out['thr'] = dout("dbg_thr", [128, 1])
            S.dma('sp', dbg_out['thr'], thr_dbg[:], reads=['thr'])
        with ExitStack() as pe_:
            Wg = [sb("e_Wg%d" % i, [128, 8, D], BF16, pe_) for i in range(2)]
            Wu = [sb("e_Wu%d" % i, [128, 8, D], BF16, pe_) for i in range(2)]
            Wd = [sb("e_Wd%d" % i, [128, 8, D], BF16, pe_) for i in range(2)]
            xe = [sb("e_xe%d" % i, [128, D], BF16, pe_) for i in range(8)]
            ag = [[sb("e_ag%d_%d" % (p_, i), [128, 16], F32, pe_) for i in range(8)] for p_ in range(2)]
            cq = [sb("e_cq%d" % i, [128, 2048], F32, pe_) for i in range(2)]
            junkq = sb("e_junkq", [128, 2048], BF16, pe_)
            acc = [sb("e_acc%d" % i, [128, 4, 8], F32, pe_) for i in range(3)]
            idxs = sb("e_idxs", [128, 8], F32, pe_)
            idxi = [sb("e_idxi%d" % i, [128, 8], I32, pe_) for i in range(3)]
            xeT = sb("e_xeT", [128, 8, CAP], BF16, pe_)
            hidT = sb("e_hidT", [128, 8, CAP], BF16, pe_)
            sg = [sb("e_sg%d" % i, [128, 512], F32, pe_) for i in range(2)]
            ye = [sb("e_ye%d" % i, [128, D], F32, pe_) for i in range(2)]
            pGU = [ps("e_pGU%d" % i, [128, 512], F32, pe_) for i in range(3)]
            pY = ps("e_pY", [128, D], F32, pe_)
            pT = ps("e_pT", [128, 8, 128], BF16, pe_)
            pTi = pT[:].bitcast(F32) if False else None

            def load_w(e):
                i2 = e % 2
                for dc in range(8):
                    S.dma('pool', Wg[i2][:, dc, :], wg_in[e, dc * 128:(dc + 1) * 128, :], writes=[('Wg', i2)])
                    S.dma('pool', Wu[i2][:, dc, :], wu_in[e, dc * 128:(dc + 1) * 128, :], writes=[('Wu', i2)])
                for dc in range(8):
                    S.dma('pool', Wd[i2][:, dc, :], wd_in[e, dc * 128:(dc + 1) * 128, :], writes=[('Wd', i2)])

            def load_cq(n):
                e_, qd = n // 4, n % 4
                if e_ >= nexp:
                    return
                S.dma('sp', cq[n % 2][:], cum_d[e_:e_ + 1, qd * 2048:(qd + 1) * 2048].to_broadcast([128, 2048]),
                      reads=['cum_d'], writes=[('cq', n % 2)])

            def idx_piece(e, piece):
                j3 = e % 3
                n = e * 4 + piece // 2
                if piece % 2 == 0:
                    if n == 0:
                        load_cq(0)
                    load_cq(n + 1)
                for sc in range(4 * (piece % 2), 4 * (piece % 2) + 4):
                    S.op('dve', lambda e_: e_.tensor_scalar(out=junkq[:], in0=cq[n % 2][:], scalar1=slotv[:, sc:sc + 1], scalar2=0.0,
                                                            op0=ALU.is_le, op1=ALU.add, accum_out=acc[j3][:, piece // 2, sc:sc + 1]),
                         reads=[('cq', n % 2), 'slotv'], writes=['junkq', ('acc', j3)])
                if piece == 7:
                    S.op('dve', lambda e_: e_.tensor_tensor(out=idxs[:], in0=acc[j3][:, 0, :], in1=acc[j3][:, 1, :], op=ALU.add),
                         reads=[('acc', j3)], writes=['idxs'])
                    S.op('dve', lambda e_: e_.tensor_tensor(out=idxs[:], in0=idxs[:], in1=acc[j3][:, 2, :], op=ALU.add),
                         reads=[('acc', j3), 'idxs'], writes=['idxs'])
                    S.op('dve', lambda e_: e_.tensor_tensor(out=idxs[:], in0=idxs[:], in1=acc[j3][:, 3, :], op=ALU.add),
                         reads=[('acc', j3), 'idxs'], writes=['idxs'])
                    S.op('dve', lambda e_: e_.tensor_copy(out=idxi[j3][:], in_=idxs[:]), reads=['idxs'], writes=[('idxi', j3)])

            def gather(e):
                j2 = e % 3
                for sc in range(8):
                    S.custom_dma('pool', lambda e_: e_.indirect_dma_start(out=xe[sc][:, :], out_offset=None, in_=h2_d[:, :],
                                 in_offset=bass.IndirectOffsetOnAxis(ap=idxi[j2][:, sc:sc + 1], axis=0)),
                                 reads=[('idxi', j2), 'h2_d'], writes=[('xe', sc)])
                    S.custom_dma('pool', lambda e_: e_.indirect_dma_start(out=ag[e % 2][sc][:, :], out_offset=None, in_=aff_d[:, :],
                                 in_offset=bass.IndirectOffsetOnAxis(ap=idxi[j2][:, sc:sc + 1], axis=0)),
                                 reads=[('idxi', j2), 'aff_d'], writes=[('ag', e % 2, sc)])

            gu_ctr = [0]

            def xpose(e):
                for sc in range(8):
                    for dc in range(8):
                        S.op('pe', lambda e_: e_.transpose(out=pT[:, dc, :], in_=xe[sc][:, dc * 128:(dc + 1) * 128], identity=identb[:]),
                             reads=[('xe', sc), 'identb'], writes=['pT'])
                    S.op('act', lambda e_: e_.activation(out=xeT[:, :, sc * 128:(sc + 1) * 128], in_=pT[:], func=AF.Copy),
                         reads=['pT'], writes=['xeT'])
            def ffn(e, nxt):
                i2 = e % 2; j2 = e % 3
                for fc in range(8):
                    for hf in range(2):
                        a = gu_ctr[0] % 3; gu_ctr[0] += 1
                        b = gu_ctr[0] % 3; gu_ctr[0] += 1
                        for dc in range(8):
                            S.op('pe', lambda e_: e_.matmul(pGU[a][:], lhsT=Wg[i2][:, dc, fc * 128:(fc + 1) * 128], rhs=xeT[:, dc, hf * 512:(hf + 1) * 512],
                                                            start=(dc == 0), stop=(dc == 7)), reads=[('Wg', i2), 'xeT'], writes=[('pGU', a)])
                        for dc in range(8):
                            S.op('pe', lambda e_: e_.matmul(pGU[b][:], lhsT=Wu[i2][:, dc, fc * 128:(fc + 1) * 128], rhs=xeT[:, dc, hf * 512:(hf + 1) * 512],
                                                            start=(dc == 0), stop=(dc == 7)), reads=[('Wu', i2), 'xeT'], writes=[('pGU', b)])
                        s2 = (fc * 2 + hf) % 2
                        S.op('act', lambda e_: e_.activation(out=sg[s2][:], in_=pGU[a][:], func=AF.Silu), reads=[('pGU', a)], writes=[('sg', s2)])
                        S.op('dve', lambda e_: e_.tensor_tensor(out=hidT[:, fc, hf * 512:(hf + 1) * 512], in0=pGU[b][:], in1=sg[s2][:], op=ALU.mult),
                             reads=[('pGU', b), ('sg', s2)], writes=['hidT'])
                    if nxt is not None:
                        idx_piece(nxt, fc)
                for sc in range(8):
                    for hf in range(2):
                        for fc in range(8):
                            S.op('pe', lambda e_: e_.matmul(pY[:, hf * 512:(hf + 1) * 512], lhsT=hidT[:, fc, sc * 128:(sc + 1) * 128],
                                                            rhs=Wd[i2][:, fc, hf * 512:(hf + 1) * 512], start=(fc == 0), stop=(fc == 7)),
                                 reads=['hidT', ('Wd', i2)], writes=['pY'])
                    y2 = sc % 2
                    S.op('dve', lambda e_: e_.scalar_tensor_tensor(out=ye[y2][:], in0=pY[:], scalar=ag[e % 2][sc][:, e:e + 1], in1=bc_g2[:],
                                                                   op0=ALU.mult, op1=ALU.mult), reads=['pY', ('ag', e % 2, sc), 'bc_g2'], writes=[('ye', y2)])
                    S.custom_dma('pool', lambda e_: e_.indirect_dma_start(out=out[:, :], out_offset=bass.IndirectOffsetOnAxis(ap=idxi[j2][:, sc:sc + 1], axis=0),
                                 in_=ye[y2][:, :], in_offset=None, compute_op=ALU.add), reads=[('ye', y2), ('idxi', j2)], writes=['out'])

            load_w(0)
            for p in range(8):
                idx_piece(0, p)
            gather(0)
            if nexp > 1:
                load_w(1)
                for p in range(8):
                    idx_piece(1, p)
            for e in range(nexp):
                xpose(e)
                if e + 1 < nexp:
                    gather(e + 1)
                    if e >= 1:
                        load_w(e + 1)
                ffn(e, e + 2 if e + 2 < nexp else None)
            if 'moe_idx' in dbg:
                dbg_out['idxi'] = dout("dbg_idxi", [128, 8], I32)
                S.dma('sp', dbg_out['idxi'], idxi[(nexp - 1) % 3][:], reads=[('idxi', (nexp - 1) % 3)])
            S.barrier()


def moe_host(inp):
    f = np.float32
    m16 = np.zeros((128, 128), f)
    for s in range(8):
        for s2 in range(8):
            m16[16 * s:16 * s + 16, 16 * s2:16 * s2 + 16] = np.eye(16, dtype=f)
    return {'w_e_gate': np.ascontiguousarray(inp['w_e_gate'][0]), 'w_e_up': np.ascontiguousarray(inp['w_e_up'][0]),
            'w_e_down': np.ascontiguousarray(inp['w_e_down'][0]), 'm16': m16}


D = 1024
NLAT = 8192
NCTX = 256
NEXT = NLAT + NCTX
NE2 = NEXT + NCTX
EPS = 1e-6
INCOLS = 4096


def build(stop_after=99, dbg=(), nblk_lim=17, skip_s5=False, mg_blocks=16, nexp=16):
    nc = bass.Bass("TRN2", target_bir_lowering=False)
    es = ExitStack()
    S = Sched(nc, es)
    dbg_out = {}

    def din(name, shape, dt=F32):
        return nc.dram_tensor(name, list(shape), dt, kind="ExternalInput").ap()

    def dscr(name, shape, dt):
        return nc.dram_tensor(name, list(shape), dt, kind="Internal").ap()

    def dout(name, shape, dt=F32):
        return nc.dram_tensor(name, list(shape), dt, kind="ExternalOutput").ap()

    def sb(name, shape, dt, stack=None):
        return (stack or es).enter_context(nc.sbuf_tensor(name, list(shape), dt))

    def ps(name, shape, dt, stack=None):
        return (stack or es).enter_context(nc.psum_tensor(name, list(shape), dt))

    xcat = din("xcat", [NEXT, D])
    cT = din("cT", [128, 8, 2])
    w_ada = din("w_ada", [D, 6 * D])
    b_ada = din("b_ada", [1, 6 * D])
    nmixT = din("nmixT", [128, 8])
    nffn = din("nffn", [1, D])
    w_in = din("w_in", [D, INCOLS])
    ident_in = din("ident", [128, 128])
    qkg = din("qkg", [128, 2])

    out = dout("out", [NLAT, D])

    uT_d = dscr("uT_d", [512, NE2], BF16)
    qT_d = dscr("qT_d", [512, NLAT], BF16)
    kT_d = dscr("kT_d", [512, NEXT], BF16)
    v_d = dscr("v_d", [NEXT, 8, 65], BF16)
    sgaT_d = dscr("sgaT_d", [D, NLAT], BF16)
    sgbT_d = dscr("sgbT_d", [D, NLAT], BF16)

    identf = sb("identf", [128, 128], F32)
    identb = sb("identb", [128, 128], BF16)
    mul1 = sb("mul1", [128, 8, 2], F32)
    add1 = sb("add1", [128, 8, 2], F32)
    bc_g1 = sb("bc_g1", [128, D], F32)
    bc_mul2 = sb("bc_mul2", [128, D], F32)
    bc_add2 = sb("bc_add2", [128, D], F32)
    bc_g2 = sb("bc_g2", [128, D], F32)

    epsc = sb("epsc", [128, 1], F32)
    S.op('dve', lambda e: e.memset(epsc[:], EPS), writes=['epsc'])
    S.dma('sp', identf[:], ident_in, writes=['identf'])
    S.dma('pool', identb[:], ident_in, writes=['identb'])

    with ExitStack() as p0:
        wada = sb("wada", [128, 8, 6 * D], BF16, p0)
        cs_f = sb("cs_f", [128, 8, 2], F32, p0)
        cs = sb("cs", [128, 8, 2], BF16, p0)
        brow = sb("brow", [2, 6 * D], F32, p0)
        modrow = sb("modrow", [2, 6 * D], F32, p0)
        sel = sb("sel", [2, 128], F32, p0)
        nmix = sb("nmix", [128, 8], F32, p0)
        colsb = sb("colsb", [128, 16, 2], F32, p0)
        pm = [ps("pm%d" % i, [2, 512], F32, p0) for i in range(2)]
        pcol = ps("pcol", [128, 16, 2], F32, p0)
        pbc = [ps("pbc%d" % i, [128, 512], F32, p0) for i in range(2)]

        S.dma('sp', cs_f[:], cT, writes=['cs_f'])
        S.dma('sp', brow[0:1, :], b_ada, writes=['brow0'])
        S.dma('sp', brow[1:2, :], b_ada, writes=['brow1'])
        S.dma('sp', nmix[:], nmixT, writes=['nmix'])
        S.dma('sp', bc_mul2[:], nffn.to_broadcast([128, D]), writes=['bc_mul2'])
        for dc in range(8):
            S.dma('pool', wada[:, dc, :].rearrange("p (a b) -> p a b", b=2048),
                  w_ada[dc * 128:(dc + 1) * 128, :].rearrange("p (a b) -> p a b", b=2048),
                  writes=[('wada', dc)])
        S.op('act', lambda e: e.activation(out=cs[:], in_=cs_f[:], func=AF.Silu), reads=['cs_f'], writes=['cs'])
        S.op('dve', lambda e: e.memset(sel[:], 0.0), writes=['sel'])
        S.op('dve', lambda e: e.memset(sel[0:1, :], 1.0), writes=['sel'])
        for cc in range(12):
            pt = pm[cc % 2]
            for dc in range(8):
                S.op('pe', lambda e: e.matmul(pt[:], lhsT=cs[:, dc, :], rhs=wada[:, dc, cc * 512:(cc + 1) * 512],
                                              start=(dc == 0), stop=(dc == 7)),
                     reads=['cs', ('wada', dc)], writes=[('pm', cc % 2)])
            S.op('dve', lambda e: e.tensor_tensor(out=modrow[:, cc * 512:(cc + 1) * 512], in0=pt[:],
                                                  in1=brow[:, cc * 512:(cc + 1) * 512], op=ALU.add),
                 reads=[('pm', cc % 2), 'brow0', 'brow1'], writes=[('modrow', cc)])
        for i in range(16):
            S.op('pe', lambda e: e.transpose(out=pcol[:, i, :], in_=modrow[0:2, i * 128:(i + 1) * 128],
                                             identity=identf[0:2, 0:2]),
                 reads=[('modrow', i // 4), 'identf'], writes=['pcol'])
        S.op('dve', lambda e: e.tensor_copy(out=colsb[:], in_=pcol[:]), reads=['pcol'], writes=['colsb'])
        S.op('dve', lambda e: e.tensor_copy(out=add1[:], in_=colsb[:, 0:8, :]), reads=['colsb'], writes=['add1'])
        S.op('dve', lambda e: e.tensor_scalar(out=colsb[:, 8:16, :], in0=colsb[:, 8:16, :], scalar1=1.0, scalar2=None,
                                              op0=ALU.add), reads=['colsb'], writes=['colsb'])
        S.op('dve', lambda e: e.tensor_tensor(out=mul1[:], in0=colsb[:, 8:16, :],
                                              in1=nmix[:].unsqueeze(2).to_broadcast([128, 8, 2]), op=ALU.mult),
             reads=['colsb', 'nmix'], writes=['mul1'])

        def bcast(dst, col0, mode, dkey):
            for h in range(2):
                pt = pbc[h]
                S.op('pe', lambda e: e.matmul(pt[:], lhsT=sel[:], rhs=modrow[:, col0 + h * 512: col0 + (h + 1) * 512],
                                              start=True, stop=True),
                     reads=['sel'] + [('modrow', c) for c in range(12)], writes=[('pbc', h)])
                dsl = dst[:, h * 512:(h + 1) * 512]
                if mode == 'copy':
                    S.op('dve', lambda e: e.tensor_copy(out=dsl, in_=pt[:]), reads=[('pbc', h)], writes=[dkey])
                else:
                    S.op('dve', lambda e: e.scalar_tensor_tensor(out=dsl, in0=pt[:], scalar=1.0, in1=dsl,
                                                                 op0=ALU.add, op1=ALU.mult),
                         reads=[('pbc', h), dkey], writes=[dkey])
        bcast(bc_g1, 2048, 'copy', 'bc_g1')
        bcast(bc_add2, 3072, 'copy', 'bc_add2')
        bcast(bc_mul2, 4096, 'mul1p', 'bc_mul2')
        bcast(bc_g2, 5120, 'copy', 'bc_g2')
        if 'mod' in dbg:
            dbg_out['mod'] = dout("dbg_mod", [2, 6 * D])
            S.dma('sp', dbg_out['mod'], modrow[:], reads=[('modrow', c) for c in range(12)])
            dbg_out['mul1'] = dout("dbg_mul1", [128, 8, 2])
            S.dma('sp', dbg_out['mul1'], mul1[:], reads=['mul1'])
            dbg_out['bcm2'] = dout("dbg_bcm2", [128, D])
            S.dma('sp', dbg_out['bcm2'], bc_mul2[:], reads=['bc_mul2'])
        S.barrier()

    if stop_after <= 0:
        return finish(nc, S, es, out, dbg_out)

    with ExitStack() as p1:
        win = sb("win", [128, 8, INCOLS], BF16, p1)
        xt = [sb("xt%d" % i, [128, D], F32, p1) for i in range(2)]
        junk = sb("junk", [128, D], BF16, p1)
        xn = [sb("xn%d" % i, [128, D], BF16, p1) for i in range(4)]
        ss = [sb("ss%d" % i, [128, 1], F32, p1) for i in range(4)]
        rstd = [sb("rstd%d" % i, [128, 1], F32, p1) for i in range(4)]
        hT = [sb("hT%d" % i, [128, 8, 512], BF16, p1) for i in range(2)]
        stg = [sb("stg%d" % i, [128, 512], BF16, p1) for i in range(4)]
        sq = [sb("sq%d" % i, [128, 512], BF16, p1) for i in range(2)]
        qk32 = [sb("qk32_%d" % i, [128, 512], F32, p1) for i in range(2)]
        rs = [sb("rs%d" % i, [128, 512], F32, p1) for i in range(2)]
        vst = [sb("vst%d" % i, [128, 8, 65], BF16, p1) for i in range(2)]
        bones = sb("bones", [128, 128], BF16, p1)
        gq = sb("gq", [128, 2], F32, p1)
        pT = [ps("pT%d" % i, [128, 8, 128], BF16, p1) for i in range(2)]
        pz = [ps("pz%d" % i, [128, 512], F32, p1) for i in range(4)]
        pms = [ps("pms%d" % i, [128, 512], F32, p1) for i in range(2)]

        for dc in range(8):
            S.dma('pool', win[:, dc, :].rearrange("p (a b) -> p a b", b=2048),
                  w_in[dc * 128:(dc + 1) * 128, :].rearrange("p (a b) -> p a b", b=2048),
                  writes=[('win', dc)])
        S.dma('sp', gq[:], qkg, writes=['gq'])
        S.op('dve', lambda e: e.tensor_scalar(out=gq[:, 0:1], in0=gq[:, 0:1], scalar1=0.125, scalar2=None, op0=ALU.mult), reads=['gq'], writes=['gq'])
        for i in range(2):
            S.op('dve', lambda e: e.memset(vst[i][:], 1.0), writes=[('vst', i)])
        S.op('dve', lambda e: e.memset(bones[:], 0.0), writes=['bones'])
        S.op('dve', lambda e: e.memset(bones[0:64, 0:64], 1.0 / 64), writes=['bones'])
        S.op('dve', lambda e: e.memset(bones[64:128, 64:128], 1.0 / 64), writes=['bones'])

        nblk = (NEXT + 511) // 512
        ctr = {'tile': 0, 'tpe': 0, 'stg': 0, 'z': 0, 'v': 0}

        def blk_info(b):
            if b == 0:
                return 0, 256
            return 256 + (b - 1) * 512, 512

        def head_pre(b):
            t0, nt = blk_info(b)
            for ti in range(nt // 128):
                i2 = ctr['tile'] % 2
                i4 = ctr['tile'] % 4
                ctr['tile'] += 1
                r0 = t0 + ti * 128
                S.dma('sp', xt[i2][:], xcat[r0:r0 + 128, :], writes=[('xt', i2)])
                S.op('act', lambda e: e.activation(out=junk[:], in_=xt[i2][:], func=AF.Square, accum_out=ss[i4][:]),
                     reads=[('xt', i2)], writes=['junk', ('ss', i4)])
                S.op('act', lambda e: e.activation(out=rstd[i4][:], in_=ss[i4][:], func=AF.Ln, scale=1.0 / D, bias=epsc[:, 0:1]),
                     reads=[('ss', i4), 'epsc'], writes=[('rstd', i4)])
                S.op('act', lambda e: e.activation(out=rstd[i4][:], in_=rstd[i4][:], func=AF.Exp, scale=-0.5),
                     reads=[('rstd', i4)], writes=[('rstd', i4)])
                S.op('act', lambda e: e.activation(out=xn[i4][:], in_=xt[i2][:], func=AF.Copy, scale=rstd[i4][:, 0:1]),
                     reads=[('xt', i2), ('rstd', i4)], writes=[('xn', i4)])

        def head_pe(b):
            t0, nt = blk_info(b)
            j = 1 if b == 0 else 0
            hb = hT[b % 2]
            hkey = ('hT', b % 2)
            for ti in range(nt // 128):
                i2 = ctr['tpe'] % 2
                i4 = ctr['tpe'] % 4
                ctr['tpe'] += 1
                for dc in range(8):
                    S.op('pe', lambda e: e.transpose(out=pT[i2][:, dc, :], in_=xn[i4][:, dc * 128:(dc + 1) * 128],
                                                     identity=identb[:]),
                         reads=[('xn', i4), 'identb'], writes=[('pT', i2)])
                for dc in range(8):
                    S.op('dve', lambda e: e.tensor_scalar(out=hb[:, dc, ti * 128:(ti + 1) * 128], in0=pT[i2][:, dc, :],
                                                          scalar1=mul1[:, dc, j:j + 1], scalar2=add1[:, dc, j:j + 1],
                                                          op0=ALU.mult, op1=ALU.add),
                         reads=[('pT', i2), 'mul1', 'add1'], writes=[hkey])

        pend_qk = []

        def chunk(b, cc):
            t0, nt = blk_info(b)
            is_ctx = (b == 0)
            hb = hT[b % 2]
            hkey = ('hT', b % 2)
            zi = ctr['z'] % 4
            ctr['z'] += 1
            pzt = pz[zi]
            for dc in range(8):
                S.op('pe', lambda e: e.matmul(pzt[:, 0:nt], lhsT=win[:, dc, cc * 128:(cc + 1) * 128], rhs=hb[:, dc, 0:nt],
                                              start=(dc == 0), stop=(dc == 7)),
                     reads=[('win', dc), hkey], writes=[('pz', zi)], sig=(dc == 7))
            si = ctr['stg'] % 4
            ctr['stg'] += 1
            st = stg[si]
            skey = ('stg', si)
            if not (4 <= cc < 12):
                while pend_qk:
                    pend_qk.pop(0)()
            if cc < 4:
                S.op('act', lambda e: e.activation(out=st[:, 0:nt], in_=pzt[:, 0:nt], func=AF.Copy),
                     reads=[('pz', zi)], writes=[skey])
                S.dma('sp', uT_d[cc * 128:(cc + 1) * 128, t0:t0 + nt], st[:, 0:nt], reads=[skey], writes=['uT_d'])
                if is_ctx:
                    S.dma('sp', uT_d[cc * 128:(cc + 1) * 128, NEXT:NEXT + nt], st[:, 0:nt], reads=[skey], writes=['uT_d'])
            elif cc < 12:
                isq = cc < 8
                qi = ctr['z'] % 2
                S.op('act', lambda e: e.activation(out=sq[qi][:, 0:nt], in_=pzt[:, 0:nt], func=AF.Square),
                     reads=[('pz', zi)], writes=[('sq', qi)])
                while pend_qk:
                    pend_qk.pop(0)()

                def tail(qi=qi, zi=zi, pzt=pzt, st=st, skey=skey, isq=isq, cc=cc, t0=t0, nt=nt):
                    S.op('pe', lambda e: e.matmul(pms[qi][:, 0:nt], lhsT=bones[:], rhs=sq[qi][:, 0:nt], start=True, stop=True),
                         reads=['bones', ('sq', qi)], writes=[('pms', qi)])
                    S.op('act', lambda e: e.activation(out=rs[qi][:, 0:nt], in_=pms[qi][:, 0:nt], func=AF.Ln, bias=epsc[:, 0:1]),
                         reads=[('pms', qi), 'epsc'], writes=[('rs', qi)])
                    S.op('act', lambda e: e.activation(out=rs[qi][:, 0:nt], in_=rs[qi][:, 0:nt], func=AF.Exp, scale=-0.5),
                         reads=[('rs', qi)], writes=[('rs', qi)])
                    gcol = gq[:, 0:1] if isq else gq[:, 1:2]
                    S.op('dve', lambda e: e.scalar_tensor_tensor(out=st[:, 0:nt], in0=pzt[:, 0:nt], scalar=gcol,
                                                                 in1=rs[qi][:, 0:nt], op0=ALU.mult, op1=ALU.mult),
                         reads=[('pz', zi), ('rs', qi), 'gq'], writes=[skey])
                    if isq:
                        S.dma('sp', qT_d[(cc - 4) * 128:(cc - 3) * 128, t0 - NCTX:t0 - NCTX + nt], st[:, 0:nt],
                              reads=[skey], writes=['qT_d'])
                    else:
                        S.dma('sp', kT_d[(cc - 8) * 128:(cc - 7) * 128, t0:t0 + nt], st[:, 0:nt], reads=[skey], writes=['kT_d'])
                pend_qk.append(tail)
            else:
                S.op('act', lambda e: e.activation(out=st[:, 0:nt], in_=pzt[:, 0:nt], func=AF.Sigmoid),
                     reads=[('pz', zi)], writes=[skey])
                if cc < 24:
                    dst = sgaT_d[(cc - 16) * 128:(cc - 15) * 128, t0 - NCTX:t0 - NCTX + nt]
                    dk = 'sgaT_d'
                else:
                    dst = sgbT_d[(cc - 24) * 128:(cc - 23) * 128, t0 - NCTX:t0 - NCTX + nt]
                    dk = 'sgbT_d'
                S.dma('sp', dst, st[:, 0:nt], reads=[skey], writes=[dk])

        def vpart(b):
            t0, nt = blk_info(b)
            hb = hT[b % 2]
            hkey = ('hT', b % 2)
            for ti in range(nt // 128):
                zi = ctr['z'] % 4
                ctr['z'] += 1
                pzt = pz[zi]
                for dc in range(8):
                    S.op('pe', lambda e: e.matmul(pzt[:], lhsT=hb[:, dc, ti * 128:(ti + 1) * 128], rhs=win[:, dc, 1536:2048],
                                                  start=(dc == 0), stop=(dc == 7)),
                         reads=[('win', dc), hkey], writes=[('pz', zi)], sig=(dc == 7))
                while pend_qk:
                    pend_qk.pop(0)()
                vi = ctr['v'] % 2
                ctr['v'] += 1
                S.op('act', lambda e: e.activation(out=vst[vi][:, :, 0:64], in_=pzt[:].rearrange("p (h d) -> p h d", d=64), func=AF.Copy),
                     reads=[('pz', zi)], writes=[('vst', vi)])
                S.dma('sp', v_d[t0 + ti * 128:t0 + (ti + 1) * 128], vst[vi][:], reads=[('vst', vi)], writes=['v_d'])

        head_pre(0)
        head_pe(0)
        for b in range(nblk_lim):
            if b == 0:
                ccs = list(range(0, 4)) + list(range(8, 12))
            else:
                ccs = list(range(0, 12)) + list(range(16, 32))
            n1, n2 = len(ccs) // 4, (3 * len(ccs)) // 4
            for cc in ccs[:n1]:
                chunk(b, cc)
            if b + 1 < nblk_lim:
                head_pre(b + 1)
            for cc in ccs[n1:n2]:
                chunk(b, cc)
            if b + 1 < nblk_lim:
                head_pe(b + 1)
            for cc in ccs[n2:]:
                chunk(b, cc)
            vpart(b)
        S.barrier()
    if 'p1' in dbg:
        for nm, t in (('uT', uT_d), ('qT', qT_d), ('kT', kT_d), ('sgaT', sgaT_d), ('sgbT', sgbT_d)):
            dbg_out[nm] = dout("dbg_" + nm, list(t.shape), BF16)
            for r0 in range(0, t.shape[0], 128):
                S.dma('sp', dbg_out[nm][r0:r0 + 128, :], t[r0:r0 + 128, :], reads=[])
    if stop_after <= 1:
        return finish(nc, S, es, out, dbg_out)
    yaT_d = dscr("yaT_d", [512, NLAT], BF16)
    r = 'ok' if skip_s5 else s5_phase(nc, S, es, sb, ps, din, dscr, dout, dbg, dbg_out, identf, identb, uT_d, yaT_d)
    if r == 'stop' or stop_after <= 2:
        return finish(nc, S, es, out, dbg_out)
    ybT_d = dscr("ybT_d", [512, NLAT], BF16)
    na_phase(nc, S, es, sb, ps, din, dscr, dout, dbg, dbg_out, identb, qT_d, kT_d, v_d, ybT_d)
    if stop_after <= 3:
        return finish(nc, S, es, out, dbg_out)
    h2_d = dscr("h2_d", [NLAT, D], BF16)
    aff_d = dscr("aff_d", [NLAT, 16], F32)
    affT_d = dscr("affT_d", [16, NLAT], F32)
    merge_phase(nc, S, es, sb, ps, din, dscr, dout, dbg, dbg_out, identf, identb, epsc, bc_g1, bc_mul2, bc_add2,
                xcat, out, yaT_d, ybT_d, sgaT_d, sgbT_d, h2_d, aff_d, affT_d, nblocks=mg_blocks)
    if stop_after <= 4:
        return finish(nc, S, es, out, dbg_out)
    moe_phase(nc, S, es, sb, ps, din, dscr, dout, dbg, dbg_out, identf, identb, bc_g2, out, h2_d, aff_d, affT_d, nexp=nexp)
    return finish(nc, S, es, out, dbg_out)


def finish(nc, S, es, out, dbg_out):
    S.barrier(['sp'])
    print("instructions", S.nins, "waits", S.nwaits)
    es.close()
    return nc, dbg_out


def host_inputs(inp, b):
    f = np.float32
    xcat = np.concatenate([inp['ctx'][b], inp['x'][b]], axis=0).astype(f)
    cT = np.stack([inp['c'][b].reshape(8, 128).T, inp['c_ctx'].reshape(8, 128).T], axis=-1).astype(f)
    qkg = np.stack([np.tile(inp['q_norm'][0], 2), np.tile(inp['k_norm'][0], 2)], axis=-1).astype(f)
    m = {
        'xcat': np.ascontiguousarray(xcat),
        'cT': np.ascontiguousarray(cT),
        'w_ada': np.ascontiguousarray(inp['w_ada'][0]),
        'b_ada': np.ascontiguousarray(inp['b_ada'][0][None, :]),
        'nmixT': np.ascontiguousarray(inp['norm_mix'][0].reshape(8, 128).T),
        'nffn': np.ascontiguousarray(inp['norm_ffn'][0][None, :]),
        'w_in': np.ascontiguousarray(inp['w_in'][0]),
        'ident': np.eye(128, dtype=f),
        'qkg': np.ascontiguousarray(qkg),
    }
    f32c = lambda a: np.ascontiguousarray(a.astype(f))
    lr = inp['ssm_lam_re'][0].reshape(2, 16, 2, 64)
    m['lamre_l'] = f32c(lr.transpose(2, 3, 0, 1).reshape(128, 32))
    li = inp['ssm_lam_im'][0].reshape(2, 16, 2, 64)
    m['lamim_l'] = f32c(li.transpose(2, 3, 0, 1).reshape(128, 32))
    ld = inp['ssm_log_dt'][0].reshape(2, 16, 2)
    m['logdt_l'] = f32c(np.broadcast_to(ld.transpose(2, 0, 1)[:, None, :, :], (2, 64, 2, 16)).reshape(128, 32))
    for nm, key in (('Bre_c', 'ssm_b_re'), ('Bim_c', 'ssm_b_im')):
        bb = inp[key][0].reshape(2, 16, 2, 64, 16)
        m[nm] = f32c(bb.transpose(2, 3, 0, 1, 4).reshape(128, 32, 16))
    for nm, key in (('Cre_c', 'ssm_c_re'), ('Cim_c', 'ssm_c_im')):
        cc = inp[key][0].reshape(2, 16, 2, 16, 64)
        m[nm] = f32c(cc.transpose(2, 4, 0, 1, 3).reshape(128, 32, 16))
    m['dskipT'] = f32c(inp['ssm_d'][0].reshape(4, 128).T)
    m['w_glu'] = f32c(inp['w_glu'][0])
    m['bgluT'] = f32c(inp['b_glu'][0].reshape(4, 128).T)
    m.update(na_host(inp))
    m['w_proj_a'] = f32c(inp['w_proj_a'][0]); m['w_proj_b'] = f32c(inp['w_proj_b'][0]); m['w_out'] = f32c(inp['w_out'][0])
    m.update(moe_host(inp))
    m['w_routerT'] = f32c(inp['w_router'][0].reshape(8, 128, 16).transpose(1, 0, 2))
    return m


_NC_CACHE = {}


def kernel(**inputs):
    inp = {k_: np.asarray(v_) for k_, v_ in inputs.items()}
    if 'nc' not in _NC_CACHE:
        nc, _ = build()
        _NC_CACHE['nc'] = nc
    nc = _NC_CACHE['nc']
    nb = inp['x'].shape[0]
    in_maps = [host_inputs(inp, b) for b in range(nb)]
    res = run_bass_kernel_spmd(nc, in_maps, core_ids=list(range(nb)))
    outs = [np.asarray(res.results[b]["out"], dtype=np.float32) for b in range(nb)]
    return np.stack(outs, axis=0)
```

```python
import numpy as np
from contextlib import ExitStack
import concourse.bass as bass
import concourse.mybir as mybir
from concourse.bass_utils import run_bass_kernel_spmd

F32 = mybir.dt.float32
BF16 = mybir.dt.bfloat16
I32 = mybir.dt.int32
U32 = mybir.dt.uint32
ALU = mybir.AluOpType
AF = mybir.ActivationFunctionType
AX = mybir.AxisListType
STRICT = False


class Sched:
    def __init__(self, nc, es, n_dma_sems=24):
        self.nc = nc
        self.e = {'pe': nc.tensor, 'act': nc.scalar, 'dve': nc.vector, 'pool': nc.gpsimd, 'sp': nc.sync}
        self.sems = {}
        self.cnt = {}
        for k in self.e:
            self.sems[k] = es.enter_context(nc.semaphore("sem_" + k))
            self.cnt[k] = 0
        self.nd = n_dma_sems
        self.qsems = {}
        for q in ('sp', 'pool', 'act'):
            n = n_dma_sems if q != 'act' else 16
            self.qsems[q] = []
            for i in range(n):
                sk = 'd_%s%d' % (q, i)
                self.sems[sk] = es.enter_context(nc.semaphore("dsem_%s%d" % (q, i)))
                self.cnt[sk] = 0
                self.qsems[q].append(sk)
        self.qrr = {'sp': 0, 'pool': 0, 'act': 0}
        self.drr = 0
        self.waited = {}
        self.lastw = {}
        self.readers = {}
        self.nwaits = 0
        self.nins = 0

    def _wait(self, eng, tok):
        sk, val, src = tok
        if self.waited.get((eng, sk), 0) >= val:
            return
        self.e[eng].wait_ge(self.sems[sk], val)
        self.waited[(eng, sk)] = val
        self.nwaits += 1

    def _deps(self, eng, reads, writes):
        for k in reads:
            t = self.lastw.get(k)
            if t is not None:
                self._wait(eng, t)
        for k in writes:
            t = self.lastw.get(k)
            if t is not None and (t[2] != eng or (STRICT and eng != 'pe')):
                self._wait(eng, t)
            for sk, (val, src) in self.readers.get(k, {}).items():
                if src != eng or (STRICT and eng != 'pe'):
                    self._wait(eng, (sk, val, src))

    def _record(self, tok, reads, writes):
        sk, val, src = tok
        for k in reads:
            self.readers.setdefault(k, {})[sk] = (val, src)
        for k in writes:
            self.lastw[k] = tok
            self.readers[k] = {}

    def op(self, eng, fn, reads=(), writes=(), sig=True):
        self._deps(eng, reads, writes)
        ins = fn(self.e[eng])
        if sig:
            self.cnt[eng] += 1
            ins.then_inc(self.sems[eng], 1)
            tok = (eng, self.cnt[eng], eng)
        else:
            tok = (eng, self.cnt[eng] + 1, eng)
        self._record(tok, reads, writes)
        self.nins += 1
        return tok

    def _dma_common(self, q, fn, reads, writes):
        self._deps(q, reads, writes)
        i = self.qrr[q]
        self.qrr[q] = (i + 1) % len(self.qsems[q])
        sk = self.qsems[q][i]
        if self.cnt[sk] > 0:
            self._wait(q, (sk, self.cnt[sk], None))
        ins = fn(self.e[q])
        self.cnt[sk] += 16
        ins.then_inc(self.sems[sk], 16)
        tok = (sk, self.cnt[sk], None)
        self._record(tok, reads, writes)
        self.nins += 1
        return tok

    def dma(self, q, out, in_, reads=(), writes=(), **kw):
        return self._dma_common(q, lambda e: e.dma_start(out=out, in_=in_, **kw), reads, writes)

    def custom_dma(self, q, fn, reads=(), writes=()):
        return self._dma_common(q, fn, reads, writes)

    def barrier(self, engines=None):
        for e in (engines or self.e):
            for sk, c in self.cnt.items():
                if c > 0:
                    self._wait(e, (sk, c, None))
        self.lastw = {}
        self.readers = {}

PI = float(np.pi)


def kn(t):
    n = t.name
    return n[2:] if n.startswith('s_') else n


def s5_phase(nc, S, es, sb, ps, din, dscr, dout, dbg, dbg_out, identf, identb, uT_d, yaT_d, nbanks=16):
    NEXT, NE2 = 8448, 8704
    lre_in = din("lamre_l", [128, 32]); lim_in = din("lamim_l", [128, 32]); ldt_in = din("logdt_l", [128, 32])
    Bre_in = din("Bre_c", [128, 32, 16]); Bim_in = din("Bim_c", [128, 32, 16])
    Cre_in = din("Cre_c", [128, 32, 16]); Cim_in = din("Cim_c", [128, 32, 16])
    dsk_in = din("dskipT", [128, 4]); wglu_in = din("w_glu", [512, 512]); bglu_in = din("bgluT", [128, 4])
    E_d = dscr("E_d", [128, 2, 4, 16, 2, 2, 128], BF16)
    G_d = dscr("G_d", [128, 64, 640], BF16)
    H_d = dscr("H_d", [2, 128, 16, 2, 544], BF16)

    with ExitStack() as s5:
        Kt_sb = sb("s_Kt_sb", [128, 128, 128], BF16, s5)
        aL = sb("s_aL", [128, 32], F32, s5); bL = sb("s_bL", [128, 32], F32, s5)
        diagD = sb("s_diagD", [128, 4, 128], BF16, s5)
        with ExitStack() as pa:
            def t32(name):
                return sb(name, [128, 32], F32, pa)
            lre, lim, ldt, dt, a, b = (t32(n) for n in ("lre", "lim", "ldt", "dt", "a_", "b_"))
            u1, u2, u3, u4, c16, s16, wre, wim = (t32(n) for n in ("u1", "u2", "u3", "u4", "c16", "s16", "wre", "wim"))
            Bcr = sb("s_Bcr", [128, 32, 16], F32, pa); Bci = sb("s_Bci", [128, 32, 16], F32, pa)
            Dr = sb("s_Dr", [128, 32, 32], F32, pa); Di = sb("s_Di", [128, 32, 32], F32, pa)
            Dr2 = sb("s_Dr2", [128, 32, 32], F32, pa); Di2 = sb("s_Di2", [128, 32, 32], F32, pa)
            Gr = sb("s_Gr", [128, 32, 32], F32, pa); Gi = sb("s_Gi", [128, 32, 32], F32, pa)
            T1 = sb("s_T1", [128, 32, 32], F32, pa); T2 = sb("s_T2", [128, 32, 32], F32, pa)
            T3 = sb("s_T3", [128, 32, 32], F32, pa); T4 = sb("s_T4", [128, 32, 32], F32, pa)
            P1 = sb("s_P1", [128, 32, 32], F32, pa); P2 = sb("s_P2", [128, 32, 32], F32, pa)
            P3 = sb("s_P3", [128, 32, 32], F32, pa); P4 = sb("s_P4", [128, 32, 32], F32, pa)
            Cbr = sb("s_Cbr", [128, 32, 32], BF16, pa); Cbni = sb("s_Cbni", [128, 32, 32], BF16, pa)
            Dbr = sb("s_Dbr", [128, 8, 160], BF16, pa); Dbi = sb("s_Dbi", [128, 8, 160], BF16, pa)
            Gbr = sb("s_Gbr", [128, 8, 160], BF16, pa); Gbni = sb("s_Gbni", [128, 8, 160], BF16, pa)
            Es = [sb("s_Es%d" % i, [128, 2, 4, 2, 2, 128], BF16, pa) for i in range(2)]
            m3 = sb("s_m3", [128, 1], F32, pa)
            dsk = sb("s_dsk", [128, 4], F32, pa)
            pk = [ps("s_pk%d" % i, [128, 128], F32, pa) for i in range(2)]
            pE = [ps("s_pE%d" % i, [128, 128], F32, pa) for i in range(4)]

            S.dma('sp', lre[:], lre_in, writes=['lre']); S.dma('sp', lim[:], lim_in, writes=['lim'])
            S.dma('sp', ldt[:], ldt_in, writes=['ldt']); S.dma('sp', dsk[:], dsk_in, writes=['dsk'])
            for X in (Dr, Di, Gr, Gi, Dr2, Di2):
                S.op('dve', lambda e: e.memset(X[:], 0.0), writes=[kn(X)])
            for X in (Dbr, Dbi, Gbr, Gbni):
                S.op('pool', lambda e: e.memset(X[:], 0.0), writes=[kn(X)])
            S.op('pool', lambda e: e.memset(Kt_sb[:], 0.0), writes=['Kt_sb'])
            for i in range(2):
                S.op('pool', lambda e: e.memset(Es[i][:], 0.0), writes=[('Es', i)])
            S.op('dve', lambda e: e.memset(m3[:], 1.0), writes=['m3'])
            S.op('dve', lambda e: e.memset(m3[64:96, :], 0.0), writes=['m3'])
            S.dma('sp', Bcr[:], Bre_in, writes=['Bcr']); S.dma('sp', Bci[:], Bim_in, writes=['Bci'])
            for h in range(2):
                S.dma('sp', Gr[64 * h:64 * h + 64, :, 16 * h:16 * h + 16], Cre_in[64 * h:64 * h + 64], reads=[], writes=['Gr'])
                S.dma('sp', Gi[64 * h:64 * h + 64, :, 16 * h:16 * h + 16], Cim_in[64 * h:64 * h + 64], reads=[], writes=['Gi'])
            for q in range(4):
                S.op('dve', lambda e: e.tensor_scalar(out=diagD[:, q, :], in0=identf[:], scalar1=dsk[:, q:q + 1], scalar2=None,
                                                      op0=ALU.mult), reads=['identf', 'dsk'], writes=['diagD'])
            V = lambda e: e
            def tt(out, in0, in1, op, r, w, eng='dve'):
                S.op(eng, lambda e: e.tensor_tensor(out=out, in0=in0, in1=in1, op=op), reads=r, writes=w)
            S.op('act', lambda e: e.activation(out=dt[:], in_=ldt[:], func=AF.Exp), reads=['ldt'], writes=['dt'])
            tt(u1[:], lre[:], dt[:], ALU.mult, ['lre', 'dt'], ['u1'])
            S.op('act', lambda e: e.activation(out=u1[:], in_=u1[:], func=AF.Exp), reads=['u1'], writes=['u1'])
            tt(u2[:], lim[:], dt[:], ALU.mult, ['lim', 'dt'], ['u2'])
            S.op('act', lambda e: e.activation(out=s16[:], in_=u2[:], func=AF.Sin, scale=1.0 / 16), reads=['u2'], writes=['s16'])
            S.op('act', lambda e: e.activation(out=u3[:], in_=u2[:], func=AF.Sin, scale=1.0 / 32), reads=['u2'], writes=['u3'])
            tt(u3[:], u3[:], u3[:], ALU.mult, ['u3'], ['u3'])
            S.op('dve', lambda e: e.tensor_scalar(out=c16[:], in0=u3[:], scalar1=-2.0, scalar2=1.0, op0=ALU.mult, op1=ALU.add),
                 reads=['u3'], writes=['c16'])

            def csquare(cr, ci, n):
                for _ in range(n):
                    tt(u3[:], cr[:], cr[:], ALU.mult, [kn(cr)], ['u3'])
                    tt(u4[:], ci[:], ci[:], ALU.mult, [kn(ci)], ['u4'])
                    tt(ci[:], cr[:], ci[:], ALU.mult, [kn(cr), kn(ci)], [kn(ci)])
                    S.op('dve', lambda e: e.tensor_scalar(out=ci[:], in0=ci[:], scalar1=2.0, scalar2=None, op0=ALU.mult),
                         reads=[kn(ci)], writes=[kn(ci)])
                    tt(cr[:], u3[:], u4[:], ALU.subtract, ['u3', 'u4'], [kn(cr)])
            csquare(c16, s16, 4)
            tt(a[:], u1[:], c16[:], ALU.mult, ['u1', 'c16'], ['a_'])
            tt(b[:], u1[:], s16[:], ALU.mult, ['u1', 's16'], ['b_'])
            S.op('dve', lambda e: e.tensor_copy(out=aL[:], in_=c16[:]), reads=['c16'], writes=['aL'])
            S.op('dve', lambda e: e.tensor_copy(out=bL[:], in_=s16[:]), reads=['s16'], writes=['bL'])
            csquare(aL, bL, 4)
            tt(u3[:], lre[:], dt[:], ALU.mult, ['lre', 'dt'], ['u3'])
            S.op('act', lambda e: e.activation(out=u3[:], in_=u3[:], func=AF.Exp, scale=16.0), reads=['u3'], writes=['u3'])
            tt(aL[:], aL[:], u3[:], ALU.mult, ['aL', 'u3'], ['aL'])
            tt(bL[:], bL[:], u3[:], ALU.mult, ['bL', 'u3'], ['bL'])
            S.op('dve', lambda e: e.tensor_scalar(out=u1[:], in0=a[:], scalar1=-1.0, scalar2=None, op0=ALU.add), reads=['a_'], writes=['u1'])
            tt(u2[:], lre[:], lre[:], ALU.mult, ['lre'], ['u2'])
            tt(u3[:], lim[:], lim[:], ALU.mult, ['lim'], ['u3'])
            tt(u2[:], u2[:], u3[:], ALU.add, ['u2', 'u3'], ['u2'])
            S.op('dve', lambda e: e.reciprocal(out=u2[:], in_=u2[:]), reads=['u2'], writes=['u2'])
            tt(u3[:], u1[:], lre[:], ALU.mult, ['u1', 'lre'], ['u3'])
            tt(u4[:], b[:], lim[:], ALU.mult, ['b_', 'lim'], ['u4'])
            tt(u3[:], u3[:], u4[:], ALU.add, ['u3', 'u4'], ['u3'])
            tt(wre[:], u3[:], u2[:], ALU.mult, ['u3', 'u2'], ['wre'])
            tt(u3[:], b[:], lre[:], ALU.mult, ['b_', 'lre'], ['u3'])
            tt(u4[:], u1[:], lim[:], ALU.mult, ['u1', 'lim'], ['u4'])
            tt(u3[:], u3[:], u4[:], ALU.subtract, ['u3', 'u4'], ['u3'])
            tt(wim[:], u3[:], u2[:], ALU.mult, ['u3', 'u2'], ['wim'])
            bc16 = lambda t: t[:].unsqueeze(2).to_broadcast([128, 32, 16])
            Tc = [T1[:, :, 0:16], T2[:, :, 0:16], T3[:, :, 0:16], T4[:, :, 0:16]]
            tt(Tc[0], Bcr[:], bc16(wre), ALU.mult, ['Bcr', 'wre'], ['T1'])
            tt(Tc[1], Bci[:], bc16(wim), ALU.mult, ['Bci', 'wim'], ['T2'])
            tt(Tc[2], Bci[:], bc16(wre), ALU.mult, ['Bci', 'wre'], ['T3'])
            tt(Tc[3], Bcr[:], bc16(wim), ALU.mult, ['Bcr', 'wim'], ['T4'])
            for h in range(2):
                sl = slice(64 * h, 64 * h + 64)
                tt(Dr[sl, :, 16 * h:16 * h + 16], T1[sl, :, 0:16], T2[sl, :, 0:16], ALU.subtract, ['T1', 'T2'], ['Dr'])
                tt(Di[sl, :, 16 * h:16 * h + 16], T3[sl, :, 0:16], T4[sl, :, 0:16], ALU.add, ['T3', 'T4'], ['Di'])
            S.op('dve', lambda e: e.tensor_copy(out=Cbr[:], in_=Gr[:]), reads=['Gr'], writes=['Cbr'])
            S.op('dve', lambda e: e.tensor_scalar(out=Cbni[:], in0=Gi[:], scalar1=-1.0, scalar2=None, op0=ALU.mult),
                 reads=['Gi'], writes=['Cbni'])
            bc32 = lambda t: t[:].unsqueeze(2).to_broadcast([128, 32, 32])

            def cmul(Xr, Xi, eng, Yr=None, Yi=None):
                Yr = Yr or Xr; Yi = Yi or Xi
                A1, A2, A3, A4 = (T1, T2, T3, T4) if eng == 'dve' else (P1, P2, P3, P4)
                tt(A1[:], Xr[:], bc32(a), ALU.mult, [kn(Xr), 'a_'], [kn(A1)], eng)
                tt(A2[:], Xi[:], bc32(b), ALU.mult, [kn(Xi), 'b_'], [kn(A2)], eng)
                tt(A3[:], Xi[:], bc32(a), ALU.mult, [kn(Xi), 'a_'], [kn(A3)], eng)
                tt(A4[:], Xr[:], bc32(b), ALU.mult, [kn(Xr), 'b_'], [kn(A4)], eng)
                tt(Yr[:], A1[:], A2[:], ALU.subtract, [kn(A1), kn(A2)], [kn(Yr)], eng)
                tt(Yi[:], A3[:], A4[:], ALU.add, [kn(A3), kn(A4)], [kn(Yi)], eng)

            def to5(dst, src, scale, key, eng='dve'):
                d4 = dst[:].rearrange("p g (s c) -> p g s c", c=32)
                s4 = src[:].rearrange("p (g s) c -> p g s c", s=4)
                if eng == 'act':
                    S.op('act', lambda e: e.activation(out=d4[:, :, 0:3, :], in_=s4[:, :, 0:3, :], func=AF.Copy, scale=scale),
                         reads=[kn(src)], writes=[key])
                    S.op('act', lambda e: e.activation(out=d4[:, :, 4, :], in_=s4[:, :, 3, :], func=AF.Copy, scale=scale),
                         reads=[kn(src)], writes=[key])
                    return
                S.op(eng, lambda e: e.tensor_scalar(out=d4[:, :, 0:3, :], in0=s4[:, :, 0:3, :], scalar1=scale, scalar2=None, op0=ALU.mult),
                     reads=[kn(src)], writes=[key])
                S.op(eng, lambda e: e.tensor_scalar(out=d4[:, :, 4, :], in0=s4[:, :, 3, :], scalar1=scale, scalar2=None, op0=ALU.mult),
                     reads=[kn(src)], writes=[key])

            Dpairs = [(Dr, Di), (Dr2, Di2)]
            for j in range(16):
                Dr, Di = Dpairs[j % 2]
                to5(Dbr, Dr, 1.0, 'Dbr', 'act'); to5(Dbi, Di, 1.0, 'Dbi', 'act')
                if j < 15:
                    cmul(Dr, Di, 'dve', Dpairs[(j + 1) % 2][0], Dpairs[(j + 1) % 2][1])
                for d in range(2):
                    for q in range(4):
                        idx = (d * 4 + q) * 16 + j
                        pkt = pk[idx % 2]; pkk = ('pk', idx % 2)
                        for pl in range(4):
                            pair = d * 16 + q * 4 + pl
                            if pl < 3:
                                osl = pkt[32 * pl:32 * pl + 32, 32 * pl:32 * pl + 32]; csl = slice(32 * pl, 32 * pl + 32)
                            else:
                                osl = pkt[64:128, 96:128]; csl = slice(96, 160)
                            S.op('pe', lambda e: e.matmul(osl, lhsT=Dbr[:, d * 4 + q, csl], rhs=Cbr[:, pair, :], start=True, stop=False),
                                 reads=['Dbr', 'Cbr'], writes=[pkk])
                            S.op('pe', lambda e: e.matmul(osl, lhsT=Dbi[:, d * 4 + q, csl], rhs=Cbni[:, pair, :], start=False, stop=True),
                                 reads=['Dbi', 'Cbni'], writes=[pkk])
                        for pl in range(4):
                            if pl < 3:
                                rs_, cs_ = slice(32 * pl, 32 * pl + 32), slice(32 * pl, 32 * pl + 32)
                            else:
                                rs_, cs_ = slice(64, 128), slice(96, 128)
                            S.op('act', lambda e: e.activation(out=Kt_sb[rs_, idx, cs_], in_=pkt[rs_, cs_], func=AF.Copy),
                                 reads=[pkk], writes=['Kt_sb'])
                Et = Es[j % 2]; ek = ('Es', j % 2)
                n_e = 0
                for reim, X in ((0, Dr), (1, Di)):
                    for d in range(2):
                        for q in range(4):
                            pet = pE[n_e % 4]; pek = ('pE', n_e % 4); n_e += 1
                            src = X[:, d * 16 + q * 4:d * 16 + q * 4 + 4, :].rearrange("p a b -> p (a b)")
                            S.op('pe', lambda e: e.transpose(out=pet[:], in_=src, identity=identf[:]), reads=[kn(X), 'identf'], writes=[pek])
                            S.op('act', lambda e: e.activation(out=Et[:, d, q, reim, 0, :], in_=pet[:], func=AF.Copy), reads=[pek], writes=[ek])
                            S.op('dve', lambda e: e.tensor_scalar(out=Et[64:128, d, q, reim, 1, :], in0=pet[64:128, :], scalar1=m3[64:128, 0:1],
                                                                  scalar2=None, op0=ALU.mult), reads=[pek, 'm3'], writes=[ek])
                for d in range(2):
                    S.dma('sp', E_d[:, d, :, j, :, :, :], Et[:, d], reads=[ek], writes=['E_d'])
                cmul(Gr, Gi, 'pool')
                to5(Gbr, Gr, 1.0, 'Gbr'); to5(Gbni, Gi, -1.0, 'Gbni')
                for d in range(2):
                    S.dma('sp', G_d[:, (d * 16 + j) * 2 + 0, :], Gbr[:, d * 4:d * 4 + 4, :].rearrange("p a b -> p (a b)"), reads=['Gbr'], writes=['G_d'])
                    S.dma('sp', G_d[:, (d * 16 + j) * 2 + 1, :], Gbni[:, d * 4:d * 4 + 4, :].rearrange("p a b -> p (a b)"), reads=['Gbni'], writes=['G_d'])
            S.barrier()
        if 's5prep' in dbg:
            dbg_out['Kt'] = dout("dbg_Kt", [128, 128, 128], BF16)
            S.dma('sp', dbg_out['Kt'], Kt_sb[:], reads=['Kt_sb'])
            dbg_out['aL'] = dout("dbg_aL", [128, 2, 32])
            S.dma('sp', dbg_out['aL'][:, 0, :], aL[:], reads=['aL']); S.dma('sp', dbg_out['aL'][:, 1, :], bL[:], reads=['bL'])
            dbg_out['G'] = dout("dbg_G", [128, 64, 640], BF16)
            S.dma('sp', dbg_out['G'], G_d, reads=['G_d'])
            dbg_out['E'] = dout("dbg_E", [128, 2, 4, 16, 2, 2, 128], BF16)
            for d in range(2):
                S.dma('sp', dbg_out['E'][:, d], E_d[:, d], reads=['E_d'])
            return 'stop'
        return s5_main(nc, S, es, sb, ps, din, dscr, dout, dbg, dbg_out, identf, identb, uT_d, yaT_d, nbanks,
                       s5, Kt_sb, aL, bL, diagD, E_d, G_d, H_d, wglu_in, bglu_in)


def s5_main(nc, S, es, sb, ps, din, dscr, dout, dbg, dbg_out, identf, identb, uT_d, yaT_d, nbanks,
            s5, Kt_sb, aL, bL, diagD, E_d, G_d, H_d, wglu_in, bglu_in):
    NCH = 528
    with ExitStack() as pb:
        uTb = [sb("s_uTb%d" % i, [128, 2048], BF16, pb) for i in range(1)]
        uTc = [sb("s_uTc%d" % i, [128, 16, 128], BF16, pb) for i in range(2)]
        Esl = [sb("s_Esl%d" % i, [128, 16, 2, 2, 128], BF16, pb) for i in range(2)]
        Ssb = [[sb("s_Ssb%d_%d" % (d, i), [128, 16, 2, 128], F32, pb) for i in range(2)] for d in range(2)]
        hist = [sb("s_hist%d" % d, [128, 16, 2, 129], F32, pb) for d in range(2)]
        histb = [sb("s_histb%d" % d, [128, 16, 2, 128], BF16, pb) for d in range(2)]
        tPQ = [sb("s_tPQ%d" % d, [128, 16, 2, 2], F32, pb) for d in range(2)]
        ABt = [sb("s_ABt%d" % d, [128, 16, 2, 2], F32, pb) for d in range(2)]
        for d in range(2):
            S.op('dve', lambda e: e.tensor_copy(out=ABt[d][:, :, :, 0], in_=aL[:, d * 16:(d + 1) * 16].unsqueeze(2).to_broadcast([128, 16, 2])),
                 reads=['aL'], writes=[('ABt', d)])
            S.op('dve', lambda e: e.tensor_copy(out=ABt[d][:, :, :, 1], in_=bL[:, d * 16:(d + 1) * 16].unsqueeze(2).to_broadcast([128, 16, 2])),
                 reads=['bL'], writes=[('ABt', d)])
        pS = [ps("s_pS%d" % i, [128, 4, 128], F32, pb) for i in range(4)]
        for d in range(2):
            S.op('dve' if d == 0 else 'pool', lambda e: e.memset(hist[d][:], 0.0), writes=[('hist', d)])
        fblocks = [(0, 128), (128, 128), (256, 128), (384, 128), (512, 16)]
        bblocks = [(512, 16), (384, 128), (256, 128), (128, 128), (0, 128)]
        esl_ctr = 0
        ps_ctr = 0
        for step in range(5):
            for d in range(2):
                k0, nk = (fblocks if d == 0 else bblocks)[step]
                Sb = Ssb[d][step % 2]; skey = ('Ssb', d, step % 2)
                base = 0 if d == 0 else 256
                for q in range(4):
                    et = Esl[esl_ctr % 2]; ekey = ('Esl', esl_ctr % 2); esl_ctr += 1
                    S.dma('sp', et[:], E_d[:, d, q], reads=['E_d'], writes=[ekey])
                    ut0 = uTb[0]; u0key = ('uTb', 0)
                    S.dma('sp', ut0[:, 0:16 * nk], uT_d[q * 128:(q + 1) * 128, base + 16 * k0: base + 16 * k0 + 16 * nk], reads=['uT_d'], writes=[u0key])
                    ut = uTc[(esl_ctr - 1) % 2]; ukey = ('uTc', (esl_ctr - 1) % 2)
                    S.op('act', lambda e: e.activation(out=ut[:, :, 0:nk], in_=ut0[:, 0:16 * nk].rearrange("p (k i) -> p i k", i=16), func=AF.Copy),
                         reads=[u0key], writes=[ukey])
                    for pl in range(4):
                        pst = pS[ps_ctr % 4]; pskey = ('pS', ps_ctr % 4); ps_ctr += 1
                        if pl < 3:
                            rsl, alt = slice(32 * pl, 32 * pl + 32), 0
                        else:
                            rsl, alt = slice(64, 128), 1
                        for reim in range(2):
                            for i in range(16):
                                j = 15 - i if d == 0 else i
                                rhs = ut[rsl, i, 0:nk]
                                S.op('pe', lambda e: e.matmul(pst[:, reim, 0:nk], lhsT=et[rsl, j, reim, alt, :], rhs=rhs,
                                                              start=(i == 0), stop=(i == 15)),
                                     reads=[ekey, ukey], writes=[pskey])
                        S.op('act', lambda e: e.activation(out=Sb[:, q * 4 + pl, :, 0:nk], in_=pst[:, 0:2, 0:nk], func=AF.Copy),
                             reads=[pskey], writes=[skey])
            def chain_ops(d, kk):
                k0, nk = (fblocks if d == 0 else bblocks)[step]
                Sb = Ssb[d][step % 2]; skey = ('Ssb', d, step % 2)
                hk = ('hist', d)
                A2 = aL[:, d * 16:(d + 1) * 16].unsqueeze(2).to_broadcast([128, 16, 2])
                B2 = bL[:, d * 16:(d + 1) * 16].unsqueeze(2).to_broadcast([128, 16, 2])
                H = hist[d]
                if d == 0:
                    cprev, ccur, scol = kk, kk + 1, kk
                else:
                    cprev, ccur, scol = nk - kk, nk - 1 - kk, nk - 1 - kk
                prev = H[:, :, :, cprev]
                PQ = tPQ[d]
                eng = 'dve'
                pk = ('tPQ', d)
                return [
                    lambda: S.op(eng, lambda e: e.tensor_tensor(out=PQ[:], in0=prev.unsqueeze(3).to_broadcast([128, 16, 2, 2]), in1=ABt[d][:], op=ALU.mult),
                                 reads=[hk, ('ABt', d)], writes=[pk]),
                    lambda: S.op(eng, lambda e: e.tensor_tensor(out=PQ[:, :, :, 0], in0=PQ[:, :, :, 0], in1=Sb[:, :, :, scol], op=ALU.add),
                                 reads=[pk, skey], writes=[pk]),
                    lambda: S.op(eng, lambda e: e.tensor_tensor(out=H[:, :, 0, ccur], in0=PQ[:, :, 0, 0], in1=PQ[:, :, 1, 1], op=ALU.subtract),
                                 reads=[pk], writes=[hk]),
                    lambda: S.op(eng, lambda e: e.tensor_tensor(out=H[:, :, 1, ccur], in0=PQ[:, :, 1, 0], in1=PQ[:, :, 0, 1], op=ALU.add),
                                 reads=[pk], writes=[hk]),
                ]
            nks = [(fblocks if d == 0 else bblocks)[step][1] for d in range(2)]
            for kk in range(max(nks)):
                lists = [chain_ops(d, kk) for d in range(2) if kk < nks[d]]
                for oi in range(4):
                    for l in lists:
                        l[oi]()
            for d in range(2):
                eng = 'dve' if d == 0 else 'pool'
                k0, nk = (fblocks if d == 0 else bblocks)[step]
                hk = ('hist', d)
                H = hist[d]
                if d == 0:
                    S.op(eng, lambda e: e.tensor_copy(out=histb[d][:, :, :, 0:nk], in_=H[:, :, :, 1:nk + 1]), reads=[hk], writes=[('histb', d)])
                    S.op(eng, lambda e: e.tensor_copy(out=H[:, :, :, 0], in_=H[:, :, :, nk]), reads=[hk], writes=[hk])
                else:
                    S.op(eng, lambda e: e.tensor_copy(out=histb[d][:, :, :, 0:nk], in_=H[:, :, :, 0:nk]), reads=[hk], writes=[('histb', d)])
                    S.op(eng, lambda e: e.tensor_copy(out=H[:, :, :, 128], in_=H[:, :, :, 0]), reads=[hk], writes=[hk])
                S.dma('pool', H_d[d, :, :, :, k0:k0 + nk], histb[d][:, :, :, 0:nk], reads=[('histb', d)], writes=['H_d'])
        S.barrier()
    if 's5lvl3' in dbg:
        dbg_out['H'] = dout("dbg_H", [2, 128, 16, 2, 544], BF16)
        for d in range(2):
            S.dma('sp', dbg_out['H'][d], H_d[d], reads=['H_d'])
        return 'stop'

    with ExitStack() as pc:
        Gsb = sb("s_Gsb", [128, 64, 640], BF16, pc)
        wglu = sb("s_wglu", [128, 4, 512], BF16, pc)
        bglu = sb("s_bglu", [128, 4], F32, pc)
        ub = [sb("s_ub%d" % i, [128, 4, 544], BF16, pc) for i in range(2)]
        Hf = [sb("s_Hf%d" % i, [128, 16, 2, 32], BF16, pc) for i in range(2)]
        Hb = [sb("s_Hb%d" % i, [128, 16, 2, 32], BF16, pc) for i in range(2)]
        aT = [sb("s_aT%d" % i, [128, 4, 512], BF16, pc) for i in range(2)]
        sig = [sb("s_sig%d" % i, [128, 512], BF16, pc) for i in range(2)]
        yo = [sb("s_yo%d" % i, [128, 512], BF16, pc) for i in range(4)]
        py = [ps("s_py%d" % i, [128, 512], F32, pc) for i in range(2)]
        py4 = [ps("s_py4_%d" % i, [128, 16, 32], F32, pc) for i in range(2)]
        y4 = [sb("s_y4_%d" % i, [128, 512], F32, pc) for i in range(2)]
        ysum = [sb("s_ysum%d" % i, [128, 512], F32, pc) for i in range(2)]
        zlhs = sb("s_zlhs", [128, 128], BF16, pc)
        S.op('dve', lambda e: e.memset(zlhs[:], 0.0), writes=['zlhs'])
        pg = [ps("s_pg%d" % i, [128, 512], F32, pc) for i in range(2)]
        for c in range(4):
            S.dma('sp', Gsb[:, c * 16:(c + 1) * 16, :], G_d[:, c * 16:(c + 1) * 16, :], reads=['G_d'], writes=[('Gsb', c)])
        gkeys = [('Gsb', c) for c in range(4)]
        for q in range(4):
            S.dma('pool', wglu[:, q, :], wglu_in[q * 128:(q + 1) * 128, :], writes=['wglu'])
        S.dma('sp', bglu[:], bglu_in, writes=['bglu'])
        yo_ctr = 0
        for n in range(nbanks):
            i2 = n % 2
            e0 = 256 + 512 * n
            S.dma('sp', ub[i2][:], uT_d[:, e0 - 16:e0 + 528].rearrange("(q p) t -> p q t", p=128), reads=['uT_d'], writes=[('ub', i2)])
            kf = 16 + 32 * n
            kb = 32 * n
            S.dma('sp', Hf[i2][:], H_d[0, :, :, :, kf - 1:kf + 31], reads=['H_d'], writes=[('Hf', i2)])
            S.dma('sp', Hb[i2][:], H_d[1, :, :, :, kb + 1:kb + 33], reads=['H_d'], writes=[('Hb', i2)])
            for q in range(4):
                pyt = py[q % 2]; pk_ = ('py', q % 2)
                p4 = py4[q % 2]; p4k = ('py4', q % 2)
                pyv = pyt[:].rearrange("p (k j) -> p k j", j=16)
                uc = ub[i2][:, q, 16:528]
                ucv = uc.rearrange("p (k j) -> p k j", j=16)
                rd = [('ub', i2), 'Kt_sb', 'diagD']
                S.op('pe', lambda e: e.matmul(pyt[:], lhsT=diagD[:, q, :], rhs=uc, start=True, stop=False), reads=rd, writes=[pk_])
                S.op('pe', lambda e: e.matmul(pyt[:], lhsT=Kt_sb[:, (0 * 4 + q) * 16 + 0, :], rhs=uc, start=False, stop=False), reads=rd, writes=[pk_])
                for tau in range(1, 16):
                    S.op('pe', lambda e: e.matmul(pyv[:, :, tau:16], lhsT=Kt_sb[:, (0 * 4 + q) * 16 + tau, :], rhs=ucv[:, :, 0:16 - tau],
                                                  start=False, stop=False), reads=rd, writes=[pk_])
                S.op('pe', lambda e: e.matmul(pyt[:], lhsT=Kt_sb[:, (1 * 4 + q) * 16 + 0, :], rhs=uc, start=False, stop=False), reads=rd, writes=[pk_])
                for tau in range(1, 16):
                    S.op('pe', lambda e: e.matmul(pyv[:, :, 0:16 - tau], lhsT=Kt_sb[:, (1 * 4 + q) * 16 + tau, :], rhs=ucv[:, :, tau:16],
                                                  start=False, stop=(tau == 15)), reads=rd, writes=[pk_])
                S.op('pe', lambda e: e.matmul(p4[:].rearrange("p j k -> p (j k)"), lhsT=zlhs[:], rhs=uc, start=True, stop=False),
                     reads=['zlhs', ('ub', i2)], writes=[p4k])
                for pl in range(4):
                    pair = q * 4 + pl
                    for d in range(2):
                        Hx = Hf[i2] if d == 0 else Hb[i2]
                        hkey = ('Hf', i2) if d == 0 else ('Hb', i2)
                        for j in range(16):
                            m = j + 1 if d == 0 else 16 - j
                            if pl < 3:
                                osl = p4[32 * pl:32 * pl + 32, j, :]; csl = slice(q * 160 + 32 * pl, q * 160 + 32 * pl + 32)
                            else:
                                osl = p4[64:128, j, :]; csl = slice(q * 160 + 96, q * 160 + 160)
                            for reim in range(2):
                                last = (pl == 3 and d == 1 and j == 15 and reim == 1)
                                S.op('pe', lambda e: e.matmul(osl, lhsT=Gsb[:, (d * 16 + m - 1) * 2 + reim, csl], rhs=Hx[:, pair, reim, :],
                                                              start=False, stop=last), reads=gkeys + [hkey], writes=[p4k])
                S.op('act', lambda e: e.activation(out=y4[q % 2][:].rearrange("p (k j) -> p j k", j=16), in_=p4[:], func=AF.Copy),
                     reads=[p4k], writes=[('y4', q % 2)])
                S.op('dve', lambda e: e.tensor_tensor(out=ysum[q % 2][:], in0=pyt[:], in1=y4[q % 2][:], op=ALU.add),
                     reads=[pk_, ('y4', q % 2)], writes=[('ysum', q % 2)])
                S.op('act', lambda e: e.activation(out=aT[i2][:, q, :], in_=ysum[q % 2][:], func=AF.Gelu), reads=[('ysum', q % 2)], writes=[('aT', i2, q)])
            for oc in range(4):
                pgt = pg[oc % 2]; pgk = ('pg', oc % 2)
                for q in range(4):
                    S.op('pe', lambda e: e.matmul(pgt[:], lhsT=wglu[:, q, oc * 128:(oc + 1) * 128], rhs=aT[i2][:, q, :],
                                                  start=(q == 0), stop=(q == 3)), reads=['wglu', ('aT', i2, q)], writes=[pgk])
                sg = sig[oc % 2]; sgk = ('sig', oc % 2)
                S.op('act', lambda e: e.activation(out=sg[:], in_=pgt[:], func=AF.Sigmoid, bias=bglu[:, oc:oc + 1]),
                     reads=[pgk, 'bglu'], writes=[sgk])
                yt = yo[yo_ctr % 4]; yk = ('yo', yo_ctr % 4); yo_ctr += 1
                S.op('dve', lambda e: e.tensor_tensor(out=yt[:], in0=aT[i2][:, oc, :], in1=sg[:], op=ALU.mult),
                     reads=[('aT', i2, oc), sgk], writes=[yk])
                S.dma('pool', yaT_d[oc * 128:(oc + 1) * 128, 512 * n:512 * n + 512], yt[:], reads=[yk], writes=['yaT_d'])
        S.barrier()
    if 's5' in dbg:
        dbg_out['yaT'] = dout("dbg_yaT", [512, 8192], BF16)
        for r0 in range(0, 512, 128):
            S.dma('sp', dbg_out['yaT'][r0:r0 + 128, :], yaT_d[r0:r0 + 128, :], reads=['yaT_d'])
    return 'ok'

NEG = -30000.0


def na_phase(nc, S, es, sb, ps, din, dscr, dout, dbg, dbg_out, identb, qT_d, kT_d, v_d, ybT_d, nblocks=16):
    rpbT_in = din("rpbT", [128, 8 * 14 * 64])
    maskT_in = din("maskT", [128, 64])
    with ExitStack() as pa:
        Tb = sb("n_Tb", [128, 8, 14, 64], F32, pa)
        mk = sb("n_mk", [128, 64], F32, pa)
        kctx = sb("n_kctx", [128, 4, 256], BF16, pa)
        vctx = sb("n_vctx", [128, 2, 8, 65], BF16, pa)
        qb = [sb("n_qb%d" % i, [128, 4, 512], BF16, pa) for i in range(2)]
        kw = [sb("n_kw%d" % i, [128, 4, 960], BF16, pa) for i in range(2)]
        vw = [sb("n_vw%d" % i, [128, 15, 8, 65], BF16, pa) for i in range(2)]
        sS = [sb("n_sS%d" % i, [128, 4, 64], F32, pa) for i in range(3)]
        P = [sb("n_Pn%d" % i, [128, 6, 64], BF16, pa) for i in range(4)]
        rc = [sb("n_rc%d" % i, [128, 8], F32, pa) for i in range(2)]
        ybt = [sb("n_ybt%d" % i, [128, 8, 64], BF16, pa) for i in range(2)]
        ybT = [sb("n_ybT%d" % i, [128, 4, 512], BF16, pa) for i in range(2)]
        pS = [ps("n_pS%d" % i, [128, 6, 64], F32, pa) for i in range(3)]
        pO = [ps("n_pO%d" % i, [128, 4, 65], F32, pa) for i in range(4)]
        pT = ps("n_pTn", [128, 4, 128], BF16, pa)

        S.dma('sp', Tb[:].rearrange("p h a q -> p (h a q)"), rpbT_in, writes=['Tb'])
        S.dma('sp', mk[:], maskT_in, writes=['mk'])
        S.dma('sp', kctx[:], kT_d[:, 0:256].rearrange("(c p) t -> p c t", p=128), reads=['kT_d'], writes=['kctx'])
        S.dma('sp', vctx[:], v_d[0:256].rearrange("(m p) h d -> p m h d", p=128), reads=['v_d'], writes=['vctx'])
        S.op('dve', lambda e: e.tensor_tensor(out=Tb[:].rearrange("p h a q -> p (h a) q"), in0=Tb[:].rearrange("p h a q -> p (h a) q"),
                                              in1=mk[:].unsqueeze(1).to_broadcast([128, 112, 64]), op=ALU.add),
             reads=['Tb', 'mk'], writes=['Tb'])
        item = 0
        pend = []
        for b in range(nblocks):
            i2 = b % 2
            r0 = 8 * b
            lo = min(max(r0 - 4, 0), 120)
            hi = min(max(r0 + 7 - 4, 0), 120) + 8
            nrows = hi - lo
            S.dma('sp', qb[i2][:], qT_d[:, 512 * b:512 * b + 512].rearrange("(c p) t -> p c t", p=128), reads=['qT_d'], writes=[('qb', i2)])
            S.dma('sp', kw[i2][:, :, 0:64 * nrows], kT_d[:, 256 + 64 * lo:256 + 64 * hi].rearrange("(c p) t -> p c t", p=128),
                  reads=['kT_d'], writes=[('kw', i2)])
            for s in range(nrows - 1):
                S.dma('sp', vw[i2][:, s], v_d[256 + 64 * (lo + s):256 + 64 * (lo + s) + 128], reads=['v_d'], writes=[('vw', i2)])
            for rr in range(8):
                r = r0 + rr
                krs = min(max(r - 4, 0), 120)
                a0 = krs - r + 7
                half = 64 * (r % 2)
                pp = (r // 2) % 2
                for h in range(8):
                    hc, hb = h // 2, 64 * (h % 2)
                    it = item % 3
                    ip = item % 4
                    item += 1
                    pst = pS[it]; psk = ('pS', it)
                    for c in range(4):
                        kc0 = 64 * (krs + 2 * c - lo)
                        S.op('pe', lambda e: e.matmul(pst[:, c, :], lhsT=kw[i2][hb:hb + 64, hc, kc0:kc0 + 128],
                                                      rhs=qb[i2][hb:hb + 64, hc, 64 * rr:64 * rr + 64], start=True, stop=True),
                             reads=[('kw', i2), ('qb', i2)], writes=[psk])
                    for c in range(2):
                        S.op('pe', lambda e: e.matmul(pst[:, 4 + c, :], lhsT=kctx[hb:hb + 64, hc, 128 * c:128 * c + 128],
                                                      rhs=qb[i2][hb:hb + 64, hc, 64 * rr:64 * rr + 64], start=True, stop=True),
                             reads=['kctx', ('qb', i2)], writes=[psk])
                    S.op('dve', lambda e: e.tensor_tensor(out=sS[it][:], in0=pst[:, 0:4, :], in1=Tb[:, h, a0:a0 + 7:2, :], op=ALU.add),
                         reads=[psk, 'Tb'], writes=[('sS', it)])
                    S.op('act', lambda e: e.activation(out=P[ip][:, 0:4, :], in_=sS[it][:], func=AF.Exp), reads=[('sS', it)], writes=[('P', ip)])
                    S.op('act', lambda e: e.activation(out=P[ip][:, 4:6, :], in_=pst[:, 4:6, :], func=AF.Exp), reads=[psk], writes=[('P', ip)])
                    if len(pend) == 2:
                        pend.pop(0)()
                    def mk_pv(it=ip, i2=i2, krs=krs, lo=lo, h=h, half=half, pp=pp, r=r, rr=rr, b=b):
                        def pv():
                            pot = pO[pp * 2 + h // 4]; pok = ('pO', pp * 2 + h // 4)
                            for c in range(6):
                                if c < 4:
                                    rhs = vw[i2][:, krs + 2 * c - lo, h, :]
                                    rk = ('vw', i2)
                                else:
                                    rhs = vctx[:, c - 4, h, :]
                                    rk = 'vctx'
                                S.op('pe', lambda e: e.matmul(pot[half:half + 64, h % 4, :], lhsT=P[it][:, c, :], rhs=rhs,
                                                              start=(c == 0), stop=(c == 5)), reads=[('P', it), rk], writes=[pok])
                            if h % 4 == 3:
                                hh = h // 4
                                yk = ('ybt', pp)
                                S.op('dve', lambda e: e.reciprocal(out=rc[pp][half:half + 64, 4 * hh:4 * hh + 4], in_=pot[half:half + 64, :, 64]),
                                     reads=[pok], writes=[('rc', pp)])
                                S.op('dve', lambda e: e.tensor_tensor(out=ybt[pp][half:half + 64, 4 * hh:4 * hh + 4, :], in0=pot[half:half + 64, :, 0:64],
                                                                      in1=rc[pp][half:half + 64, 4 * hh:4 * hh + 4].unsqueeze(2).to_broadcast([64, 4, 64]),
                                                                      op=ALU.mult), reads=[pok, ('rc', pp)], writes=[yk])
                            if h == 7 and r % 2 == 1:
                                tk = ('ybT', b % 2)
                                for c4 in range(4):
                                    S.op('pe', lambda e: e.transpose(out=pT[:, c4, :], in_=ybt[pp][:, 2 * c4:2 * c4 + 2, :].rearrange("p a d -> p (a d)"),
                                                                     identity=identb[:]), reads=[('ybt', pp), 'identb'], writes=['pTn'])
                                S.op('act', lambda e: e.activation(out=ybT[b % 2][:, :, 64 * (rr - 1):64 * (rr - 1) + 128], in_=pT[:], func=AF.Copy),
                                     reads=['pTn'], writes=[tk])
                                if rr == 7:
                                    S.dma('act', ybT_d[:, 512 * b:512 * b + 512].rearrange("(c p) t -> p c t", p=128), ybT[b % 2][:],
                                          reads=[tk], writes=['ybT_d'])
                        return pv
                    pend.append(mk_pv())
        for p_ in pend:
            p_()
        S.barrier()
    if 'na' in dbg:
        dbg_out['ybT'] = dout("dbg_ybT", [512, 8192], BF16)
        for r0 in range(0, 512, 128):
            S.dma('sp', dbg_out['ybT'][r0:r0 + 128, :], ybT_d[r0:r0 + 128, :], reads=['ybT_d'])


def na_host(inp):
    rpb = inp['na_rpb'][0].astype(np.float32)
    i = np.arange(2)[:, None, None, None]
    kc = np.arange(64)[None, :, None, None]
    a = np.arange(14)[None, None, :, None]
    qc = np.arange(64)[None, None, None, :]
    dcol = np.clip(kc - qc + 15, 0, 30)
    drow = a + i
    T = rpb[:, drow, dcol]
    T = np.broadcast_to(T, (8, 2, 64, 14, 64)).transpose(1, 2, 0, 3, 4).reshape(128, 8 * 14 * 64)
    q = np.arange(64)
    qcs = np.clip(q - 8, 0, 48)
    k = np.arange(64)[:, None]
    inw = (k >= qcs[None, :]) & (k < qcs[None, :] + 16)
    m = np.where(inw, 0.0, NEG).astype(np.float32)
    m = np.concatenate([m, m], axis=0)
    return {'rpbT': np.ascontiguousarray(T.astype(np.float32)), 'maskT': np.ascontiguousarray(m)}

D = 1024


def merge_phase(nc, S, es, sb, ps, din, dscr, dout, dbg, dbg_out, identf, identb, epsc, bc_g1, bc_mul2, bc_add2,
                xcat, out, yaT_d, ybT_d, sgaT_d, sgbT_d, h2_d, aff_d, affT_d, nblocks=16):
    wpa_in = din("w_proj_a", [512, D]); wpb_in = din("w_proj_b", [512, D]); wout_in = din("w_out", [D, D])
    wr_in = din("w_routerT", [128, 8, 16])
    with ExitStack() as pa:
        wpa = sb("g_wpa", [128, 4, D], BF16, pa); wpb = sb("g_wpb", [128, 4, D], BF16, pa)
        wout = sb("g_wout", [128, 8, D], BF16, pa)
        wr = sb("g_wr", [128, 8, 16], BF16, pa)
        ya = [sb("g_mya%d" % i, [128, 4, 512], BF16, pa) for i in range(2)]
        yb = [sb("g_myb%d" % i, [128, 4, 512], BF16, pa) for i in range(2)]
        sga = [sb("g_msga%d" % i, [128, 8, 512], BF16, pa) for i in range(2)]
        sgb = [sb("g_msgb%d" % i, [128, 8, 512], BF16, pa) for i in range(2)]
        mT = [sb("g_mT%d" % i, [128, 8, 512], BF16, pa) for i in range(2)]
        t1 = [sb("g_mt1_%d" % i, [128, 512], F32, pa) for i in range(2)]
        t2 = [sb("g_mt2_%d" % i, [128, 512], F32, pa) for i in range(2)]
        xt = [sb("g_mxt%d" % i, [128, D], F32, pa) for i in range(2)]
        x1 = [sb("g_mx1_%d" % i, [128, D], F32, pa) for i in range(3)]
        h2 = [sb("g_mh2_%d" % i, [128, D], F32, pa) for i in range(3)]
        h2b = [sb("g_mh2b_%d" % i, [128, D], BF16, pa) for i in range(3)]
        h2T = [sb("g_mh2T_%d" % i, [128, 8, 128], BF16, pa) for i in range(3)]
        junk = sb("g_mjunk", [128, D], BF16, pa)
        st = [sb("g_mst%d" % i, [128, 8], F32, pa) for i in range(3)]
        lg = [sb("g_mlg%d" % i, [128, 16], F32, pa) for i in range(3)]
        af = [sb("g_maf%d" % i, [128, 16], F32, pa) for i in range(3)]
        A16 = sb("g_A16", [16, 8192], F32, pa)
        pab = [ps("g_mpab%d" % i, [128, 512], F32, pa) for i in range(4)]
        pyo = ps("g_mpyo", [128, D], F32, pa)
        phT = ps("g_mphT", [128, 8, 128], BF16, pa)
        pmisc = ps("g_mpmisc", [128, 256], F32, pa)
        plg = pmisc[:, 0:16]
        paT = pmisc[0:16, 128:256]

        for q in range(4):
            S.dma('pool', wpa[:, q, :], wpa_in[q * 128:(q + 1) * 128, :], writes=['wpa'])
            S.dma('pool', wpb[:, q, :], wpb_in[q * 128:(q + 1) * 128, :], writes=['wpb'])
        for dc in range(8):
            S.dma('pool', wout[:, dc, :], wout_in[dc * 128:(dc + 1) * 128, :], writes=['wout'])
        S.dma('pool', wr[:], wr_in, writes=['wr'])
        tile_ctr = 0
        pend = []
        for b in range(nblocks):
            i2 = b % 2
            cs = slice(512 * b, 512 * b + 512)
            S.dma('sp', ya[i2][:], yaT_d[:, cs].rearrange("(c p) t -> p c t", p=128), reads=['yaT_d'], writes=[('ya', i2)])
            S.dma('sp', yb[i2][:], ybT_d[:, cs].rearrange("(c p) t -> p c t", p=128), reads=['ybT_d'], writes=[('yb', i2)])
            S.dma('sp', sga[i2][:], sgaT_d[:, cs].rearrange("(c p) t -> p c t", p=128), reads=['sgaT_d'], writes=[('sga', i2)])
            S.dma('sp', sgb[i2][:], sgbT_d[:, cs].rearrange("(c p) t -> p c t", p=128), reads=['sgbT_d'], writes=[('sgb', i2)])
            for dc in range(8):
                j2 = dc % 2
                pA_, pB_ = pab[2 * j2], pab[2 * j2 + 1]
                for q in range(4):
                    S.op('pe', lambda e: e.matmul(pA_[:], lhsT=wpa[:, q, dc * 128:(dc + 1) * 128], rhs=ya[i2][:, q, :],
                                                  start=(q == 0), stop=(q == 3)), reads=['wpa', ('ya', i2)], writes=[('pA', j2)])
                for q in range(4):
                    S.op('pe', lambda e: e.matmul(pB_[:], lhsT=wpb[:, q, dc * 128:(dc + 1) * 128], rhs=yb[i2][:, q, :],
                                                  start=(q == 0), stop=(q == 3)), reads=['wpb', ('yb', i2)], writes=[('pB', j2)])
                S.op('dve', lambda e: e.tensor_tensor(out=t1[j2][:], in0=pA_[:], in1=sga[i2][:, dc, :], op=ALU.mult),
                     reads=[('pA', j2), ('sga', i2)], writes=[('t1', j2)])
                S.op('dve', lambda e: e.tensor_tensor(out=t2[j2][:], in0=pB_[:], in1=sgb[i2][:, dc, :], op=ALU.mult),
                     reads=[('pB', j2), ('sgb', i2)], writes=[('t2', j2)])
                S.op('pool', lambda e: e.tensor_tensor(out=mT[i2][:, dc, :], in0=t1[j2][:], in1=t2[j2][:], op=ALU.add),
                     reads=[('t1', j2), ('t2', j2)], writes=[('mT', i2)])
            for ti in range(4):
                k2 = tile_ctr % 3
                kx = tile_ctr % 2
                tile_ctr += 1
                tok0 = 512 * b + 128 * ti
                S.dma('sp', xt[kx][:], xcat[256 + tok0:256 + tok0 + 128, :], writes=[('xt', kx)])
                for hf in range(2):
                    for dc in range(8):
                        S.op('pe', lambda e: e.matmul(pyo[:, hf * 512:(hf + 1) * 512], lhsT=mT[i2][:, dc, ti * 128:(ti + 1) * 128],
                                                      rhs=wout[:, dc, hf * 512:(hf + 1) * 512], start=(dc == 0), stop=(dc == 7)),
                             reads=[('mT', i2), 'wout'], writes=['pyo'])
                S.op('dve', lambda e: e.tensor_tensor(out=x1[k2][:], in0=pyo[:], in1=bc_g1[:], op=ALU.mult),
                     reads=['pyo', 'bc_g1'], writes=[('x1', k2)])
                S.op('dve', lambda e: e.tensor_tensor(out=x1[k2][:], in0=x1[k2][:], in1=xt[kx][:], op=ALU.add),
                     reads=[('x1', k2), ('xt', kx)], writes=[('x1', k2)])
                S.dma('pool', out[tok0:tok0 + 128, :], x1[k2][:], reads=[('x1', k2)], writes=['out'])
                s_ = st[k2]; sk = ('st', k2)
                S.op('act', lambda e: e.activation(out=junk[:], in_=x1[k2][:], func=AF.Square, accum_out=s_[:, 0:1]),
                     reads=[('x1', k2)], writes=['mjunk', sk])
                S.op('act', lambda e: e.activation(out=s_[:, 6:7], in_=s_[:, 0:1], func=AF.Ln, scale=1.0 / D, bias=epsc[:, 0:1]),
                     reads=[sk, 'epsc'], writes=[sk])
                S.op('act', lambda e: e.activation(out=s_[:, 1:2], in_=s_[:, 6:7], func=AF.Exp, scale=-0.5), reads=[sk], writes=[sk])
                S.op('dve', lambda e: e.scalar_tensor_tensor(out=h2[k2][:], in0=x1[k2][:], scalar=s_[:, 1:2], in1=bc_mul2[:],
                                                             op0=ALU.mult, op1=ALU.mult), reads=[('x1', k2), sk, 'bc_mul2'], writes=[('h2', k2)])
                S.op('dve', lambda e: e.tensor_tensor(out=h2[k2][:], in0=h2[k2][:], in1=bc_add2[:], op=ALU.add),
                     reads=[('h2', k2), 'bc_add2'], writes=[('h2', k2)])
                S.op('act', lambda e: e.activation(out=h2b[k2][:], in_=h2[k2][:], func=AF.Copy), reads=[('h2', k2)], writes=[('h2b', k2)])
                S.dma('act', h2_d[tok0:tok0 + 128, :], h2b[k2][:], reads=[('h2b', k2)], writes=['h2_d'])
                def mk_tail(k2=k2, tok0=tok0, s_=s_, sk=sk):
                    def tail():
                        for dc in range(8):
                            S.op('pe', lambda e: e.transpose(out=phT[:, dc, :], in_=h2b[k2][:, dc * 128:(dc + 1) * 128], identity=identb[:]),
                                 reads=[('h2b', k2), 'identb'], writes=['phT'])
                        S.op('act', lambda e: e.activation(out=h2T[k2][:], in_=phT[:], func=AF.Copy), reads=['phT'], writes=[('h2T', k2)])
                        for dc in range(8):
                            S.op('pe', lambda e: e.matmul(plg, lhsT=h2T[k2][:, dc, :], rhs=wr[:, dc, :], start=(dc == 0), stop=(dc == 7)),
                                 reads=[('h2T', k2), 'wr'], writes=['plg'])
                        S.op('dve', lambda e: e.tensor_copy(out=lg[k2][:], in_=plg), reads=['plg'], writes=[('lg', k2)])
                        S.op('dve', lambda e: e.reduce_max(out=s_[:, 2:3], in_=lg[k2][:], axis=AX.X), reads=[('lg', k2)], writes=[sk])
                        S.op('dve', lambda e: e.tensor_scalar(out=s_[:, 3:4], in0=s_[:, 2:3], scalar1=-1.0, scalar2=None, op0=ALU.mult),
                             reads=[sk], writes=[sk])
                        S.op('act', lambda e: e.activation(out=af[k2][:], in_=lg[k2][:], func=AF.Exp, bias=s_[:, 3:4], accum_out=s_[:, 4:5]),
                             reads=[('lg', k2), sk], writes=[('af', k2), sk])
                        S.op('dve', lambda e: e.reciprocal(out=s_[:, 5:6], in_=s_[:, 4:5]), reads=[sk], writes=[sk])
                        S.op('dve', lambda e: e.tensor_scalar(out=af[k2][:], in0=af[k2][:], scalar1=s_[:, 5:6], scalar2=None, op0=ALU.mult),
                             reads=[('af', k2), sk], writes=[('af', k2)])
                        S.dma('pool', aff_d[tok0:tok0 + 128, :], af[k2][:], reads=[('af', k2)], writes=['aff_d'])
                        S.op('pe', lambda e: e.transpose(out=paT, in_=af[k2][:], identity=identf[:]), reads=[('af', k2), 'identf'], writes=['paT'])
                        S.op('act', lambda e: e.activation(out=A16[:, tok0:tok0 + 128], in_=paT, func=AF.Copy), reads=['paT'], writes=['A16'])
                    return tail
                if len(pend) == 2:
                    pend.pop(0)()
                pend.append(mk_tail())
        for t_ in pend:
            t_()
        S.dma('sp', affT_d, A16[:], reads=['A16'], writes=['affT_d'])
        S.barrier()
        if 'mg2' in dbg:
            for nm, t, shp, dt in (('bcg1', bc_g1, [128, D], F32), ('mT0', mT[(nblocks - 1) % 2], [128, 8, 512], BF16), ('t1', t1[1], [128, 512], F32),
                                   ('sga0', sga[(nblocks - 1) % 2], [128, 8, 512], BF16), ('ya0', ya[(nblocks - 1) % 2], [128, 4, 512], BF16),
                                   ('yb0', yb[(nblocks - 1) % 2], [128, 4, 512], BF16), ('wpa', wpa, [128, 4, D], BF16), ('x1t', x1[1], [128, D], F32),
                                   ('xt', xt[1], [128, D], F32)):
                dbg_out[nm] = dout("dbg_" + nm, shp, dt)
                S.dma('sp', dbg_out[nm], t[:], reads=[])
    if 'mg' in dbg:
        dbg_out['aff'] = dout("dbg_aff", [8192, 16])
        S.dma('sp', dbg_out['aff'], aff_d, reads=['aff_d'])
        dbg_out['affT'] = dout("dbg_affT", [16, 8192])
        S.dma('sp', dbg_out['affT'], affT_d, reads=['affT_d'])
        dbg_out['h2'] = dout("dbg_h2", [8192, D], BF16)
        for r0 in range(0, 8192, 1024):
            S.dma('sp', dbg_out['h2'][r0:r0 + 1024, :], h2_d[r0:r0 + 1024, :], reads=['h2_d'])

D = 1024
CAP = 1024
NT = 8192


def moe_phase(nc, S, es, sb, ps, din, dscr, dout, dbg, dbg_out, identf, identb, bc_g2, out, h2_d, aff_d, affT_d, nexp=16):
    wg_in = din("w_e_gate", [16, D, D]); wu_in = din("w_e_up", [16, D, D]); wd_in = din("w_e_down", [16, D, D])
    m16_in = din("m16", [128, 128])
    with ExitStack() as pm:
        cum_d = dscr("cum_d", [16, NT], F32)
        slotv = sb("e_slotv", [128, 8], F32, pm)
        thr_dbg = sb("e_thr", [128, 1], F32, pm)
        S.op('pool', lambda e: e.iota(slotv[:], pattern=[[128, 8]], base=0, channel_multiplier=1, allow_small_or_imprecise_dtypes=True),
             writes=['slotv'])
        with ExitStack() as pa:
            A128 = sb("e_A128", [128, 1024], F32, pa)
            A16 = sb("e_A16", [16, NT], F32, pa)
            C16 = sb("e_C16", [16, NT], F32, pa)
            M16 = sb("e_M16", [128, 128], F32, pa)
            junk = sb("e_junk", [128, 1024], BF16, pa)
            lo = sb("e_lo", [128, 1], F32, pa); mid = sb("e_mid", [128, 1], F32, pa)
            cnt = sb("e_cnt", [128, 1], F32, pa); g = sb("e_g", [128, 1], F32, pa)
            one16 = sb("e_one16", [16, 1], F32, pa)
            ptot = ps("e_ptot", [128, 1], F32, pa)
            for s in range(8):
                S.dma('sp', A128[16 * s:16 * s + 16, :], affT_d[:, 1024 * s:1024 * s + 1024], reads=['affT_d'], writes=['A128'])
            S.dma('sp', A16[:], affT_d, reads=['affT_d'], writes=['A16'])
            S.dma('sp', M16[:], m16_in, writes=['M16'])
            S.op('dve', lambda e: e.memset(lo[:], 0.0), writes=['lo'])
            S.op('dve', lambda e: e.memset(one16[:], 1.0), writes=['one16'])
            for k in range(30):
                hk = 2.0 ** -(k + 1)
                S.op('dve', lambda e: e.tensor_scalar(out=mid[:], in0=lo[:], scalar1=hk, scalar2=None, op0=ALU.add), reads=['lo'], writes=['mid'])
                S.op('dve', lambda e: e.tensor_scalar(out=junk[:], in0=A128[:], scalar1=mid[:, 0:1], scalar2=0.0, op0=ALU.is_gt, op1=ALU.add,
                                                      accum_out=cnt[:]), reads=['A128', 'mid'], writes=['junk', 'cnt'])
                S.op('pe', lambda e: e.matmul(ptot[:], lhsT=M16[:], rhs=cnt[:], start=True, stop=True), reads=['M16', 'cnt'], writes=['ptot'])
                S.op('dve', lambda e: e.tensor_scalar(out=g[:], in0=ptot[:], scalar1=float(CAP), scalar2=hk, op0=ALU.is_ge, op1=ALU.mult),
                     reads=['ptot'], writes=['g'])
                S.op('dve', lambda e: e.tensor_tensor(out=lo[:], in0=lo[:], in1=g[:], op=ALU.add), reads=['lo', 'g'], writes=['lo'])
            S.op('dve', lambda e: e.tensor_copy(out=thr_dbg[:], in_=lo[:]), reads=['lo'], writes=['thr'])
            S.op('dve', lambda e: e.tensor_scalar(out=A16[:], in0=A16[:], scalar1=lo[0:16, 0:1], scalar2=None, op0=ALU.is_gt),
                 reads=['A16', 'lo'], writes=['A16'])
            S.op('dve', lambda e: e.tensor_tensor_scan(out=C16[:], data0=one16[:, 0:1].to_broadcast([16, NT]), data1=A16[:], initial=0.0,
                                                       op0=ALU.mult, op1=ALU.add), reads=['A16', 'one16'], writes=['C16'])
            S.dma('sp', cum_d, C16[:], reads=['C16'], writes=['cum_d'])
            S.barrier()
        if 'moe_thr' in dbg:
            dbg_out['thr'] = dout("dbg_thr", [128, 1])
            S.dma('sp', dbg_out['thr'], thr_dbg[:], reads=['thr'])
        with ExitStack() as pe_:
            Wg = [sb("e_Wg%d" % i, [128, 8, D], BF16, pe_) for i in range(2)]
            Wu = [sb("e_Wu%d" % i, [128, 8, D], BF16, pe_) for i in range(2)]
            Wd = [sb("e_Wd%d" % i, [128, 8, D], BF16, pe_) for i in range(2)]
            xe = [sb("e_xe%d" % i, [128, D], BF16, pe_) for i in range(8)]
            ag = [[sb("e_ag%d_%d" % (p_, i), [128, 16], F32, pe_) for i in range(8)] for p_ in range(2)]
            cq = [sb("e_cq%d" % i, [128, 2048], F32, pe_) for i in range(2)]
            junkq = sb("e_junkq", [128, 2048], BF16, pe_)
            acc = [sb("e_acc%d" % i, [128, 4, 8], F32, pe_) for i in range(3)]
            idxs = sb("e_idxs", [128, 8], F32, pe_)
            idxi = [sb("e_idxi%d" % i, [128, 8], I32, pe_) for i in range(3)]
            xeT = sb("e_xeT", [128, 8, CAP], BF16, pe_)
            hidT = sb("e_hidT", [128, 8, CAP], BF16, pe_)
            sg = [sb("e_sg%d" % i, [128, 512], F32, pe_) for i in range(2)]
            ye = [sb("e_ye%d" % i, [128, D], F32, pe_) for i in range(2)]
            pGU = [ps("e_pGU%d" % i, [128, 512], F32, pe_) for i in range(3)]
            pY = ps("e_pY", [128, D], F32, pe_)
            pT = ps("e_pT", [128, 8, 128], BF16, pe_)
            pTi = pT[:].bitcast(F32) if False else None

            def load_w(e):
                i2 = e % 2
                for dc in range(8):
                    S.dma('pool', Wg[i2][:, dc, :], wg_in[e, dc * 128:(dc + 1) * 128, :], writes=[('Wg', i2)])
                    S.dma('pool', Wu[i2][:, dc, :], wu_in[e, dc * 128:(dc + 1) * 128, :], writes=[('Wu', i2)])
                for dc in range(8):
                    S.dma('pool', Wd[i2][:, dc, :], wd_in[e, dc * 128:(dc + 1) * 128, :], writes=[('Wd', i2)])

            def load_cq(n):
                e_, qd = n // 4, n % 4
                if e_ >= nexp:
                    return
                S.dma('sp', cq[n % 2][:], cum_d[e_:e_ + 1, qd * 2048:(qd + 1) * 2048].to_broadcast([128, 2048]),
                      reads=['cum_d'], writes=[('cq', n % 2)])

            def idx_piece(e, piece):
                j3 = e % 3
                n = e * 4 + piece // 2
                if piece % 2 == 0:
                    if n == 0:
                        load_cq(0)
                    load_cq(n + 1)
                for sc in range(4 * (piece % 2), 4 * (piece % 2) + 4):
                    S.op('dve', lambda e_: e_.tensor_scalar(out=junkq[:], in0=cq[n % 2][:], scalar1=slotv[:, sc:sc + 1], scalar2=0.0,
                                                            op0=ALU.is_le, op1=ALU.add, accum_out=acc[j3][:, piece // 2, sc:sc + 1]),
                         reads=[('cq', n % 2), 'slotv'], writes=['junkq', ('acc', j3)])
                if piece == 7:
                    S.op('dve', lambda e_: e_.tensor_tensor(out=idxs[:], in0=acc[j3][:, 0, :], in1=acc[j3][:, 1, :], op=ALU.add),
                         reads=[('acc', j3)], writes=['idxs'])
                    S.op('dve', lambda e_: e_.tensor_tensor(out=idxs[:], in0=idxs[:], in1=acc[j3][:, 2, :], op=ALU.add),
                         reads=[('acc', j3), 'idxs'], writes=['idxs'])
                    S.op('dve', lambda e_: e_.tensor_tensor(out=idxs[:], in0=idxs[:], in1=acc[j3][:, 3, :], op=ALU.add),
                         reads=[('acc', j3), 'idxs'], writes=['idxs'])
                    S.op('dve', lambda e_: e_.tensor_copy(out=idxi[j3][:], in_=idxs[:]), reads=['idxs'], writes=[('idxi', j3)])

            def gather(e):
                j2 = e % 3
                for sc in range(8):
                    S.custom_dma('pool', lambda e_: e_.indirect_dma_start(out=xe[sc][:, :], out_offset=None, in_=h2_d[:, :],
                                 in_offset=bass.IndirectOffsetOnAxis(ap=idxi[j2][:, sc:sc + 1], axis=0)),
                                 reads=[('idxi', j2), 'h2_d'], writes=[('xe', sc)])
                    S.custom_dma('pool', lambda e_: e_.indirect_dma_start(out=ag[e % 2][sc][:, :], out_offset=None, in_=aff_d[:, :],
                                 in_offset=bass.IndirectOffsetOnAxis(ap=idxi[j2][:, sc:sc + 1], axis=0)),
                                 reads=[('idxi', j2), 'aff_d'], writes=[('ag', e % 2, sc)])

            gu_ctr = [0]

            def xpose(e):
                for sc in range(8):
                    for dc in range(8):
                        S.op('pe', lambda e_: e_.transpose(out=pT[:, dc, :], in_=xe[sc][:, dc * 128:(dc + 1) * 128], identity=identb[:]),
                             reads=[('xe', sc), 'identb'], writes=['pT'])
                    S.op('act', lambda e_: e_.activation(out=xeT[:, :, sc * 128:(sc + 1) * 128], in_=pT[:], func=AF.Copy),
                         reads=['pT'], writes=['xeT'])
            def ffn(e, nxt):
                i2 = e % 2; j2 = e % 3
                for fc in range(8):
                    for hf in range(2):
                        a = gu_ctr[0] % 3; gu_ctr[0] += 1
                        b = gu_ctr[0] % 3; gu_ctr[0] += 1
                        for dc in range(8):
                            S.op('pe', lambda e_: e_.matmul(pGU[a][:], lhsT=Wg[i2][:, dc, fc * 128:(fc + 1) * 128], rhs=xeT[:, dc, hf * 512:(hf + 1) * 512],
                                                            start=(dc == 0), stop=(dc == 7)), reads=[('Wg', i2), 'xeT'], writes=[('pGU', a)])
                        for dc in range(8):
                            S.op('pe', lambda e_: e_.matmul(pGU[b][:], lhsT=Wu[i2][:, dc, fc * 128:(fc + 1) * 128], rhs=xeT[:, dc, hf * 512:(hf + 1) * 512],
                                                            start=(dc == 0), stop=(dc == 7)), reads=[('Wu', i2), 'xeT'], writes=[('pGU', b)])
                        s2 = (fc * 2 + hf) % 2
                        S.op('act', lambda e_: e_.activation(out=sg[s2][:], in_=pGU[a][:], func=AF.Silu), reads=[('pGU', a)], writes=[('sg', s2)])
                        S.op('dve', lambda e_: e_.tensor_tensor(out=hidT[:, fc, hf * 512:(hf + 1) * 512], in0=pGU[b][:], in1=sg[s2][:], op=ALU.mult),
                             reads=[('pGU', b), ('sg', s2)], writes=['hidT'])
                    if nxt is not None:
                        idx_piece(nxt, fc)
                for sc in range(8):
                    for hf in range(2):
                        for fc in range(8):
                            S.op('pe', lambda e_: e_.matmul(pY[:, hf * 512:(hf + 1) * 512], lhsT=hidT[:, fc, sc * 128:(sc + 1) * 128],
                                                            rhs=Wd[i2][:, fc, hf * 512:(hf + 1) * 512], start=(fc == 0), stop=(fc == 7)),
                                 reads=['hidT', ('Wd', i2)], writes=['pY'])
                    y2 = sc % 2
                    S.op('dve', lambda e_: e_.scalar_tensor_tensor(out=ye[y2][:], in0=pY[:], scalar=ag[e % 2][sc][:, e:e + 1], in1=bc_g2[:],
                                                                   op0=ALU.mult, op1=ALU.mult), reads=['pY', ('ag', e % 2, sc), 'bc_g2'], writes=[('ye', y2)])
                    S.custom_dma('pool', lambda e_: e_.indirect_dma_start(out=out[:, :], out_offset=bass.IndirectOffsetOnAxis(ap=idxi[j2][:, sc:sc + 1], axis=0),
                                 in_=ye[y2][:, :], in_offset=None, compute_op=ALU.add), reads=[('ye', y2), ('idxi', j2)], writes=['out'])

            load_w(0)
            for p in range(8):
                idx_piece(0, p)
            gather(0)
            if nexp > 1:
                load_w(1)
                for p in range(8):
                    idx_piece(1, p)
            for e in range(nexp):
                xpose(e)
                if e + 1 < nexp:
                    gather(e + 1)
                    if e >= 1:
                        load_w(e + 1)
                ffn(e, e + 2 if e + 2 < nexp else None)
            if 'moe_idx' in dbg:
                dbg_out['idxi'] = dout("dbg_idxi", [128, 8], I32)
                S.dma('sp', dbg_out['idxi'], idxi[(nexp - 1) % 3][:], reads=[('idxi', (nexp - 1) % 3)])
            S.barrier()


def moe_host(inp):
    f = np.float32
    m16 = np.zeros((128, 128), f)
    for s in range(8):
        for s2 in range(8):
            m16[16 * s:16 * s + 16, 16 * s2:16 * s2 + 16] = np.eye(16, dtype=f)
    return {'w_e_gate': np.ascontiguousarray(inp['w_e_gate'][0]), 'w_e_up': np.ascontiguousarray(inp['w_e_up'][0]),
            'w_e_down': np.ascontiguousarray(inp['w_e_down'][0]), 'm16': m16}


D = 1024
NLAT = 8192
NCTX = 256
NEXT = NLAT + NCTX
NE2 = NEXT + NCTX
EPS = 1e-6
INCOLS = 4096


def build(stop_after=99, dbg=(), nblk_lim=17, skip_s5=False, mg_blocks=16, nexp=16):
    nc = bass.Bass("TRN2", target_bir_lowering=False)
    es = ExitStack()
    S = Sched(nc, es)
    dbg_out = {}

    def din(name, shape, dt=F32):
        return nc.dram_tensor(name, list(shape), dt, kind="ExternalInput").ap()

    def dscr(name, shape, dt):
        return nc.dram_tensor(name, list(shape), dt, kind="Internal").ap()

    def dout(name, shape, dt=F32):
        return nc.dram_tensor(name, list(shape), dt, kind="ExternalOutput").ap()

    def sb(name, shape, dt, stack=None):
        return (stack or es).enter_context(nc.sbuf_tensor(name, list(shape), dt))

    def ps(name, shape, dt, stack=None):
        return (stack or es).enter_context(nc.psum_tensor(name, list(shape), dt))

    xcat = din("xcat", [NEXT, D])
    cT = din("cT", [128, 8, 2])
    w_ada = din("w_ada", [D, 6 * D])
    b_ada = din("b_ada", [1, 6 * D])
    nmixT = din("nmixT", [128, 8])
    nffn = din("nffn", [1, D])
    w_in = din("w_in", [D, INCOLS])
    ident_in = din("ident", [128, 128])
    qkg = din("qkg", [128, 2])

    out = dout("out", [NLAT, D])

    uT_d = dscr("uT_d", [512, NE2], BF16)
    qT_d = dscr("qT_d", [512, NLAT], BF16)
    kT_d = dscr("kT_d", [512, NEXT], BF16)
    v_d = dscr("v_d", [NEXT, 8, 65], BF16)
    sgaT_d = dscr("sgaT_d", [D, NLAT], BF16)
    sgbT_d = dscr("sgbT_d", [D, NLAT], BF16)

    identf = sb("identf", [128, 128], F32)
    identb = sb("identb", [128, 128], BF16)
    mul1 = sb("mul1", [128, 8, 2], F32)
    add1 = sb("add1", [128, 8, 2], F32)
    bc_g1 = sb("bc_g1", [128, D], F32)
    bc_mul2 = sb("bc_mul2", [128, D], F32)
    bc_add2 = sb("bc_add2", [128, D], F32)
    bc_g2 = sb("bc_g2", [128, D], F32)

    epsc = sb("epsc", [128, 1], F32)
    S.op('dve', lambda e: e.memset(epsc[:], EPS), writes=['epsc'])
    S.dma('sp', identf[:], ident_in, writes=['identf'])
    S.dma('pool', identb[:], ident_in, writes=['identb'])

    with ExitStack() as p0:
        wada = sb("wada", [128, 8, 6 * D], BF16, p0)
        cs_f = sb("cs_f", [128, 8, 2], F32, p0)
        cs = sb("cs", [128, 8, 2], BF16, p0)
        brow = sb("brow", [2, 6 * D], F32, p0)
        modrow = sb("modrow", [2, 6 * D], F32, p0)
        sel = sb("sel", [2, 128], F32, p0)
        nmix = sb("nmix", [128, 8], F32, p0)
        colsb = sb("colsb", [128, 16, 2], F32, p0)
        pm = [ps("pm%d" % i, [2, 512], F32, p0) for i in range(2)]
        pcol = ps("pcol", [128, 16, 2], F32, p0)
        pbc = [ps("pbc%d" % i, [128, 512], F32, p0) for i in range(2)]

        S.dma('sp', cs_f[:], cT, writes=['cs_f'])
        S.dma('sp', brow[0:1, :], b_ada, writes=['brow0'])
        S.dma('sp', brow[1:2, :], b_ada, writes=['brow1'])
        S.dma('sp', nmix[:], nmixT, writes=['nmix'])
        S.dma('sp', bc_mul2[:], nffn.to_broadcast([128, D]), writes=['bc_mul2'])
        for dc in range(8):
            S.dma('pool', wada[:, dc, :].rearrange("p (a b) -> p a b", b=2048),
                  w_ada[dc * 128:(dc + 1) * 128, :].rearrange("p (a b) -> p a b", b=2048),
                  writes=[('wada', dc)])
        S.op('act', lambda e: e.activation(out=cs[:], in_=cs_f[:], func=AF.Silu), reads=['cs_f'], writes=['cs'])
        S.op('dve', lambda e: e.memset(sel[:], 0.0), writes=['sel'])
        S.op('dve', lambda e: e.memset(sel[0:1, :], 1.0), writes=['sel'])
        for cc in range(12):
            pt = pm[cc % 2]
            for dc in range(8):
                S.op('pe', lambda e: e.matmul(pt[:], lhsT=cs[:, dc, :], rhs=wada[:, dc, cc * 512:(cc + 1) * 512],
                                              start=(dc == 0), stop=(dc == 7)),
                     reads=['cs', ('wada', dc)], writes=[('pm', cc % 2)])
            S.op('dve', lambda e: e.tensor_tensor(out=modrow[:, cc * 512:(cc + 1) * 512], in0=pt[:],
                                                  in1=brow[:, cc * 512:(cc + 1) * 512], op=ALU.add),
                 reads=[('pm', cc % 2), 'brow0', 'brow1'], writes=[('modrow', cc)])
        for i in range(16):
            S.op('pe', lambda e: e.transpose(out=pcol[:, i, :], in_=modrow[0:2, i * 128:(i + 1) * 128],
                                             identity=identf[0:2, 0:2]),
                 reads=[('modrow', i // 4), 'identf'], writes=['pcol'])
        S.op('dve', lambda e: e.tensor_copy(out=colsb[:], in_=pcol[:]), reads=['pcol'], writes=['colsb'])
        S.op('dve', lambda e: e.tensor_copy(out=add1[:], in_=colsb[:, 0:8, :]), reads=['colsb'], writes=['add1'])
        S.op('dve', lambda e: e.tensor_scalar(out=colsb[:, 8:16, :], in0=colsb[:, 8:16, :], scalar1=1.0, scalar2=None,
                                              op0=ALU.add), reads=['colsb'], writes=['colsb'])
        S.op('dve', lambda e: e.tensor_tensor(out=mul1[:], in0=colsb[:, 8:16, :],
                                              in1=nmix[:].unsqueeze(2).to_broadcast([128, 8, 2]), op=ALU.mult),
             reads=['colsb', 'nmix'], writes=['mul1'])

        def bcast(dst, col0, mode, dkey):
            for h in range(2):
                pt = pbc[h]
                S.op('pe', lambda e: e.matmul(pt[:], lhsT=sel[:], rhs=modrow[:, col0 + h * 512: col0 + (h + 1) * 512],
                                              start=True, stop=True),
                     reads=['sel'] + [('modrow', c) for c in range(12)], writes=[('pbc', h)])
                dsl = dst[:, h * 512:(h + 1) * 512]
                if mode == 'copy':
                    S.op('dve', lambda e: e.tensor_copy(out=dsl, in_=pt[:]), reads=[('pbc', h)], writes=[dkey])
                else:
                    S.op('dve', lambda e: e.scalar_tensor_tensor(out=dsl, in0=pt[:], scalar=1.0, in1=dsl,
                                                                 op0=ALU.add, op1=ALU.mult),
                         reads=[('pbc', h), dkey], writes=[dkey])
        bcast(bc_g1, 2048, 'copy', 'bc_g1')
        bcast(bc_add2, 3072, 'copy', 'bc_add2')
        bcast(bc_mul2, 4096, 'mul1p', 'bc_mul2')
        bcast(bc_g2, 5120, 'copy', 'bc_g2')
        if 'mod' in dbg:
            dbg_out['mod'] = dout("dbg_mod", [2, 6 * D])
            S.dma('sp', dbg_out['mod'], modrow[:], reads=[('modrow', c) for c in range(12)])
            dbg_out['mul1'] = dout("dbg_mul1", [128, 8, 2])
            S.dma('sp', dbg_out['mul1'], mul1[:], reads=['mul1'])
            dbg_out['bcm2'] = dout("dbg_bcm2", [128, D])
            S.dma('sp', dbg_out['bcm2'], bc_mul2[:], reads=['bc_mul2'])
        S.barrier()

    if stop_after <= 0:
        return finish(nc, S, es, out, dbg_out)

    with ExitStack() as p1:
        win = sb("win", [128, 8, INCOLS], BF16, p1)
        xt = [sb("xt%d" % i, [128, D], F32, p1) for i in range(2)]
        junk = sb("junk", [128, D], BF16, p1)
        xn = [sb("xn%d" % i, [128, D], BF16, p1) for i in range(4)]
        ss = [sb("ss%d" % i, [128, 1], F32, p1) for i in range(4)]
        rstd = [sb("rstd%d" % i, [128, 1], F32, p1) for i in range(4)]
        hT = [sb("hT%d" % i, [128, 8, 512], BF16, p1) for i in range(2)]
        stg = [sb("stg%d" % i, [128, 512], BF16, p1) for i in range(4)]
        sq = [sb("sq%d" % i, [128, 512], BF16, p1) for i in range(2)]
        qk32 = [sb("qk32_%d" % i, [128, 512], F32, p1) for i in range(2)]
        rs = [sb("rs%d" % i, [128, 512], F32, p1) for i in range(2)]
        vst = [sb("vst%d" % i, [128, 8, 65], BF16, p1) for i in range(2)]
        bones = sb("bones", [128, 128], BF16, p1)
        gq = sb("gq", [128, 2], F32, p1)
        pT = [ps("pT%d" % i, [128, 8, 128], BF16, p1) for i in range(2)]
        pz = [ps("pz%d" % i, [128, 512], F32, p1) for i in range(4)]
        pms = [ps("pms%d" % i, [128, 512], F32, p1) for i in range(2)]

        for dc in range(8):
            S.dma('pool', win[:, dc, :].rearrange("p (a b) -> p a b", b=2048),
                  w_in[dc * 128:(dc + 1) * 128, :].rearrange("p (a b) -> p a b", b=2048),
                  writes=[('win', dc)])
        S.dma('sp', gq[:], qkg, writes=['gq'])
        S.op('dve', lambda e: e.tensor_scalar(out=gq[:, 0:1], in0=gq[:, 0:1], scalar1=0.125, scalar2=None, op0=ALU.mult), reads=['gq'], writes=['gq'])
        for i in range(2):
            S.op('dve', lambda e: e.memset(vst[i][:], 1.0), writes=[('vst', i)])
        S.op('dve', lambda e: e.memset(bones[:], 0.0), writes=['bones'])
        S.op('dve', lambda e: e.memset(bones[0:64, 0:64], 1.0 / 64), writes=['bones'])
        S.op('dve', lambda e: e.memset(bones[64:128, 64:128], 1.0 / 64), writes=['bones'])

        nblk = (NEXT + 511) // 512
        ctr = {'tile': 0, 'tpe': 0, 'stg': 0, 'z': 0, 'v': 0}

        def blk_info(b):
            if b == 0:
                return 0, 256
            return 256 + (b - 1) * 512, 512

        def head_pre(b):
            t0, nt = blk_info(b)
            for ti in range(nt // 128):
                i2 = ctr['tile'] % 2
                i4 = ctr['tile'] % 4
                ctr['tile'] += 1
                r0 = t0 + ti * 128
                S.dma('sp', xt[i2][:], xcat[r0:r0 + 128, :], writes=[('xt', i2)])
                S.op('act', lambda e: e.activation(out=junk[:], in_=xt[i2][:], func=AF.Square, accum_out=ss[i4][:]),
                     reads=[('xt', i2)], writes=['junk', ('ss', i4)])
                S.op('act', lambda e: e.activation(out=rstd[i4][:], in_=ss[i4][:], func=AF.Ln, scale=1.0 / D, bias=epsc[:, 0:1]),
                     reads=[('ss', i4), 'epsc'], writes=[('rstd', i4)])
                S.op('act', lambda e: e.activation(out=rstd[i4][:], in_=rstd[i4][:], func=AF.Exp, scale=-0.5),
                     reads=[('rstd', i4)], writes=[('rstd', i4)])
                S.op('act', lambda e: e.activation(out=xn[i4][:], in_=xt[i2][:], func=AF.Copy, scale=rstd[i4][:, 0:1]),
                     reads=[('xt', i2), ('rstd', i4)], writes=[('xn', i4)])

        def head_pe(b):
            t0, nt = blk_info(b)
            j = 1 if b == 0 else 0
            hb = hT[b % 2]
            hkey = ('hT', b % 2)
            for ti in range(nt // 128):
                i2 = ctr['tpe'] % 2
                i4 = ctr['tpe'] % 4
                ctr['tpe'] += 1
                for dc in range(8):
                    S.op('pe', lambda e: e.transpose(out=pT[i2][:, dc, :], in_=xn[i4][:, dc * 128:(dc + 1) * 128],
                                                     identity=identb[:]),
                         reads=[('xn', i4), 'identb'], writes=[('pT', i2)])
                for dc in range(8):
                    S.op('dve', lambda e: e.tensor_scalar(out=hb[:, dc, ti * 128:(ti + 1) * 128], in0=pT[i2][:, dc, :],
                                                          scalar1=mul1[:, dc, j:j + 1], scalar2=add1[:, dc, j:j + 1],
                                                          op0=ALU.mult, op1=ALU.add),
                         reads=[('pT', i2), 'mul1', 'add1'], writes=[hkey])

        pend_qk = []

        def chunk(b, cc):
            t0, nt = blk_info(b)
            is_ctx = (b == 0)
            hb = hT[b % 2]
            hkey = ('hT', b % 2)
            zi = ctr['z'] % 4
            ctr['z'] += 1
            pzt = pz[zi]
            for dc in range(8):
                S.op('pe', lambda e: e.matmul(pzt[:, 0:nt], lhsT=win[:, dc, cc * 128:(cc + 1) * 128], rhs=hb[:, dc, 0:nt],
                                              start=(dc == 0), stop=(dc == 7)),
                     reads=[('win', dc), hkey], writes=[('pz', zi)], sig=(dc == 7))
            si = ctr['stg'] % 4
            ctr['stg'] += 1
            st = stg[si]
            skey = ('stg', si)
            if not (4 <= cc < 12):
                while pend_qk:
                    pend_qk.pop(0)()
            if cc < 4:
                S.op('act', lambda e: e.activation(out=st[:, 0:nt], in_=pzt[:, 0:nt], func=AF.Copy),
                     reads=[('pz', zi)], writes=[skey])
                S.dma('sp', uT_d[cc * 128:(cc + 1) * 128, t0:t0 + nt], st[:, 0:nt], reads=[skey], writes=['uT_d'])
                if is_ctx:
                    S.dma('sp', uT_d[cc * 128:(cc + 1) * 128, NEXT:NEXT + nt], st[:, 0:nt], reads=[skey], writes=['uT_d'])
            elif cc < 12:
                isq = cc < 8
                qi = ctr['z'] % 2
                S.op('act', lambda e: e.activation(out=sq[qi][:, 0:nt], in_=pzt[:, 0:nt], func=AF.Square),
                     reads=[('pz', zi)], writes=[('sq', qi)])
                while pend_qk:
                    pend_qk.pop(0)()

                def tail(qi=qi, zi=zi, pzt=pzt, st=st, skey=skey, isq=isq, cc=cc, t0=t0, nt=nt):
                    S.op('pe', lambda e: e.matmul(pms[qi][:, 0:nt], lhsT=bones[:], rhs=sq[qi][:, 0:nt], start=True, stop=True),
                         reads=['bones', ('sq', qi)], writes=[('pms', qi)])
                    S.op('act', lambda e: e.activation(out=rs[qi][:, 0:nt], in_=pms[qi][:, 0:nt], func=AF.Ln, bias=epsc[:, 0:1]),
                         reads=[('pms', qi), 'epsc'], writes=[('rs', qi)])
                    S.op('act', lambda e: e.activation(out=rs[qi][:, 0:nt], in_=rs[qi][:, 0:nt], func=AF.Exp, scale=-0.5),
                         reads=[('rs', qi)], writes=[('rs', qi)])
                    gcol = gq[:, 0:1] if isq else gq[:, 1:2]
                    S.op('dve', lambda e: e.scalar_tensor_tensor(out=st[:, 0:nt], in0=pzt[:, 0:nt], scalar=gcol,
                                                                 in1=rs[qi][:, 0:nt], op0=ALU.mult, op1=ALU.mult),
                         reads=[('pz', zi), ('rs', qi), 'gq'], writes=[skey])
                    if isq:
                        S.dma('sp', qT_d[(cc - 4) * 128:(cc - 3) * 128, t0 - NCTX:t0 - NCTX + nt], st[:, 0:nt],
                              reads=[skey], writes=['qT_d'])
                    else:
                        S.dma('sp', kT_d[(cc - 8) * 128:(cc - 7) * 128, t0:t0 + nt], st[:, 0:nt], reads=[skey], writes=['kT_d'])
                pend_qk.append(tail)
            else:
                S.op('act', lambda e: e.activation(out=st[:, 0:nt], in_=pzt[:, 0:nt], func=AF.Sigmoid),
                     reads=[('pz', zi)], writes=[skey])
                if cc < 24:
                    dst = sgaT_d[(cc - 16) * 128:(cc - 15) * 128, t0 - NCTX:t0 - NCTX + nt]
                    dk = 'sgaT_d'
                else:
                    dst = sgbT_d[(cc - 24) * 128:(cc - 23) * 128, t0 - NCTX:t0 - NCTX + nt]
                    dk = 'sgbT_d'
                S.dma('sp', dst, st[:, 0:nt], reads=[skey], writes=[dk])

        def vpart(b):
            t0, nt = blk_info(b)
            hb = hT[b % 2]
            hkey = ('hT', b % 2)
            for ti in range(nt // 128):
                zi = ctr['z'] % 4
                ctr['z'] += 1
                pzt = pz[zi]
                for dc in range(8):
                    S.op('pe', lambda e: e.matmul(pzt[:], lhsT=hb[:, dc, ti * 128:(ti + 1) * 128], rhs=win[:, dc, 1536:2048],
                                                  start=(dc == 0), stop=(dc == 7)),
                         reads=[('win', dc), hkey], writes=[('pz', zi)], sig=(dc == 7))
                while pend_qk:
                    pend_qk.pop(0)()
                vi = ctr['v'] % 2
                ctr['v'] += 1
                S.op('act', lambda e: e.activation(out=vst[vi][:, :, 0:64], in_=pzt[:].rearrange("p (h d) -> p h d", d=64), func=AF.Copy),
                     reads=[('pz', zi)], writes=[('vst', vi)])
                S.dma('sp', v_d[t0 + ti * 128:t0 + (ti + 1) * 128], vst[vi][:], reads=[('vst', vi)], writes=['v_d'])

        head_pre(0)
        head_pe(0)
        for b in range(nblk_lim):
            if b == 0:
                ccs = list(range(0, 4)) + list(range(8, 12))
            else:
                ccs = list(range(0, 12)) + list(range(16, 32))
            n1, n2 = len(ccs) // 4, (3 * len(ccs)) // 4
            for cc in ccs[:n1]:
                chunk(b, cc)
            if b + 1 < nblk_lim:
                head_pre(b + 1)
            for cc in ccs[n1:n2]:
                chunk(b, cc)
            if b + 1 < nblk_lim:
                head_pe(b + 1)
            for cc in ccs[n2:]:
                chunk(b, cc)
            vpart(b)
        S.barrier()
    if 'p1' in dbg:
        for nm, t in (('uT', uT_d), ('qT', qT_d), ('kT', kT_d), ('sgaT', sgaT_d), ('sgbT', sgbT_d)):
            dbg_out[nm] = dout("dbg_" + nm, list(t.shape), BF16)
            for r0 in range(0, t.shape[0], 128):
                S.dma('sp', dbg_out[nm][r0:r0 + 128, :], t[r0:r0 + 128, :], reads=[])
    if stop_after <= 1:
        return finish(nc, S, es, out, dbg_out)
    yaT_d = dscr("yaT_d", [512, NLAT], BF16)
    r = 'ok' if skip_s5 else s5_phase(nc, S, es, sb, ps, din, dscr, dout, dbg, dbg_out, identf, identb, uT_d, yaT_d)
    if r == 'stop' or stop_after <= 2:
        return finish(nc, S, es, out, dbg_out)
    ybT_d = dscr("ybT_d", [512, NLAT], BF16)
    na_phase(nc, S, es, sb, ps, din, dscr, dout, dbg, dbg_out, identb, qT_d, kT_d, v_d, ybT_d)
    if stop_after <= 3:
        return finish(nc, S, es, out, dbg_out)
    h2_d = dscr("h2_d", [NLAT, D], BF16)
    aff_d = dscr("aff_d", [NLAT, 16], F32)
    affT_d = dscr("affT_d", [16, NLAT], F32)
    merge_phase(nc, S, es, sb, ps, din, dscr, dout, dbg, dbg_out, identf, identb, epsc, bc_g1, bc_mul2, bc_add2,
                xcat, out, yaT_d, ybT_d, sgaT_d, sgbT_d, h2_d, aff_d, affT_d, nblocks=mg_blocks)
    if stop_after <= 4:
        return finish(nc, S, es, out, dbg_out)
    moe_phase(nc, S, es, sb, ps, din, dscr, dout, dbg, dbg_out, identf, identb, bc_g2, out, h2_d, aff_d, affT_d, nexp=nexp)
    return finish(nc, S, es, out, dbg_out)


def finish(nc, S, es, out, dbg_out):
    S.barrier(['sp'])
    print("instructions", S.nins, "waits", S.nwaits)
    es.close()
    return nc, dbg_out


def host_inputs(inp, b):
    f = np.float32
    xcat = np.concatenate([inp['ctx'][b], inp['x'][b]], axis=0).astype(f)
    cT = np.stack([inp['c'][b].reshape(8, 128).T, inp['c_ctx'].reshape(8, 128).T], axis=-1).astype(f)
    qkg = np.stack([np.tile(inp['q_norm'][0], 2), np.tile(inp['k_norm'][0], 2)], axis=-1).astype(f)
    m = {
        'xcat': np.ascontiguousarray(xcat),
        'cT': np.ascontiguousarray(cT),
        'w_ada': np.ascontiguousarray(inp['w_ada'][0]),
        'b_ada': np.ascontiguousarray(inp['b_ada'][0][None, :]),
        'nmixT': np.ascontiguousarray(inp['norm_mix'][0].reshape(8, 128).T),
        'nffn': np.ascontiguousarray(inp['norm_ffn'][0][None, :]),
        'w_in': np.ascontiguousarray(inp['w_in'][0]),
        'ident': np.eye(128, dtype=f),
        'qkg': np.ascontiguousarray(qkg),
    }
    f32c = lambda a: np.ascontiguousarray(a.astype(f))
    lr = inp['ssm_lam_re'][0].reshape(2, 16, 2, 64)
    m['lamre_l'] = f32c(lr.transpose(2, 3, 0, 1).reshape(128, 32))
    li = inp['ssm_lam_im'][0].reshape(2, 16, 2, 64)
    m['lamim_l'] = f32c(li.transpose(2, 3, 0, 1).reshape(128, 32))
    ld = inp['ssm_log_dt'][0].reshape(2, 16, 2)
    m['logdt_l'] = f32c(np.broadcast_to(ld.transpose(2, 0, 1)[:, None, :, :], (2, 64, 2, 16)).reshape(128, 32))
    for nm, key in (('Bre_c', 'ssm_b_re'), ('Bim_c', 'ssm_b_im')):
        bb = inp[key][0].reshape(2, 16, 2, 64, 16)
        m[nm] = f32c(bb.transpose(2, 3, 0, 1, 4).reshape(128, 32, 16))
    for nm, key in (('Cre_c', 'ssm_c_re'), ('Cim_c', 'ssm_c_im')):
        cc = inp[key][0].reshape(2, 16, 2, 16, 64)
        m[nm] = f32c(cc.transpose(2, 4, 0, 1, 3).reshape(128, 32, 16))
    m['dskipT'] = f32c(inp['ssm_d'][0].reshape(4, 128).T)
    m['w_glu'] = f32c(inp['w_glu'][0])
    m['bgluT'] = f32c(inp['b_glu'][0].reshape(4, 128).T)
    m.update(na_host(inp))
    m['w_proj_a'] = f32c(inp['w_proj_a'][0]); m['w_proj_b'] = f32c(inp['w_proj_b'][0]); m['w_out'] = f32c(inp['w_out'][0])
    m.update(moe_host(inp))
    m['w_routerT'] = f32c(inp['w_router'][0].reshape(8, 128, 16).transpose(1, 0, 2))
    return m


_NC_CACHE = {}


def kernel(**inputs):
    inp = {k_: np.asarray(v_) for k_, v_ in inputs.items()}
    if 'nc' not in _NC_CACHE:
        nc, _ = build()
        _NC_CACHE['nc'] = nc
    nc = _NC_CACHE['nc']
    nb = inp['x'].shape[0]
    in_maps = [host_inputs(inp, b) for b in range(nb)]
    res = run_bass_kernel_spmd(nc, in_maps, core_ids=list(range(nb)))
    outs = [np.asarray(res.results[b]["out"], dtype=np.float32) for b in range(nb)]
    return np.stack(outs, axis=0)
```
